# Optimizing a Trainium2 kernel written in Bass

```python
import math
import jax, jax.numpy as jnp
from jax import lax
import numpy as np

D_MODEL = 1024
BATCH = 8
SEQ = 4096
DEPTH = 2

GRID_W = 64
CTX_LEN = 256
N_EVEN = (DEPTH + 1) // 2
N_ODD = DEPTH // 2
N_MOD = 6
EPS = 1e-6
CONV_WIDTH = 5
RWKV_HEADS = 8
RWKV_HEAD_DIM = 64
RWKV_WIDTH = RWKV_HEADS * RWKV_HEAD_DIM
RWKV_DECAY_RANK = 64
RWKV_A_RANK = 64
RWKV_GATE_RANK = 128
RWKV_GN_EPS = 64e-5
RWKV_PROJ = 3 * RWKV_WIDTH + 2 * RWKV_DECAY_RANK + 2 * RWKV_A_RANK + RWKV_GATE_RANK
HGRN_HEADS = 4
HGRN_DK = 128
HGRN_DV = 128
HGRN_KW = HGRN_HEADS * HGRN_DK
HGRN_VW = HGRN_HEADS * HGRN_DV
HGRN_CHUNK = 32
HGRN_PROJ = HGRN_KW + 2 * HGRN_KW + 2 * HGRN_VW
EVEN_PROJ = RWKV_PROJ + HGRN_PROJ
EVEN_OUT = RWKV_WIDTH + HGRN_VW
SSD_HEADS = 16
SSD_HEAD_DIM = 64
SSD_INNER = SSD_HEADS * SSD_HEAD_DIM
SSD_GROUPS = 2
SSD_STATE = 128
SSD_CHUNK = 64
SSD_CONV_CH = SSD_INNER + 2 * SSD_GROUPS * SSD_STATE
SSD_PROJ = SSD_INNER + SSD_CONV_CH + 2 * SSD_HEADS
MLSTM_HEADS = 4
MLSTM_DQK = 128
MLSTM_DV = 256
MLSTM_QK_W = MLSTM_HEADS * MLSTM_DQK
MLSTM_V_W = MLSTM_HEADS * MLSTM_DV
MLSTM_CHUNK = 64
MLSTM_M_INIT = -1e30
MLSTM_PROJ = 2 * MLSTM_QK_W + 2 * MLSTM_V_W + 4 * MLSTM_HEADS
ODD_PROJ = SSD_PROJ + MLSTM_PROJ
ODD_OUT = SSD_INNER + MLSTM_V_W
N_EXPERTS = 32
TOP_K = 4
D_FF_EXPERT = 1024
SWIGLU_LIMIT = 7.0
SWIGLU_ALPHA = 1.702
MOE_BLOCK = 256

kernel_name = 'bidir_hybrid_recurrent_moe_dit'

F32 = jnp.float32


def _split(t, sizes):
    return jnp.split(t, np.cumsum(sizes)[:-1].tolist(), axis=-1)


def rms_norm(x, gain):
    xf = x.astype(F32)
    y = xf * lax.rsqrt(jnp.mean(xf * xf, axis=-1, keepdims=True) + EPS)
    return (y * gain.astype(F32)).astype(x.dtype)


def head_layernorm(o, w, b, eps):
    mu = jnp.mean(o, axis=-1, keepdims=True)
    var = jnp.mean(jnp.square(o - mu), axis=-1, keepdims=True)
    return (o - mu) * lax.rsqrt(var + eps) * w + b


def centred_shift(p):
    pp = jnp.pad(p, ((0, 0), (1, 1), (0, 0)))
    return 0.5 * (pp[:, :-2] + pp[:, 2:])


def depthwise_conv_centred(u, w, b):
    ch = u.shape[-1]
    pad = w.shape[0] // 2
    y = lax.conv_general_dilated(u, w[:, None, :].astype(u.dtype), (1,), [(pad, pad)],
                                 dimension_numbers=('NWC', 'WIO', 'NWC'), feature_group_count=ch)
    return y + b


def to_col_major(t, rows):
    b, s, ch = t.shape
    return t.reshape(b, rows, GRID_W, ch).transpose(0, 2, 1, 3).reshape(b, s, ch)


def to_row_major(t, rows):
    b, s, ch = t.shape
    return t.reshape(b, GRID_W, rows, ch).transpose(0, 2, 1, 3).reshape(b, s, ch)


def dir_stack(tc, tl, per_dir):
    if per_dir:
        cf, cb, lf, lb = tc[:, :, 0], tc[:, :, 1], tl[:, :, 0], tl[:, :, 1]
    else:
        cf, cb, lf, lb = tc, tc, tl, tl
    fwd = jnp.concatenate([cf, lf], axis=1)
    bwd = jnp.concatenate([jnp.flip(cb, 1), jnp.flip(lb, 1)], axis=1)
    return jnp.stack([fwd, bwd], axis=0)


def dir_merge(y, lc, need_ctx):
    yl = y[0, :, lc:] + jnp.flip(y[1, :, lc:], 1)
    yc = (y[0, :, :lc] + jnp.flip(y[1, :, :lc], 1)) if need_ctx else None
    return yc, yl


def rwkv7_scan(r, w, k, v, kk, b):
    xs = tuple(jnp.moveaxis(t, 2, 0) for t in (r, w, k, v, kk, b))
    s0 = jnp.zeros(r.shape[:2] + r.shape[3:] + (r.shape[-1],), r.dtype)

    def step(S, inp):
        r_t, w_t, k_t, v_t, kk_t, b_t = inp
        sa = jnp.einsum('zbhvk,zbhk->zbhv', S, kk_t)
        S = S * w_t[..., None, :] - sa[..., :, None] * b_t[..., None, :] + v_t[..., :, None] * k_t[..., None, :]
        return S, jnp.einsum('zbhvk,zbhk->zbhv', S, r_t)

    _, o = lax.scan(step, s0, xs)
    return jnp.moveaxis(o, 0, 2)


def gla_chunked(q, k, v, log_f, chunk):
    z, L, h, dk = q.shape
    dv = v.shape[-1]
    n = L // chunk
    q, k, log_f = (t.reshape(z, n, chunk, h, dk) for t in (q, k, log_f))
    v = v.reshape(z, n, chunk, h, dv)
    b = jnp.cumsum(log_f, axis=2)
    q_in = q * jnp.exp(b)
    k_in = k * jnp.exp(-b)
    lower = jnp.tril(jnp.ones((chunk, chunk), bool))[:, :, None]
    A = jnp.where(lower, jnp.einsum('zcthd,zcshd->zctsh', q_in, k_in), 0.0)
    o_intra = jnp.einsum('zctsh,zcshv->zcthv', A, v)
    b_last = b[:, :, -1:]
    k_end = k * jnp.exp(b_last - b)
    chunk_decay = jnp.exp(b_last[:, :, 0])

    def step(S, inp):
        q_c, k_c, v_c, d_c = inp
        o_c = jnp.einsum('zthd,zhdv->zthv', q_c, S)
        S = d_c[..., None] * S + jnp.einsum('zshd,zshv->zhdv', k_c, v_c)
        return S, o_c

    s0 = jnp.zeros((z, h, dk, dv), q.dtype)
    _, o_inter = lax.scan(step, s0, tuple(jnp.moveaxis(t, 1, 0) for t in (q_in, k_end, v, chunk_decay)))
    return (o_intra + jnp.moveaxis(o_inter, 0, 1)).reshape(z, L, h, dv)


def ssd_chunked(x, log_a, Bm, Cm, chunk):
    z, L, g, hg, p = x.shape
    nst = Bm.shape[-1]
    n = L // chunk
    x = x.reshape(z, n, chunk, g, hg, p)
    log_a = log_a.reshape(z, n, chunk, g, hg)
    Bm = Bm.reshape(z, n, chunk, g, nst)
    Cm = Cm.reshape(z, n, chunk, g, nst)
    a_cum = jnp.cumsum(log_a, axis=2)
    lower = jnp.tril(jnp.ones((chunk, chunk), bool))[:, :, None, None]
    seg = a_cum[:, :, :, None] - a_cum[:, :, None, :]
    decay = jnp.exp(jnp.where(lower, seg, -jnp.inf))
    cb = jnp.einsum('zctgn,zcsgn->zctsg', Cm, Bm)
    y_intra = jnp.einsum('zctsg,zctsgh,zcsghp->zctghp', cb, decay, x)
    decay_end = jnp.exp(a_cum[:, :, -1:] - a_cum)
    decay_in = jnp.exp(a_cum)
    chunk_decay = jnp.exp(a_cum[:, :, -1])

    def step(hs, inp):
        c_c, b_c, x_c, din, dend, dch = inp
        y_c = jnp.einsum('ztgn,zghpn,ztgh->ztghp', c_c, hs, din)
        hs = dch[..., None, None] * hs + jnp.einsum('zsgn,zsgh,zsghp->zghpn', b_c, dend, x_c)
        return hs, y_c

    h0 = jnp.zeros((z, g, hg, p, nst), x.dtype)
    _, y_inter = lax.scan(step, h0, tuple(jnp.moveaxis(t, 1, 0) for t in (Cm, Bm, x, decay_in, decay_end, chunk_decay)))
    return (y_intra + jnp.moveaxis(y_inter, 0, 1)).reshape(z, L, g, hg, p)


def mlstm_chunked(q, k, v, log_i, log_f, chunk):
    z, L, h, dk = q.shape
    dv = v.shape[-1]
    n = L // chunk
    q = q.reshape(z, n, chunk, h, dk)
    k = k.reshape(z, n, chunk, h, dk)
    v = v.reshape(z, n, chunk, h, dv)
    log_i = log_i.reshape(z, n, chunk, h)
    F = jnp.cumsum(log_f.reshape(z, n, chunk, h), axis=2)
    F_last = F[:, :, -1]
    logw_end = F_last[:, :, None] - F + log_i
    m_end = jnp.max(logw_end, axis=2)
    w_end = jnp.exp(logw_end - m_end[:, :, None])
    lower = jnp.tril(jnp.ones((chunk, chunk), bool))[:, :, None]

    def step(carry, inp):
        Cs, ns, m = carry
        q_c, k_c, v_c, F_c, li_c, Fl_c, me_c, we_c = inp
        logw = jnp.where(lower, F_c[:, :, None] - F_c[:, None, :] + li_c[:, None, :], -jnp.inf)
        m_t = jnp.maximum(jnp.max(logw, axis=2), F_c + m[:, None])
        w_inter = jnp.exp(F_c + m[:, None] - m_t)
        scores = jnp.einsum('zthd,zshd->ztsh', q_c, k_c) * jnp.exp(logw - m_t[:, :, None])
        num = jnp.einsum('ztsh,zshv->zthv', scores, v_c) + w_inter[..., None] * jnp.einsum('zthd,zhdv->zthv', q_c, Cs)
        den = jnp.sum(scores, axis=2) + w_inter * jnp.einsum('zthd,zhd->zth', q_c, ns)
        h_c = num / jnp.maximum(jnp.abs(den), jnp.exp(-m_t))[..., None]
        m_new = jnp.maximum(Fl_c + m, me_c)
        s_old = jnp.exp(Fl_c + m - m_new)
        s_loc = jnp.exp(me_c - m_new)
        Cs = s_old[..., None, None] * Cs + s_loc[..., None, None] * jnp.einsum('zsh,zshd,zshv->zhdv', we_c, k_c, v_c)
        ns = s_old[..., None] * ns + s_loc[..., None] * jnp.einsum('zsh,zshd->zhd', we_c, k_c)
        return (Cs, ns, m_new), h_c

    init = (jnp.zeros((z, h, dk, dv), q.dtype), jnp.zeros((z, h, dk), q.dtype), jnp.full((z, h), MLSTM_M_INIT, q.dtype))
    _, hs = lax.scan(step, init, tuple(jnp.moveaxis(t, 1, 0) for t in (q, k, v, F, log_i, F_last, m_end, w_end)))
    return jnp.moveaxis(hs, 0, 1).reshape(z, L, h, dv)


def rwkv7_group(pc, pl, mu, w0, w2, a0, a2, g2, k_k, k_a, r_k, ln_w, ln_b, need_ctx):
    H, N = RWKV_HEADS, RWKV_HEAD_DIM
    kk_gain = k_k.astype(F32).reshape(H, N)
    ka_gain = k_a.astype(F32).reshape(H, N)

    def prep(p):
        bsz, L = p.shape[:2]
        p = p + (centred_shift(p) - p) * mu
        r, k, v, wd, ad, gd = _split(p, [RWKV_WIDTH] * 3 + [2 * RWKV_DECAY_RANK, 2 * RWKV_A_RANK, RWKV_GATE_RANK])
        w_pre = w0 + jnp.einsum('bldr,drc->bldc', jnp.tanh(wd.reshape(bsz, L, 2, RWKV_DECAY_RANK)), w2)
        decay = jnp.exp(-jnp.exp(-jax.nn.softplus(-w_pre.astype(F32)) - 0.5)).reshape(bsz, L, 2, H, N)
        a = jax.nn.sigmoid((a0 + jnp.einsum('bldr,drc->bldc', ad.reshape(bsz, L, 2, RWKV_A_RANK), a2)).astype(F32))
        a = a.reshape(bsz, L, 2, H, N)
        g = jax.nn.sigmoid(gd) @ g2
        r, k, v = (t.astype(F32).reshape(bsz, L, H, N) for t in (r, k, v))
        kk = k * kk_gain
        kk = kk * lax.rsqrt(jnp.sum(kk * kk, axis=-1, keepdims=True) + 1e-12)
        k_mod = k[:, :, None] * (1.0 + (a - 1.0) * ka_gain)
        return r, v, kk, decay, k_mod, kk[:, :, None] * a, g

    rc, vc, kkc, dc, kmc, bc, gc = prep(pc)
    rl, vl, kkl, dl, kml, bl, gl = prep(pl)
    o = rwkv7_scan(dir_stack(rc, rl, False), dir_stack(dc, dl, True), dir_stack(kmc, kml, True),
                   dir_stack(vc, vl, False), dir_stack(kkc, kkl, False), dir_stack(bc, bl, True))
    oc, ol = dir_merge(o, pc.shape[1], need_ctx)

    def finish(o, r, v, k_mod, g):
        o = head_layernorm(o, ln_w.astype(F32).reshape(H, N), ln_b.astype(F32).reshape(H, N), RWKV_GN_EPS)
        bonus = jnp.sum(r[:, :, None] * k_mod * r_k.astype(F32).reshape(H, N), axis=(2, 4))
        o = (o + bonus[..., None] * v).reshape(o.shape[0], o.shape[1], RWKV_WIDTH)
        return (o * g.astype(F32)).astype(g.dtype)

    yl = finish(ol, rl, vl, kml, gl)
    yc = finish(oc, rc, vc, kmc, gc) if need_ctx else None
    return yc, yl


def hgrn2_group(pc, pl, lb, norm_w, need_ctx):
    H, DK, DV = HGRN_HEADS, HGRN_DK, HGRN_DV
    lb = lb.astype(F32)

    def prep(p):
        bsz, L = p.shape[:2]
        q, f, i, g = _split(p, [HGRN_KW, 2 * HGRN_KW, HGRN_VW, HGRN_VW])
        q = (jax.nn.silu(q.astype(F32)) * DK ** -0.5).reshape(bsz, L, H, DK)
        f = f.astype(F32).reshape(bsz, L, 2, HGRN_KW)
        log_f = jnp.log(lb + (1.0 - lb) * jax.nn.sigmoid(f)).reshape(bsz, L, 2, H, DK)
        k = ((1.0 - lb) * jax.nn.sigmoid(-f)).reshape(bsz, L, 2, H, DK)
        return q, k, log_f, i.astype(F32).reshape(bsz, L, H, DV), g

    qc, kc, lfc, vc, gc = prep(pc)
    ql, kl, lfl, vl, gl = prep(pl)
    bsz, lc = pl.shape[0], pc.shape[1]
    L = lc + pl.shape[1]
    q = dir_stack(qc, ql, False).reshape(2 * bsz, L, H, DK)
    k = dir_stack(kc, kl, True).reshape(2 * bsz, L, H, DK)
    log_f = dir_stack(lfc, lfl, True).reshape(2 * bsz, L, H, DK)
    v = dir_stack(vc, vl, False).reshape(2 * bsz, L, H, DV)
    o = gla_chunked(q, k, v, log_f, HGRN_CHUNK).reshape(2, bsz, L, H, DV)
    oc, ol = dir_merge(o, lc, need_ctx)

    def finish(o, g):
        o = rms_norm(o, norm_w.reshape(H, DV)).reshape(o.shape[0], o.shape[1], HGRN_VW)
        return (o * jax.nn.silu(g.astype(F32))).astype(g.dtype)

    yl = finish(ol, gl)
    yc = finish(oc, gc) if need_ctx else None
    return yc, yl


def ssd_group(pc, pl, conv_w, conv_b, dt_bias, a_log, d_skip, norm_w, need_ctx):
    H, P, G, N = SSD_HEADS, SSD_HEAD_DIM, SSD_GROUPS, SSD_STATE
    hg = H // G

    def prep(p):
        bsz, L = p.shape[:2]
        zg, xbc, dt = _split(p, [SSD_INNER, SSD_CONV_CH, 2 * H])
        xbc = jax.nn.silu(depthwise_conv_centred(xbc, conv_w, conv_b))
        x, Bm, Cm = _split(xbc, [SSD_INNER, G * N, G * N])
        dt = jax.nn.softplus((dt.reshape(bsz, L, 2, H) + dt_bias).astype(F32))
        log_a = dt * -jnp.exp(a_log.astype(F32))
        x = x.astype(F32).reshape(bsz, L, H, P)
        x_dt = x[:, :, None] * dt[..., None]
        return zg, x, x_dt, log_a, Bm.astype(F32).reshape(bsz, L, G, N), Cm.astype(F32).reshape(bsz, L, G, N)

    zc, xc, xdc, lac, Bc, Cc = prep(pc)
    zl, xl, xdl, lal, Bl, Cl = prep(pl)
    bsz, lc = pl.shape[0], pc.shape[1]
    L = lc + pl.shape[1]
    y = ssd_chunked(dir_stack(xdc, xdl, True).reshape(2 * bsz, L, G, hg, P),
                    dir_stack(lac, lal, True).reshape(2 * bsz, L, G, hg),
                    dir_stack(Bc, Bl, False).reshape(2 * bsz, L, G, N),
                    dir_stack(Cc, Cl, False).reshape(2 * bsz, L, G, N), SSD_CHUNK)
    yc, yl = dir_merge(y.reshape(2, bsz, L, H, P), lc, need_ctx)

    def finish(y, x, zg):
        b_, l_ = y.shape[:2]
        y = (y + d_skip.astype(F32)[:, None] * x).reshape(b_, l_, SSD_INNER) * jax.nn.silu(zg.astype(F32))
        y = rms_norm(y.reshape(b_, l_, G, SSD_INNER // G), norm_w.reshape(G, SSD_INNER // G))
        return y.reshape(b_, l_, SSD_INNER).astype(zg.dtype)

    out_l = finish(yl, xl, zl)
    out_c = finish(yc, xc, zc) if need_ctx else None
    return out_c, out_l


def mlstm_group(pc, pl, conv_w, conv_b, i_bias, f_bias, norm_w, need_ctx):
    H, DK, DV = MLSTM_HEADS, MLSTM_DQK, MLSTM_DV

    def prep(p):
        bsz, L = p.shape[:2]
        qk, v, og, ig, fg = _split(p, [2 * MLSTM_QK_W, MLSTM_V_W, MLSTM_V_W, 2 * H, 2 * H])
        qk = jax.nn.silu(depthwise_conv_centred(qk, conv_w, conv_b)).astype(F32)
        q, k = jnp.split(qk, 2, axis=-1)
        q = q.reshape(bsz, L, H, DK)
        k = k.reshape(bsz, L, H, DK) * DK ** -0.5
        v = v.astype(F32).reshape(bsz, L, H, DV)
        log_i = ig.astype(F32).reshape(bsz, L, 2, H) + i_bias.astype(F32)
        log_f = jax.nn.log_sigmoid(fg.astype(F32).reshape(bsz, L, 2, H) + f_bias.astype(F32))
        return q, k, v, og, log_i, log_f

    qc, kc, vc, oc_g, lic, lfc = prep(pc)
    ql, kl, vl, ol_g, lil, lfl = prep(pl)
    bsz, lc = pl.shape[0], pc.shape[1]
    L = lc + pl.shape[1]
    h = mlstm_chunked(dir_stack(qc, ql, False).reshape(2 * bsz, L, H, DK),
                      dir_stack(kc, kl, False).reshape(2 * bsz, L, H, DK),
                      dir_stack(vc, vl, False).reshape(2 * bsz, L, H, DV),
                      dir_stack(lic, lil, True).reshape(2 * bsz, L, H),
                      dir_stack(lfc, lfl, True).reshape(2 * bsz, L, H), MLSTM_CHUNK)
    hc, hl = dir_merge(h.reshape(2, bsz, L, H, DV), lc, need_ctx)

    def finish(h, og):
        h = rms_norm(h, norm_w.reshape(H, DV)).reshape(h.shape[0], h.shape[1], MLSTM_V_W)
        return (h * jax.nn.sigmoid(og.astype(F32))).astype(og.dtype)

    yl = finish(hl, ol_g)
    yc = finish(hc, oc_g) if need_ctx else None
    return yc, yl


def moe_ffn(h, router_w, router_b, w_gate, b_gate, w_up, b_up, w_down, b_down):
    T, D = h.shape
    logits = (h @ router_w).astype(F32) + router_b.astype(F32)
    top_val, top_idx = lax.top_k(logits, TOP_K)
    probs = jax.nn.softmax(top_val, axis=-1)
    TK = T * TOP_K
    flat_e = top_idx.reshape(-1)
    order = jnp.argsort(flat_e)
    sorted_e = flat_e[order]
    sorted_tok = order // TOP_K
    sorted_w = probs.reshape(-1)[order].astype(h.dtype)
    counts = jnp.bincount(flat_e, length=N_EXPERTS)
    padded = (counts + MOE_BLOCK - 1) // MOE_BLOCK * MOE_BLOCK
    start = jnp.cumsum(counts) - counts
    ends = jnp.cumsum(padded)
    pstart = ends - padded
    dest = pstart[sorted_e] + jnp.arange(TK) - start[sorted_e]
    n_blocks = -(-TK // MOE_BLOCK) + N_EXPERTS
    block_expert = jnp.clip(jnp.searchsorted(ends, jnp.arange(n_blocks) * MOE_BLOCK, side='right'), 0, N_EXPERTS - 1)
    x_buf = jnp.zeros((n_blocks * MOE_BLOCK, D), h.dtype).at[dest].set(h[sorted_tok])

    def expert_block(args):
        xb, e = args
        g = xb @ w_gate[e] + b_gate[e]
        u = xb @ w_up[e] + b_up[e]
        g = jnp.minimum(g, SWIGLU_LIMIT)
        u = jnp.clip(u, -SWIGLU_LIMIT, SWIGLU_LIMIT)
        return (g * jax.nn.sigmoid(SWIGLU_ALPHA * g) * (u + 1.0)) @ w_down[e] + b_down[e]

    y_blocks = lax.map(expert_block, (x_buf.reshape(n_blocks, MOE_BLOCK, D), block_expert))
    y_sorted = y_blocks.reshape(n_blocks * MOE_BLOCK, D)[dest]
    return jnp.zeros((T, D), h.dtype).at[sorted_tok].add(y_sorted * sorted_w[:, None])


def setup_inputs(seed: int = 0) -> dict:
    key = jax.random.key(seed)
    keys = iter(jax.random.split(key, 64))
    D = D_MODEL

    def normal(shape, scale):
        return jax.random.normal(next(keys), shape, F32) * scale

    def gain(shape):
        return 1.0 + normal(shape, 0.05)

    def uniform(shape, lo, hi):
        return jax.random.uniform(next(keys), shape, F32, lo, hi)

    dt = jnp.exp(uniform((N_ODD, 2, SSD_HEADS), math.log(1e-3), math.log(1e-1)))
    return {
        'x': normal((BATCH, SEQ, D), 1.0),
        'c': normal((BATCH, D), 1.0),
        'ctx': normal((BATCH, CTX_LEN, D), 1.0),
        'c_ctx': normal((D,), 1.0),
        'ada_w': normal((DEPTH, D, N_MOD * D), 0.5 * D ** -0.5),
        'ada_b': normal((DEPTH, N_MOD * D), 0.02),
        'norm1_w': gain((DEPTH, D)),
        'norm2_w': gain((DEPTH, D)),
        'even_w_in': normal((N_EVEN, D, EVEN_PROJ), D ** -0.5),
        'even_w_out': normal((N_EVEN, EVEN_OUT, D), EVEN_OUT ** -0.5),
        'rwkv_mu': uniform((N_EVEN, RWKV_PROJ), 0.0, 1.0),
        'rwkv_w0': uniform((N_EVEN, 2, RWKV_WIDTH), -6.0, 0.0),
        'rwkv_w2': normal((N_EVEN, 2, RWKV_DECAY_RANK, RWKV_WIDTH), 0.5 * RWKV_DECAY_RANK ** -0.5),
        'rwkv_a0': normal((N_EVEN, 2, RWKV_WIDTH), 0.1),
        'rwkv_a2': normal((N_EVEN, 2, RWKV_A_RANK, RWKV_WIDTH), 0.5 * RWKV_A_RANK ** -0.5),
        'rwkv_g2': normal((N_EVEN, RWKV_GATE_RANK, RWKV_WIDTH), RWKV_GATE_RANK ** -0.5),
        'rwkv_k_k': 0.85 + normal((N_EVEN, RWKV_WIDTH), 0.05),
        'rwkv_k_a': gain((N_EVEN, RWKV_WIDTH)),
        'rwkv_r_k': normal((N_EVEN, RWKV_WIDTH), 0.1),
        'rwkv_ln_w': gain((N_EVEN, RWKV_WIDTH)),
        'rwkv_ln_b': normal((N_EVEN, RWKV_WIDTH), 0.02),
        'hgrn_lower_bounds': normal((DEPTH + 1, HGRN_KW), 0.1),
        'hgrn_norm_w': gain((N_EVEN, HGRN_VW)),
        'odd_w_in': normal((N_ODD, D, ODD_PROJ), D ** -0.5),
        'odd_w_out': normal((N_ODD, ODD_OUT, D), ODD_OUT ** -0.5),
        'ssd_conv_w': normal((N_ODD, CONV_WIDTH, SSD_CONV_CH), CONV_WIDTH ** -0.5),
        'ssd_conv_b': normal((N_ODD, SSD_CONV_CH), 0.02),
        'ssd_dt_bias': dt + jnp.log(-jnp.expm1(-dt)),
        'ssd_a_log': jnp.log(uniform((N_ODD, 2, SSD_HEADS), 1.0, 16.0)),
        'ssd_d': gain((N_ODD, SSD_HEADS)),
        'ssd_norm_w': gain((N_ODD, SSD_INNER)),
        'mlstm_conv_w': normal((N_ODD, CONV_WIDTH, 2 * MLSTM_QK_W), CONV_WIDTH ** -0.5),
        'mlstm_conv_b': normal((N_ODD, 2 * MLSTM_QK_W), 0.02),
        'mlstm_i_bias': normal((N_ODD, 2, MLSTM_HEADS), 0.1),
        'mlstm_f_bias': uniform((N_ODD, 2, MLSTM_HEADS), 3.0, 6.0),
        'mlstm_norm_w': gain((N_ODD, MLSTM_V_W)),
        'router_w': normal((DEPTH, D, N_EXPERTS), D ** -0.5),
        'router_b': normal((DEPTH, N_EXPERTS), 0.01),
        'exp_w_gate': normal((DEPTH, N_EXPERTS, D, D_FF_EXPERT), D ** -0.5),
        'exp_b_gate': normal((DEPTH, N_EXPERTS, D_FF_EXPERT), 0.02),
        'exp_w_up': normal((DEPTH, N_EXPERTS, D, D_FF_EXPERT), D ** -0.5),
        'exp_b_up': normal((DEPTH, N_EXPERTS, D_FF_EXPERT), 0.02),
        'exp_w_down': normal((DEPTH, N_EXPERTS, D_FF_EXPERT, D), D_FF_EXPERT ** -0.5),
        'exp_b_down': normal((DEPTH, N_EXPERTS, D), 0.02),
        'final_norm_w': gain((D,)),
    }


def reference(x, c, ctx, c_ctx, ada_w, ada_b, norm1_w, norm2_w, even_w_in, even_w_out, rwkv_mu, rwkv_w0,
              rwkv_w2, rwkv_a0, rwkv_a2, rwkv_g2, rwkv_k_k, rwkv_k_a, rwkv_r_k, rwkv_ln_w, rwkv_ln_b,
              hgrn_lower_bounds, hgrn_norm_w, odd_w_in, odd_w_out, ssd_conv_w, ssd_conv_b, ssd_dt_bias,
              ssd_a_log, ssd_d, ssd_norm_w, mlstm_conv_w, mlstm_conv_b, mlstm_i_bias, mlstm_f_bias,
              mlstm_norm_w, router_w, router_b, exp_w_gate, exp_b_gate, exp_w_up, exp_b_up, exp_w_down,
              exp_b_down, final_norm_w):
    bsz, seq, _ = x.shape
    rows = seq // GRID_W
    lc = ctx.shape[1]
    lower_bounds = jnp.cumsum(jax.nn.softmax(hgrn_lower_bounds.astype(F32), axis=0), axis=0)
    xl, xc = x, ctx
    for l in range(DEPTH):
        need_ctx = l < DEPTH - 1
        mod = (jax.nn.silu(c) @ ada_w[l] + ada_b[l]).reshape(bsz, N_MOD, 1, D_MODEL)
        mod_c = (jax.nn.silu(c_ctx) @ ada_w[l] + ada_b[l]).reshape(N_MOD, D_MODEL)
        hl = rms_norm(xl, norm1_w[l]) * (1.0 + mod[:, 1]) + mod[:, 0]
        hc = rms_norm(xc, norm1_w[l]) * (1.0 + mod_c[1]) + mod_c[0]
        if l % 2 == 0:
            e = l // 2
            pc_r, pc_h = _split(hc @ even_w_in[e], [RWKV_PROJ, HGRN_PROJ])
            pl_r, pl_h = _split(hl @ even_w_in[e], [RWKV_PROJ, HGRN_PROJ])
            rc, rl = rwkv7_group(pc_r, pl_r, rwkv_mu[e], rwkv_w0[e], rwkv_w2[e], rwkv_a0[e], rwkv_a2[e],
                                 rwkv_g2[e], rwkv_k_k[e], rwkv_k_a[e], rwkv_r_k[e], rwkv_ln_w[e], rwkv_ln_b[e], need_ctx)
            gc_, gl_ = hgrn2_group(pc_h, pl_h, lower_bounds[l], hgrn_norm_w[e], need_ctx)
            yl = jnp.concatenate([rl, gl_], axis=-1) @ even_w_out[e]
            yc = (jnp.concatenate([rc, gc_], axis=-1) @ even_w_out[e]) if need_ctx else None
        else:
            o = l // 2
            hl_cm = to_col_major(hl, rows)
            pc_s, pc_m = _split(hc @ odd_w_in[o], [SSD_PROJ, MLSTM_PROJ])
            pl_s, pl_m = _split(hl_cm @ odd_w_in[o], [SSD_PROJ, MLSTM_PROJ])
            sc, sl = ssd_group(pc_s, pl_s, ssd_conv_w[o], ssd_conv_b[o], ssd_dt_bias[o], ssd_a_log[o], ssd_d[o],
                               ssd_norm_w[o], need_ctx)
            mc, ml = mlstm_group(pc_m, pl_m, mlstm_conv_w[o], mlstm_conv_b[o], mlstm_i_bias[o], mlstm_f_bias[o],
                                 mlstm_norm_w[o], need_ctx)
            yl = to_row_major(jnp.concatenate([sl, ml], axis=-1) @ odd_w_out[o], rows)
            yc = (jnp.concatenate([sc, mc], axis=-1) @ odd_w_out[o]) if need_ctx else None
        xl = xl + mod[:, 2] * yl
        hl = rms_norm(xl, norm2_w[l]) * (1.0 + mod[:, 4]) + mod[:, 3]
        moe_args = (router_w[l], router_b[l], exp_w_gate[l], exp_b_gate[l], exp_w_up[l], exp_b_up[l],
                    exp_w_down[l], exp_b_down[l])
        if need_ctx:
            xc = xc + mod_c[2] * yc
            hc = rms_norm(xc, norm2_w[l]) * (1.0 + mod_c[4]) + mod_c[3]
            tokens = jnp.concatenate([hc.reshape(-1, D_MODEL), hl.reshape(-1, D_MODEL)], axis=0)
            f_out = moe_ffn(tokens, *moe_args)
            xc = xc + mod_c[5] * f_out[:bsz * lc].reshape(bsz, lc, D_MODEL)
            xl = xl + mod[:, 5] * f_out[bsz * lc:].reshape(bsz, seq, D_MODEL)
        else:
            f_out = moe_ffn(hl.reshape(-1, D_MODEL), *moe_args)
            xl = xl + mod[:, 5] * f_out.reshape(bsz, seq, D_MODEL)
    return rms_norm(xl, final_norm_w)
```

```python
import numpy as np
import concourse.bass as bass
import concourse.mybir as mybir
from concourse.bass_utils import run_bass_kernel_spmd
from contextlib import ExitStack

F32 = mybir.dt.float32
BF16 = mybir.dt.bfloat16
AF = mybir.ActivationFunctionType
ALU = mybir.AluOpType
AX = mybir.AxisListType

ENGS = ("pe", "dve", "act", "pool", "sp")
D = 1024
KO = 8
LC = 256
SEQ = 4096
L = LC + SEQ
NT = L // 128
EPS = 1e-6
CH = 64
NCH = L // CH


class Cell:
    __slots__ = ("w", "r")

    def __init__(self):
        self.w = None
        self.r = []


class Buf:
    def __init__(self, name, h, is_dram=False, is_psum=False):
        self.name = name
        self.h = h
        self.is_dram = is_dram
        self.is_psum = is_psum
        self.base = Cell()
        self.parts = {}

    def cells(self, key):
        if key is None:
            return [self.base] + list(self.parts.values())
        c = self.parts.get(key)
        if c is None:
            c = Cell()
            c.w = self.base.w
            c.r = list(self.base.r)
            self.parts[key] = c
        return [c]

    def __getitem__(self, idx):
        return self.h[idx]

    def a(self):
        return self.h[:]


class Prog:
    def __init__(self, nc, es):
        self.nc = nc
        self.es = es
        self.q = {e: [] for e in ENGS}
        self.cnt = {e: 0 for e in ENGS}
        self.sem = {e: es.enter_context(nc.semaphore("s_" + e)) for e in ENGS}
        self.known = {e: {} for e in ENGS}
        self.dsem = {}
        self.dsem_by_id = {}
        self.phase_slots = {}
        self.ninst = 0
        self.uid = 0

    def sb(self, name, shape, dt=F32, es=None):
        self.uid += 1
        h = (es or self.es).enter_context(self.nc.sbuf_tensor(f"{name}_{self.uid}", list(shape), dt))
        return Buf(name, h)

    def ps(self, name, shape, dt=F32, es=None):
        self.uid += 1
        h = (es or self.es).enter_context(self.nc.psum_tensor(f"{name}_{self.uid}", list(shape), dt))
        return Buf(name, h, is_psum=True)

    def dram(self, name, shape, dt=F32, kind="Internal"):
        h = self.nc.dram_tensor(name, list(shape), dt, kind=kind)
        return Buf(name, h.ap(), is_dram=True)

    def dma_sem(self, name):
        if name not in self.dsem:
            self.dsem[name] = [self.es.enter_context(self.nc.semaphore("d_" + name)), 0]
            self.dsem_by_id[id(self.dsem[name][0])] = self.dsem[name]
        return self.dsem[name]

    def _norm(self, lst):
        return [(r, None) if isinstance(r, Buf) else ((r[0], None) if r[0].is_psum else r) for r in lst]

    def _deps(self, eng, reads, writes, pe_acc=False):
        need = {}

        def add(tok):
            if tok is None:
                return
            k = id(tok[0])
            if k not in need or need[k][1] < tok[1]:
                need[k] = tok

        for (b, key) in reads:
            for c in b.cells(key):
                add(c.w)
        for (b, key) in writes:
            for c in b.cells(key):
                if not (pe_acc and c.w is not None and c.w[2] == "pe"):
                    add(c.w)
                for t in c.r:
                    add(t)
        out = []
        kn = self.known[eng]
        for k, tok in need.items():
            val = tok[1]
            if k in self.dsem_by_id:
                val = self.dsem_by_id[k][1]
            if kn.get(k, 0) >= val:
                continue
            kn[k] = val
            out.append((tok[0], val))
        return out

    def _record(self, tok, reads, writes):
        for (b, key) in reads:
            for c in b.cells(key):
                c.r.append(tok)
                if len(c.r) > 16:
                    best = {}
                    for t in c.r:
                        kk = id(t[0])
                        if kk not in best or best[kk][1] < t[1]:
                            best[kk] = t
                    c.r = list(best.values())
        for (b, key) in writes:
            for c in b.cells(key):
                c.w = tok
                c.r = []

    def op(self, eng, fn, reads=(), writes=(), pe_acc=False):
        reads = self._norm(reads)
        writes = self._norm(writes)
        writes = writes + [r for r in reads if r[0].is_psum]
        waits = self._deps(eng, reads, writes, pe_acc)
        self.cnt[eng] += 1
        sem = self.sem[eng]
        tok = (sem, self.cnt[eng], eng)
        self.ninst += 1

        def run(e, waits=waits, fn=fn, sem=sem):
            for s, v in waits:
                e.wait_ge(s, v)
            fn(e).then_inc(sem, 1)

        self.q[eng].append(run)
        self._record(tok, reads, writes)
        return tok

    def dma(self, eng, out_ap, in_ap, reads=(), writes=(), semname=None, **kw):
        reads = self._norm(reads)
        writes = self._norm(writes)
        waits = self._deps(eng, reads, writes)
        if semname is None:
            sbs = [b for (b, _) in list(writes) + list(reads) if not b.is_dram]
            semname = sbs[0].name if sbs else "dram2dram"
        if semname not in self.phase_slots:
            self.phase_slots[semname] = len(self.phase_slots)
        ds = self.dma_sem(f"slot{self.phase_slots[semname]}")
        ds[1] += 16
        tok = (ds[0], ds[1], "dma")
        self.ninst += 1

        def run(e, waits=waits, sem=ds[0]):
            for s, v in waits:
                e.wait_ge(s, v)
            e.dma_start(out=out_ap, in_=in_ap, **kw).then_inc(sem, 16)

        self.q[eng].append(run)
        self._record(tok, reads, writes)
        return tok

    def barrier(self):
        self.phase_slots = {}
        toks = [(self.sem[f], self.cnt[f]) for f in ENGS if self.cnt[f] > 0]
        toks += [(v[0], v[1]) for v in self.dsem.values() if v[1] > 0]
        for e in ENGS:
            kn = self.known[e]
            ws = []
            for s, v in toks:
                if kn.get(id(s), 0) < v:
                    kn[id(s)] = v
                    ws.append((s, v))

            def run(en, ws=ws):
                for s, v in ws:
                    en.wait_ge(s, v)
            self.q[e].append(run)

    def finish(self):
        nc = self.nc
        self.barrier()
        with nc.Block() as block:
            @block.tensor
            def _(e):
                for f in self.q["pe"]:
                    f(e)

            @block.vector
            def _(e):
                for f in self.q["dve"]:
                    f(e)

            @block.scalar
            def _(e):
                for f in self.q["act"]:
                    f(e)

            @block.gpsimd
            def _(e):
                for f in self.q["pool"]:
                    f(e)

            @block.sync
            def _(e):
                for f in self.q["sp"]:
                    f(e)


def _rk(x):
    return (x[0], x[2] if len(x) > 2 else None)


class K:
    def __init__(self, P):
        self.P = P
        self.dq = 0

    def mm(self, out, lhsT, rhs, start=True, stop=True):
        return self.P.op("pe", lambda e: e.matmul(out[1], lhsT=lhsT[1], rhs=rhs[1], start=start, stop=stop),
                         reads=[_rk(lhsT), _rk(rhs)], writes=[_rk(out)], pe_acc=not start)

    def tr(self, out, in_, ident):
        return self.P.op("pe", lambda e: e.transpose(out[1], in_[1], ident[1]),
                         reads=[_rk(in_), _rk(ident)], writes=[_rk(out)])

    def act(self, out, in_, func, bias=None, scale=None, accum=None, eng="act"):
        reads = [_rk(in_)]
        kw = {}
        if bias is not None:
            if isinstance(bias, tuple):
                reads.append(_rk(bias)); kw["bias"] = bias[1]
            else:
                kw["bias"] = bias
        if scale is not None:
            if isinstance(scale, tuple):
                reads.append(_rk(scale)); kw["scale"] = scale[1]
            else:
                kw["scale"] = scale
        writes = [_rk(out)]
        if accum is not None:
            writes.append(_rk(accum)); kw["accum_out"] = accum[1]
        return self.P.op("act", lambda e: e.activation(out=out[1], in_=in_[1], func=func, **kw), reads=reads, writes=writes)

    def tt(self, eng, out, in0, in1, op):
        return self.P.op(eng, lambda e: e.tensor_tensor(out=out[1], in0=in0[1], in1=in1[1], op=op),
                         reads=[_rk(in0), _rk(in1)], writes=[_rk(out)])

    def ts(self, eng, out, in0, s1, op0, s2=None, op1=None, accum=None):
        reads = [_rk(in0)]
        a1 = s1
        if isinstance(s1, tuple):
            reads.append(_rk(s1)); a1 = s1[1]
        a2 = s2
        if isinstance(s2, tuple):
            reads.append(_rk(s2)); a2 = s2[1]
        kw = {}
        if op1 is not None:
            kw["op1"] = op1
        writes = [_rk(out)]
        if accum is not None:
            writes.append(_rk(accum)); kw["accum_out"] = accum[1]
        return self.P.op(eng, lambda e: e.tensor_scalar(out=out[1], in0=in0[1], scalar1=a1, scalar2=a2, op0=op0, **kw),
                         reads=reads, writes=writes)

    def stt(self, eng, out, in0, scalar, in1, op0, op1):
        reads = [_rk(in0), _rk(in1)]
        sc = scalar
        if isinstance(scalar, tuple):
            reads.append(_rk(scalar)); sc = scalar[1]
        return self.P.op(eng, lambda e: e.scalar_tensor_tensor(out=out[1], in0=in0[1], scalar=sc, in1=in1[1], op0=op0, op1=op1),
                         reads=reads, writes=[_rk(out)])

    def cp(self, eng, out, in_):
        if eng == "act":
            return self.P.op("act", lambda e: e.copy(out=out[1], in_=in_[1]), reads=[_rk(in_)], writes=[_rk(out)])
        return self.P.op(eng, lambda e: e.tensor_copy(out=out[1], in_=in_[1]), reads=[_rk(in_)], writes=[_rk(out)])

    def memset(self, eng, out, val):
        return self.P.op(eng, lambda e: e.memset(out[1], val), writes=[_rk(out)])

    def scan(self, out, d0, d1, init, op0, op1):
        return self.P.op("dve", lambda e: e.tensor_tensor_scan(out=out[1], data0=d0[1], data1=d1[1], initial=init, op0=op0, op1=op1),
                         reads=[_rk(d0), _rk(d1)], writes=[_rk(out)])

    def recip(self, out, in_):
        return self.P.op("dve", lambda e: e.reciprocal(out=out[1], in_=in_[1]), reads=[_rk(in_)], writes=[_rk(out)])

    def dma(self, out, in_, eng=None, **kw):
        if eng is None:
            eng = "sp" if in_[0].is_dram else "act"
        reads = [_rk(in_)]
        writes = [_rk(out)]
        return self.P.dma(eng, out[1], in_[1], reads=reads, writes=writes, **kw)


def V(buf, ap=None, key=None):
    return (buf, buf.a() if ap is None else ap, key)


def build(debug=(), stop_after=None):
    nc = bass.Bass("TRN2", target_bir_lowering=False)
    es = ExitStack()
    with es:
        P = Prog(nc, es)
        k = K(P)
        I = {}

        def inp(name, shape):
            I[name] = P.dram(name, shape, F32, kind="ExternalInput")

        inp("x", [SEQ, D]); inp("c", [D]); inp("ctx", [LC, D]); inp("c_ctx", [D])
        inp("ada_w", [2, D, 6 * D]); inp("ada_b", [2, 6 * D]); inp("norm1_w", [2, D]); inp("norm2_w", [2, D])
        inp("even_w_in", [1, D, 4480]); inp("even_w_out", [1, 1024, D])
        inp("rwkv_mu", [1, 1920]); inp("rwkv_w0", [1, 2, 512]); inp("rwkv_w2", [1, 2, 64, 512])
        inp("rwkv_a0", [1, 2, 512]); inp("rwkv_a2", [1, 2, 64, 512]); inp("rwkv_g2", [1, 128, 512])
        for n in ("rwkv_k_k", "rwkv_k_a", "rwkv_r_k", "rwkv_ln_w", "rwkv_ln_b"):
            inp(n, [1, 512])
        inp("hgrn_lower_bounds", [3, 512]); inp("hgrn_norm_w", [1, 512])
        inp("odd_w_in", [1, D, 5680]); inp("odd_w_out", [1, 2048, D])
        inp("ssd_conv_w", [1, 5, 1536]); inp("ssd_conv_b", [1, 1536]); inp("ssd_dt_bias", [1, 2, 16])
        inp("ssd_a_log", [1, 2, 16]); inp("ssd_d", [1, 16]); inp("ssd_norm_w", [1, 1024])
        inp("mlstm_conv_w", [1, 5, 1024]); inp("mlstm_conv_b", [1, 1024]); inp("mlstm_i_bias", [1, 2, 4])
        inp("mlstm_f_bias", [1, 2, 4]); inp("mlstm_norm_w", [1, 1024])
        inp("router_w", [2, D, 32]); inp("router_b", [2, 32])
        inp("exp_w_gate", [2, 32, D, 1024]); inp("exp_b_gate", [2, 32, 1024])
        inp("exp_w_up", [2, 32, D, 1024]); inp("exp_b_up", [2, 32, 1024])
        inp("exp_w_down", [2, 32, 1024, D]); inp("exp_b_down", [2, 32, D])
        inp("final_norm_w", [D])
        OUT = P.dram("out", [SEQ, D], F32, kind="ExternalOutput")

        def scratch(name, shape, dt=F32):
            return P.dram(name, shape, dt, kind="ExternalOutput" if name in debug else "Internal")

        XL = scratch("XL", [L, D])

        ident = P.sb("ident", [128, 128], F32)
        identb = P.sb("identb", [128, 128], BF16)
        ones = P.sb("ones", [128, 128], F32)
        k.memset("pool", V(ones), 1.0)
        P.op("pool", lambda e: e.affine_select(out=ident.a(), in_=ones.a(), pattern=[[-1, 128]], compare_op=ALU.is_equal,
                                               fill=0.0, base=0, channel_multiplier=1), reads=[ones], writes=[ident])
        k.cp("dve", V(identb), V(ident))

        modF = P.sb("modF", [128, 6, KO, 2], F32)
        modB = P.sb("modB", [128, 2, 2, D], F32)
        g1F = P.sb("g1F", [128, KO, 2], F32)
        g2F = P.sb("g2F", [128, KO, 2], F32)
        nw1 = P.sb("nw1", [128, 2, KO], F32)
        nw2 = P.sb("nw2", [128, 2, KO], F32)
        k.dma(V(nw1), V(I["norm1_w"], I["norm1_w"].a().rearrange("l (ko p) -> p l ko", p=128)), allow_slow_non_contiguous=True)
        k.dma(V(nw2), V(I["norm2_w"], I["norm2_w"].a().rearrange("l (ko p) -> p l ko", p=128)), allow_slow_non_contiguous=True)

        def stage_mods(l):
            with ExitStack() as ph:
                c0 = P.sb("c0", [128, KO, 2], F32, es=ph)
                s = P.sb("s", [128, KO, 2], F32, es=ph)
                sB = P.sb("sB", [128, KO, 2, 128], F32, es=ph)
                abF = P.sb("abF", [128, 48], F32, es=ph)
                abB = P.sb("abB", [128, 2, D], F32, es=ph)
                awm = [P.sb(f"awm{i}", [128, KO, D], F32, es=ph) for i in range(2)]
                psF = P.ps("psF", [128, KO, 2], F32, es=ph)
                psB = [P.ps(f"psB{i}", [128, 512], F32, es=ph) for i in range(2)]
                tmp = P.sb("tmpm", [128, KO, 2], F32, es=ph)
                k.dma((c0, c0[:, :, 0]), V(I["c"], I["c"].a().rearrange("(ko p) -> p ko", p=128)), allow_slow_non_contiguous=True)
                k.dma((c0, c0[:, :, 1]), V(I["c_ctx"], I["c_ctx"].a().rearrange("(ko p) -> p ko", p=128)), allow_slow_non_contiguous=True)
                k.act(V(s), V(c0), AF.Silu)
                k.cp("dve", V(sB), (s, s.a().rearrange("p k (j o) -> p k j o", o=1).to_broadcast([128, KO, 2, 128])))
                k.dma(V(abF), V(I["ada_b"], I["ada_b"][l].rearrange("(nb p) -> p nb", p=128)), allow_slow_non_contiguous=True)
                k.dma((abB, abB[:, 0, :]), V(I["ada_b"], I["ada_b"][l, 2 * D:3 * D].partition_broadcast(128)))
                k.dma((abB, abB[:, 1, :]), V(I["ada_b"], I["ada_b"][l, 5 * D:6 * D].partition_broadcast(128)))
                for m in range(6):
                    aw = awm[m % 2]
                    k.dma(V(aw), V(I["ada_w"], I["ada_w"][l, :, m * D:(m + 1) * D].rearrange("(ko p) n -> p ko n", p=128)))
                    if m in (0, 1, 3, 4):
                        for nb in range(KO):
                            for ko in range(KO):
                                k.mm((psF, psF[:, nb, :]), (aw, aw[:, ko, nb * 128:(nb + 1) * 128]), (s, s[:, ko, :]),
                                     start=(ko == 0), stop=(ko == KO - 1))
                        k.tt("dve", (modF, modF[:, m, :, :]), V(psF),
                             (abF, abF[:, m * 8:(m + 1) * 8].rearrange("p (k o) -> p k o", o=1).to_broadcast([128, KO, 2])), ALU.add)
                    else:
                        mi = 0 if m == 2 else 1
                        for j in range(2):
                            for nblk in range(2):
                                pb = psB[(j * 2 + nblk) % 2]
                                for ko in range(KO):
                                    k.mm(V(pb), (sB, sB[:, ko, j, :]), (aw, aw[:, ko, nblk * 512:(nblk + 1) * 512]),
                                         start=(ko == 0), stop=(ko == KO - 1))
                                k.tt("dve", (modB, modB[:, mi, j, nblk * 512:(nblk + 1) * 512]), V(pb),
                                     (abB, abB[:, mi, nblk * 512:(nblk + 1) * 512]), ALU.add)
                for (gF, nw, mi) in ((g1F, nw1, 1), (g2F, nw2, 4)):
                    k.ts("dve", V(tmp), (modF, modF[:, mi, :, :]), 1.0, ALU.add)
                    k.tt("dve", V(gF), V(tmp), (nw, nw[:, l, :].rearrange("p (k o) -> p k o", o=1).to_broadcast([128, KO, 2])), ALU.mult)
                P.barrier()

        def stage_norm(l, which, HT, src_tiles, perm):
            gF = g1F if which == 1 else g2F
            mi = 0 if which == 1 else 3
            with ExitStack() as ph:
                xt = [P.sb(f"xt{i}", [128, D], F32, es=ph) for i in range(3)]
                xn = [P.sb(f"xn{i}", [128, D], F32, es=ph) for i in range(2)]
                junk = P.sb("junk", [128, D], F32, es=ph)
                st = [P.sb(f"st{i}", [128, 4], F32, es=ph) for i in range(2)]
                tmp = [P.sb(f"tmpn{i}", [128, KO, 128], F32, es=ph) for i in range(2)]
                pT = [P.ps(f"pT{i}", [128, KO, 128], F32, es=ph) for i in range(2)]
                for i in range(NT):
                    j = 1 if i < 2 else 0
                    x_ = xt[i % 3]; n_ = xn[i % 2]; s_ = st[i % 2]; t_ = tmp[i % 2]; p_ = pT[i % 2]
                    sb_, sap = src_tiles(i)
                    k.dma(V(x_), (sb_, sap))
                    k.memset("pool", (s_, s_[:, 0:1]), 0.0)
                    k.act(V(junk), V(x_), AF.Square, accum=(s_, s_[:, 0:1]))
                    k.ts("dve", (s_, s_[:, 1:2]), (s_, s_[:, 0:1]), 1.0 / D, ALU.mult, EPS, ALU.add)
                    k.act((s_, s_[:, 2:3]), (s_, s_[:, 1:2]), AF.Sqrt)
                    k.recip((s_, s_[:, 3:4]), (s_, s_[:, 2:3]))
                    k.ts("dve", V(n_), V(x_), (s_, s_[:, 3:4]), ALU.mult)
                    for ko in range(KO):
                        k.tr((p_, p_[:, ko, :]), (n_, n_[:, ko * 128:(ko + 1) * 128]), V(ident))
                    k.tt("dve", V(t_), V(p_), (gF, gF[:, :, j:j + 1].to_broadcast([128, KO, 128])), ALU.mult)
                    if perm and i >= 2:
                        r0 = 2 * (i - 2)
                        dst = HT[:, :, LC:].rearrange("p k (c r) -> p k c r", r=64)[:, :, :, r0:r0 + 2]
                        src0 = t_.a().rearrange("p k (r c) -> p k c r", r=2)
                        src1 = modF[:, mi, :, j:j + 1].rearrange("p k (a b) -> p k a b", b=1).to_broadcast([128, KO, 64, 2])
                        k.tt("pool", (HT, dst, i), (t_, src0), (modF, src1), ALU.add)
                    else:
                        k.tt("pool", (HT, HT[:, :, i * 128:(i + 1) * 128], i), V(t_),
                             (modF, modF[:, mi, :, j:j + 1].to_broadcast([128, KO, 128])), ALU.add)
                P.barrier()

        def stage_proj(HT, W, ncols, f_list, t_list):
            with ExitStack() as ph:
                wst = [P.sb(f"wst{i}", [128, KO, 512], BF16, es=ph) for i in range(2)]
                stg = [P.sb(f"stg{i}", [128, 512], F32, es=ph) for i in range(4)]
                pp = [P.ps(f"pp{i}", [128, 512], F32, es=ph) for i in range(4)]
                cnt = 0
                npan = (ncols + 511) // 512
                for pn in range(npan):
                    c0 = pn * 512
                    cw = min(512, ncols - c0)
                    w_ = wst[pn % 2]
                    k.dma((w_, w_[:, :, 0:cw]), (W[0], W[1][:, c0:c0 + cw].rearrange("(ko p) n -> p ko n", p=128)), eng="pool")
                    for (f0, f1, PF, roff) in f_list:
                        fa, fb = max(c0, f0), min(c0 + cw, f1)
                        if fa >= fb:
                            continue
                        for n0 in range(fa, fb, 128):
                            nw_ = min(128, fb - n0)
                            for tb in range(0, L, 512):
                                tw = min(512, L - tb)
                                p_ = pp[cnt % 4]; s_ = stg[cnt % 4]; cnt += 1
                                for ko in range(KO):
                                    k.mm((p_, p_[0:nw_, 0:tw]), (w_, w_[:, ko, n0 - c0:n0 - c0 + nw_]), (HT, HT[:, ko, tb:tb + tw]),
                                         start=(ko == 0), stop=(ko == KO - 1))
                                k.cp("act" if cnt % 2 else "dve", (s_, s_[0:nw_, 0:tw]), (p_, p_[0:nw_, 0:tw]))
                                r0 = n0 - f0 + roff
                                k.dma((PF, PF[r0:r0 + nw_, tb:tb + tw], ("f", n0)), (s_, s_[0:nw_, 0:tw]))
                    for (t0, t1, PT, coff) in t_list:
                        ta, tb_ = max(c0, t0), min(c0 + cw, t1)
                        if ta >= tb_:
                            continue
                        tw = tb_ - ta
                        for tt in range(NT):
                            p_ = pp[cnt % 4]; s_ = stg[cnt % 4]; cnt += 1
                            for ko in range(KO):
                                k.mm((p_, p_[:, 0:tw]), (HT, HT[:, ko, tt * 128:(tt + 1) * 128]), (w_, w_[:, ko, ta - c0:ta - c0 + tw]),
                                     start=(ko == 0), stop=(ko == KO - 1))
                            k.cp("act" if cnt % 2 else "dve", (s_, s_[:, 0:tw]), (p_, p_[:, 0:tw]))
                            cc = ta - t0 + coff
                            k.dma((PT, PT[tt * 128:(tt + 1) * 128, cc:cc + tw], ("t", tt, ta)), (s_, s_[:, 0:tw]))
                P.barrier()

        def chunk_order(z):
            if z == 0:
                return list(range(NCH))
            return [3, 2, 1, 0] + list(range(NCH - 1, 3, -1))

        def make_masks(ph):
            ms = []
            for z in range(2):
                m = P.sb(f"mask{z}", [64, 64], F32, es=ph)
                if z == 0:
                    P.op("pool", lambda e, m=m: e.affine_select(out=m.a(), in_=ones[0:64, 0:64], pattern=[[1, 64]], compare_op=ALU.is_ge,
                                                           fill=0.0, base=0, channel_multiplier=-1), reads=[ones], writes=[m])
                else:
                    P.op("pool", lambda e, m=m: e.affine_select(out=m.a(), in_=ones[0:64, 0:64], pattern=[[-1, 64]], compare_op=ALU.is_ge,
                                                           fill=0.0, base=0, channel_multiplier=1), reads=[ones], writes=[m])
                ms.append(m)
            return ms

        def mixer_hgrn(PF, PT, YM):
            BW = 1088
            NB = L // BW
            CB = BW // CH
            with ExitStack() as ph:
                masks = make_masks(ph)
                lbr = P.sb("lbr", [128, 3, 4], F32, es=ph)
                lbe = P.sb("lbe", [128, 3, 4], F32, es=ph)
                lbs = P.sb("lbs", [128, 4], F32, es=ph)
                lb = P.sb("lb", [128, 4], F32, es=ph)
                oml = P.sb("oml", [128, 4], F32, es=ph)
                noml = P.sb("noml", [128, 4], F32, es=ph)
                k.dma(V(lbr), V(I["hgrn_lower_bounds"], I["hgrn_lower_bounds"].a().rearrange("j (h p) -> p j h", p=128)),
                      allow_slow_non_contiguous=True)
                k.act(V(lbe), V(lbr), AF.Exp)
                k.tt("dve", V(lbs), (lbe, lbe[:, 0, :]), (lbe, lbe[:, 1, :]), ALU.add)
                k.tt("dve", V(lbs), V(lbs), (lbe, lbe[:, 2, :]), ALU.add)
                k.recip(V(lbs), V(lbs))
                k.tt("dve", V(lb), (lbe, lbe[:, 0, :]), V(lbs), ALU.mult)
                k.ts("dve", V(oml), V(lb), -1.0, ALU.mult, 1.0, ALU.add)
                k.ts("dve", V(noml), V(oml), -1.0, ALU.mult)
                rst = P.sb("rst", [128, BW], F32, es=ph)
                k.memset("pool", V(rst), 1.0)
                k.memset("pool", (rst, rst.a().rearrange("p (c t) -> p c t", t=CH)[:, :, 0:1]), 0.0)
                nwB = P.sb("nwB", [64, 512], F32, es=ph)
                k.dma(V(nwB), V(I["hgrn_norm_w"], I["hgrn_norm_w"][0].partition_broadcast(64)))
                oacc = P.sb("oacc", [64, NCH, 128], F32, es=ph)
                ssn = P.sb("ssn", [64, NCH], F32, es=ph)
                for h in range(4):
                    with ExitStack() as hs1:
                        t_f = P.sb("t_f", [128, BW], F32, es=hs1)
                        t_s = P.sb("t_s", [128, BW], F32, es=hs1)
                        t_lf = P.sb("t_lf", [128, BW], F32, es=hs1)
                        t_k = P.sb("t_k", [128, BW], F32, es=hs1)
                        t_b = P.sb("t_b", [128, BW], F32, es=hs1)
                        t_e = P.sb("t_e", [128, BW], F32, es=hs1)
                        t_q = P.sb("t_q", [128, BW], F32, es=hs1)
                        cd = [P.sb(f"cd{z}", [128, NCH], F32, es=hs1) for z in range(2)]
                        q_in = [P.sb(f"q_in{z}", [128, L], BF16, es=hs1) for z in range(2)]
                        k_in = [P.sb(f"k_in{z}", [128, L], BF16, es=hs1) for z in range(2)]
                        k_end1 = P.sb("k_end", [128, L], BF16, es=hs1)
                        k_end = [k_end1, k_end1]
                        kET = [P.sb(f"kET{z}", [64, NCH, 128], BF16, es=hs1) for z in range(2)]
                        vb = P.sb("vb", [64, NCH, 128], BF16, es=hs1)
                        S = [P.sb(f"S{z}", [128, 128], F32, es=hs1) for z in range(2)]
                        Sb = [P.sb(f"Sb{z}", [128, 128], BF16, es=hs1) for z in range(2)]
                        Am = [P.sb(f"Am{i}", [64, 64], BF16, es=hs1) for i in range(4)]
                        psA = [P.ps(f"psA{i}", [64, 64], F32, es=hs1) for i in range(2)]
                        pso = [P.ps(f"pso{i}", [64, 128], F32, es=hs1) for i in range(2)]
                        pskv = [P.ps(f"pskv{i}", [128, 128], F32, es=hs1) for i in range(2)]
                        pst = [P.ps(f"pst{i}", [64, 4, 128], BF16, es=hs1) for i in range(2)]
                        k.dma(V(vb), (PT, PT[:, h * 128:(h + 1) * 128].rearrange("(c p) d -> p c d", p=CH)), eng="pool")
                        for z in range(2):
                            end = CH - 1 if z == 0 else 0
                            for blk in range(NB):
                                tsl = slice(blk * BW, (blk + 1) * BW)
                                frow = 2432 + z * 512 + h * 128
                                k.dma(V(t_f), (PF, PF[frow:frow + 128, tsl]))
                                k.dma(V(t_q), (PF, PF[1920 + h * 128:1920 + (h + 1) * 128, tsl]))
                                k.act(V(t_s), V(t_f), AF.Sigmoid)
                                k.act(V(t_lf), V(t_s), AF.Ln, bias=(lb, lb[:, h:h + 1]), scale=(oml, oml[:, h:h + 1]))
                                k.ts("dve", V(t_k), V(t_s), (noml, noml[:, h:h + 1]), ALU.mult, (oml, oml[:, h:h + 1]), ALU.add)
                                k.scan(V(t_b), V(rst), V(t_lf), 0.0, ALU.mult, ALU.add)
                                if z == 1:
                                    k.tt("dve", V(t_f), V(t_lf), V(t_b), ALU.subtract)
                                    b3 = t_b.a().rearrange("p (c t) -> p c t", t=CH)
                                    k.tt("dve", (t_lf, t_lf.a().rearrange("p (c t) -> p c t", t=CH)),
                                         (t_f, t_f.a().rearrange("p (c t) -> p c t", t=CH)),
                                         (t_b, b3[:, :, CH - 1:CH].to_broadcast([128, CB, CH])), ALU.add)
                                    bb = t_lf
                                else:
                                    bb = t_b
                                k.act(V(t_e), V(bb), AF.Exp)
                                k.act(V(t_s), V(bb), AF.Exp, scale=-1.0)
                                e3 = t_e.a().rearrange("p (c t) -> p c t", t=CH)
                                k.cp("pool", (cd[z], cd[z][:, blk * CB:(blk + 1) * CB]), (t_e, e3[:, :, end]))
                                k.act(V(t_f), V(t_q), AF.Silu)
                                k.stt("dve", (q_in[z], q_in[z][:, tsl]), V(t_f), 128 ** -0.5, V(t_e), ALU.mult, ALU.mult)
                                k.tt("dve", V(t_k), V(t_k), V(t_s), ALU.mult)
                                k.cp("pool", (k_in[z], k_in[z][:, tsl]), V(t_k))
                                k.tt("pool", (k_end[z], k_end[z][:, tsl].rearrange("p (c t) -> p c t", t=CH)),
                                     (t_k, t_k.a().rearrange("p (c t) -> p c t", t=CH)),
                                     (t_e, e3[:, :, end:end + 1].to_broadcast([128, CB, CH])), ALU.mult)
                            for c4 in range(0, NCH, 4):
                                p_ = pst[(c4 // 4) % 2]
                                for j in range(4):
                                    c = c4 + j
                                    k.tr((p_, p_[:, j, :]), (k_end[z], k_end[z][:, c * CH:(c + 1) * CH]), V(identb))
                                k.cp("act", (kET[z], kET[z][:, c4:c4 + 4, :]), V(p_))
                        k.memset("pool", V(oacc), 0.0)
                        for z in range(2):
                            k.memset("pool", V(S[z]), 0.0)
                            k.memset("pool", V(Sb[z]), 0.0)
                        orders = [chunk_order(0), chunk_order(1)]
                        for step in range(NCH):
                            for z in range(2):
                                c = orders[z][step]
                                csl = slice(c * CH, (c + 1) * CH)
                                pa = psA[z]; po = pso[z]; pk = pskv[z]; am = Am[(step % 2) * 2 + z]
                                k.mm(V(pa), (k_in[z], k_in[z][:, csl]), (q_in[z], q_in[z][:, csl]))
                                k.tt("dve", V(am), V(pa), V(masks[z]), ALU.mult)
                                k.mm(V(po), V(am), (vb, vb[:, c, :]), start=True, stop=False)
                                k.mm(V(po), (q_in[z], q_in[z][:, csl]), V(Sb[z]), start=False, stop=True)
                                k.tt("dve", (oacc, oacc[:, c, :], c), (oacc, oacc[:, c, :], c), V(po), ALU.add)
                                k.mm(V(pk), (kET[z], kET[z][:, c, :]), (vb, vb[:, c, :]))
                                k.stt("dve", V(S[z]), V(S[z]), (cd[z], cd[z][:, c:c + 1]), V(pk), ALU.mult, ALU.add)
                                k.cp("act", V(Sb[z]), V(S[z]))
                    P.barrier()
                    with ExitStack() as hs2:
                        gt = P.sb("gt", [64, NCH, 128], F32, es=hs2)
                        k.tt("dve", V(gt), V(oacc), V(oacc), ALU.mult)
                        P.op("dve", lambda e: e.tensor_reduce(out=ssn.a(), in_=gt.a(), axis=AX.X, op=ALU.add), reads=[gt], writes=[ssn])
                        k.ts("dve", V(ssn), V(ssn), 1.0 / 128, ALU.mult, EPS, ALU.add)
                        k.act(V(ssn), V(ssn), AF.Sqrt)
                        k.recip(V(ssn), V(ssn))
                        k.tt("dve", V(oacc), V(oacc), (ssn, ssn.a().rearrange("p (c o) -> p c o", o=1).to_broadcast([64, NCH, 128])), ALU.mult)
                        k.tt("pool", V(oacc), V(oacc), (nwB, nwB[:, h * 128:(h + 1) * 128].rearrange("p (o d) -> p o d", o=1).to_broadcast([64, NCH, 128])), ALU.mult)
                        k.dma(V(gt), (PT, PT[:, 512 + h * 128:512 + (h + 1) * 128].rearrange("(c p) d -> p c d", p=CH)))
                        k.act(V(gt), V(gt), AF.Silu)
                        k.tt("dve", V(oacc), V(oacc), V(gt), ALU.mult)
                        k.dma((YM, YM[:, 512 + h * 128:512 + (h + 1) * 128].rearrange("(c p) d -> p c d", p=CH), ("h", h)), V(oacc))
                    P.barrier()
                P.barrier()

        def rwkv_shift(PF, PR):
            with ExitStack() as ph:
                mu = P.sb("mu", [128, 15], F32, es=ph)
                omu = P.sb("omu", [128, 15], F32, es=ph)
                hmu = P.sb("hmu", [128, 15], F32, es=ph)
                k.dma(V(mu), V(I["rwkv_mu"], I["rwkv_mu"][0].rearrange("(b p) -> p b", p=128)), allow_slow_non_contiguous=True)
                k.ts("dve", V(omu), V(mu), -1.0, ALU.mult, 1.0, ALU.add)
                k.ts("dve", V(hmu), V(mu), 0.5, ALU.mult)
                pt = [P.sb(f"shp{i}", [128, L + 2], F32, es=ph) for i in range(2)]
                sm = [P.sb(f"shs{i}", [128, L], F32, es=ph) for i in range(2)]
                for i in range(2):
                    k.memset("pool", (pt[i], pt[i][:, 0:1], "h0"), 0.0)
                    k.memset("pool", (pt[i], pt[i][:, L + 1:L + 2], "h1"), 0.0)
                for b in range(15):
                    p_ = pt[b % 2]; s_ = sm[b % 2]
                    k.dma((p_, p_[:, 1:L + 1], "m"), (PF, PF[b * 128:(b + 1) * 128, :]))
                    k.tt("dve", (s_, s_.a(), "a"), V(p_), (p_, p_[:, 2:L + 2]), ALU.add) if False else None
                    P.op("dve", lambda e, p_=p_, s_=s_: e.tensor_tensor(out=s_.a(), in0=p_[:, 0:L], in1=p_[:, 2:L + 2], op=ALU.add),
                         reads=[p_], writes=[s_])
                    k.cp("dve", (s_, s_[:, 255:256]), (p_, p_[:, 255:256]))
                    k.cp("dve", (s_, s_[:, 256:257]), (p_, p_[:, 258:259]))
                    k.ts("pool", V(s_), V(s_), (hmu, hmu[:, b:b + 1]), ALU.mult)
                    k.stt("dve", V(s_), (p_, p_[:, 1:L + 1]), (omu, omu[:, b:b + 1]), V(s_), ALU.mult, ALU.add)
                    k.dma((PR, PR[b * 128:(b + 1) * 128, :], b), V(s_))
                P.barrier()

        def rwkv_gate(PR, GT):
            with ExitStack() as ph:
                gd = P.sb("gd", [128, L], F32, es=ph)
                gs = P.sb("gs", [128, L], BF16, es=ph)
                g2f = P.sb("g2f", [128, 512], F32, es=ph)
                g2b = P.sb("g2b", [128, 512], BF16, es=ph)
                pg = [P.ps(f"pg{i}", [128, 512], F32, es=ph) for i in range(2)]
                sg = [P.sb(f"sg{i}", [128, 512], F32, es=ph) for i in range(2)]
                k.dma(V(gd), (PR, PR[1792:1920, :]))
                k.dma(V(g2f), V(I["rwkv_g2"], I["rwkv_g2"][0]))
                k.cp("dve", V(g2b), V(g2f))
                for q4 in range(4):
                    k.act((gs, gs[:, q4 * 1088:(q4 + 1) * 1088], q4), (gd, gd[:, q4 * 1088:(q4 + 1) * 1088]), AF.Sigmoid)
                for tt in range(NT if "gate_nomm" not in debug else 0):
                    k.mm(V(pg[tt % 2]), (gs, gs[:, tt * 128:(tt + 1) * 128]), V(g2b))
                    k.cp("dve" if tt % 2 else "act", V(sg[tt % 2]), V(pg[tt % 2]))
                    k.dma((GT, GT[tt * 128:(tt + 1) * 128, :], tt), V(sg[tt % 2]))
                P.barrier()

        def mixer_rwkv(PR, GT, YM):
            BW = 256
            NB = L // BW
            CB = BW // CH
            NL = 5
            with ExitStack() as ph:
                w2all = P.sb("w2all", [128, 512], F32, es=ph)
                a2all = P.sb("a2all", [128, 512], F32, es=ph)
                k.dma(V(w2all), V(I["rwkv_w2"], I["rwkv_w2"][0].rearrange("z r c -> (z r) c")))
                k.dma(V(a2all), V(I["rwkv_a2"], I["rwkv_a2"][0].rearrange("z r c -> (z r) c")))
                def hv(name, src):
                    t = P.sb(name, [64, 8], F32, es=ph)
                    k.dma(V(t), (src[0], src[1].rearrange("(h n) -> n h", n=64)), allow_slow_non_contiguous=True)
                    return t
                w0 = [hv(f"w0_{z}", (I["rwkv_w0"], I["rwkv_w0"][0, z])) for z in range(2)]
                a0 = [hv(f"a0_{z}", (I["rwkv_a0"], I["rwkv_a0"][0, z])) for z in range(2)]
                kkg = hv("kkg", (I["rwkv_k_k"], I["rwkv_k_k"][0]))
                kag = hv("kag", (I["rwkv_k_a"], I["rwkv_k_a"][0]))
                rkg = hv("rkg", (I["rwkv_r_k"], I["rwkv_r_k"][0]))
                oka = P.sb("oka", [64, 8], F32, es=ph)
                k.ts("dve", V(oka), V(kag), -1.0, ALU.mult, 1.0, ALU.add)
                lnw = P.sb("lnw", [64, 512], F32, es=ph)
                lnb = P.sb("lnb", [64, 512], F32, es=ph)
                k.dma(V(lnw), V(I["rwkv_ln_w"], I["rwkv_ln_w"][0].partition_broadcast(64)))
                k.dma(V(lnb), V(I["rwkv_ln_b"], I["rwkv_ln_b"][0].partition_broadcast(64)))
                rst = P.sb("rst", [64, BW], F32, es=ph)
                k.memset("pool", V(rst), 1.0)
                k.memset("pool", (rst, rst.a().rearrange("p (c t) -> p c t", t=CH)[:, :, 0:1]), 0.0)
                ones64 = P.sb("ones64", [64, 64], F32, es=ph)
                k.memset("pool", V(ones64), 1.0)
                m4 = []
                m3 = []
                for z in range(2):
                    m = P.sb(f"m4_{z}", [128, 2, 64], F32, es=ph)
                    for half in range(2):
                        for col in range(2):
                            sgn = 1 if z == 0 else -1
                            base = (-1 if col == 0 else 0)
                            P.op("pool", lambda e, m=m, half=half, col=col, sgn=sgn, base=base: e.affine_select(
                                out=m[half * 64:(half + 1) * 64, col, :], in_=ones[half * 64:(half + 1) * 64, 0:64],
                                pattern=[[sgn, 64]], compare_op=ALU.is_ge, fill=0.0, base=base, channel_multiplier=-sgn),
                                reads=[ones], writes=[m])
                    m4.append(m)
                    mm3 = P.sb(f"m3_{z}", [64, 64], F32, es=ph)
                    P.op("pool", lambda e, mm3=mm3, z=z: e.affine_select(
                        out=mm3.a(), in_=ones[0:64, 0:64], pattern=[[-1 if z == 0 else 1, 64]], compare_op=ALU.is_ge, fill=0.0,
                        base=-1, channel_multiplier=1 if z == 0 else -1), reads=[ones], writes=[mm3])
                    m3.append(mm3)
                QI0 = P.sb("QI0", [64, 64], BF16, es=ph)
                k.cp("dve", V(QI0), (ident, ident[0:64, 0:64]))

                nheads = 0 if "rw1" in debug else (1 if ("rw2" in debug or "rw3" in debug) else 8)
                for h in range(nheads):
                    with ExitStack() as hs:
                        Vt = P.sb("Vt", [64, NCH, CH], F32, es=hs)
                        oacc = P.sb("oaccr", [64, NCH, CH], F32, es=hs)
                        bon = P.sb("bon", [64, NCH], F32, es=hs)
                        hs2 = ExitStack()
                        AR = [P.sb(f"AR{z}", [64, NCH, 2, CH], BF16, es=hs2) for z in range(2)]
                        BK = [P.sb(f"BK{z}", [64, NCH, 2, CH], BF16, es=hs2) for z in range(2)]
                        BKeT = [P.sb(f"BKeT{z}", [128, NCH, CH], BF16, es=hs2) for z in range(2)]
                        UV = [P.sb(f"UV{z}", [128, NCH, CH], BF16, es=hs2) for z in range(2)]
                        ZV = P.sb("ZV", [128, NCH, CH], BF16, es=hs2)
                        gC = [P.sb(f"gC{z}", [64, NCH], F32, es=hs2) for z in range(2)]
                        k.memset("pool", (ZV, ZV[0:64, :, :], "z"), 0.0)
                        with ExitStack() as pp_:
                            def T(name, dt=F32, w=BW):
                                return P.sb(name, [64, w], dt, es=pp_)
                            t_r = T("t_r"); t_k = T("t_k"); t_v = T("t_v")
                            t_wd = P.sb("t_wd", [128, BW], F32, es=pp_)
                            t_ad = P.sb("t_ad", [128, BW], F32, es=pp_)
                            t_kk = T("t_kk"); t_q = T("t_q"); t_rn = T("t_rn")
                            t_sg = T("t_sg"); t_cs = T("t_cs"); t_x = T("t_x"); t_y = T("t_y")
                            t_eg = T("t_eg"); t_eng = T("t_eng"); t_egp = T("t_egp")
                            t_a = T("t_a"); t_km = [T("t_km0"), T("t_km1")]; t_b = T("t_b")
                            bke = P.sb("bke", [64, CB, 2, CH], BF16, es=pp_)
                            t_v2 = P.sb("t_v2", [64, CB, 2, CH], F32, es=pp_)
                            pwa = [P.ps(f"pwa{i}", [64, BW], F32, es=pp_) for i in range(2)]
                            pss = P.ps("pss", [64, BW], F32, es=pp_)
                            ptv = P.ps("ptv", [128, CB, CH], F32, es=pp_)
                            ptb = P.ps("ptb", [128, CB, CH], BF16, es=pp_)
                            pbn = P.ps("pbn", [64, NCH], F32, es=pp_)
                            for blk in range(NB):
                                tsl = slice(blk * BW, (blk + 1) * BW)
                                csl = slice(blk * CB, (blk + 1) * CB)
                                k.dma(V(t_r), (PR, PR[h * 64:(h + 1) * 64, tsl]))
                                k.dma(V(t_k), (PR, PR[512 + h * 64:512 + (h + 1) * 64, tsl]))
                                k.dma(V(t_v), (PR, PR[1024 + h * 64:1024 + (h + 1) * 64, tsl]))
                                k.dma(V(t_wd), (PR, PR[1536:1664, tsl]))
                                k.dma(V(t_ad), (PR, PR[1664:1792, tsl]))
                                k.act(V(t_wd), V(t_wd), AF.Tanh)
                                k.cp("pool", V(t_v2), (t_v, t_v.a().rearrange("p (c o t) -> p c o t", o=1, t=CH).to_broadcast([64, CB, 2, CH])))
                                for j in range(CB):
                                    k.tr((ptv, ptv[:, j, :]), (t_v2, t_v2[:, j, :, :].rearrange("p a t -> p (a t)")), (ident, ident[0:64, 0:64]))
                                k.cp("act", (Vt, Vt[:, csl, :], blk), (ptv, ptv[0:64, :, :]))
                                for z in range(2):
                                    k.cp("dve", (UV[z], UV[z][64:128, csl, :], ("v", blk)), (ptv, ptv[64:128, :, :]))
                                k.cp("dve", (ZV, ZV[64:128, csl, :], ("v", blk)), (ptv, ptv[64:128, :, :]))
                                k.ts("dve", V(t_kk), V(t_k), (kkg, kkg[:, h:h + 1]), ALU.mult)
                                k.tt("pool", V(t_q), V(t_kk), V(t_kk), ALU.mult)
                                k.mm(V(pss), V(ones64), V(t_q))
                                k.ts("dve", V(t_rn), V(pss), 1e-12, ALU.add)
                                k.act(V(t_rn), V(t_rn), AF.Sqrt)
                                k.recip(V(t_rn), V(t_rn))
                                k.tt("dve", V(t_kk), V(t_kk), V(t_rn), ALU.mult)
                                for z in range(2):
                                    end = CH - 1 if z == 0 else 0
                                    zs = slice(z * 64, (z + 1) * 64)
                                    pw = pwa[0]; pa = pwa[1]
                                    k.mm(V(pw), (w2all, w2all[zs, h * 64:(h + 1) * 64]), (t_wd, t_wd[zs, :]))
                                    k.mm(V(pa), (a2all, a2all[zs, h * 64:(h + 1) * 64]), (t_ad, t_ad[zs, :]))
                                    k.act(V(t_sg), V(pw), AF.Sigmoid, bias=(w0[z], w0[z][:, h:h + 1]))
                                    k.act(V(t_a), V(pa), AF.Sigmoid, bias=(a0[z], a0[z][:, h:h + 1]))
                                    k.scan(V(t_cs), V(rst), V(t_sg), 0.0, ALU.mult, ALU.add)
                                    if z == 1:
                                        k.tt("dve", V(t_x), V(t_sg), V(t_cs), ALU.subtract)
                                        c3 = t_cs.a().rearrange("p (c t) -> p c t", t=CH)
                                        k.tt("dve", (t_y, t_y.a().rearrange("p (c t) -> p c t", t=CH)),
                                             (t_x, t_x.a().rearrange("p (c t) -> p c t", t=CH)),
                                             (t_cs, c3[:, :, CH - 1:CH].to_broadcast([64, CB, CH])), ALU.add)
                                        cs = t_y
                                    else:
                                        cs = t_cs
                                    k.act(V(t_eg), V(cs), AF.Exp, scale=-0.6065306597126334)
                                    k.act(V(t_eng), V(cs), AF.Exp, scale=0.6065306597126334)
                                    k.tt("dve", V(t_x), V(cs), V(t_sg), ALU.subtract)
                                    k.act(V(t_egp), V(t_x), AF.Exp, scale=-0.6065306597126334)
                                    eg3 = t_eg.a().rearrange("p (c t) -> p c t", t=CH)
                                    k.cp("pool", (gC[z], gC[z][:, csl], blk), (t_eg, eg3[:, :, end]))
                                    k.ts("dve", V(t_x), V(t_a), (kag, kag[:, h:h + 1]), ALU.mult, (oka, oka[:, h:h + 1]), ALU.add)
                                    k.tt("dve", V(t_km[z]), V(t_k), V(t_x), ALU.mult)
                                    k.tt("pool", V(t_b), V(t_kk), V(t_a), ALU.mult)
                                    arz = AR[z]; bkz = BK[z]
                                    k.stt("dve", (arz, arz[:, csl, 0, :], blk), (t_kk, t_kk.a().rearrange("p (c t) -> p c t", t=CH)), -1.0,
                                          (t_egp, t_egp.a().rearrange("p (c t) -> p c t", t=CH)), ALU.mult, ALU.mult)
                                    k.tt("pool", (arz, arz[:, csl, 1, :], blk), (t_r, t_r.a().rearrange("p (c t) -> p c t", t=CH)),
                                         (t_eg, eg3), ALU.mult)
                                    k.tt("dve", V(t_b), V(t_b), V(t_eng), ALU.mult)
                                    k.tt("dve", V(t_x), V(t_km[z]), V(t_eng), ALU.mult)
                                    k.cp("pool", (bkz, bkz[:, csl, 0, :], blk), (t_b, t_b.a().rearrange("p (c t) -> p c t", t=CH)))
                                    k.cp("act", (bkz, bkz[:, csl, 1, :], blk), (t_x, t_x.a().rearrange("p (c t) -> p c t", t=CH)))
                                    gcb = eg3[:, :, end:end + 1].to_broadcast([64, CB, CH])
                                    k.tt("dve", (bke, bke[:, :, 0, :]), (t_b, t_b.a().rearrange("p (c t) -> p c t", t=CH)), (t_eg, gcb), ALU.mult)
                                    k.tt("pool", (bke, bke[:, :, 1, :]), (t_x, t_x.a().rearrange("p (c t) -> p c t", t=CH)), (t_eg, gcb), ALU.mult)
                                    for j in range(CB):
                                        k.tr((ptb, ptb[:, j, :]), (bke, bke[:, j, :, :].rearrange("p a t -> p (a t)")), (identb, identb[0:64, 0:64]))
                                    k.cp("act", (BKeT[z], BKeT[z][:, csl, :], blk), V(ptb))
                                k.tt("dve", V(t_x), V(t_km[0]), V(t_km[1]), ALU.add)
                                k.stt("dve", V(t_x), V(t_r), (rkg, rkg[:, h:h + 1]), V(t_x), ALU.mult, ALU.mult)
                                for j in range(CB):
                                    c = blk * CB + j
                                    k.mm((pbn, pbn[:, c:c + 1]), (t_x, t_x[:, j * CH:(j + 1) * CH]), (ones64, ones64[:, 0:1]))
                            k.cp("dve", V(bon), V(pbn))
                        P.barrier()
                        with ExitStack() as us:
                            Hs = [P.sb(f"Hs{z}", [64, 64], F32, es=us) for z in range(2)]
                            Hb = [P.sb(f"Hb{z}", [64, 64], BF16, es=us) for z in range(2)]
                            AM = [[P.sb(f"AM{z}{i}", [128, 128], BF16, es=us) for i in range(2)] for z in range(2)]
                            Pm = [[P.sb(f"Pm{z}{i}", [64, 64], BF16, es=us) for i in range(2)] for z in range(2)]
                            QR = [[P.sb(f"QR{z}{i}", [64, 2, 64], BF16, es=us) for i in range(2)] for z in range(2)]
                            TT = [[P.sb(f"TT{z}{i}", [64, 64], BF16, es=us) for i in range(2)] for z in range(2)]
                            Xs = [P.sb(f"Xs{z}", [64, 64], BF16, es=us) for z in range(2)]
                            bA = [P.ps(f"bA{z}", [128, 512], F32, es=us) for z in range(2)]
                            bB = [P.ps(f"bB{z}", [64, 128], F32, es=us) for z in range(2)]
                            bC = [P.ps(f"bC{z}", [64, 64], F32, es=us) for z in range(2)]
                            bD = [P.ps(f"bD{z}", [64, 128], F32, es=us) for z in range(2)]
                            vM = [bA[z][:, 0:128] for z in range(2)]
                            vX = [bA[z][0:64, 128:192] for z in range(2)]
                            vU = [bA[z][0:64, 192:256] for z in range(2)]
                            vL = [bB[z].a() for z in range(2)]
                            vP = [bC[z].a() for z in range(2)]
                            vO = [bD[z][:, 0:64] for z in range(2)]
                            vH = [bD[z][:, 64:128] for z in range(2)]
                            pM = [(bA[z], vM[z]) for z in range(2)]
                            pL = [(bB[z], vL[z]) for z in range(2)]
                            pP = [(bC[z], vP[z]) for z in range(2)]
                            pX = [(bA[z], vX[z]) for z in range(2)]
                            pU = [(bA[z], vU[z]) for z in range(2)]
                            pO = [(bD[z], vO[z]) for z in range(2)]
                            pH = [(bD[z], vH[z]) for z in range(2)]
                            k.memset("pool", V(oacc), 0.0)
                            for z in range(2):
                                k.memset("pool", V(Hs[z]), 0.0)
                                k.memset("pool", V(Hb[z]), 0.0)
                            orders = [chunk_order(0), chunk_order(1)]
                            for step in range(NCH if "rw2" not in debug else 0):
                                for z in range(2):
                                    c = orders[z][step]
                                    am = AM[z][step % 2]
                                    arc = AR[z][:, c, :, :].rearrange("p a t -> p (a t)")
                                    bkc = BK[z][:, c, :, :].rearrange("p a t -> p (a t)")
                                    k.mm(pM[z], (BK[z], bkc), (AR[z], arc))
                                    k.tt("dve", V(am), pM[z], (m4[z], m4[z].a().rearrange("p a t -> p (a t)")), ALU.mult)
                                    k.mm(pP[z], (AR[z], AR[z][:, c, 0, :]), (BK[z], BK[z][:, c, 0, :]))
                                    pm_ = Pm[z][0]
                                    k.tt("pool" if False else "dve", V(pm_), pP[z], V(m3[z]), ALU.mult)
                                    if "u1" in debug:
                                        continue
                                    qr = QR[z][0]
                                    k.cp("act", (qr, qr[:, 0, :]), (am, am[0:64, 0:64]))
                                    k.cp("pool", (qr, qr[:, 1, :]), V(QI0))
                                    for lv in range(1, NL + 1):
                                        qn = QR[z][lv % 2]
                                        pn = Pm[z][lv % 2]
                                        k.mm(pL[z], V(pm_), (qr, qr.a().rearrange("p a t -> p (a t)")))
                                        k.mm(pP[z], (qr, qr[:, 0, :]), V(pm_))
                                        k.cp("act", (qn, qn[:, 0, :]), (bB[z], vL[z][:, 0:64]))
                                        k.tt("dve", (qn, qn[:, 1, :]), (bB[z], vL[z][:, 64:128]), (qr, qr[:, 1, :]), ALU.add)
                                        k.cp("act", V(pn), pP[z])
                                        qr = qn; pm_ = pn
                                    tt_ = TT[z][step % 2]
                                    k.mm((bB[z], vL[z][:, 0:64]), V(pm_), (qr, qr[:, 1, :]))
                                    k.tt("dve", V(tt_), (bB[z], vL[z][:, 0:64]), (qr, qr[:, 1, :]), ALU.add)
                                    if "u2" in debug:
                                        continue
                                    k.mm(pX[z], (AR[z], AR[z][:, c, 0, :]), V(Hb[z]), start=True, stop=False)
                                    P.op("pe", lambda e, px=vX[z], am=am, c=c: e.matmul(px, lhsT=am[:, 0:64], rhs=ZV[:, c, :], start=False, stop=True),
                                         reads=[(am, None), (ZV, "z"), (ZV, ("v", c // CB))], writes=[(bA[z], None)], pe_acc=True)
                                    k.cp("act", V(Xs[z]), pX[z])
                                    k.mm(pU[z], V(tt_), V(Xs[z]))
                                    k.cp("dve", (UV[z], UV[z][0:64, c, :], ("u", c)), pU[z])
                                    if "u3" in debug:
                                        continue
                                    k.mm(pO[z], (AR[z], AR[z][:, c, 1, :]), V(Hb[z]), start=True, stop=False)
                                    P.op("pe", lambda e, po=vO[z], am=am, uv=UV[z], c=c: e.matmul(po, lhsT=am[:, 64:128], rhs=uv[:, c, :], start=False, stop=True),
                                         reads=[(am, None), (UV[z], ("u", c)), (UV[z], ("v", c // CB))], writes=[(bD[z], None)], pe_acc=True)
                                    k.tt("dve", (oacc, oacc[:, c, :], c), (oacc, oacc[:, c, :], c), pO[z], ALU.add)
                                    P.op("pe", lambda e, ph_=vH[z], bt=BKeT[z], uv=UV[z], c=c: e.matmul(ph_, lhsT=bt[:, c, :], rhs=uv[:, c, :], start=True, stop=True),
                                         reads=[(BKeT[z], None), (UV[z], ("u", c)), (UV[z], ("v", c // CB))], writes=[(bD[z], None)])
                                    k.stt("dve", V(Hs[z]), V(Hs[z]), (gC[z], gC[z][:, c:c + 1]), pH[z], ALU.mult, ALU.add)
                                    k.cp("act", V(Hb[z]), V(Hs[z]))
                        if "d_oacc" in debug and h == 0:
                            do = P.dram("d_oacc", [64, NCH * CH], F32, kind="ExternalOutput")
                            k.dma(V(do), (oacc, oacc.a().rearrange("p a b -> p (a b)")))
                            dm4 = P.dram("d_m4", [128, 2 * 128], F32, kind="ExternalOutput")
                            for z in range(2):
                                k.dma((dm4, dm4[:, z * 128:(z + 1) * 128]), (m4[z], m4[z].a().rearrange("p a b -> p (a b)")))
                            dm3 = P.dram("d_m3", [64, 2 * 64], F32, kind="ExternalOutput")
                            for z in range(2):
                                k.dma((dm3, dm3[:, z * 64:(z + 1) * 64]), V(m3[z]))
                            dgc = P.dram("d_gC", [64, 2 * NCH], F32, kind="ExternalOutput")
                            for z in range(2):
                                k.dma((dgc, dgc[:, z * NCH:(z + 1) * NCH]), V(gC[z]))
                            dbon = P.dram("d_bon", [64, NCH], F32, kind="ExternalOutput")
                            k.dma(V(dbon), V(bon))
                            dar = P.dram("d_AR", [64, 2 * NCH * 2 * CH], BF16, kind="ExternalOutput")
                            dbk = P.dram("d_BK", [64, 2 * NCH * 2 * CH], BF16, kind="ExternalOutput")
                            for z in range(2):
                                k.dma((dar, dar[:, z * NCH * 128:(z + 1) * NCH * 128]), (AR[z], AR[z].a().rearrange("p a b c -> p (a b c)")))
                                k.dma((dbk, dbk[:, z * NCH * 128:(z + 1) * NCH * 128]), (BK[z], BK[z].a().rearrange("p a b c -> p (a b c)")))
                        P.barrier()
                        hs2.close()
                        with ExitStack() as fs:
                            gt = P.sb("gtr", [64, NCH, CH], F32, es=fs)
                            cen = P.sb("cen", [64, NCH, CH], F32, es=fs)
                            mu_ = P.sb("mu_", [64, NCH], F32, es=fs)
                            var = P.sb("var", [64, NCH], F32, es=fs)
                            k.dma(V(gt), (GT, GT[:, h * 64:(h + 1) * 64].rearrange("(c p) d -> p c d", p=CH)))
                            P.op("dve", lambda e: e.tensor_reduce(out=mu_.a(), in_=oacc.a(), axis=AX.X, op=ALU.add), reads=[oacc], writes=[mu_])
                            k.ts("dve", V(mu_), V(mu_), 1.0 / 64, ALU.mult)
                            b3 = lambda t: t.a().rearrange("p (c o) -> p c o", o=1).to_broadcast([64, NCH, CH])
                            k.tt("dve", V(oacc), V(oacc), (mu_, b3(mu_)), ALU.subtract)
                            k.tt("pool", V(cen), V(oacc), V(oacc), ALU.mult)
                            P.op("dve", lambda e: e.tensor_reduce(out=var.a(), in_=cen.a(), axis=AX.X, op=ALU.add), reads=[cen], writes=[var])
                            k.ts("dve", V(var), V(var), 1.0 / 64, ALU.mult, 64e-5, ALU.add)
                            k.act(V(var), V(var), AF.Sqrt)
                            k.recip(V(var), V(var))
                            k.tt("dve", V(oacc), V(oacc), (var, b3(var)), ALU.mult)
                            rb = lambda t: t[:, h * 64:(h + 1) * 64].rearrange("p (o d) -> p o d", o=1).to_broadcast([64, NCH, CH])
                            k.tt("pool", V(oacc), V(oacc), (lnw, rb(lnw)), ALU.mult)
                            k.tt("dve", V(oacc), V(oacc), (lnb, rb(lnb)), ALU.add)
                            k.tt("pool", V(cen), V(Vt), (bon, b3(bon)), ALU.mult)
                            k.tt("dve", V(oacc), V(oacc), V(cen), ALU.add)
                            k.tt("dve", V(oacc), V(oacc), V(gt), ALU.mult)
                            k.dma((YM, YM[:, h * 64:(h + 1) * 64].rearrange("(c p) d -> p c d", p=CH), ("r", h)), V(oacc))
                        P.barrier()

        def lat_rows(tt, cc):
            c0 = 2 * (tt - 2) + cc
            return XL[LC:, :].rearrange("(r c) d -> c r d", c=64)[c0]

        def stage_outproj(YM, nfeat, W, xsrc, perm, tiles):
            kf = nfeat // 128
            with ExitStack() as ph:
                wb = P.sb("wob", [128, kf, D], BF16, es=ph)
                k.dma(V(wb), (W[0], W[1].rearrange("(ko p) n -> p ko n", p=128)), eng="pool")
                yt = [P.sb(f"yt{i}", [128, nfeat], F32, es=ph) for i in range(2)]
                yT = [P.sb(f"yT{i}", [128, kf, 128], BF16, es=ph) for i in range(2)]
                xo = [P.sb(f"xo{i}", [128, D], F32, es=ph) for i in range(2)]
                xn_ = [P.sb(f"xq{i}", [128, D], F32, es=ph) for i in range(2)]
                pT = [P.ps(f"poT{i}", [128, 8, 128], F32, es=ph) for i in range(2)]
                po = [P.ps(f"poo{i}", [128, 512], F32, es=ph) for i in range(2)]
                for n_, tt in enumerate(tiles):
                    j = 1 if tt < 2 else 0
                    y_ = yt[n_ % 2]; yT_ = yT[n_ % 2]; x_ = xo[n_ % 2]; q_ = xn_[n_ % 2]
                    k.dma(V(y_), (YM, YM[tt * 128:(tt + 1) * 128, :]))
                    if perm and tt >= 2:
                        for cc in range(2):
                            k.dma((x_, x_[cc * 64:(cc + 1) * 64, :], cc), (xsrc(tt)[0], lat_rows(tt, cc), tt))
                    else:
                        sb_, sap = xsrc(tt)
                        k.dma(V(x_), (sb_, sap, tt))
                    for g8 in range(0, kf, 8):
                        p_ = pT[(g8 // 8 + n_) % 2]
                        for ko in range(8):
                            k.tr((p_, p_[:, ko, :]), (y_, y_[:, (g8 + ko) * 128:(g8 + ko + 1) * 128]), V(ident))
                        k.cp("act", (yT_, yT_[:, g8:g8 + 8, :]), V(p_))
                    for nb in range(2):
                        o_ = po[nb]
                        for ko in range(kf):
                            k.mm(V(o_), (yT_, yT_[:, ko, :]), (wb, wb[:, ko, nb * 512:(nb + 1) * 512]), start=(ko == 0), stop=(ko == kf - 1))
                        k.tt("dve", (q_, q_[:, nb * 512:(nb + 1) * 512]), V(o_), (modB, modB[:, 0, j, nb * 512:(nb + 1) * 512]), ALU.mult)
                    k.tt("pool", V(q_), V(q_), V(x_), ALU.add)
                    if perm and tt >= 2:
                        for cc in range(2):
                            k.dma((XL, lat_rows(tt, cc), tt), (q_, q_[cc * 64:(cc + 1) * 64, :]))
                    else:
                        k.dma((XL, XL[tt * 128:(tt + 1) * 128, :], tt), V(q_))
                P.barrier()

        def stage_moe(l, tiles):
            nh = len(tiles) // 2
            with ExitStack() as ph:
                rw32 = P.sb("rw32", [128, KO, 32], F32, es=ph)
                k.dma(V(rw32), V(I["router_w"], I["router_w"][l].rearrange("(ko p) e -> p ko e", p=128)))
                rbB = P.sb("rbB", [128, 32], F32, es=ph)
                k.dma(V(rbB), V(I["router_b"], I["router_b"][l].partition_broadcast(128)))
                BD = P.sb("BD", [32, D], F32, es=ph)
                k.dma(V(BD), V(I["exp_b_down"], I["exp_b_down"][l]))
                bgF = P.sb("bgF", [128, 32, 8], F32, es=ph)
                buF = P.sb("buF", [128, 32, 8], F32, es=ph)
                with ExitStack() as t0:
                    btmp = P.sb("btmp", [128, 128], F32, es=t0)
                    pbt = P.ps("pbt", [128, 128], F32, es=t0)
                    for (dst, src) in ((bgF, I["exp_b_gate"]), (buF, I["exp_b_up"])):
                        for hf in range(2):
                            k.dma(V(btmp), (src, src[l, hf * 16:(hf + 1) * 16, :].rearrange("e (fb p) -> (e fb) p", p=128)))
                            k.tr(V(pbt), V(btmp), V(ident))
                            k.cp("dve", (dst, dst[:, hf * 16:(hf + 1) * 16, :].rearrange("p e f -> p (e f)")), V(pbt))
                    P.barrier()
                for half in range(2):
                    htiles = tiles[half * nh:(half + 1) * nh]
                    NTK = nh * 128
                    with ExitStack() as hs:
                        HTh = P.sb("HTh", [128, KO, NTK], BF16, es=hs)
                        LG = P.sb("LG", [128, nh, 32], F32, es=hs)
                        GW = P.sb("GW", [128, nh, 32], F32, es=hs)
                        acc = P.sb("acc", [128, nh, D], F32, es=hs)
                        with ExitStack() as ns:
                            xt = [P.sb(f"mxt{i}", [128, D], F32, es=ns) for i in range(2)]
                            xn = [P.sb(f"mxn{i}", [128, D], F32, es=ns) for i in range(2)]
                            junk = P.sb("mjunk", [128, D], F32, es=ns)
                            st = [P.sb(f"mst{i}", [128, 4], F32, es=ns) for i in range(2)]
                            tmp = [P.sb(f"mtmp{i}", [128, KO, 128], F32, es=ns) for i in range(2)]
                            h32 = [P.sb(f"mh32{i}", [128, KO, 128], F32, es=ns) for i in range(2)]
                            pT = [P.ps(f"mpT{i}", [128, KO, 128], F32, es=ns) for i in range(2)]
                            plg = [P.ps(f"plg{i}", [128, 32], F32, es=ns) for i in range(2)]
                            pgt = P.ps("pgt", [32, 128], F32, es=ns)
                            GWT = P.sb("GWT", [32, nh, 128], F32, es=ns)
                            pini = [P.ps("pini0", [128, 512], F32, es=ns)] * 2
                            m8 = P.sb("m8", [128, 8], F32, es=ns)
                            msk = P.sb("msk", [128, 32], F32, es=ns)
                            ex = P.sb("ex", [128, 32], F32, es=ns)
                            sm = P.sb("smx", [128, 4], F32, es=ns)
                            for i, tt in enumerate(htiles):
                                j = 1 if tt < 2 else 0
                                x_ = xt[i % 2]; n_ = xn[i % 2]; s_ = st[i % 2]; t_ = tmp[i % 2]; p_ = pT[i % 2]; h_ = h32[i % 2]
                                k.dma(V(x_), (XL, XL[tt * 128:(tt + 1) * 128, :]))
                                k.memset("pool", (s_, s_[:, 0:1]), 0.0)
                                k.act(V(junk), V(x_), AF.Square, accum=(s_, s_[:, 0:1]))
                                k.ts("dve", (s_, s_[:, 1:2]), (s_, s_[:, 0:1]), 1.0 / D, ALU.mult, EPS, ALU.add)
                                k.act((s_, s_[:, 2:3]), (s_, s_[:, 1:2]), AF.Sqrt)
                                k.recip((s_, s_[:, 3:4]), (s_, s_[:, 2:3]))
                                k.ts("dve", V(n_), V(x_), (s_, s_[:, 3:4]), ALU.mult)
                                for ko in range(KO):
                                    k.tr((p_, p_[:, ko, :]), (n_, n_[:, ko * 128:(ko + 1) * 128]), V(ident))
                                k.tt("dve", V(t_), V(p_), (g2F, g2F[:, :, j:j + 1].to_broadcast([128, KO, 128])), ALU.mult)
                                k.tt("pool", V(h_), V(t_), (modF, modF[:, 3, :, j:j + 1].to_broadcast([128, KO, 128])), ALU.add)
                                k.cp("act", (HTh, HTh[:, :, i * 128:(i + 1) * 128], i), V(h_))
                                pl = plg[i % 2]
                                for ko in range(KO):
                                    k.mm(V(pl), (h_, h_[:, ko, :]), (rw32, rw32[:, ko, :]), start=(ko == 0), stop=(ko == KO - 1))
                                lg = (LG, LG[:, i, :], i)
                                k.tt("dve", lg, V(pl), V(rbB), ALU.add)
                                P.op("dve", lambda e, i=i: e.max(out=m8.a(), in_=LG[:, i, :]), reads=[(LG, i)], writes=[m8])
                                k.ts("dve", V(msk), lg, (m8, m8[:, 3:4]), ALU.is_ge)
                                k.ts("dve", (sm, sm[:, 0:1]), (m8, m8[:, 0:1]), -1.0, ALU.mult)
                                k.act(V(ex), lg, AF.Exp, bias=(sm, sm[:, 0:1]))
                                k.tt("dve", V(ex), V(ex), V(msk), ALU.mult)
                                P.op("dve", lambda e: e.tensor_reduce(out=sm[:, 1:2], in_=ex.a(), axis=AX.X, op=ALU.add), reads=[ex], writes=[sm])
                                k.recip((sm, sm[:, 2:3]), (sm, sm[:, 1:2]))
                                k.ts("dve", (GW, GW[:, i, :], i), V(ex), (sm, sm[:, 2:3]), ALU.mult)
                                k.tr(V(pgt), (GW, GW[:, i, :], i), V(ident))
                                k.cp("act", (GWT, GWT[:, i, :], i), V(pgt))
                                for nb in range(2):
                                    k.mm(V(pini[nb]), (GWT, GWT[:, i, :], i), (BD, BD[:, nb * 512:(nb + 1) * 512]))
                                    k.cp("act" if nb else "dve", (acc, acc[:, i, nb * 512:(nb + 1) * 512], i), V(pini[nb]))
                            P.barrier()
                        if f"LG{l}" in debug and half == 0:
                            dl = P.dram(f"d_GW{l}", [128, nh * 32], F32, kind="ExternalOutput")
                            k.dma(V(dl), (GW, GW.a().rearrange("p a b -> p (a b)")))
                        with ExitStack() as xs:
                            wg = P.sb("wg", [128, KO, D], BF16, es=xs)
                            wu = P.sb("wu", [128, KO, D], BF16, es=xs)
                            wd = P.sb("wd", [128, KO, D], BF16, es=xs)
                            aT = P.sb("aT", [128, KO, 512], BF16, es=xs)
                            dtmp = [P.sb(f"dtmp{i}", [128, 512], F32, es=xs) for i in range(2)]
                            g1 = [P.sb(f"g1_{i}", [128, 512], F32, es=xs) for i in range(2)]
                            sg = [P.sb(f"sg_{i}", [128, 512], F32, es=xs) for i in range(2)]
                            u1 = [P.sb(f"u1_{i}", [128, 512], F32, es=xs) for i in range(2)]
                            pg = [P.ps(f"mpg{i}", [128, 512], F32, es=xs) for i in range(2)]
                            pu = [P.ps(f"mpu{i}", [128, 512], F32, es=xs) for i in range(2)]
                            pd = [P.ps(f"mpd{i}", [128, 512], F32, es=xs) for i in range(2)]
                            tblocks = [(t0_, min(512, NTK - t0_)) for t0_ in range(0, NTK, 512)]
                            nexp = 32 if "moe_fast" not in debug else 2
                            cnt = 0
                            for e in range(nexp):
                                k.dma(V(wg), (I["exp_w_gate"], I["exp_w_gate"][l, e].rearrange("(ko p) n -> p ko n", p=128)), eng="pool")
                                k.dma(V(wu), (I["exp_w_up"], I["exp_w_up"][l, e].rearrange("(ko p) n -> p ko n", p=128)), eng="pool")
                                k.dma(V(wd), (I["exp_w_down"], I["exp_w_down"][l, e].rearrange("(ko p) n -> p ko n", p=128)), eng="pool")
                                for (t0_, tw) in tblocks:
                                    for fb in range(8):
                                        pg_ = pg[cnt % 2]; pu_ = pu[cnt % 2]; g_ = g1[cnt % 2]; s_ = sg[cnt % 2]; u_ = u1[cnt % 2]; cnt += 1
                                        for ko in range(KO):
                                            k.mm((pg_, pg_[:, 0:tw]), (wg, wg[:, ko, fb * 128:(fb + 1) * 128]), (HTh, HTh[:, ko, t0_:t0_ + tw]),
                                                 start=(ko == 0), stop=(ko == KO - 1))
                                        for ko in range(KO):
                                            k.mm((pu_, pu_[:, 0:tw]), (wu, wu[:, ko, fb * 128:(fb + 1) * 128]), (HTh, HTh[:, ko, t0_:t0_ + tw]),
                                                 start=(ko == 0), stop=(ko == KO - 1))
                                        k.ts("dve", (g_, g_[:, 0:tw]), (pg_, pg_[:, 0:tw]), (bgF, bgF[:, e, fb:fb + 1]), ALU.add, 7.0, ALU.min)
                                        k.act((s_, s_[:, 0:tw]), (g_, g_[:, 0:tw]), AF.Sigmoid, scale=1.702)
                                        k.act((u_, u_[:, 0:tw]), (pu_, pu_[:, 0:tw]), AF.Identity, bias=(buF, buF[:, e, fb:fb + 1]))
                                        k.ts("pool", (u_, u_[:, 0:tw]), (u_, u_[:, 0:tw]), 7.0, ALU.min, -7.0, ALU.max)
                                        k.tt("pool", (g_, g_[:, 0:tw]), (g_, g_[:, 0:tw]), (s_, s_[:, 0:tw]), ALU.mult)
                                        k.stt("dve", (aT, aT[:, fb, 0:tw], fb), (u_, u_[:, 0:tw]), 1.0, (g_, g_[:, 0:tw]), ALU.add, ALU.mult)
                                    for ti in range(tw // 128):
                                        i = t0_ // 128 + ti
                                        for nb in range(2):
                                            pd_ = pd[nb]
                                            for fo in range(8):
                                                k.mm(V(pd_), (aT, aT[:, fo, ti * 128:(ti + 1) * 128], fo), (wd, wd[:, fo, nb * 512:(nb + 1) * 512]),
                                                     start=(fo == 0), stop=(fo == 7))
                                            asl = (acc, acc[:, i, nb * 512:(nb + 1) * 512], i)
                                            if nb == 0:
                                                k.stt("dve", asl, V(pd_), (GW, GW[:, i, e:e + 1], i), asl, ALU.mult, ALU.add)
                                            else:
                                                dt_ = dtmp[i % 2]
                                                k.act(V(dt_), V(pd_), AF.Copy, scale=(GW, GW[:, i, e:e + 1], i))
                                                k.tt("pool", asl, asl, V(dt_), ALU.add)
                            P.barrier()
                        with ExitStack() as rs:
                            xr = [P.sb(f"xr{i}", [128, D], F32, es=rs) for i in range(2)]
                            for i, tt in enumerate(htiles):
                                j = 1 if tt < 2 else 0
                                x_ = xr[i % 2]
                                k.dma(V(x_), (XL, XL[tt * 128:(tt + 1) * 128, :], ("m", tt)))
                                k.tt("dve", (acc, acc[:, i, :], i), (acc, acc[:, i, :], i), (modB, modB[:, 1, j, :]), ALU.mult)
                                k.tt("pool", V(x_), V(x_), (acc, acc[:, i, :], i), ALU.add)
                                k.dma((XL, XL[tt * 128:(tt + 1) * 128, :], ("m", tt)), V(x_))
                            P.barrier()

        SEGS = ((0, LC), (LC, SEQ))

        def stage_conv(PF, PC, specs):
            with ExitStack() as ph:
                u = [P.sb(f"cu{i}", [128, L + 8], F32, es=ph) for i in range(2)]
                acc = [P.sb(f"ca{i}", [128, L], F32, es=ph) for i in range(2)]
                cw = P.sb("cw", [128, 20, 5], F32, es=ph)
                cb = P.sb("cb", [128, 20], F32, es=ph)
                for i in range(2):
                    k.memset("pool", (u[i], u[i][:, 0:2], "h0"), 0.0)
                    k.memset("pool", (u[i], u[i][:, LC + 2:LC + 6], "h1"), 0.0)
                    k.memset("pool", (u[i], u[i][:, L + 6:L + 8], "h2"), 0.0)
                bi = 0
                for (row0, nblk, wsrc, bsrc) in specs:
                    for b in range(nblk):
                        k.dma((cw, cw[:, bi, :], bi), (wsrc[0], wsrc[1][:, b * 128:(b + 1) * 128].rearrange("j p -> p j")), allow_slow_non_contiguous=True)
                        k.dma((cb, cb[:, bi:bi + 1], bi), (bsrc[0], bsrc[1][b * 128:(b + 1) * 128].rearrange("(p o) -> p o", o=1)), allow_slow_non_contiguous=True)
                        u_ = u[bi % 2]; a_ = acc[bi % 2]
                        r0 = row0 + b * 128
                        k.dma((u_, u_[:, 2:LC + 2], "c"), (PF, PF[r0:r0 + 128, 0:LC]))
                        k.dma((u_, u_[:, LC + 6:L + 6], "l"), (PF, PF[r0:r0 + 128, LC:L]))
                        for (s0, sl_) in SEGS:
                            off = 0 if s0 == 0 else 4
                            for j in range(5):
                                src = (u_, u_[:, s0 + off + j:s0 + off + j + sl_])
                                dst = (a_, a_[:, s0:s0 + sl_], s0)
                                if j == 0:
                                    k.ts("dve", dst, src, (cw, cw[:, bi, 0:1], bi), ALU.mult)
                                else:
                                    k.stt("dve", dst, src, (cw, cw[:, bi, j:j + 1], bi), dst, ALU.mult, ALU.add)
                            k.act((a_, a_[:, s0:s0 + sl_], s0), (a_, a_[:, s0:s0 + sl_], s0), AF.Silu, bias=(cb, cb[:, bi:bi + 1], bi))
                        k.dma((PC, PC[r0:r0 + 128, :], r0), V(a_))
                        bi += 1
                P.barrier()

        def mixer_mlstm(PC, PF, PT, YM, GSD):
            TB = [(t0_, min(512, L - t0_)) for t0_ in range(0, L, 512)]
            with ExitStack() as ph:
                GI = P.sb("GI", [8, L], F32, es=ph)
                GF = P.sb("GF", [8, L], F32, es=ph)
                t1 = P.sb("gt1", [8, L], F32, es=ph)
                t2 = P.sb("gt2", [8, L], F32, es=ph)
                t3 = P.sb("gt3", [8, L], F32, es=ph)
                rst = P.sb("grst", [8, L], F32, es=ph)
                ib = P.sb("ib", [8, 1], F32, es=ph)
                fb = P.sb("fb", [8, 1], F32, es=ph)
                k.dma(V(GI), (PF, PF[2560:2568, :]))
                k.dma(V(GF), (PF, PF[2568:2576, :]))
                k.dma(V(ib), V(I["mlstm_i_bias"], I["mlstm_i_bias"][0].rearrange("z (h o) -> (z h) o", o=1)), allow_slow_non_contiguous=True)
                k.dma(V(fb), V(I["mlstm_f_bias"], I["mlstm_f_bias"][0].rearrange("z (h o) -> (z h) o", o=1)), allow_slow_non_contiguous=True)
                k.memset("pool", V(rst), 1.0)
                k.memset("pool", (rst, rst.a().rearrange("p (c t) -> p c t", t=CH)[:, :, 0:1]), 0.0)
                k.ts("dve", V(GI), V(GI), (ib, ib[:, 0:1]), ALU.add)
                k.ts("dve", V(fb), V(fb), -1.0, ALU.mult)
                k.act(V(t1), V(GF), AF.Exp, bias=(fb, fb[:, 0:1]), scale=-1.0)
                k.act(V(t1), V(t1), AF.Ln, bias=1.0)
                k.ts("dve", V(t1), V(t1), -1.0, ALU.mult)
                k.scan(V(t2), V(rst), V(t1), 0.0, ALU.mult, ALU.add)
                k.tt("dve", V(t3), V(t1), V(t2), ALU.subtract)
                p3 = t2.a().rearrange("p (c t) -> p c t", t=CH)
                k.tt("dve", (t3, t3.a().rearrange("p (c t) -> p c t", t=CH)), (t3, t3.a().rearrange("p (c t) -> p c t", t=CH)),
                     (t2, p3[:, :, CH - 1:CH].to_broadcast([8, NCH, CH])), ALU.add)
                k.dma((GSD, GSD[0:4, :], 0), (t2, t2[0:4, :]))
                k.dma((GSD, GSD[4:8, :], 1), (t3, t3[4:8, :]))
                k.tt("dve", V(t2), V(GI), V(t2), ALU.subtract)
                k.tt("dve", V(t3), V(GI), V(t3), ALU.subtract)
                k.dma((GSD, GSD[8:12, :], 2), (t2, t2[0:4, :]))
                k.dma((GSD, GSD[12:16, :], 3), (t3, t3[4:8, :]))
                P.barrier()
            with ExitStack() as ph:
                masks = make_masks(ph)
                nwB = P.sb("mnwB", [64, 1024], F32, es=ph)
                k.dma(V(nwB), V(I["mlstm_norm_w"], I["mlstm_norm_w"][0].partition_broadcast(64)))
                oacc = P.sb("moacc", [64, NCH, 256], F32, es=ph)
                ssn = P.sb("mssn", [64, NCH], F32, es=ph)
                for h in range(4):
                    with ExitStack() as hs1:
                        vaug = P.sb("vaug", [64, NCH, 257], BF16, es=hs1)
                        t_q = P.sb("mt_q", [128, 512], F32, es=hs1)
                        t_k = P.sb("mt_k", [128, 512], F32, es=hs1)
                        t_e = P.sb("mt_e", [128, 512], F32, es=hs1)
                        t_s = P.sb("mt_s", [128, 512], F32, es=hs1)
                        cd = P.sb("mcd", [128, NCH], F32, es=hs1)
                        q_in = P.sb("mq_in", [128, L], BF16, es=hs1)
                        k_in = P.sb("mk_in", [128, L], BF16, es=hs1)
                        k_end = P.sb("mk_end", [128, L], BF16, es=hs1)
                        kET = P.sb("mkET", [64, NCH, 128], BF16, es=hs1)
                        S = P.sb("mS", [128, 257], F32, es=hs1)
                        Sb = P.sb("mSb", [128, 257], BF16, es=hs1)
                        Am = [P.sb(f"mAm{i}", [64, 64], BF16, es=hs1) for i in range(2)]
                        dn = P.sb("mdn", [64, 2], F32, es=hs1)
                        psA = P.ps("mpsA", [64, 64], F32, es=hs1)
                        pso = P.ps("mpso", [64, 257], F32, es=hs1)
                        pskv = P.ps("mpskv", [128, 257], F32, es=hs1)
                        pst = [P.ps(f"mpst{i}", [64, 4, 128], BF16, es=hs1) for i in range(2)]
                        k.dma((vaug, vaug[:, :, 0:256], "v"), (PT, PT[:, 1056 + h * 256:1056 + (h + 1) * 256].rearrange("(c p) d -> p c d", p=CH)), eng="pool")
                        k.memset("pool", (vaug, vaug[:, :, 256:257], "o"), 1.0)
                        k.memset("pool", V(oacc), 0.0)
                        for z in range(2):
                            end = CH - 1 if z == 0 else 0
                            for (t0_, tw) in TB:
                                tsl = slice(t0_, t0_ + tw)
                                ncb = tw // CH
                                c0 = t0_ // CH
                                k.dma((t_e, t_e[:, 0:tw]), (GSD, GSD[z * 4 + h, tsl].partition_broadcast(128)))
                                k.dma((t_s, t_s[:, 0:tw]), (GSD, GSD[8 + z * 4 + h, tsl].partition_broadcast(128)))
                                k.dma((t_q, t_q[:, 0:tw]), (PC, PC[1536 + h * 128:1536 + (h + 1) * 128, tsl]))
                                k.dma((t_k, t_k[:, 0:tw]), (PC, PC[2048 + h * 128:2048 + (h + 1) * 128, tsl]))
                                k.act((t_e, t_e[:, 0:tw]), (t_e, t_e[:, 0:tw]), AF.Exp)
                                k.act((t_s, t_s[:, 0:tw]), (t_s, t_s[:, 0:tw]), AF.Exp)
                                e3 = t_e[:, 0:tw].rearrange("p (c t) -> p c t", t=CH)
                                k.cp("pool", (cd, cd[:, c0:c0 + ncb]), (t_e, e3[:, :, end]))
                                k.tt("dve", (q_in, q_in[:, tsl]), (t_q, t_q[:, 0:tw]), (t_e, t_e[:, 0:tw]), ALU.mult)
                                k.stt("dve", (t_k, t_k[:, 0:tw]), (t_k, t_k[:, 0:tw]), 128 ** -0.5, (t_s, t_s[:, 0:tw]), ALU.mult, ALU.mult)
                                k.cp("pool", (k_in, k_in[:, tsl]), (t_k, t_k[:, 0:tw]))
                                k.tt("pool", (k_end, k_end[:, tsl].rearrange("p (c t) -> p c t", t=CH)),
                                     (t_k, t_k[:, 0:tw].rearrange("p (c t) -> p c t", t=CH)),
                                     (t_e, e3[:, :, end:end + 1].to_broadcast([128, ncb, CH])), ALU.mult)
                            for c4 in range(0, NCH, 4):
                                p_ = pst[(c4 // 4) % 2]
                                for j in range(4):
                                    c = c4 + j
                                    k.tr((p_, p_[:, j, :]), (k_end, k_end[:, c * CH:(c + 1) * CH]), V(identb))
                                k.cp("act", (kET, kET[:, c4:c4 + 4, :]), V(p_))
                            k.memset("pool", V(S), 0.0)
                            k.memset("pool", V(Sb), 0.0)
                            for step, c in enumerate(chunk_order(z)):
                                csl = slice(c * CH, (c + 1) * CH)
                                am = Am[step % 2]
                                k.mm(V(psA), (k_in, k_in[:, csl]), (q_in, q_in[:, csl]))
                                k.tt("dve", V(am), V(psA), V(masks[z]), ALU.mult)
                                k.mm(V(pso), V(am), (vaug, vaug[:, c, :]), start=True, stop=False)
                                k.mm(V(pso), (q_in, q_in[:, csl]), V(Sb), start=False, stop=True)
                                k.act((dn, dn[:, 0:1]), (pso, pso[:, 256:257]), AF.Abs)
                                k.ts("dve", (dn, dn[:, 0:1]), (dn, dn[:, 0:1]), 1.0, ALU.max)
                                k.recip((dn, dn[:, 1:2]), (dn, dn[:, 0:1]))
                                k.stt("dve", (oacc, oacc[:, c, :], c), (pso, pso[:, 0:256]), (dn, dn[:, 1:2]), (oacc, oacc[:, c, :], c), ALU.mult, ALU.add)
                                k.mm(V(pskv), (kET, kET[:, c, :]), (vaug, vaug[:, c, :]))
                                k.stt("dve", V(S), V(S), (cd, cd[:, c:c + 1]), V(pskv), ALU.mult, ALU.add)
                                k.cp("act", V(Sb), V(S))
                    P.barrier()
                    with ExitStack() as hs2:
                        gt = P.sb("mgt", [64, NCH, 256], F32, es=hs2)
                        k.tt("dve", V(gt), V(oacc), V(oacc), ALU.mult)
                        P.op("dve", lambda e: e.tensor_reduce(out=ssn.a(), in_=gt.a(), axis=AX.X, op=ALU.add), reads=[gt], writes=[ssn])
                        k.ts("dve", V(ssn), V(ssn), 1.0 / 256, ALU.mult, EPS, ALU.add)
                        k.act(V(ssn), V(ssn), AF.Sqrt)
                        k.recip(V(ssn), V(ssn))
                        k.tt("dve", V(oacc), V(oacc), (ssn, ssn.a().rearrange("p (c o) -> p c o", o=1).to_broadcast([64, NCH, 256])), ALU.mult)
                        k.tt("pool", V(oacc), V(oacc), (nwB, nwB[:, h * 256:(h + 1) * 256].rearrange("p (o d) -> p o d", o=1).to_broadcast([64, NCH, 256])), ALU.mult)
                        k.dma(V(gt), (PT, PT[:, 2080 + h * 256:2080 + (h + 1) * 256].rearrange("(c p) d -> p c d", p=CH)))
                        for q4 in range(4):
                            k.act((gt, gt[:, q4 * 17:(q4 + 1) * 17, :], q4), (gt, gt[:, q4 * 17:(q4 + 1) * 17, :], q4), AF.Sigmoid)
                        k.tt("dve", V(oacc), V(oacc), V(gt), ALU.mult)
                        k.dma((YM, YM[:, 1024 + h * 256:1024 + (h + 1) * 256].rearrange("(c p) d -> p c d", p=CH), ("m", h)), V(oacc))
                    P.barrier()

        def stage_xbt(PC, XBT):
            with ExitStack() as ph:
                ft = [P.sb(f"xft{i}", [128, 10, 128], F32, es=ph) for i in range(2)]
                ot = [P.sb(f"xot{i}", [128, 10, 128], F32, es=ph) for i in range(2)]
                pt = [P.ps(f"xpt{i}", [128, 4, 128], F32, es=ph) for i in range(3)]
                for tt in range(NT):
                    f_ = ft[tt % 2]; o_ = ot[tt % 2]
                    k.dma(V(f_), (PC, PC[0:1280, tt * 128:(tt + 1) * 128].rearrange("(b p) t -> p b t", p=128)))
                    for gi, (b0, nb_) in enumerate(((0, 4), (4, 4), (8, 2))):
                        p_ = pt[gi]
                        for j in range(nb_):
                            k.tr((p_, p_[:, j, :]), (f_, f_[:, b0 + j, :]), V(ident))
                        k.cp("act" if gi % 2 else "dve", (o_, o_[:, b0:b0 + nb_, :]), (p_, p_[:, 0:nb_, :]))
                    k.dma((XBT, XBT[tt * 128:(tt + 1) * 128, :], tt), (o_, o_.a().rearrange("p b c -> p (b c)")))
                P.barrier()

        def mixer_ssd(PC, PT, XBT, YS):
            with ExitStack() as ph:
                masks = make_masks(ph)
                ones64 = P.sb("sones64", [64, 64], F32, es=ph)
                k.memset("pool", V(ones64), 1.0)
                selend = []
                for z in range(2):
                    se = P.sb(f"selend{z}", [64, 128], F32, es=ph)
                    endp = CH - 1 if z == 0 else 0
                    P.op("pool", lambda e, se=se, endp=endp: e.affine_select(out=se.a(), in_=ones[0:64, :], pattern=[[0, 128]], compare_op=ALU.is_equal,
                                                                           fill=0.0, base=-endp, channel_multiplier=1), reads=[ones], writes=[se])
                    selend.append(se)
                dtT = P.sb("dtT", [64, NCH, 32], F32, es=ph)
                laT = P.sb("laT", [64, NCH, 32], F32, es=ph)
                dbB = P.sb("dbB", [64, 32], F32, es=ph)
                naB = P.sb("naB", [64, 32], F32, es=ph)
                dskB = P.sb("dskB", [64, 16], F32, es=ph)
                k.dma(V(dbB), V(I["ssd_dt_bias"], I["ssd_dt_bias"][0].rearrange("z h -> (z h)").partition_broadcast(64)))
                k.dma(V(naB), V(I["ssd_a_log"], I["ssd_a_log"][0].rearrange("z h -> (z h)").partition_broadcast(64)))
                k.dma(V(dskB), V(I["ssd_d"], I["ssd_d"][0].partition_broadcast(64)))
                k.act(V(naB), V(naB), AF.Exp)
                k.ts("dve", V(naB), V(naB), -1.0, ALU.mult)
                k.dma(V(dtT), (PT, PT[:, 1024:1056].rearrange("(c p) d -> p c d", p=CH)))
                k.tt("dve", V(dtT), V(dtT), (dbB, dbB.a().rearrange("p (o d) -> p o d", o=1).to_broadcast([64, NCH, 32])), ALU.add)
                k.act(V(dtT), V(dtT), AF.Exp)
                k.act(V(dtT), V(dtT), AF.Ln, bias=1.0)
                k.tt("dve", V(laT), V(dtT), (naB, naB.a().rearrange("p (o d) -> p o d", o=1).to_broadcast([64, NCH, 32])), ALU.mult)
                yacc = P.sb("yacc", [64, NCH, 256], F32, es=ph)
                for q4 in range(4):
                    g = q4 // 2
                    with ExitStack() as qs:
                        xq = P.sb("xq", [64, NCH, 256], BF16, es=qs)
                        Bf = P.sb("Bf", [128, L], BF16, es=qs)
                        Cf = P.sb("Cf", [128, L], BF16, es=qs)
                        BTt = P.sb("BTt", [64, NCH, 128], BF16, es=qs)
                        k.dma(V(xq), (XBT, XBT[:, q4 * 256:(q4 + 1) * 256].rearrange("(c p) d -> p c d", p=CH)), eng="pool")
                        k.dma(V(BTt), (XBT, XBT[:, 1024 + g * 128:1024 + (g + 1) * 128].rearrange("(c p) d -> p c d", p=CH)), eng="pool")
                        k.dma(V(Bf), (PC, PC[1024 + g * 128:1024 + (g + 1) * 128, :]), eng="pool")
                        k.dma(V(Cf), (PC, PC[1280 + g * 128:1280 + (g + 1) * 128, :]), eng="pool")
                        k.memset("pool", V(yacc), 0.0)
                        hs = [P.sb(f"hs{z}", [128, 256], F32, es=qs) for z in range(2)]
                        hsb = [P.sb(f"hsb{z}", [128, 256], BF16, es=qs) for z in range(2)]
                        CBm = [P.sb(f"CBm{z}", [64, 64], F32, es=qs) for z in range(2)]
                        acs = [P.sb(f"acs{z}", [64, 4], F32, es=qs) for z in range(2)]
                        dg = [P.sb(f"dg{z}", [64, 4, 64], F32, es=qs) for z in range(2)]
                        seg = [P.sb(f"seg{z}", [64, 4, 64], F32, es=qs) for z in range(2)]
                        AT = [P.sb(f"AT{z}", [64, 4, 64], BF16, es=qs) for z in range(2)]
                        xdt = [P.sb(f"xdt{z}", [64, 4, 64], BF16, es=qs) for z in range(2)]
                        xde = [P.sb(f"xde{z}", [64, 4, 64], BF16, es=qs) for z in range(2)]
                        din = [P.sb(f"din{z}", [64, 4], F32, es=qs) for z in range(2)]
                        dend = [P.sb(f"dend{z}", [64, 4], F32, es=qs) for z in range(2)]
                        dchB = [P.sb(f"dchB{z}", [128, 4], F32, es=qs) for z in range(2)]
                        ytmp = [P.sb(f"ytmp{z}", [64, 4, 64], F32, es=qs) for z in range(2)]
                        pCB = P.ps("pCB", [64, 64], F32, es=qs)
                        pac = P.ps("pac", [64, 4], F32, es=qs)
                        pbc = P.ps("pbc", [64, 4, 64], F32, es=qs)
                        py = P.ps("py", [64, 4, 64], F32, es=qs)
                        pyi = P.ps("pyi", [64, 4, 64], F32, es=qs)
                        phs = P.ps("phs", [128, 256], F32, es=qs)
                        pdc = P.ps("pdc", [128, 4], F32, es=qs)
                        for z in range(2):
                            k.memset("pool", V(hs[z]), 0.0)
                            k.memset("pool", V(hsb[z]), 0.0)
                        orders = [chunk_order(0), chunk_order(1)]
                        for step in range(NCH):
                            for z in range(2):
                                c = orders[z][step]
                                csl = slice(c * CH, (c + 1) * CH)
                                end = CH - 1 if z == 0 else 0
                                hsl = slice(z * 16 + q4 * 4, z * 16 + q4 * 4 + 4)
                                b4 = lambda t: t.a().rearrange("p (h o) -> p h o", o=1).to_broadcast([64, 4, 64])
                                k.mm(V(pCB), (Bf, Bf[:, csl]), (Cf, Cf[:, csl]))
                                k.tt("dve", V(CBm[z]), V(pCB), V(masks[z]), ALU.mult)
                                k.mm(V(pac), V(masks[z]), (laT, laT[:, c, hsl]))
                                k.cp("act", V(acs[z]), V(pac))
                                k.tt("pool", V(dg[z]), (ident, ident[0:64, 0:64].rearrange("p (o t) -> p o t", o=1).to_broadcast([64, 4, 64])),
                                     (acs[z], b4(acs[z])), ALU.mult)
                                k.mm(V(pbc), V(ones64), (dg[z], dg[z].a().rearrange("p h t -> p (h t)")))
                                k.tt("dve", V(seg[z]), V(pbc), (acs[z], b4(acs[z])), ALU.subtract)
                                k.tt("dve", V(dend[z]), (pbc, pbc[:, :, end]), V(acs[z]), ALU.subtract)
                                k.act(V(seg[z]), V(seg[z]), AF.Exp)
                                k.act(V(dend[z]), V(dend[z]), AF.Exp)
                                k.act(V(din[z]), V(acs[z]), AF.Exp)
                                k.stt("dve", V(AT[z]), V(seg[z]), 1.0, (CBm[z], CBm[z].a().rearrange("p (o t) -> p o t", o=1).to_broadcast([64, 4, 64])),
                                      ALU.min, ALU.mult)
                                k.tt("pool", V(xdt[z]), (xq, xq[:, c, :].rearrange("p (h d) -> p h d", d=64)),
                                     (dtT, dtT[:, c, hsl].rearrange("p (h o) -> p h o", o=1).to_broadcast([64, 4, 64])), ALU.mult)
                                for i in range(4):
                                    k.mm((py, py[:, i, :]), (AT[z], AT[z][:, i, :]), (xdt[z], xdt[z][:, i, :]))
                                k.mm(V(pyi), (Cf, Cf[:, csl]), V(hsb[z]))
                                ya = (yacc, yacc[:, c, :].rearrange("p (h d) -> p h d", d=64), c)
                                k.tt("dve", ya, V(py), ya, ALU.add)
                                k.tt("dve", V(ytmp[z]), V(pyi), (din[z], b4(din[z])), ALU.mult)
                                k.tt("pool", ya, ya, V(ytmp[z]), ALU.add)
                                k.tt("pool", V(xde[z]), V(xdt[z]), (dend[z], b4(dend[z])), ALU.mult)
                                k.mm(V(phs), (BTt, BTt[:, c, :]), (xde[z], xde[z].a().rearrange("p h d -> p (h d)")))
                                k.mm(V(pdc), V(selend[z]), V(acs[z]))
                                k.act(V(dchB[z]), V(pdc), AF.Exp)
                                h3 = (hs[z], hs[z].a().rearrange("p (h d) -> p h d", d=64))
                                k.tt("pool", h3, h3, (dchB[z], dchB[z].a().rearrange("p (h o) -> p h o", o=1).to_broadcast([128, 4, 64])), ALU.mult)
                                k.tt("dve", V(hs[z]), V(phs), V(hs[z]), ALU.add)
                                k.cp("act", V(hsb[z]), V(hs[z]))
                    P.barrier()
                    with ExitStack() as fs:
                        xf = P.sb("sxf", [64, 17, 256], F32, es=fs)
                        zf = P.sb("szf", [64, 17, 256], F32, es=fs)
                        for c17 in range(4):
                            cs_ = slice(c17 * 17, (c17 + 1) * 17)
                            rows = slice(c17 * 17 * CH, (c17 + 1) * 17 * CH)
                            k.dma(V(xf), (XBT, XBT[rows, q4 * 256:(q4 + 1) * 256].rearrange("(c p) d -> p c d", p=CH)))
                            k.dma(V(zf), (PT, PT[rows, q4 * 256:(q4 + 1) * 256].rearrange("(c p) d -> p c d", p=CH)))
                            dsk4 = dskB[:, q4 * 4:(q4 + 1) * 4].rearrange("p (a h o) -> p a h o", a=1, o=1).to_broadcast([64, 17, 4, 64])
                            k.tt("pool", (xf, xf.a().rearrange("p c (h d) -> p c h d", d=64)), (xf, xf.a().rearrange("p c (h d) -> p c h d", d=64)),
                                 (dskB, dsk4), ALU.mult)
                            k.tt("dve", V(xf), V(xf), (yacc, yacc[:, cs_, :], ("f", c17)), ALU.add)
                            k.act(V(zf), V(zf), AF.Silu)
                            k.tt("dve", V(xf), V(xf), V(zf), ALU.mult)
                            k.dma((YS, YS[rows, q4 * 256:(q4 + 1) * 256].rearrange("(c p) d -> p c d", p=CH), (q4, c17)), V(xf))
                    P.barrier()

        def stage_ssd_norm(YS, YM):
            with ExitStack() as ph:
                nwB = P.sb("snwB", [128, 1024], F32, es=ph)
                k.dma(V(nwB), V(I["ssd_norm_w"], I["ssd_norm_w"][0].partition_broadcast(128)))
                yt = [P.sb(f"syt{i}", [128, 1024], F32, es=ph) for i in range(2)]
                sq = P.sb("ssq", [128, 1024], F32, es=ph)
                st = [P.sb(f"sst{i}", [128, 2], F32, es=ph) for i in range(2)]
                for tt in range(NT):
                    y_ = yt[tt % 2]; s_ = st[tt % 2]
                    k.dma(V(y_), (YS, YS[tt * 128:(tt + 1) * 128, :]))
                    k.tt("pool", V(sq), V(y_), V(y_), ALU.mult)
                    P.op("dve", lambda e, s_=s_: e.tensor_reduce(out=s_.a(), in_=sq.a().rearrange("p (g d) -> p g d", g=2), axis=AX.X, op=ALU.add),
                         reads=[sq], writes=[s_])
                    k.ts("dve", V(s_), V(s_), 1.0 / 512, ALU.mult, EPS, ALU.add)
                    k.act(V(s_), V(s_), AF.Sqrt)
                    k.recip(V(s_), V(s_))
                    k.tt("dve", (y_, y_.a().rearrange("p (g d) -> p g d", g=2)), (y_, y_.a().rearrange("p (g d) -> p g d", g=2)),
                         (s_, s_.a().rearrange("p (g o) -> p g o", o=1).to_broadcast([128, 2, 512])), ALU.mult)
                    k.tt("pool", V(y_), V(y_), V(nwB), ALU.mult)
                    k.dma((YM, YM[tt * 128:(tt + 1) * 128, 0:1024], ("s", tt)), V(y_))
                P.barrier()

        def stage_final():
            with ExitStack() as ph:
                fnB = P.sb("fnB", [128, D], F32, es=ph)
                k.dma(V(fnB), V(I["final_norm_w"], I["final_norm_w"].a().partition_broadcast(128)))
                xt = [P.sb(f"fxt{i}", [128, D], F32, es=ph) for i in range(2)]
                junk = P.sb("fjunk", [128, D], F32, es=ph)
                st = [P.sb(f"fst{i}", [128, 4], F32, es=ph) for i in range(2)]
                for i in range(SEQ // 128):
                    x_ = xt[i % 2]; s_ = st[i % 2]
                    k.dma(V(x_), (XL, XL[LC + i * 128:LC + (i + 1) * 128, :], i))
                    k.memset("pool", (s_, s_[:, 0:1]), 0.0)
                    k.act(V(junk), V(x_), AF.Square, accum=(s_, s_[:, 0:1]))
                    k.ts("dve", (s_, s_[:, 1:2]), (s_, s_[:, 0:1]), 1.0 / D, ALU.mult, EPS, ALU.add)
                    k.act((s_, s_[:, 2:3]), (s_, s_[:, 1:2]), AF.Sqrt)
                    k.recip((s_, s_[:, 3:4]), (s_, s_[:, 2:3]))
                    k.ts("dve", V(x_), V(x_), (s_, s_[:, 3:4]), ALU.mult)
                    k.tt("pool", V(x_), V(x_), V(fnB), ALU.mult)
                    k.dma((OUT, OUT[i * 128:(i + 1) * 128, :], i), V(x_))
                P.barrier()

        def src0(i):
            if i < 2:
                return (I["ctx"], I["ctx"][i * 128:(i + 1) * 128, :])
            return (I["x"], I["x"][(i - 2) * 128:(i - 1) * 128, :])

        def src1(i):
            return (XL, XL[i * 128:(i + 1) * 128, :])

        def layer0():
            stage_mods(0)
            if "modF" in debug:
                dm = P.dram("d_modF", [128, 6 * KO * 2], F32, kind="ExternalOutput")
                k.dma(V(dm), (modF, modF.a().rearrange("p a b c -> p (a b c)")))
                dm2 = P.dram("d_modB", [128, 4 * D], F32, kind="ExternalOutput")
                k.dma(V(dm2), (modB, modB.a().rearrange("p a b c -> p (a b c)")))
            PF0 = scratch("PF0", [3456, L])
            PT0 = scratch("PT0", [L, 1024])
            YM0 = scratch("YM0", [L, 1024])
            with ExitStack() as lay:
                HT = P.sb("HT", [128, KO, L], BF16, es=lay)
                stage_norm(0, 1, HT, src0, perm=False)
                if "HT0" in debug:
                    dh = P.dram("d_HT0", [128, KO * L], BF16, kind="ExternalOutput")
                    k.dma(V(dh), (HT, HT.a().rearrange("p a b -> p (a b)")))
                if stop_after == "norm0":
                    return
                stage_proj(HT, (I["even_w_in"], I["even_w_in"][0]), 4480, [(0, 3456, PF0, 0)], [(3456, 4480, PT0, 0)])
            if stop_after == "proj0":
                return
            if "skip_hgrn" not in debug:
                mixer_hgrn(PF0, PT0, YM0)
            PR0 = scratch("PR0", [1920, L])
            GT0 = scratch("GT0", [L, 512])
            rwkv_shift(PF0, PR0)
            if "no_gate" not in debug:
                rwkv_gate(PR0, GT0)
            if stop_after == "shift0":
                return
            if "skip_rwkv" not in debug:
                mixer_rwkv(PR0, GT0, YM0)
            if stop_after == "mix0":
                return
            stage_outproj(YM0, 1024, (I["even_w_out"], I["even_w_out"][0]), src0, False, list(range(NT)))
            if stop_after == "out0":
                return
            stage_moe(0, list(range(NT)))

        def layer1():
            stage_mods(1)
            PF1 = scratch("PF1", [2576, L])
            PT1 = scratch("PT1", [L, 3104])
            PC1 = scratch("PC1", [2560, L])
            YM1 = scratch("YM1", [L, 2048])
            GSD = scratch("GSD", [16, L])
            YS = scratch("YS", [L, 1024])
            with ExitStack() as lay:
                HT = P.sb("HT", [128, KO, L], BF16, es=lay)
                stage_norm(1, 1, HT, src1, perm=True)
                stage_proj(HT, (I["odd_w_in"], I["odd_w_in"][0]), 5680,
                           [(1024, 2560, PF1, 0), (2592, 3616, PF1, 1536), (5664, 5680, PF1, 2560)],
                           [(0, 1024, PT1, 0), (2560, 2592, PT1, 1024), (3616, 5664, PT1, 1056)])
            stage_conv(PF1, PC1, [(0, 12, (I["ssd_conv_w"], I["ssd_conv_w"][0]), (I["ssd_conv_b"], I["ssd_conv_b"][0])),
                                  (1536, 8, (I["mlstm_conv_w"], I["mlstm_conv_w"][0]), (I["mlstm_conv_b"], I["mlstm_conv_b"][0]))])
            if stop_after == "L1conv":
                return
            if "skip_mlstm" not in debug:
                mixer_mlstm(PC1, PF1, PT1, YM1, GSD)
            if stop_after == "L1mlstm":
                return
            XBT = scratch("XBT", [L, 1280])
            stage_xbt(PC1, XBT)
            mixer_ssd(PC1, PT1, XBT, YS)
            stage_ssd_norm(YS, YM1)
            if stop_after == "L1ssd":
                return
            stage_outproj(YM1, 2048, (I["odd_w_out"], I["odd_w_out"][0]), src1, True, list(range(2, NT)))
            if stop_after == "L1out":
                return
            stage_moe(1, list(range(2, NT)))
            stage_final()

        if "L1only" in debug:
            XLin = P.dram("XLin", [L, D], F32, kind="ExternalInput")
            for i4 in range(4):
                k.dma((XL, XL[i4 * 1088:(i4 + 1) * 1088, :], ("in", i4)), (XLin, XLin[i4 * 1088:(i4 + 1) * 1088, :]))
            P.barrier()
        else:
            layer0()
        if stop_after is None or stop_after.startswith("L1"):
            layer1()
        P.finish()
    return nc


_NC = None


def kernel(**inputs):
    global _NC
    if _NC is None:
        _NC = build()
    nc = _NC
    n = 8
    in_maps = []
    for b in range(n):
        m = {}
        for kk_, v in inputs.items():
            v = np.asarray(v)
            if kk_ == "x":
                m[kk_] = np.ascontiguousarray(v[b])
            elif kk_ == "c":
                m[kk_] = np.ascontiguousarray(v[b])
            elif kk_ == "ctx":
                m[kk_] = np.ascontiguousarray(v[b])
            else:
                m[kk_] = v
        in_maps.append(m)
    res = run_bass_kernel_spmd(nc, in_maps, core_ids=list(range(n)))
    return np.stack([r["out"] for r in res.results], axis=0)
```

```python
import numpy as np
import concourse.bass as bass
import concourse.mybir as mybir
from concourse.bass_utils import run_bass_kernel_spmd
from contextlib import ExitStack

F32 = mybir.dt.float32
BF16 = mybir.dt.bfloat16
AF = mybir.ActivationFunctionType
ALU = mybir.AluOpType
AX = mybir.AxisListType

ENGS = ("pe", "dve", "act", "pool", "sp")
D = 1024
KO = 8
LC = 256
SEQ = 4096
L = LC + SEQ
NT = L // 128
EPS = 1e-6
CH = 64
NCH = L // CH


class Cell:
    __slots__ = ("w", "r")

    def __init__(self):
        self.w = None
        self.r = []


class Buf:
    def __init__(self, name, h, is_dram=False, is_psum=False):
        self.name = name
        self.h = h
        self.is_dram = is_dram
        self.is_psum = is_psum
        self.base = Cell()
        self.parts = {}

    def cells(self, key):
        if key is None:
            return [self.base] + list(self.parts.values())
        c = self.parts.get(key)
        if c is None:
            c = Cell()
            c.w = self.base.w
            c.r = list(self.base.r)
            self.parts[key] = c
        return [c]

    def __getitem__(self, idx):
        return self.h[idx]

    def a(self):
        return self.h[:]


class Prog:
    def __init__(self, nc, es):
        self.nc = nc
        self.es = es
        self.q = {e: [] for e in ENGS}
        self.cnt = {e: 0 for e in ENGS}
        self.sem = {e: es.enter_context(nc.semaphore("s_" + e)) for e in ENGS}
        self.known = {e: {} for e in ENGS}
        self.dsem = {}
        self.dsem_by_id = {}
        self.phase_slots = {}
        self.ninst = 0
        self.uid = 0

    def sb(self, name, shape, dt=F32, es=None):
        self.uid += 1
        h = (es or self.es).enter_context(self.nc.sbuf_tensor(f"{name}_{self.uid}", list(shape), dt))
        return Buf(name, h)

    def ps(self, name, shape, dt=F32, es=None):
        self.uid += 1
        h = (es or self.es).enter_context(self.nc.psum_tensor(f"{name}_{self.uid}", list(shape), dt))
        return Buf(name, h, is_psum=True)

    def dram(self, name, shape, dt=F32, kind="Internal"):
        h = self.nc.dram_tensor(name, list(shape), dt, kind=kind)
        return Buf(name, h.ap(), is_dram=True)

    def dma_sem(self, name):
        if name not in self.dsem:
            self.dsem[name] = [self.es.enter_context(self.nc.semaphore("d_" + name)), 0]
            self.dsem_by_id[id(self.dsem[name][0])] = self.dsem[name]
        return self.dsem[name]

    def _norm(self, lst):
        return [(r, None) if isinstance(r, Buf) else ((r[0], None) if r[0].is_psum else r) for r in lst]

    def _deps(self, eng, reads, writes, pe_acc=False):
        need = {}

        def add(tok):
            if tok is None:
                return
            k = id(tok[0])
            if k not in need or need[k][1] < tok[1]:
                need[k] = tok

        for (b, key) in reads:
            for c in b.cells(key):
                add(c.w)
        for (b, key) in writes:
            for c in b.cells(key):
                if not (pe_acc and c.w is not None and c.w[2] == "pe"):
                    add(c.w)
                for t in c.r:
                    add(t)
        out = []
        kn = self.known[eng]
        for k, tok in need.items():
            val = tok[1]
            if k in self.dsem_by_id:
                val = self.dsem_by_id[k][1]
            if kn.get(k, 0) >= val:
                continue
            kn[k] = val
            out.append((tok[0], val))
        return out

    def _record(self, tok, reads, writes):
        for (b, key) in reads:
            for c in b.cells(key):
                c.r.append(tok)
                if len(c.r) > 16:
                    best = {}
                    for t in c.r:
                        kk = id(t[0])
                        if kk not in best or best[kk][1] < t[1]:
                            best[kk] = t
                    c.r = list(best.values())
        for (b, key) in writes:
            for c in b.cells(key):
                c.w = tok
                c.r = []

    def op(self, eng, fn, reads=(), writes=(), pe_acc=False):
        reads = self._norm(reads)
        writes = self._norm(writes)
        writes = writes + [r for r in reads if r[0].is_psum]
        waits = self._deps(eng, reads, writes, pe_acc)
        self.cnt[eng] += 1
        sem = self.sem[eng]
        tok = (sem, self.cnt[eng], eng)
        self.ninst += 1

        def run(e, waits=waits, fn=fn, sem=sem):
            for s, v in waits:
                e.wait_ge(s, v)
            fn(e).then_inc(sem, 1)

        self.q[eng].append(run)
        self._record(tok, reads, writes)
        return tok

    def dma(self, eng, out_ap, in_ap, reads=(), writes=(), semname=None, **kw):
        reads = self._norm(reads)
        writes = self._norm(writes)
        waits = self._deps(eng, reads, writes)
        if semname is None:
            sbs = [b for (b, _) in list(writes) + list(reads) if not b.is_dram]
            semname = sbs[0].name if sbs else "dram2dram"
        if semname not in self.phase_slots:
            self.phase_slots[semname] = len(self.phase_slots)
        ds = self.dma_sem(f"slot{self.phase_slots[semname]}")
        ds[1] += 16
        tok = (ds[0], ds[1], "dma")
        self.ninst += 1

        def run(e, waits=waits, sem=ds[0]):
            for s, v in waits:
                e.wait_ge(s, v)
            e.dma_start(out=out_ap, in_=in_ap, **kw).then_inc(sem, 16)

        self.q[eng].append(run)
        self._record(tok, reads, writes)
        return tok

    def barrier(self):
        self.phase_slots = {}
        toks = [(self.sem[f], self.cnt[f]) for f in ENGS if self.cnt[f] > 0]
        toks += [(v[0], v[1]) for v in self.dsem.values() if v[1] > 0]
        for e in ENGS:
            kn = self.known[e]
            ws = []
            for s, v in toks:
                if kn.get(id(s), 0) < v:
                    kn[id(s)] = v
                    ws.append((s, v))

            def run(en, ws=ws):
                for s, v in ws:
                    en.wait_ge(s, v)
            self.q[e].append(run)

    def finish(self):
        nc = self.nc
        self.barrier()
        with nc.Block() as block:
            @block.tensor
            def _(e):
                for f in self.q["pe"]:
                    f(e)

            @block.vector
            def _(e):
                for f in self.q["dve"]:
                    f(e)

            @block.scalar
            def _(e):
                for f in self.q["act"]:
                    f(e)

            @block.gpsimd
            def _(e):
                for f in self.q["pool"]:
                    f(e)

            @block.sync
            def _(e):
                for f in self.q["sp"]:
                    f(e)


def _rk(x):
    return (x[0], x[2] if len(x) > 2 else None)


class K:
    def __init__(self, P):
        self.P = P
        self.dq = 0

    def mm(self, out, lhsT, rhs, start=True, stop=True):
        return self.P.op("pe", lambda e: e.matmul(out[1], lhsT=lhsT[1], rhs=rhs[1], start=start, stop=stop),
                         reads=[_rk(lhsT), _rk(rhs)], writes=[_rk(out)], pe_acc=not start)

    def tr(self, out, in_, ident):
        return self.P.op("pe", lambda e: e.transpose(out[1], in_[1], ident[1]),
                         reads=[_rk(in_), _rk(ident)], writes=[_rk(out)])

    def act(self, out, in_, func, bias=None, scale=None, accum=None, eng="act"):
        reads = [_rk(in_)]
        kw = {}
        if bias is not None:
            if isinstance(bias, tuple):
                reads.append(_rk(bias)); kw["bias"] = bias[1]
            else:
                kw["bias"] = bias
        if scale is not None:
            if isinstance(scale, tuple):
                reads.append(_rk(scale)); kw["scale"] = scale[1]
            else:
                kw["scale"] = scale
        writes = [_rk(out)]
        if accum is not None:
            writes.append(_rk(accum)); kw["accum_out"] = accum[1]
        return self.P.op("act", lambda e: e.activation(out=out[1], in_=in_[1], func=func, **kw), reads=reads, writes=writes)

    def tt(self, eng, out, in0, in1, op):
        return self.P.op(eng, lambda e: e.tensor_tensor(out=out[1], in0=in0[1], in1=in1[1], op=op),
                         reads=[_rk(in0), _rk(in1)], writes=[_rk(out)])

    def ts(self, eng, out, in0, s1, op0, s2=None, op1=None, accum=None):
        reads = [_rk(in0)]
        a1 = s1
        if isinstance(s1, tuple):
            reads.append(_rk(s1)); a1 = s1[1]
        a2 = s2
        if isinstance(s2, tuple):
            reads.append(_rk(s2)); a2 = s2[1]
        kw = {}
        if op1 is not None:
            kw["op1"] = op1
        writes = [_rk(out)]
        if accum is not None:
            writes.append(_rk(accum)); kw["accum_out"] = accum[1]
        return self.P.op(eng, lambda e: e.tensor_scalar(out=out[1], in0=in0[1], scalar1=a1, scalar2=a2, op0=op0, **kw),
                         reads=reads, writes=writes)

    def stt(self, eng, out, in0, scalar, in1, op0, op1):
        reads = [_rk(in0), _rk(in1)]
        sc = scalar
        if isinstance(scalar, tuple):
            reads.append(_rk(scalar)); sc = scalar[1]
        return self.P.op(eng, lambda e: e.scalar_tensor_tensor(out=out[1], in0=in0[1], scalar=sc, in1=in1[1], op0=op0, op1=op1),
                         reads=reads, writes=[_rk(out)])

    def cp(self, eng, out, in_):
        if eng == "act":
            return self.P.op("act", lambda e: e.copy(out=out[1], in_=in_[1]), reads=[_rk(in_)], writes=[_rk(out)])
        return self.P.op(eng, lambda e: e.tensor_copy(out=out[1], in_=in_[1]), reads=[_rk(in_)], writes=[_rk(out)])

    def memset(self, eng, out, val):
        return self.P.op(eng, lambda e: e.memset(out[1], val), writes=[_rk(out)])

    def scan(self, out, d0, d1, init, op0, op1):
        return self.P.op("dve", lambda e: e.tensor_tensor_scan(out=out[1], data0=d0[1], data1=d1[1], initial=init, op0=op0, op1=op1),
                         reads=[_rk(d0), _rk(d1)], writes=[_rk(out)])

    def recip(self, out, in_):
        return self.P.op("dve", lambda e: e.reciprocal(out=out[1], in_=in_[1]), reads=[_rk(in_)], writes=[_rk(out)])

    def dma(self, out, in_, eng=None, **kw):
        if eng is None:
            eng = "sp" if in_[0].is_dram else "act"
        reads = [_rk(in_)]
        writes = [_rk(out)]
        return self.P.dma(eng, out[1], in_[1], reads=reads, writes=writes, **kw)


def V(buf, ap=None, key=None):
    return (buf, buf.a() if ap is None else ap, key)


def build(debug=(), stop_after=None):
    nc = bass.Bass("TRN2", target_bir_lowering=False)
    es = ExitStack()
    with es:
        P = Prog(nc, es)
        k = K(P)
        I = {}

        def inp(name, shape):
            I[name] = P.dram(name, shape, F32, kind="ExternalInput")

        inp("x", [SEQ, D]); inp("c", [D]); inp("ctx", [LC, D]); inp("c_ctx", [D])
        inp("ada_w", [2, D, 6 * D]); inp("ada_b", [2, 6 * D]); inp("norm1_w", [2, D]); inp("norm2_w", [2, D])
        inp("even_w_in", [1, D, 4480]); inp("even_w_out", [1, 1024, D])
        inp("rwkv_mu", [1, 1920]); inp("rwkv_w0", [1, 2, 512]); inp("rwkv_w2", [1, 2, 64, 512])
        inp("rwkv_a0", [1, 2, 512]); inp("rwkv_a2", [1, 2, 64, 512]); inp("rwkv_g2", [1, 128, 512])
        for n in ("rwkv_k_k", "rwkv_k_a", "rwkv_r_k", "rwkv_ln_w", "rwkv_ln_b"):
            inp(n, [1, 512])
        inp("hgrn_lower_bounds", [3, 512]); inp("hgrn_norm_w", [1, 512])
        inp("odd_w_in", [1, D, 5680]); inp("odd_w_out", [1, 2048, D])
        inp("ssd_conv_w", [1, 5, 1536]); inp("ssd_conv_b", [1, 1536]); inp("ssd_dt_bias", [1, 2, 16])
        inp("ssd_a_log", [1, 2, 16]); inp("ssd_d", [1, 16]); inp("ssd_norm_w", [1, 1024])
        inp("mlstm_conv_w", [1, 5, 1024]); inp("mlstm_conv_b", [1, 1024]); inp("mlstm_i_bias", [1, 2, 4])
        inp("mlstm_f_bias", [1, 2, 4]); inp("mlstm_norm_w", [1, 1024])
        inp("router_w", [2, D, 32]); inp("router_b", [2, 32])
        inp("exp_w_gate", [2, 32, D, 1024]); inp("exp_b_gate", [2, 32, 1024])
        inp("exp_w_up", [2, 32, D, 1024]); inp("exp_b_up", [2, 32, 1024])
        inp("exp_w_down", [2, 32, 1024, D]); inp("exp_b_down", [2, 32, D])
        inp("final_norm_w", [D])
        OUT = P.dram("out", [SEQ, D], F32, kind="ExternalOutput")

        def scratch(name, shape, dt=F32):
            return P.dram(name, shape, dt, kind="ExternalOutput" if name in debug else "Internal")

        XL = scratch("XL", [L, D])

        ident = P.sb("ident", [128, 128], F32)
        identb = P.sb("identb", [128, 128], BF16)
        ones = P.sb("ones", [128, 128], F32)
        k.memset("pool", V(ones), 1.0)
        P.op("pool", lambda e: e.affine_select(out=ident.a(), in_=ones.a(), pattern=[[-1, 128]], compare_op=ALU.is_equal,
                                               fill=0.0, base=0, channel_multiplier=1), reads=[ones], writes=[ident])
        k.cp("dve", V(identb), V(ident))

        modF = P.sb("modF", [128, 6, KO, 2], F32)
        modB = P.sb("modB", [128, 2, 2, D], F32)
        g1F = P.sb("g1F", [128, KO, 2], F32)
        g2F = P.sb("g2F", [128, KO, 2], F32)
        nw1 = P.sb("nw1", [128, 2, KO], F32)
        nw2 = P.sb("nw2", [128, 2, KO], F32)
        k.dma(V(nw1), V(I["norm1_w"], I["norm1_w"].a().rearrange("l (ko p) -> p l ko", p=128)), allow_slow_non_contiguous=True)
        k.dma(V(nw2), V(I["norm2_w"], I["norm2_w"].a().rearrange("l (ko p) -> p l ko", p=128)), allow_slow_non_contiguous=True)

        def stage_mods(l):
            with ExitStack() as ph:
                c0 = P.sb("c0", [128, KO, 2], F32, es=ph)
                s = P.sb("s", [128, KO, 2], F32, es=ph)
                sB = P.sb("sB", [128, KO, 2, 128], F32, es=ph)
                abF = P.sb("abF", [128, 48], F32, es=ph)
                abB = P.sb("abB", [128, 2, D], F32, es=ph)
                awm = [P.sb(f"awm{i}", [128, KO, D], F32, es=ph) for i in range(2)]
                psF = P.ps("psF", [128, KO, 2], F32, es=ph)
                psB = [P.ps(f"psB{i}", [128, 512], F32, es=ph) for i in range(2)]
                tmp = P.sb("tmpm", [128, KO, 2], F32, es=ph)
                k.dma((c0, c0[:, :, 0]), V(I["c"], I["c"].a().rearrange("(ko p) -> p ko", p=128)), allow_slow_non_contiguous=True)
                k.dma((c0, c0[:, :, 1]), V(I["c_ctx"], I["c_ctx"].a().rearrange("(ko p) -> p ko", p=128)), allow_slow_non_contiguous=True)
                k.act(V(s), V(c0), AF.Silu)
                k.cp("dve", V(sB), (s, s.a().rearrange("p k (j o) -> p k j o", o=1).to_broadcast([128, KO, 2, 128])))
                k.dma(V(abF), V(I["ada_b"], I["ada_b"][l].rearrange("(nb p) -> p nb", p=128)), allow_slow_non_contiguous=True)
                k.dma((abB, abB[:, 0, :]), V(I["ada_b"], I["ada_b"][l, 2 * D:3 * D].partition_broadcast(128)))
                k.dma((abB, abB[:, 1, :]), V(I["ada_b"], I["ada_b"][l, 5 * D:6 * D].partition_broadcast(128)))
                for m in range(6):
                    aw = awm[m % 2]
                    k.dma(V(aw), V(I["ada_w"], I["ada_w"][l, :, m * D:(m + 1) * D].rearrange("(ko p) n -> p ko n", p=128)))
                    if m in (0, 1, 3, 4):
                        for nb in range(KO):
                            for ko in range(KO):
                                k.mm((psF, psF[:, nb, :]), (aw, aw[:, ko, nb * 128:(nb + 1) * 128]), (s, s[:, ko, :]),
                                     start=(ko == 0), stop=(ko == KO - 1))
                        k.tt("dve", (modF, modF[:, m, :, :]), V(psF),
                             (abF, abF[:, m * 8:(m + 1) * 8].rearrange("p (k o) -> p k o", o=1).to_broadcast([128, KO, 2])), ALU.add)
                    else:
                        mi = 0 if m == 2 else 1
                        for j in range(2):
                            for nblk in range(2):
                                pb = psB[(j * 2 + nblk) % 2]
                                for ko in range(KO):
                                    k.mm(V(pb), (sB, sB[:, ko, j, :]), (aw, aw[:, ko, nblk * 512:(nblk + 1) * 512]),
                                         start=(ko == 0), stop=(ko == KO - 1))
                                k.tt("dve", (modB, modB[:, mi, j, nblk * 512:(nblk + 1) * 512]), V(pb),
                                     (abB, abB[:, mi, nblk * 512:(nblk + 1) * 512]), ALU.add)
                for (gF, nw, mi) in ((g1F, nw1, 1), (g2F, nw2, 4)):
                    k.ts("dve", V(tmp), (modF, modF[:, mi, :, :]), 1.0, ALU.add)
                    k.tt("dve", V(gF), V(tmp), (nw, nw[:, l, :].rearrange("p (k o) -> p k o", o=1).to_broadcast([128, KO, 2])), ALU.mult)
                P.barrier()

        def stage_norm(l, which, HT, src_tiles, perm):
            gF = g1F if which == 1 else g2F
            mi = 0 if which == 1 else 3
            with ExitStack() as ph:
                xt = [P.sb(f"xt{i}", [128, D], F32, es=ph) for i in range(3)]
                xn = [P.sb(f"xn{i}", [128, D], F32, es=ph) for i in range(2)]
                junk = P.sb("junk", [128, D], F32, es=ph)
                st = [P.sb(f"st{i}", [128, 4], F32, es=ph) for i in range(2)]
                tmp = [P.sb(f"tmpn{i}", [128, KO, 128], F32, es=ph) for i in range(2)]
                pT = [P.ps(f"pT{i}", [128, KO, 128], F32, es=ph) for i in range(2)]
                for i in range(NT):
                    j = 1 if i < 2 else 0
                    x_ = xt[i % 3]; n_ = xn[i % 2]; s_ = st[i % 2]; t_ = tmp[i % 2]; p_ = pT[i % 2]
                    sb_, sap = src_tiles(i)
                    k.dma(V(x_), (sb_, sap))
                    k.memset("pool", (s_, s_[:, 0:1]), 0.0)
                    k.act(V(junk), V(x_), AF.Square, accum=(s_, s_[:, 0:1]))
                    k.ts("dve", (s_, s_[:, 1:2]), (s_, s_[:, 0:1]), 1.0 / D, ALU.mult, EPS, ALU.add)
                    k.act((s_, s_[:, 2:3]), (s_, s_[:, 1:2]), AF.Sqrt)
                    k.recip((s_, s_[:, 3:4]), (s_, s_[:, 2:3]))
                    k.ts("dve", V(n_), V(x_), (s_, s_[:, 3:4]), ALU.mult)
                    for ko in range(KO):
                        k.tr((p_, p_[:, ko, :]), (n_, n_[:, ko * 128:(ko + 1) * 128]), V(ident))
                    k.tt("dve", V(t_), V(p_), (gF, gF[:, :, j:j + 1].to_broadcast([128, KO, 128])), ALU.mult)
                    if perm and i >= 2:
                        r0 = 2 * (i - 2)
                        dst = HT[:, :, LC:].rearrange("p k (c r) -> p k c r", r=64)[:, :, :, r0:r0 + 2]
                        src0 = t_.a().rearrange("p k (r c) -> p k c r", r=2)
                        src1 = modF[:, mi, :, j:j + 1].rearrange("p k (a b) -> p k a b", b=1).to_broadcast([128, KO, 64, 2])
                        k.tt("pool", (HT, dst, i), (t_, src0), (modF, src1), ALU.add)
                    else:
                        k.tt("pool", (HT, HT[:, :, i * 128:(i + 1) * 128], i), V(t_),
                             (modF, modF[:, mi, :, j:j + 1].to_broadcast([128, KO, 128])), ALU.add)
                P.barrier()

        def stage_proj(HT, W, ncols, f_list, t_list):
            with ExitStack() as ph:
                wst = [P.sb(f"wst{i}", [128, KO, 512], BF16, es=ph) for i in range(2)]
                stg = [P.sb(f"stg{i}", [128, 512], F32, es=ph) for i in range(4)]
                pp = [P.ps(f"pp{i}", [128, 512], F32, es=ph) for i in range(4)]
                cnt = 0
                npan = (ncols + 511) // 512
                for pn in range(npan):
                    c0 = pn * 512
                    cw = min(512, ncols - c0)
                    w_ = wst[pn % 2]
                    k.dma((w_, w_[:, :, 0:cw]), (W[0], W[1][:, c0:c0 + cw].rearrange("(ko p) n -> p ko n", p=128)), eng="pool")
                    for (f0, f1, PF, roff) in f_list:
                        fa, fb = max(c0, f0), min(c0 + cw, f1)
                        if fa >= fb:
                            continue
                        for n0 in range(fa, fb, 128):
                            nw_ = min(128, fb - n0)
                            for tb in range(0, L, 512):
                                tw = min(512, L - tb)
                                p_ = pp[cnt % 4]; s_ = stg[cnt % 4]; cnt += 1
                                for ko in range(KO):
                                    k.mm((p_, p_[0:nw_, 0:tw]), (w_, w_[:, ko, n0 - c0:n0 - c0 + nw_]), (HT, HT[:, ko, tb:tb + tw]),
                                         start=(ko == 0), stop=(ko == KO - 1))
                                k.cp("act" if cnt % 2 else "dve", (s_, s_[0:nw_, 0:tw]), (p_, p_[0:nw_, 0:tw]))
                                r0 = n0 - f0 + roff
                                k.dma((PF, PF[r0:r0 + nw_, tb:tb + tw], ("f", n0)), (s_, s_[0:nw_, 0:tw]))
                    for (t0, t1, PT, coff) in t_list:
                        ta, tb_ = max(c0, t0), min(c0 + cw, t1)
                        if ta >= tb_:
                            continue
                        tw = tb_ - ta
                        for tt in range(NT):
                            p_ = pp[cnt % 4]; s_ = stg[cnt % 4]; cnt += 1
                            for ko in range(KO):
                                k.mm((p_, p_[:, 0:tw]), (HT, HT[:, ko, tt * 128:(tt + 1) * 128]), (w_, w_[:, ko, ta - c0:ta - c0 + tw]),
                                     start=(ko == 0), stop=(ko == KO - 1))
                            k.cp("act" if cnt % 2 else "dve", (s_, s_[:, 0:tw]), (p_, p_[:, 0:tw]))
                            cc = ta - t0 + coff
                            k.dma((PT, PT[tt * 128:(tt + 1) * 128, cc:cc + tw], ("t", tt, ta)), (s_, s_[:, 0:tw]))
                P.barrier()

        def chunk_order(z):
            if z == 0:
                return list(range(NCH))
            return [3, 2, 1, 0] + list(range(NCH - 1, 3, -1))

        def make_masks(ph):
            ms = []
            for z in range(2):
                m = P.sb(f"mask{z}", [64, 64], F32, es=ph)
                if z == 0:
                    P.op("pool", lambda e, m=m: e.affine_select(out=m.a(), in_=ones[0:64, 0:64], pattern=[[1, 64]], compare_op=ALU.is_ge,
                                                           fill=0.0, base=0, channel_multiplier=-1), reads=[ones], writes=[m])
                else:
                    P.op("pool", lambda e, m=m: e.affine_select(out=m.a(), in_=ones[0:64, 0:64], pattern=[[-1, 64]], compare_op=ALU.is_ge,
                                                           fill=0.0, base=0, channel_multiplier=1), reads=[ones], writes=[m])
                ms.append(m)
            return ms

        def mixer_hgrn(PF, PT, YM):
            BW = 1088
            NB = L // BW
            CB = BW // CH
            with ExitStack() as ph:
                masks = make_masks(ph)
                lbr = P.sb("lbr", [128, 3, 4], F32, es=ph)
                lbe = P.sb("lbe", [128, 3, 4], F32, es=ph)
                lbs = P.sb("lbs", [128, 4], F32, es=ph)
                lb = P.sb("lb", [128, 4], F32, es=ph)
                oml = P.sb("oml", [128, 4], F32, es=ph)
                noml = P.sb("noml", [128, 4], F32, es=ph)
                k.dma(V(lbr), V(I["hgrn_lower_bounds"], I["hgrn_lower_bounds"].a().rearrange("j (h p) -> p j h", p=128)),
                      allow_slow_non_contiguous=True)
                k.act(V(lbe), V(lbr), AF.Exp)
                k.tt("dve", V(lbs), (lbe, lbe[:, 0, :]), (lbe, lbe[:, 1, :]), ALU.add)
                k.tt("dve", V(lbs), V(lbs), (lbe, lbe[:, 2, :]), ALU.add)
                k.recip(V(lbs), V(lbs))
                k.tt("dve", V(lb), (lbe, lbe[:, 0, :]), V(lbs), ALU.mult)
                k.ts("dve", V(oml), V(lb), -1.0, ALU.mult, 1.0, ALU.add)
                k.ts("dve", V(noml), V(oml), -1.0, ALU.mult)
                rst = P.sb("rst", [128, BW], F32, es=ph)
                k.memset("pool", V(rst), 1.0)
                k.memset("pool", (rst, rst.a().rearrange("p (c t) -> p c t", t=CH)[:, :, 0:1]), 0.0)
                nwB = P.sb("nwB", [64, 512], F32, es=ph)
                k.dma(V(nwB), V(I["hgrn_norm_w"], I["hgrn_norm_w"][0].partition_broadcast(64)))
                oacc = P.sb("oacc", [64, NCH, 128], F32, es=ph)
                ssn = P.sb("ssn", [64, NCH], F32, es=ph)
                for h in range(4):
                    with ExitStack() as hs1:
                        t_f = P.sb("t_f", [128, BW], F32, es=hs1)
                        t_s = P.sb("t_s", [128, BW], F32, es=hs1)
                        t_lf = P.sb("t_lf", [128, BW], F32, es=hs1)
                        t_k = P.sb("t_k", [128, BW], F32, es=hs1)
                        t_b = P.sb("t_b", [128, BW], F32, es=hs1)
                        t_e = P.sb("t_e", [128, BW], F32, es=hs1)
                        t_q = P.sb("t_q", [128, BW], F32, es=hs1)
                        cd = [P.sb(f"cd{z}", [128, NCH], F32, es=hs1) for z in range(2)]
                        q_in = [P.sb(f"q_in{z}", [128, L], BF16, es=hs1) for z in range(2)]
                        k_in = [P.sb(f"k_in{z}", [128, L], BF16, es=hs1) for z in range(2)]
                        k_end1 = P.sb("k_end", [128, L], BF16, es=hs1)
                        k_end = [k_end1, k_end1]
                        kET = [P.sb(f"kET{z}", [64, NCH, 128], BF16, es=hs1) for z in range(2)]
                        vb = P.sb("vb", [64, NCH, 128], BF16, es=hs1)
                        S = [P.sb(f"S{z}", [128, 128], F32, es=hs1) for z in range(2)]
                        Sb = [P.sb(f"Sb{z}", [128, 128], BF16, es=hs1) for z in range(2)]
                        Am = [P.sb(f"Am{i}", [64, 64], BF16, es=hs1) for i in range(4)]
                        psA = [P.ps(f"psA{i}", [64, 64], F32, es=hs1) for i in range(2)]
                        pso = [P.ps(f"pso{i}", [64, 128], F32, es=hs1) for i in range(2)]
                        pskv = [P.ps(f"pskv{i}", [128, 128], F32, es=hs1) for i in range(2)]
                        pst = [P.ps(f"pst{i}", [64, 4, 128], BF16, es=hs1) for i in range(2)]
                        k.dma(V(vb), (PT, PT[:, h * 128:(h + 1) * 128].rearrange("(c p) d -> p c d", p=CH)), eng="pool")
                        for z in range(2):
                            end = CH - 1 if z == 0 else 0
                            for blk in range(NB):
                                tsl = slice(blk * BW, (blk + 1) * BW)
                                frow = 2432 + z * 512 + h * 128
                                k.dma(V(t_f), (PF, PF[frow:frow + 128, tsl]))
                                k.dma(V(t_q), (PF, PF[1920 + h * 128:1920 + (h + 1) * 128, tsl]))
                                k.act(V(t_s), V(t_f), AF.Sigmoid)
                                k.act(V(t_lf), V(t_s), AF.Ln, bias=(lb, lb[:, h:h + 1]), scale=(oml, oml[:, h:h + 1]))
                                k.ts("dve", V(t_k), V(t_s), (noml, noml[:, h:h + 1]), ALU.mult, (oml, oml[:, h:h + 1]), ALU.add)
                                k.scan(V(t_b), V(rst), V(t_lf), 0.0, ALU.mult, ALU.add)
                                if z == 1:
                                    k.tt("dve", V(t_f), V(t_lf), V(t_b), ALU.subtract)
                                    b3 = t_b.a().rearrange("p (c t) -> p c t", t=CH)
                                    k.tt("dve", (t_lf, t_lf.a().rearrange("p (c t) -> p c t", t=CH)),
                                         (t_f, t_f.a().rearrange("p (c t) -> p c t", t=CH)),
                                         (t_b, b3[:, :, CH - 1:CH].to_broadcast([128, CB, CH])), ALU.add)
                                    bb = t_lf
                                else:
                                    bb = t_b
                                k.act(V(t_e), V(bb), AF.Exp)
                                k.act(V(t_s), V(bb), AF.Exp, scale=-1.0)
                                e3 = t_e.a().rearrange("p (c t) -> p c t", t=CH)
                                k.cp("pool", (cd[z], cd[z][:, blk * CB:(blk + 1) * CB]), (t_e, e3[:, :, end]))
                                k.act(V(t_f), V(t_q), AF.Silu)
                                k.stt("dve", (q_in[z], q_in[z][:, tsl]), V(t_f), 128 ** -0.5, V(t_e), ALU.mult, ALU.mult)
                                k.tt("dve", V(t_k), V(t_k), V(t_s), ALU.mult)
                                k.cp("pool", (k_in[z], k_in[z][:, tsl]), V(t_k))
                                k.tt("pool", (k_end[z], k_end[z][:, tsl].rearrange("p (c t) -> p c t", t=CH)),
                                     (t_k, t_k.a().rearrange("p (c t) -> p c t", t=CH)),
                                     (t_e, e3[:, :, end:end + 1].to_broadcast([128, CB, CH])), ALU.mult)
                            for c4 in range(0, NCH, 4):
                                p_ = pst[(c4 // 4) % 2]
                                for j in range(4):
                                    c = c4 + j
                                    k.tr((p_, p_[:, j, :]), (k_end[z], k_end[z][:, c * CH:(c + 1) * CH]), V(identb))
                                k.cp("act", (kET[z], kET[z][:, c4:c4 + 4, :]), V(p_))
                        k.memset("pool", V(oacc), 0.0)
                        for z in range(2):
                            k.memset("pool", V(S[z]), 0.0)
                            k.memset("pool", V(Sb[z]), 0.0)
                        orders = [chunk_order(0), chunk_order(1)]
                        for step in range(NCH):
                            for z in range(2):
                                c = orders[z][step]
                                csl = slice(c * CH, (c + 1) * CH)
                                pa = psA[z]; po = pso[z]; pk = pskv[z]; am = Am[(step % 2) * 2 + z]
                                k.mm(V(pa), (k_in[z], k_in[z][:, csl]), (q_in[z], q_in[z][:, csl]))
                                k.tt("dve", V(am), V(pa), V(masks[z]), ALU.mult)
                                k.mm(V(po), V(am), (vb, vb[:, c, :]), start=True, stop=False)
                                k.mm(V(po), (q_in[z], q_in[z][:, csl]), V(Sb[z]), start=False, stop=True)
                                k.tt("dve", (oacc, oacc[:, c, :], c), (oacc, oacc[:, c, :], c), V(po), ALU.add)
                                k.mm(V(pk), (kET[z], kET[z][:, c, :]), (vb, vb[:, c, :]))
                                k.stt("dve", V(S[z]), V(S[z]), (cd[z], cd[z][:, c:c + 1]), V(pk), ALU.mult, ALU.add)
                                k.cp("act", V(Sb[z]), V(S[z]))
                    P.barrier()
                    with ExitStack() as hs2:
                        gt = P.sb("gt", [64, NCH, 128], F32, es=hs2)
                        k.tt("dve", V(gt), V(oacc), V(oacc), ALU.mult)
                        P.op("dve", lambda e: e.tensor_reduce(out=ssn.a(), in_=gt.a(), axis=AX.X, op=ALU.add), reads=[gt], writes=[ssn])
                        k.ts("dve", V(ssn), V(ssn), 1.0 / 128, ALU.mult, EPS, ALU.add)
                        k.act(V(ssn), V(ssn), AF.Sqrt)
                        k.recip(V(ssn), V(ssn))
                        k.tt("dve", V(oacc), V(oacc), (ssn, ssn.a().rearrange("p (c o) -> p c o", o=1).to_broadcast([64, NCH, 128])), ALU.mult)
                        k.tt("pool", V(oacc), V(oacc), (nwB, nwB[:, h * 128:(h + 1) * 128].rearrange("p (o d) -> p o d", o=1).to_broadcast([64, NCH, 128])), ALU.mult)
                        k.dma(V(gt), (PT, PT[:, 512 + h * 128:512 + (h + 1) * 128].rearrange("(c p) d -> p c d", p=CH)))
                        k.act(V(gt), V(gt), AF.Silu)
                        k.tt("dve", V(oacc), V(oacc), V(gt), ALU.mult)
                        k.dma((YM, YM[:, 512 + h * 128:512 + (h + 1) * 128].rearrange("(c p) d -> p c d", p=CH), ("h", h)), V(oacc))
                    P.barrier()
                P.barrier()

        def rwkv_shift(PF, PR):
            with ExitStack() as ph:
                mu = P.sb("mu", [128, 15], F32, es=ph)
                omu = P.sb("omu", [128, 15], F32, es=ph)
                hmu = P.sb("hmu", [128, 15], F32, es=ph)
                k.dma(V(mu), V(I["rwkv_mu"], I["rwkv_mu"][0].rearrange("(b p) -> p b", p=128)), allow_slow_non_contiguous=True)
                k.ts("dve", V(omu), V(mu), -1.0, ALU.mult, 1.0, ALU.add)
                k.ts("dve", V(hmu), V(mu), 0.5, ALU.mult)
                pt = [P.sb(f"shp{i}", [128, L + 2], F32, es=ph) for i in range(2)]
                sm = [P.sb(f"shs{i}", [128, L], F32, es=ph) for i in range(2)]
                for i in range(2):
                    k.memset("pool", (pt[i], pt[i][:, 0:1], "h0"), 0.0)
                    k.memset("pool", (pt[i], pt[i][:, L + 1:L + 2], "h1"), 0.0)
                for b in range(15):
                    p_ = pt[b % 2]; s_ = sm[b % 2]
                    k.dma((p_, p_[:, 1:L + 1], "m"), (PF, PF[b * 128:(b + 1) * 128, :]))
                    k.tt("dve", (s_, s_.a(), "a"), V(p_), (p_, p_[:, 2:L + 2]), ALU.add) if False else None
                    P.op("dve", lambda e, p_=p_, s_=s_: e.tensor_tensor(out=s_.a(), in0=p_[:, 0:L], in1=p_[:, 2:L + 2], op=ALU.add),
                         reads=[p_], writes=[s_])
                    k.cp("dve", (s_, s_[:, 255:256]), (p_, p_[:, 255:256]))
                    k.cp("dve", (s_, s_[:, 256:257]), (p_, p_[:, 258:259]))
                    k.ts("pool", V(s_), V(s_), (hmu, hmu[:, b:b + 1]), ALU.mult)
                    k.stt("dve", V(s_), (p_, p_[:, 1:L + 1]), (omu, omu[:, b:b + 1]), V(s_), ALU.mult, ALU.add)
                    k.dma((PR, PR[b * 128:(b + 1) * 128, :], b), V(s_))
                P.barrier()

        def rwkv_gate(PR, GT):
            with ExitStack() as ph:
                gd = P.sb("gd", [128, L], F32, es=ph)
                gs = P.sb("gs", [128, L], BF16, es=ph)
                g2f = P.sb("g2f", [128, 512], F32, es=ph)
                g2b = P.sb("g2b", [128, 512], BF16, es=ph)
                pg = [P.ps(f"pg{i}", [128, 512], F32, es=ph) for i in range(2)]
                sg = [P.sb(f"sg{i}", [128, 512], F32, es=ph) for i in range(2)]
                k.dma(V(gd), (PR, PR[1792:1920, :]))
                k.dma(V(g2f), V(I["rwkv_g2"], I["rwkv_g2"][0]))
                k.cp("dve", V(g2b), V(g2f))
                for q4 in range(4):
                    k.act((gs, gs[:, q4 * 1088:(q4 + 1) * 1088], q4), (gd, gd[:, q4 * 1088:(q4 + 1) * 1088]), AF.Sigmoid)
                for tt in range(NT if "gate_nomm" not in debug else 0):
                    k.mm(V(pg[tt % 2]), (gs, gs[:, tt * 128:(tt + 1) * 128]), V(g2b))
                    k.cp("dve" if tt % 2 else "act", V(sg[tt % 2]), V(pg[tt % 2]))
                    k.dma((GT, GT[tt * 128:(tt + 1) * 128, :], tt), V(sg[tt % 2]))
                P.barrier()

        def mixer_rwkv(PR, GT, YM):
            BW = 256
            NB = L // BW
            CB = BW // CH
            NL = 5
            with ExitStack() as ph:
                w2all = P.sb("w2all", [128, 512], F32, es=ph)
                a2all = P.sb("a2all", [128, 512], F32, es=ph)
                k.dma(V(w2all), V(I["rwkv_w2"], I["rwkv_w2"][0].rearrange("z r c -> (z r) c")))
                k.dma(V(a2all), V(I["rwkv_a2"], I["rwkv_a2"][0].rearrange("z r c -> (z r) c")))
                def hv(name, src):
                    t = P.sb(name, [64, 8], F32, es=ph)
                    k.dma(V(t), (src[0], src[1].rearrange("(h n) -> n h", n=64)), allow_slow_non_contiguous=True)
                    return t
                w0 = [hv(f"w0_{z}", (I["rwkv_w0"], I["rwkv_w0"][0, z])) for z in range(2)]
                a0 = [hv(f"a0_{z}", (I["rwkv_a0"], I["rwkv_a0"][0, z])) for z in range(2)]
                kkg = hv("kkg", (I["rwkv_k_k"], I["rwkv_k_k"][0]))
                kag = hv("kag", (I["rwkv_k_a"], I["rwkv_k_a"][0]))
                rkg = hv("rkg", (I["rwkv_r_k"], I["rwkv_r_k"][0]))
                oka = P.sb("oka", [64, 8], F32, es=ph)
                k.ts("dve", V(oka), V(kag), -1.0, ALU.mult, 1.0, ALU.add)
                lnw = P.sb("lnw", [64, 512], F32, es=ph)
                lnb = P.sb("lnb", [64, 512], F32, es=ph)
                k.dma(V(lnw), V(I["rwkv_ln_w"], I["rwkv_ln_w"][0].partition_broadcast(64)))
                k.dma(V(lnb), V(I["rwkv_ln_b"], I["rwkv_ln_b"][0].partition_broadcast(64)))
                rst = P.sb("rst", [64, BW], F32, es=ph)
                k.memset("pool", V(rst), 1.0)
                k.memset("pool", (rst, rst.a().rearrange("p (c t) -> p c t", t=CH)[:, :, 0:1]), 0.0)
                ones64 = P.sb("ones64", [64, 64], F32, es=ph)
                k.memset("pool", V(ones64), 1.0)
                m4 = []
                m3 = []
                for z in range(2):
                    m = P.sb(f"m4_{z}", [128, 2, 64], F32, es=ph)
                    for half in range(2):
                        for col in range(2):
                            sgn = 1 if z == 0 else -1
                            base = (-1 if col == 0 else 0)
                            P.op("pool", lambda e, m=m, half=half, col=col, sgn=sgn, base=base: e.affine_select(
                                out=m[half * 64:(half + 1) * 64, col, :], in_=ones[half * 64:(half + 1) * 64, 0:64],
                                pattern=[[sgn, 64]], compare_op=ALU.is_ge, fill=0.0, base=base, channel_multiplier=-sgn),
                                reads=[ones], writes=[m])
                    m4.append(m)
                    mm3 = P.sb(f"m3_{z}", [64, 64], F32, es=ph)
                    P.op("pool", lambda e, mm3=mm3, z=z: e.affine_select(
                        out=mm3.a(), in_=ones[0:64, 0:64], pattern=[[-1 if z == 0 else 1, 64]], compare_op=ALU.is_ge, fill=0.0,
                        base=-1, channel_multiplier=1 if z == 0 else -1), reads=[ones], writes=[mm3])
                    m3.append(mm3)
                QI0 = P.sb("QI0", [64, 64], BF16, es=ph)
                k.cp("dve", V(QI0), (ident, ident[0:64, 0:64]))

                nheads = 0 if "rw1" in debug else (1 if ("rw2" in debug or "rw3" in debug) else 8)
                for h in range(nheads):
                    with ExitStack() as hs:
                        Vt = P.sb("Vt", [64, NCH, CH], F32, es=hs)
                        oacc = P.sb("oaccr", [64, NCH, CH], F32, es=hs)
                        bon = P.sb("bon", [64, NCH], F32, es=hs)
                        hs2 = ExitStack()
                        AR = [P.sb(f"AR{z}", [64, NCH, 2, CH], BF16, es=hs2) for z in range(2)]
                        BK = [P.sb(f"BK{z}", [64, NCH, 2, CH], BF16, es=hs2) for z in range(2)]
                        BKeT = [P.sb(f"BKeT{z}", [128, NCH, CH], BF16, es=hs2) for z in range(2)]
                        UV = [P.sb(f"UV{z}", [128, NCH, CH], BF16, es=hs2) for z in range(2)]
                        ZV = P.sb("ZV", [128, NCH, CH], BF16, es=hs2)
                        gC = [P.sb(f"gC{z}", [64, NCH], F32, es=hs2) for z in range(2)]
                        k.memset("pool", (ZV, ZV[0:64, :, :], "z"), 0.0)
                        with ExitStack() as pp_:
                            def T(name, dt=F32, w=BW):
                                return P.sb(name, [64, w], dt, es=pp_)
                            t_r = T("t_r"); t_k = T("t_k"); t_v = T("t_v")
                            t_wd = P.sb("t_wd", [128, BW], F32, es=pp_)
                            t_ad = P.sb("t_ad", [128, BW], F32, es=pp_)
                            t_kk = T("t_kk"); t_q = T("t_q"); t_rn = T("t_rn")
                            t_sg = T("t_sg"); t_cs = T("t_cs"); t_x = T("t_x"); t_y = T("t_y")
                            t_eg = T("t_eg"); t_eng = T("t_eng"); t_egp = T("t_egp")
                            t_a = T("t_a"); t_km = [T("t_km0"), T("t_km1")]; t_b = T("t_b")
                            bke = P.sb("bke", [64, CB, 2, CH], BF16, es=pp_)
                            t_v2 = P.sb("t_v2", [64, CB, 2, CH], F32, es=pp_)
                            pwa = [P.ps(f"pwa{i}", [64, BW], F32, es=pp_) for i in range(2)]
                            pss = P.ps("pss", [64, BW], F32, es=pp_)
                            ptv = P.ps("ptv", [128, CB, CH], F32, es=pp_)
                            ptb = P.ps("ptb", [128, CB, CH], BF16, es=pp_)
                            pbn = P.ps("pbn", [64, NCH], F32, es=pp_)
                            for blk in range(NB):
                                tsl = slice(blk * BW, (blk + 1) * BW)
                                csl = slice(blk * CB, (blk + 1) * CB)
                                k.dma(V(t_r), (PR, PR[h * 64:(h + 1) * 64, tsl]))
                                k.dma(V(t_k), (PR, PR[512 + h * 64:512 + (h + 1) * 64, tsl]))
                                k.dma(V(t_v), (PR, PR[1024 + h * 64:1024 + (h + 1) * 64, tsl]))
                                k.dma(V(t_wd), (PR, PR[1536:1664, tsl]))
                                k.dma(V(t_ad), (PR, PR[1664:1792, tsl]))
                                k.act(V(t_wd), V(t_wd), AF.Tanh)
                                k.cp("pool", V(t_v2), (t_v, t_v.a().rearrange("p (c o t) -> p c o t", o=1, t=CH).to_broadcast([64, CB, 2, CH])))
                                for j in range(CB):
                                    k.tr((ptv, ptv[:, j, :]), (t_v2, t_v2[:, j, :, :].rearrange("p a t -> p (a t)")), (ident, ident[0:64, 0:64]))
                                k.cp("act", (Vt, Vt[:, csl, :], blk), (ptv, ptv[0:64, :, :]))
                                for z in range(2):
                                    k.cp("dve", (UV[z], UV[z][64:128, csl, :], ("v", blk)), (ptv, ptv[64:128, :, :]))
                                k.cp("dve", (ZV, ZV[64:128, csl, :], ("v", blk)), (ptv, ptv[64:128, :, :]))
                                k.ts("dve", V(t_kk), V(t_k), (kkg, kkg[:, h:h + 1]), ALU.mult)
                                k.tt("pool", V(t_q), V(t_kk), V(t_kk), ALU.mult)
                                k.mm(V(pss), V(ones64), V(t_q))
                                k.ts("dve", V(t_rn), V(pss), 1e-12, ALU.add)
                                k.act(V(t_rn), V(t_rn), AF.Sqrt)
                                k.recip(V(t_rn), V(t_rn))
                                k.tt("dve", V(t_kk), V(t_kk), V(t_rn), ALU.mult)
                                for z in range(2):
                                    end = CH - 1 if z == 0 else 0
                                    zs = slice(z * 64, (z + 1) * 64)
                                    pw = pwa[0]; pa = pwa[1]
                                    k.mm(V(pw), (w2all, w2all[zs, h * 64:(h + 1) * 64]), (t_wd, t_wd[zs, :]))
                                    k.mm(V(pa), (a2all, a2all[zs, h * 64:(h + 1) * 64]), (t_ad, t_ad[zs, :]))
                                    k.act(V(t_sg), V(pw), AF.Sigmoid, bias=(w0[z], w0[z][:, h:h + 1]))
                                    k.act(V(t_a), V(pa), AF.Sigmoid, bias=(a0[z], a0[z][:, h:h + 1]))
                                    k.scan(V(t_cs), V(rst), V(t_sg), 0.0, ALU.mult, ALU.add)
                                    if z == 1:
                                        k.tt("dve", V(t_x), V(t_sg), V(t_cs), ALU.subtract)
                                        c3 = t_cs.a().rearrange("p (c t) -> p c t", t=CH)
                                        k.tt("dve", (t_y, t_y.a().rearrange("p (c t) -> p c t", t=CH)),
                                             (t_x, t_x.a().rearrange("p (c t) -> p c t", t=CH)),
                                             (t_cs, c3[:, :, CH - 1:CH].to_broadcast([64, CB, CH])), ALU.add)
                                        cs = t_y
                                    else:
                                        cs = t_cs
                                    k.act(V(t_eg), V(cs), AF.Exp, scale=-0.6065306597126334)
                                    k.act(V(t_eng), V(cs), AF.Exp, scale=0.6065306597126334)
                                    k.tt("dve", V(t_x), V(cs), V(t_sg), ALU.subtract)
                                    k.act(V(t_egp), V(t_x), AF.Exp, scale=-0.6065306597126334)
                                    eg3 = t_eg.a().rearrange("p (c t) -> p c t", t=CH)
                                    k.cp("pool", (gC[z], gC[z][:, csl], blk), (t_eg, eg3[:, :, end]))
                                    k.ts("dve", V(t_x), V(t_a), (kag, kag[:, h:h + 1]), ALU.mult, (oka, oka[:, h:h + 1]), ALU.add)
                                    k.tt("dve", V(t_km[z]), V(t_k), V(t_x), ALU.mult)
                                    k.tt("pool", V(t_b), V(t_kk), V(t_a), ALU.mult)
                                    arz = AR[z]; bkz = BK[z]
                                    k.stt("dve", (arz, arz[:, csl, 0, :], blk), (t_kk, t_kk.a().rearrange("p (c t) -> p c t", t=CH)), -1.0,
                                          (t_egp, t_egp.a().rearrange("p (c t) -> p c t", t=CH)), ALU.mult, ALU.mult)
                                    k.tt("pool", (arz, arz[:, csl, 1, :], blk), (t_r, t_r.a().rearrange("p (c t) -> p c t", t=CH)),
                                         (t_eg, eg3), ALU.mult)
                                    k.tt("dve", V(t_b), V(t_b), V(t_eng), ALU.mult)
                                    k.tt("dve", V(t_x), V(t_km[z]), V(t_eng), ALU.mult)
                                    k.cp("pool", (bkz, bkz[:, csl, 0, :], blk), (t_b, t_b.a().rearrange("p (c t) -> p c t", t=CH)))
                                    k.cp("act", (bkz, bkz[:, csl, 1, :], blk), (t_x, t_x.a().rearrange("p (c t) -> p c t", t=CH)))
                                    gcb = eg3[:, :, end:end + 1].to_broadcast([64, CB, CH])
                                    k.tt("dve", (bke, bke[:, :, 0, :]), (t_b, t_b.a().rearrange("p (c t) -> p c t", t=CH)), (t_eg, gcb), ALU.mult)
                                    k.tt("pool", (bke, bke[:, :, 1, :]), (t_x, t_x.a().rearrange("p (c t) -> p c t", t=CH)), (t_eg, gcb), ALU.mult)
                                    for j in range(CB):
                                        k.tr((ptb, ptb[:, j, :]), (bke, bke[:, j, :, :].rearrange("p a t -> p (a t)")), (identb, identb[0:64, 0:64]))
                                    k.cp("act", (BKeT[z], BKeT[z][:, csl, :], blk), V(ptb))
                                k.tt("dve", V(t_x), V(t_km[0]), V(t_km[1]), ALU.add)
                                k.stt("dve", V(t_x), V(t_r), (rkg, rkg[:, h:h + 1]), V(t_x), ALU.mult, ALU.mult)
                                for j in range(CB):
                                    c = blk * CB + j
                                    k.mm((pbn, pbn[:, c:c + 1]), (t_x, t_x[:, j * CH:(j + 1) * CH]), (ones64, ones64[:, 0:1]))
                            k.cp("dve", V(bon), V(pbn))
                        P.barrier()
                        with ExitStack() as us:
                            Hs = [P.sb(f"Hs{z}", [64, 64], F32, es=us) for z in range(2)]
                            Hb = [P.sb(f"Hb{z}", [64, 64], BF16, es=us) for z in range(2)]
                            AM = [[P.sb(f"AM{z}{i}", [128, 128], BF16, es=us) for i in range(2)] for z in range(2)]
                            Pm = [[P.sb(f"Pm{z}{i}", [64, 64], BF16, es=us) for i in range(2)] for z in range(2)]
                            QR = [[P.sb(f"QR{z}{i}", [64, 2, 64], BF16, es=us) for i in range(2)] for z in range(2)]
                            TT = [[P.sb(f"TT{z}{i}", [64, 64], BF16, es=us) for i in range(2)] for z in range(2)]
                            Xs = [P.sb(f"Xs{z}", [64, 64], BF16, es=us) for z in range(2)]
                            bA = [P.ps(f"bA{z}", [128, 512], F32, es=us) for z in range(2)]
                            bB = [P.ps(f"bB{z}", [64, 128], F32, es=us) for z in range(2)]
                            bC = [P.ps(f"bC{z}", [64, 64], F32, es=us) for z in range(2)]
                            bD = [P.ps(f"bD{z}", [64, 128], F32, es=us) for z in range(2)]
                            vM = [bA[z][:, 0:128] for z in range(2)]
                            vX = [bA[z][0:64, 128:192] for z in range(2)]
                            vU = [bA[z][0:64, 192:256] for z in range(2)]
                            vL = [bB[z].a() for z in range(2)]
                            vP = [bC[z].a() for z in range(2)]
                            vO = [bD[z][:, 0:64] for z in range(2)]
                            vH = [bD[z][:, 64:128] for z in range(2)]
                            pM = [(bA[z], vM[z]) for z in range(2)]
                            pL = [(bB[z], vL[z]) for z in range(2)]
                            pP = [(bC[z], vP[z]) for z in range(2)]
                            pX = [(bA[z], vX[z]) for z in range(2)]
                            pU = [(bA[z], vU[z]) for z in range(2)]
                            pO = [(bD[z], vO[z]) for z in range(2)]
                            pH = [(bD[z], vH[z]) for z in range(2)]
                            k.memset("pool", V(oacc), 0.0)
                            for z in range(2):
                                k.memset("pool", V(Hs[z]), 0.0)
                                k.memset("pool", V(Hb[z]), 0.0)
                            orders = [chunk_order(0), chunk_order(1)]
                            for step in range(NCH if "rw2" not in debug else 0):
                                for z in range(2):
                                    c = orders[z][step]
                                    am = AM[z][step % 2]
                                    arc = AR[z][:, c, :, :].rearrange("p a t -> p (a t)")
                                    bkc = BK[z][:, c, :, :].rearrange("p a t -> p (a t)")
                                    k.mm(pM[z], (BK[z], bkc), (AR[z], arc))
                                    k.tt("dve", V(am), pM[z], (m4[z], m4[z].a().rearrange("p a t -> p (a t)")), ALU.mult)
                                    k.mm(pP[z], (AR[z], AR[z][:, c, 0, :]), (BK[z], BK[z][:, c, 0, :]))
                                    pm_ = Pm[z][0]
                                    k.tt("pool" if False else "dve", V(pm_), pP[z], V(m3[z]), ALU.mult)
                                    if "u1" in debug:
                                        continue
                                    qr = QR[z][0]
                                    k.cp("act", (qr, qr[:, 0, :]), (am, am[0:64, 0:64]))
                                    k.cp("pool", (qr, qr[:, 1, :]), V(QI0))
                                    for lv in range(1, NL + 1):
                                        qn = QR[z][lv % 2]
                                        pn = Pm[z][lv % 2]
                                        k.mm(pL[z], V(pm_), (qr, qr.a().rearrange("p a t -> p (a t)")))
                                        k.mm(pP[z], (qr, qr[:, 0, :]), V(pm_))
                                        k.cp("act", (qn, qn[:, 0, :]), (bB[z], vL[z][:, 0:64]))
                                        k.tt("dve", (qn, qn[:, 1, :]), (bB[z], vL[z][:, 64:128]), (qr, qr[:, 1, :]), ALU.add)
                                        k.cp("act", V(pn), pP[z])
                                        qr = qn; pm_ = pn
                                    tt_ = TT[z][step % 2]
                                    k.mm((bB[z], vL[z][:, 0:64]), V(pm_), (qr, qr[:, 1, :]))
                                    k.tt("dve", V(tt_), (bB[z], vL[z][:, 0:64]), (qr, qr[:, 1, :]), ALU.add)
                                    if "u2" in debug:
                                        continue
                                    k.mm(pX[z], (AR[z], AR[z][:, c, 0, :]), V(Hb[z]), start=True, stop=False)
                                    P.op("pe", lambda e, px=vX[z], am=am, c=c: e.matmul(px, lhsT=am[:, 0:64], rhs=ZV[:, c, :], start=False, stop=True),
                                         reads=[(am, None), (ZV, "z"), (ZV, ("v", c // CB))], writes=[(bA[z], None)], pe_acc=True)
                                    k.cp("act", V(Xs[z]), pX[z])
                                    k.mm(pU[z], V(tt_), V(Xs[z]))
                                    k.cp("dve", (UV[z], UV[z][0:64, c, :], ("u", c)), pU[z])
                                    if "u3" in debug:
                                        continue
                                    k.mm(pO[z], (AR[z], AR[z][:, c, 1, :]), V(Hb[z]), start=True, stop=False)
                                    P.op("pe", lambda e, po=vO[z], am=am, uv=UV[z], c=c: e.matmul(po, lhsT=am[:, 64:128], rhs=uv[:, c, :], start=False, stop=True),
                                         reads=[(am, None), (UV[z], ("u", c)), (UV[z], ("v", c // CB))], writes=[(bD[z], None)], pe_acc=True)
                                    k.tt("dve", (oacc, oacc[:, c, :], c), (oacc, oacc[:, c, :], c), pO[z], ALU.add)
                                    P.op("pe", lambda e, ph_=vH[z], bt=BKeT[z], uv=UV[z], c=c: e.matmul(ph_, lhsT=bt[:, c, :], rhs=uv[:, c, :], start=True, stop=True),
                                         reads=[(BKeT[z], None), (UV[z], ("u", c)), (UV[z], ("v", c // CB))], writes=[(bD[z], None)])
                                    k.stt("dve", V(Hs[z]), V(Hs[z]), (gC[z], gC[z][:, c:c + 1]), pH[z], ALU.mult, ALU.add)
                                    k.cp("act", V(Hb[z]), V(Hs[z]))
                        if "d_oacc" in debug and h == 0:
                            do = P.dram("d_oacc", [64, NCH * CH], F32, kind="ExternalOutput")
                            k.dma(V(do), (oacc, oacc.a().rearrange("p a b -> p (a b)")))
                            dm4 = P.dram("d_m4", [128, 2 * 128], F32, kind="ExternalOutput")
                            for z in range(2):
                                k.dma((dm4, dm4[:, z * 128:(z + 1) * 128]), (m4[z], m4[z].a().rearrange("p a b -> p (a b)")))
                            dm3 = P.dram("d_m3", [64, 2 * 64], F32, kind="ExternalOutput")
                            for z in range(2):
                                k.dma((dm3, dm3[:, z * 64:(z + 1) * 64]), V(m3[z]))
                            dgc = P.dram("d_gC", [64, 2 * NCH], F32, kind="ExternalOutput")
                            for z in range(2):
                                k.dma((dgc, dgc[:, z * NCH:(z + 1) * NCH]), V(gC[z]))
                            dbon = P.dram("d_bon", [64, NCH], F32, kind="ExternalOutput")
                            k.dma(V(dbon), V(bon))
                            dar = P.dram("d_AR", [64, 2 * NCH * 2 * CH], BF16, kind="ExternalOutput")
                            dbk = P.dram("d_BK", [64, 2 * NCH * 2 * CH], BF16, kind="ExternalOutput")
                            for z in range(2):
                                k.dma((dar, dar[:, z * NCH * 128:(z + 1) * NCH * 128]), (AR[z], AR[z].a().rearrange("p a b c -> p (a b c)")))
                                k.dma((dbk, dbk[:, z * NCH * 128:(z + 1) * NCH * 128]), (BK[z], BK[z].a().rearrange("p a b c -> p (a b c)")))
                        P.barrier()
                        hs2.close()
                        with ExitStack() as fs:
                            gt = P.sb("gtr", [64, NCH, CH], F32, es=fs)
                            cen = P.sb("cen", [64, NCH, CH], F32, es=fs)
                            mu_ = P.sb("mu_", [64, NCH], F32, es=fs)
                            var = P.sb("var", [64, NCH], F32, es=fs)
                            k.dma(V(gt), (GT, GT[:, h * 64:(h + 1) * 64].rearrange("(c p) d -> p c d", p=CH)))
                            P.op("dve", lambda e: e.tensor_reduce(out=mu_.a(), in_=oacc.a(), axis=AX.X, op=ALU.add), reads=[oacc], writes=[mu_])
                            k.ts("dve", V(mu_), V(mu_), 1.0 / 64, ALU.mult)
                            b3 = lambda t: t.a().rearrange("p (c o) -> p c o", o=1).to_broadcast([64, NCH, CH])
                            k.tt("dve", V(oacc), V(oacc), (mu_, b3(mu_)), ALU.subtract)
                            k.tt("pool", V(cen), V(oacc), V(oacc), ALU.mult)
                            P.op("dve", lambda e: e.tensor_reduce(out=var.a(), in_=cen.a(), axis=AX.X, op=ALU.add), reads=[cen], writes=[var])
                            k.ts("dve", V(var), V(var), 1.0 / 64, ALU.mult, 64e-5, ALU.add)
                            k.act(V(var), V(var), AF.Sqrt)
                            k.recip(V(var), V(var))
                            k.tt("dve", V(oacc), V(oacc), (var, b3(var)), ALU.mult)
                            rb = lambda t: t[:, h * 64:(h + 1) * 64].rearrange("p (o d) -> p o d", o=1).to_broadcast([64, NCH, CH])
                            k.tt("pool", V(oacc), V(oacc), (lnw, rb(lnw)), ALU.mult)
                            k.tt("dve", V(oacc), V(oacc), (lnb, rb(lnb)), ALU.add)
                            k.tt("pool", V(cen), V(Vt), (bon, b3(bon)), ALU.mult)
                            k.tt("dve", V(oacc), V(oacc), V(cen), ALU.add)
                            k.tt("dve", V(oacc), V(oacc), V(gt), ALU.mult)
                            k.dma((YM, YM[:, h * 64:(h + 1) * 64].rearrange("(c p) d -> p c d", p=CH), ("r", h)), V(oacc))
                        P.barrier()

        def lat_rows(tt, cc):
            c0 = 2 * (tt - 2) + cc
            return XL[LC:, :].rearrange("(r c) d -> c r d", c=64)[c0]

        def stage_outproj(YM, nfeat, W, xsrc, perm, tiles):
            kf = nfeat // 128
            with ExitStack() as ph:
                wb = P.sb("wob", [128, kf, D], BF16, es=ph)
                k.dma(V(wb), (W[0], W[1].rearrange("(ko p) n -> p ko n", p=128)), eng="pool")
                yt = [P.sb(f"yt{i}", [128, nfeat], F32, es=ph) for i in range(2)]
                yT = [P.sb(f"yT{i}", [128, kf, 128], BF16, es=ph) for i in range(2)]
                xo = [P.sb(f"xo{i}", [128, D], F32, es=ph) for i in range(2)]
                xn_ = [P.sb(f"xq{i}", [128, D], F32, es=ph) for i in range(2)]
                pT = [P.ps(f"poT{i}", [128, 8, 128], F32, es=ph) for i in range(2)]
                po = [P.ps(f"poo{i}", [128, 512], F32, es=ph) for i in range(2)]
                for n_, tt in enumerate(tiles):
                    j = 1 if tt < 2 else 0
                    y_ = yt[n_ % 2]; yT_ = yT[n_ % 2]; x_ = xo[n_ % 2]; q_ = xn_[n_ % 2]
                    k.dma(V(y_), (YM, YM[tt * 128:(tt + 1) * 128, :]))
                    if perm and tt >= 2:
                        for cc in range(2):
                            k.dma((x_, x_[cc * 64:(cc + 1) * 64, :], cc), (xsrc(tt)[0], lat_rows(tt, cc), tt))
                    else:
                        sb_, sap = xsrc(tt)
                        k.dma(V(x_), (sb_, sap, tt))
                    for g8 in range(0, kf, 8):
                        p_ = pT[(g8 // 8 + n_) % 2]
                        for ko in range(8):
                            k.tr((p_, p_[:, ko, :]), (y_, y_[:, (g8 + ko) * 128:(g8 + ko + 1) * 128]), V(ident))
                        k.cp("act", (yT_, yT_[:, g8:g8 + 8, :]), V(p_))
                    for nb in range(2):
                        o_ = po[nb]
                        for ko in range(kf):
                            k.mm(V(o_), (yT_, yT_[:, ko, :]), (wb, wb[:, ko, nb * 512:(nb + 1) * 512]), start=(ko == 0), stop=(ko == kf - 1))
                        k.tt("dve", (q_, q_[:, nb * 512:(nb + 1) * 512]), V(o_), (modB, modB[:, 0, j, nb * 512:(nb + 1) * 512]), ALU.mult)
                    k.tt("pool", V(q_), V(q_), V(x_), ALU.add)
                    if perm and tt >= 2:
                        for cc in range(2):
                            k.dma((XL, lat_rows(tt, cc), tt), (q_, q_[cc * 64:(cc + 1) * 64, :]))
                    else:
                        k.dma((XL, XL[tt * 128:(tt + 1) * 128, :], tt), V(q_))
                P.barrier()

        def stage_moe(l, tiles):
            nh = len(tiles) // 2
            with ExitStack() as ph:
                rw32 = P.sb("rw32", [128, KO, 32], F32, es=ph)
                k.dma(V(rw32), V(I["router_w"], I["router_w"][l].rearrange("(ko p) e -> p ko e", p=128)))
                rbB = P.sb("rbB", [128, 32], F32, es=ph)
                k.dma(V(rbB), V(I["router_b"], I["router_b"][l].partition_broadcast(128)))
                BD = P.sb("BD", [32, D], F32, es=ph)
                k.dma(V(BD), V(I["exp_b_down"], I["exp_b_down"][l]))
                bgF = P.sb("bgF", [128, 32, 8], F32, es=ph)
                buF = P.sb("buF", [128, 32, 8], F32, es=ph)
                with ExitStack() as t0:
                    btmp = P.sb("btmp", [128, 128], F32, es=t0)
                    pbt = P.ps("pbt", [128, 128], F32, es=t0)
                    for (dst, src) in ((bgF, I["exp_b_gate"]), (buF, I["exp_b_up"])):
                        for hf in range(2):
                            k.dma(V(btmp), (src, src[l, hf * 16:(hf + 1) * 16, :].rearrange("e (fb p) -> (e fb) p", p=128)))
                            k.tr(V(pbt), V(btmp), V(ident))
                            k.cp("dve", (dst, dst[:, hf * 16:(hf + 1) * 16, :].rearrange("p e f -> p (e f)")), V(pbt))
                    P.barrier()
                for half in range(2):
                    htiles = tiles[half * nh:(half + 1) * nh]
                    NTK = nh * 128
                    with ExitStack() as hs:
                        HTh = P.sb("HTh", [128, KO, NTK], BF16, es=hs)
                        LG = P.sb("LG", [128, nh, 32], F32, es=hs)
                        GW = P.sb("GW", [128, nh, 32], F32, es=hs)
                        acc = P.sb("acc", [128, nh, D], F32, es=hs)
                        with ExitStack() as ns:
                            xt = [P.sb(f"mxt{i}", [128, D], F32, es=ns) for i in range(2)]
                            xn = [P.sb(f"mxn{i}", [128, D], F32, es=ns) for i in range(2)]
                            junk = P.sb("mjunk", [128, D], F32, es=ns)
                            st = [P.sb(f"mst{i}", [128, 4], F32, es=ns) for i in range(2)]
                            tmp = [P.sb(f"mtmp{i}", [128, KO, 128], F32, es=ns) for i in range(2)]
                            h32 = [P.sb(f"mh32{i}", [128, KO, 128], F32, es=ns) for i in range(2)]
                            pT = [P.ps(f"mpT{i}", [128, KO, 128], F32, es=ns) for i in range(2)]
                            plg = [P.ps(f"plg{i}", [128, 32], F32, es=ns) for i in range(2)]
                            pgt = P.ps("pgt", [32, 128], F32, es=ns)
                            GWT = P.sb("GWT", [32, nh, 128], F32, es=ns)
                            pini = [P.ps("pini0", [128, 512], F32, es=ns)] * 2
                            m8 = P.sb("m8", [128, 8], F32, es=ns)
                            msk = P.sb("msk", [128, 32], F32, es=ns)
                            ex = P.sb("ex", [128, 32], F32, es=ns)
                            sm = P.sb("smx", [128, 4], F32, es=ns)
                            for i, tt in enumerate(htiles):
                                j = 1 if tt < 2 else 0
                                x_ = xt[i % 2]; n_ = xn[i % 2]; s_ = st[i % 2]; t_ = tmp[i % 2]; p_ = pT[i % 2]; h_ = h32[i % 2]
                                k.dma(V(x_), (XL, XL[tt * 128:(tt + 1) * 128, :]))
                                k.memset("pool", (s_, s_[:, 0:1]), 0.0)
                                k.act(V(junk), V(x_), AF.Square, accum=(s_, s_[:, 0:1]))
                                k.ts("dve", (s_, s_[:, 1:2]), (s_, s_[:, 0:1]), 1.0 / D, ALU.mult, EPS, ALU.add)
                                k.act((s_, s_[:, 2:3]), (s_, s_[:, 1:2]), AF.Sqrt)
                                k.recip((s_, s_[:, 3:4]), (s_, s_[:, 2:3]))
                                k.ts("dve", V(n_), V(x_), (s_, s_[:, 3:4]), ALU.mult)
                                for ko in range(KO):
                                    k.tr((p_, p_[:, ko, :]), (n_, n_[:, ko * 128:(ko + 1) * 128]), V(ident))
                                k.tt("dve", V(t_), V(p_), (g2F, g2F[:, :, j:j + 1].to_broadcast([128, KO, 128])), ALU.mult)
                                k.tt("pool", V(h_), V(t_), (modF, modF[:, 3, :, j:j + 1].to_broadcast([128, KO, 128])), ALU.add)
                                k.cp("act", (HTh, HTh[:, :, i * 128:(i + 1) * 128], i), V(h_))
                                pl = plg[i % 2]
                                for ko in range(KO):
                                    k.mm(V(pl), (h_, h_[:, ko, :]), (rw32, rw32[:, ko, :]), start=(ko == 0), stop=(ko == KO - 1))
                                lg = (LG, LG[:, i, :], i)
                                k.tt("dve", lg, V(pl), V(rbB), ALU.add)
                                P.op("dve", lambda e, i=i: e.max(out=m8.a(), in_=LG[:, i, :]), reads=[(LG, i)], writes=[m8])
                                k.ts("dve", V(msk), lg, (m8, m8[:, 3:4]), ALU.is_ge)
                                k.ts("dve", (sm, sm[:, 0:1]), (m8, m8[:, 0:1]), -1.0, ALU.mult)
                                k.act(V(ex), lg, AF.Exp, bias=(sm, sm[:, 0:1]))
                                k.tt("dve", V(ex), V(ex), V(msk), ALU.mult)
                                P.op("dve", lambda e: e.tensor_reduce(out=sm[:, 1:2], in_=ex.a(), axis=AX.X, op=ALU.add), reads=[ex], writes=[sm])
                                k.recip((sm, sm[:, 2:3]), (sm, sm[:, 1:2]))
                                k.ts("dve", (GW, GW[:, i, :], i), V(ex), (sm, sm[:, 2:3]), ALU.mult)
                                k.tr(V(pgt), (GW, GW[:, i, :], i), V(ident))
                                k.cp("act", (GWT, GWT[:, i, :], i), V(pgt))
                                for nb in range(2):
                                    k.mm(V(pini[nb]), (GWT, GWT[:, i, :], i), (BD, BD[:, nb * 512:(nb + 1) * 512]))
                                    k.cp("act" if nb else "dve", (acc, acc[:, i, nb * 512:(nb + 1) * 512], (i, nb)), V(pini[nb]))
                            P.barrier()
                        if f"LG{l}" in debug and half == 0:
                            dl = P.dram(f"d_GW{l}", [128, nh * 32], F32, kind="ExternalOutput")
                            k.dma(V(dl), (GW, GW.a().rearrange("p a b -> p (a b)")))
                        with ExitStack() as xs:
                            wgs = [P.sb(f"wg{i}", [128, KO, 512], BF16, es=xs) for i in range(2)]
                            wus = [P.sb(f"wu{i}", [128, KO, 512], BF16, es=xs) for i in range(2)]
                            wds = [P.sb(f"wd{i}", [128, 4, D], BF16, es=xs) for i in range(2)]
                            aTs = [P.sb(f"aT{i}", [128, 4, 512], BF16, es=xs) for i in range(2)]
                            dtmp = [P.sb(f"dtmp{i}", [128, 512], F32, es=xs) for i in range(2)]
                            g1 = [P.sb(f"g1_{i}", [128, 512], F32, es=xs) for i in range(2)]
                            sg = [P.sb(f"sg_{i}", [128, 512], F32, es=xs) for i in range(2)]
                            u1 = [P.sb(f"u1_{i}", [128, 512], F32, es=xs) for i in range(2)]
                            pg = [P.ps(f"mpg{i}", [128, 512], F32, es=xs) for i in range(2)]
                            pu = [P.ps(f"mpu{i}", [128, 512], F32, es=xs) for i in range(2)]
                            pd = [P.ps(f"mpd{i}", [128, 512], F32, es=xs) for i in range(2)]
                            if nh == 17:
                                tws = [512, 512, 384, 384, 384]
                            else:
                                tws = [512] * (NTK // 512)
                            tblocks = []
                            t_acc = 0
                            for tw in tws:
                                tblocks.append((t_acc, tw)); t_acc += tw
                            nexp = 32 if "moe_fast" not in debug else 2
                            cnt = 0
                            nblk = 0
                            cntbox = [0]

                            def emit_loads(he):
                                e, fh = he // 2, he % 2
                                wg = wgs[he % 2]; wu = wus[he % 2]; wd = wds[he % 2]
                                k.dma(V(wg), (I["exp_w_gate"], I["exp_w_gate"][l, e][:, fh * 512:(fh + 1) * 512].rearrange("(ko p) n -> p ko n", p=128)), eng="pool")
                                k.dma(V(wu), (I["exp_w_up"], I["exp_w_up"][l, e][:, fh * 512:(fh + 1) * 512].rearrange("(ko p) n -> p ko n", p=128)), eng="pool")
                                k.dma(V(wd), (I["exp_w_down"], I["exp_w_down"][l, e][fh * 512:(fh + 1) * 512, :].rearrange("(fo p) n -> p fo n", p=128)), eng="pool")

                            def emit_G(j, he, t0_, tw):
                                e, fh = he // 2, he % 2
                                wg = wgs[he % 2]; wu = wus[he % 2]
                                aT = aTs[j % 2]
                                for fbl in range(4):
                                    fb = fh * 4 + fbl
                                    cnt = cntbox[0]; cntbox[0] += 1
                                    pg_ = pg[cnt % 2]; pu_ = pu[cnt % 2]; g_ = g1[cnt % 2]; s_ = sg[cnt % 2]; u_ = u1[cnt % 2]
                                    for ko in range(KO):
                                        k.mm((pg_, pg_[:, 0:tw]), (wg, wg[:, ko, fbl * 128:(fbl + 1) * 128]), (HTh, HTh[:, ko, t0_:t0_ + tw]),
                                             start=(ko == 0), stop=(ko == KO - 1))
                                    for ko in range(KO):
                                        k.mm((pu_, pu_[:, 0:tw]), (wu, wu[:, ko, fbl * 128:(fbl + 1) * 128]), (HTh, HTh[:, ko, t0_:t0_ + tw]),
                                             start=(ko == 0), stop=(ko == KO - 1))
                                    k.ts("dve", (g_, g_[:, 0:tw]), (pg_, pg_[:, 0:tw]), (bgF, bgF[:, e, fb:fb + 1]), ALU.add, 7.0, ALU.min)
                                    k.act((s_, s_[:, 0:tw]), (g_, g_[:, 0:tw]), AF.Sigmoid, scale=1.702)
                                    k.act((u_, u_[:, 0:tw]), (pu_, pu_[:, 0:tw]), AF.Identity, bias=(buF, buF[:, e, fb:fb + 1]))
                                    k.ts("dve", (u_, u_[:, 0:tw]), (u_, u_[:, 0:tw]), 7.0, ALU.min, -7.0, ALU.max)
                                    k.tt("dve", (g_, g_[:, 0:tw]), (g_, g_[:, 0:tw]), (s_, s_[:, 0:tw]), ALU.mult)
                                    k.stt("dve", (aT, aT[:, fbl, 0:tw], fbl), (u_, u_[:, 0:tw]), 1.0, (g_, g_[:, 0:tw]), ALU.add, ALU.mult)

                            def emit_D(j, he, t0_, tw):
                                e = he // 2
                                wd = wds[he % 2]
                                aT = aTs[j % 2]
                                for ti in range(tw // 128):
                                    i = t0_ // 128 + ti
                                    for nb in range(2):
                                        pd_ = pd[nb]
                                        for fo in range(4):
                                            k.mm(V(pd_), (aT, aT[:, fo, ti * 128:(ti + 1) * 128], fo), (wd, wd[:, fo, nb * 512:(nb + 1) * 512]),
                                                 start=(fo == 0), stop=(fo == 3))
                                        asl = (acc, acc[:, i, nb * 512:(nb + 1) * 512], (i, nb))
                                        if nb == 0:
                                            k.stt("dve", asl, V(pd_), (GW, GW[:, i, e:e + 1], i), asl, ALU.mult, ALU.add)
                                        else:
                                            dt_ = dtmp[i % 2]
                                            k.act(V(dt_), V(pd_), AF.Copy, scale=(GW, GW[:, i, e:e + 1], i))
                                            k.tt("dve", asl, asl, V(dt_), ALU.add)

                            blocks = [(he, t0_, tw) for he in range(nexp * 2) for (t0_, tw) in tblocks]
                            emit_loads(0)
                            emit_G(0, *blocks[0])
                            for j in range(len(blocks)):
                                if j + 1 < len(blocks):
                                    if blocks[j + 1][0] != blocks[j][0]:
                                        emit_loads(blocks[j + 1][0])
                                    emit_G(j + 1, *blocks[j + 1])
                                emit_D(j, *blocks[j])
                            P.barrier()
                        with ExitStack() as rs:
                            xr = [P.sb(f"xr{i}", [128, D], F32, es=rs) for i in range(2)]
                            for i, tt in enumerate(htiles):
                                j = 1 if tt < 2 else 0
                                x_ = xr[i % 2]
                                k.dma(V(x_), (XL, XL[tt * 128:(tt + 1) * 128, :], ("m", tt)))
                                k.tt("dve", (acc, acc[:, i, :]), (acc, acc[:, i, :]), (modB, modB[:, 1, j, :]), ALU.mult)
                                k.tt("pool", V(x_), V(x_), (acc, acc[:, i, :]), ALU.add)
                                k.dma((XL, XL[tt * 128:(tt + 1) * 128, :], ("m", tt)), V(x_))
                            P.barrier()

        SEGS = ((0, LC), (LC, SEQ))

        def stage_conv(PF, PC, specs):
            with ExitStack() as ph:
                u = [P.sb(f"cu{i}", [128, L + 8], F32, es=ph) for i in range(2)]
                acc = [P.sb(f"ca{i}", [128, L], F32, es=ph) for i in range(2)]
                cw = P.sb("cw", [128, 20, 5], F32, es=ph)
                cb = P.sb("cb", [128, 20], F32, es=ph)
                for i in range(2):
                    k.memset("pool", (u[i], u[i][:, 0:2], "h0"), 0.0)
                    k.memset("pool", (u[i], u[i][:, LC + 2:LC + 6], "h1"), 0.0)
                    k.memset("pool", (u[i], u[i][:, L + 6:L + 8], "h2"), 0.0)
                bi = 0
                for (row0, nblk, wsrc, bsrc) in specs:
                    for b in range(nblk):
                        k.dma((cw, cw[:, bi, :], bi), (wsrc[0], wsrc[1][:, b * 128:(b + 1) * 128].rearrange("j p -> p j")), allow_slow_non_contiguous=True)
                        k.dma((cb, cb[:, bi:bi + 1], bi), (bsrc[0], bsrc[1][b * 128:(b + 1) * 128].rearrange("(p o) -> p o", o=1)), allow_slow_non_contiguous=True)
                        u_ = u[bi % 2]; a_ = acc[bi % 2]
                        r0 = row0 + b * 128
                        k.dma((u_, u_[:, 2:LC + 2], "c"), (PF, PF[r0:r0 + 128, 0:LC]))
                        k.dma((u_, u_[:, LC + 6:L + 6], "l"), (PF, PF[r0:r0 + 128, LC:L]))
                        for (s0, sl_) in SEGS:
                            off = 0 if s0 == 0 else 4
                            for j in range(5):
                                src = (u_, u_[:, s0 + off + j:s0 + off + j + sl_])
                                dst = (a_, a_[:, s0:s0 + sl_], s0)
                                if j == 0:
                                    k.ts("dve", dst, src, (cw, cw[:, bi, 0:1], bi), ALU.mult)
                                else:
                                    k.stt("dve", dst, src, (cw, cw[:, bi, j:j + 1], bi), dst, ALU.mult, ALU.add)
                            k.act((a_, a_[:, s0:s0 + sl_], s0), (a_, a_[:, s0:s0 + sl_], s0), AF.Silu, bias=(cb, cb[:, bi:bi + 1], bi))
                        k.dma((PC, PC[r0:r0 + 128, :], r0), V(a_))
                        bi += 1
                P.barrier()

        def mixer_mlstm(PC, PF, PT, YM, GSD):
            TB = [(t0_, min(512, L - t0_)) for t0_ in range(0, L, 512)]
            with ExitStack() as ph:
                GI = P.sb("GI", [8, L], F32, es=ph)
                GF = P.sb("GF", [8, L], F32, es=ph)
                t1 = P.sb("gt1", [8, L], F32, es=ph)
                t2 = P.sb("gt2", [8, L], F32, es=ph)
                t3 = P.sb("gt3", [8, L], F32, es=ph)
                rst = P.sb("grst", [8, L], F32, es=ph)
                ib = P.sb("ib", [8, 1], F32, es=ph)
                fb = P.sb("fb", [8, 1], F32, es=ph)
                k.dma(V(GI), (PF, PF[2560:2568, :]))
                k.dma(V(GF), (PF, PF[2568:2576, :]))
                k.dma(V(ib), V(I["mlstm_i_bias"], I["mlstm_i_bias"][0].rearrange("z (h o) -> (z h) o", o=1)), allow_slow_non_contiguous=True)
                k.dma(V(fb), V(I["mlstm_f_bias"], I["mlstm_f_bias"][0].rearrange("z (h o) -> (z h) o", o=1)), allow_slow_non_contiguous=True)
                k.memset("pool", V(rst), 1.0)
                k.memset("pool", (rst, rst.a().rearrange("p (c t) -> p c t", t=CH)[:, :, 0:1]), 0.0)
                k.ts("dve", V(GI), V(GI), (ib, ib[:, 0:1]), ALU.add)
                k.ts("dve", V(fb), V(fb), -1.0, ALU.mult)
                k.act(V(t1), V(GF), AF.Exp, bias=(fb, fb[:, 0:1]), scale=-1.0)
                k.act(V(t1), V(t1), AF.Ln, bias=1.0)
                k.ts("dve", V(t1), V(t1), -1.0, ALU.mult)
                k.scan(V(t2), V(rst), V(t1), 0.0, ALU.mult, ALU.add)
                k.tt("dve", V(t3), V(t1), V(t2), ALU.subtract)
                p3 = t2.a().rearrange("p (c t) -> p c t", t=CH)
                k.tt("dve", (t3, t3.a().rearrange("p (c t) -> p c t", t=CH)), (t3, t3.a().rearrange("p (c t) -> p c t", t=CH)),
                     (t2, p3[:, :, CH - 1:CH].to_broadcast([8, NCH, CH])), ALU.add)
                k.dma((GSD, GSD[0:4, :], 0), (t2, t2[0:4, :]))
                k.dma((GSD, GSD[4:8, :], 1), (t3, t3[4:8, :]))
                k.tt("dve", V(t2), V(GI), V(t2), ALU.subtract)
                k.tt("dve", V(t3), V(GI), V(t3), ALU.subtract)
                k.dma((GSD, GSD[8:12, :], 2), (t2, t2[0:4, :]))
                k.dma((GSD, GSD[12:16, :], 3), (t3, t3[4:8, :]))
                P.barrier()
            with ExitStack() as ph:
                masks = make_masks(ph)
                nwB = P.sb("mnwB", [64, 1024], F32, es=ph)
                k.dma(V(nwB), V(I["mlstm_norm_w"], I["mlstm_norm_w"][0].partition_broadcast(64)))
                oacc = P.sb("moacc", [64, NCH, 256], F32, es=ph)
                ssn = P.sb("mssn", [64, NCH], F32, es=ph)
                for h in range(4):
                    with ExitStack() as hs1:
                        vaug = P.sb("vaug", [64, NCH, 257], BF16, es=hs1)
                        t_q = P.sb("mt_q", [128, 512], F32, es=hs1)
                        t_k = P.sb("mt_k", [128, 512], F32, es=hs1)
                        t_e = P.sb("mt_e", [128, 512], F32, es=hs1)
                        t_s = P.sb("mt_s", [128, 512], F32, es=hs1)
                        cd = P.sb("mcd", [128, NCH], F32, es=hs1)
                        q_in = P.sb("mq_in", [128, L], BF16, es=hs1)
                        k_in = P.sb("mk_in", [128, L], BF16, es=hs1)
                        k_end = P.sb("mk_end", [128, L], BF16, es=hs1)
                        kET = P.sb("mkET", [64, NCH, 128], BF16, es=hs1)
                        S = P.sb("mS", [128, 257], F32, es=hs1)
                        Sb = P.sb("mSb", [128, 257], BF16, es=hs1)
                        Am = [P.sb(f"mAm{i}", [64, 64], BF16, es=hs1) for i in range(2)]
                        dn = P.sb("mdn", [64, 2], F32, es=hs1)
                        psA = P.ps("mpsA", [64, 64], F32, es=hs1)
                        pso = P.ps("mpso", [64, 257], F32, es=hs1)
                        pskv = P.ps("mpskv", [128, 257], F32, es=hs1)
                        pst = [P.ps(f"mpst{i}", [64, 4, 128], BF16, es=hs1) for i in range(2)]
                        k.dma((vaug, vaug[:, :, 0:256], "v"), (PT, PT[:, 1056 + h * 256:1056 + (h + 1) * 256].rearrange("(c p) d -> p c d", p=CH)), eng="pool")
                        k.memset("pool", (vaug, vaug[:, :, 256:257], "o"), 1.0)
                        k.memset("pool", V(oacc), 0.0)
                        for z in range(2):
                            end = CH - 1 if z == 0 else 0
                            for (t0_, tw) in TB:
                                tsl = slice(t0_, t0_ + tw)
                                ncb = tw // CH
                                c0 = t0_ // CH
                                k.dma((t_e, t_e[:, 0:tw]), (GSD, GSD[z * 4 + h, tsl].partition_broadcast(128)))
                                k.dma((t_s, t_s[:, 0:tw]), (GSD, GSD[8 + z * 4 + h, tsl].partition_broadcast(128)))
                                k.dma((t_q, t_q[:, 0:tw]), (PC, PC[1536 + h * 128:1536 + (h + 1) * 128, tsl]))
                                k.dma((t_k, t_k[:, 0:tw]), (PC, PC[2048 + h * 128:2048 + (h + 1) * 128, tsl]))
                                k.act((t_e, t_e[:, 0:tw]), (t_e, t_e[:, 0:tw]), AF.Exp)
                                k.act((t_s, t_s[:, 0:tw]), (t_s, t_s[:, 0:tw]), AF.Exp)
                                e3 = t_e[:, 0:tw].rearrange("p (c t) -> p c t", t=CH)
                                k.cp("pool", (cd, cd[:, c0:c0 + ncb]), (t_e, e3[:, :, end]))
                                k.tt("dve", (q_in, q_in[:, tsl]), (t_q, t_q[:, 0:tw]), (t_e, t_e[:, 0:tw]), ALU.mult)
                                k.stt("dve", (t_k, t_k[:, 0:tw]), (t_k, t_k[:, 0:tw]), 128 ** -0.5, (t_s, t_s[:, 0:tw]), ALU.mult, ALU.mult)
                                k.cp("pool", (k_in, k_in[:, tsl]), (t_k, t_k[:, 0:tw]))
                                k.tt("pool", (k_end, k_end[:, tsl].rearrange("p (c t) -> p c t", t=CH)),
                                     (t_k, t_k[:, 0:tw].rearrange("p (c t) -> p c t", t=CH)),
                                     (t_e, e3[:, :, end:end + 1].to_broadcast([128, ncb, CH])), ALU.mult)
                            for c4 in range(0, NCH, 4):
                                p_ = pst[(c4 // 4) % 2]
                                for j in range(4):
                                    c = c4 + j
                                    k.tr((p_, p_[:, j, :]), (k_end, k_end[:, c * CH:(c + 1) * CH]), V(identb))
                                k.cp("act", (kET, kET[:, c4:c4 + 4, :]), V(p_))
                            k.memset("pool", V(S), 0.0)
                            k.memset("pool", V(Sb), 0.0)
                            for step, c in enumerate(chunk_order(z)):
                                csl = slice(c * CH, (c + 1) * CH)
                                am = Am[step % 2]
                                k.mm(V(psA), (k_in, k_in[:, csl]), (q_in, q_in[:, csl]))
                                k.tt("dve", V(am), V(psA), V(masks[z]), ALU.mult)
                                k.mm(V(pso), V(am), (vaug, vaug[:, c, :]), start=True, stop=False)
                                k.mm(V(pso), (q_in, q_in[:, csl]), V(Sb), start=False, stop=True)
                                k.act((dn, dn[:, 0:1]), (pso, pso[:, 256:257]), AF.Abs)
                                k.ts("dve", (dn, dn[:, 0:1]), (dn, dn[:, 0:1]), 1.0, ALU.max)
                                k.recip((dn, dn[:, 1:2]), (dn, dn[:, 0:1]))
                                k.stt("dve", (oacc, oacc[:, c, :], c), (pso, pso[:, 0:256]), (dn, dn[:, 1:2]), (oacc, oacc[:, c, :], c), ALU.mult, ALU.add)
                                k.mm(V(pskv), (kET, kET[:, c, :]), (vaug, vaug[:, c, :]))
                                k.stt("dve", V(S), V(S), (cd, cd[:, c:c + 1]), V(pskv), ALU.mult, ALU.add)
                                k.cp("act", V(Sb), V(S))
                    P.barrier()
                    with ExitStack() as hs2:
                        gt = P.sb("mgt", [64, NCH, 256], F32, es=hs2)
                        k.tt("dve", V(gt), V(oacc), V(oacc), ALU.mult)
                        P.op("dve", lambda e: e.tensor_reduce(out=ssn.a(), in_=gt.a(), axis=AX.X, op=ALU.add), reads=[gt], writes=[ssn])
                        k.ts("dve", V(ssn), V(ssn), 1.0 / 256, ALU.mult, EPS, ALU.add)
                        k.act(V(ssn), V(ssn), AF.Sqrt)
                        k.recip(V(ssn), V(ssn))
                        k.tt("dve", V(oacc), V(oacc), (ssn, ssn.a().rearrange("p (c o) -> p c o", o=1).to_broadcast([64, NCH, 256])), ALU.mult)
                        k.tt("pool", V(oacc), V(oacc), (nwB, nwB[:, h * 256:(h + 1) * 256].rearrange("p (o d) -> p o d", o=1).to_broadcast([64, NCH, 256])), ALU.mult)
                        k.dma(V(gt), (PT, PT[:, 2080 + h * 256:2080 + (h + 1) * 256].rearrange("(c p) d -> p c d", p=CH)))
                        for q4 in range(4):
                            k.act((gt, gt[:, q4 * 17:(q4 + 1) * 17, :], q4), (gt, gt[:, q4 * 17:(q4 + 1) * 17, :], q4), AF.Sigmoid)
                        k.tt("dve", V(oacc), V(oacc), V(gt), ALU.mult)
                        k.dma((YM, YM[:, 1024 + h * 256:1024 + (h + 1) * 256].rearrange("(c p) d -> p c d", p=CH), ("m", h)), V(oacc))
                    P.barrier()

        def stage_xbt(PC, XBT):
            with ExitStack() as ph:
                ft = [P.sb(f"xft{i}", [128, 10, 128], F32, es=ph) for i in range(2)]
                ot = [P.sb(f"xot{i}", [128, 10, 128], F32, es=ph) for i in range(2)]
                pt = [P.ps(f"xpt{i}", [128, 4, 128], F32, es=ph) for i in range(3)]
                for tt in range(NT):
                    f_ = ft[tt % 2]; o_ = ot[tt % 2]
                    k.dma(V(f_), (PC, PC[0:1280, tt * 128:(tt + 1) * 128].rearrange("(b p) t -> p b t", p=128)))
                    for gi, (b0, nb_) in enumerate(((0, 4), (4, 4), (8, 2))):
                        p_ = pt[gi]
                        for j in range(nb_):
                            k.tr((p_, p_[:, j, :]), (f_, f_[:, b0 + j, :]), V(ident))
                        k.cp("act" if gi % 2 else "dve", (o_, o_[:, b0:b0 + nb_, :]), (p_, p_[:, 0:nb_, :]))
                    k.dma((XBT, XBT[tt * 128:(tt + 1) * 128, :], tt), (o_, o_.a().rearrange("p b c -> p (b c)")))
                P.barrier()

        def mixer_ssd(PC, PT, XBT, YS):
            with ExitStack() as ph:
                masks = make_masks(ph)
                ones64 = P.sb("sones64", [64, 64], F32, es=ph)
                k.memset("pool", V(ones64), 1.0)
                selend = []
                for z in range(2):
                    se = P.sb(f"selend{z}", [64, 128], F32, es=ph)
                    endp = CH - 1 if z == 0 else 0
                    P.op("pool", lambda e, se=se, endp=endp: e.affine_select(out=se.a(), in_=ones[0:64, :], pattern=[[0, 128]], compare_op=ALU.is_equal,
                                                                           fill=0.0, base=-endp, channel_multiplier=1), reads=[ones], writes=[se])
                    selend.append(se)
                dtT = P.sb("dtT", [64, NCH, 32], F32, es=ph)
                laT = P.sb("laT", [64, NCH, 32], F32, es=ph)
                dbB = P.sb("dbB", [64, 32], F32, es=ph)
                naB = P.sb("naB", [64, 32], F32, es=ph)
                dskB = P.sb("dskB", [64, 16], F32, es=ph)
                k.dma(V(dbB), V(I["ssd_dt_bias"], I["ssd_dt_bias"][0].rearrange("z h -> (z h)").partition_broadcast(64)))
                k.dma(V(naB), V(I["ssd_a_log"], I["ssd_a_log"][0].rearrange("z h -> (z h)").partition_broadcast(64)))
                k.dma(V(dskB), V(I["ssd_d"], I["ssd_d"][0].partition_broadcast(64)))
                k.act(V(naB), V(naB), AF.Exp)
                k.ts("dve", V(naB), V(naB), -1.0, ALU.mult)
                k.dma(V(dtT), (PT, PT[:, 1024:1056].rearrange("(c p) d -> p c d", p=CH)))
                k.tt("dve", V(dtT), V(dtT), (dbB, dbB.a().rearrange("p (o d) -> p o d", o=1).to_broadcast([64, NCH, 32])), ALU.add)
                k.act(V(dtT), V(dtT), AF.Exp)
                k.act(V(dtT), V(dtT), AF.Ln, bias=1.0)
                k.tt("dve", V(laT), V(dtT), (naB, naB.a().rearrange("p (o d) -> p o d", o=1).to_broadcast([64, NCH, 32])), ALU.mult)
                yacc = P.sb("yacc", [64, NCH, 256], F32, es=ph)
                for q4 in range(4):
                    g = q4 // 2
                    with ExitStack() as qs:
                        xq = P.sb("xq", [64, NCH, 256], BF16, es=qs)
                        Bf = P.sb("Bf", [128, L], BF16, es=qs)
                        Cf = P.sb("Cf", [128, L], BF16, es=qs)
                        BTt = P.sb("BTt", [64, NCH, 128], BF16, es=qs)
                        k.dma(V(xq), (XBT, XBT[:, q4 * 256:(q4 + 1) * 256].rearrange("(c p) d -> p c d", p=CH)), eng="pool")
                        k.dma(V(BTt), (XBT, XBT[:, 1024 + g * 128:1024 + (g + 1) * 128].rearrange("(c p) d -> p c d", p=CH)), eng="pool")
                        k.dma(V(Bf), (PC, PC[1024 + g * 128:1024 + (g + 1) * 128, :]), eng="pool")
                        k.dma(V(Cf), (PC, PC[1280 + g * 128:1280 + (g + 1) * 128, :]), eng="pool")
                        k.memset("pool", V(yacc), 0.0)
                        hs = [P.sb(f"hs{z}", [128, 256], F32, es=qs) for z in range(2)]
                        hsb = [P.sb(f"hsb{z}", [128, 256], BF16, es=qs) for z in range(2)]
                        CBm = [P.sb(f"CBm{z}", [64, 64], F32, es=qs) for z in range(2)]
                        acs = [P.sb(f"acs{z}", [64, 4], F32, es=qs) for z in range(2)]
                        dg = [P.sb(f"dg{z}", [64, 4, 64], F32, es=qs) for z in range(2)]
                        seg = [P.sb(f"seg{z}", [64, 4, 64], F32, es=qs) for z in range(2)]
                        AT = [P.sb(f"AT{z}", [64, 4, 64], BF16, es=qs) for z in range(2)]
                        xdt = [P.sb(f"xdt{z}", [64, 4, 64], BF16, es=qs) for z in range(2)]
                        xde = [P.sb(f"xde{z}", [64, 4, 64], BF16, es=qs) for z in range(2)]
                        din = [P.sb(f"din{z}", [64, 4], F32, es=qs) for z in range(2)]
                        dend = [P.sb(f"dend{z}", [64, 4], F32, es=qs) for z in range(2)]
                        dchB = [P.sb(f"dchB{z}", [128, 4], F32, es=qs) for z in range(2)]
                        ytmp = [P.sb(f"ytmp{z}", [64, 4, 64], F32, es=qs) for z in range(2)]
                        pCB = P.ps("pCB", [64, 64], F32, es=qs)
                        pac = P.ps("pac", [64, 4], F32, es=qs)
                        pbc = P.ps("pbc", [64, 4, 64], F32, es=qs)
                        py = P.ps("py", [64, 4, 64], F32, es=qs)
                        pyi = P.ps("pyi", [64, 4, 64], F32, es=qs)
                        phs = P.ps("phs", [128, 256], F32, es=qs)
                        pdc = P.ps("pdc", [128, 4], F32, es=qs)
                        for z in range(2):
                            k.memset("pool", V(hs[z]), 0.0)
                            k.memset("pool", V(hsb[z]), 0.0)
                        orders = [chunk_order(0), chunk_order(1)]
                        for step in range(NCH):
                            for z in range(2):
                                c = orders[z][step]
                                csl = slice(c * CH, (c + 1) * CH)
                                end = CH - 1 if z == 0 else 0
                                hsl = slice(z * 16 + q4 * 4, z * 16 + q4 * 4 + 4)
                                b4 = lambda t: t.a().rearrange("p (h o) -> p h o", o=1).to_broadcast([64, 4, 64])
                                k.mm(V(pCB), (Bf, Bf[:, csl]), (Cf, Cf[:, csl]))
                                k.tt("dve", V(CBm[z]), V(pCB), V(masks[z]), ALU.mult)
                                k.mm(V(pac), V(masks[z]), (laT, laT[:, c, hsl]))
                                k.cp("act", V(acs[z]), V(pac))
                                k.tt("pool", V(dg[z]), (ident, ident[0:64, 0:64].rearrange("p (o t) -> p o t", o=1).to_broadcast([64, 4, 64])),
                                     (acs[z], b4(acs[z])), ALU.mult)
                                k.mm(V(pbc), V(ones64), (dg[z], dg[z].a().rearrange("p h t -> p (h t)")))
                                k.tt("dve", V(seg[z]), V(pbc), (acs[z], b4(acs[z])), ALU.subtract)
                                k.tt("dve", V(dend[z]), (pbc, pbc[:, :, end]), V(acs[z]), ALU.subtract)
                                k.act(V(seg[z]), V(seg[z]), AF.Exp)
                                k.act(V(dend[z]), V(dend[z]), AF.Exp)
                                k.act(V(din[z]), V(acs[z]), AF.Exp)
                                k.stt("dve", V(AT[z]), V(seg[z]), 1.0, (CBm[z], CBm[z].a().rearrange("p (o t) -> p o t", o=1).to_broadcast([64, 4, 64])),
                                      ALU.min, ALU.mult)
                                k.tt("pool", V(xdt[z]), (xq, xq[:, c, :].rearrange("p (h d) -> p h d", d=64)),
                                     (dtT, dtT[:, c, hsl].rearrange("p (h o) -> p h o", o=1).to_broadcast([64, 4, 64])), ALU.mult)
                                for i in range(4):
                                    k.mm((py, py[:, i, :]), (AT[z], AT[z][:, i, :]), (xdt[z], xdt[z][:, i, :]))
                                k.mm(V(pyi), (Cf, Cf[:, csl]), V(hsb[z]))
                                ya = (yacc, yacc[:, c, :].rearrange("p (h d) -> p h d", d=64), c)
                                k.tt("dve", ya, V(py), ya, ALU.add)
                                k.tt("dve", V(ytmp[z]), V(pyi), (din[z], b4(din[z])), ALU.mult)
                                k.tt("pool", ya, ya, V(ytmp[z]), ALU.add)
                                k.tt("pool", V(xde[z]), V(xdt[z]), (dend[z], b4(dend[z])), ALU.mult)
                                k.mm(V(phs), (BTt, BTt[:, c, :]), (xde[z], xde[z].a().rearrange("p h d -> p (h d)")))
                                k.mm(V(pdc), V(selend[z]), V(acs[z]))
                                k.act(V(dchB[z]), V(pdc), AF.Exp)
                                h3 = (hs[z], hs[z].a().rearrange("p (h d) -> p h d", d=64))
                                k.tt("pool", h3, h3, (dchB[z], dchB[z].a().rearrange("p (h o) -> p h o", o=1).to_broadcast([128, 4, 64])), ALU.mult)
                                k.tt("dve", V(hs[z]), V(phs), V(hs[z]), ALU.add)
                                k.cp("act", V(hsb[z]), V(hs[z]))
                    P.barrier()
                    with ExitStack() as fs:
                        xf = P.sb("sxf", [64, 17, 256], F32, es=fs)
                        zf = P.sb("szf", [64, 17, 256], F32, es=fs)
                        for c17 in range(4):
                            cs_ = slice(c17 * 17, (c17 + 1) * 17)
                            rows = slice(c17 * 17 * CH, (c17 + 1) * 17 * CH)
                            k.dma(V(xf), (XBT, XBT[rows, q4 * 256:(q4 + 1) * 256].rearrange("(c p) d -> p c d", p=CH)))
                            k.dma(V(zf), (PT, PT[rows, q4 * 256:(q4 + 1) * 256].rearrange("(c p) d -> p c d", p=CH)))
                            dsk4 = dskB[:, q4 * 4:(q4 + 1) * 4].rearrange("p (a h o) -> p a h o", a=1, o=1).to_broadcast([64, 17, 4, 64])
                            k.tt("pool", (xf, xf.a().rearrange("p c (h d) -> p c h d", d=64)), (xf, xf.a().rearrange("p c (h d) -> p c h d", d=64)),
                                 (dskB, dsk4), ALU.mult)
                            k.tt("dve", V(xf), V(xf), (yacc, yacc[:, cs_, :], ("f", c17)), ALU.add)
                            k.act(V(zf), V(zf), AF.Silu)
                            k.tt("dve", V(xf), V(xf), V(zf), ALU.mult)
                            k.dma((YS, YS[rows, q4 * 256:(q4 + 1) * 256].rearrange("(c p) d -> p c d", p=CH), (q4, c17)), V(xf))
                    P.barrier()

        def stage_ssd_norm(YS, YM):
            with ExitStack() as ph:
                nwB = P.sb("snwB", [128, 1024], F32, es=ph)
                k.dma(V(nwB), V(I["ssd_norm_w"], I["ssd_norm_w"][0].partition_broadcast(128)))
                yt = [P.sb(f"syt{i}", [128, 1024], F32, es=ph) for i in range(2)]
                sq = P.sb("ssq", [128, 1024], F32, es=ph)
                st = [P.sb(f"sst{i}", [128, 2], F32, es=ph) for i in range(2)]
                for tt in range(NT):
                    y_ = yt[tt % 2]; s_ = st[tt % 2]
                    k.dma(V(y_), (YS, YS[tt * 128:(tt + 1) * 128, :]))
                    k.tt("pool", V(sq), V(y_), V(y_), ALU.mult)
                    P.op("dve", lambda e, s_=s_: e.tensor_reduce(out=s_.a(), in_=sq.a().rearrange("p (g d) -> p g d", g=2), axis=AX.X, op=ALU.add),
                         reads=[sq], writes=[s_])
                    k.ts("dve", V(s_), V(s_), 1.0 / 512, ALU.mult, EPS, ALU.add)
                    k.act(V(s_), V(s_), AF.Sqrt)
                    k.recip(V(s_), V(s_))
                    k.tt("dve", (y_, y_.a().rearrange("p (g d) -> p g d", g=2)), (y_, y_.a().rearrange("p (g d) -> p g d", g=2)),
                         (s_, s_.a().rearrange("p (g o) -> p g o", o=1).to_broadcast([128, 2, 512])), ALU.mult)
                    k.tt("pool", V(y_), V(y_), V(nwB), ALU.mult)
                    k.dma((YM, YM[tt * 128:(tt + 1) * 128, 0:1024], ("s", tt)), V(y_))
                P.barrier()

        def stage_final():
            with ExitStack() as ph:
                fnB = P.sb("fnB", [128, D], F32, es=ph)
                k.dma(V(fnB), V(I["final_norm_w"], I["final_norm_w"].a().partition_broadcast(128)))
                xt = [P.sb(f"fxt{i}", [128, D], F32, es=ph) for i in range(2)]
                junk = P.sb("fjunk", [128, D], F32, es=ph)
                st = [P.sb(f"fst{i}", [128, 4], F32, es=ph) for i in range(2)]
                for i in range(SEQ // 128):
                    x_ = xt[i % 2]; s_ = st[i % 2]
                    k.dma(V(x_), (XL, XL[LC + i * 128:LC + (i + 1) * 128, :], i))
                    k.memset("pool", (s_, s_[:, 0:1]), 0.0)
                    k.act(V(junk), V(x_), AF.Square, accum=(s_, s_[:, 0:1]))
                    k.ts("dve", (s_, s_[:, 1:2]), (s_, s_[:, 0:1]), 1.0 / D, ALU.mult, EPS, ALU.add)
                    k.act((s_, s_[:, 2:3]), (s_, s_[:, 1:2]), AF.Sqrt)
                    k.recip((s_, s_[:, 3:4]), (s_, s_[:, 2:3]))
                    k.ts("dve", V(x_), V(x_), (s_, s_[:, 3:4]), ALU.mult)
                    k.tt("pool", V(x_), V(x_), V(fnB), ALU.mult)
                    k.dma((OUT, OUT[i * 128:(i + 1) * 128, :], i), V(x_))
                P.barrier()

        def src0(i):
            if i < 2:
                return (I["ctx"], I["ctx"][i * 128:(i + 1) * 128, :])
            return (I["x"], I["x"][(i - 2) * 128:(i - 1) * 128, :])

        def src1(i):
            return (XL, XL[i * 128:(i + 1) * 128, :])

        def layer0():
            stage_mods(0)
            if "modF" in debug:
                dm = P.dram("d_modF", [128, 6 * KO * 2], F32, kind="ExternalOutput")
                k.dma(V(dm), (modF, modF.a().rearrange("p a b c -> p (a b c)")))
                dm2 = P.dram("d_modB", [128, 4 * D], F32, kind="ExternalOutput")
                k.dma(V(dm2), (modB, modB.a().rearrange("p a b c -> p (a b c)")))
            PF0 = scratch("PF0", [3456, L])
            PT0 = scratch("PT0", [L, 1024])
            YM0 = scratch("YM0", [L, 1024])
            with ExitStack() as lay:
                HT = P.sb("HT", [128, KO, L], BF16, es=lay)
                stage_norm(0, 1, HT, src0, perm=False)
                if "HT0" in debug:
                    dh = P.dram("d_HT0", [128, KO * L], BF16, kind="ExternalOutput")
                    k.dma(V(dh), (HT, HT.a().rearrange("p a b -> p (a b)")))
                if stop_after == "norm0":
                    return
                stage_proj(HT, (I["even_w_in"], I["even_w_in"][0]), 4480, [(0, 3456, PF0, 0)], [(3456, 4480, PT0, 0)])
            if stop_after == "proj0":
                return
            if "skip_hgrn" not in debug:
                mixer_hgrn(PF0, PT0, YM0)
            PR0 = scratch("PR0", [1920, L])
            GT0 = scratch("GT0", [L, 512])
            rwkv_shift(PF0, PR0)
            if "no_gate" not in debug:
                rwkv_gate(PR0, GT0)
            if stop_after == "shift0":
                return
            if "skip_rwkv" not in debug:
                mixer_rwkv(PR0, GT0, YM0)
            if stop_after == "mix0":
                return
            stage_outproj(YM0, 1024, (I["even_w_out"], I["even_w_out"][0]), src0, False, list(range(NT)))
            if stop_after == "out0":
                return
            stage_moe(0, list(range(NT)))

        def layer1():
            stage_mods(1)
            PF1 = scratch("PF1", [2576, L])
            PT1 = scratch("PT1", [L, 3104])
            PC1 = scratch("PC1", [2560, L])
            YM1 = scratch("YM1", [L, 2048])
            GSD = scratch("GSD", [16, L])
            YS = scratch("YS", [L, 1024])
            with ExitStack() as lay:
                HT = P.sb("HT", [128, KO, L], BF16, es=lay)
                stage_norm(1, 1, HT, src1, perm=True)
                stage_proj(HT, (I["odd_w_in"], I["odd_w_in"][0]), 5680,
                           [(1024, 2560, PF1, 0), (2592, 3616, PF1, 1536), (5664, 5680, PF1, 2560)],
                           [(0, 1024, PT1, 0), (2560, 2592, PT1, 1024), (3616, 5664, PT1, 1056)])
            stage_conv(PF1, PC1, [(0, 12, (I["ssd_conv_w"], I["ssd_conv_w"][0]), (I["ssd_conv_b"], I["ssd_conv_b"][0])),
                                  (1536, 8, (I["mlstm_conv_w"], I["mlstm_conv_w"][0]), (I["mlstm_conv_b"], I["mlstm_conv_b"][0]))])
            if stop_after == "L1conv":
                return
            if "skip_mlstm" not in debug:
                mixer_mlstm(PC1, PF1, PT1, YM1, GSD)
            if stop_after == "L1mlstm":
                return
            XBT = scratch("XBT", [L, 1280])
            stage_xbt(PC1, XBT)
            mixer_ssd(PC1, PT1, XBT, YS)
            stage_ssd_norm(YS, YM1)
            if stop_after == "L1ssd":
                return
            stage_outproj(YM1, 2048, (I["odd_w_out"], I["odd_w_out"][0]), src1, True, list(range(2, NT)))
            if stop_after == "L1out":
                return
            stage_moe(1, list(range(2, NT)))
            stage_final()

        if "L1only" in debug:
            XLin = P.dram("XLin", [L, D], F32, kind="ExternalInput")
            for i4 in range(4):
                k.dma((XL, XL[i4 * 1088:(i4 + 1) * 1088, :], ("in", i4)), (XLin, XLin[i4 * 1088:(i4 + 1) * 1088, :]))
            P.barrier()
        else:
            layer0()
        if stop_after is None or stop_after.startswith("L1"):
            layer1()
        P.finish()
    return nc


_NC = None


def kernel(**inputs):
    global _NC
    if _NC is None:
        _NC = build()
    nc = _NC
    n = 8
    in_maps = []
    for b in range(n):
        m = {}
        for kk_, v in inputs.items():
            v = np.asarray(v)
            if kk_ == "x":
                m[kk_] = np.ascontiguousarray(v[b])
            elif kk_ == "c":
                m[kk_] = np.ascontiguousarray(v[b])
            elif kk_ == "ctx":
                m[kk_] = np.ascontiguousarray(v[b])
            else:
                m[kk_] = v
        in_maps.append(m)
    res = run_bass_kernel_spmd(nc, in_maps, core_ids=list(range(n)))
    return np.stack([r["out"] for r in res.results], axis=0)
```

```python
import numpy as np
import concourse.bass as bass
import concourse.mybir as mybir
from concourse.bass_utils import run_bass_kernel_spmd
from contextlib import ExitStack

F32 = mybir.dt.float32
BF16 = mybir.dt.bfloat16
AF = mybir.ActivationFunctionType
ALU = mybir.AluOpType
AX = mybir.AxisListType

ENGS = ("pe", "dve", "act", "pool", "sp")
D = 1024
KO = 8
LC = 256
SEQ = 4096
L = LC + SEQ
NT = L // 128
EPS = 1e-6
CH = 64
NCH = L // CH


class Cell:
    __slots__ = ("w", "r")

    def __init__(self):
        self.w = None
        self.r = []


class Buf:
    def __init__(self, name, h, is_dram=False, is_psum=False):
        self.name = name
        self.h = h
        self.is_dram = is_dram
        self.is_psum = is_psum
        self.base = Cell()
        self.parts = {}

    def cells(self, key):
        if key is None:
            return [self.base] + list(self.parts.values())
        c = self.parts.get(key)
        if c is None:
            c = Cell()
            c.w = self.base.w
            c.r = list(self.base.r)
            self.parts[key] = c
        return [c]

    def __getitem__(self, idx):
        return self.h[idx]

    def a(self):
        return self.h[:]


class Prog:
    def __init__(self, nc, es):
        self.nc = nc
        self.es = es
        self.q = {e: [] for e in ENGS}
        self.cnt = {e: 0 for e in ENGS}
        self.sem = {e: es.enter_context(nc.semaphore("s_" + e)) for e in ENGS}
        self.known = {e: {} for e in ENGS}
        self.dsem = {}
        self.dsem_by_id = {}
        self.phase_slots = {}
        self.ninst = 0
        self.uid = 0

    def sb(self, name, shape, dt=F32, es=None):
        self.uid += 1
        h = (es or self.es).enter_context(self.nc.sbuf_tensor(f"{name}_{self.uid}", list(shape), dt))
        return Buf(name, h)

    def ps(self, name, shape, dt=F32, es=None):
        self.uid += 1
        h = (es or self.es).enter_context(self.nc.psum_tensor(f"{name}_{self.uid}", list(shape), dt))
        return Buf(name, h, is_psum=True)

    def dram(self, name, shape, dt=F32, kind="Internal"):
        h = self.nc.dram_tensor(name, list(shape), dt, kind=kind)
        return Buf(name, h.ap(), is_dram=True)

    def dma_sem(self, name):
        if name not in self.dsem:
            self.dsem[name] = [self.es.enter_context(self.nc.semaphore("d_" + name)), 0]
            self.dsem_by_id[id(self.dsem[name][0])] = self.dsem[name]
        return self.dsem[name]

    def _norm(self, lst):
        return [(r, None) if isinstance(r, Buf) else ((r[0], None) if r[0].is_psum else r) for r in lst]

    def _deps(self, eng, reads, writes, pe_acc=False):
        need = {}

        def add(tok):
            if tok is None:
                return
            k = id(tok[0])
            if k not in need or need[k][1] < tok[1]:
                need[k] = tok

        for (b, key) in reads:
            for c in b.cells(key):
                add(c.w)
        for (b, key) in writes:
            for c in b.cells(key):
                if not (pe_acc and c.w is not None and c.w[2] == "pe"):
                    add(c.w)
                for t in c.r:
                    add(t)
        out = []
        kn = self.known[eng]
        for k, tok in need.items():
            val = tok[1]
            if k in self.dsem_by_id:
                val = self.dsem_by_id[k][1]
            if kn.get(k, 0) >= val:
                continue
            kn[k] = val
            out.append((tok[0], val))
        return out

    def _record(self, tok, reads, writes):
        for (b, key) in reads:
            for c in b.cells(key):
                c.r.append(tok)
                if len(c.r) > 16:
                    best = {}
                    for t in c.r:
                        kk = id(t[0])
                        if kk not in best or best[kk][1] < t[1]:
                            best[kk] = t
                    c.r = list(best.values())
        for (b, key) in writes:
            for c in b.cells(key):
                c.w = tok
                c.r = []

    def op(self, eng, fn, reads=(), writes=(), pe_acc=False):
        reads = self._norm(reads)
        writes = self._norm(writes)
        writes = writes + [r for r in reads if r[0].is_psum]
        waits = self._deps(eng, reads, writes, pe_acc)
        self.cnt[eng] += 1
        sem = self.sem[eng]
        tok = (sem, self.cnt[eng], eng)
        self.ninst += 1

        def run(e, waits=waits, fn=fn, sem=sem):
            for s, v in waits:
                e.wait_ge(s, v)
            fn(e).then_inc(sem, 1)

        self.q[eng].append(run)
        self._record(tok, reads, writes)
        return tok

    def dma(self, eng, out_ap, in_ap, reads=(), writes=(), semname=None, **kw):
        reads = self._norm(reads)
        writes = self._norm(writes)
        waits = self._deps(eng, reads, writes)
        if semname is None:
            sbs = [b for (b, _) in list(writes) + list(reads) if not b.is_dram]
            semname = sbs[0].name if sbs else "dram2dram"
        if semname not in self.phase_slots:
            self.phase_slots[semname] = len(self.phase_slots)
        ds = self.dma_sem(f"slot{self.phase_slots[semname]}")
        ds[1] += 16
        tok = (ds[0], ds[1], "dma")
        self.ninst += 1

        def run(e, waits=waits, sem=ds[0]):
            for s, v in waits:
                e.wait_ge(s, v)
            e.dma_start(out=out_ap, in_=in_ap, **kw).then_inc(sem, 16)

        self.q[eng].append(run)
        self._record(tok, reads, writes)
        return tok

    def barrier(self):
        self.phase_slots = {}
        toks = [(self.sem[f], self.cnt[f]) for f in ENGS if self.cnt[f] > 0]
        toks += [(v[0], v[1]) for v in self.dsem.values() if v[1] > 0]
        for e in ENGS:
            kn = self.known[e]
            ws = []
            for s, v in toks:
                if kn.get(id(s), 0) < v:
                    kn[id(s)] = v
                    ws.append((s, v))

            def run(en, ws=ws):
                for s, v in ws:
                    en.wait_ge(s, v)
            self.q[e].append(run)

    def finish(self):
        nc = self.nc
        self.barrier()
        with nc.Block() as block:
            @block.tensor
            def _(e):
                for f in self.q["pe"]:
                    f(e)

            @block.vector
            def _(e):
                for f in self.q["dve"]:
                    f(e)

            @block.scalar
            def _(e):
                for f in self.q["act"]:
                    f(e)

            @block.gpsimd
            def _(e):
                for f in self.q["pool"]:
                    f(e)

            @block.sync
            def _(e):
                for f in self.q["sp"]:
                    f(e)


def _rk(x):
    return (x[0], x[2] if len(x) > 2 else None)


class K:
    def __init__(self, P):
        self.P = P
        self.dq = 0

    def mm(self, out, lhsT, rhs, start=True, stop=True):
        return self.P.op("pe", lambda e: e.matmul(out[1], lhsT=lhsT[1], rhs=rhs[1], start=start, stop=stop),
                         reads=[_rk(lhsT), _rk(rhs)], writes=[_rk(out)], pe_acc=not start)

    def tr(self, out, in_, ident):
        return self.P.op("pe", lambda e: e.transpose(out[1], in_[1], ident[1]),
                         reads=[_rk(in_), _rk(ident)], writes=[_rk(out)])

    def act(self, out, in_, func, bias=None, scale=None, accum=None, eng="act"):
        reads = [_rk(in_)]
        kw = {}
        if bias is not None:
            if isinstance(bias, tuple):
                reads.append(_rk(bias)); kw["bias"] = bias[1]
            else:
                kw["bias"] = bias
        if scale is not None:
            if isinstance(scale, tuple):
                reads.append(_rk(scale)); kw["scale"] = scale[1]
            else:
                kw["scale"] = scale
        writes = [_rk(out)]
        if accum is not None:
            writes.append(_rk(accum)); kw["accum_out"] = accum[1]
        return self.P.op("act", lambda e: e.activation(out=out[1], in_=in_[1], func=func, **kw), reads=reads, writes=writes)

    def tt(self, eng, out, in0, in1, op):
        return self.P.op(eng, lambda e: e.tensor_tensor(out=out[1], in0=in0[1], in1=in1[1], op=op),
                         reads=[_rk(in0), _rk(in1)], writes=[_rk(out)])

    def ts(self, eng, out, in0, s1, op0, s2=None, op1=None, accum=None):
        reads = [_rk(in0)]
        a1 = s1
        if isinstance(s1, tuple):
            reads.append(_rk(s1)); a1 = s1[1]
        a2 = s2
        if isinstance(s2, tuple):
            reads.append(_rk(s2)); a2 = s2[1]
        kw = {}
        if op1 is not None:
            kw["op1"] = op1
        writes = [_rk(out)]
        if accum is not None:
            writes.append(_rk(accum)); kw["accum_out"] = accum[1]
        return self.P.op(eng, lambda e: e.tensor_scalar(out=out[1], in0=in0[1], scalar1=a1, scalar2=a2, op0=op0, **kw),
                         reads=reads, writes=writes)

    def stt(self, eng, out, in0, scalar, in1, op0, op1):
        reads = [_rk(in0), _rk(in1)]
        sc = scalar
        if isinstance(scalar, tuple):
            reads.append(_rk(scalar)); sc = scalar[1]
        return self.P.op(eng, lambda e: e.scalar_tensor_tensor(out=out[1], in0=in0[1], scalar=sc, in1=in1[1], op0=op0, op1=op1),
                         reads=reads, writes=[_rk(out)])

    def cp(self, eng, out, in_):
        if eng == "act":
            return self.P.op("act", lambda e: e.copy(out=out[1], in_=in_[1]), reads=[_rk(in_)], writes=[_rk(out)])
        return self.P.op(eng, lambda e: e.tensor_copy(out=out[1], in_=in_[1]), reads=[_rk(in_)], writes=[_rk(out)])

    def memset(self, eng, out, val):
        return self.P.op(eng, lambda e: e.memset(out[1], val), writes=[_rk(out)])

    def scan(self, out, d0, d1, init, op0, op1):
        return self.P.op("dve", lambda e: e.tensor_tensor_scan(out=out[1], data0=d0[1], data1=d1[1], initial=init, op0=op0, op1=op1),
                         reads=[_rk(d0), _rk(d1)], writes=[_rk(out)])

    def recip(self, out, in_):
        return self.P.op("dve", lambda e: e.reciprocal(out=out[1], in_=in_[1]), reads=[_rk(in_)], writes=[_rk(out)])

    def dma(self, out, in_, eng=None, **kw):
        if eng is None:
            eng = "sp" if in_[0].is_dram else "act"
        reads = [_rk(in_)]
        writes = [_rk(out)]
        return self.P.dma(eng, out[1], in_[1], reads=reads, writes=writes, **kw)


def V(buf, ap=None, key=None):
    return (buf, buf.a() if ap is None else ap, key)


def build(debug=(), stop_after=None):
    nc = bass.Bass("TRN2", target_bir_lowering=False)
    es = ExitStack()
    with es:
        P = Prog(nc, es)
        k = K(P)
        I = {}

        def inp(name, shape):
            I[name] = P.dram(name, shape, F32, kind="ExternalInput")

        inp("x", [SEQ, D]); inp("c", [D]); inp("ctx", [LC, D]); inp("c_ctx", [D])
        inp("ada_w", [2, D, 6 * D]); inp("ada_b", [2, 6 * D]); inp("norm1_w", [2, D]); inp("norm2_w", [2, D])
        inp("even_w_in", [1, D, 4480]); inp("even_w_out", [1, 1024, D])
        inp("rwkv_mu", [1, 1920]); inp("rwkv_w0", [1, 2, 512]); inp("rwkv_w2", [1, 2, 64, 512])
        inp("rwkv_a0", [1, 2, 512]); inp("rwkv_a2", [1, 2, 64, 512]); inp("rwkv_g2", [1, 128, 512])
        for n in ("rwkv_k_k", "rwkv_k_a", "rwkv_r_k", "rwkv_ln_w", "rwkv_ln_b"):
            inp(n, [1, 512])
        inp("hgrn_lower_bounds", [3, 512]); inp("hgrn_norm_w", [1, 512])
        inp("odd_w_in", [1, D, 5680]); inp("odd_w_out", [1, 2048, D])
        inp("ssd_conv_w", [1, 5, 1536]); inp("ssd_conv_b", [1, 1536]); inp("ssd_dt_bias", [1, 2, 16])
        inp("ssd_a_log", [1, 2, 16]); inp("ssd_d", [1, 16]); inp("ssd_norm_w", [1, 1024])
        inp("mlstm_conv_w", [1, 5, 1024]); inp("mlstm_conv_b", [1, 1024]); inp("mlstm_i_bias", [1, 2, 4])
        inp("mlstm_f_bias", [1, 2, 4]); inp("mlstm_norm_w", [1, 1024])
        inp("router_w", [2, D, 32]); inp("router_b", [2, 32])
        inp("exp_w_gate", [2, 32, D, 1024]); inp("exp_b_gate", [2, 32, 1024])
        inp("exp_w_up", [2, 32, D, 1024]); inp("exp_b_up", [2, 32, 1024])
        inp("exp_w_down", [2, 32, 1024, D]); inp("exp_b_down", [2, 32, D])
        inp("final_norm_w", [D])
        OUT = P.dram("out", [SEQ, D], F32, kind="ExternalOutput")

        def scratch(name, shape, dt=F32):
            return P.dram(name, shape, dt, kind="ExternalOutput" if name in debug else "Internal")

        XL = scratch("XL", [L, D])

        ident = P.sb("ident", [128, 128], F32)
        identb = P.sb("identb", [128, 128], BF16)
        ones = P.sb("ones", [128, 128], F32)
        k.memset("pool", V(ones), 1.0)
        P.op("pool", lambda e: e.affine_select(out=ident.a(), in_=ones.a(), pattern=[[-1, 128]], compare_op=ALU.is_equal,
                                               fill=0.0, base=0, channel_multiplier=1), reads=[ones], writes=[ident])
        k.cp("dve", V(identb), V(ident))

        modF = P.sb("modF", [128, 6, KO, 2], F32)
        modB = P.sb("modB", [128, 2, 2, D], F32)
        g1F = P.sb("g1F", [128, KO, 2], F32)
        g2F = P.sb("g2F", [128, KO, 2], F32)
        nw1 = P.sb("nw1", [128, 2, KO], F32)
        nw2 = P.sb("nw2", [128, 2, KO], F32)
        k.dma(V(nw1), V(I["norm1_w"], I["norm1_w"].a().rearrange("l (ko p) -> p l ko", p=128)), allow_slow_non_contiguous=True)
        k.dma(V(nw2), V(I["norm2_w"], I["norm2_w"].a().rearrange("l (ko p) -> p l ko", p=128)), allow_slow_non_contiguous=True)

        def stage_mods(l):
            with ExitStack() as ph:
                c0 = P.sb("c0", [128, KO, 2], F32, es=ph)
                s = P.sb("s", [128, KO, 2], F32, es=ph)
                sB = P.sb("sB", [128, KO, 2, 128], F32, es=ph)
                abF = P.sb("abF", [128, 48], F32, es=ph)
                abB = P.sb("abB", [128, 2, D], F32, es=ph)
                awm = [P.sb(f"awm{i}", [128, KO, D], F32, es=ph) for i in range(2)]
                psF = P.ps("psF", [128, KO, 2], F32, es=ph)
                psB = [P.ps(f"psB{i}", [128, 512], F32, es=ph) for i in range(2)]
                tmp = P.sb("tmpm", [128, KO, 2], F32, es=ph)
                k.dma((c0, c0[:, :, 0]), V(I["c"], I["c"].a().rearrange("(ko p) -> p ko", p=128)), allow_slow_non_contiguous=True)
                k.dma((c0, c0[:, :, 1]), V(I["c_ctx"], I["c_ctx"].a().rearrange("(ko p) -> p ko", p=128)), allow_slow_non_contiguous=True)
                k.act(V(s), V(c0), AF.Silu)
                k.cp("dve", V(sB), (s, s.a().rearrange("p k (j o) -> p k j o", o=1).to_broadcast([128, KO, 2, 128])))
                k.dma(V(abF), V(I["ada_b"], I["ada_b"][l].rearrange("(nb p) -> p nb", p=128)), allow_slow_non_contiguous=True)
                k.dma((abB, abB[:, 0, :]), V(I["ada_b"], I["ada_b"][l, 2 * D:3 * D].partition_broadcast(128)))
                k.dma((abB, abB[:, 1, :]), V(I["ada_b"], I["ada_b"][l, 5 * D:6 * D].partition_broadcast(128)))
                for m in range(6):
                    aw = awm[m % 2]
                    k.dma(V(aw), V(I["ada_w"], I["ada_w"][l, :, m * D:(m + 1) * D].rearrange("(ko p) n -> p ko n", p=128)))
                    if m in (0, 1, 3, 4):
                        for nb in range(KO):
                            for ko in range(KO):
                                k.mm((psF, psF[:, nb, :]), (aw, aw[:, ko, nb * 128:(nb + 1) * 128]), (s, s[:, ko, :]),
                                     start=(ko == 0), stop=(ko == KO - 1))
                        k.tt("dve", (modF, modF[:, m, :, :]), V(psF),
                             (abF, abF[:, m * 8:(m + 1) * 8].rearrange("p (k o) -> p k o", o=1).to_broadcast([128, KO, 2])), ALU.add)
                    else:
                        mi = 0 if m == 2 else 1
                        for j in range(2):
                            for nblk in range(2):
                                pb = psB[(j * 2 + nblk) % 2]
                                for ko in range(KO):
                                    k.mm(V(pb), (sB, sB[:, ko, j, :]), (aw, aw[:, ko, nblk * 512:(nblk + 1) * 512]),
                                         start=(ko == 0), stop=(ko == KO - 1))
                                k.tt("dve", (modB, modB[:, mi, j, nblk * 512:(nblk + 1) * 512]), V(pb),
                                     (abB, abB[:, mi, nblk * 512:(nblk + 1) * 512]), ALU.add)
                for (gF, nw, mi) in ((g1F, nw1, 1), (g2F, nw2, 4)):
                    k.ts("dve", V(tmp), (modF, modF[:, mi, :, :]), 1.0, ALU.add)
                    k.tt("dve", V(gF), V(tmp), (nw, nw[:, l, :].rearrange("p (k o) -> p k o", o=1).to_broadcast([128, KO, 2])), ALU.mult)
                P.barrier()

        def stage_norm(l, which, HT, src_tiles, perm):
            gF = g1F if which == 1 else g2F
            mi = 0 if which == 1 else 3
            with ExitStack() as ph:
                xt = [P.sb(f"xt{i}", [128, D], F32, es=ph) for i in range(3)]
                xn = [P.sb(f"xn{i}", [128, D], F32, es=ph) for i in range(2)]
                junk = P.sb("junk", [128, D], F32, es=ph)
                st = [P.sb(f"st{i}", [128, 4], F32, es=ph) for i in range(2)]
                tmp = [P.sb(f"tmpn{i}", [128, KO, 128], F32, es=ph) for i in range(2)]
                pT = [P.ps(f"pT{i}", [128, KO, 128], F32, es=ph) for i in range(2)]
                for i in range(NT):
                    j = 1 if i < 2 else 0
                    x_ = xt[i % 3]; n_ = xn[i % 2]; s_ = st[i % 2]; t_ = tmp[i % 2]; p_ = pT[i % 2]
                    sb_, sap = src_tiles(i)
                    k.dma(V(x_), (sb_, sap))
                    k.memset("pool", (s_, s_[:, 0:1]), 0.0)
                    k.act(V(junk), V(x_), AF.Square, accum=(s_, s_[:, 0:1]))
                    k.ts("dve", (s_, s_[:, 1:2]), (s_, s_[:, 0:1]), 1.0 / D, ALU.mult, EPS, ALU.add)
                    k.act((s_, s_[:, 2:3]), (s_, s_[:, 1:2]), AF.Sqrt)
                    k.recip((s_, s_[:, 3:4]), (s_, s_[:, 2:3]))
                    k.ts("dve", V(n_), V(x_), (s_, s_[:, 3:4]), ALU.mult)
                    for ko in range(KO):
                        k.tr((p_, p_[:, ko, :]), (n_, n_[:, ko * 128:(ko + 1) * 128]), V(ident))
                    k.tt("dve", V(t_), V(p_), (gF, gF[:, :, j:j + 1].to_broadcast([128, KO, 128])), ALU.mult)
                    if perm and i >= 2:
                        r0 = 2 * (i - 2)
                        dst = HT[:, :, LC:].rearrange("p k (c r) -> p k c r", r=64)[:, :, :, r0:r0 + 2]
                        src0 = t_.a().rearrange("p k (r c) -> p k c r", r=2)
                        src1 = modF[:, mi, :, j:j + 1].rearrange("p k (a b) -> p k a b", b=1).to_broadcast([128, KO, 64, 2])
                        k.tt("pool", (HT, dst, i), (t_, src0), (modF, src1), ALU.add)
                    else:
                        k.tt("pool", (HT, HT[:, :, i * 128:(i + 1) * 128], i), V(t_),
                             (modF, modF[:, mi, :, j:j + 1].to_broadcast([128, KO, 128])), ALU.add)
                P.barrier()

        def stage_proj(HT, W, ncols, f_list, t_list):
            with ExitStack() as ph:
                wst = [P.sb(f"wst{i}", [128, KO, 512], BF16, es=ph) for i in range(2)]
                stg = [P.sb(f"stg{i}", [128, 512], F32, es=ph) for i in range(4)]
                pp = [P.ps(f"pp{i}", [128, 512], F32, es=ph) for i in range(4)]
                cnt = 0
                npan = (ncols + 511) // 512
                for pn in range(npan):
                    c0 = pn * 512
                    cw = min(512, ncols - c0)
                    w_ = wst[pn % 2]
                    k.dma((w_, w_[:, :, 0:cw]), (W[0], W[1][:, c0:c0 + cw].rearrange("(ko p) n -> p ko n", p=128)), eng="pool")
                    for (f0, f1, PF, roff) in f_list:
                        fa, fb = max(c0, f0), min(c0 + cw, f1)
                        if fa >= fb:
                            continue
                        for n0 in range(fa, fb, 128):
                            nw_ = min(128, fb - n0)
                            for tb in range(0, L, 512):
                                tw = min(512, L - tb)
                                p_ = pp[cnt % 4]; s_ = stg[cnt % 4]; cnt += 1
                                for ko in range(KO):
                                    k.mm((p_, p_[0:nw_, 0:tw]), (w_, w_[:, ko, n0 - c0:n0 - c0 + nw_]), (HT, HT[:, ko, tb:tb + tw]),
                                         start=(ko == 0), stop=(ko == KO - 1))
                                k.cp("act" if cnt % 2 else "dve", (s_, s_[0:nw_, 0:tw]), (p_, p_[0:nw_, 0:tw]))
                                r0 = n0 - f0 + roff
                                k.dma((PF, PF[r0:r0 + nw_, tb:tb + tw], ("f", n0)), (s_, s_[0:nw_, 0:tw]))
                    for (t0, t1, PT, coff) in t_list:
                        ta, tb_ = max(c0, t0), min(c0 + cw, t1)
                        if ta >= tb_:
                            continue
                        tw = tb_ - ta
                        for tt in range(NT):
                            p_ = pp[cnt % 4]; s_ = stg[cnt % 4]; cnt += 1
                            for ko in range(KO):
                                k.mm((p_, p_[:, 0:tw]), (HT, HT[:, ko, tt * 128:(tt + 1) * 128]), (w_, w_[:, ko, ta - c0:ta - c0 + tw]),
                                     start=(ko == 0), stop=(ko == KO - 1))
                            k.cp("act" if cnt % 2 else "dve", (s_, s_[:, 0:tw]), (p_, p_[:, 0:tw]))
                            cc = ta - t0 + coff
                            k.dma((PT, PT[tt * 128:(tt + 1) * 128, cc:cc + tw], ("t", tt, ta)), (s_, s_[:, 0:tw]))
                P.barrier()

        def chunk_order(z):
            if z == 0:
                return list(range(NCH))
            return [3, 2, 1, 0] + list(range(NCH - 1, 3, -1))

        def make_masks(ph):
            ms = []
            for z in range(2):
                m = P.sb(f"mask{z}", [64, 64], F32, es=ph)
                if z == 0:
                    P.op("pool", lambda e, m=m: e.affine_select(out=m.a(), in_=ones[0:64, 0:64], pattern=[[1, 64]], compare_op=ALU.is_ge,
                                                           fill=0.0, base=0, channel_multiplier=-1), reads=[ones], writes=[m])
                else:
                    P.op("pool", lambda e, m=m: e.affine_select(out=m.a(), in_=ones[0:64, 0:64], pattern=[[-1, 64]], compare_op=ALU.is_ge,
                                                           fill=0.0, base=0, channel_multiplier=1), reads=[ones], writes=[m])
                ms.append(m)
            return ms

        def mixer_hgrn(PF, PT, YM):
            BW = 1088
            NB = L // BW
            CB = BW // CH
            with ExitStack() as ph:
                masks = make_masks(ph)
                lbr = P.sb("lbr", [128, 3, 4], F32, es=ph)
                lbe = P.sb("lbe", [128, 3, 4], F32, es=ph)
                lbs = P.sb("lbs", [128, 4], F32, es=ph)
                lb = P.sb("lb", [128, 4], F32, es=ph)
                oml = P.sb("oml", [128, 4], F32, es=ph)
                noml = P.sb("noml", [128, 4], F32, es=ph)
                k.dma(V(lbr), V(I["hgrn_lower_bounds"], I["hgrn_lower_bounds"].a().rearrange("j (h p) -> p j h", p=128)),
                      allow_slow_non_contiguous=True)
                k.act(V(lbe), V(lbr), AF.Exp)
                k.tt("dve", V(lbs), (lbe, lbe[:, 0, :]), (lbe, lbe[:, 1, :]), ALU.add)
                k.tt("dve", V(lbs), V(lbs), (lbe, lbe[:, 2, :]), ALU.add)
                k.recip(V(lbs), V(lbs))
                k.tt("dve", V(lb), (lbe, lbe[:, 0, :]), V(lbs), ALU.mult)
                k.ts("dve", V(oml), V(lb), -1.0, ALU.mult, 1.0, ALU.add)
                k.ts("dve", V(noml), V(oml), -1.0, ALU.mult)
                rst = P.sb("rst", [128, BW], F32, es=ph)
                k.memset("pool", V(rst), 1.0)
                k.memset("pool", (rst, rst.a().rearrange("p (c t) -> p c t", t=CH)[:, :, 0:1]), 0.0)
                nwB = P.sb("nwB", [64, 512], F32, es=ph)
                k.dma(V(nwB), V(I["hgrn_norm_w"], I["hgrn_norm_w"][0].partition_broadcast(64)))
                oacc = P.sb("oacc", [64, NCH, 128], F32, es=ph)
                ssn = P.sb("ssn", [64, NCH], F32, es=ph)
                for h in range(4):
                    with ExitStack() as hs1:
                        t_f = P.sb("t_f", [128, BW], F32, es=hs1)
                        t_s = P.sb("t_s", [128, BW], F32, es=hs1)
                        t_lf = P.sb("t_lf", [128, BW], F32, es=hs1)
                        t_k = P.sb("t_k", [128, BW], F32, es=hs1)
                        t_b = P.sb("t_b", [128, BW], F32, es=hs1)
                        t_e = P.sb("t_e", [128, BW], F32, es=hs1)
                        t_q = P.sb("t_q", [128, BW], F32, es=hs1)
                        cd = [P.sb(f"cd{z}", [128, NCH], F32, es=hs1) for z in range(2)]
                        q_in = [P.sb(f"q_in{z}", [128, L], BF16, es=hs1) for z in range(2)]
                        k_in = [P.sb(f"k_in{z}", [128, L], BF16, es=hs1) for z in range(2)]
                        k_end1 = P.sb("k_end", [128, L], BF16, es=hs1)
                        k_end = [k_end1, k_end1]
                        kET = [P.sb(f"kET{z}", [64, NCH, 128], BF16, es=hs1) for z in range(2)]
                        vb = P.sb("vb", [64, NCH, 128], BF16, es=hs1)
                        S = [P.sb(f"S{z}", [128, 128], F32, es=hs1) for z in range(2)]
                        Sb = [P.sb(f"Sb{z}", [128, 128], BF16, es=hs1) for z in range(2)]
                        Am = [P.sb(f"Am{i}", [64, 64], BF16, es=hs1) for i in range(4)]
                        psA = [P.ps(f"psA{i}", [64, 64], F32, es=hs1) for i in range(2)]
                        pso = [P.ps(f"pso{i}", [64, 128], F32, es=hs1) for i in range(2)]
                        pskv = [P.ps(f"pskv{i}", [128, 128], F32, es=hs1) for i in range(2)]
                        pst = [P.ps(f"pst{i}", [64, 4, 128], BF16, es=hs1) for i in range(2)]
                        k.dma(V(vb), (PT, PT[:, h * 128:(h + 1) * 128].rearrange("(c p) d -> p c d", p=CH)), eng="pool")
                        for z in range(2):
                            end = CH - 1 if z == 0 else 0
                            for blk in range(NB):
                                tsl = slice(blk * BW, (blk + 1) * BW)
                                frow = 2432 + z * 512 + h * 128
                                k.dma(V(t_f), (PF, PF[frow:frow + 128, tsl]))
                                k.dma(V(t_q), (PF, PF[1920 + h * 128:1920 + (h + 1) * 128, tsl]))
                                k.act(V(t_s), V(t_f), AF.Sigmoid)
                                k.act(V(t_lf), V(t_s), AF.Ln, bias=(lb, lb[:, h:h + 1]), scale=(oml, oml[:, h:h + 1]))
                                k.ts("dve", V(t_k), V(t_s), (noml, noml[:, h:h + 1]), ALU.mult, (oml, oml[:, h:h + 1]), ALU.add)
                                k.scan(V(t_b), V(rst), V(t_lf), 0.0, ALU.mult, ALU.add)
                                if z == 1:
                                    k.tt("dve", V(t_f), V(t_lf), V(t_b), ALU.subtract)
                                    b3 = t_b.a().rearrange("p (c t) -> p c t", t=CH)
                                    k.tt("dve", (t_lf, t_lf.a().rearrange("p (c t) -> p c t", t=CH)),
                                         (t_f, t_f.a().rearrange("p (c t) -> p c t", t=CH)),
                                         (t_b, b3[:, :, CH - 1:CH].to_broadcast([128, CB, CH])), ALU.add)
                                    bb = t_lf
                                else:
                                    bb = t_b
                                k.act(V(t_e), V(bb), AF.Exp)
                                k.act(V(t_s), V(bb), AF.Exp, scale=-1.0)
                                e3 = t_e.a().rearrange("p (c t) -> p c t", t=CH)
                                k.cp("pool", (cd[z], cd[z][:, blk * CB:(blk + 1) * CB]), (t_e, e3[:, :, end]))
                                k.act(V(t_f), V(t_q), AF.Silu)
                                k.stt("dve", (q_in[z], q_in[z][:, tsl]), V(t_f), 128 ** -0.5, V(t_e), ALU.mult, ALU.mult)
                                k.tt("dve", V(t_k), V(t_k), V(t_s), ALU.mult)
                                k.cp("pool", (k_in[z], k_in[z][:, tsl]), V(t_k))
                                k.tt("pool", (k_end[z], k_end[z][:, tsl].rearrange("p (c t) -> p c t", t=CH)),
                                     (t_k, t_k.a().rearrange("p (c t) -> p c t", t=CH)),
                                     (t_e, e3[:, :, end:end + 1].to_broadcast([128, CB, CH])), ALU.mult)
                            for c4 in range(0, NCH, 4):
                                p_ = pst[(c4 // 4) % 2]
                                for j in range(4):
                                    c = c4 + j
                                    k.tr((p_, p_[:, j, :]), (k_end[z], k_end[z][:, c * CH:(c + 1) * CH]), V(identb))
                                k.cp("act", (kET[z], kET[z][:, c4:c4 + 4, :]), V(p_))
                        k.memset("pool", V(oacc), 0.0)
                        for z in range(2):
                            k.memset("pool", V(S[z]), 0.0)
                            k.memset("pool", V(Sb[z]), 0.0)
                        orders = [chunk_order(0), chunk_order(1)]
                        for step in range(NCH):
                            for z in range(2):
                                c = orders[z][step]
                                csl = slice(c * CH, (c + 1) * CH)
                                pa = psA[z]; po = pso[z]; pk = pskv[z]; am = Am[(step % 2) * 2 + z]
                                k.mm(V(pa), (k_in[z], k_in[z][:, csl]), (q_in[z], q_in[z][:, csl]))
                                k.tt("dve", V(am), V(pa), V(masks[z]), ALU.mult)
                                k.mm(V(po), V(am), (vb, vb[:, c, :]), start=True, stop=False)
                                k.mm(V(po), (q_in[z], q_in[z][:, csl]), V(Sb[z]), start=False, stop=True)
                                k.tt("dve", (oacc, oacc[:, c, :], c), (oacc, oacc[:, c, :], c), V(po), ALU.add)
                                k.mm(V(pk), (kET[z], kET[z][:, c, :]), (vb, vb[:, c, :]))
                                k.stt("dve", V(S[z]), V(S[z]), (cd[z], cd[z][:, c:c + 1]), V(pk), ALU.mult, ALU.add)
                                k.cp("act", V(Sb[z]), V(S[z]))
                    P.barrier()
                    with ExitStack() as hs2:
                        gt = P.sb("gt", [64, NCH, 128], F32, es=hs2)
                        k.tt("dve", V(gt), V(oacc), V(oacc), ALU.mult)
                        P.op("dve", lambda e: e.tensor_reduce(out=ssn.a(), in_=gt.a(), axis=AX.X, op=ALU.add), reads=[gt], writes=[ssn])
                        k.ts("dve", V(ssn), V(ssn), 1.0 / 128, ALU.mult, EPS, ALU.add)
                        k.act(V(ssn), V(ssn), AF.Sqrt)
                        k.recip(V(ssn), V(ssn))
                        k.tt("dve", V(oacc), V(oacc), (ssn, ssn.a().rearrange("p (c o) -> p c o", o=1).to_broadcast([64, NCH, 128])), ALU.mult)
                        k.tt("pool", V(oacc), V(oacc), (nwB, nwB[:, h * 128:(h + 1) * 128].rearrange("p (o d) -> p o d", o=1).to_broadcast([64, NCH, 128])), ALU.mult)
                        k.dma(V(gt), (PT, PT[:, 512 + h * 128:512 + (h + 1) * 128].rearrange("(c p) d -> p c d", p=CH)))
                        k.act(V(gt), V(gt), AF.Silu)
                        k.tt("dve", V(oacc), V(oacc), V(gt), ALU.mult)
                        k.dma((YM, YM[:, 512 + h * 128:512 + (h + 1) * 128].rearrange("(c p) d -> p c d", p=CH), ("h", h)), V(oacc))
                    P.barrier()
                P.barrier()

        def rwkv_shift(PF, PR):
            with ExitStack() as ph:
                mu = P.sb("mu", [128, 15], F32, es=ph)
                omu = P.sb("omu", [128, 15], F32, es=ph)
                hmu = P.sb("hmu", [128, 15], F32, es=ph)
                k.dma(V(mu), V(I["rwkv_mu"], I["rwkv_mu"][0].rearrange("(b p) -> p b", p=128)), allow_slow_non_contiguous=True)
                k.ts("dve", V(omu), V(mu), -1.0, ALU.mult, 1.0, ALU.add)
                k.ts("dve", V(hmu), V(mu), 0.5, ALU.mult)
                pt = [P.sb(f"shp{i}", [128, L + 2], F32, es=ph) for i in range(2)]
                sm = [P.sb(f"shs{i}", [128, L], F32, es=ph) for i in range(2)]
                for i in range(2):
                    k.memset("pool", (pt[i], pt[i][:, 0:1], "h0"), 0.0)
                    k.memset("pool", (pt[i], pt[i][:, L + 1:L + 2], "h1"), 0.0)
                for b in range(15):
                    p_ = pt[b % 2]; s_ = sm[b % 2]
                    k.dma((p_, p_[:, 1:L + 1], "m"), (PF, PF[b * 128:(b + 1) * 128, :]))
                    k.tt("dve", (s_, s_.a(), "a"), V(p_), (p_, p_[:, 2:L + 2]), ALU.add) if False else None
                    P.op("dve", lambda e, p_=p_, s_=s_: e.tensor_tensor(out=s_.a(), in0=p_[:, 0:L], in1=p_[:, 2:L + 2], op=ALU.add),
                         reads=[p_], writes=[s_])
                    k.cp("dve", (s_, s_[:, 255:256]), (p_, p_[:, 255:256]))
                    k.cp("dve", (s_, s_[:, 256:257]), (p_, p_[:, 258:259]))
                    k.ts("pool", V(s_), V(s_), (hmu, hmu[:, b:b + 1]), ALU.mult)
                    k.stt("dve", V(s_), (p_, p_[:, 1:L + 1]), (omu, omu[:, b:b + 1]), V(s_), ALU.mult, ALU.add)
                    k.dma((PR, PR[b * 128:(b + 1) * 128, :], b), V(s_))
                P.barrier()

        def rwkv_gate(PR, GT):
            with ExitStack() as ph:
                gd = P.sb("gd", [128, L], F32, es=ph)
                gs = P.sb("gs", [128, L], BF16, es=ph)
                g2f = P.sb("g2f", [128, 512], F32, es=ph)
                g2b = P.sb("g2b", [128, 512], BF16, es=ph)
                pg = [P.ps(f"pg{i}", [128, 512], F32, es=ph) for i in range(2)]
                sg = [P.sb(f"sg{i}", [128, 512], F32, es=ph) for i in range(2)]
                k.dma(V(gd), (PR, PR[1792:1920, :]))
                k.dma(V(g2f), V(I["rwkv_g2"], I["rwkv_g2"][0]))
                k.cp("dve", V(g2b), V(g2f))
                for q4 in range(4):
                    k.act((gs, gs[:, q4 * 1088:(q4 + 1) * 1088], q4), (gd, gd[:, q4 * 1088:(q4 + 1) * 1088]), AF.Sigmoid)
                for tt in range(NT if "gate_nomm" not in debug else 0):
                    k.mm(V(pg[tt % 2]), (gs, gs[:, tt * 128:(tt + 1) * 128]), V(g2b))
                    k.cp("dve" if tt % 2 else "act", V(sg[tt % 2]), V(pg[tt % 2]))
                    k.dma((GT, GT[tt * 128:(tt + 1) * 128, :], tt), V(sg[tt % 2]))
                P.barrier()

        def mixer_rwkv(PR, GT, YM):
            BW = 256
            NB = L // BW
            CB = BW // CH
            NL = 5
            with ExitStack() as ph:
                w2all = P.sb("w2all", [128, 512], F32, es=ph)
                a2all = P.sb("a2all", [128, 512], F32, es=ph)
                k.dma(V(w2all), V(I["rwkv_w2"], I["rwkv_w2"][0].rearrange("z r c -> (z r) c")))
                k.dma(V(a2all), V(I["rwkv_a2"], I["rwkv_a2"][0].rearrange("z r c -> (z r) c")))
                def hv(name, src):
                    t = P.sb(name, [64, 8], F32, es=ph)
                    k.dma(V(t), (src[0], src[1].rearrange("(h n) -> n h", n=64)), allow_slow_non_contiguous=True)
                    return t
                w0 = [hv(f"w0_{z}", (I["rwkv_w0"], I["rwkv_w0"][0, z])) for z in range(2)]
                a0 = [hv(f"a0_{z}", (I["rwkv_a0"], I["rwkv_a0"][0, z])) for z in range(2)]
                kkg = hv("kkg", (I["rwkv_k_k"], I["rwkv_k_k"][0]))
                kag = hv("kag", (I["rwkv_k_a"], I["rwkv_k_a"][0]))
                rkg = hv("rkg", (I["rwkv_r_k"], I["rwkv_r_k"][0]))
                oka = P.sb("oka", [64, 8], F32, es=ph)
                k.ts("dve", V(oka), V(kag), -1.0, ALU.mult, 1.0, ALU.add)
                lnw = P.sb("lnw", [64, 512], F32, es=ph)
                lnb = P.sb("lnb", [64, 512], F32, es=ph)
                k.dma(V(lnw), V(I["rwkv_ln_w"], I["rwkv_ln_w"][0].partition_broadcast(64)))
                k.dma(V(lnb), V(I["rwkv_ln_b"], I["rwkv_ln_b"][0].partition_broadcast(64)))
                rst = P.sb("rst", [64, BW], F32, es=ph)
                k.memset("pool", V(rst), 1.0)
                k.memset("pool", (rst, rst.a().rearrange("p (c t) -> p c t", t=CH)[:, :, 0:1]), 0.0)
                ones64 = P.sb("ones64", [64, 64], F32, es=ph)
                k.memset("pool", V(ones64), 1.0)
                m4 = []
                m3 = []
                for z in range(2):
                    m = P.sb(f"m4_{z}", [128, 2, 64], F32, es=ph)
                    for half in range(2):
                        for col in range(2):
                            sgn = 1 if z == 0 else -1
                            base = (-1 if col == 0 else 0)
                            P.op("pool", lambda e, m=m, half=half, col=col, sgn=sgn, base=base: e.affine_select(
                                out=m[half * 64:(half + 1) * 64, col, :], in_=ones[half * 64:(half + 1) * 64, 0:64],
                                pattern=[[sgn, 64]], compare_op=ALU.is_ge, fill=0.0, base=base, channel_multiplier=-sgn),
                                reads=[ones], writes=[m])
                    m4.append(m)
                    mm3 = P.sb(f"m3_{z}", [64, 64], F32, es=ph)
                    P.op("pool", lambda e, mm3=mm3, z=z: e.affine_select(
                        out=mm3.a(), in_=ones[0:64, 0:64], pattern=[[-1 if z == 0 else 1, 64]], compare_op=ALU.is_ge, fill=0.0,
                        base=-1, channel_multiplier=1 if z == 0 else -1), reads=[ones], writes=[mm3])
                    m3.append(mm3)
                QI0 = P.sb("QI0", [64, 64], BF16, es=ph)
                k.cp("dve", V(QI0), (ident, ident[0:64, 0:64]))

                nheads = 0 if "rw1" in debug else (1 if ("rw2" in debug or "rw3" in debug) else 8)
                for h in range(nheads):
                    with ExitStack() as hs:
                        Vt = P.sb("Vt", [64, NCH, CH], F32, es=hs)
                        oacc = P.sb("oaccr", [64, NCH, CH], F32, es=hs)
                        bon = P.sb("bon", [64, NCH], F32, es=hs)
                        hs2 = ExitStack()
                        AR = [P.sb(f"AR{z}", [64, NCH, 2, CH], BF16, es=hs2) for z in range(2)]
                        BK = [P.sb(f"BK{z}", [64, NCH, 2, CH], BF16, es=hs2) for z in range(2)]
                        BKeT = [P.sb(f"BKeT{z}", [128, NCH, CH], BF16, es=hs2) for z in range(2)]
                        UV = [P.sb(f"UV{z}", [128, NCH, CH], BF16, es=hs2) for z in range(2)]
                        ZV = P.sb("ZV", [128, NCH, CH], BF16, es=hs2)
                        gC = [P.sb(f"gC{z}", [64, NCH], F32, es=hs2) for z in range(2)]
                        k.memset("pool", (ZV, ZV[0:64, :, :], "z"), 0.0)
                        with ExitStack() as pp_:
                            def T(name, dt=F32, w=BW):
                                return P.sb(name, [64, w], dt, es=pp_)
                            t_r = T("t_r"); t_k = T("t_k"); t_v = T("t_v")
                            t_wd = P.sb("t_wd", [128, BW], F32, es=pp_)
                            t_ad = P.sb("t_ad", [128, BW], F32, es=pp_)
                            t_kk = T("t_kk"); t_q = T("t_q"); t_rn = T("t_rn")
                            t_sg = T("t_sg"); t_cs = T("t_cs"); t_x = T("t_x"); t_y = T("t_y")
                            t_eg = T("t_eg"); t_eng = T("t_eng"); t_egp = T("t_egp")
                            t_a = T("t_a"); t_km = [T("t_km0"), T("t_km1")]; t_b = T("t_b")
                            bke = P.sb("bke", [64, CB, 2, CH], BF16, es=pp_)
                            t_v2 = P.sb("t_v2", [64, CB, 2, CH], F32, es=pp_)
                            pwa = [P.ps(f"pwa{i}", [64, BW], F32, es=pp_) for i in range(2)]
                            pss = P.ps("pss", [64, BW], F32, es=pp_)
                            ptv = P.ps("ptv", [128, CB, CH], F32, es=pp_)
                            ptb = P.ps("ptb", [128, CB, CH], BF16, es=pp_)
                            pbn = P.ps("pbn", [64, NCH], F32, es=pp_)
                            for blk in range(NB):
                                tsl = slice(blk * BW, (blk + 1) * BW)
                                csl = slice(blk * CB, (blk + 1) * CB)
                                k.dma(V(t_r), (PR, PR[h * 64:(h + 1) * 64, tsl]))
                                k.dma(V(t_k), (PR, PR[512 + h * 64:512 + (h + 1) * 64, tsl]))
                                k.dma(V(t_v), (PR, PR[1024 + h * 64:1024 + (h + 1) * 64, tsl]))
                                k.dma(V(t_wd), (PR, PR[1536:1664, tsl]))
                                k.dma(V(t_ad), (PR, PR[1664:1792, tsl]))
                                k.act(V(t_wd), V(t_wd), AF.Tanh)
                                k.cp("pool", V(t_v2), (t_v, t_v.a().rearrange("p (c o t) -> p c o t", o=1, t=CH).to_broadcast([64, CB, 2, CH])))
                                for j in range(CB):
                                    k.tr((ptv, ptv[:, j, :]), (t_v2, t_v2[:, j, :, :].rearrange("p a t -> p (a t)")), (ident, ident[0:64, 0:64]))
                                k.cp("act", (Vt, Vt[:, csl, :], blk), (ptv, ptv[0:64, :, :]))
                                for z in range(2):
                                    k.cp("dve", (UV[z], UV[z][64:128, csl, :], ("v", blk)), (ptv, ptv[64:128, :, :]))
                                k.cp("dve", (ZV, ZV[64:128, csl, :], ("v", blk)), (ptv, ptv[64:128, :, :]))
                                k.ts("dve", V(t_kk), V(t_k), (kkg, kkg[:, h:h + 1]), ALU.mult)
                                k.tt("pool", V(t_q), V(t_kk), V(t_kk), ALU.mult)
                                k.mm(V(pss), V(ones64), V(t_q))
                                k.ts("dve", V(t_rn), V(pss), 1e-12, ALU.add)
                                k.act(V(t_rn), V(t_rn), AF.Sqrt)
                                k.recip(V(t_rn), V(t_rn))
                                k.tt("dve", V(t_kk), V(t_kk), V(t_rn), ALU.mult)
                                for z in range(2):
                                    end = CH - 1 if z == 0 else 0
                                    zs = slice(z * 64, (z + 1) * 64)
                                    pw = pwa[0]; pa = pwa[1]
                                    k.mm(V(pw), (w2all, w2all[zs, h * 64:(h + 1) * 64]), (t_wd, t_wd[zs, :]))
                                    k.mm(V(pa), (a2all, a2all[zs, h * 64:(h + 1) * 64]), (t_ad, t_ad[zs, :]))
                                    k.act(V(t_sg), V(pw), AF.Sigmoid, bias=(w0[z], w0[z][:, h:h + 1]))
                                    k.act(V(t_a), V(pa), AF.Sigmoid, bias=(a0[z], a0[z][:, h:h + 1]))
                                    k.scan(V(t_cs), V(rst), V(t_sg), 0.0, ALU.mult, ALU.add)
                                    if z == 1:
                                        k.tt("dve", V(t_x), V(t_sg), V(t_cs), ALU.subtract)
                                        c3 = t_cs.a().rearrange("p (c t) -> p c t", t=CH)
                                        k.tt("dve", (t_y, t_y.a().rearrange("p (c t) -> p c t", t=CH)),
                                             (t_x, t_x.a().rearrange("p (c t) -> p c t", t=CH)),
                                             (t_cs, c3[:, :, CH - 1:CH].to_broadcast([64, CB, CH])), ALU.add)
                                        cs = t_y
                                    else:
                                        cs = t_cs
                                    k.act(V(t_eg), V(cs), AF.Exp, scale=-0.6065306597126334)
                                    k.act(V(t_eng), V(cs), AF.Exp, scale=0.6065306597126334)
                                    k.tt("dve", V(t_x), V(cs), V(t_sg), ALU.subtract)
                                    k.act(V(t_egp), V(t_x), AF.Exp, scale=-0.6065306597126334)
                                    eg3 = t_eg.a().rearrange("p (c t) -> p c t", t=CH)
                                    k.cp("pool", (gC[z], gC[z][:, csl], blk), (t_eg, eg3[:, :, end]))
                                    k.ts("dve", V(t_x), V(t_a), (kag, kag[:, h:h + 1]), ALU.mult, (oka, oka[:, h:h + 1]), ALU.add)
                                    k.tt("dve", V(t_km[z]), V(t_k), V(t_x), ALU.mult)
                                    k.tt("pool", V(t_b), V(t_kk), V(t_a), ALU.mult)
                                    arz = AR[z]; bkz = BK[z]
                                    k.stt("dve", (arz, arz[:, csl, 0, :], blk), (t_kk, t_kk.a().rearrange("p (c t) -> p c t", t=CH)), -1.0,
                                          (t_egp, t_egp.a().rearrange("p (c t) -> p c t", t=CH)), ALU.mult, ALU.mult)
                                    k.tt("pool", (arz, arz[:, csl, 1, :], blk), (t_r, t_r.a().rearrange("p (c t) -> p c t", t=CH)),
                                         (t_eg, eg3), ALU.mult)
                                    k.tt("dve", V(t_b), V(t_b), V(t_eng), ALU.mult)
                                    k.tt("dve", V(t_x), V(t_km[z]), V(t_eng), ALU.mult)
                                    k.cp("pool", (bkz, bkz[:, csl, 0, :], blk), (t_b, t_b.a().rearrange("p (c t) -> p c t", t=CH)))
                                    k.cp("act", (bkz, bkz[:, csl, 1, :], blk), (t_x, t_x.a().rearrange("p (c t) -> p c t", t=CH)))
                                    gcb = eg3[:, :, end:end + 1].to_broadcast([64, CB, CH])
                                    k.tt("dve", (bke, bke[:, :, 0, :]), (t_b, t_b.a().rearrange("p (c t) -> p c t", t=CH)), (t_eg, gcb), ALU.mult)
                                    k.tt("pool", (bke, bke[:, :, 1, :]), (t_x, t_x.a().rearrange("p (c t) -> p c t", t=CH)), (t_eg, gcb), ALU.mult)
                                    for j in range(CB):
                                        k.tr((ptb, ptb[:, j, :]), (bke, bke[:, j, :, :].rearrange("p a t -> p (a t)")), (identb, identb[0:64, 0:64]))
                                    k.cp("act", (BKeT[z], BKeT[z][:, csl, :], blk), V(ptb))
                                k.tt("dve", V(t_x), V(t_km[0]), V(t_km[1]), ALU.add)
                                k.stt("dve", V(t_x), V(t_r), (rkg, rkg[:, h:h + 1]), V(t_x), ALU.mult, ALU.mult)
                                for j in range(CB):
                                    c = blk * CB + j
                                    k.mm((pbn, pbn[:, c:c + 1]), (t_x, t_x[:, j * CH:(j + 1) * CH]), (ones64, ones64[:, 0:1]))
                            k.cp("dve", V(bon), V(pbn))
                        P.barrier()
                        with ExitStack() as us:
                            Hs = [P.sb(f"Hs{z}", [64, 64], F32, es=us) for z in range(2)]
                            Hb = [P.sb(f"Hb{z}", [64, 64], BF16, es=us) for z in range(2)]
                            AM = [[P.sb(f"AM{z}{i}", [128, 128], BF16, es=us) for i in range(2)] for z in range(2)]
                            Pm = [[P.sb(f"Pm{z}{i}", [64, 64], BF16, es=us) for i in range(2)] for z in range(2)]
                            QR = [[P.sb(f"QR{z}{i}", [64, 2, 64], BF16, es=us) for i in range(2)] for z in range(2)]
                            TT = [[P.sb(f"TT{z}{i}", [64, 64], BF16, es=us) for i in range(2)] for z in range(2)]
                            Xs = [P.sb(f"Xs{z}", [64, 64], BF16, es=us) for z in range(2)]
                            bA = [P.ps(f"bA{z}", [128, 512], F32, es=us) for z in range(2)]
                            bB = [P.ps(f"bB{z}", [64, 128], F32, es=us) for z in range(2)]
                            bC = [P.ps(f"bC{z}", [64, 64], F32, es=us) for z in range(2)]
                            bD = [P.ps(f"bD{z}", [64, 128], F32, es=us) for z in range(2)]
                            vM = [bA[z][:, 0:128] for z in range(2)]
                            vX = [bA[z][0:64, 128:192] for z in range(2)]
                            vU = [bA[z][0:64, 192:256] for z in range(2)]
                            vL = [bB[z].a() for z in range(2)]
                            vP = [bC[z].a() for z in range(2)]
                            vO = [bD[z][:, 0:64] for z in range(2)]
                            vH = [bD[z][:, 64:128] for z in range(2)]
                            pM = [(bA[z], vM[z]) for z in range(2)]
                            pL = [(bB[z], vL[z]) for z in range(2)]
                            pP = [(bC[z], vP[z]) for z in range(2)]
                            pX = [(bA[z], vX[z]) for z in range(2)]
                            pU = [(bA[z], vU[z]) for z in range(2)]
                            pO = [(bD[z], vO[z]) for z in range(2)]
                            pH = [(bD[z], vH[z]) for z in range(2)]
                            k.memset("pool", V(oacc), 0.0)
                            for z in range(2):
                                k.memset("pool", V(Hs[z]), 0.0)
                                k.memset("pool", V(Hb[z]), 0.0)
                            orders = [chunk_order(0), chunk_order(1)]
                            for step in range(NCH if "rw2" not in debug else 0):
                                for z in range(2):
                                    c = orders[z][step]
                                    am = AM[z][step % 2]
                                    arc = AR[z][:, c, :, :].rearrange("p a t -> p (a t)")
                                    bkc = BK[z][:, c, :, :].rearrange("p a t -> p (a t)")
                                    k.mm(pM[z], (BK[z], bkc), (AR[z], arc))
                                    k.tt("dve", V(am), pM[z], (m4[z], m4[z].a().rearrange("p a t -> p (a t)")), ALU.mult)
                                    k.mm(pP[z], (AR[z], AR[z][:, c, 0, :]), (BK[z], BK[z][:, c, 0, :]))
                                    pm_ = Pm[z][0]
                                    k.tt("pool" if False else "dve", V(pm_), pP[z], V(m3[z]), ALU.mult)
                                    if "u1" in debug:
                                        continue
                                    qr = QR[z][0]
                                    k.cp("act", (qr, qr[:, 0, :]), (am, am[0:64, 0:64]))
                                    k.cp("pool", (qr, qr[:, 1, :]), V(QI0))
                                    for lv in range(1, NL + 1):
                                        qn = QR[z][lv % 2]
                                        pn = Pm[z][lv % 2]
                                        k.mm(pL[z], V(pm_), (qr, qr.a().rearrange("p a t -> p (a t)")))
                                        k.mm(pP[z], (qr, qr[:, 0, :]), V(pm_))
                                        k.cp("act", (qn, qn[:, 0, :]), (bB[z], vL[z][:, 0:64]))
                                        k.tt("dve", (qn, qn[:, 1, :]), (bB[z], vL[z][:, 64:128]), (qr, qr[:, 1, :]), ALU.add)
                                        k.cp("act", V(pn), pP[z])
                                        qr = qn; pm_ = pn
                                    tt_ = TT[z][step % 2]
                                    k.mm((bB[z], vL[z][:, 0:64]), V(pm_), (qr, qr[:, 1, :]))
                                    k.tt("dve", V(tt_), (bB[z], vL[z][:, 0:64]), (qr, qr[:, 1, :]), ALU.add)
                                    if "u2" in debug:
                                        continue
                                    k.mm(pX[z], (AR[z], AR[z][:, c, 0, :]), V(Hb[z]), start=True, stop=False)
                                    P.op("pe", lambda e, px=vX[z], am=am, c=c: e.matmul(px, lhsT=am[:, 0:64], rhs=ZV[:, c, :], start=False, stop=True),
                                         reads=[(am, None), (ZV, "z"), (ZV, ("v", c // CB))], writes=[(bA[z], None)], pe_acc=True)
                                    k.cp("act", V(Xs[z]), pX[z])
                                    k.mm(pU[z], V(tt_), V(Xs[z]))
                                    k.cp("dve", (UV[z], UV[z][0:64, c, :], ("u", c)), pU[z])
                                    if "u3" in debug:
                                        continue
                                    k.mm(pO[z], (AR[z], AR[z][:, c, 1, :]), V(Hb[z]), start=True, stop=False)
                                    P.op("pe", lambda e, po=vO[z], am=am, uv=UV[z], c=c: e.matmul(po, lhsT=am[:, 64:128], rhs=uv[:, c, :], start=False, stop=True),
                                         reads=[(am, None), (UV[z], ("u", c)), (UV[z], ("v", c // CB))], writes=[(bD[z], None)], pe_acc=True)
                                    k.tt("dve", (oacc, oacc[:, c, :], c), (oacc, oacc[:, c, :], c), pO[z], ALU.add)
                                    P.op("pe", lambda e, ph_=vH[z], bt=BKeT[z], uv=UV[z], c=c: e.matmul(ph_, lhsT=bt[:, c, :], rhs=uv[:, c, :], start=True, stop=True),
                                         reads=[(BKeT[z], None), (UV[z], ("u", c)), (UV[z], ("v", c // CB))], writes=[(bD[z], None)])
                                    k.stt("dve", V(Hs[z]), V(Hs[z]), (gC[z], gC[z][:, c:c + 1]), pH[z], ALU.mult, ALU.add)
                                    k.cp("act", V(Hb[z]), V(Hs[z]))
                        if "d_oacc" in debug and h == 0:
                            do = P.dram("d_oacc", [64, NCH * CH], F32, kind="ExternalOutput")
                            k.dma(V(do), (oacc, oacc.a().rearrange("p a b -> p (a b)")))
                            dm4 = P.dram("d_m4", [128, 2 * 128], F32, kind="ExternalOutput")
                            for z in range(2):
                                k.dma((dm4, dm4[:, z * 128:(z + 1) * 128]), (m4[z], m4[z].a().rearrange("p a b -> p (a b)")))
                            dm3 = P.dram("d_m3", [64, 2 * 64], F32, kind="ExternalOutput")
                            for z in range(2):
                                k.dma((dm3, dm3[:, z * 64:(z + 1) * 64]), V(m3[z]))
                            dgc = P.dram("d_gC", [64, 2 * NCH], F32, kind="ExternalOutput")
                            for z in range(2):
                                k.dma((dgc, dgc[:, z * NCH:(z + 1) * NCH]), V(gC[z]))
                            dbon = P.dram("d_bon", [64, NCH], F32, kind="ExternalOutput")
                            k.dma(V(dbon), V(bon))
                            dar = P.dram("d_AR", [64, 2 * NCH * 2 * CH], BF16, kind="ExternalOutput")
                            dbk = P.dram("d_BK", [64, 2 * NCH * 2 * CH], BF16, kind="ExternalOutput")
                            for z in range(2):
                                k.dma((dar, dar[:, z * NCH * 128:(z + 1) * NCH * 128]), (AR[z], AR[z].a().rearrange("p a b c -> p (a b c)")))
                                k.dma((dbk, dbk[:, z * NCH * 128:(z + 1) * NCH * 128]), (BK[z], BK[z].a().rearrange("p a b c -> p (a b c)")))
                        P.barrier()
                        hs2.close()
                        with ExitStack() as fs:
                            gt = P.sb("gtr", [64, NCH, CH], F32, es=fs)
                            cen = P.sb("cen", [64, NCH, CH], F32, es=fs)
                            mu_ = P.sb("mu_", [64, NCH], F32, es=fs)
                            var = P.sb("var", [64, NCH], F32, es=fs)
                            k.dma(V(gt), (GT, GT[:, h * 64:(h + 1) * 64].rearrange("(c p) d -> p c d", p=CH)))
                            P.op("dve", lambda e: e.tensor_reduce(out=mu_.a(), in_=oacc.a(), axis=AX.X, op=ALU.add), reads=[oacc], writes=[mu_])
                            k.ts("dve", V(mu_), V(mu_), 1.0 / 64, ALU.mult)
                            b3 = lambda t: t.a().rearrange("p (c o) -> p c o", o=1).to_broadcast([64, NCH, CH])
                            k.tt("dve", V(oacc), V(oacc), (mu_, b3(mu_)), ALU.subtract)
                            k.tt("pool", V(cen), V(oacc), V(oacc), ALU.mult)
                            P.op("dve", lambda e: e.tensor_reduce(out=var.a(), in_=cen.a(), axis=AX.X, op=ALU.add), reads=[cen], writes=[var])
                            k.ts("dve", V(var), V(var), 1.0 / 64, ALU.mult, 64e-5, ALU.add)
                            k.act(V(var), V(var), AF.Sqrt)
                            k.recip(V(var), V(var))
                            k.tt("dve", V(oacc), V(oacc), (var, b3(var)), ALU.mult)
                            rb = lambda t: t[:, h * 64:(h + 1) * 64].rearrange("p (o d) -> p o d", o=1).to_broadcast([64, NCH, CH])
                            k.tt("pool", V(oacc), V(oacc), (lnw, rb(lnw)), ALU.mult)
                            k.tt("dve", V(oacc), V(oacc), (lnb, rb(lnb)), ALU.add)
                            k.tt("pool", V(cen), V(Vt), (bon, b3(bon)), ALU.mult)
                            k.tt("dve", V(oacc), V(oacc), V(cen), ALU.add)
                            k.tt("dve", V(oacc), V(oacc), V(gt), ALU.mult)
                            k.dma((YM, YM[:, h * 64:(h + 1) * 64].rearrange("(c p) d -> p c d", p=CH), ("r", h)), V(oacc))
                        P.barrier()

        def lat_rows(tt, cc):
            c0 = 2 * (tt - 2) + cc
            return XL[LC:, :].rearrange("(r c) d -> c r d", c=64)[c0]

        def stage_outproj(YM, nfeat, W, xsrc, perm, tiles):
            kf = nfeat // 128
            with ExitStack() as ph:
                wb = P.sb("wob", [128, kf, D], BF16, es=ph)
                k.dma(V(wb), (W[0], W[1].rearrange("(ko p) n -> p ko n", p=128)), eng="pool")
                yt = [P.sb(f"yt{i}", [128, nfeat], F32, es=ph) for i in range(2)]
                yT = [P.sb(f"yT{i}", [128, kf, 128], BF16, es=ph) for i in range(2)]
                xo = [P.sb(f"xo{i}", [128, D], F32, es=ph) for i in range(2)]
                xn_ = [P.sb(f"xq{i}", [128, D], F32, es=ph) for i in range(2)]
                pT = [P.ps(f"poT{i}", [128, 8, 128], F32, es=ph) for i in range(2)]
                po = [P.ps(f"poo{i}", [128, 512], F32, es=ph) for i in range(2)]
                for n_, tt in enumerate(tiles):
                    j = 1 if tt < 2 else 0
                    y_ = yt[n_ % 2]; yT_ = yT[n_ % 2]; x_ = xo[n_ % 2]; q_ = xn_[n_ % 2]
                    k.dma(V(y_), (YM, YM[tt * 128:(tt + 1) * 128, :]))
                    if perm and tt >= 2:
                        for cc in range(2):
                            k.dma((x_, x_[cc * 64:(cc + 1) * 64, :], cc), (xsrc(tt)[0], lat_rows(tt, cc), tt))
                    else:
                        sb_, sap = xsrc(tt)
                        k.dma(V(x_), (sb_, sap, tt))
                    for g8 in range(0, kf, 8):
                        p_ = pT[(g8 // 8 + n_) % 2]
                        for ko in range(8):
                            k.tr((p_, p_[:, ko, :]), (y_, y_[:, (g8 + ko) * 128:(g8 + ko + 1) * 128]), V(ident))
                        k.cp("act", (yT_, yT_[:, g8:g8 + 8, :]), V(p_))
                    for nb in range(2):
                        o_ = po[nb]
                        for ko in range(kf):
                            k.mm(V(o_), (yT_, yT_[:, ko, :]), (wb, wb[:, ko, nb * 512:(nb + 1) * 512]), start=(ko == 0), stop=(ko == kf - 1))
                        k.tt("dve", (q_, q_[:, nb * 512:(nb + 1) * 512]), V(o_), (modB, modB[:, 0, j, nb * 512:(nb + 1) * 512]), ALU.mult)
                    k.tt("pool", V(q_), V(q_), V(x_), ALU.add)
                    if perm and tt >= 2:
                        for cc in range(2):
                            k.dma((XL, lat_rows(tt, cc), tt), (q_, q_[cc * 64:(cc + 1) * 64, :]))
                    else:
                        k.dma((XL, XL[tt * 128:(tt + 1) * 128, :], tt), V(q_))
                P.barrier()

        def stage_moe(l, tiles):
            nh = len(tiles) // 2
            with ExitStack() as ph:
                rw32 = P.sb("rw32", [128, KO, 32], F32, es=ph)
                k.dma(V(rw32), V(I["router_w"], I["router_w"][l].rearrange("(ko p) e -> p ko e", p=128)))
                rbB = P.sb("rbB", [128, 32], F32, es=ph)
                k.dma(V(rbB), V(I["router_b"], I["router_b"][l].partition_broadcast(128)))
                BD = P.sb("BD", [32, D], F32, es=ph)
                k.dma(V(BD), V(I["exp_b_down"], I["exp_b_down"][l]))
                bgF = P.sb("bgF", [128, 32, 8], F32, es=ph)
                buF = P.sb("buF", [128, 32, 8], F32, es=ph)
                with ExitStack() as t0:
                    btmp = P.sb("btmp", [128, 128], F32, es=t0)
                    pbt = P.ps("pbt", [128, 128], F32, es=t0)
                    for (dst, src) in ((bgF, I["exp_b_gate"]), (buF, I["exp_b_up"])):
                        for hf in range(2):
                            k.dma(V(btmp), (src, src[l, hf * 16:(hf + 1) * 16, :].rearrange("e (fb p) -> (e fb) p", p=128)))
                            k.tr(V(pbt), V(btmp), V(ident))
                            k.cp("dve", (dst, dst[:, hf * 16:(hf + 1) * 16, :].rearrange("p e f -> p (e f)")), V(pbt))
                    P.barrier()
                for half in range(2):
                    htiles = tiles[half * nh:(half + 1) * nh]
                    NTK = nh * 128
                    with ExitStack() as hs:
                        HTh = P.sb("HTh", [128, KO, NTK], BF16, es=hs)
                        LG = P.sb("LG", [128, nh, 32], F32, es=hs)
                        GW = P.sb("GW", [128, nh, 32], F32, es=hs)
                        acc = P.sb("acc", [128, nh, D], F32, es=hs)
                        with ExitStack() as ns:
                            xt = [P.sb(f"mxt{i}", [128, D], F32, es=ns) for i in range(2)]
                            xn = [P.sb(f"mxn{i}", [128, D], F32, es=ns) for i in range(2)]
                            junk = P.sb("mjunk", [128, D], F32, es=ns)
                            st = [P.sb(f"mst{i}", [128, 4], F32, es=ns) for i in range(2)]
                            tmp = [P.sb(f"mtmp{i}", [128, KO, 128], F32, es=ns) for i in range(2)]
                            h32 = [P.sb(f"mh32{i}", [128, KO, 128], F32, es=ns) for i in range(2)]
                            pT = [P.ps(f"mpT{i}", [128, KO, 128], F32, es=ns) for i in range(2)]
                            plg = [P.ps(f"plg{i}", [128, 32], F32, es=ns) for i in range(2)]
                            pgt = P.ps("pgt", [32, 128], F32, es=ns)
                            GWT = P.sb("GWT", [32, nh, 128], F32, es=ns)
                            pini = [P.ps("pini0", [128, 512], F32, es=ns)] * 2
                            m8 = P.sb("m8", [128, 8], F32, es=ns)
                            msk = P.sb("msk", [128, 32], F32, es=ns)
                            ex = P.sb("ex", [128, 32], F32, es=ns)
                            sm = P.sb("smx", [128, 4], F32, es=ns)
                            for i, tt in enumerate(htiles):
                                j = 1 if tt < 2 else 0
                                x_ = xt[i % 2]; n_ = xn[i % 2]; s_ = st[i % 2]; t_ = tmp[i % 2]; p_ = pT[i % 2]; h_ = h32[i % 2]
                                k.dma(V(x_), (XL, XL[tt * 128:(tt + 1) * 128, :]))
                                k.memset("pool", (s_, s_[:, 0:1]), 0.0)
                                k.act(V(junk), V(x_), AF.Square, accum=(s_, s_[:, 0:1]))
                                k.ts("dve", (s_, s_[:, 1:2]), (s_, s_[:, 0:1]), 1.0 / D, ALU.mult, EPS, ALU.add)
                                k.act((s_, s_[:, 2:3]), (s_, s_[:, 1:2]), AF.Sqrt)
                                k.recip((s_, s_[:, 3:4]), (s_, s_[:, 2:3]))
                                k.ts("dve", V(n_), V(x_), (s_, s_[:, 3:4]), ALU.mult)
                                for ko in range(KO):
                                    k.tr((p_, p_[:, ko, :]), (n_, n_[:, ko * 128:(ko + 1) * 128]), V(ident))
                                k.tt("dve", V(t_), V(p_), (g2F, g2F[:, :, j:j + 1].to_broadcast([128, KO, 128])), ALU.mult)
                                k.tt("pool", V(h_), V(t_), (modF, modF[:, 3, :, j:j + 1].to_broadcast([128, KO, 128])), ALU.add)
                                k.cp("act", (HTh, HTh[:, :, i * 128:(i + 1) * 128], i), V(h_))
                                pl = plg[i % 2]
                                for ko in range(KO):
                                    k.mm(V(pl), (h_, h_[:, ko, :]), (rw32, rw32[:, ko, :]), start=(ko == 0), stop=(ko == KO - 1))
                                lg = (LG, LG[:, i, :], i)
                                k.tt("dve", lg, V(pl), V(rbB), ALU.add)
                                P.op("dve", lambda e, i=i: e.max(out=m8.a(), in_=LG[:, i, :]), reads=[(LG, i)], writes=[m8])
                                k.ts("dve", V(msk), lg, (m8, m8[:, 3:4]), ALU.is_ge)
                                k.ts("dve", (sm, sm[:, 0:1]), (m8, m8[:, 0:1]), -1.0, ALU.mult)
                                k.act(V(ex), lg, AF.Exp, bias=(sm, sm[:, 0:1]))
                                k.tt("dve", V(ex), V(ex), V(msk), ALU.mult)
                                P.op("dve", lambda e: e.tensor_reduce(out=sm[:, 1:2], in_=ex.a(), axis=AX.X, op=ALU.add), reads=[ex], writes=[sm])
                                k.recip((sm, sm[:, 2:3]), (sm, sm[:, 1:2]))
                                k.ts("dve", (GW, GW[:, i, :], i), V(ex), (sm, sm[:, 2:3]), ALU.mult)
                                k.tr(V(pgt), (GW, GW[:, i, :], i), V(ident))
                                k.cp("act", (GWT, GWT[:, i, :], i), V(pgt))
                                for nb in range(2):
                                    k.mm(V(pini[nb]), (GWT, GWT[:, i, :], i), (BD, BD[:, nb * 512:(nb + 1) * 512]))
                                    k.cp("act" if nb else "dve", (acc, acc[:, i, nb * 512:(nb + 1) * 512], (i, nb)), V(pini[nb]))
                            P.barrier()
                        if f"LG{l}" in debug and half == 0:
                            dl = P.dram(f"d_GW{l}", [128, nh * 32], F32, kind="ExternalOutput")
                            k.dma(V(dl), (GW, GW.a().rearrange("p a b -> p (a b)")))
                        with ExitStack() as xs:
                            wgs = [P.sb(f"wg{i}", [128, KO, 512], BF16, es=xs) for i in range(2)]
                            wus = [P.sb(f"wu{i}", [128, KO, 512], BF16, es=xs) for i in range(2)]
                            wds = [P.sb(f"wd{i}", [128, 4, D], BF16, es=xs) for i in range(2)]
                            aTs = [P.sb(f"aT{i}", [128, 4, 512], BF16, es=xs) for i in range(2)]
                            dtmp = [P.sb(f"dtmp{i}", [128, 512], F32, es=xs) for i in range(2)]
                            g1 = [P.sb(f"g1_{i}", [128, 512], F32, es=xs) for i in range(2)]
                            sg = [P.sb(f"sg_{i}", [128, 512], F32, es=xs) for i in range(2)]
                            u1 = [P.sb(f"u1_{i}", [128, 512], F32, es=xs) for i in range(2)]
                            pg = [P.ps(f"mpg{i}", [128, 512], F32, es=xs) for i in range(2)]
                            pu = [P.ps(f"mpu{i}", [128, 512], F32, es=xs) for i in range(2)]
                            pd = [P.ps(f"mpd{i}", [128, 512], F32, es=xs) for i in range(2)]
                            if nh == 17:
                                tws = [512, 512, 384, 384, 384]
                            else:
                                tws = [512] * (NTK // 512)
                            tblocks = []
                            t_acc = 0
                            for tw in tws:
                                tblocks.append((t_acc, tw)); t_acc += tw
                            nexp = 32 if "moe_fast" not in debug else 2
                            cnt = 0
                            nblk = 0
                            cntbox = [0]

                            def emit_loads(he):
                                e, fh = he // 2, he % 2
                                wg = wgs[he % 2]; wu = wus[he % 2]; wd = wds[he % 2]
                                k.dma(V(wg), (I["exp_w_gate"], I["exp_w_gate"][l, e][:, fh * 512:(fh + 1) * 512].rearrange("(ko p) n -> p ko n", p=128)), eng="pool")
                                k.dma(V(wu), (I["exp_w_up"], I["exp_w_up"][l, e][:, fh * 512:(fh + 1) * 512].rearrange("(ko p) n -> p ko n", p=128)), eng="pool")
                                k.dma(V(wd), (I["exp_w_down"], I["exp_w_down"][l, e][fh * 512:(fh + 1) * 512, :].rearrange("(fo p) n -> p fo n", p=128)), eng="pool")

                            def emit_G(j, he, t0_, tw):
                                e, fh = he // 2, he % 2
                                wg = wgs[he % 2]; wu = wus[he % 2]
                                aT = aTs[j % 2]
                                for fbl in range(4):
                                    fb = fh * 4 + fbl
                                    cnt = cntbox[0]; cntbox[0] += 1
                                    pg_ = pg[cnt % 2]; pu_ = pu[cnt % 2]; g_ = g1[cnt % 2]; s_ = sg[cnt % 2]; u_ = u1[cnt % 2]
                                    for ko in range(KO):
                                        k.mm((pg_, pg_[:, 0:tw]), (wg, wg[:, ko, fbl * 128:(fbl + 1) * 128]), (HTh, HTh[:, ko, t0_:t0_ + tw]),
                                             start=(ko == 0), stop=(ko == KO - 1))
                                    for ko in range(KO):
                                        k.mm((pu_, pu_[:, 0:tw]), (wu, wu[:, ko, fbl * 128:(fbl + 1) * 128]), (HTh, HTh[:, ko, t0_:t0_ + tw]),
                                             start=(ko == 0), stop=(ko == KO - 1))
                                    k.ts("dve", (g_, g_[:, 0:tw]), (pg_, pg_[:, 0:tw]), (bgF, bgF[:, e, fb:fb + 1]), ALU.add, 7.0, ALU.min)
                                    k.act((s_, s_[:, 0:tw]), (g_, g_[:, 0:tw]), AF.Sigmoid, scale=1.702)
                                    k.act((u_, u_[:, 0:tw]), (pu_, pu_[:, 0:tw]), AF.Identity, bias=(buF, buF[:, e, fb:fb + 1]))
                                    k.ts("dve", (u_, u_[:, 0:tw]), (u_, u_[:, 0:tw]), 7.0, ALU.min, -7.0, ALU.max)
                                    k.tt("dve", (g_, g_[:, 0:tw]), (g_, g_[:, 0:tw]), (s_, s_[:, 0:tw]), ALU.mult)
                                    k.stt("dve", (aT, aT[:, fbl, 0:tw], fbl), (u_, u_[:, 0:tw]), 1.0, (g_, g_[:, 0:tw]), ALU.add, ALU.mult)

                            def emit_D(j, he, t0_, tw):
                                e = he // 2
                                wd = wds[he % 2]
                                aT = aTs[j % 2]
                                for ti in range(tw // 128):
                                    i = t0_ // 128 + ti
                                    for nb in range(2):
                                        pd_ = pd[nb]
                                        for fo in range(4):
                                            k.mm(V(pd_), (aT, aT[:, fo, ti * 128:(ti + 1) * 128], fo), (wd, wd[:, fo, nb * 512:(nb + 1) * 512]),
                                                 start=(fo == 0), stop=(fo == 3))
                                        asl = (acc, acc[:, i, nb * 512:(nb + 1) * 512], (i, nb))
                                        if nb == 0:
                                            k.stt("dve", asl, V(pd_), (GW, GW[:, i, e:e + 1], i), asl, ALU.mult, ALU.add)
                                        else:
                                            dt_ = dtmp[i % 2]
                                            k.act(V(dt_), V(pd_), AF.Copy, scale=(GW, GW[:, i, e:e + 1], i))
                                            k.tt("dve", asl, asl, V(dt_), ALU.add)

                            blocks = [(he, t0_, tw) for he in range(nexp * 2) for (t0_, tw) in tblocks]
                            emit_loads(0)
                            emit_G(0, *blocks[0])
                            for j in range(len(blocks)):
                                if j + 1 < len(blocks):
                                    if blocks[j + 1][0] != blocks[j][0]:
                                        emit_loads(blocks[j + 1][0])
                                    emit_G(j + 1, *blocks[j + 1])
                                emit_D(j, *blocks[j])
                            P.barrier()
                        with ExitStack() as rs:
                            xr = [P.sb(f"xr{i}", [128, D], F32, es=rs) for i in range(2)]
                            for i, tt in enumerate(htiles):
                                j = 1 if tt < 2 else 0
                                x_ = xr[i % 2]
                                k.dma(V(x_), (XL, XL[tt * 128:(tt + 1) * 128, :], ("m", tt)))
                                k.tt("dve", (acc, acc[:, i, :]), (acc, acc[:, i, :]), (modB, modB[:, 1, j, :]), ALU.mult)
                                k.tt("pool", V(x_), V(x_), (acc, acc[:, i, :]), ALU.add)
                                k.dma((XL, XL[tt * 128:(tt + 1) * 128, :], ("m", tt)), V(x_))
                            P.barrier()

        def mixer_rwkv2(PR, GT, YM):
            BW = 1088
            NB = L // BW
            CB = BW // CH
            NL = 5
            CBU = 4
            NSS = NCH // CBU
            SUB = ((0, 512), (512, 512), (1024, 64))
            ARD = P.dram("ARD", [16, 64, NCH * 128], BF16)
            BKD = P.dram("BKD", [16, 64, NCH * 128], BF16)
            BKTD = P.dram("BKTD", [16, 128, NCH * 64], BF16)
            VVD = P.dram("VVD", [8, 128, NCH * 64], BF16)
            VTD = P.dram("VTD", [8, 64, NCH * 64], F32)
            OD = P.dram("OD", [2, L, 512], F32)
            with ExitStack() as ph:
                w2all = P.sb("w2all", [128, 512], F32, es=ph)
                a2all = P.sb("a2all", [128, 512], F32, es=ph)
                k.dma(V(w2all), V(I["rwkv_w2"], I["rwkv_w2"][0].rearrange("z r c -> (z r) c")))
                k.dma(V(a2all), V(I["rwkv_a2"], I["rwkv_a2"][0].rearrange("z r c -> (z r) c")))

                def hv(name, src):
                    t = P.sb(name, [64, 8], F32, es=ph)
                    k.dma(V(t), (src[0], src[1].rearrange("(h n) -> n h", n=64)), allow_slow_non_contiguous=True)
                    return t
                w0 = [hv(f"w0_{z}", (I["rwkv_w0"], I["rwkv_w0"][0, z])) for z in range(2)]
                a0 = [hv(f"a0_{z}", (I["rwkv_a0"], I["rwkv_a0"][0, z])) for z in range(2)]
                kkg = hv("kkg", (I["rwkv_k_k"], I["rwkv_k_k"][0]))
                kag = hv("kag", (I["rwkv_k_a"], I["rwkv_k_a"][0]))
                rkg = hv("rkg", (I["rwkv_r_k"], I["rwkv_r_k"][0]))
                oka = P.sb("oka", [64, 8], F32, es=ph)
                k.ts("dve", V(oka), V(kag), -1.0, ALU.mult, 1.0, ALU.add)
                lnw = P.sb("lnw", [64, 512], F32, es=ph)
                lnb = P.sb("lnb", [64, 512], F32, es=ph)
                k.dma(V(lnw), V(I["rwkv_ln_w"], I["rwkv_ln_w"][0].partition_broadcast(64)))
                k.dma(V(lnb), V(I["rwkv_ln_b"], I["rwkv_ln_b"][0].partition_broadcast(64)))
                ones64 = P.sb("ones64", [64, 64], F32, es=ph)
                k.memset("pool", V(ones64), 1.0)
                m4 = []
                m3 = []
                for z in range(2):
                    m = P.sb(f"m4_{z}", [128, 2, 64], F32, es=ph)
                    sgn = 1 if z == 0 else -1
                    for half in range(2):
                        for col in range(2):
                            base = (-1 if col == 0 else 0)
                            P.op("pool", lambda e, m=m, half=half, col=col, sgn=sgn, base=base: e.affine_select(
                                out=m[half * 64:(half + 1) * 64, col, :], in_=ones[half * 64:(half + 1) * 64, 0:64],
                                pattern=[[sgn, 64]], compare_op=ALU.is_ge, fill=0.0, base=base, channel_multiplier=-sgn),
                                reads=[ones], writes=[m])
                    m4.append(m)
                    mm3 = P.sb(f"m3_{z}", [64, 64], F32, es=ph)
                    P.op("pool", lambda e, mm3=mm3, z=z: e.affine_select(
                        out=mm3.a(), in_=ones[0:64, 0:64], pattern=[[-1 if z == 0 else 1, 64]], compare_op=ALU.is_ge, fill=0.0,
                        base=-1, channel_multiplier=1 if z == 0 else -1), reads=[ones], writes=[mm3])
                    m3.append(mm3)
                QI0 = P.sb("QI0", [64, 64], BF16, es=ph)
                k.cp("dve", V(QI0), (ident, ident[0:64, 0:64]))
                gCall = P.sb("gCall", [64, 16, NCH], F32, es=ph)
                bonall = P.sb("bonall", [64, 8, NCH], F32, es=ph)
                with ExitStack() as pp_:
                    rst = P.sb("rst", [64, BW], F32, es=pp_)
                    k.memset("pool", V(rst), 1.0)
                    k.memset("pool", (rst, rst.a().rearrange("p (c t) -> p c t", t=CH)[:, :, 0:1]), 0.0)

                    def T(name, dt=F32):
                        return P.sb(name, [64, BW], dt, es=pp_)
                    t_r = T("t_r"); t_k = T("t_k"); t_v = T("t_v")
                    t_wd = P.sb("t_wd", [128, BW], F32, es=pp_)
                    t_ad = P.sb("t_ad", [128, BW], F32, es=pp_)
                    t_kk = T("t_kk"); t_q = T("t_q"); t_rn = T("t_rn")
                    t_sg = T("t_sg"); t_cs = T("t_cs"); t_x = T("t_x"); t_y = T("t_y")
                    t_eg = T("t_eg"); t_eng = T("t_eng"); t_egp = T("t_egp")
                    t_a = T("t_a"); t_km = [T("t_km0"), T("t_km1")]; t_b = T("t_b")
                    t_v2 = P.sb("t_v2", [64, CB, 2, CH], F32, es=pp_)
                    arb = P.sb("arb", [64, CB, 2, CH], BF16, es=pp_)
                    bkb = P.sb("bkb", [64, CB, 2, CH], BF16, es=pp_)
                    bke = P.sb("bke", [64, CB, 2, CH], BF16, es=pp_)
                    bktb = P.sb("bktb", [128, CB, CH], BF16, es=pp_)
                    zvb = P.sb("zvb", [128, CB, CH], BF16, es=pp_)
                    vtb = P.sb("vtb", [64, CB, CH], F32, es=pp_)
                    pwa = [P.ps(f"pwa{i}", [64, 512], F32, es=pp_) for i in range(2)]
                    pss = P.ps("pss", [64, 512], F32, es=pp_)
                    ptv = P.ps("ptv", [128, 4, CH], F32, es=pp_)
                    ptb = P.ps("ptb", [128, 4, CH], BF16, es=pp_)
                    pbn = P.ps("pbn", [64, NCH], F32, es=pp_)
                    k.memset("pool", (zvb, zvb[0:64, :, :], "z"), 0.0)
                    c3 = lambda t: t.a().rearrange("p (c t) -> p c t", t=CH)
                    for h in range(8):
                        for blk in range(NB):
                            tsl = slice(blk * BW, (blk + 1) * BW)
                            csl = slice(blk * CB, (blk + 1) * CB)
                            k.dma(V(t_r), (PR, PR[h * 64:(h + 1) * 64, tsl]))
                            k.dma(V(t_k), (PR, PR[512 + h * 64:512 + (h + 1) * 64, tsl]))
                            k.dma(V(t_v), (PR, PR[1024 + h * 64:1024 + (h + 1) * 64, tsl]))
                            k.dma(V(t_wd), (PR, PR[1536:1664, tsl]))
                            k.dma(V(t_ad), (PR, PR[1664:1792, tsl]))
                            k.act(V(t_wd), V(t_wd), AF.Tanh)
                            k.cp("pool", V(t_v2), (t_v, t_v.a().rearrange("p (c o t) -> p c o t", o=1, t=CH).to_broadcast([64, CB, 2, CH])))
                            for j0 in range(0, CB, 4):
                                nj = min(4, CB - j0)
                                for j in range(nj):
                                    k.tr((ptv, ptv[:, j, :]), (t_v2, t_v2[:, j0 + j, :, :].rearrange("p a t -> p (a t)")), (ident, ident[0:64, 0:64]))
                                k.cp("act", (vtb, vtb[:, j0:j0 + nj, :], j0), (ptv, ptv[0:64, 0:nj, :]))
                                k.cp("dve", (zvb, zvb[64:128, j0:j0 + nj, :], ("v", j0)), (ptv, ptv[64:128, 0:nj, :]))
                            k.dma((VTD, VTD[h][:, blk * CB * CH:(blk + 1) * CB * CH], (h, blk)), (vtb, vtb.a().rearrange("p c t -> p (c t)")))
                            k.dma((VVD, VVD[h][:, blk * CB * CH:(blk + 1) * CB * CH], (h, blk)), (zvb, zvb.a().rearrange("p c t -> p (c t)")))
                            k.ts("dve", V(t_kk), V(t_k), (kkg, kkg[:, h:h + 1]), ALU.mult)
                            k.tt("pool", V(t_q), V(t_kk), V(t_kk), ALU.mult)
                            for (s0, sw) in SUB:
                                k.mm((pss, pss[:, 0:sw]), V(ones64), (t_q, t_q[:, s0:s0 + sw]))
                                k.ts("dve", (t_rn, t_rn[:, s0:s0 + sw], s0), (pss, pss[:, 0:sw]), 1e-12, ALU.add)
                            k.act(V(t_rn), V(t_rn), AF.Sqrt)
                            k.recip(V(t_rn), V(t_rn))
                            k.tt("dve", V(t_kk), V(t_kk), V(t_rn), ALU.mult)
                            for z in range(2):
                                end = CH - 1 if z == 0 else 0
                                zs = slice(z * 64, (z + 1) * 64)
                                for (s0, sw) in SUB:
                                    pw = pwa[0]; pa = pwa[1]
                                    k.mm((pw, pw[:, 0:sw]), (w2all, w2all[zs, h * 64:(h + 1) * 64]), (t_wd, t_wd[zs, s0:s0 + sw]))
                                    k.mm((pa, pa[:, 0:sw]), (a2all, a2all[zs, h * 64:(h + 1) * 64]), (t_ad, t_ad[zs, s0:s0 + sw]))
                                    k.act((t_sg, t_sg[:, s0:s0 + sw], s0), (pw, pw[:, 0:sw]), AF.Sigmoid, bias=(w0[z], w0[z][:, h:h + 1]))
                                    k.act((t_a, t_a[:, s0:s0 + sw], s0), (pa, pa[:, 0:sw]), AF.Sigmoid, bias=(a0[z], a0[z][:, h:h + 1]))
                                k.scan(V(t_cs), V(rst), V(t_sg), 0.0, ALU.mult, ALU.add)
                                if z == 1:
                                    k.tt("dve", V(t_x), V(t_sg), V(t_cs), ALU.subtract)
                                    k.tt("dve", (t_y, c3(t_y)), (t_x, c3(t_x)), (t_cs, c3(t_cs)[:, :, CH - 1:CH].to_broadcast([64, CB, CH])), ALU.add)
                                    cs = t_y
                                else:
                                    cs = t_cs
                                k.act(V(t_eg), V(cs), AF.Exp, scale=-0.6065306597126334)
                                k.act(V(t_eng), V(cs), AF.Exp, scale=0.6065306597126334)
                                k.tt("dve", V(t_x), V(cs), V(t_sg), ALU.subtract)
                                k.act(V(t_egp), V(t_x), AF.Exp, scale=-0.6065306597126334)
                                eg3 = c3(t_eg)
                                k.cp("pool", (gCall, gCall[:, h * 2 + z, csl], (h, z, blk)), (t_eg, eg3[:, :, end]))
                                k.ts("dve", V(t_x), V(t_a), (kag, kag[:, h:h + 1]), ALU.mult, (oka, oka[:, h:h + 1]), ALU.add)
                                k.tt("dve", V(t_km[z]), V(t_k), V(t_x), ALU.mult)
                                k.tt("pool", V(t_b), V(t_kk), V(t_a), ALU.mult)
                                k.stt("dve", (arb, arb[:, :, 0, :], 0), (t_kk, c3(t_kk)), -1.0, (t_egp, c3(t_egp)), ALU.mult, ALU.mult)
                                k.tt("pool", (arb, arb[:, :, 1, :], 1), (t_r, c3(t_r)), (t_eg, eg3), ALU.mult)
                                k.tt("dve", V(t_b), V(t_b), V(t_eng), ALU.mult)
                                k.tt("dve", V(t_x), V(t_km[z]), V(t_eng), ALU.mult)
                                k.cp("pool", (bkb, bkb[:, :, 0, :], 0), (t_b, c3(t_b)))
                                k.cp("act", (bkb, bkb[:, :, 1, :], 1), (t_x, c3(t_x)))
                                gcb = eg3[:, :, end:end + 1].to_broadcast([64, CB, CH])
                                k.tt("dve", (bke, bke[:, :, 0, :], 0), (t_b, c3(t_b)), (t_eg, gcb), ALU.mult)
                                k.tt("pool", (bke, bke[:, :, 1, :], 1), (t_x, c3(t_x)), (t_eg, gcb), ALU.mult)
                                for j0 in range(0, CB, 4):
                                    nj = min(4, CB - j0)
                                    for j in range(nj):
                                        k.tr((ptb, ptb[:, j, :]), (bke, bke[:, j0 + j, :, :].rearrange("p a t -> p (a t)")), (identb, identb[0:64, 0:64]))
                                    k.cp("act", (bktb, bktb[:, j0:j0 + nj, :], j0), (ptb, ptb[:, 0:nj, :]))
                                hz = h * 2 + z
                                k.dma((ARD, ARD[hz][:, blk * CB * 128:(blk + 1) * CB * 128], (hz, blk)), (arb, arb.a().rearrange("p c a t -> p (c a t)")))
                                k.dma((BKD, BKD[hz][:, blk * CB * 128:(blk + 1) * CB * 128], (hz, blk)), (bkb, bkb.a().rearrange("p c a t -> p (c a t)")))
                                k.dma((BKTD, BKTD[hz][:, blk * CB * CH:(blk + 1) * CB * CH], (hz, blk)), (bktb, bktb.a().rearrange("p c t -> p (c t)")))
                            k.tt("dve", V(t_x), V(t_km[0]), V(t_km[1]), ALU.add)
                            k.stt("dve", V(t_x), V(t_r), (rkg, rkg[:, h:h + 1]), V(t_x), ALU.mult, ALU.mult)
                            for j in range(CB):
                                c = blk * CB + j
                                k.mm((pbn, pbn[:, c:c + 1]), (t_x, t_x[:, j * CH:(j + 1) * CH]), (ones64, ones64[:, 0:1]))
                        k.cp("dve", (bonall, bonall[:, h, :], h), V(pbn))
                    P.barrier()
                for ps_ in range(2):
                    with ExitStack() as us:
                        chains = [(ps_ * 4 + hl, z) for hl in range(4) for z in range(2)]
                        CHN = []
                        for ci, (h, z) in enumerate(chains):
                            d_ = {}
                            d_["h"] = h; d_["z"] = z; d_["hz"] = h * 2 + z
                            d_["Hs"] = P.sb(f"Hs{ci}", [64, 64], F32, es=us)
                            d_["Hb"] = P.sb(f"Hb{ci}", [64, 64], BF16, es=us)
                            d_["AM"] = [P.sb(f"AM{ci}_{i}", [128, 128], BF16, es=us) for i in range(2)]
                            d_["Pm"] = [P.sb(f"Pm{ci}_{i}", [64, 64], BF16, es=us) for i in range(2)]
                            d_["QR"] = [P.sb(f"QR{ci}_{i}", [64, 2, 64], BF16, es=us) for i in range(2)]
                            d_["TT"] = [P.sb(f"TT{ci}_{i}", [64, 64], BF16, es=us) for i in range(2)]
                            d_["Xs"] = P.sb(f"Xs{ci}", [64, 64], BF16, es=us)
                            d_["ARb"] = [P.sb(f"ARb{ci}_{i}", [64, CBU, 2, CH], BF16, es=us) for i in range(2)]
                            d_["BKb"] = [P.sb(f"BKb{ci}_{i}", [64, CBU, 2, CH], BF16, es=us) for i in range(2)]
                            d_["BKTb"] = [P.sb(f"BKTb{ci}_{i}", [128, CBU, CH], BF16, es=us) for i in range(2)]
                            d_["UVb"] = [P.sb(f"UVb{ci}_{i}", [128, CBU, CH], BF16, es=us) for i in range(2)]
                            d_["ZVb"] = [P.sb(f"ZVb{ci}_{i}", [128, CBU, CH], BF16, es=us) for i in range(2)]
                            d_["Ob"] = [P.sb(f"Ob{ci}_{i}", [64, CBU, CH], F32, es=us) for i in range(2)]
                            d_["bank"] = P.ps(f"bank{ci}", [128, 512], F32, es=us)
                            k.memset("pool", V(d_["Hs"]), 0.0)
                            k.memset("pool", V(d_["Hb"]), 0.0)
                            CHN.append(d_)

                        def clo(z, ss):
                            if z == 0:
                                return ss * CBU
                            return 0 if ss == 0 else NCH - CBU * ss

                        def loads(ss):
                            for d_ in CHN:
                                c0 = clo(d_["z"], ss); i = ss % 2; hz = d_["hz"]; h = d_["h"]
                                k.dma((d_["ARb"][i], d_["ARb"][i].a().rearrange("p c a t -> p (c a t)")), (ARD, ARD[hz][:, c0 * 128:(c0 + CBU) * 128]))
                                k.dma((d_["BKb"][i], d_["BKb"][i].a().rearrange("p c a t -> p (c a t)")), (BKD, BKD[hz][:, c0 * 128:(c0 + CBU) * 128]))
                                k.dma((d_["BKTb"][i], d_["BKTb"][i].a().rearrange("p c t -> p (c t)")), (BKTD, BKTD[hz][:, c0 * CH:(c0 + CBU) * CH]))
                                k.dma((d_["ZVb"][i], d_["ZVb"][i].a().rearrange("p c t -> p (c t)")), (VVD, VVD[h][:, c0 * CH:(c0 + CBU) * CH]))
                                k.dma((d_["UVb"][i], d_["UVb"][i][64:128, :, :].rearrange("p c t -> p (c t)"), "v"), (VVD, VVD[h][64:128, c0 * CH:(c0 + CBU) * CH]))

                        def unit_stage(d_, ss, jj, st):
                            z = d_["z"]; h = d_["h"]; hz = d_["hz"]
                            i = ss % 2
                            c0 = clo(z, ss)
                            cl = jj if z == 0 else CBU - 1 - jj
                            c = c0 + cl
                            step = ss * CBU + jj
                            bk_ = d_["bank"]
                            vM = bk_[:, 0:128]; vL = bk_[0:64, 128:256]; vP = bk_[0:64, 256:320]
                            vX = bk_[0:64, 320:384]; vU = bk_[0:64, 384:448]; vH = bk_[0:64, 448:512]
                            ARb = d_["ARb"][i]; BKb = d_["BKb"][i]; BKTb = d_["BKTb"][i]; UVb = d_["UVb"][i]; ZVb = d_["ZVb"][i]; Ob = d_["Ob"][i]
                            am = d_["AM"][step % 2]
                            tt_ = d_["TT"][step % 2]
                            Hb = d_["Hb"]; Hs = d_["Hs"]; Xs = d_["Xs"]
                            if st == 0:
                                arc = ARb[:, cl, :, :].rearrange("p a t -> p (a t)")
                                bkc = BKb[:, cl, :, :].rearrange("p a t -> p (a t)")
                                k.mm((bk_, vM), (BKb, bkc), (ARb, arc))
                                k.mm((bk_, vP), (ARb, ARb[:, cl, 0, :]), (BKb, BKb[:, cl, 0, :]))
                                k.tt("dve", V(am), (bk_, vM), (m4[z], m4[z].a().rearrange("p a t -> p (a t)")), ALU.mult)
                                pm_ = d_["Pm"][0]
                                k.tt("dve", V(pm_), (bk_, vP), V(m3[z]), ALU.mult)
                                qr = d_["QR"][0]
                                k.cp("act", (qr, qr[:, 0, :]), (am, am[0:64, 0:64]))
                                k.cp("pool", (qr, qr[:, 1, :]), V(QI0))
                            elif 1 <= st <= NL:
                                lv = st
                                qr = d_["QR"][(lv - 1) % 2]; pm_ = d_["Pm"][(lv - 1) % 2]
                                qn = d_["QR"][lv % 2]; pn = d_["Pm"][lv % 2]
                                k.mm((bk_, vL), V(pm_), (qr, qr.a().rearrange("p a t -> p (a t)")))
                                k.mm((bk_, vP), (qr, qr[:, 0, :]), V(pm_))
                                k.cp("act", (qn, qn[:, 0, :]), (bk_, vL[:, 0:64]))
                                k.tt("dve", (qn, qn[:, 1, :]), (bk_, vL[:, 64:128]), (qr, qr[:, 1, :]), ALU.add)
                                k.cp("act", V(pn), (bk_, vP))
                            elif st == NL + 1:
                                qr = d_["QR"][NL % 2]; pm_ = d_["Pm"][NL % 2]
                                k.mm((bk_, vL[:, 0:64]), V(pm_), (qr, qr[:, 1, :]))
                                k.tt("dve", V(tt_), (bk_, vL[:, 0:64]), (qr, qr[:, 1, :]), ALU.add)
                            elif st == NL + 2:
                                k.mm((bk_, vX), (ARb, ARb[:, cl, 0, :]), V(Hb), start=True, stop=False)
                                P.op("pe", lambda e, vX=vX, am=am, ZVb=ZVb, cl=cl: e.matmul(vX, lhsT=am[:, 0:64], rhs=ZVb[:, cl, :], start=False, stop=True),
                                     reads=[(am, None), (ZVb, None)], writes=[(bk_, None)], pe_acc=True)
                                k.cp("act", V(Xs), (bk_, vX))
                            elif st == NL + 3:
                                k.mm((bk_, vU), V(tt_), V(Xs))
                                k.cp("dve", (UVb, UVb[0:64, cl, :], ("u", cl)), (bk_, vU))
                            elif st == NL + 4:
                                k.mm((bk_, vX), (ARb, ARb[:, cl, 1, :]), V(Hb), start=True, stop=False)
                                P.op("pe", lambda e, vX=vX, am=am, UVb=UVb, cl=cl: e.matmul(vX, lhsT=am[:, 64:128], rhs=UVb[:, cl, :], start=False, stop=True),
                                     reads=[(am, None), (UVb, ("u", cl)), (UVb, "v")], writes=[(bk_, None)], pe_acc=True)
                                P.op("pe", lambda e, vH=vH, BKTb=BKTb, UVb=UVb, cl=cl: e.matmul(vH, lhsT=BKTb[:, cl, :], rhs=UVb[:, cl, :], start=True, stop=True),
                                     reads=[(BKTb, None), (UVb, ("u", cl)), (UVb, "v")], writes=[(bk_, None)], pe_acc=True)
                                k.cp("act", (Ob, Ob[:, cl, :], cl), (bk_, vX))
                                k.stt("dve", V(Hs), V(Hs), (gCall, gCall[:, hz, c:c + 1]), (bk_, vH), ALU.mult, ALU.add)
                                k.cp("act", V(Hb), V(Hs))

                        loads(0)
                        for ss in range(NSS):
                            if ss + 1 < NSS:
                                loads(ss + 1)
                            i = ss % 2
                            for jj in range(CBU):
                                for st in range(NL + 5):
                                    for d_ in CHN:
                                        unit_stage(d_, ss, jj, st)
                            for d_ in CHN:
                                c0 = clo(d_["z"], ss); h = d_["h"]; z = d_["z"]
                                k.dma((OD, OD[z][c0 * CH:(c0 + CBU) * CH, h * 64:(h + 1) * 64].rearrange("(c p) d -> p c d", p=CH), (z, h, ss)), V(d_["Ob"][i]))
                        P.barrier()
                with ExitStack() as fs:
                    oacc = P.sb("oaccr", [64, NCH, CH], F32, es=fs)
                    o1 = P.sb("o1r", [64, NCH, CH], F32, es=fs)
                    Vt = P.sb("Vtr", [64, NCH, CH], F32, es=fs)
                    gt = P.sb("gtr", [64, NCH, CH], F32, es=fs)
                    cen = P.sb("cen", [64, NCH, CH], F32, es=fs)
                    mu_ = P.sb("mu_", [64, NCH], F32, es=fs)
                    var = P.sb("var", [64, NCH], F32, es=fs)
                    for h in range(8):
                        k.dma(V(oacc), (OD, OD[0][:, h * 64:(h + 1) * 64].rearrange("(c p) d -> p c d", p=CH)))
                        k.dma(V(o1), (OD, OD[1][:, h * 64:(h + 1) * 64].rearrange("(c p) d -> p c d", p=CH)))
                        k.dma(V(Vt), (VTD, VTD[h].rearrange("p (c t) -> p c t", t=CH)))
                        k.dma(V(gt), (GT, GT[:, h * 64:(h + 1) * 64].rearrange("(c p) d -> p c d", p=CH)))
                        k.tt("pool", V(oacc), V(oacc), V(o1), ALU.add)
                        P.op("dve", lambda e: e.tensor_reduce(out=mu_.a(), in_=oacc.a(), axis=AX.X, op=ALU.add), reads=[oacc], writes=[mu_])
                        k.ts("dve", V(mu_), V(mu_), 1.0 / 64, ALU.mult)
                        b3 = lambda t: t.a().rearrange("p (c o) -> p c o", o=1).to_broadcast([64, NCH, CH])
                        k.tt("dve", V(oacc), V(oacc), (mu_, b3(mu_)), ALU.subtract)
                        k.tt("pool", V(cen), V(oacc), V(oacc), ALU.mult)
                        P.op("dve", lambda e: e.tensor_reduce(out=var.a(), in_=cen.a(), axis=AX.X, op=ALU.add), reads=[cen], writes=[var])
                        k.ts("dve", V(var), V(var), 1.0 / 64, ALU.mult, 64e-5, ALU.add)
                        k.act(V(var), V(var), AF.Sqrt)
                        k.recip(V(var), V(var))
                        k.tt("dve", V(oacc), V(oacc), (var, b3(var)), ALU.mult)
                        rb = lambda t: t[:, h * 64:(h + 1) * 64].rearrange("p (o d) -> p o d", o=1).to_broadcast([64, NCH, CH])
                        k.tt("pool", V(oacc), V(oacc), (lnw, rb(lnw)), ALU.mult)
                        k.tt("dve", V(oacc), V(oacc), (lnb, rb(lnb)), ALU.add)
                        k.tt("pool", V(cen), V(Vt), (bonall, bonall[:, h, :].rearrange("p (c o) -> p c o", o=1).to_broadcast([64, NCH, CH])), ALU.mult)
                        k.tt("dve", V(oacc), V(oacc), V(cen), ALU.add)
                        k.tt("dve", V(oacc), V(oacc), V(gt), ALU.mult)
                        k.dma((YM, YM[:, h * 64:(h + 1) * 64].rearrange("(c p) d -> p c d", p=CH), ("r", h)), V(oacc))
                    P.barrier()

        SEGS = ((0, LC), (LC, SEQ))

        def stage_conv(PF, PC, specs):
            with ExitStack() as ph:
                u = [P.sb(f"cu{i}", [128, L + 8], F32, es=ph) for i in range(2)]
                acc = [P.sb(f"ca{i}", [128, L], F32, es=ph) for i in range(2)]
                cw = P.sb("cw", [128, 20, 5], F32, es=ph)
                cb = P.sb("cb", [128, 20], F32, es=ph)
                for i in range(2):
                    k.memset("pool", (u[i], u[i][:, 0:2], "h0"), 0.0)
                    k.memset("pool", (u[i], u[i][:, LC + 2:LC + 6], "h1"), 0.0)
                    k.memset("pool", (u[i], u[i][:, L + 6:L + 8], "h2"), 0.0)
                bi = 0
                for (row0, nblk, wsrc, bsrc) in specs:
                    for b in range(nblk):
                        k.dma((cw, cw[:, bi, :], bi), (wsrc[0], wsrc[1][:, b * 128:(b + 1) * 128].rearrange("j p -> p j")), allow_slow_non_contiguous=True)
                        k.dma((cb, cb[:, bi:bi + 1], bi), (bsrc[0], bsrc[1][b * 128:(b + 1) * 128].rearrange("(p o) -> p o", o=1)), allow_slow_non_contiguous=True)
                        u_ = u[bi % 2]; a_ = acc[bi % 2]
                        r0 = row0 + b * 128
                        k.dma((u_, u_[:, 2:LC + 2], "c"), (PF, PF[r0:r0 + 128, 0:LC]))
                        k.dma((u_, u_[:, LC + 6:L + 6], "l"), (PF, PF[r0:r0 + 128, LC:L]))
                        for (s0, sl_) in SEGS:
                            off = 0 if s0 == 0 else 4
                            for j in range(5):
                                src = (u_, u_[:, s0 + off + j:s0 + off + j + sl_])
                                dst = (a_, a_[:, s0:s0 + sl_], s0)
                                if j == 0:
                                    k.ts("dve", dst, src, (cw, cw[:, bi, 0:1], bi), ALU.mult)
                                else:
                                    k.stt("dve", dst, src, (cw, cw[:, bi, j:j + 1], bi), dst, ALU.mult, ALU.add)
                            k.act((a_, a_[:, s0:s0 + sl_], s0), (a_, a_[:, s0:s0 + sl_], s0), AF.Silu, bias=(cb, cb[:, bi:bi + 1], bi))
                        k.dma((PC, PC[r0:r0 + 128, :], r0), V(a_))
                        bi += 1
                P.barrier()

        def mixer_mlstm(PC, PF, PT, YM, GSD):
            TB = [(t0_, min(512, L - t0_)) for t0_ in range(0, L, 512)]
            with ExitStack() as ph:
                GI = P.sb("GI", [8, L], F32, es=ph)
                GF = P.sb("GF", [8, L], F32, es=ph)
                t1 = P.sb("gt1", [8, L], F32, es=ph)
                t2 = P.sb("gt2", [8, L], F32, es=ph)
                t3 = P.sb("gt3", [8, L], F32, es=ph)
                rst = P.sb("grst", [8, L], F32, es=ph)
                ib = P.sb("ib", [8, 1], F32, es=ph)
                fb = P.sb("fb", [8, 1], F32, es=ph)
                k.dma(V(GI), (PF, PF[2560:2568, :]))
                k.dma(V(GF), (PF, PF[2568:2576, :]))
                k.dma(V(ib), V(I["mlstm_i_bias"], I["mlstm_i_bias"][0].rearrange("z (h o) -> (z h) o", o=1)), allow_slow_non_contiguous=True)
                k.dma(V(fb), V(I["mlstm_f_bias"], I["mlstm_f_bias"][0].rearrange("z (h o) -> (z h) o", o=1)), allow_slow_non_contiguous=True)
                k.memset("pool", V(rst), 1.0)
                k.memset("pool", (rst, rst.a().rearrange("p (c t) -> p c t", t=CH)[:, :, 0:1]), 0.0)
                k.ts("dve", V(GI), V(GI), (ib, ib[:, 0:1]), ALU.add)
                k.ts("dve", V(fb), V(fb), -1.0, ALU.mult)
                k.act(V(t1), V(GF), AF.Exp, bias=(fb, fb[:, 0:1]), scale=-1.0)
                k.act(V(t1), V(t1), AF.Ln, bias=1.0)
                k.ts("dve", V(t1), V(t1), -1.0, ALU.mult)
                k.scan(V(t2), V(rst), V(t1), 0.0, ALU.mult, ALU.add)
                k.tt("dve", V(t3), V(t1), V(t2), ALU.subtract)
                p3 = t2.a().rearrange("p (c t) -> p c t", t=CH)
                k.tt("dve", (t3, t3.a().rearrange("p (c t) -> p c t", t=CH)), (t3, t3.a().rearrange("p (c t) -> p c t", t=CH)),
                     (t2, p3[:, :, CH - 1:CH].to_broadcast([8, NCH, CH])), ALU.add)
                k.dma((GSD, GSD[0:4, :], 0), (t2, t2[0:4, :]))
                k.dma((GSD, GSD[4:8, :], 1), (t3, t3[4:8, :]))
                k.tt("dve", V(t2), V(GI), V(t2), ALU.subtract)
                k.tt("dve", V(t3), V(GI), V(t3), ALU.subtract)
                k.dma((GSD, GSD[8:12, :], 2), (t2, t2[0:4, :]))
                k.dma((GSD, GSD[12:16, :], 3), (t3, t3[4:8, :]))
                P.barrier()
            with ExitStack() as ph:
                masks = make_masks(ph)
                nwB = P.sb("mnwB", [64, 1024], F32, es=ph)
                k.dma(V(nwB), V(I["mlstm_norm_w"], I["mlstm_norm_w"][0].partition_broadcast(64)))
                oacc = P.sb("moacc", [64, NCH, 256], F32, es=ph)
                ssn = P.sb("mssn", [64, NCH], F32, es=ph)
                for h in range(4):
                    with ExitStack() as hs1:
                        vaug = P.sb("vaug", [64, NCH, 257], BF16, es=hs1)
                        t_q = P.sb("mt_q", [128, 512], F32, es=hs1)
                        t_k = P.sb("mt_k", [128, 512], F32, es=hs1)
                        t_e = P.sb("mt_e", [128, 512], F32, es=hs1)
                        t_s = P.sb("mt_s", [128, 512], F32, es=hs1)
                        cd = P.sb("mcd", [128, NCH], F32, es=hs1)
                        q_in = P.sb("mq_in", [128, L], BF16, es=hs1)
                        k_in = P.sb("mk_in", [128, L], BF16, es=hs1)
                        k_end = P.sb("mk_end", [128, L], BF16, es=hs1)
                        kET = P.sb("mkET", [64, NCH, 128], BF16, es=hs1)
                        S = P.sb("mS", [128, 257], F32, es=hs1)
                        Sb = P.sb("mSb", [128, 257], BF16, es=hs1)
                        Am = [P.sb(f"mAm{i}", [64, 64], BF16, es=hs1) for i in range(2)]
                        dn = P.sb("mdn", [64, 2], F32, es=hs1)
                        psA = P.ps("mpsA", [64, 64], F32, es=hs1)
                        pso = P.ps("mpso", [64, 257], F32, es=hs1)
                        pskv = P.ps("mpskv", [128, 257], F32, es=hs1)
                        pst = [P.ps(f"mpst{i}", [64, 4, 128], BF16, es=hs1) for i in range(2)]
                        k.dma((vaug, vaug[:, :, 0:256], "v"), (PT, PT[:, 1056 + h * 256:1056 + (h + 1) * 256].rearrange("(c p) d -> p c d", p=CH)), eng="pool")
                        k.memset("pool", (vaug, vaug[:, :, 256:257], "o"), 1.0)
                        k.memset("pool", V(oacc), 0.0)
                        for z in range(2):
                            end = CH - 1 if z == 0 else 0
                            for (t0_, tw) in TB:
                                tsl = slice(t0_, t0_ + tw)
                                ncb = tw // CH
                                c0 = t0_ // CH
                                k.dma((t_e, t_e[:, 0:tw]), (GSD, GSD[z * 4 + h, tsl].partition_broadcast(128)))
                                k.dma((t_s, t_s[:, 0:tw]), (GSD, GSD[8 + z * 4 + h, tsl].partition_broadcast(128)))
                                k.dma((t_q, t_q[:, 0:tw]), (PC, PC[1536 + h * 128:1536 + (h + 1) * 128, tsl]))
                                k.dma((t_k, t_k[:, 0:tw]), (PC, PC[2048 + h * 128:2048 + (h + 1) * 128, tsl]))
                                k.act((t_e, t_e[:, 0:tw]), (t_e, t_e[:, 0:tw]), AF.Exp)
                                k.act((t_s, t_s[:, 0:tw]), (t_s, t_s[:, 0:tw]), AF.Exp)
                                e3 = t_e[:, 0:tw].rearrange("p (c t) -> p c t", t=CH)
                                k.cp("pool", (cd, cd[:, c0:c0 + ncb]), (t_e, e3[:, :, end]))
                                k.tt("dve", (q_in, q_in[:, tsl]), (t_q, t_q[:, 0:tw]), (t_e, t_e[:, 0:tw]), ALU.mult)
                                k.stt("dve", (t_k, t_k[:, 0:tw]), (t_k, t_k[:, 0:tw]), 128 ** -0.5, (t_s, t_s[:, 0:tw]), ALU.mult, ALU.mult)
                                k.cp("pool", (k_in, k_in[:, tsl]), (t_k, t_k[:, 0:tw]))
                                k.tt("pool", (k_end, k_end[:, tsl].rearrange("p (c t) -> p c t", t=CH)),
                                     (t_k, t_k[:, 0:tw].rearrange("p (c t) -> p c t", t=CH)),
                                     (t_e, e3[:, :, end:end + 1].to_broadcast([128, ncb, CH])), ALU.mult)
                            for c4 in range(0, NCH, 4):
                                p_ = pst[(c4 // 4) % 2]
                                for j in range(4):
                                    c = c4 + j
                                    k.tr((p_, p_[:, j, :]), (k_end, k_end[:, c * CH:(c + 1) * CH]), V(identb))
                                k.cp("act", (kET, kET[:, c4:c4 + 4, :]), V(p_))
                            k.memset("pool", V(S), 0.0)
                            k.memset("pool", V(Sb), 0.0)
                            for step, c in enumerate(chunk_order(z)):
                                csl = slice(c * CH, (c + 1) * CH)
                                am = Am[step % 2]
                                k.mm(V(psA), (k_in, k_in[:, csl]), (q_in, q_in[:, csl]))
                                k.tt("dve", V(am), V(psA), V(masks[z]), ALU.mult)
                                k.mm(V(pso), V(am), (vaug, vaug[:, c, :]), start=True, stop=False)
                                k.mm(V(pso), (q_in, q_in[:, csl]), V(Sb), start=False, stop=True)
                                k.act((dn, dn[:, 0:1]), (pso, pso[:, 256:257]), AF.Abs)
                                k.ts("dve", (dn, dn[:, 0:1]), (dn, dn[:, 0:1]), 1.0, ALU.max)
                                k.recip((dn, dn[:, 1:2]), (dn, dn[:, 0:1]))
                                k.stt("dve", (oacc, oacc[:, c, :], c), (pso, pso[:, 0:256]), (dn, dn[:, 1:2]), (oacc, oacc[:, c, :], c), ALU.mult, ALU.add)
                                k.mm(V(pskv), (kET, kET[:, c, :]), (vaug, vaug[:, c, :]))
                                k.stt("dve", V(S), V(S), (cd, cd[:, c:c + 1]), V(pskv), ALU.mult, ALU.add)
                                k.cp("act", V(Sb), V(S))
                    P.barrier()
                    with ExitStack() as hs2:
                        gt = P.sb("mgt", [64, NCH, 256], F32, es=hs2)
                        k.tt("dve", V(gt), V(oacc), V(oacc), ALU.mult)
                        P.op("dve", lambda e: e.tensor_reduce(out=ssn.a(), in_=gt.a(), axis=AX.X, op=ALU.add), reads=[gt], writes=[ssn])
                        k.ts("dve", V(ssn), V(ssn), 1.0 / 256, ALU.mult, EPS, ALU.add)
                        k.act(V(ssn), V(ssn), AF.Sqrt)
                        k.recip(V(ssn), V(ssn))
                        k.tt("dve", V(oacc), V(oacc), (ssn, ssn.a().rearrange("p (c o) -> p c o", o=1).to_broadcast([64, NCH, 256])), ALU.mult)
                        k.tt("pool", V(oacc), V(oacc), (nwB, nwB[:, h * 256:(h + 1) * 256].rearrange("p (o d) -> p o d", o=1).to_broadcast([64, NCH, 256])), ALU.mult)
                        k.dma(V(gt), (PT, PT[:, 2080 + h * 256:2080 + (h + 1) * 256].rearrange("(c p) d -> p c d", p=CH)))
                        for q4 in range(4):
                            k.act((gt, gt[:, q4 * 17:(q4 + 1) * 17, :], q4), (gt, gt[:, q4 * 17:(q4 + 1) * 17, :], q4), AF.Sigmoid)
                        k.tt("dve", V(oacc), V(oacc), V(gt), ALU.mult)
                        k.dma((YM, YM[:, 1024 + h * 256:1024 + (h + 1) * 256].rearrange("(c p) d -> p c d", p=CH), ("m", h)), V(oacc))
                    P.barrier()

        def stage_xbt(PC, XBT):
            with ExitStack() as ph:
                ft = [P.sb(f"xft{i}", [128, 10, 128], F32, es=ph) for i in range(2)]
                ot = [P.sb(f"xot{i}", [128, 10, 128], F32, es=ph) for i in range(2)]
                pt = [P.ps(f"xpt{i}", [128, 4, 128], F32, es=ph) for i in range(3)]
                for tt in range(NT):
                    f_ = ft[tt % 2]; o_ = ot[tt % 2]
                    k.dma(V(f_), (PC, PC[0:1280, tt * 128:(tt + 1) * 128].rearrange("(b p) t -> p b t", p=128)))
                    for gi, (b0, nb_) in enumerate(((0, 4), (4, 4), (8, 2))):
                        p_ = pt[gi]
                        for j in range(nb_):
                            k.tr((p_, p_[:, j, :]), (f_, f_[:, b0 + j, :]), V(ident))
                        k.cp("act" if gi % 2 else "dve", (o_, o_[:, b0:b0 + nb_, :]), (p_, p_[:, 0:nb_, :]))
                    k.dma((XBT, XBT[tt * 128:(tt + 1) * 128, :], tt), (o_, o_.a().rearrange("p b c -> p (b c)")))
                P.barrier()

        def mixer_ssd(PC, PT, XBT, YS):
            with ExitStack() as ph:
                masks = make_masks(ph)
                ones64 = P.sb("sones64", [64, 64], F32, es=ph)
                k.memset("pool", V(ones64), 1.0)
                selend = []
                for z in range(2):
                    se = P.sb(f"selend{z}", [64, 128], F32, es=ph)
                    endp = CH - 1 if z == 0 else 0
                    P.op("pool", lambda e, se=se, endp=endp: e.affine_select(out=se.a(), in_=ones[0:64, :], pattern=[[0, 128]], compare_op=ALU.is_equal,
                                                                           fill=0.0, base=-endp, channel_multiplier=1), reads=[ones], writes=[se])
                    selend.append(se)
                dtT = P.sb("dtT", [64, NCH, 32], F32, es=ph)
                laT = P.sb("laT", [64, NCH, 32], F32, es=ph)
                dbB = P.sb("dbB", [64, 32], F32, es=ph)
                naB = P.sb("naB", [64, 32], F32, es=ph)
                dskB = P.sb("dskB", [64, 16], F32, es=ph)
                k.dma(V(dbB), V(I["ssd_dt_bias"], I["ssd_dt_bias"][0].rearrange("z h -> (z h)").partition_broadcast(64)))
                k.dma(V(naB), V(I["ssd_a_log"], I["ssd_a_log"][0].rearrange("z h -> (z h)").partition_broadcast(64)))
                k.dma(V(dskB), V(I["ssd_d"], I["ssd_d"][0].partition_broadcast(64)))
                k.act(V(naB), V(naB), AF.Exp)
                k.ts("dve", V(naB), V(naB), -1.0, ALU.mult)
                k.dma(V(dtT), (PT, PT[:, 1024:1056].rearrange("(c p) d -> p c d", p=CH)))
                k.tt("dve", V(dtT), V(dtT), (dbB, dbB.a().rearrange("p (o d) -> p o d", o=1).to_broadcast([64, NCH, 32])), ALU.add)
                k.act(V(dtT), V(dtT), AF.Exp)
                k.act(V(dtT), V(dtT), AF.Ln, bias=1.0)
                k.tt("dve", V(laT), V(dtT), (naB, naB.a().rearrange("p (o d) -> p o d", o=1).to_broadcast([64, NCH, 32])), ALU.mult)
                yacc = P.sb("yacc", [64, NCH, 256], F32, es=ph)
                for q4 in range(4):
                    g = q4 // 2
                    with ExitStack() as qs:
                        xq = P.sb("xq", [64, NCH, 256], BF16, es=qs)
                        Bf = P.sb("Bf", [128, L], BF16, es=qs)
                        Cf = P.sb("Cf", [128, L], BF16, es=qs)
                        BTt = P.sb("BTt", [64, NCH, 128], BF16, es=qs)
                        k.dma(V(xq), (XBT, XBT[:, q4 * 256:(q4 + 1) * 256].rearrange("(c p) d -> p c d", p=CH)), eng="pool")
                        k.dma(V(BTt), (XBT, XBT[:, 1024 + g * 128:1024 + (g + 1) * 128].rearrange("(c p) d -> p c d", p=CH)), eng="pool")
                        k.dma(V(Bf), (PC, PC[1024 + g * 128:1024 + (g + 1) * 128, :]), eng="pool")
                        k.dma(V(Cf), (PC, PC[1280 + g * 128:1280 + (g + 1) * 128, :]), eng="pool")
                        k.memset("pool", V(yacc), 0.0)
                        hs = [P.sb(f"hs{z}", [128, 256], F32, es=qs) for z in range(2)]
                        hsb = [P.sb(f"hsb{z}", [128, 256], BF16, es=qs) for z in range(2)]
                        CBm = [P.sb(f"CBm{z}", [64, 64], F32, es=qs) for z in range(2)]
                        acs = [P.sb(f"acs{z}", [64, 4], F32, es=qs) for z in range(2)]
                        dg = [P.sb(f"dg{z}", [64, 4, 64], F32, es=qs) for z in range(2)]
                        seg = [P.sb(f"seg{z}", [64, 4, 64], F32, es=qs) for z in range(2)]
                        AT = [P.sb(f"AT{z}", [64, 4, 64], BF16, es=qs) for z in range(2)]
                        xdt = [P.sb(f"xdt{z}", [64, 4, 64], BF16, es=qs) for z in range(2)]
                        xde = [P.sb(f"xde{z}", [64, 4, 64], BF16, es=qs) for z in range(2)]
                        din = [P.sb(f"din{z}", [64, 4], F32, es=qs) for z in range(2)]
                        dend = [P.sb(f"dend{z}", [64, 4], F32, es=qs) for z in range(2)]
                        dchB = [P.sb(f"dchB{z}", [128, 4], F32, es=qs) for z in range(2)]
                        ytmp = [P.sb(f"ytmp{z}", [64, 4, 64], F32, es=qs) for z in range(2)]
                        pCB = P.ps("pCB", [64, 64], F32, es=qs)
                        pac = P.ps("pac", [64, 4], F32, es=qs)
                        pbc = P.ps("pbc", [64, 4, 64], F32, es=qs)
                        py = P.ps("py", [64, 4, 64], F32, es=qs)
                        pyi = P.ps("pyi", [64, 4, 64], F32, es=qs)
                        phs = P.ps("phs", [128, 256], F32, es=qs)
                        pdc = P.ps("pdc", [128, 4], F32, es=qs)
                        for z in range(2):
                            k.memset("pool", V(hs[z]), 0.0)
                            k.memset("pool", V(hsb[z]), 0.0)
                        orders = [chunk_order(0), chunk_order(1)]
                        for step in range(NCH):
                            for z in range(2):
                                c = orders[z][step]
                                csl = slice(c * CH, (c + 1) * CH)
                                end = CH - 1 if z == 0 else 0
                                hsl = slice(z * 16 + q4 * 4, z * 16 + q4 * 4 + 4)
                                b4 = lambda t: t.a().rearrange("p (h o) -> p h o", o=1).to_broadcast([64, 4, 64])
                                k.mm(V(pCB), (Bf, Bf[:, csl]), (Cf, Cf[:, csl]))
                                k.tt("dve", V(CBm[z]), V(pCB), V(masks[z]), ALU.mult)
                                k.mm(V(pac), V(masks[z]), (laT, laT[:, c, hsl]))
                                k.cp("act", V(acs[z]), V(pac))
                                k.tt("pool", V(dg[z]), (ident, ident[0:64, 0:64].rearrange("p (o t) -> p o t", o=1).to_broadcast([64, 4, 64])),
                                     (acs[z], b4(acs[z])), ALU.mult)
                                k.mm(V(pbc), V(ones64), (dg[z], dg[z].a().rearrange("p h t -> p (h t)")))
                                k.tt("dve", V(seg[z]), V(pbc), (acs[z], b4(acs[z])), ALU.subtract)
                                k.tt("dve", V(dend[z]), (pbc, pbc[:, :, end]), V(acs[z]), ALU.subtract)
                                k.act(V(seg[z]), V(seg[z]), AF.Exp)
                                k.act(V(dend[z]), V(dend[z]), AF.Exp)
                                k.act(V(din[z]), V(acs[z]), AF.Exp)
                                k.stt("dve", V(AT[z]), V(seg[z]), 1.0, (CBm[z], CBm[z].a().rearrange("p (o t) -> p o t", o=1).to_broadcast([64, 4, 64])),
                                      ALU.min, ALU.mult)
                                k.tt("pool", V(xdt[z]), (xq, xq[:, c, :].rearrange("p (h d) -> p h d", d=64)),
                                     (dtT, dtT[:, c, hsl].rearrange("p (h o) -> p h o", o=1).to_broadcast([64, 4, 64])), ALU.mult)
                                for i in range(4):
                                    k.mm((py, py[:, i, :]), (AT[z], AT[z][:, i, :]), (xdt[z], xdt[z][:, i, :]))
                                k.mm(V(pyi), (Cf, Cf[:, csl]), V(hsb[z]))
                                ya = (yacc, yacc[:, c, :].rearrange("p (h d) -> p h d", d=64), c)
                                k.tt("dve", ya, V(py), ya, ALU.add)
                                k.tt("dve", V(ytmp[z]), V(pyi), (din[z], b4(din[z])), ALU.mult)
                                k.tt("pool", ya, ya, V(ytmp[z]), ALU.add)
                                k.tt("pool", V(xde[z]), V(xdt[z]), (dend[z], b4(dend[z])), ALU.mult)
                                k.mm(V(phs), (BTt, BTt[:, c, :]), (xde[z], xde[z].a().rearrange("p h d -> p (h d)")))
                                k.mm(V(pdc), V(selend[z]), V(acs[z]))
                                k.act(V(dchB[z]), V(pdc), AF.Exp)
                                h3 = (hs[z], hs[z].a().rearrange("p (h d) -> p h d", d=64))
                                k.tt("pool", h3, h3, (dchB[z], dchB[z].a().rearrange("p (h o) -> p h o", o=1).to_broadcast([128, 4, 64])), ALU.mult)
                                k.tt("dve", V(hs[z]), V(phs), V(hs[z]), ALU.add)
                                k.cp("act", V(hsb[z]), V(hs[z]))
                    P.barrier()
                    with ExitStack() as fs:
                        xf = P.sb("sxf", [64, 17, 256], F32, es=fs)
                        zf = P.sb("szf", [64, 17, 256], F32, es=fs)
                        for c17 in range(4):
                            cs_ = slice(c17 * 17, (c17 + 1) * 17)
                            rows = slice(c17 * 17 * CH, (c17 + 1) * 17 * CH)
                            k.dma(V(xf), (XBT, XBT[rows, q4 * 256:(q4 + 1) * 256].rearrange("(c p) d -> p c d", p=CH)))
                            k.dma(V(zf), (PT, PT[rows, q4 * 256:(q4 + 1) * 256].rearrange("(c p) d -> p c d", p=CH)))
                            dsk4 = dskB[:, q4 * 4:(q4 + 1) * 4].rearrange("p (a h o) -> p a h o", a=1, o=1).to_broadcast([64, 17, 4, 64])
                            k.tt("pool", (xf, xf.a().rearrange("p c (h d) -> p c h d", d=64)), (xf, xf.a().rearrange("p c (h d) -> p c h d", d=64)),
                                 (dskB, dsk4), ALU.mult)
                            k.tt("dve", V(xf), V(xf), (yacc, yacc[:, cs_, :], ("f", c17)), ALU.add)
                            k.act(V(zf), V(zf), AF.Silu)
                            k.tt("dve", V(xf), V(xf), V(zf), ALU.mult)
                            k.dma((YS, YS[rows, q4 * 256:(q4 + 1) * 256].rearrange("(c p) d -> p c d", p=CH), (q4, c17)), V(xf))
                    P.barrier()

        def stage_ssd_norm(YS, YM):
            with ExitStack() as ph:
                nwB = P.sb("snwB", [128, 1024], F32, es=ph)
                k.dma(V(nwB), V(I["ssd_norm_w"], I["ssd_norm_w"][0].partition_broadcast(128)))
                yt = [P.sb(f"syt{i}", [128, 1024], F32, es=ph) for i in range(2)]
                sq = P.sb("ssq", [128, 1024], F32, es=ph)
                st = [P.sb(f"sst{i}", [128, 2], F32, es=ph) for i in range(2)]
                for tt in range(NT):
                    y_ = yt[tt % 2]; s_ = st[tt % 2]
                    k.dma(V(y_), (YS, YS[tt * 128:(tt + 1) * 128, :]))
                    k.tt("pool", V(sq), V(y_), V(y_), ALU.mult)
                    P.op("dve", lambda e, s_=s_: e.tensor_reduce(out=s_.a(), in_=sq.a().rearrange("p (g d) -> p g d", g=2), axis=AX.X, op=ALU.add),
                         reads=[sq], writes=[s_])
                    k.ts("dve", V(s_), V(s_), 1.0 / 512, ALU.mult, EPS, ALU.add)
                    k.act(V(s_), V(s_), AF.Sqrt)
                    k.recip(V(s_), V(s_))
                    k.tt("dve", (y_, y_.a().rearrange("p (g d) -> p g d", g=2)), (y_, y_.a().rearrange("p (g d) -> p g d", g=2)),
                         (s_, s_.a().rearrange("p (g o) -> p g o", o=1).to_broadcast([128, 2, 512])), ALU.mult)
                    k.tt("pool", V(y_), V(y_), V(nwB), ALU.mult)
                    k.dma((YM, YM[tt * 128:(tt + 1) * 128, 0:1024], ("s", tt)), V(y_))
                P.barrier()

        def stage_final():
            with ExitStack() as ph:
                fnB = P.sb("fnB", [128, D], F32, es=ph)
                k.dma(V(fnB), V(I["final_norm_w"], I["final_norm_w"].a().partition_broadcast(128)))
                xt = [P.sb(f"fxt{i}", [128, D], F32, es=ph) for i in range(2)]
                junk = P.sb("fjunk", [128, D], F32, es=ph)
                st = [P.sb(f"fst{i}", [128, 4], F32, es=ph) for i in range(2)]
                for i in range(SEQ // 128):
                    x_ = xt[i % 2]; s_ = st[i % 2]
                    k.dma(V(x_), (XL, XL[LC + i * 128:LC + (i + 1) * 128, :], i))
                    k.memset("pool", (s_, s_[:, 0:1]), 0.0)
                    k.act(V(junk), V(x_), AF.Square, accum=(s_, s_[:, 0:1]))
                    k.ts("dve", (s_, s_[:, 1:2]), (s_, s_[:, 0:1]), 1.0 / D, ALU.mult, EPS, ALU.add)
                    k.act((s_, s_[:, 2:3]), (s_, s_[:, 1:2]), AF.Sqrt)
                    k.recip((s_, s_[:, 3:4]), (s_, s_[:, 2:3]))
                    k.ts("dve", V(x_), V(x_), (s_, s_[:, 3:4]), ALU.mult)
                    k.tt("pool", V(x_), V(x_), V(fnB), ALU.mult)
                    k.dma((OUT, OUT[i * 128:(i + 1) * 128, :], i), V(x_))
                P.barrier()

        def src0(i):
            if i < 2:
                return (I["ctx"], I["ctx"][i * 128:(i + 1) * 128, :])
            return (I["x"], I["x"][(i - 2) * 128:(i - 1) * 128, :])

        def src1(i):
            return (XL, XL[i * 128:(i + 1) * 128, :])

        def layer0():
            stage_mods(0)
            if "modF" in debug:
                dm = P.dram("d_modF", [128, 6 * KO * 2], F32, kind="ExternalOutput")
                k.dma(V(dm), (modF, modF.a().rearrange("p a b c -> p (a b c)")))
                dm2 = P.dram("d_modB", [128, 4 * D], F32, kind="ExternalOutput")
                k.dma(V(dm2), (modB, modB.a().rearrange("p a b c -> p (a b c)")))
            PF0 = scratch("PF0", [3456, L])
            PT0 = scratch("PT0", [L, 1024])
            YM0 = scratch("YM0", [L, 1024])
            with ExitStack() as lay:
                HT = P.sb("HT", [128, KO, L], BF16, es=lay)
                stage_norm(0, 1, HT, src0, perm=False)
                if "HT0" in debug:
                    dh = P.dram("d_HT0", [128, KO * L], BF16, kind="ExternalOutput")
                    k.dma(V(dh), (HT, HT.a().rearrange("p a b -> p (a b)")))
                if stop_after == "norm0":
                    return
                stage_proj(HT, (I["even_w_in"], I["even_w_in"][0]), 4480, [(0, 3456, PF0, 0)], [(3456, 4480, PT0, 0)])
            if stop_after == "proj0":
                return
            if "skip_hgrn" not in debug:
                mixer_hgrn(PF0, PT0, YM0)
            PR0 = scratch("PR0", [1920, L])
            GT0 = scratch("GT0", [L, 512])
            rwkv_shift(PF0, PR0)
            if "no_gate" not in debug:
                rwkv_gate(PR0, GT0)
            if stop_after == "shift0":
                return
            if "skip_rwkv" not in debug:
                if "rwkv_old" in debug:
                    mixer_rwkv(PR0, GT0, YM0)
                else:
                    mixer_rwkv2(PR0, GT0, YM0)
            if stop_after == "mix0":
                return
            stage_outproj(YM0, 1024, (I["even_w_out"], I["even_w_out"][0]), src0, False, list(range(NT)))
            if stop_after == "out0":
                return
            stage_moe(0, list(range(NT)))

        def layer1():
            stage_mods(1)
            PF1 = scratch("PF1", [2576, L])
            PT1 = scratch("PT1", [L, 3104])
            PC1 = scratch("PC1", [2560, L])
            YM1 = scratch("YM1", [L, 2048])
            GSD = scratch("GSD", [16, L])
            YS = scratch("YS", [L, 1024])
            with ExitStack() as lay:
                HT = P.sb("HT", [128, KO, L], BF16, es=lay)
                stage_norm(1, 1, HT, src1, perm=True)
                stage_proj(HT, (I["odd_w_in"], I["odd_w_in"][0]), 5680,
                           [(1024, 2560, PF1, 0), (2592, 3616, PF1, 1536), (5664, 5680, PF1, 2560)],
                           [(0, 1024, PT1, 0), (2560, 2592, PT1, 1024), (3616, 5664, PT1, 1056)])
            stage_conv(PF1, PC1, [(0, 12, (I["ssd_conv_w"], I["ssd_conv_w"][0]), (I["ssd_conv_b"], I["ssd_conv_b"][0])),
                                  (1536, 8, (I["mlstm_conv_w"], I["mlstm_conv_w"][0]), (I["mlstm_conv_b"], I["mlstm_conv_b"][0]))])
            if stop_after == "L1conv":
                return
            if "skip_mlstm" not in debug:
                mixer_mlstm(PC1, PF1, PT1, YM1, GSD)
            if stop_after == "L1mlstm":
                return
            XBT = scratch("XBT", [L, 1280])
            stage_xbt(PC1, XBT)
            mixer_ssd(PC1, PT1, XBT, YS)
            stage_ssd_norm(YS, YM1)
            if stop_after == "L1ssd":
                return
            stage_outproj(YM1, 2048, (I["odd_w_out"], I["odd_w_out"][0]), src1, True, list(range(2, NT)))
            if stop_after == "L1out":
                return
            stage_moe(1, list(range(2, NT)))
            stage_final()

        if "L1only" in debug:
            XLin = P.dram("XLin", [L, D], F32, kind="ExternalInput")
            for i4 in range(4):
                k.dma((XL, XL[i4 * 1088:(i4 + 1) * 1088, :], ("in", i4)), (XLin, XLin[i4 * 1088:(i4 + 1) * 1088, :]))
            P.barrier()
        else:
            layer0()
        if stop_after is None or stop_after.startswith("L1"):
            layer1()
        P.finish()
    return nc


_NC = None


def kernel(**inputs):
    global _NC
    if _NC is None:
        _NC = build()
    nc = _NC
    n = 8
    in_maps = []
    for b in range(n):
        m = {}
        for kk_, v in inputs.items():
            v = np.asarray(v)
            if kk_ == "x":
                m[kk_] = np.ascontiguousarray(v[b])
            elif kk_ == "c":
                m[kk_] = np.ascontiguousarray(v[b])
            elif kk_ == "ctx":
                m[kk_] = np.ascontiguousarray(v[b])
            else:
                m[kk_] = v
        in_maps.append(m)
    res = run_bass_kernel_spmd(nc, in_maps, core_ids=list(range(n)))
    return np.stack([r["out"] for r in res.results], axis=0)
```

```python
import numpy as np
import concourse.bass as bass
import concourse.mybir as mybir
from concourse.bass_utils import run_bass_kernel_spmd
from contextlib import ExitStack

F32 = mybir.dt.float32
BF16 = mybir.dt.bfloat16
AF = mybir.ActivationFunctionType
ALU = mybir.AluOpType
AX = mybir.AxisListType

ENGS = ("pe", "dve", "act", "pool", "sp")
D = 1024
KO = 8
LC = 256
SEQ = 4096
L = LC + SEQ
NT = L // 128
EPS = 1e-6
CH = 64
NCH = L // CH


class Cell:
    __slots__ = ("w", "r")

    def __init__(self):
        self.w = None
        self.r = []


class Buf:
    def __init__(self, name, h, is_dram=False, is_psum=False):
        self.name = name
        self.h = h
        self.is_dram = is_dram
        self.is_psum = is_psum
        self.base = Cell()
        self.parts = {}

    def cells(self, key):
        if key is None:
            return [self.base] + list(self.parts.values())
        c = self.parts.get(key)
        if c is None:
            c = Cell()
            c.w = self.base.w
            c.r = list(self.base.r)
            self.parts[key] = c
        return [c]

    def __getitem__(self, idx):
        return self.h[idx]

    def a(self):
        return self.h[:]


class Prog:
    def __init__(self, nc, es):
        self.nc = nc
        self.es = es
        self.q = {e: [] for e in ENGS}
        self.cnt = {e: 0 for e in ENGS}
        self.sem = {e: es.enter_context(nc.semaphore("s_" + e)) for e in ENGS}
        self.known = {e: {} for e in ENGS}
        self.dsem = {}
        self.dsem_by_id = {}
        self.phase_slots = {}
        self.ninst = 0
        self.uid = 0

    def sb(self, name, shape, dt=F32, es=None):
        self.uid += 1
        h = (es or self.es).enter_context(self.nc.sbuf_tensor(f"{name}_{self.uid}", list(shape), dt))
        return Buf(name, h)

    def ps(self, name, shape, dt=F32, es=None):
        self.uid += 1
        h = (es or self.es).enter_context(self.nc.psum_tensor(f"{name}_{self.uid}", list(shape), dt))
        return Buf(name, h, is_psum=True)

    def dram(self, name, shape, dt=F32, kind="Internal"):
        h = self.nc.dram_tensor(name, list(shape), dt, kind=kind)
        return Buf(name, h.ap(), is_dram=True)

    def dma_sem(self, name):
        if name not in self.dsem:
            self.dsem[name] = [self.es.enter_context(self.nc.semaphore("d_" + name)), 0]
            self.dsem_by_id[id(self.dsem[name][0])] = self.dsem[name]
        return self.dsem[name]

    def _norm(self, lst):
        return [(r, None) if isinstance(r, Buf) else ((r[0], None) if r[0].is_psum else r) for r in lst]

    def _deps(self, eng, reads, writes, pe_acc=False):
        need = {}

        def add(tok):
            if tok is None:
                return
            k = id(tok[0])
            if k not in need or need[k][1] < tok[1]:
                need[k] = tok

        for (b, key) in reads:
            for c in b.cells(key):
                add(c.w)
        for (b, key) in writes:
            for c in b.cells(key):
                if not (pe_acc and c.w is not None and c.w[2] == "pe"):
                    add(c.w)
                for t in c.r:
                    add(t)
        out = []
        kn = self.known[eng]
        for k, tok in need.items():
            val = tok[1]
            if k in self.dsem_by_id:
                val = self.dsem_by_id[k][1]
            if kn.get(k, 0) >= val:
                continue
            kn[k] = val
            out.append((tok[0], val))
        return out

    def _record(self, tok, reads, writes):
        for (b, key) in reads:
            for c in b.cells(key):
                c.r.append(tok)
                if len(c.r) > 16:
                    best = {}
                    for t in c.r:
                        kk = id(t[0])
                        if kk not in best or best[kk][1] < t[1]:
                            best[kk] = t
                    c.r = list(best.values())
        for (b, key) in writes:
            for c in b.cells(key):
                c.w = tok
                c.r = []

    def op(self, eng, fn, reads=(), writes=(), pe_acc=False):
        reads = self._norm(reads)
        writes = self._norm(writes)
        writes = writes + [r for r in reads if r[0].is_psum]
        waits = self._deps(eng, reads, writes, pe_acc)
        self.cnt[eng] += 1
        sem = self.sem[eng]
        tok = (sem, self.cnt[eng], eng)
        self.ninst += 1

        def run(e, waits=waits, fn=fn, sem=sem):
            for s, v in waits:
                e.wait_ge(s, v)
            fn(e).then_inc(sem, 1)

        self.q[eng].append(run)
        self._record(tok, reads, writes)
        return tok

    def dma(self, eng, out_ap, in_ap, reads=(), writes=(), semname=None, **kw):
        reads = self._norm(reads)
        writes = self._norm(writes)
        waits = self._deps(eng, reads, writes)
        if semname is None:
            sbs = [b for (b, _) in list(writes) + list(reads) if not b.is_dram]
            semname = sbs[0].name if sbs else "dram2dram"
        if semname not in self.phase_slots:
            self.phase_slots[semname] = len(self.phase_slots)
        ds = self.dma_sem(f"slot{self.phase_slots[semname]}")
        ds[1] += 16
        tok = (ds[0], ds[1], "dma")
        self.ninst += 1

        def run(e, waits=waits, sem=ds[0]):
            for s, v in waits:
                e.wait_ge(s, v)
            e.dma_start(out=out_ap, in_=in_ap, **kw).then_inc(sem, 16)

        self.q[eng].append(run)
        self._record(tok, reads, writes)
        return tok

    def barrier(self):
        self.phase_slots = {}
        toks = [(self.sem[f], self.cnt[f]) for f in ENGS if self.cnt[f] > 0]
        toks += [(v[0], v[1]) for v in self.dsem.values() if v[1] > 0]
        for e in ENGS:
            kn = self.known[e]
            ws = []
            for s, v in toks:
                if kn.get(id(s), 0) < v:
                    kn[id(s)] = v
                    ws.append((s, v))

            def run(en, ws=ws):
                for s, v in ws:
                    en.wait_ge(s, v)
            self.q[e].append(run)

    def finish(self):
        nc = self.nc
        self.barrier()
        with nc.Block() as block:
            @block.tensor
            def _(e):
                for f in self.q["pe"]:
                    f(e)

            @block.vector
            def _(e):
                for f in self.q["dve"]:
                    f(e)

            @block.scalar
            def _(e):
                for f in self.q["act"]:
                    f(e)

            @block.gpsimd
            def _(e):
                for f in self.q["pool"]:
                    f(e)

            @block.sync
            def _(e):
                for f in self.q["sp"]:
                    f(e)


def _rk(x):
    return (x[0], x[2] if len(x) > 2 else None)


class K:
    def __init__(self, P):
        self.P = P
        self.dq = 0

    def mm(self, out, lhsT, rhs, start=True, stop=True):
        return self.P.op("pe", lambda e: e.matmul(out[1], lhsT=lhsT[1], rhs=rhs[1], start=start, stop=stop),
                         reads=[_rk(lhsT), _rk(rhs)], writes=[_rk(out)], pe_acc=not start)

    def tr(self, out, in_, ident):
        return self.P.op("pe", lambda e: e.transpose(out[1], in_[1], ident[1]),
                         reads=[_rk(in_), _rk(ident)], writes=[_rk(out)])

    def act(self, out, in_, func, bias=None, scale=None, accum=None, eng="act"):
        reads = [_rk(in_)]
        kw = {}
        if bias is not None:
            if isinstance(bias, tuple):
                reads.append(_rk(bias)); kw["bias"] = bias[1]
            else:
                kw["bias"] = bias
        if scale is not None:
            if isinstance(scale, tuple):
                reads.append(_rk(scale)); kw["scale"] = scale[1]
            else:
                kw["scale"] = scale
        writes = [_rk(out)]
        if accum is not None:
            writes.append(_rk(accum)); kw["accum_out"] = accum[1]
        return self.P.op("act", lambda e: e.activation(out=out[1], in_=in_[1], func=func, **kw), reads=reads, writes=writes)

    def tt(self, eng, out, in0, in1, op):
        if eng == "pool":
            eng = "dve"
        return self.P.op(eng, lambda e: e.tensor_tensor(out=out[1], in0=in0[1], in1=in1[1], op=op),
                         reads=[_rk(in0), _rk(in1)], writes=[_rk(out)])

    def ts(self, eng, out, in0, s1, op0, s2=None, op1=None, accum=None):
        reads = [_rk(in0)]
        a1 = s1
        if isinstance(s1, tuple):
            reads.append(_rk(s1)); a1 = s1[1]
        a2 = s2
        if isinstance(s2, tuple):
            reads.append(_rk(s2)); a2 = s2[1]
        kw = {}
        if op1 is not None:
            kw["op1"] = op1
        writes = [_rk(out)]
        if accum is not None:
            writes.append(_rk(accum)); kw["accum_out"] = accum[1]
        if eng == "pool":
            eng = "dve"
        return self.P.op(eng, lambda e: e.tensor_scalar(out=out[1], in0=in0[1], scalar1=a1, scalar2=a2, op0=op0, **kw),
                         reads=reads, writes=writes)

    def stt(self, eng, out, in0, scalar, in1, op0, op1):
        reads = [_rk(in0), _rk(in1)]
        sc = scalar
        if isinstance(scalar, tuple):
            reads.append(_rk(scalar)); sc = scalar[1]
        return self.P.op(eng, lambda e: e.scalar_tensor_tensor(out=out[1], in0=in0[1], scalar=sc, in1=in1[1], op0=op0, op1=op1),
                         reads=reads, writes=[_rk(out)])

    def cp(self, eng, out, in_):
        if eng == "pool":
            eng = "act"
        if eng == "act":
            return self.P.op("act", lambda e: e.copy(out=out[1], in_=in_[1]), reads=[_rk(in_)], writes=[_rk(out)])
        return self.P.op(eng, lambda e: e.tensor_copy(out=out[1], in_=in_[1]), reads=[_rk(in_)], writes=[_rk(out)])

    def memset(self, eng, out, val):
        return self.P.op(eng, lambda e: e.memset(out[1], val), writes=[_rk(out)])

    def scan(self, out, d0, d1, init, op0, op1):
        return self.P.op("dve", lambda e: e.tensor_tensor_scan(out=out[1], data0=d0[1], data1=d1[1], initial=init, op0=op0, op1=op1),
                         reads=[_rk(d0), _rk(d1)], writes=[_rk(out)])

    def recip(self, out, in_):
        return self.P.op("dve", lambda e: e.reciprocal(out=out[1], in_=in_[1]), reads=[_rk(in_)], writes=[_rk(out)])

    def dma(self, out, in_, eng=None, **kw):
        if eng is None:
            eng = "sp" if in_[0].is_dram else "act"
        reads = [_rk(in_)]
        writes = [_rk(out)]
        return self.P.dma(eng, out[1], in_[1], reads=reads, writes=writes, **kw)


def V(buf, ap=None, key=None):
    return (buf, buf.a() if ap is None else ap, key)


def build(debug=(), stop_after=None):
    nc = bass.Bass("TRN2", target_bir_lowering=False)
    es = ExitStack()
    with es:
        P = Prog(nc, es)
        k = K(P)
        I = {}

        def inp(name, shape):
            I[name] = P.dram(name, shape, F32, kind="ExternalInput")

        inp("x", [SEQ, D]); inp("c", [D]); inp("ctx", [LC, D]); inp("c_ctx", [D])
        inp("ada_w", [2, D, 6 * D]); inp("ada_b", [2, 6 * D]); inp("norm1_w", [2, D]); inp("norm2_w", [2, D])
        inp("even_w_in", [1, D, 4480]); inp("even_w_out", [1, 1024, D])
        inp("rwkv_mu", [1, 1920]); inp("rwkv_w0", [1, 2, 512]); inp("rwkv_w2", [1, 2, 64, 512])
        inp("rwkv_a0", [1, 2, 512]); inp("rwkv_a2", [1, 2, 64, 512]); inp("rwkv_g2", [1, 128, 512])
        for n in ("rwkv_k_k", "rwkv_k_a", "rwkv_r_k", "rwkv_ln_w", "rwkv_ln_b"):
            inp(n, [1, 512])
        inp("hgrn_lower_bounds", [3, 512]); inp("hgrn_norm_w", [1, 512])
        inp("odd_w_in", [1, D, 5680]); inp("odd_w_out", [1, 2048, D])
        inp("ssd_conv_w", [1, 5, 1536]); inp("ssd_conv_b", [1, 1536]); inp("ssd_dt_bias", [1, 2, 16])
        inp("ssd_a_log", [1, 2, 16]); inp("ssd_d", [1, 16]); inp("ssd_norm_w", [1, 1024])
        inp("mlstm_conv_w", [1, 5, 1024]); inp("mlstm_conv_b", [1, 1024]); inp("mlstm_i_bias", [1, 2, 4])
        inp("mlstm_f_bias", [1, 2, 4]); inp("mlstm_norm_w", [1, 1024])
        inp("router_w", [2, D, 32]); inp("router_b", [2, 32])
        inp("exp_w_gate", [2, 32, D, 1024]); inp("exp_b_gate", [2, 32, 1024])
        inp("exp_w_up", [2, 32, D, 1024]); inp("exp_b_up", [2, 32, 1024])
        inp("exp_w_down", [2, 32, 1024, D]); inp("exp_b_down", [2, 32, D])
        inp("final_norm_w", [D])
        OUT = P.dram("out", [SEQ, D], F32, kind="ExternalOutput")

        def scratch(name, shape, dt=F32):
            return P.dram(name, shape, dt, kind="ExternalOutput" if name in debug else "Internal")

        XL = scratch("XL", [L, D])

        ident = P.sb("ident", [128, 128], F32)
        identb = P.sb("identb", [128, 128], BF16)
        ones = P.sb("ones", [128, 128], F32)
        k.memset("pool", V(ones), 1.0)
        P.op("pool", lambda e: e.affine_select(out=ident.a(), in_=ones.a(), pattern=[[-1, 128]], compare_op=ALU.is_equal,
                                               fill=0.0, base=0, channel_multiplier=1), reads=[ones], writes=[ident])
        k.cp("dve", V(identb), V(ident))

        modF = P.sb("modF", [128, 6, KO, 2], F32)
        modB = P.sb("modB", [128, 2, 2, D], F32)
        g1F = P.sb("g1F", [128, KO, 2], F32)
        g2F = P.sb("g2F", [128, KO, 2], F32)
        nw1 = P.sb("nw1", [128, 2, KO], F32)
        nw2 = P.sb("nw2", [128, 2, KO], F32)
        k.dma(V(nw1), V(I["norm1_w"], I["norm1_w"].a().rearrange("l (ko p) -> p l ko", p=128)), allow_slow_non_contiguous=True)
        k.dma(V(nw2), V(I["norm2_w"], I["norm2_w"].a().rearrange("l (ko p) -> p l ko", p=128)), allow_slow_non_contiguous=True)

        def stage_mods(l):
            with ExitStack() as ph:
                c0 = P.sb("c0", [128, KO, 2], F32, es=ph)
                s = P.sb("s", [128, KO, 2], F32, es=ph)
                sB = P.sb("sB", [128, KO, 2, 128], F32, es=ph)
                abF = P.sb("abF", [128, 48], F32, es=ph)
                abB = P.sb("abB", [128, 2, D], F32, es=ph)
                awm = [P.sb(f"awm{i}", [128, KO, D], F32, es=ph) for i in range(2)]
                psF = P.ps("psF", [128, KO, 2], F32, es=ph)
                psB = [P.ps(f"psB{i}", [128, 512], F32, es=ph) for i in range(2)]
                tmp = P.sb("tmpm", [128, KO, 2], F32, es=ph)
                k.dma((c0, c0[:, :, 0]), V(I["c"], I["c"].a().rearrange("(ko p) -> p ko", p=128)), allow_slow_non_contiguous=True)
                k.dma((c0, c0[:, :, 1]), V(I["c_ctx"], I["c_ctx"].a().rearrange("(ko p) -> p ko", p=128)), allow_slow_non_contiguous=True)
                k.act(V(s), V(c0), AF.Silu)
                k.cp("dve", V(sB), (s, s.a().rearrange("p k (j o) -> p k j o", o=1).to_broadcast([128, KO, 2, 128])))
                k.dma(V(abF), V(I["ada_b"], I["ada_b"][l].rearrange("(nb p) -> p nb", p=128)), allow_slow_non_contiguous=True)
                k.dma((abB, abB[:, 0, :]), V(I["ada_b"], I["ada_b"][l, 2 * D:3 * D].partition_broadcast(128)))
                k.dma((abB, abB[:, 1, :]), V(I["ada_b"], I["ada_b"][l, 5 * D:6 * D].partition_broadcast(128)))
                for m in range(6):
                    aw = awm[m % 2]
                    k.dma(V(aw), V(I["ada_w"], I["ada_w"][l, :, m * D:(m + 1) * D].rearrange("(ko p) n -> p ko n", p=128)))
                    if m in (0, 1, 3, 4):
                        for nb in range(KO):
                            for ko in range(KO):
                                k.mm((psF, psF[:, nb, :]), (aw, aw[:, ko, nb * 128:(nb + 1) * 128]), (s, s[:, ko, :]),
                                     start=(ko == 0), stop=(ko == KO - 1))
                        k.tt("dve", (modF, modF[:, m, :, :]), V(psF),
                             (abF, abF[:, m * 8:(m + 1) * 8].rearrange("p (k o) -> p k o", o=1).to_broadcast([128, KO, 2])), ALU.add)
                    else:
                        mi = 0 if m == 2 else 1
                        for j in range(2):
                            for nblk in range(2):
                                pb = psB[(j * 2 + nblk) % 2]
                                for ko in range(KO):
                                    k.mm(V(pb), (sB, sB[:, ko, j, :]), (aw, aw[:, ko, nblk * 512:(nblk + 1) * 512]),
                                         start=(ko == 0), stop=(ko == KO - 1))
                                k.tt("dve", (modB, modB[:, mi, j, nblk * 512:(nblk + 1) * 512]), V(pb),
                                     (abB, abB[:, mi, nblk * 512:(nblk + 1) * 512]), ALU.add)
                for (gF, nw, mi) in ((g1F, nw1, 1), (g2F, nw2, 4)):
                    k.ts("dve", V(tmp), (modF, modF[:, mi, :, :]), 1.0, ALU.add)
                    k.tt("dve", V(gF), V(tmp), (nw, nw[:, l, :].rearrange("p (k o) -> p k o", o=1).to_broadcast([128, KO, 2])), ALU.mult)
                P.barrier()

        def stage_norm(l, which, HT, src_tiles, perm):
            gF = g1F if which == 1 else g2F
            mi = 0 if which == 1 else 3
            with ExitStack() as ph:
                xt = [P.sb(f"xt{i}", [128, D], F32, es=ph) for i in range(3)]
                xn = [P.sb(f"xn{i}", [128, D], F32, es=ph) for i in range(2)]
                junk = P.sb("junk", [128, D], F32, es=ph)
                st = [P.sb(f"st{i}", [128, 4], F32, es=ph) for i in range(2)]
                tmp = [P.sb(f"tmpn{i}", [128, KO, 128], F32, es=ph) for i in range(2)]
                pT = [P.ps(f"pT{i}", [128, KO, 128], F32, es=ph) for i in range(2)]
                for i in range(NT):
                    j = 1 if i < 2 else 0
                    x_ = xt[i % 3]; n_ = xn[i % 2]; s_ = st[i % 2]; t_ = tmp[i % 2]; p_ = pT[i % 2]
                    sb_, sap = src_tiles(i)
                    k.dma(V(x_), (sb_, sap))
                    k.memset("pool", (s_, s_[:, 0:1]), 0.0)
                    k.act(V(junk), V(x_), AF.Square, accum=(s_, s_[:, 0:1]))
                    k.ts("dve", (s_, s_[:, 1:2]), (s_, s_[:, 0:1]), 1.0 / D, ALU.mult, EPS, ALU.add)
                    k.act((s_, s_[:, 2:3]), (s_, s_[:, 1:2]), AF.Sqrt)
                    k.recip((s_, s_[:, 3:4]), (s_, s_[:, 2:3]))
                    k.ts("dve", V(n_), V(x_), (s_, s_[:, 3:4]), ALU.mult)
                    for ko in range(KO):
                        k.tr((p_, p_[:, ko, :]), (n_, n_[:, ko * 128:(ko + 1) * 128]), V(ident))
                    k.tt("dve", V(t_), V(p_), (gF, gF[:, :, j:j + 1].to_broadcast([128, KO, 128])), ALU.mult)
                    if perm and i >= 2:
                        r0 = 2 * (i - 2)
                        dst = HT[:, :, LC:].rearrange("p k (c r) -> p k c r", r=64)[:, :, :, r0:r0 + 2]
                        src0 = t_.a().rearrange("p k (r c) -> p k c r", r=2)
                        src1 = modF[:, mi, :, j:j + 1].rearrange("p k (a b) -> p k a b", b=1).to_broadcast([128, KO, 64, 2])
                        k.tt("pool", (HT, dst, i), (t_, src0), (modF, src1), ALU.add)
                    else:
                        k.tt("pool", (HT, HT[:, :, i * 128:(i + 1) * 128], i), V(t_),
                             (modF, modF[:, mi, :, j:j + 1].to_broadcast([128, KO, 128])), ALU.add)
                P.barrier()

        def stage_proj(HT, W, ncols, f_list, t_list):
            with ExitStack() as ph:
                wst = [P.sb(f"wst{i}", [128, KO, 512], BF16, es=ph) for i in range(2)]
                stg = [P.sb(f"stg{i}", [128, 512], F32, es=ph) for i in range(4)]
                pp = [P.ps(f"pp{i}", [128, 512], F32, es=ph) for i in range(4)]
                cnt = 0
                npan = (ncols + 511) // 512
                for pn in range(npan):
                    c0 = pn * 512
                    cw = min(512, ncols - c0)
                    w_ = wst[pn % 2]
                    k.dma((w_, w_[:, :, 0:cw]), (W[0], W[1][:, c0:c0 + cw].rearrange("(ko p) n -> p ko n", p=128)), eng="pool")
                    for (f0, f1, PF, roff) in f_list:
                        fa, fb = max(c0, f0), min(c0 + cw, f1)
                        if fa >= fb:
                            continue
                        for n0 in range(fa, fb, 128):
                            nw_ = min(128, fb - n0)
                            for tb in range(0, L, 512):
                                tw = min(512, L - tb)
                                p_ = pp[cnt % 4]; s_ = stg[cnt % 4]; cnt += 1
                                for ko in range(KO):
                                    k.mm((p_, p_[0:nw_, 0:tw]), (w_, w_[:, ko, n0 - c0:n0 - c0 + nw_]), (HT, HT[:, ko, tb:tb + tw]),
                                         start=(ko == 0), stop=(ko == KO - 1))
                                k.cp("act" if cnt % 2 else "dve", (s_, s_[0:nw_, 0:tw]), (p_, p_[0:nw_, 0:tw]))
                                r0 = n0 - f0 + roff
                                k.dma((PF, PF[r0:r0 + nw_, tb:tb + tw], ("f", n0)), (s_, s_[0:nw_, 0:tw]))
                    for (t0, t1, PT, coff) in t_list:
                        ta, tb_ = max(c0, t0), min(c0 + cw, t1)
                        if ta >= tb_:
                            continue
                        tw = tb_ - ta
                        for tt in range(NT):
                            p_ = pp[cnt % 4]; s_ = stg[cnt % 4]; cnt += 1
                            for ko in range(KO):
                                k.mm((p_, p_[:, 0:tw]), (HT, HT[:, ko, tt * 128:(tt + 1) * 128]), (w_, w_[:, ko, ta - c0:ta - c0 + tw]),
                                     start=(ko == 0), stop=(ko == KO - 1))
                            k.cp("act" if cnt % 2 else "dve", (s_, s_[:, 0:tw]), (p_, p_[:, 0:tw]))
                            cc = ta - t0 + coff
                            k.dma((PT, PT[tt * 128:(tt + 1) * 128, cc:cc + tw], ("t", tt, ta)), (s_, s_[:, 0:tw]))
                P.barrier()

        def chunk_order(z):
            if z == 0:
                return list(range(NCH))
            return [3, 2, 1, 0] + list(range(NCH - 1, 3, -1))

        def make_masks(ph):
            ms = []
            for z in range(2):
                m = P.sb(f"mask{z}", [64, 64], F32, es=ph)
                if z == 0:
                    P.op("pool", lambda e, m=m: e.affine_select(out=m.a(), in_=ones[0:64, 0:64], pattern=[[1, 64]], compare_op=ALU.is_ge,
                                                           fill=0.0, base=0, channel_multiplier=-1), reads=[ones], writes=[m])
                else:
                    P.op("pool", lambda e, m=m: e.affine_select(out=m.a(), in_=ones[0:64, 0:64], pattern=[[-1, 64]], compare_op=ALU.is_ge,
                                                           fill=0.0, base=0, channel_multiplier=1), reads=[ones], writes=[m])
                ms.append(m)
            return ms

        def mixer_hgrn(PF, PT, YM):
            BW = 1088
            NB = L // BW
            CB = BW // CH
            with ExitStack() as ph:
                masks = make_masks(ph)
                lbr = P.sb("lbr", [128, 3, 4], F32, es=ph)
                lbe = P.sb("lbe", [128, 3, 4], F32, es=ph)
                lbs = P.sb("lbs", [128, 4], F32, es=ph)
                lb = P.sb("lb", [128, 4], F32, es=ph)
                oml = P.sb("oml", [128, 4], F32, es=ph)
                noml = P.sb("noml", [128, 4], F32, es=ph)
                k.dma(V(lbr), V(I["hgrn_lower_bounds"], I["hgrn_lower_bounds"].a().rearrange("j (h p) -> p j h", p=128)),
                      allow_slow_non_contiguous=True)
                k.act(V(lbe), V(lbr), AF.Exp)
                k.tt("dve", V(lbs), (lbe, lbe[:, 0, :]), (lbe, lbe[:, 1, :]), ALU.add)
                k.tt("dve", V(lbs), V(lbs), (lbe, lbe[:, 2, :]), ALU.add)
                k.recip(V(lbs), V(lbs))
                k.tt("dve", V(lb), (lbe, lbe[:, 0, :]), V(lbs), ALU.mult)
                k.ts("dve", V(oml), V(lb), -1.0, ALU.mult, 1.0, ALU.add)
                k.ts("dve", V(noml), V(oml), -1.0, ALU.mult)
                rst = P.sb("rst", [128, BW], F32, es=ph)
                k.memset("pool", V(rst), 1.0)
                k.memset("pool", (rst, rst.a().rearrange("p (c t) -> p c t", t=CH)[:, :, 0:1]), 0.0)
                nwB = P.sb("nwB", [64, 512], F32, es=ph)
                k.dma(V(nwB), V(I["hgrn_norm_w"], I["hgrn_norm_w"][0].partition_broadcast(64)))
                oacc = P.sb("oacc", [64, NCH, 128], F32, es=ph)
                ssn = P.sb("ssn", [64, NCH], F32, es=ph)
                for h in range(4):
                    with ExitStack() as hs1:
                        t_f = P.sb("t_f", [128, BW], F32, es=hs1)
                        t_s = P.sb("t_s", [128, BW], F32, es=hs1)
                        t_lf = P.sb("t_lf", [128, BW], F32, es=hs1)
                        t_k = P.sb("t_k", [128, BW], F32, es=hs1)
                        t_b = P.sb("t_b", [128, BW], F32, es=hs1)
                        t_e = P.sb("t_e", [128, BW], F32, es=hs1)
                        t_q = P.sb("t_q", [128, BW], F32, es=hs1)
                        cd = [P.sb(f"cd{z}", [128, NCH], F32, es=hs1) for z in range(2)]
                        q_in = [P.sb(f"q_in{z}", [128, L], BF16, es=hs1) for z in range(2)]
                        k_in = [P.sb(f"k_in{z}", [128, L], BF16, es=hs1) for z in range(2)]
                        k_end1 = P.sb("k_end", [128, L], BF16, es=hs1)
                        k_end = [k_end1, k_end1]
                        kET = [P.sb(f"kET{z}", [64, NCH, 128], BF16, es=hs1) for z in range(2)]
                        vb = P.sb("vb", [64, NCH, 128], BF16, es=hs1)
                        S = [P.sb(f"S{z}", [128, 128], F32, es=hs1) for z in range(2)]
                        Sb = [P.sb(f"Sb{z}", [128, 128], BF16, es=hs1) for z in range(2)]
                        Am = [P.sb(f"Am{i}", [64, 64], BF16, es=hs1) for i in range(4)]
                        kvS = [[P.sb(f"kvS{z}_{i}", [128, 128], F32, es=hs1) for i in range(2)] for z in range(2)]
                        psA = [P.ps(f"psA{i}", [64, 64], F32, es=hs1) for i in range(2)]
                        pso = [P.ps(f"pso{i}", [64, 128], F32, es=hs1) for i in range(2)]
                        pskv = [P.ps(f"pskv{i}", [128, 128], F32, es=hs1) for i in range(2)]
                        pst = [P.ps(f"pst{i}", [64, 4, 128], BF16, es=hs1) for i in range(2)]
                        k.dma(V(vb), (PT, PT[:, h * 128:(h + 1) * 128].rearrange("(c p) d -> p c d", p=CH)), eng="pool")
                        for z in range(2):
                            end = CH - 1 if z == 0 else 0
                            for blk in range(NB):
                                tsl = slice(blk * BW, (blk + 1) * BW)
                                frow = 2432 + z * 512 + h * 128
                                k.dma(V(t_f), (PF, PF[frow:frow + 128, tsl]))
                                k.dma(V(t_q), (PF, PF[1920 + h * 128:1920 + (h + 1) * 128, tsl]))
                                k.act(V(t_s), V(t_f), AF.Sigmoid)
                                k.act(V(t_lf), V(t_s), AF.Ln, bias=(lb, lb[:, h:h + 1]), scale=(oml, oml[:, h:h + 1]))
                                k.ts("dve", V(t_k), V(t_s), (noml, noml[:, h:h + 1]), ALU.mult, (oml, oml[:, h:h + 1]), ALU.add)
                                k.scan(V(t_b), V(rst), V(t_lf), 0.0, ALU.mult, ALU.add)
                                if z == 1:
                                    k.tt("dve", V(t_f), V(t_lf), V(t_b), ALU.subtract)
                                    b3 = t_b.a().rearrange("p (c t) -> p c t", t=CH)
                                    k.tt("dve", (t_lf, t_lf.a().rearrange("p (c t) -> p c t", t=CH)),
                                         (t_f, t_f.a().rearrange("p (c t) -> p c t", t=CH)),
                                         (t_b, b3[:, :, CH - 1:CH].to_broadcast([128, CB, CH])), ALU.add)
                                    bb = t_lf
                                else:
                                    bb = t_b
                                k.act(V(t_e), V(bb), AF.Exp)
                                k.act(V(t_s), V(bb), AF.Exp, scale=-1.0)
                                e3 = t_e.a().rearrange("p (c t) -> p c t", t=CH)
                                k.cp("pool", (cd[z], cd[z][:, blk * CB:(blk + 1) * CB]), (t_e, e3[:, :, end]))
                                k.act(V(t_f), V(t_q), AF.Silu)
                                k.stt("dve", (q_in[z], q_in[z][:, tsl]), V(t_f), 128 ** -0.5, V(t_e), ALU.mult, ALU.mult)
                                k.tt("dve", V(t_k), V(t_k), V(t_s), ALU.mult)
                                k.cp("pool", (k_in[z], k_in[z][:, tsl]), V(t_k))
                                k.tt("pool", (k_end[z], k_end[z][:, tsl].rearrange("p (c t) -> p c t", t=CH)),
                                     (t_k, t_k.a().rearrange("p (c t) -> p c t", t=CH)),
                                     (t_e, e3[:, :, end:end + 1].to_broadcast([128, CB, CH])), ALU.mult)
                            for c4 in range(0, NCH, 4):
                                p_ = pst[(c4 // 4) % 2]
                                for j in range(4):
                                    c = c4 + j
                                    k.tr((p_, p_[:, j, :]), (k_end[z], k_end[z][:, c * CH:(c + 1) * CH]), V(identb))
                                k.cp("act", (kET[z], kET[z][:, c4:c4 + 4, :]), V(p_))
                        k.memset("pool", V(oacc), 0.0)
                        for z in range(2):
                            k.memset("pool", V(S[z]), 0.0)
                            k.memset("pool", V(Sb[z]), 0.0)
                        orders = [chunk_order(0), chunk_order(1)]

                        def hg_A(step, z):
                            c = orders[z][step]
                            csl = slice(c * CH, (c + 1) * CH)
                            am = Am[(step % 2) * 2 + z]
                            k.mm(V(psA[z]), (k_in[z], k_in[z][:, csl]), (q_in[z], q_in[z][:, csl]))
                            k.tt("dve", V(am), V(psA[z]), V(masks[z]), ALU.mult)
                            k.mm(V(pskv[z]), (kET[z], kET[z][:, c, :]), (vb, vb[:, c, :]))
                            k.cp("act", V(kvS[z][step % 2]), V(pskv[z]))

                        def hg_B(step, z):
                            c = orders[z][step]
                            csl = slice(c * CH, (c + 1) * CH)
                            am = Am[(step % 2) * 2 + z]
                            po = pso[z]
                            k.mm(V(po), V(am), (vb, vb[:, c, :]), start=True, stop=False)
                            k.mm(V(po), (q_in[z], q_in[z][:, csl]), V(Sb[z]), start=False, stop=True)
                            k.tt("dve", (oacc, oacc[:, c, :], c), (oacc, oacc[:, c, :], c), V(po), ALU.add)
                            k.stt("dve", V(S[z]), V(S[z]), (cd[z], cd[z][:, c:c + 1]), V(kvS[z][step % 2]), ALU.mult, ALU.add)
                            k.cp("act", V(Sb[z]), V(S[z]))

                        for z in range(2):
                            hg_A(0, z)
                        for step in range(NCH):
                            if step + 1 < NCH:
                                for z in range(2):
                                    hg_A(step + 1, z)
                            for z in range(2):
                                hg_B(step, z)
                    P.barrier()
                    with ExitStack() as hs2:
                        gt = P.sb("gt", [64, NCH, 128], F32, es=hs2)
                        k.tt("dve", V(gt), V(oacc), V(oacc), ALU.mult)
                        P.op("dve", lambda e: e.tensor_reduce(out=ssn.a(), in_=gt.a(), axis=AX.X, op=ALU.add), reads=[gt], writes=[ssn])
                        k.ts("dve", V(ssn), V(ssn), 1.0 / 128, ALU.mult, EPS, ALU.add)
                        k.act(V(ssn), V(ssn), AF.Sqrt)
                        k.recip(V(ssn), V(ssn))
                        k.tt("dve", V(oacc), V(oacc), (ssn, ssn.a().rearrange("p (c o) -> p c o", o=1).to_broadcast([64, NCH, 128])), ALU.mult)
                        k.tt("pool", V(oacc), V(oacc), (nwB, nwB[:, h * 128:(h + 1) * 128].rearrange("p (o d) -> p o d", o=1).to_broadcast([64, NCH, 128])), ALU.mult)
                        k.dma(V(gt), (PT, PT[:, 512 + h * 128:512 + (h + 1) * 128].rearrange("(c p) d -> p c d", p=CH)))
                        k.act(V(gt), V(gt), AF.Silu)
                        k.tt("dve", V(oacc), V(oacc), V(gt), ALU.mult)
                        k.dma((YM, YM[:, 512 + h * 128:512 + (h + 1) * 128].rearrange("(c p) d -> p c d", p=CH), ("h", h)), V(oacc))
                    P.barrier()
                P.barrier()

        def rwkv_shift(PF, PR):
            with ExitStack() as ph:
                mu = P.sb("mu", [128, 15], F32, es=ph)
                omu = P.sb("omu", [128, 15], F32, es=ph)
                hmu = P.sb("hmu", [128, 15], F32, es=ph)
                k.dma(V(mu), V(I["rwkv_mu"], I["rwkv_mu"][0].rearrange("(b p) -> p b", p=128)), allow_slow_non_contiguous=True)
                k.ts("dve", V(omu), V(mu), -1.0, ALU.mult, 1.0, ALU.add)
                k.ts("dve", V(hmu), V(mu), 0.5, ALU.mult)
                pt = [P.sb(f"shp{i}", [128, L + 2], F32, es=ph) for i in range(2)]
                sm = [P.sb(f"shs{i}", [128, L], F32, es=ph) for i in range(2)]
                for i in range(2):
                    k.memset("pool", (pt[i], pt[i][:, 0:1], "h0"), 0.0)
                    k.memset("pool", (pt[i], pt[i][:, L + 1:L + 2], "h1"), 0.0)
                for b in range(15):
                    p_ = pt[b % 2]; s_ = sm[b % 2]
                    k.dma((p_, p_[:, 1:L + 1], "m"), (PF, PF[b * 128:(b + 1) * 128, :]))
                    k.tt("dve", (s_, s_.a(), "a"), V(p_), (p_, p_[:, 2:L + 2]), ALU.add) if False else None
                    P.op("dve", lambda e, p_=p_, s_=s_: e.tensor_tensor(out=s_.a(), in0=p_[:, 0:L], in1=p_[:, 2:L + 2], op=ALU.add),
                         reads=[p_], writes=[s_])
                    k.cp("dve", (s_, s_[:, 255:256]), (p_, p_[:, 255:256]))
                    k.cp("dve", (s_, s_[:, 256:257]), (p_, p_[:, 258:259]))
                    k.ts("pool", V(s_), V(s_), (hmu, hmu[:, b:b + 1]), ALU.mult)
                    k.stt("dve", V(s_), (p_, p_[:, 1:L + 1]), (omu, omu[:, b:b + 1]), V(s_), ALU.mult, ALU.add)
                    k.dma((PR, PR[b * 128:(b + 1) * 128, :], b), V(s_))
                P.barrier()

        def rwkv_gate(PR, GT):
            with ExitStack() as ph:
                gd = P.sb("gd", [128, L], F32, es=ph)
                gs = P.sb("gs", [128, L], BF16, es=ph)
                g2f = P.sb("g2f", [128, 512], F32, es=ph)
                g2b = P.sb("g2b", [128, 512], BF16, es=ph)
                pg = [P.ps(f"pg{i}", [128, 512], F32, es=ph) for i in range(2)]
                sg = [P.sb(f"sg{i}", [128, 512], F32, es=ph) for i in range(2)]
                k.dma(V(gd), (PR, PR[1792:1920, :]))
                k.dma(V(g2f), V(I["rwkv_g2"], I["rwkv_g2"][0]))
                k.cp("dve", V(g2b), V(g2f))
                for q4 in range(4):
                    k.act((gs, gs[:, q4 * 1088:(q4 + 1) * 1088], q4), (gd, gd[:, q4 * 1088:(q4 + 1) * 1088]), AF.Sigmoid)
                for tt in range(NT if "gate_nomm" not in debug else 0):
                    k.mm(V(pg[tt % 2]), (gs, gs[:, tt * 128:(tt + 1) * 128]), V(g2b))
                    k.cp("dve" if tt % 2 else "act", V(sg[tt % 2]), V(pg[tt % 2]))
                    k.dma((GT, GT[tt * 128:(tt + 1) * 128, :], tt), V(sg[tt % 2]))
                P.barrier()

        def mixer_rwkv(PR, GT, YM):
            BW = 256
            NB = L // BW
            CB = BW // CH
            NL = 5
            with ExitStack() as ph:
                w2all = P.sb("w2all", [128, 512], F32, es=ph)
                a2all = P.sb("a2all", [128, 512], F32, es=ph)
                k.dma(V(w2all), V(I["rwkv_w2"], I["rwkv_w2"][0].rearrange("z r c -> (z r) c")))
                k.dma(V(a2all), V(I["rwkv_a2"], I["rwkv_a2"][0].rearrange("z r c -> (z r) c")))
                def hv(name, src):
                    t = P.sb(name, [64, 8], F32, es=ph)
                    k.dma(V(t), (src[0], src[1].rearrange("(h n) -> n h", n=64)), allow_slow_non_contiguous=True)
                    return t
                w0 = [hv(f"w0_{z}", (I["rwkv_w0"], I["rwkv_w0"][0, z])) for z in range(2)]
                a0 = [hv(f"a0_{z}", (I["rwkv_a0"], I["rwkv_a0"][0, z])) for z in range(2)]
                kkg = hv("kkg", (I["rwkv_k_k"], I["rwkv_k_k"][0]))
                kag = hv("kag", (I["rwkv_k_a"], I["rwkv_k_a"][0]))
                rkg = hv("rkg", (I["rwkv_r_k"], I["rwkv_r_k"][0]))
                oka = P.sb("oka", [64, 8], F32, es=ph)
                k.ts("dve", V(oka), V(kag), -1.0, ALU.mult, 1.0, ALU.add)
                lnw = P.sb("lnw", [64, 512], F32, es=ph)
                lnb = P.sb("lnb", [64, 512], F32, es=ph)
                k.dma(V(lnw), V(I["rwkv_ln_w"], I["rwkv_ln_w"][0].partition_broadcast(64)))
                k.dma(V(lnb), V(I["rwkv_ln_b"], I["rwkv_ln_b"][0].partition_broadcast(64)))
                rst = P.sb("rst", [64, BW], F32, es=ph)
                k.memset("pool", V(rst), 1.0)
                k.memset("pool", (rst, rst.a().rearrange("p (c t) -> p c t", t=CH)[:, :, 0:1]), 0.0)
                ones64 = P.sb("ones64", [64, 64], F32, es=ph)
                k.memset("pool", V(ones64), 1.0)
                m4 = []
                m3 = []
                for z in range(2):
                    m = P.sb(f"m4_{z}", [128, 2, 64], F32, es=ph)
                    for half in range(2):
                        for col in range(2):
                            sgn = 1 if z == 0 else -1
                            base = (-1 if col == 0 else 0)
                            P.op("pool", lambda e, m=m, half=half, col=col, sgn=sgn, base=base: e.affine_select(
                                out=m[half * 64:(half + 1) * 64, col, :], in_=ones[half * 64:(half + 1) * 64, 0:64],
                                pattern=[[sgn, 64]], compare_op=ALU.is_ge, fill=0.0, base=base, channel_multiplier=-sgn),
                                reads=[ones], writes=[m])
                    m4.append(m)
                    mm3 = P.sb(f"m3_{z}", [64, 64], F32, es=ph)
                    P.op("pool", lambda e, mm3=mm3, z=z: e.affine_select(
                        out=mm3.a(), in_=ones[0:64, 0:64], pattern=[[-1 if z == 0 else 1, 64]], compare_op=ALU.is_ge, fill=0.0,
                        base=-1, channel_multiplier=1 if z == 0 else -1), reads=[ones], writes=[mm3])
                    m3.append(mm3)
                QI0 = P.sb("QI0", [64, 64], BF16, es=ph)
                k.cp("dve", V(QI0), (ident, ident[0:64, 0:64]))

                nheads = 0 if "rw1" in debug else (1 if ("rw2" in debug or "rw3" in debug) else 8)
                for h in range(nheads):
                    with ExitStack() as hs:
                        Vt = P.sb("Vt", [64, NCH, CH], F32, es=hs)
                        oacc = P.sb("oaccr", [64, NCH, CH], F32, es=hs)
                        bon = P.sb("bon", [64, NCH], F32, es=hs)
                        hs2 = ExitStack()
                        AR = [P.sb(f"AR{z}", [64, NCH, 2, CH], BF16, es=hs2) for z in range(2)]
                        BK = [P.sb(f"BK{z}", [64, NCH, 2, CH], BF16, es=hs2) for z in range(2)]
                        BKeT = [P.sb(f"BKeT{z}", [128, NCH, CH], BF16, es=hs2) for z in range(2)]
                        UV = [P.sb(f"UV{z}", [128, NCH, CH], BF16, es=hs2) for z in range(2)]
                        ZV = P.sb("ZV", [128, NCH, CH], BF16, es=hs2)
                        gC = [P.sb(f"gC{z}", [64, NCH], F32, es=hs2) for z in range(2)]
                        k.memset("pool", (ZV, ZV[0:64, :, :], "z"), 0.0)
                        with ExitStack() as pp_:
                            def T(name, dt=F32, w=BW):
                                return P.sb(name, [64, w], dt, es=pp_)
                            t_r = T("t_r"); t_k = T("t_k"); t_v = T("t_v")
                            t_wd = P.sb("t_wd", [128, BW], F32, es=pp_)
                            t_ad = P.sb("t_ad", [128, BW], F32, es=pp_)
                            t_kk = T("t_kk"); t_q = T("t_q"); t_rn = T("t_rn")
                            t_sg = T("t_sg"); t_cs = T("t_cs"); t_x = T("t_x"); t_y = T("t_y")
                            t_eg = T("t_eg"); t_eng = T("t_eng"); t_egp = T("t_egp")
                            t_a = T("t_a"); t_km = [T("t_km0"), T("t_km1")]; t_b = T("t_b")
                            bke = P.sb("bke", [64, CB, 2, CH], BF16, es=pp_)
                            t_v2 = P.sb("t_v2", [64, CB, 2, CH], F32, es=pp_)
                            pwa = [P.ps(f"pwa{i}", [64, BW], F32, es=pp_) for i in range(2)]
                            pss = P.ps("pss", [64, BW], F32, es=pp_)
                            ptv = P.ps("ptv", [128, CB, CH], F32, es=pp_)
                            ptb = P.ps("ptb", [128, CB, CH], BF16, es=pp_)
                            pbn = P.ps("pbn", [64, NCH], F32, es=pp_)
                            for blk in range(NB):
                                tsl = slice(blk * BW, (blk + 1) * BW)
                                csl = slice(blk * CB, (blk + 1) * CB)
                                k.dma(V(t_r), (PR, PR[h * 64:(h + 1) * 64, tsl]))
                                k.dma(V(t_k), (PR, PR[512 + h * 64:512 + (h + 1) * 64, tsl]))
                                k.dma(V(t_v), (PR, PR[1024 + h * 64:1024 + (h + 1) * 64, tsl]))
                                k.dma(V(t_wd), (PR, PR[1536:1664, tsl]))
                                k.dma(V(t_ad), (PR, PR[1664:1792, tsl]))
                                k.act(V(t_wd), V(t_wd), AF.Tanh)
                                k.cp("pool", V(t_v2), (t_v, t_v.a().rearrange("p (c o t) -> p c o t", o=1, t=CH).to_broadcast([64, CB, 2, CH])))
                                for j in range(CB):
                                    k.tr((ptv, ptv[:, j, :]), (t_v2, t_v2[:, j, :, :].rearrange("p a t -> p (a t)")), (ident, ident[0:64, 0:64]))
                                k.cp("act", (Vt, Vt[:, csl, :], blk), (ptv, ptv[0:64, :, :]))
                                for z in range(2):
                                    k.cp("dve", (UV[z], UV[z][64:128, csl, :], ("v", blk)), (ptv, ptv[64:128, :, :]))
                                k.cp("dve", (ZV, ZV[64:128, csl, :], ("v", blk)), (ptv, ptv[64:128, :, :]))
                                k.ts("dve", V(t_kk), V(t_k), (kkg, kkg[:, h:h + 1]), ALU.mult)
                                k.tt("pool", V(t_q), V(t_kk), V(t_kk), ALU.mult)
                                k.mm(V(pss), V(ones64), V(t_q))
                                k.ts("dve", V(t_rn), V(pss), 1e-12, ALU.add)
                                k.act(V(t_rn), V(t_rn), AF.Sqrt)
                                k.recip(V(t_rn), V(t_rn))
                                k.tt("dve", V(t_kk), V(t_kk), V(t_rn), ALU.mult)
                                for z in range(2):
                                    end = CH - 1 if z == 0 else 0
                                    zs = slice(z * 64, (z + 1) * 64)
                                    pw = pwa[0]; pa = pwa[1]
                                    k.mm(V(pw), (w2all, w2all[zs, h * 64:(h + 1) * 64]), (t_wd, t_wd[zs, :]))
                                    k.mm(V(pa), (a2all, a2all[zs, h * 64:(h + 1) * 64]), (t_ad, t_ad[zs, :]))
                                    k.act(V(t_sg), V(pw), AF.Sigmoid, bias=(w0[z], w0[z][:, h:h + 1]))
                                    k.act(V(t_a), V(pa), AF.Sigmoid, bias=(a0[z], a0[z][:, h:h + 1]))
                                    k.scan(V(t_cs), V(rst), V(t_sg), 0.0, ALU.mult, ALU.add)
                                    if z == 1:
                                        k.tt("dve", V(t_x), V(t_sg), V(t_cs), ALU.subtract)
                                        c3 = t_cs.a().rearrange("p (c t) -> p c t", t=CH)
                                        k.tt("dve", (t_y, t_y.a().rearrange("p (c t) -> p c t", t=CH)),
                                             (t_x, t_x.a().rearrange("p (c t) -> p c t", t=CH)),
                                             (t_cs, c3[:, :, CH - 1:CH].to_broadcast([64, CB, CH])), ALU.add)
                                        cs = t_y
                                    else:
                                        cs = t_cs
                                    k.act(V(t_eg), V(cs), AF.Exp, scale=-0.6065306597126334)
                                    k.act(V(t_eng), V(cs), AF.Exp, scale=0.6065306597126334)
                                    k.tt("dve", V(t_x), V(cs), V(t_sg), ALU.subtract)
                                    k.act(V(t_egp), V(t_x), AF.Exp, scale=-0.6065306597126334)
                                    eg3 = t_eg.a().rearrange("p (c t) -> p c t", t=CH)
                                    k.cp("pool", (gC[z], gC[z][:, csl], blk), (t_eg, eg3[:, :, end]))
                                    k.ts("dve", V(t_x), V(t_a), (kag, kag[:, h:h + 1]), ALU.mult, (oka, oka[:, h:h + 1]), ALU.add)
                                    k.tt("dve", V(t_km[z]), V(t_k), V(t_x), ALU.mult)
                                    k.tt("pool", V(t_b), V(t_kk), V(t_a), ALU.mult)
                                    arz = AR[z]; bkz = BK[z]
                                    k.stt("dve", (arz, arz[:, csl, 0, :], blk), (t_kk, t_kk.a().rearrange("p (c t) -> p c t", t=CH)), -1.0,
                                          (t_egp, t_egp.a().rearrange("p (c t) -> p c t", t=CH)), ALU.mult, ALU.mult)
                                    k.tt("pool", (arz, arz[:, csl, 1, :], blk), (t_r, t_r.a().rearrange("p (c t) -> p c t", t=CH)),
                                         (t_eg, eg3), ALU.mult)
                                    k.tt("dve", V(t_b), V(t_b), V(t_eng), ALU.mult)
                                    k.tt("dve", V(t_x), V(t_km[z]), V(t_eng), ALU.mult)
                                    k.cp("pool", (bkz, bkz[:, csl, 0, :], blk), (t_b, t_b.a().rearrange("p (c t) -> p c t", t=CH)))
                                    k.cp("act", (bkz, bkz[:, csl, 1, :], blk), (t_x, t_x.a().rearrange("p (c t) -> p c t", t=CH)))
                                    gcb = eg3[:, :, end:end + 1].to_broadcast([64, CB, CH])
                                    k.tt("dve", (bke, bke[:, :, 0, :]), (t_b, t_b.a().rearrange("p (c t) -> p c t", t=CH)), (t_eg, gcb), ALU.mult)
                                    k.tt("pool", (bke, bke[:, :, 1, :]), (t_x, t_x.a().rearrange("p (c t) -> p c t", t=CH)), (t_eg, gcb), ALU.mult)
                                    for j in range(CB):
                                        k.tr((ptb, ptb[:, j, :]), (bke, bke[:, j, :, :].rearrange("p a t -> p (a t)")), (identb, identb[0:64, 0:64]))
                                    k.cp("act", (BKeT[z], BKeT[z][:, csl, :], blk), V(ptb))
                                k.tt("dve", V(t_x), V(t_km[0]), V(t_km[1]), ALU.add)
                                k.stt("dve", V(t_x), V(t_r), (rkg, rkg[:, h:h + 1]), V(t_x), ALU.mult, ALU.mult)
                                for j in range(CB):
                                    c = blk * CB + j
                                    k.mm((pbn, pbn[:, c:c + 1]), (t_x, t_x[:, j * CH:(j + 1) * CH]), (ones64, ones64[:, 0:1]))
                            k.cp("dve", V(bon), V(pbn))
                        P.barrier()
                        with ExitStack() as us:
                            Hs = [P.sb(f"Hs{z}", [64, 64], F32, es=us) for z in range(2)]
                            Hb = [P.sb(f"Hb{z}", [64, 64], BF16, es=us) for z in range(2)]
                            AM = [[P.sb(f"AM{z}{i}", [128, 128], BF16, es=us) for i in range(2)] for z in range(2)]
                            Pm = [[P.sb(f"Pm{z}{i}", [64, 64], BF16, es=us) for i in range(2)] for z in range(2)]
                            QR = [[P.sb(f"QR{z}{i}", [64, 2, 64], BF16, es=us) for i in range(2)] for z in range(2)]
                            TT = [[P.sb(f"TT{z}{i}", [64, 64], BF16, es=us) for i in range(2)] for z in range(2)]
                            Xs = [P.sb(f"Xs{z}", [64, 64], BF16, es=us) for z in range(2)]
                            bA = [P.ps(f"bA{z}", [128, 512], F32, es=us) for z in range(2)]
                            bB = [P.ps(f"bB{z}", [64, 128], F32, es=us) for z in range(2)]
                            bC = [P.ps(f"bC{z}", [64, 64], F32, es=us) for z in range(2)]
                            bD = [P.ps(f"bD{z}", [64, 128], F32, es=us) for z in range(2)]
                            vM = [bA[z][:, 0:128] for z in range(2)]
                            vX = [bA[z][0:64, 128:192] for z in range(2)]
                            vU = [bA[z][0:64, 192:256] for z in range(2)]
                            vL = [bB[z].a() for z in range(2)]
                            vP = [bC[z].a() for z in range(2)]
                            vO = [bD[z][:, 0:64] for z in range(2)]
                            vH = [bD[z][:, 64:128] for z in range(2)]
                            pM = [(bA[z], vM[z]) for z in range(2)]
                            pL = [(bB[z], vL[z]) for z in range(2)]
                            pP = [(bC[z], vP[z]) for z in range(2)]
                            pX = [(bA[z], vX[z]) for z in range(2)]
                            pU = [(bA[z], vU[z]) for z in range(2)]
                            pO = [(bD[z], vO[z]) for z in range(2)]
                            pH = [(bD[z], vH[z]) for z in range(2)]
                            k.memset("pool", V(oacc), 0.0)
                            for z in range(2):
                                k.memset("pool", V(Hs[z]), 0.0)
                                k.memset("pool", V(Hb[z]), 0.0)
                            orders = [chunk_order(0), chunk_order(1)]
                            for step in range(NCH if "rw2" not in debug else 0):
                                for z in range(2):
                                    c = orders[z][step]
                                    am = AM[z][step % 2]
                                    arc = AR[z][:, c, :, :].rearrange("p a t -> p (a t)")
                                    bkc = BK[z][:, c, :, :].rearrange("p a t -> p (a t)")
                                    k.mm(pM[z], (BK[z], bkc), (AR[z], arc))
                                    k.tt("dve", V(am), pM[z], (m4[z], m4[z].a().rearrange("p a t -> p (a t)")), ALU.mult)
                                    k.mm(pP[z], (AR[z], AR[z][:, c, 0, :]), (BK[z], BK[z][:, c, 0, :]))
                                    pm_ = Pm[z][0]
                                    k.tt("pool" if False else "dve", V(pm_), pP[z], V(m3[z]), ALU.mult)
                                    if "u1" in debug:
                                        continue
                                    qr = QR[z][0]
                                    k.cp("act", (qr, qr[:, 0, :]), (am, am[0:64, 0:64]))
                                    k.cp("pool", (qr, qr[:, 1, :]), V(QI0))
                                    for lv in range(1, NL + 1):
                                        qn = QR[z][lv % 2]
                                        pn = Pm[z][lv % 2]
                                        k.mm(pL[z], V(pm_), (qr, qr.a().rearrange("p a t -> p (a t)")))
                                        k.mm(pP[z], (qr, qr[:, 0, :]), V(pm_))
                                        k.cp("act", (qn, qn[:, 0, :]), (bB[z], vL[z][:, 0:64]))
                                        k.tt("dve", (qn, qn[:, 1, :]), (bB[z], vL[z][:, 64:128]), (qr, qr[:, 1, :]), ALU.add)
                                        k.cp("act", V(pn), pP[z])
                                        qr = qn; pm_ = pn
                                    tt_ = TT[z][step % 2]
                                    k.mm((bB[z], vL[z][:, 0:64]), V(pm_), (qr, qr[:, 1, :]))
                                    k.tt("dve", V(tt_), (bB[z], vL[z][:, 0:64]), (qr, qr[:, 1, :]), ALU.add)
                                    if "u2" in debug:
                                        continue
                                    k.mm(pX[z], (AR[z], AR[z][:, c, 0, :]), V(Hb[z]), start=True, stop=False)
                                    P.op("pe", lambda e, px=vX[z], am=am, c=c: e.matmul(px, lhsT=am[:, 0:64], rhs=ZV[:, c, :], start=False, stop=True),
                                         reads=[(am, None), (ZV, "z"), (ZV, ("v", c // CB))], writes=[(bA[z], None)], pe_acc=True)
                                    k.cp("act", V(Xs[z]), pX[z])
                                    k.mm(pU[z], V(tt_), V(Xs[z]))
                                    k.cp("dve", (UV[z], UV[z][0:64, c, :], ("u", c)), pU[z])
                                    if "u3" in debug:
                                        continue
                                    k.mm(pO[z], (AR[z], AR[z][:, c, 1, :]), V(Hb[z]), start=True, stop=False)
                                    P.op("pe", lambda e, po=vO[z], am=am, uv=UV[z], c=c: e.matmul(po, lhsT=am[:, 64:128], rhs=uv[:, c, :], start=False, stop=True),
                                         reads=[(am, None), (UV[z], ("u", c)), (UV[z], ("v", c // CB))], writes=[(bD[z], None)], pe_acc=True)
                                    k.tt("dve", (oacc, oacc[:, c, :], c), (oacc, oacc[:, c, :], c), pO[z], ALU.add)
                                    P.op("pe", lambda e, ph_=vH[z], bt=BKeT[z], uv=UV[z], c=c: e.matmul(ph_, lhsT=bt[:, c, :], rhs=uv[:, c, :], start=True, stop=True),
                                         reads=[(BKeT[z], None), (UV[z], ("u", c)), (UV[z], ("v", c // CB))], writes=[(bD[z], None)])
                                    k.stt("dve", V(Hs[z]), V(Hs[z]), (gC[z], gC[z][:, c:c + 1]), pH[z], ALU.mult, ALU.add)
                                    k.cp("act", V(Hb[z]), V(Hs[z]))
                        if "d_oacc" in debug and h == 0:
                            do = P.dram("d_oacc", [64, NCH * CH], F32, kind="ExternalOutput")
                            k.dma(V(do), (oacc, oacc.a().rearrange("p a b -> p (a b)")))
                            dm4 = P.dram("d_m4", [128, 2 * 128], F32, kind="ExternalOutput")
                            for z in range(2):
                                k.dma((dm4, dm4[:, z * 128:(z + 1) * 128]), (m4[z], m4[z].a().rearrange("p a b -> p (a b)")))
                            dm3 = P.dram("d_m3", [64, 2 * 64], F32, kind="ExternalOutput")
                            for z in range(2):
                                k.dma((dm3, dm3[:, z * 64:(z + 1) * 64]), V(m3[z]))
                            dgc = P.dram("d_gC", [64, 2 * NCH], F32, kind="ExternalOutput")
                            for z in range(2):
                                k.dma((dgc, dgc[:, z * NCH:(z + 1) * NCH]), V(gC[z]))
                            dbon = P.dram("d_bon", [64, NCH], F32, kind="ExternalOutput")
                            k.dma(V(dbon), V(bon))
                            dar = P.dram("d_AR", [64, 2 * NCH * 2 * CH], BF16, kind="ExternalOutput")
                            dbk = P.dram("d_BK", [64, 2 * NCH * 2 * CH], BF16, kind="ExternalOutput")
                            for z in range(2):
                                k.dma((dar, dar[:, z * NCH * 128:(z + 1) * NCH * 128]), (AR[z], AR[z].a().rearrange("p a b c -> p (a b c)")))
                                k.dma((dbk, dbk[:, z * NCH * 128:(z + 1) * NCH * 128]), (BK[z], BK[z].a().rearrange("p a b c -> p (a b c)")))
                        P.barrier()
                        hs2.close()
                        with ExitStack() as fs:
                            gt = P.sb("gtr", [64, NCH, CH], F32, es=fs)
                            cen = P.sb("cen", [64, NCH, CH], F32, es=fs)
                            mu_ = P.sb("mu_", [64, NCH], F32, es=fs)
                            var = P.sb("var", [64, NCH], F32, es=fs)
                            k.dma(V(gt), (GT, GT[:, h * 64:(h + 1) * 64].rearrange("(c p) d -> p c d", p=CH)))
                            P.op("dve", lambda e: e.tensor_reduce(out=mu_.a(), in_=oacc.a(), axis=AX.X, op=ALU.add), reads=[oacc], writes=[mu_])
                            k.ts("dve", V(mu_), V(mu_), 1.0 / 64, ALU.mult)
                            b3 = lambda t: t.a().rearrange("p (c o) -> p c o", o=1).to_broadcast([64, NCH, CH])
                            k.tt("dve", V(oacc), V(oacc), (mu_, b3(mu_)), ALU.subtract)
                            k.tt("pool", V(cen), V(oacc), V(oacc), ALU.mult)
                            P.op("dve", lambda e: e.tensor_reduce(out=var.a(), in_=cen.a(), axis=AX.X, op=ALU.add), reads=[cen], writes=[var])
                            k.ts("dve", V(var), V(var), 1.0 / 64, ALU.mult, 64e-5, ALU.add)
                            k.act(V(var), V(var), AF.Sqrt)
                            k.recip(V(var), V(var))
                            k.tt("dve", V(oacc), V(oacc), (var, b3(var)), ALU.mult)
                            rb = lambda t: t[:, h * 64:(h + 1) * 64].rearrange("p (o d) -> p o d", o=1).to_broadcast([64, NCH, CH])
                            k.tt("pool", V(oacc), V(oacc), (lnw, rb(lnw)), ALU.mult)
                            k.tt("dve", V(oacc), V(oacc), (lnb, rb(lnb)), ALU.add)
                            k.tt("pool", V(cen), V(Vt), (bon, b3(bon)), ALU.mult)
                            k.tt("dve", V(oacc), V(oacc), V(cen), ALU.add)
                            k.tt("dve", V(oacc), V(oacc), V(gt), ALU.mult)
                            k.dma((YM, YM[:, h * 64:(h + 1) * 64].rearrange("(c p) d -> p c d", p=CH), ("r", h)), V(oacc))
                        P.barrier()

        def lat_rows(tt, cc):
            c0 = 2 * (tt - 2) + cc
            return XL[LC:, :].rearrange("(r c) d -> c r d", c=64)[c0]

        def stage_outproj(YM, nfeat, W, xsrc, perm, tiles):
            kf = nfeat // 128
            with ExitStack() as ph:
                wb = P.sb("wob", [128, kf, D], BF16, es=ph)
                k.dma(V(wb), (W[0], W[1].rearrange("(ko p) n -> p ko n", p=128)), eng="pool")
                yt = [P.sb(f"yt{i}", [128, nfeat], F32, es=ph) for i in range(2)]
                yT = [P.sb(f"yT{i}", [128, kf, 128], BF16, es=ph) for i in range(2)]
                xo = [P.sb(f"xo{i}", [128, D], F32, es=ph) for i in range(2)]
                xn_ = [P.sb(f"xq{i}", [128, D], F32, es=ph) for i in range(2)]
                pT = [P.ps(f"poT{i}", [128, 8, 128], F32, es=ph) for i in range(2)]
                po = [P.ps(f"poo{i}", [128, 512], F32, es=ph) for i in range(2)]
                for n_, tt in enumerate(tiles):
                    j = 1 if tt < 2 else 0
                    y_ = yt[n_ % 2]; yT_ = yT[n_ % 2]; x_ = xo[n_ % 2]; q_ = xn_[n_ % 2]
                    k.dma(V(y_), (YM, YM[tt * 128:(tt + 1) * 128, :]))
                    if perm and tt >= 2:
                        for cc in range(2):
                            k.dma((x_, x_[cc * 64:(cc + 1) * 64, :], cc), (xsrc(tt)[0], lat_rows(tt, cc), tt))
                    else:
                        sb_, sap = xsrc(tt)
                        k.dma(V(x_), (sb_, sap, tt))
                    for g8 in range(0, kf, 8):
                        p_ = pT[(g8 // 8 + n_) % 2]
                        for ko in range(8):
                            k.tr((p_, p_[:, ko, :]), (y_, y_[:, (g8 + ko) * 128:(g8 + ko + 1) * 128]), V(ident))
                        k.cp("act", (yT_, yT_[:, g8:g8 + 8, :]), V(p_))
                    for nb in range(2):
                        o_ = po[nb]
                        for ko in range(kf):
                            k.mm(V(o_), (yT_, yT_[:, ko, :]), (wb, wb[:, ko, nb * 512:(nb + 1) * 512]), start=(ko == 0), stop=(ko == kf - 1))
                        k.tt("dve", (q_, q_[:, nb * 512:(nb + 1) * 512]), V(o_), (modB, modB[:, 0, j, nb * 512:(nb + 1) * 512]), ALU.mult)
                    k.tt("pool", V(q_), V(q_), V(x_), ALU.add)
                    if perm and tt >= 2:
                        for cc in range(2):
                            k.dma((XL, lat_rows(tt, cc), tt), (q_, q_[cc * 64:(cc + 1) * 64, :]))
                    else:
                        k.dma((XL, XL[tt * 128:(tt + 1) * 128, :], tt), V(q_))
                P.barrier()

        def stage_moe(l, tiles):
            nh = len(tiles) // 2
            with ExitStack() as ph:
                rw32 = P.sb("rw32", [128, KO, 32], F32, es=ph)
                k.dma(V(rw32), V(I["router_w"], I["router_w"][l].rearrange("(ko p) e -> p ko e", p=128)))
                rbB = P.sb("rbB", [128, 32], F32, es=ph)
                k.dma(V(rbB), V(I["router_b"], I["router_b"][l].partition_broadcast(128)))
                BD = P.sb("BD", [32, D], F32, es=ph)
                k.dma(V(BD), V(I["exp_b_down"], I["exp_b_down"][l]))
                bgF = P.sb("bgF", [128, 32, 8], F32, es=ph)
                buF = P.sb("buF", [128, 32, 8], F32, es=ph)
                with ExitStack() as t0:
                    btmp = P.sb("btmp", [128, 128], F32, es=t0)
                    pbt = P.ps("pbt", [128, 128], F32, es=t0)
                    for (dst, src) in ((bgF, I["exp_b_gate"]), (buF, I["exp_b_up"])):
                        for hf in range(2):
                            k.dma(V(btmp), (src, src[l, hf * 16:(hf + 1) * 16, :].rearrange("e (fb p) -> (e fb) p", p=128)))
                            k.tr(V(pbt), V(btmp), V(ident))
                            k.cp("dve", (dst, dst[:, hf * 16:(hf + 1) * 16, :].rearrange("p e f -> p (e f)")), V(pbt))
                    P.barrier()
                for half in range(2):
                    htiles = tiles[half * nh:(half + 1) * nh]
                    NTK = nh * 128
                    with ExitStack() as hs:
                        HTh = P.sb("HTh", [128, KO, NTK], BF16, es=hs)
                        LG = P.sb("LG", [128, nh, 32], F32, es=hs)
                        GW = P.sb("GW", [128, nh, 32], F32, es=hs)
                        acc = P.sb("acc", [128, nh, D], F32, es=hs)
                        with ExitStack() as ns:
                            xt = [P.sb(f"mxt{i}", [128, D], F32, es=ns) for i in range(2)]
                            xn = [P.sb(f"mxn{i}", [128, D], F32, es=ns) for i in range(2)]
                            junk = P.sb("mjunk", [128, D], F32, es=ns)
                            st = [P.sb(f"mst{i}", [128, 4], F32, es=ns) for i in range(2)]
                            tmp = [P.sb(f"mtmp{i}", [128, KO, 128], F32, es=ns) for i in range(2)]
                            h32 = [P.sb(f"mh32{i}", [128, KO, 128], F32, es=ns) for i in range(2)]
                            pT = [P.ps(f"mpT{i}", [128, KO, 128], F32, es=ns) for i in range(2)]
                            plg = [P.ps(f"plg{i}", [128, 32], F32, es=ns) for i in range(2)]
                            pgt = P.ps("pgt", [32, 128], F32, es=ns)
                            GWT = P.sb("GWT", [32, nh, 128], F32, es=ns)
                            pini = [P.ps("pini0", [128, 512], F32, es=ns)] * 2
                            m8 = P.sb("m8", [128, 8], F32, es=ns)
                            msk = P.sb("msk", [128, 32], F32, es=ns)
                            ex = P.sb("ex", [128, 32], F32, es=ns)
                            sm = P.sb("smx", [128, 4], F32, es=ns)
                            for i, tt in enumerate(htiles):
                                j = 1 if tt < 2 else 0
                                x_ = xt[i % 2]; n_ = xn[i % 2]; s_ = st[i % 2]; t_ = tmp[i % 2]; p_ = pT[i % 2]; h_ = h32[i % 2]
                                k.dma(V(x_), (XL, XL[tt * 128:(tt + 1) * 128, :]))
                                k.memset("pool", (s_, s_[:, 0:1]), 0.0)
                                k.act(V(junk), V(x_), AF.Square, accum=(s_, s_[:, 0:1]))
                                k.ts("dve", (s_, s_[:, 1:2]), (s_, s_[:, 0:1]), 1.0 / D, ALU.mult, EPS, ALU.add)
                                k.act((s_, s_[:, 2:3]), (s_, s_[:, 1:2]), AF.Sqrt)
                                k.recip((s_, s_[:, 3:4]), (s_, s_[:, 2:3]))
                                k.ts("dve", V(n_), V(x_), (s_, s_[:, 3:4]), ALU.mult)
                                for ko in range(KO):
                                    k.tr((p_, p_[:, ko, :]), (n_, n_[:, ko * 128:(ko + 1) * 128]), V(ident))
                                k.tt("dve", V(t_), V(p_), (g2F, g2F[:, :, j:j + 1].to_broadcast([128, KO, 128])), ALU.mult)
                                k.tt("pool", V(h_), V(t_), (modF, modF[:, 3, :, j:j + 1].to_broadcast([128, KO, 128])), ALU.add)
                                k.cp("act", (HTh, HTh[:, :, i * 128:(i + 1) * 128], i), V(h_))
                                pl = plg[i % 2]
                                for ko in range(KO):
                                    k.mm(V(pl), (h_, h_[:, ko, :]), (rw32, rw32[:, ko, :]), start=(ko == 0), stop=(ko == KO - 1))
                                lg = (LG, LG[:, i, :], i)
                                k.tt("dve", lg, V(pl), V(rbB), ALU.add)
                                P.op("dve", lambda e, i=i: e.max(out=m8.a(), in_=LG[:, i, :]), reads=[(LG, i)], writes=[m8])
                                k.ts("dve", V(msk), lg, (m8, m8[:, 3:4]), ALU.is_ge)
                                k.ts("dve", (sm, sm[:, 0:1]), (m8, m8[:, 0:1]), -1.0, ALU.mult)
                                k.act(V(ex), lg, AF.Exp, bias=(sm, sm[:, 0:1]))
                                k.tt("dve", V(ex), V(ex), V(msk), ALU.mult)
                                P.op("dve", lambda e: e.tensor_reduce(out=sm[:, 1:2], in_=ex.a(), axis=AX.X, op=ALU.add), reads=[ex], writes=[sm])
                                k.recip((sm, sm[:, 2:3]), (sm, sm[:, 1:2]))
                                k.ts("dve", (GW, GW[:, i, :], i), V(ex), (sm, sm[:, 2:3]), ALU.mult)
                                k.tr(V(pgt), (GW, GW[:, i, :], i), V(ident))
                                k.cp("act", (GWT, GWT[:, i, :], i), V(pgt))
                                for nb in range(2):
                                    k.mm(V(pini[nb]), (GWT, GWT[:, i, :], i), (BD, BD[:, nb * 512:(nb + 1) * 512]))
                                    k.cp("act" if nb else "dve", (acc, acc[:, i, nb * 512:(nb + 1) * 512], (i, nb)), V(pini[nb]))
                            P.barrier()
                        if f"LG{l}" in debug and half == 0:
                            dl = P.dram(f"d_GW{l}", [128, nh * 32], F32, kind="ExternalOutput")
                            k.dma(V(dl), (GW, GW.a().rearrange("p a b -> p (a b)")))
                        with ExitStack() as xs:
                            wgs = [P.sb(f"wg{i}", [128, KO, 512], BF16, es=xs) for i in range(2)]
                            wus = [P.sb(f"wu{i}", [128, KO, 512], BF16, es=xs) for i in range(2)]
                            wds = [P.sb(f"wd{i}", [128, 4, D], BF16, es=xs) for i in range(2)]
                            aTs = [P.sb(f"aT{i}", [128, 4, 512], BF16, es=xs) for i in range(2)]
                            dtmp = [P.sb(f"dtmp{i}", [128, 512], F32, es=xs) for i in range(2)]
                            g1 = [P.sb(f"g1_{i}", [128, 512], F32, es=xs) for i in range(2)]
                            sg = [P.sb(f"sg_{i}", [128, 512], F32, es=xs) for i in range(2)]
                            u1 = [P.sb(f"u1_{i}", [128, 512], F32, es=xs) for i in range(2)]
                            pg = [P.ps(f"mpg{i}", [128, 512], F32, es=xs) for i in range(2)]
                            pu = [P.ps(f"mpu{i}", [128, 512], F32, es=xs) for i in range(2)]
                            pd = [P.ps(f"mpd{i}", [128, 512], F32, es=xs) for i in range(2)]
                            if nh == 17:
                                tws = [512, 512, 384, 384, 384]
                            else:
                                tws = [512] * (NTK // 512)
                            tblocks = []
                            t_acc = 0
                            for tw in tws:
                                tblocks.append((t_acc, tw)); t_acc += tw
                            nexp = 32 if "moe_fast" not in debug else 2
                            cnt = 0
                            nblk = 0
                            cntbox = [0]

                            def emit_loads(he):
                                e, fh = he // 2, he % 2
                                wg = wgs[he % 2]; wu = wus[he % 2]; wd = wds[he % 2]
                                k.dma(V(wg), (I["exp_w_gate"], I["exp_w_gate"][l, e][:, fh * 512:(fh + 1) * 512].rearrange("(ko p) n -> p ko n", p=128)), eng="pool")
                                k.dma(V(wu), (I["exp_w_up"], I["exp_w_up"][l, e][:, fh * 512:(fh + 1) * 512].rearrange("(ko p) n -> p ko n", p=128)), eng="pool")
                                k.dma(V(wd), (I["exp_w_down"], I["exp_w_down"][l, e][fh * 512:(fh + 1) * 512, :].rearrange("(fo p) n -> p fo n", p=128)), eng="pool")

                            def emit_G(j, he, t0_, tw):
                                e, fh = he // 2, he % 2
                                wg = wgs[he % 2]; wu = wus[he % 2]
                                aT = aTs[j % 2]
                                for fbl in range(4):
                                    fb = fh * 4 + fbl
                                    cnt = cntbox[0]; cntbox[0] += 1
                                    pg_ = pg[cnt % 2]; pu_ = pu[cnt % 2]; g_ = g1[cnt % 2]; s_ = sg[cnt % 2]; u_ = u1[cnt % 2]
                                    for ko in range(KO):
                                        k.mm((pg_, pg_[:, 0:tw]), (wg, wg[:, ko, fbl * 128:(fbl + 1) * 128]), (HTh, HTh[:, ko, t0_:t0_ + tw]),
                                             start=(ko == 0), stop=(ko == KO - 1))
                                    for ko in range(KO):
                                        k.mm((pu_, pu_[:, 0:tw]), (wu, wu[:, ko, fbl * 128:(fbl + 1) * 128]), (HTh, HTh[:, ko, t0_:t0_ + tw]),
                                             start=(ko == 0), stop=(ko == KO - 1))
                                    k.ts("dve", (g_, g_[:, 0:tw]), (pg_, pg_[:, 0:tw]), (bgF, bgF[:, e, fb:fb + 1]), ALU.add, 7.0, ALU.min)
                                    k.act((s_, s_[:, 0:tw]), (g_, g_[:, 0:tw]), AF.Sigmoid, scale=1.702)
                                    k.act((u_, u_[:, 0:tw]), (pu_, pu_[:, 0:tw]), AF.Identity, bias=(buF, buF[:, e, fb:fb + 1]))
                                    k.ts("dve", (u_, u_[:, 0:tw]), (u_, u_[:, 0:tw]), 7.0, ALU.min, -7.0, ALU.max)
                                    k.tt("dve", (g_, g_[:, 0:tw]), (g_, g_[:, 0:tw]), (s_, s_[:, 0:tw]), ALU.mult)
                                    k.stt("dve", (aT, aT[:, fbl, 0:tw], fbl), (u_, u_[:, 0:tw]), 1.0, (g_, g_[:, 0:tw]), ALU.add, ALU.mult)

                            def emit_D(j, he, t0_, tw):
                                e = he // 2
                                wd = wds[he % 2]
                                aT = aTs[j % 2]
                                for ti in range(tw // 128):
                                    i = t0_ // 128 + ti
                                    for nb in range(2):
                                        pd_ = pd[nb]
                                        for fo in range(4):
                                            k.mm(V(pd_), (aT, aT[:, fo, ti * 128:(ti + 1) * 128], fo), (wd, wd[:, fo, nb * 512:(nb + 1) * 512]),
                                                 start=(fo == 0), stop=(fo == 3))
                                        asl = (acc, acc[:, i, nb * 512:(nb + 1) * 512], (i, nb))
                                        if nb == 0:
                                            k.stt("dve", asl, V(pd_), (GW, GW[:, i, e:e + 1], i), asl, ALU.mult, ALU.add)
                                        else:
                                            dt_ = dtmp[i % 2]
                                            k.act(V(dt_), V(pd_), AF.Copy, scale=(GW, GW[:, i, e:e + 1], i))
                                            k.tt("dve", asl, asl, V(dt_), ALU.add)

                            blocks = [(he, t0_, tw) for he in range(nexp * 2) for (t0_, tw) in tblocks]
                            emit_loads(0)
                            emit_G(0, *blocks[0])
                            for j in range(len(blocks)):
                                if j + 1 < len(blocks):
                                    if blocks[j + 1][0] != blocks[j][0]:
                                        emit_loads(blocks[j + 1][0])
                                    emit_G(j + 1, *blocks[j + 1])
                                emit_D(j, *blocks[j])
                            P.barrier()
                        with ExitStack() as rs:
                            xr = [P.sb(f"xr{i}", [128, D], F32, es=rs) for i in range(2)]
                            for i, tt in enumerate(htiles):
                                j = 1 if tt < 2 else 0
                                x_ = xr[i % 2]
                                k.dma(V(x_), (XL, XL[tt * 128:(tt + 1) * 128, :], ("m", tt)))
                                k.tt("dve", (acc, acc[:, i, :]), (acc, acc[:, i, :]), (modB, modB[:, 1, j, :]), ALU.mult)
                                k.tt("pool", V(x_), V(x_), (acc, acc[:, i, :]), ALU.add)
                                k.dma((XL, XL[tt * 128:(tt + 1) * 128, :], ("m", tt)), V(x_))
                            P.barrier()

        def mixer_rwkv2(PR, GT, YM):
            BW = 1088
            NB = L // BW
            CB = BW // CH
            NL = 5
            CBU = 4
            NSS = NCH // CBU
            SUB = ((0, 512), (512, 512), (1024, 64))
            ARD = P.dram("ARD", [16, 64, NCH * 128], BF16)
            BKD = P.dram("BKD", [16, 64, NCH * 128], BF16)
            BKTD = P.dram("BKTD", [16, 128, NCH * 64], BF16)
            VVD = P.dram("VVD", [8, 128, NCH * 64], BF16)
            VTD = P.dram("VTD", [8, 64, NCH * 64], F32)
            OD = P.dram("OD", [2, L, 512], F32)
            with ExitStack() as ph:
                w2all = P.sb("w2all", [128, 512], F32, es=ph)
                a2all = P.sb("a2all", [128, 512], F32, es=ph)
                k.dma(V(w2all), V(I["rwkv_w2"], I["rwkv_w2"][0].rearrange("z r c -> (z r) c")))
                k.dma(V(a2all), V(I["rwkv_a2"], I["rwkv_a2"][0].rearrange("z r c -> (z r) c")))

                def hv(name, src):
                    t = P.sb(name, [64, 8], F32, es=ph)
                    k.dma(V(t), (src[0], src[1].rearrange("(h n) -> n h", n=64)), allow_slow_non_contiguous=True)
                    return t
                w0 = [hv(f"w0_{z}", (I["rwkv_w0"], I["rwkv_w0"][0, z])) for z in range(2)]
                a0 = [hv(f"a0_{z}", (I["rwkv_a0"], I["rwkv_a0"][0, z])) for z in range(2)]
                kkg = hv("kkg", (I["rwkv_k_k"], I["rwkv_k_k"][0]))
                kag = hv("kag", (I["rwkv_k_a"], I["rwkv_k_a"][0]))
                rkg = hv("rkg", (I["rwkv_r_k"], I["rwkv_r_k"][0]))
                oka = P.sb("oka", [64, 8], F32, es=ph)
                k.ts("dve", V(oka), V(kag), -1.0, ALU.mult, 1.0, ALU.add)
                lnw = P.sb("lnw", [64, 512], F32, es=ph)
                lnb = P.sb("lnb", [64, 512], F32, es=ph)
                k.dma(V(lnw), V(I["rwkv_ln_w"], I["rwkv_ln_w"][0].partition_broadcast(64)))
                k.dma(V(lnb), V(I["rwkv_ln_b"], I["rwkv_ln_b"][0].partition_broadcast(64)))
                ones64 = P.sb("ones64", [64, 64], F32, es=ph)
                k.memset("pool", V(ones64), 1.0)
                m4 = []
                m3 = []
                for z in range(2):
                    m = P.sb(f"m4_{z}", [128, 2, 64], F32, es=ph)
                    sgn = 1 if z == 0 else -1
                    for half in range(2):
                        for col in range(2):
                            base = (-1 if col == 1 else 0)
                            P.op("pool", lambda e, m=m, half=half, col=col, sgn=sgn, base=base: e.affine_select(
                                out=m[half * 64:(half + 1) * 64, col, :], in_=ones[half * 64:(half + 1) * 64, 0:64],
                                pattern=[[sgn, 64]], compare_op=ALU.is_ge, fill=0.0, base=base, channel_multiplier=-sgn),
                                reads=[ones], writes=[m])
                    m4.append(m)
                    mm3 = P.sb(f"m3_{z}", [64, 64], F32, es=ph)
                    P.op("pool", lambda e, mm3=mm3, z=z: e.affine_select(
                        out=mm3.a(), in_=ones[0:64, 0:64], pattern=[[-1 if z == 0 else 1, 64]], compare_op=ALU.is_ge, fill=0.0,
                        base=-1, channel_multiplier=1 if z == 0 else -1), reads=[ones], writes=[mm3])
                    m3.append(mm3)
                QI0 = P.sb("QI0", [64, 64], BF16, es=ph)
                k.cp("dve", V(QI0), (ident, ident[0:64, 0:64]))
                gCall = P.sb("gCall", [64, 16, NCH], F32, es=ph)
                bonall = P.sb("bonall", [64, 8, NCH], F32, es=ph)
                with ExitStack() as pp_:
                    rst = P.sb("rst", [64, BW], F32, es=pp_)
                    k.memset("pool", V(rst), 1.0)
                    k.memset("pool", (rst, rst.a().rearrange("p (c t) -> p c t", t=CH)[:, :, 0:1]), 0.0)

                    def T(name, dt=F32):
                        return P.sb(name, [64, BW], dt, es=pp_)
                    t_r = T("t_r"); t_k = T("t_k"); t_v = T("t_v")
                    t_wd = P.sb("t_wd", [128, BW], F32, es=pp_)
                    t_ad = P.sb("t_ad", [128, BW], F32, es=pp_)
                    t_kk = T("t_kk"); t_q = T("t_q"); t_rn = T("t_rn")
                    t_sg = T("t_sg"); t_cs = T("t_cs"); t_x = T("t_x"); t_y = T("t_y")
                    t_eg = T("t_eg"); t_eng = T("t_eng"); t_egp = T("t_egp")
                    t_a = T("t_a"); t_km = [T("t_km0"), T("t_km1")]; t_b = T("t_b")
                    t_v2 = P.sb("t_v2", [64, CB, 2, CH], F32, es=pp_)
                    arb = P.sb("arb", [64, CB, 2, CH], BF16, es=pp_)
                    bkb = P.sb("bkb", [64, CB, 2, CH], BF16, es=pp_)
                    bke = P.sb("bke", [64, CB, 2, CH], BF16, es=pp_)
                    bktb = P.sb("bktb", [128, CB, CH], BF16, es=pp_)
                    zvb = P.sb("zvb", [128, CB, CH], BF16, es=pp_)
                    vtb = P.sb("vtb", [64, CB, CH], F32, es=pp_)
                    pwa = [P.ps(f"pwa{i}", [64, 512], F32, es=pp_) for i in range(2)]
                    pss = P.ps("pss", [64, 512], F32, es=pp_)
                    ptv = P.ps("ptv", [128, 4, CH], F32, es=pp_)
                    ptb = P.ps("ptb", [128, 4, CH], BF16, es=pp_)
                    pbn = P.ps("pbn", [64, NCH], F32, es=pp_)
                    k.memset("pool", (zvb, zvb[0:64, :, :], "z"), 0.0)
                    c3 = lambda t: t.a().rearrange("p (c t) -> p c t", t=CH)
                    for h in range(8):
                        for blk in range(NB):
                            tsl = slice(blk * BW, (blk + 1) * BW)
                            csl = slice(blk * CB, (blk + 1) * CB)
                            k.dma(V(t_r), (PR, PR[h * 64:(h + 1) * 64, tsl]))
                            k.dma(V(t_k), (PR, PR[512 + h * 64:512 + (h + 1) * 64, tsl]))
                            k.dma(V(t_v), (PR, PR[1024 + h * 64:1024 + (h + 1) * 64, tsl]))
                            k.dma(V(t_wd), (PR, PR[1536:1664, tsl]))
                            k.dma(V(t_ad), (PR, PR[1664:1792, tsl]))
                            k.act(V(t_wd), V(t_wd), AF.Tanh)
                            k.cp("pool", V(t_v2), (t_v, t_v.a().rearrange("p (c o t) -> p c o t", o=1, t=CH).to_broadcast([64, CB, 2, CH])))
                            for j0 in range(0, CB, 4):
                                nj = min(4, CB - j0)
                                for j in range(nj):
                                    k.tr((ptv, ptv[:, j, :]), (t_v2, t_v2[:, j0 + j, :, :].rearrange("p a t -> p (a t)")), (ident, ident[0:64, 0:64]))
                                k.cp("act", (vtb, vtb[:, j0:j0 + nj, :], j0), (ptv, ptv[0:64, 0:nj, :]))
                                k.cp("dve", (zvb, zvb[64:128, j0:j0 + nj, :], ("v", j0)), (ptv, ptv[64:128, 0:nj, :]))
                            k.dma((VTD, VTD[h][:, blk * CB * CH:(blk + 1) * CB * CH], (h, blk)), (vtb, vtb.a().rearrange("p c t -> p (c t)")))
                            k.dma((VVD, VVD[h][:, blk * CB * CH:(blk + 1) * CB * CH], (h, blk)), (zvb, zvb.a().rearrange("p c t -> p (c t)")))
                            k.ts("dve", V(t_kk), V(t_k), (kkg, kkg[:, h:h + 1]), ALU.mult)
                            k.tt("pool", V(t_q), V(t_kk), V(t_kk), ALU.mult)
                            for (s0, sw) in SUB:
                                k.mm((pss, pss[:, 0:sw]), V(ones64), (t_q, t_q[:, s0:s0 + sw]))
                                k.ts("dve", (t_rn, t_rn[:, s0:s0 + sw], s0), (pss, pss[:, 0:sw]), 1e-12, ALU.add)
                            k.act(V(t_rn), V(t_rn), AF.Sqrt)
                            k.recip(V(t_rn), V(t_rn))
                            k.tt("dve", V(t_kk), V(t_kk), V(t_rn), ALU.mult)
                            for z in range(2):
                                end = CH - 1 if z == 0 else 0
                                zs = slice(z * 64, (z + 1) * 64)
                                for (s0, sw) in SUB:
                                    pw = pwa[0]; pa = pwa[1]
                                    k.mm((pw, pw[:, 0:sw]), (w2all, w2all[zs, h * 64:(h + 1) * 64]), (t_wd, t_wd[zs, s0:s0 + sw]))
                                    k.mm((pa, pa[:, 0:sw]), (a2all, a2all[zs, h * 64:(h + 1) * 64]), (t_ad, t_ad[zs, s0:s0 + sw]))
                                    k.act((t_sg, t_sg[:, s0:s0 + sw], s0), (pw, pw[:, 0:sw]), AF.Sigmoid, bias=(w0[z], w0[z][:, h:h + 1]))
                                    k.act((t_a, t_a[:, s0:s0 + sw], s0), (pa, pa[:, 0:sw]), AF.Sigmoid, bias=(a0[z], a0[z][:, h:h + 1]))
                                k.scan(V(t_cs), V(rst), V(t_sg), 0.0, ALU.mult, ALU.add)
                                if z == 1:
                                    k.tt("dve", V(t_x), V(t_sg), V(t_cs), ALU.subtract)
                                    k.tt("dve", (t_y, c3(t_y)), (t_x, c3(t_x)), (t_cs, c3(t_cs)[:, :, CH - 1:CH].to_broadcast([64, CB, CH])), ALU.add)
                                    cs = t_y
                                else:
                                    cs = t_cs
                                k.act(V(t_eg), V(cs), AF.Exp, scale=-0.6065306597126334)
                                k.act(V(t_eng), V(cs), AF.Exp, scale=0.6065306597126334)
                                k.tt("dve", V(t_x), V(cs), V(t_sg), ALU.subtract)
                                k.act(V(t_egp), V(t_x), AF.Exp, scale=-0.6065306597126334)
                                eg3 = c3(t_eg)
                                k.cp("pool", (gCall, gCall[:, h * 2 + z, csl], (h, z, blk)), (t_eg, eg3[:, :, end]))
                                k.ts("dve", V(t_x), V(t_a), (kag, kag[:, h:h + 1]), ALU.mult, (oka, oka[:, h:h + 1]), ALU.add)
                                k.tt("dve", V(t_km[z]), V(t_k), V(t_x), ALU.mult)
                                k.tt("pool", V(t_b), V(t_kk), V(t_a), ALU.mult)
                                k.stt("dve", (arb, arb[:, :, 1, :], 1), (t_kk, c3(t_kk)), -1.0, (t_egp, c3(t_egp)), ALU.mult, ALU.mult)
                                k.tt("pool", (arb, arb[:, :, 0, :], 0), (t_r, c3(t_r)), (t_eg, eg3), ALU.mult)
                                k.tt("dve", V(t_b), V(t_b), V(t_eng), ALU.mult)
                                k.tt("dve", V(t_x), V(t_km[z]), V(t_eng), ALU.mult)
                                k.cp("pool", (bkb, bkb[:, :, 0, :], 0), (t_b, c3(t_b)))
                                k.cp("act", (bkb, bkb[:, :, 1, :], 1), (t_x, c3(t_x)))
                                gcb = eg3[:, :, end:end + 1].to_broadcast([64, CB, CH])
                                k.tt("dve", (bke, bke[:, :, 0, :], 0), (t_b, c3(t_b)), (t_eg, gcb), ALU.mult)
                                k.tt("pool", (bke, bke[:, :, 1, :], 1), (t_x, c3(t_x)), (t_eg, gcb), ALU.mult)
                                for j0 in range(0, CB, 4):
                                    nj = min(4, CB - j0)
                                    for j in range(nj):
                                        k.tr((ptb, ptb[:, j, :]), (bke, bke[:, j0 + j, :, :].rearrange("p a t -> p (a t)")), (identb, identb[0:64, 0:64]))
                                    k.cp("act", (bktb, bktb[:, j0:j0 + nj, :], j0), (ptb, ptb[:, 0:nj, :]))
                                hz = h * 2 + z
                                k.dma((ARD, ARD[hz][:, blk * CB * 128:(blk + 1) * CB * 128], (hz, blk)), (arb, arb.a().rearrange("p c a t -> p (c a t)")))
                                k.dma((BKD, BKD[hz][:, blk * CB * 128:(blk + 1) * CB * 128], (hz, blk)), (bkb, bkb.a().rearrange("p c a t -> p (c a t)")))
                                k.dma((BKTD, BKTD[hz][:, blk * CB * CH:(blk + 1) * CB * CH], (hz, blk)), (bktb, bktb.a().rearrange("p c t -> p (c t)")))
                            k.tt("dve", V(t_x), V(t_km[0]), V(t_km[1]), ALU.add)
                            k.stt("dve", V(t_x), V(t_r), (rkg, rkg[:, h:h + 1]), V(t_x), ALU.mult, ALU.mult)
                            for j in range(CB):
                                c = blk * CB + j
                                k.mm((pbn, pbn[:, c:c + 1]), (t_x, t_x[:, j * CH:(j + 1) * CH]), (ones64, ones64[:, 0:1]))
                        k.cp("dve", (bonall, bonall[:, h, :], h), V(pbn))
                    P.barrier()
                for ps_ in range(2 if "rw_preponly" not in debug else 0):
                    with ExitStack() as us:
                        chains = [(ps_ * 4 + hl, z) for hl in range(4) for z in range(2)]
                        CHN = []
                        for ci, (h, z) in enumerate(chains):
                            d_ = {}
                            d_["h"] = h; d_["z"] = z; d_["hz"] = h * 2 + z; d_["ci"] = ci
                            d_["Hs"] = P.sb(f"Hs{ci}", [64, 64], F32, es=us)
                            d_["Hb"] = P.sb(f"Hb{ci}", [64, 64], BF16, es=us)
                            d_["AM"] = [P.sb(f"AM{ci}_{i}", [128, 192], BF16, es=us) for i in range(2)]
                            for am_ in d_["AM"]:
                                k.cp("dve", (am_, am_[0:64, 128:192], "I"), (ident, ident[0:64, 0:64]))
                            d_["Pm"] = [P.sb(f"Pm{ci}_{i}", [64, 64], BF16, es=us) for i in range(1)]
                            d_["QR"] = [P.sb(f"QR{ci}_{i}", [64, 3, 64], BF16, es=us) for i in range(2)]
                            d_["TT"] = [P.sb(f"TT{ci}_{i}", [64, 64], BF16, es=us) for i in range(2)]
                            d_["Xs"] = P.sb(f"Xs{ci}", [64, 64], BF16, es=us)
                            d_["ARb"] = [P.sb(f"ARb{ci}_{i}", [64, CBU, 2, CH], BF16, es=us) for i in range(2)]
                            d_["BKb"] = [P.sb(f"BKb{ci}_{i}", [64, CBU, 2, CH], BF16, es=us) for i in range(2)]
                            d_["BKTb"] = [P.sb(f"BKTb{ci}_{i}", [128, CBU, CH], BF16, es=us) for i in range(2)]
                            d_["UVb"] = [P.sb(f"UVb{ci}_{i}", [128, CBU, CH], BF16, es=us) for i in range(2)]
                            d_["ZVb"] = [P.sb(f"ZVb{ci}_{i}", [128, CBU, CH], BF16, es=us) for i in range(2)]
                            d_["Ob"] = [P.sb(f"Ob{ci}_{i}", [64, CBU, CH], F32, es=us) for i in range(2)]
                            d_["bank"] = P.ps(f"bank{ci}", [128, 512], F32, es=us)
                            k.memset("pool", V(d_["Hs"]), 0.0)
                            k.memset("pool", V(d_["Hb"]), 0.0)
                            CHN.append(d_)

                        def clo(z, ss):
                            if z == 0:
                                return ss * CBU
                            return 0 if ss == 0 else NCH - CBU * ss

                        def loads(ss):
                            for d_ in CHN:
                                c0 = clo(d_["z"], ss); i = ss % 2; hz = d_["hz"]; h = d_["h"]
                                k.dma((d_["ARb"][i], d_["ARb"][i].a().rearrange("p c a t -> p (c a t)")), (ARD, ARD[hz][:, c0 * 128:(c0 + CBU) * 128]))
                                k.dma((d_["BKb"][i], d_["BKb"][i].a().rearrange("p c a t -> p (c a t)")), (BKD, BKD[hz][:, c0 * 128:(c0 + CBU) * 128]))
                                k.dma((d_["BKTb"][i], d_["BKTb"][i].a().rearrange("p c t -> p (c t)")), (BKTD, BKTD[hz][:, c0 * CH:(c0 + CBU) * CH]))
                                k.dma((d_["ZVb"][i], d_["ZVb"][i].a().rearrange("p c t -> p (c t)")), (VVD, VVD[h][:, c0 * CH:(c0 + CBU) * CH]))
                                k.dma((d_["UVb"][i], d_["UVb"][i][64:128, :, :].rearrange("p c t -> p (c t)"), "v"), (VVD, VVD[h][64:128, c0 * CH:(c0 + CBU) * CH]))

                        def unit_stage(d_, ss, jj, st):
                            z = d_["z"]; h = d_["h"]; hz = d_["hz"]
                            i = ss % 2
                            c0 = clo(z, ss)
                            cl = jj if z == 0 else CBU - 1 - jj
                            c = c0 + cl
                            step = ss * CBU + jj
                            ci = d_["ci"]
                            bk_ = d_["bank"]
                            vM = bk_[:, 0:128]; vL = bk_[0:64, 128:256]; vP = bk_[0:64, 256:320]; vLP = bk_[0:64, 128:320]
                            vX = bk_[0:64, 320:384]; vU = bk_[0:64, 384:448]; vH = bk_[0:64, 448:512]
                            ARb = d_["ARb"][i]; BKb = d_["BKb"][i]; BKTb = d_["BKTb"][i]; UVb = d_["UVb"][i]; ZVb = d_["ZVb"][i]; Ob = d_["Ob"][i]
                            am = d_["AM"][step % 2]
                            tt_ = d_["TT"][step % 2]
                            Hb = d_["Hb"]; Hs = d_["Hs"]; Xs = d_["Xs"]
                            ev = "act" if ci % 2 else "dve"
                            if st == 0:
                                arc = ARb[:, cl, :, :].rearrange("p a t -> p (a t)")
                                bkc = BKb[:, cl, :, :].rearrange("p a t -> p (a t)")
                                k.mm((bk_, vM), (BKb, bkc), (ARb, arc))
                                k.mm((bk_, vP), (ARb, ARb[:, cl, 1, :]), (BKb, BKb[:, cl, 0, :]))
                                k.tt("dve", (am, am[:, 0:128], "m"), (bk_, vM), (m4[z], m4[z].a().rearrange("p a t -> p (a t)")), ALU.mult)
                                k.tt("dve", V(d_["Pm"][0]), (bk_, vP), V(m3[z]), ALU.mult)
                            elif 1 <= st <= NL:
                                lv = st
                                qn = d_["QR"][lv % 2]
                                if lv == 1:
                                    rqr = (am, am[0:64, 64:192]); rr = (am, am[0:64, 128:192]); lq = (am, am[0:64, 64:128]); pm_ = V(d_["Pm"][0])
                                else:
                                    qp = d_["QR"][(lv - 1) % 2]
                                    rqr = (qp, qp[:, 0:2, :].rearrange("p a t -> p (a t)")); rr = (qp, qp[:, 1, :]); lq = (qp, qp[:, 0, :]); pm_ = (qp, qp[:, 2, :])
                                k.mm((bk_, vL), pm_, rqr, start=True, stop=False)
                                k.mm((bk_, vL[:, 64:128]), V(QI0), rr, start=False, stop=True)
                                k.mm((bk_, vP), lq, pm_)
                                k.cp(ev, (qn, qn.a().rearrange("p a t -> p (a t)")), (bk_, vLP))
                            elif st == NL + 1:
                                qp = d_["QR"][NL % 2]
                                k.mm((bk_, vL[:, 0:64]), (qp, qp[:, 2, :]), (qp, qp[:, 1, :]), start=True, stop=False)
                                k.mm((bk_, vL[:, 0:64]), V(QI0), (qp, qp[:, 1, :]), start=False, stop=True)
                                k.cp(ev, V(tt_), (bk_, vL[:, 0:64]))
                            elif st == NL + 2:
                                k.mm((bk_, vX), (ARb, ARb[:, cl, 1, :]), V(Hb), start=True, stop=False)
                                P.op("pe", lambda e, vX=vX, am=am, ZVb=ZVb, cl=cl: e.matmul(vX, lhsT=am[:, 64:128], rhs=ZVb[:, cl, :], start=False, stop=True),
                                     reads=[(am, "m"), (ZVb, None)], writes=[(bk_, None)], pe_acc=True)
                                k.cp("act", V(Xs), (bk_, vX))
                            elif st == NL + 3:
                                k.mm((bk_, vU), V(tt_), V(Xs))
                                k.cp("dve", (UVb, UVb[0:64, cl, :], ("u", cl)), (bk_, vU))
                            elif st == NL + 4:
                                k.mm((bk_, vX), (ARb, ARb[:, cl, 0, :]), V(Hb), start=True, stop=False)
                                P.op("pe", lambda e, vX=vX, am=am, UVb=UVb, cl=cl: e.matmul(vX, lhsT=am[:, 0:64], rhs=UVb[:, cl, :], start=False, stop=True),
                                     reads=[(am, "m"), (UVb, ("u", cl)), (UVb, "v")], writes=[(bk_, None)], pe_acc=True)
                                P.op("pe", lambda e, vH=vH, BKTb=BKTb, UVb=UVb, cl=cl: e.matmul(vH, lhsT=BKTb[:, cl, :], rhs=UVb[:, cl, :], start=True, stop=True),
                                     reads=[(BKTb, None), (UVb, ("u", cl)), (UVb, "v")], writes=[(bk_, None)], pe_acc=True)
                                k.cp("act", (Ob, Ob[:, cl, :], cl), (bk_, vX))
                                k.stt("dve", V(Hs), V(Hs), (gCall, gCall[:, hz, c:c + 1]), (bk_, vH), ALU.mult, ALU.add)
                                k.cp("act", V(Hb), V(Hs))

                        loads(0)
                        for ss in range(NSS):
                            if ss + 1 < NSS:
                                loads(ss + 1)
                            i = ss % 2
                            for jj in range(CBU):
                                for st in range(NL + 5):
                                    for d_ in CHN:
                                        unit_stage(d_, ss, jj, st)
                            for d_ in CHN:
                                c0 = clo(d_["z"], ss); h = d_["h"]; z = d_["z"]
                                k.dma((OD, OD[z][c0 * CH:(c0 + CBU) * CH, h * 64:(h + 1) * 64].rearrange("(c p) d -> p c d", p=CH), (z, h, ss)), V(d_["Ob"][i]))
                        P.barrier()
                with ExitStack() as fs:
                    oacc = P.sb("oaccr", [64, NCH, CH], F32, es=fs)
                    o1 = P.sb("o1r", [64, NCH, CH], F32, es=fs)
                    Vt = P.sb("Vtr", [64, NCH, CH], F32, es=fs)
                    gt = P.sb("gtr", [64, NCH, CH], F32, es=fs)
                    cen = P.sb("cen", [64, NCH, CH], F32, es=fs)
                    mu_ = P.sb("mu_", [64, NCH], F32, es=fs)
                    var = P.sb("var", [64, NCH], F32, es=fs)
                    for h in range(8):
                        k.dma(V(oacc), (OD, OD[0][:, h * 64:(h + 1) * 64].rearrange("(c p) d -> p c d", p=CH)))
                        k.dma(V(o1), (OD, OD[1][:, h * 64:(h + 1) * 64].rearrange("(c p) d -> p c d", p=CH)))
                        k.dma(V(Vt), (VTD, VTD[h].rearrange("p (c t) -> p c t", t=CH)))
                        k.dma(V(gt), (GT, GT[:, h * 64:(h + 1) * 64].rearrange("(c p) d -> p c d", p=CH)))
                        k.tt("pool", V(oacc), V(oacc), V(o1), ALU.add)
                        P.op("dve", lambda e: e.tensor_reduce(out=mu_.a(), in_=oacc.a(), axis=AX.X, op=ALU.add), reads=[oacc], writes=[mu_])
                        k.ts("dve", V(mu_), V(mu_), 1.0 / 64, ALU.mult)
                        b3 = lambda t: t.a().rearrange("p (c o) -> p c o", o=1).to_broadcast([64, NCH, CH])
                        k.tt("dve", V(oacc), V(oacc), (mu_, b3(mu_)), ALU.subtract)
                        k.tt("pool", V(cen), V(oacc), V(oacc), ALU.mult)
                        P.op("dve", lambda e: e.tensor_reduce(out=var.a(), in_=cen.a(), axis=AX.X, op=ALU.add), reads=[cen], writes=[var])
                        k.ts("dve", V(var), V(var), 1.0 / 64, ALU.mult, 64e-5, ALU.add)
                        k.act(V(var), V(var), AF.Sqrt)
                        k.recip(V(var), V(var))
                        k.tt("dve", V(oacc), V(oacc), (var, b3(var)), ALU.mult)
                        rb = lambda t: t[:, h * 64:(h + 1) * 64].rearrange("p (o d) -> p o d", o=1).to_broadcast([64, NCH, CH])
                        k.tt("pool", V(oacc), V(oacc), (lnw, rb(lnw)), ALU.mult)
                        k.tt("dve", V(oacc), V(oacc), (lnb, rb(lnb)), ALU.add)
                        k.tt("pool", V(cen), V(Vt), (bonall, bonall[:, h, :].rearrange("p (c o) -> p c o", o=1).to_broadcast([64, NCH, CH])), ALU.mult)
                        k.tt("dve", V(oacc), V(oacc), V(cen), ALU.add)
                        k.tt("dve", V(oacc), V(oacc), V(gt), ALU.mult)
                        k.dma((YM, YM[:, h * 64:(h + 1) * 64].rearrange("(c p) d -> p c d", p=CH), ("r", h)), V(oacc))
                    P.barrier()

        SEGS = ((0, LC), (LC, SEQ))

        def stage_conv(PF, PC, specs):
            with ExitStack() as ph:
                u = [P.sb(f"cu{i}", [128, L + 8], F32, es=ph) for i in range(2)]
                acc = [P.sb(f"ca{i}", [128, L], F32, es=ph) for i in range(2)]
                cw = P.sb("cw", [128, 20, 5], F32, es=ph)
                cb = P.sb("cb", [128, 20], F32, es=ph)
                for i in range(2):
                    k.memset("pool", (u[i], u[i][:, 0:2], "h0"), 0.0)
                    k.memset("pool", (u[i], u[i][:, LC + 2:LC + 6], "h1"), 0.0)
                    k.memset("pool", (u[i], u[i][:, L + 6:L + 8], "h2"), 0.0)
                bi = 0
                for (row0, nblk, wsrc, bsrc) in specs:
                    for b in range(nblk):
                        k.dma((cw, cw[:, bi, :], bi), (wsrc[0], wsrc[1][:, b * 128:(b + 1) * 128].rearrange("j p -> p j")), allow_slow_non_contiguous=True)
                        k.dma((cb, cb[:, bi:bi + 1], bi), (bsrc[0], bsrc[1][b * 128:(b + 1) * 128].rearrange("(p o) -> p o", o=1)), allow_slow_non_contiguous=True)
                        u_ = u[bi % 2]; a_ = acc[bi % 2]
                        r0 = row0 + b * 128
                        k.dma((u_, u_[:, 2:LC + 2], "c"), (PF, PF[r0:r0 + 128, 0:LC]))
                        k.dma((u_, u_[:, LC + 6:L + 6], "l"), (PF, PF[r0:r0 + 128, LC:L]))
                        for (s0, sl_) in SEGS:
                            off = 0 if s0 == 0 else 4
                            for j in range(5):
                                src = (u_, u_[:, s0 + off + j:s0 + off + j + sl_])
                                dst = (a_, a_[:, s0:s0 + sl_], s0)
                                if j == 0:
                                    k.ts("dve", dst, src, (cw, cw[:, bi, 0:1], bi), ALU.mult)
                                else:
                                    k.stt("dve", dst, src, (cw, cw[:, bi, j:j + 1], bi), dst, ALU.mult, ALU.add)
                            k.act((a_, a_[:, s0:s0 + sl_], s0), (a_, a_[:, s0:s0 + sl_], s0), AF.Silu, bias=(cb, cb[:, bi:bi + 1], bi))
                        k.dma((PC, PC[r0:r0 + 128, :], r0), V(a_))
                        bi += 1
                P.barrier()

        def mixer_mlstm(PC, PF, PT, YM, GSD):
            TB = [(t0_, min(512, L - t0_)) for t0_ in range(0, L, 512)]
            with ExitStack() as ph:
                GI = P.sb("GI", [8, L], F32, es=ph)
                GF = P.sb("GF", [8, L], F32, es=ph)
                t1 = P.sb("gt1", [8, L], F32, es=ph)
                t2 = P.sb("gt2", [8, L], F32, es=ph)
                t3 = P.sb("gt3", [8, L], F32, es=ph)
                rst = P.sb("grst", [8, L], F32, es=ph)
                ib = P.sb("ib", [8, 1], F32, es=ph)
                fb = P.sb("fb", [8, 1], F32, es=ph)
                k.dma(V(GI), (PF, PF[2560:2568, :]))
                k.dma(V(GF), (PF, PF[2568:2576, :]))
                k.dma(V(ib), V(I["mlstm_i_bias"], I["mlstm_i_bias"][0].rearrange("z (h o) -> (z h) o", o=1)), allow_slow_non_contiguous=True)
                k.dma(V(fb), V(I["mlstm_f_bias"], I["mlstm_f_bias"][0].rearrange("z (h o) -> (z h) o", o=1)), allow_slow_non_contiguous=True)
                k.memset("pool", V(rst), 1.0)
                k.memset("pool", (rst, rst.a().rearrange("p (c t) -> p c t", t=CH)[:, :, 0:1]), 0.0)
                k.ts("dve", V(GI), V(GI), (ib, ib[:, 0:1]), ALU.add)
                k.ts("dve", V(fb), V(fb), -1.0, ALU.mult)
                k.act(V(t1), V(GF), AF.Exp, bias=(fb, fb[:, 0:1]), scale=-1.0)
                k.act(V(t1), V(t1), AF.Ln, bias=1.0)
                k.ts("dve", V(t1), V(t1), -1.0, ALU.mult)
                k.scan(V(t2), V(rst), V(t1), 0.0, ALU.mult, ALU.add)
                k.tt("dve", V(t3), V(t1), V(t2), ALU.subtract)
                p3 = t2.a().rearrange("p (c t) -> p c t", t=CH)
                k.tt("dve", (t3, t3.a().rearrange("p (c t) -> p c t", t=CH)), (t3, t3.a().rearrange("p (c t) -> p c t", t=CH)),
                     (t2, p3[:, :, CH - 1:CH].to_broadcast([8, NCH, CH])), ALU.add)
                k.dma((GSD, GSD[0:4, :], 0), (t2, t2[0:4, :]))
                k.dma((GSD, GSD[4:8, :], 1), (t3, t3[4:8, :]))
                k.tt("dve", V(t2), V(GI), V(t2), ALU.subtract)
                k.tt("dve", V(t3), V(GI), V(t3), ALU.subtract)
                k.dma((GSD, GSD[8:12, :], 2), (t2, t2[0:4, :]))
                k.dma((GSD, GSD[12:16, :], 3), (t3, t3[4:8, :]))
                P.barrier()
            with ExitStack() as ph:
                masks = make_masks(ph)
                nwB = P.sb("mnwB", [64, 1024], F32, es=ph)
                k.dma(V(nwB), V(I["mlstm_norm_w"], I["mlstm_norm_w"][0].partition_broadcast(64)))
                oacc = P.sb("moacc", [64, NCH, 256], F32, es=ph)
                ssn = P.sb("mssn", [64, NCH], F32, es=ph)
                for h in range(4):
                    with ExitStack() as hs1:
                        vaug = P.sb("vaug", [64, NCH, 257], BF16, es=hs1)
                        t_q = P.sb("mt_q", [128, 512], F32, es=hs1)
                        t_k = P.sb("mt_k", [128, 512], F32, es=hs1)
                        t_e = P.sb("mt_e", [128, 512], F32, es=hs1)
                        t_s = P.sb("mt_s", [128, 512], F32, es=hs1)
                        cd = P.sb("mcd", [128, NCH], F32, es=hs1)
                        q_in = P.sb("mq_in", [128, L], BF16, es=hs1)
                        k_in = P.sb("mk_in", [128, L], BF16, es=hs1)
                        k_end = P.sb("mk_end", [128, L], BF16, es=hs1)
                        kET = P.sb("mkET", [64, NCH, 128], BF16, es=hs1)
                        S = P.sb("mS", [128, 257], F32, es=hs1)
                        Sb = P.sb("mSb", [128, 257], BF16, es=hs1)
                        Am = [P.sb(f"mAm{i}", [64, 64], BF16, es=hs1) for i in range(2)]
                        dn = P.sb("mdn", [64, 2], F32, es=hs1)
                        kvS = [P.sb(f"mkvS{i}", [128, 257], F32, es=hs1) for i in range(2)]
                        psA = P.ps("mpsA", [64, 64], F32, es=hs1)
                        pso = P.ps("mpso", [64, 257], F32, es=hs1)
                        pskv = P.ps("mpskv", [128, 257], F32, es=hs1)
                        pst = [P.ps(f"mpst{i}", [64, 4, 128], BF16, es=hs1) for i in range(2)]
                        k.dma((vaug, vaug[:, :, 0:256], "v"), (PT, PT[:, 1056 + h * 256:1056 + (h + 1) * 256].rearrange("(c p) d -> p c d", p=CH)), eng="pool")
                        k.memset("pool", (vaug, vaug[:, :, 256:257], "o"), 1.0)
                        k.memset("pool", V(oacc), 0.0)
                        for z in range(2):
                            end = CH - 1 if z == 0 else 0
                            for (t0_, tw) in TB:
                                tsl = slice(t0_, t0_ + tw)
                                ncb = tw // CH
                                c0 = t0_ // CH
                                k.dma((t_e, t_e[:, 0:tw]), (GSD, GSD[z * 4 + h, tsl].partition_broadcast(128)))
                                k.dma((t_s, t_s[:, 0:tw]), (GSD, GSD[8 + z * 4 + h, tsl].partition_broadcast(128)))
                                k.dma((t_q, t_q[:, 0:tw]), (PC, PC[1536 + h * 128:1536 + (h + 1) * 128, tsl]))
                                k.dma((t_k, t_k[:, 0:tw]), (PC, PC[2048 + h * 128:2048 + (h + 1) * 128, tsl]))
                                k.act((t_e, t_e[:, 0:tw]), (t_e, t_e[:, 0:tw]), AF.Exp)
                                k.act((t_s, t_s[:, 0:tw]), (t_s, t_s[:, 0:tw]), AF.Exp)
                                e3 = t_e[:, 0:tw].rearrange("p (c t) -> p c t", t=CH)
                                k.cp("pool", (cd, cd[:, c0:c0 + ncb]), (t_e, e3[:, :, end]))
                                k.tt("dve", (q_in, q_in[:, tsl]), (t_q, t_q[:, 0:tw]), (t_e, t_e[:, 0:tw]), ALU.mult)
                                k.stt("dve", (t_k, t_k[:, 0:tw]), (t_k, t_k[:, 0:tw]), 128 ** -0.5, (t_s, t_s[:, 0:tw]), ALU.mult, ALU.mult)
                                k.cp("pool", (k_in, k_in[:, tsl]), (t_k, t_k[:, 0:tw]))
                                k.tt("pool", (k_end, k_end[:, tsl].rearrange("p (c t) -> p c t", t=CH)),
                                     (t_k, t_k[:, 0:tw].rearrange("p (c t) -> p c t", t=CH)),
                                     (t_e, e3[:, :, end:end + 1].to_broadcast([128, ncb, CH])), ALU.mult)
                            for c4 in range(0, NCH, 4):
                                p_ = pst[(c4 // 4) % 2]
                                for j in range(4):
                                    c = c4 + j
                                    k.tr((p_, p_[:, j, :]), (k_end, k_end[:, c * CH:(c + 1) * CH]), V(identb))
                                k.cp("act", (kET, kET[:, c4:c4 + 4, :]), V(p_))
                            k.memset("pool", V(S), 0.0)
                            k.memset("pool", V(Sb), 0.0)
                            order_ = chunk_order(z)

                            def ml_A(step):
                                c = order_[step]
                                csl = slice(c * CH, (c + 1) * CH)
                                am = Am[step % 2]
                                k.mm(V(psA), (k_in, k_in[:, csl]), (q_in, q_in[:, csl]))
                                k.tt("dve", V(am), V(psA), V(masks[z]), ALU.mult)
                                k.mm(V(pskv), (kET, kET[:, c, :]), (vaug, vaug[:, c, :]))
                                k.cp("act", V(kvS[step % 2]), V(pskv))

                            def ml_B(step):
                                c = order_[step]
                                csl = slice(c * CH, (c + 1) * CH)
                                am = Am[step % 2]
                                k.mm(V(pso), V(am), (vaug, vaug[:, c, :]), start=True, stop=False)
                                k.mm(V(pso), (q_in, q_in[:, csl]), V(Sb), start=False, stop=True)
                                k.act((dn, dn[:, 0:1]), (pso, pso[:, 256:257]), AF.Abs)
                                k.ts("dve", (dn, dn[:, 0:1]), (dn, dn[:, 0:1]), 1.0, ALU.max)
                                k.recip((dn, dn[:, 1:2]), (dn, dn[:, 0:1]))
                                k.stt("dve", (oacc, oacc[:, c, :], c), (pso, pso[:, 0:256]), (dn, dn[:, 1:2]), (oacc, oacc[:, c, :], c), ALU.mult, ALU.add)
                                k.stt("dve", V(S), V(S), (cd, cd[:, c:c + 1]), V(kvS[step % 2]), ALU.mult, ALU.add)
                                k.cp("act", V(Sb), V(S))

                            ml_A(0)
                            for step in range(NCH):
                                if step + 1 < NCH:
                                    ml_A(step + 1)
                                ml_B(step)
                    P.barrier()
                    with ExitStack() as hs2:
                        gt = P.sb("mgt", [64, NCH, 256], F32, es=hs2)
                        k.tt("dve", V(gt), V(oacc), V(oacc), ALU.mult)
                        P.op("dve", lambda e: e.tensor_reduce(out=ssn.a(), in_=gt.a(), axis=AX.X, op=ALU.add), reads=[gt], writes=[ssn])
                        k.ts("dve", V(ssn), V(ssn), 1.0 / 256, ALU.mult, EPS, ALU.add)
                        k.act(V(ssn), V(ssn), AF.Sqrt)
                        k.recip(V(ssn), V(ssn))
                        k.tt("dve", V(oacc), V(oacc), (ssn, ssn.a().rearrange("p (c o) -> p c o", o=1).to_broadcast([64, NCH, 256])), ALU.mult)
                        k.tt("pool", V(oacc), V(oacc), (nwB, nwB[:, h * 256:(h + 1) * 256].rearrange("p (o d) -> p o d", o=1).to_broadcast([64, NCH, 256])), ALU.mult)
                        k.dma(V(gt), (PT, PT[:, 2080 + h * 256:2080 + (h + 1) * 256].rearrange("(c p) d -> p c d", p=CH)))
                        for q4 in range(4):
                            k.act((gt, gt[:, q4 * 17:(q4 + 1) * 17, :], q4), (gt, gt[:, q4 * 17:(q4 + 1) * 17, :], q4), AF.Sigmoid)
                        k.tt("dve", V(oacc), V(oacc), V(gt), ALU.mult)
                        k.dma((YM, YM[:, 1024 + h * 256:1024 + (h + 1) * 256].rearrange("(c p) d -> p c d", p=CH), ("m", h)), V(oacc))
                    P.barrier()

        def stage_xbt(PC, XBT):
            with ExitStack() as ph:
                ft = [P.sb(f"xft{i}", [128, 10, 128], F32, es=ph) for i in range(2)]
                ot = [P.sb(f"xot{i}", [128, 10, 128], F32, es=ph) for i in range(2)]
                pt = [P.ps(f"xpt{i}", [128, 4, 128], F32, es=ph) for i in range(3)]
                for tt in range(NT):
                    f_ = ft[tt % 2]; o_ = ot[tt % 2]
                    k.dma(V(f_), (PC, PC[0:1280, tt * 128:(tt + 1) * 128].rearrange("(b p) t -> p b t", p=128)))
                    for gi, (b0, nb_) in enumerate(((0, 4), (4, 4), (8, 2))):
                        p_ = pt[gi]
                        for j in range(nb_):
                            k.tr((p_, p_[:, j, :]), (f_, f_[:, b0 + j, :]), V(ident))
                        k.cp("act" if gi % 2 else "dve", (o_, o_[:, b0:b0 + nb_, :]), (p_, p_[:, 0:nb_, :]))
                    k.dma((XBT, XBT[tt * 128:(tt + 1) * 128, :], tt), (o_, o_.a().rearrange("p b c -> p (b c)")))
                P.barrier()

        def mixer_ssd(PC, PT, XBT, YS):
            with ExitStack() as ph:
                masks = make_masks(ph)
                ones64 = P.sb("sones64", [64, 64], F32, es=ph)
                k.memset("pool", V(ones64), 1.0)
                selend = []
                for z in range(2):
                    se = P.sb(f"selend{z}", [64, 128], F32, es=ph)
                    endp = CH - 1 if z == 0 else 0
                    P.op("pool", lambda e, se=se, endp=endp: e.affine_select(out=se.a(), in_=ones[0:64, :], pattern=[[0, 128]], compare_op=ALU.is_equal,
                                                                           fill=0.0, base=-endp, channel_multiplier=1), reads=[ones], writes=[se])
                    selend.append(se)
                dtT = P.sb("dtT", [64, NCH, 32], F32, es=ph)
                laT = P.sb("laT", [64, NCH, 32], F32, es=ph)
                dbB = P.sb("dbB", [64, 32], F32, es=ph)
                naB = P.sb("naB", [64, 32], F32, es=ph)
                dskB = P.sb("dskB", [64, 16], F32, es=ph)
                k.dma(V(dbB), V(I["ssd_dt_bias"], I["ssd_dt_bias"][0].rearrange("z h -> (z h)").partition_broadcast(64)))
                k.dma(V(naB), V(I["ssd_a_log"], I["ssd_a_log"][0].rearrange("z h -> (z h)").partition_broadcast(64)))
                k.dma(V(dskB), V(I["ssd_d"], I["ssd_d"][0].partition_broadcast(64)))
                k.act(V(naB), V(naB), AF.Exp)
                k.ts("dve", V(naB), V(naB), -1.0, ALU.mult)
                k.dma(V(dtT), (PT, PT[:, 1024:1056].rearrange("(c p) d -> p c d", p=CH)))
                k.tt("dve", V(dtT), V(dtT), (dbB, dbB.a().rearrange("p (o d) -> p o d", o=1).to_broadcast([64, NCH, 32])), ALU.add)
                k.act(V(dtT), V(dtT), AF.Exp)
                k.act(V(dtT), V(dtT), AF.Ln, bias=1.0)
                k.tt("dve", V(laT), V(dtT), (naB, naB.a().rearrange("p (o d) -> p o d", o=1).to_broadcast([64, NCH, 32])), ALU.mult)
                yacc = P.sb("yacc", [64, NCH, 256], F32, es=ph)
                for q4 in range(4):
                    g = q4 // 2
                    with ExitStack() as qs:
                        xq = P.sb("xq", [64, NCH, 256], BF16, es=qs)
                        Bf = P.sb("Bf", [128, L], BF16, es=qs)
                        Cf = P.sb("Cf", [128, L], BF16, es=qs)
                        BTt = P.sb("BTt", [64, NCH, 128], BF16, es=qs)
                        k.dma(V(xq), (XBT, XBT[:, q4 * 256:(q4 + 1) * 256].rearrange("(c p) d -> p c d", p=CH)), eng="pool")
                        k.dma(V(BTt), (XBT, XBT[:, 1024 + g * 128:1024 + (g + 1) * 128].rearrange("(c p) d -> p c d", p=CH)), eng="pool")
                        k.dma(V(Bf), (PC, PC[1024 + g * 128:1024 + (g + 1) * 128, :]), eng="pool")
                        k.dma(V(Cf), (PC, PC[1280 + g * 128:1280 + (g + 1) * 128, :]), eng="pool")
                        k.memset("pool", V(yacc), 0.0)
                        hs = [P.sb(f"hs{z}", [128, 256], F32, es=qs) for z in range(2)]
                        hsb = [P.sb(f"hsb{z}", [128, 256], BF16, es=qs) for z in range(2)]
                        CBm = [P.sb(f"CBm{z}", [64, 64], F32, es=qs) for z in range(2)]
                        acs = [P.sb(f"acs{z}", [64, 4], F32, es=qs) for z in range(2)]
                        dg = [P.sb(f"dg{z}", [64, 4, 64], F32, es=qs) for z in range(2)]
                        seg = [P.sb(f"seg{z}", [64, 4, 64], F32, es=qs) for z in range(2)]
                        AT = [P.sb(f"AT{z}", [64, 4, 64], BF16, es=qs) for z in range(2)]
                        xdt = [P.sb(f"xdt{z}", [64, 4, 64], BF16, es=qs) for z in range(2)]
                        xde = [P.sb(f"xde{z}", [64, 4, 64], BF16, es=qs) for z in range(2)]
                        din = [[P.sb(f"din{z}_{i}", [64, 4], F32, es=qs) for i in range(2)] for z in range(2)]
                        phsS = [[P.sb(f"phsS{z}_{i}", [128, 256], F32, es=qs) for i in range(2)] for z in range(2)]
                        dend = [P.sb(f"dend{z}", [64, 4], F32, es=qs) for z in range(2)]
                        dchB = [[P.sb(f"dchB{z}_{i}", [128, 4], F32, es=qs) for i in range(2)] for z in range(2)]
                        ytmp = [P.sb(f"ytmp{z}", [64, 4, 64], F32, es=qs) for z in range(2)]
                        pCB = P.ps("pCB", [64, 64], F32, es=qs)
                        pac = P.ps("pac", [64, 4], F32, es=qs)
                        pbc = P.ps("pbc", [64, 4, 64], F32, es=qs)
                        py = P.ps("py", [64, 4, 64], F32, es=qs)
                        pyi = P.ps("pyi", [64, 4, 64], F32, es=qs)
                        phs = P.ps("phs", [128, 256], F32, es=qs)
                        pdc = P.ps("pdc", [128, 4], F32, es=qs)
                        for z in range(2):
                            k.memset("pool", V(hs[z]), 0.0)
                            k.memset("pool", V(hsb[z]), 0.0)
                        orders = [chunk_order(0), chunk_order(1)]
                        b4 = lambda t: t.a().rearrange("p (h o) -> p h o", o=1).to_broadcast([64, 4, 64])

                        def ssd_A(step, z):
                            c = orders[z][step]
                            par = step % 2
                            csl = slice(c * CH, (c + 1) * CH)
                            end = CH - 1 if z == 0 else 0
                            hsl = slice(z * 16 + q4 * 4, z * 16 + q4 * 4 + 4)
                            k.mm(V(pCB), (Bf, Bf[:, csl]), (Cf, Cf[:, csl]))
                            k.tt("dve", V(CBm[z]), V(pCB), V(masks[z]), ALU.mult)
                            k.mm(V(pac), V(masks[z]), (laT, laT[:, c, hsl]))
                            k.cp("act", V(acs[z]), V(pac))
                            k.tt("pool", V(dg[z]), (ident, ident[0:64, 0:64].rearrange("p (o t) -> p o t", o=1).to_broadcast([64, 4, 64])),
                                 (acs[z], b4(acs[z])), ALU.mult)
                            k.mm(V(pbc), V(ones64), (dg[z], dg[z].a().rearrange("p h t -> p (h t)")))
                            k.tt("dve", V(seg[z]), V(pbc), (acs[z], b4(acs[z])), ALU.subtract)
                            k.tt("dve", V(dend[z]), (pbc, pbc[:, :, end]), V(acs[z]), ALU.subtract)
                            k.act(V(seg[z]), V(seg[z]), AF.Exp)
                            k.act(V(dend[z]), V(dend[z]), AF.Exp)
                            k.act(V(din[z][par]), V(acs[z]), AF.Exp)
                            k.stt("dve", V(AT[z]), V(seg[z]), 1.0, (CBm[z], CBm[z].a().rearrange("p (o t) -> p o t", o=1).to_broadcast([64, 4, 64])),
                                  ALU.min, ALU.mult)
                            k.tt("pool", V(xdt[z]), (xq, xq[:, c, :].rearrange("p (h d) -> p h d", d=64)),
                                 (dtT, dtT[:, c, hsl].rearrange("p (h o) -> p h o", o=1).to_broadcast([64, 4, 64])), ALU.mult)
                            for i in range(4):
                                k.mm((py, py[:, i, :]), (AT[z], AT[z][:, i, :]), (xdt[z], xdt[z][:, i, :]))
                            ya = (yacc, yacc[:, c, :].rearrange("p (h d) -> p h d", d=64), c)
                            k.tt("dve", ya, V(py), ya, ALU.add)
                            k.tt("pool", V(xde[z]), V(xdt[z]), (dend[z], b4(dend[z])), ALU.mult)
                            k.mm(V(phs), (BTt, BTt[:, c, :]), (xde[z], xde[z].a().rearrange("p h d -> p (h d)")))
                            k.cp("act", V(phsS[z][par]), V(phs))
                            k.mm(V(pdc), V(selend[z]), V(acs[z]))
                            k.act(V(dchB[z][par]), V(pdc), AF.Exp)

                        def ssd_B(step, z):
                            c = orders[z][step]
                            par = step % 2
                            csl = slice(c * CH, (c + 1) * CH)
                            k.mm(V(pyi), (Cf, Cf[:, csl]), V(hsb[z]))
                            ya = (yacc, yacc[:, c, :].rearrange("p (h d) -> p h d", d=64), c)
                            k.tt("dve", V(ytmp[z]), V(pyi), (din[z][par], b4(din[z][par])), ALU.mult)
                            k.tt("pool", ya, ya, V(ytmp[z]), ALU.add)
                            h3 = (hs[z], hs[z].a().rearrange("p (h d) -> p h d", d=64))
                            k.tt("pool", h3, h3, (dchB[z][par], dchB[z][par].a().rearrange("p (h o) -> p h o", o=1).to_broadcast([128, 4, 64])), ALU.mult)
                            k.tt("dve", V(hs[z]), V(phsS[z][par]), V(hs[z]), ALU.add)
                            k.cp("act", V(hsb[z]), V(hs[z]))

                        for z in range(2):
                            ssd_A(0, z)
                        for step in range(NCH):
                            if step + 1 < NCH:
                                for z in range(2):
                                    ssd_A(step + 1, z)
                            for z in range(2):
                                ssd_B(step, z)
                    P.barrier()
                    with ExitStack() as fs:
                        xf = P.sb("sxf", [64, 17, 256], F32, es=fs)
                        zf = P.sb("szf", [64, 17, 256], F32, es=fs)
                        for c17 in range(4):
                            cs_ = slice(c17 * 17, (c17 + 1) * 17)
                            rows = slice(c17 * 17 * CH, (c17 + 1) * 17 * CH)
                            k.dma(V(xf), (XBT, XBT[rows, q4 * 256:(q4 + 1) * 256].rearrange("(c p) d -> p c d", p=CH)))
                            k.dma(V(zf), (PT, PT[rows, q4 * 256:(q4 + 1) * 256].rearrange("(c p) d -> p c d", p=CH)))
                            dsk4 = dskB[:, q4 * 4:(q4 + 1) * 4].rearrange("p (a h o) -> p a h o", a=1, o=1).to_broadcast([64, 17, 4, 64])
                            k.tt("pool", (xf, xf.a().rearrange("p c (h d) -> p c h d", d=64)), (xf, xf.a().rearrange("p c (h d) -> p c h d", d=64)),
                                 (dskB, dsk4), ALU.mult)
                            k.tt("dve", V(xf), V(xf), (yacc, yacc[:, cs_, :], ("f", c17)), ALU.add)
                            k.act(V(zf), V(zf), AF.Silu)
                            k.tt("dve", V(xf), V(xf), V(zf), ALU.mult)
                            k.dma((YS, YS[rows, q4 * 256:(q4 + 1) * 256].rearrange("(c p) d -> p c d", p=CH), (q4, c17)), V(xf))
                    P.barrier()

        def stage_ssd_norm(YS, YM):
            with ExitStack() as ph:
                nwB = P.sb("snwB", [128, 1024], F32, es=ph)
                k.dma(V(nwB), V(I["ssd_norm_w"], I["ssd_norm_w"][0].partition_broadcast(128)))
                yt = [P.sb(f"syt{i}", [128, 1024], F32, es=ph) for i in range(2)]
                sq = P.sb("ssq", [128, 1024], F32, es=ph)
                st = [P.sb(f"sst{i}", [128, 2], F32, es=ph) for i in range(2)]
                for tt in range(NT):
                    y_ = yt[tt % 2]; s_ = st[tt % 2]
                    k.dma(V(y_), (YS, YS[tt * 128:(tt + 1) * 128, :]))
                    k.tt("pool", V(sq), V(y_), V(y_), ALU.mult)
                    P.op("dve", lambda e, s_=s_: e.tensor_reduce(out=s_.a(), in_=sq.a().rearrange("p (g d) -> p g d", g=2), axis=AX.X, op=ALU.add),
                         reads=[sq], writes=[s_])
                    k.ts("dve", V(s_), V(s_), 1.0 / 512, ALU.mult, EPS, ALU.add)
                    k.act(V(s_), V(s_), AF.Sqrt)
                    k.recip(V(s_), V(s_))
                    k.tt("dve", (y_, y_.a().rearrange("p (g d) -> p g d", g=2)), (y_, y_.a().rearrange("p (g d) -> p g d", g=2)),
                         (s_, s_.a().rearrange("p (g o) -> p g o", o=1).to_broadcast([128, 2, 512])), ALU.mult)
                    k.tt("pool", V(y_), V(y_), V(nwB), ALU.mult)
                    k.dma((YM, YM[tt * 128:(tt + 1) * 128, 0:1024], ("s", tt)), V(y_))
                P.barrier()

        def stage_final():
            with ExitStack() as ph:
                fnB = P.sb("fnB", [128, D], F32, es=ph)
                k.dma(V(fnB), V(I["final_norm_w"], I["final_norm_w"].a().partition_broadcast(128)))
                xt = [P.sb(f"fxt{i}", [128, D], F32, es=ph) for i in range(2)]
                junk = P.sb("fjunk", [128, D], F32, es=ph)
                st = [P.sb(f"fst{i}", [128, 4], F32, es=ph) for i in range(2)]
                for i in range(SEQ // 128):
                    x_ = xt[i % 2]; s_ = st[i % 2]
                    k.dma(V(x_), (XL, XL[LC + i * 128:LC + (i + 1) * 128, :], i))
                    k.memset("pool", (s_, s_[:, 0:1]), 0.0)
                    k.act(V(junk), V(x_), AF.Square, accum=(s_, s_[:, 0:1]))
                    k.ts("dve", (s_, s_[:, 1:2]), (s_, s_[:, 0:1]), 1.0 / D, ALU.mult, EPS, ALU.add)
                    k.act((s_, s_[:, 2:3]), (s_, s_[:, 1:2]), AF.Sqrt)
                    k.recip((s_, s_[:, 3:4]), (s_, s_[:, 2:3]))
                    k.ts("dve", V(x_), V(x_), (s_, s_[:, 3:4]), ALU.mult)
                    k.tt("pool", V(x_), V(x_), V(fnB), ALU.mult)
                    k.dma((OUT, OUT[i * 128:(i + 1) * 128, :], i), V(x_))
                P.barrier()

        def src0(i):
            if i < 2:
                return (I["ctx"], I["ctx"][i * 128:(i + 1) * 128, :])
            return (I["x"], I["x"][(i - 2) * 128:(i - 1) * 128, :])

        def src1(i):
            return (XL, XL[i * 128:(i + 1) * 128, :])

        def layer0():
            stage_mods(0)
            if "modF" in debug:
                dm = P.dram("d_modF", [128, 6 * KO * 2], F32, kind="ExternalOutput")
                k.dma(V(dm), (modF, modF.a().rearrange("p a b c -> p (a b c)")))
                dm2 = P.dram("d_modB", [128, 4 * D], F32, kind="ExternalOutput")
                k.dma(V(dm2), (modB, modB.a().rearrange("p a b c -> p (a b c)")))
            PF0 = scratch("PF0", [3456, L])
            PT0 = scratch("PT0", [L, 1024])
            YM0 = scratch("YM0", [L, 1024])
            with ExitStack() as lay:
                HT = P.sb("HT", [128, KO, L], BF16, es=lay)
                stage_norm(0, 1, HT, src0, perm=False)
                if "HT0" in debug:
                    dh = P.dram("d_HT0", [128, KO * L], BF16, kind="ExternalOutput")
                    k.dma(V(dh), (HT, HT.a().rearrange("p a b -> p (a b)")))
                if stop_after == "norm0":
                    return
                stage_proj(HT, (I["even_w_in"], I["even_w_in"][0]), 4480, [(0, 3456, PF0, 0)], [(3456, 4480, PT0, 0)])
            if stop_after == "proj0":
                return
            if "skip_hgrn" not in debug:
                mixer_hgrn(PF0, PT0, YM0)
            PR0 = scratch("PR0", [1920, L])
            GT0 = scratch("GT0", [L, 512])
            rwkv_shift(PF0, PR0)
            if "no_gate" not in debug:
                rwkv_gate(PR0, GT0)
            if stop_after == "shift0":
                return
            if "skip_rwkv" not in debug:
                if "rwkv_old" in debug:
                    mixer_rwkv(PR0, GT0, YM0)
                else:
                    mixer_rwkv2(PR0, GT0, YM0)
            if stop_after == "mix0":
                return
            stage_outproj(YM0, 1024, (I["even_w_out"], I["even_w_out"][0]), src0, False, list(range(NT)))
            if stop_after == "out0":
                return
            stage_moe(0, list(range(NT)))

        def layer1():
            stage_mods(1)
            PF1 = scratch("PF1", [2576, L])
            PT1 = scratch("PT1", [L, 3104])
            PC1 = scratch("PC1", [2560, L])
            YM1 = scratch("YM1", [L, 2048])
            GSD = scratch("GSD", [16, L])
            YS = scratch("YS", [L, 1024])
            with ExitStack() as lay:
                HT = P.sb("HT", [128, KO, L], BF16, es=lay)
                stage_norm(1, 1, HT, src1, perm=True)
                stage_proj(HT, (I["odd_w_in"], I["odd_w_in"][0]), 5680,
                           [(1024, 2560, PF1, 0), (2592, 3616, PF1, 1536), (5664, 5680, PF1, 2560)],
                           [(0, 1024, PT1, 0), (2560, 2592, PT1, 1024), (3616, 5664, PT1, 1056)])
            stage_conv(PF1, PC1, [(0, 12, (I["ssd_conv_w"], I["ssd_conv_w"][0]), (I["ssd_conv_b"], I["ssd_conv_b"][0])),
                                  (1536, 8, (I["mlstm_conv_w"], I["mlstm_conv_w"][0]), (I["mlstm_conv_b"], I["mlstm_conv_b"][0]))])
            if stop_after == "L1conv":
                return
            if "skip_mlstm" not in debug:
                mixer_mlstm(PC1, PF1, PT1, YM1, GSD)
            if stop_after == "L1mlstm":
                return
            XBT = scratch("XBT", [L, 1280])
            stage_xbt(PC1, XBT)
            mixer_ssd(PC1, PT1, XBT, YS)
            stage_ssd_norm(YS, YM1)
            if stop_after == "L1ssd":
                return
            stage_outproj(YM1, 2048, (I["odd_w_out"], I["odd_w_out"][0]), src1, True, list(range(2, NT)))
            if stop_after == "L1out":
                return
            stage_moe(1, list(range(2, NT)))
            stage_final()

        if "L1only" in debug:
            XLin = P.dram("XLin", [L, D], F32, kind="ExternalInput")
            for i4 in range(4):
                k.dma((XL, XL[i4 * 1088:(i4 + 1) * 1088, :], ("in", i4)), (XLin, XLin[i4 * 1088:(i4 + 1) * 1088, :]))
            P.barrier()
        else:
            layer0()
        if stop_after is None or stop_after.startswith("L1"):
            layer1()
        P.finish()
    return nc


_NC = None


def kernel(**inputs):
    global _NC
    if _NC is None:
        _NC = build()
    nc = _NC
    n = 8
    in_maps = []
    for b in range(n):
        m = {}
        for kk_, v in inputs.items():
            v = np.asarray(v)
            if kk_ == "x":
                m[kk_] = np.ascontiguousarray(v[b])
            elif kk_ == "c":
                m[kk_] = np.ascontiguousarray(v[b])
            elif kk_ == "ctx":
                m[kk_] = np.ascontiguousarray(v[b])
            else:
                m[kk_] = v
        in_maps.append(m)
    res = run_bass_kernel_spmd(nc, in_maps, core_ids=list(range(n)))
    return np.stack([r["out"] for r in res.results], axis=0)
```

```python
import numpy as np
import concourse.bass as bass
import concourse.mybir as mybir
from concourse.bass_utils import run_bass_kernel_spmd
from contextlib import ExitStack

F32 = mybir.dt.float32
BF16 = mybir.dt.bfloat16
AF = mybir.ActivationFunctionType
ALU = mybir.AluOpType
AX = mybir.AxisListType

ENGS = ("pe", "dve", "act", "pool", "sp")
D = 1024
KO = 8
LC = 256
SEQ = 4096
L = LC + SEQ
NT = L // 128
EPS = 1e-6
CH = 64
NCH = L // CH


class Cell:
    __slots__ = ("w", "r")

    def __init__(self):
        self.w = None
        self.r = []


class Buf:
    def __init__(self, name, h, is_dram=False, is_psum=False):
        self.name = name
        self.h = h
        self.is_dram = is_dram
        self.is_psum = is_psum
        self.base = Cell()
        self.parts = {}

    def cells(self, key):
        if key is None:
            return [self.base] + list(self.parts.values())
        c = self.parts.get(key)
        if c is None:
            c = Cell()
            c.w = self.base.w
            c.r = list(self.base.r)
            self.parts[key] = c
        return [c]

    def __getitem__(self, idx):
        return self.h[idx]

    def a(self):
        return self.h[:]


class Prog:
    def __init__(self, nc, es):
        self.nc = nc
        self.es = es
        self.q = {e: [] for e in ENGS}
        self.cnt = {e: 0 for e in ENGS}
        self.sem = {e: es.enter_context(nc.semaphore("s_" + e)) for e in ENGS}
        self.known = {e: {} for e in ENGS}
        self.dsem = {}
        self.dsem_by_id = {}
        self.phase_slots = {}
        self.ninst = 0
        self.uid = 0

    def sb(self, name, shape, dt=F32, es=None):
        self.uid += 1
        h = (es or self.es).enter_context(self.nc.sbuf_tensor(f"{name}_{self.uid}", list(shape), dt))
        return Buf(name, h)

    def ps(self, name, shape, dt=F32, es=None):
        self.uid += 1
        h = (es or self.es).enter_context(self.nc.psum_tensor(f"{name}_{self.uid}", list(shape), dt))
        return Buf(name, h, is_psum=True)

    def dram(self, name, shape, dt=F32, kind="Internal"):
        h = self.nc.dram_tensor(name, list(shape), dt, kind=kind)
        return Buf(name, h.ap(), is_dram=True)

    def dma_sem(self, name):
        if name not in self.dsem:
            self.dsem[name] = [self.es.enter_context(self.nc.semaphore("d_" + name)), 0]
            self.dsem_by_id[id(self.dsem[name][0])] = self.dsem[name]
        return self.dsem[name]

    def _norm(self, lst):
        return [(r, None) if isinstance(r, Buf) else ((r[0], None) if r[0].is_psum else r) for r in lst]

    def _deps(self, eng, reads, writes, pe_acc=False):
        need = {}

        def add(tok):
            if tok is None:
                return
            k = id(tok[0])
            if k not in need or need[k][1] < tok[1]:
                need[k] = tok

        for (b, key) in reads:
            for c in b.cells(key):
                add(c.w)
        for (b, key) in writes:
            for c in b.cells(key):
                if not (pe_acc and c.w is not None and c.w[2] == "pe"):
                    add(c.w)
                for t in c.r:
                    add(t)
        out = []
        kn = self.known[eng]
        for k, tok in need.items():
            val = tok[1]
            if k in self.dsem_by_id:
                val = self.dsem_by_id[k][1]
            if kn.get(k, 0) >= val:
                continue
            kn[k] = val
            out.append((tok[0], val))
        return out

    def _record(self, tok, reads, writes):
        for (b, key) in reads:
            for c in b.cells(key):
                c.r.append(tok)
                if len(c.r) > 16:
                    best = {}
                    for t in c.r:
                        kk = id(t[0])
                        if kk not in best or best[kk][1] < t[1]:
                            best[kk] = t
                    c.r = list(best.values())
        for (b, key) in writes:
            for c in b.cells(key):
                c.w = tok
                c.r = []

    def op(self, eng, fn, reads=(), writes=(), pe_acc=False):
        reads = self._norm(reads)
        writes = self._norm(writes)
        writes = writes + [r for r in reads if r[0].is_psum]
        waits = self._deps(eng, reads, writes, pe_acc)
        self.cnt[eng] += 1
        sem = self.sem[eng]
        tok = (sem, self.cnt[eng], eng)
        self.ninst += 1

        def run(e, waits=waits, fn=fn, sem=sem):
            for s, v in waits:
                e.wait_ge(s, v)
            fn(e).then_inc(sem, 1)

        self.q[eng].append(run)
        self._record(tok, reads, writes)
        return tok

    def dma(self, eng, out_ap, in_ap, reads=(), writes=(), semname=None, **kw):
        reads = self._norm(reads)
        writes = self._norm(writes)
        waits = self._deps(eng, reads, writes)
        if semname is None:
            sbs = [b for (b, _) in list(writes) + list(reads) if not b.is_dram]
            semname = sbs[0].name if sbs else "dram2dram"
        if semname not in self.phase_slots:
            self.phase_slots[semname] = len(self.phase_slots)
        ds = self.dma_sem(f"slot{self.phase_slots[semname]}")
        ds[1] += 16
        tok = (ds[0], ds[1], "dma")
        self.ninst += 1

        def run(e, waits=waits, sem=ds[0]):
            for s, v in waits:
                e.wait_ge(s, v)
            e.dma_start(out=out_ap, in_=in_ap, **kw).then_inc(sem, 16)

        self.q[eng].append(run)
        self._record(tok, reads, writes)
        return tok

    def barrier(self):
        self.phase_slots = {}
        toks = [(self.sem[f], self.cnt[f]) for f in ENGS if self.cnt[f] > 0]
        toks += [(v[0], v[1]) for v in self.dsem.values() if v[1] > 0]
        for e in ENGS:
            kn = self.known[e]
            ws = []
            for s, v in toks:
                if kn.get(id(s), 0) < v:
                    kn[id(s)] = v
                    ws.append((s, v))

            def run(en, ws=ws):
                for s, v in ws:
                    en.wait_ge(s, v)
            self.q[e].append(run)

    def finish(self):
        nc = self.nc
        self.barrier()
        with nc.Block() as block:
            @block.tensor
            def _(e):
                for f in self.q["pe"]:
                    f(e)

            @block.vector
            def _(e):
                for f in self.q["dve"]:
                    f(e)

            @block.scalar
            def _(e):
                for f in self.q["act"]:
                    f(e)

            @block.gpsimd
            def _(e):
                for f in self.q["pool"]:
                    f(e)

            @block.sync
            def _(e):
                for f in self.q["sp"]:
                    f(e)


def _rk(x):
    return (x[0], x[2] if len(x) > 2 else None)


class K:
    def __init__(self, P):
        self.P = P
        self.dq = 0

    def mm(self, out, lhsT, rhs, start=True, stop=True):
        return self.P.op("pe", lambda e: e.matmul(out[1], lhsT=lhsT[1], rhs=rhs[1], start=start, stop=stop),
                         reads=[_rk(lhsT), _rk(rhs)], writes=[_rk(out)], pe_acc=not start)

    def tr(self, out, in_, ident):
        return self.P.op("pe", lambda e: e.transpose(out[1], in_[1], ident[1]),
                         reads=[_rk(in_), _rk(ident)], writes=[_rk(out)])

    def act(self, out, in_, func, bias=None, scale=None, accum=None, eng="act"):
        reads = [_rk(in_)]
        kw = {}
        if bias is not None:
            if isinstance(bias, tuple):
                reads.append(_rk(bias)); kw["bias"] = bias[1]
            else:
                kw["bias"] = bias
        if scale is not None:
            if isinstance(scale, tuple):
                reads.append(_rk(scale)); kw["scale"] = scale[1]
            else:
                kw["scale"] = scale
        writes = [_rk(out)]
        if accum is not None:
            writes.append(_rk(accum)); kw["accum_out"] = accum[1]
        return self.P.op("act", lambda e: e.activation(out=out[1], in_=in_[1], func=func, **kw), reads=reads, writes=writes)

    def tt(self, eng, out, in0, in1, op):
        if eng == "pool":
            eng = "dve"
        return self.P.op(eng, lambda e: e.tensor_tensor(out=out[1], in0=in0[1], in1=in1[1], op=op),
                         reads=[_rk(in0), _rk(in1)], writes=[_rk(out)])

    def ts(self, eng, out, in0, s1, op0, s2=None, op1=None, accum=None):
        reads = [_rk(in0)]
        a1 = s1
        if isinstance(s1, tuple):
            reads.append(_rk(s1)); a1 = s1[1]
        a2 = s2
        if isinstance(s2, tuple):
            reads.append(_rk(s2)); a2 = s2[1]
        kw = {}
        if op1 is not None:
            kw["op1"] = op1
        writes = [_rk(out)]
        if accum is not None:
            writes.append(_rk(accum)); kw["accum_out"] = accum[1]
        if eng == "pool":
            eng = "dve"
        return self.P.op(eng, lambda e: e.tensor_scalar(out=out[1], in0=in0[1], scalar1=a1, scalar2=a2, op0=op0, **kw),
                         reads=reads, writes=writes)

    def stt(self, eng, out, in0, scalar, in1, op0, op1):
        reads = [_rk(in0), _rk(in1)]
        sc = scalar
        if isinstance(scalar, tuple):
            reads.append(_rk(scalar)); sc = scalar[1]
        return self.P.op(eng, lambda e: e.scalar_tensor_tensor(out=out[1], in0=in0[1], scalar=sc, in1=in1[1], op0=op0, op1=op1),
                         reads=reads, writes=[_rk(out)])

    def cp(self, eng, out, in_):
        if eng == "pool":
            eng = "act"
        if eng == "act":
            return self.P.op("act", lambda e: e.copy(out=out[1], in_=in_[1]), reads=[_rk(in_)], writes=[_rk(out)])
        return self.P.op(eng, lambda e: e.tensor_copy(out=out[1], in_=in_[1]), reads=[_rk(in_)], writes=[_rk(out)])

    def memset(self, eng, out, val):
        return self.P.op(eng, lambda e: e.memset(out[1], val), writes=[_rk(out)])

    def scan(self, out, d0, d1, init, op0, op1):
        return self.P.op("dve", lambda e: e.tensor_tensor_scan(out=out[1], data0=d0[1], data1=d1[1], initial=init, op0=op0, op1=op1),
                         reads=[_rk(d0), _rk(d1)], writes=[_rk(out)])

    def recip(self, out, in_):
        return self.P.op("dve", lambda e: e.reciprocal(out=out[1], in_=in_[1]), reads=[_rk(in_)], writes=[_rk(out)])

    def dma(self, out, in_, eng=None, **kw):
        if eng is None:
            eng = "sp" if in_[0].is_dram else "act"
        reads = [_rk(in_)]
        writes = [_rk(out)]
        return self.P.dma(eng, out[1], in_[1], reads=reads, writes=writes, **kw)


def V(buf, ap=None, key=None):
    return (buf, buf.a() if ap is None else ap, key)


def build(debug=(), stop_after=None):
    nc = bass.Bass("TRN2", target_bir_lowering=False)
    es = ExitStack()
    with es:
        P = Prog(nc, es)
        k = K(P)
        I = {}

        def inp(name, shape):
            I[name] = P.dram(name, shape, F32, kind="ExternalInput")

        inp("x", [SEQ, D]); inp("c", [D]); inp("ctx", [LC, D]); inp("c_ctx", [D])
        inp("ada_w", [2, D, 6 * D]); inp("ada_b", [2, 6 * D]); inp("norm1_w", [2, D]); inp("norm2_w", [2, D])
        inp("even_w_in", [1, D, 4480]); inp("even_w_out", [1, 1024, D])
        inp("rwkv_mu", [1, 1920]); inp("rwkv_w0", [1, 2, 512]); inp("rwkv_w2", [1, 2, 64, 512])
        inp("rwkv_a0", [1, 2, 512]); inp("rwkv_a2", [1, 2, 64, 512]); inp("rwkv_g2", [1, 128, 512])
        for n in ("rwkv_k_k", "rwkv_k_a", "rwkv_r_k", "rwkv_ln_w", "rwkv_ln_b"):
            inp(n, [1, 512])
        inp("hgrn_lower_bounds", [3, 512]); inp("hgrn_norm_w", [1, 512])
        inp("odd_w_in", [1, D, 5680]); inp("odd_w_out", [1, 2048, D])
        inp("ssd_conv_w", [1, 5, 1536]); inp("ssd_conv_b", [1, 1536]); inp("ssd_dt_bias", [1, 2, 16])
        inp("ssd_a_log", [1, 2, 16]); inp("ssd_d", [1, 16]); inp("ssd_norm_w", [1, 1024])
        inp("mlstm_conv_w", [1, 5, 1024]); inp("mlstm_conv_b", [1, 1024]); inp("mlstm_i_bias", [1, 2, 4])
        inp("mlstm_f_bias", [1, 2, 4]); inp("mlstm_norm_w", [1, 1024])
        inp("router_w", [2, D, 32]); inp("router_b", [2, 32])
        inp("exp_w_gate", [2, 32, D, 1024]); inp("exp_b_gate", [2, 32, 1024])
        inp("exp_w_up", [2, 32, D, 1024]); inp("exp_b_up", [2, 32, 1024])
        inp("exp_w_down", [2, 32, 1024, D]); inp("exp_b_down", [2, 32, D])
        inp("final_norm_w", [D])
        OUT = P.dram("out", [SEQ, D], F32, kind="ExternalOutput")

        def scratch(name, shape, dt=F32):
            return P.dram(name, shape, dt, kind="ExternalOutput" if name in debug else "Internal")

        XL = scratch("XL", [L, D])

        ident = P.sb("ident", [128, 128], F32)
        identb = P.sb("identb", [128, 128], BF16)
        ones = P.sb("ones", [128, 128], F32)
        k.memset("pool", V(ones), 1.0)
        P.op("pool", lambda e: e.affine_select(out=ident.a(), in_=ones.a(), pattern=[[-1, 128]], compare_op=ALU.is_equal,
                                               fill=0.0, base=0, channel_multiplier=1), reads=[ones], writes=[ident])
        k.cp("dve", V(identb), V(ident))

        modF = P.sb("modF", [128, 6, KO, 2], F32)
        modB = P.sb("modB", [128, 2, 2, D], F32)
        g1F = P.sb("g1F", [128, KO, 2], F32)
        g2F = P.sb("g2F", [128, KO, 2], F32)
        nw1 = P.sb("nw1", [128, 2, KO], F32)
        nw2 = P.sb("nw2", [128, 2, KO], F32)
        k.dma(V(nw1), V(I["norm1_w"], I["norm1_w"].a().rearrange("l (ko p) -> p l ko", p=128)), allow_slow_non_contiguous=True)
        k.dma(V(nw2), V(I["norm2_w"], I["norm2_w"].a().rearrange("l (ko p) -> p l ko", p=128)), allow_slow_non_contiguous=True)

        def stage_mods(l):
            with ExitStack() as ph:
                c0 = P.sb("c0", [128, KO, 2], F32, es=ph)
                s = P.sb("s", [128, KO, 2], F32, es=ph)
                sB = P.sb("sB", [128, KO, 2, 128], F32, es=ph)
                abF = P.sb("abF", [128, 48], F32, es=ph)
                abB = P.sb("abB", [128, 2, D], F32, es=ph)
                awm = [P.sb(f"awm{i}", [128, KO, D], F32, es=ph) for i in range(2)]
                psF = P.ps("psF", [128, KO, 2], F32, es=ph)
                psB = [P.ps(f"psB{i}", [128, 512], F32, es=ph) for i in range(2)]
                tmp = P.sb("tmpm", [128, KO, 2], F32, es=ph)
                k.dma((c0, c0[:, :, 0]), V(I["c"], I["c"].a().rearrange("(ko p) -> p ko", p=128)), allow_slow_non_contiguous=True)
                k.dma((c0, c0[:, :, 1]), V(I["c_ctx"], I["c_ctx"].a().rearrange("(ko p) -> p ko", p=128)), allow_slow_non_contiguous=True)
                k.act(V(s), V(c0), AF.Silu)
                k.cp("dve", V(sB), (s, s.a().rearrange("p k (j o) -> p k j o", o=1).to_broadcast([128, KO, 2, 128])))
                k.dma(V(abF), V(I["ada_b"], I["ada_b"][l].rearrange("(nb p) -> p nb", p=128)), allow_slow_non_contiguous=True)
                k.dma((abB, abB[:, 0, :]), V(I["ada_b"], I["ada_b"][l, 2 * D:3 * D].partition_broadcast(128)))
                k.dma((abB, abB[:, 1, :]), V(I["ada_b"], I["ada_b"][l, 5 * D:6 * D].partition_broadcast(128)))
                for m in range(6):
                    aw = awm[m % 2]
                    k.dma(V(aw), V(I["ada_w"], I["ada_w"][l, :, m * D:(m + 1) * D].rearrange("(ko p) n -> p ko n", p=128)))
                    if m in (0, 1, 3, 4):
                        for nb in range(KO):
                            for ko in range(KO):
                                k.mm((psF, psF[:, nb, :]), (aw, aw[:, ko, nb * 128:(nb + 1) * 128]), (s, s[:, ko, :]),
                                     start=(ko == 0), stop=(ko == KO - 1))
                        k.tt("dve", (modF, modF[:, m, :, :]), V(psF),
                             (abF, abF[:, m * 8:(m + 1) * 8].rearrange("p (k o) -> p k o", o=1).to_broadcast([128, KO, 2])), ALU.add)
                    else:
                        mi = 0 if m == 2 else 1
                        for j in range(2):
                            for nblk in range(2):
                                pb = psB[(j * 2 + nblk) % 2]
                                for ko in range(KO):
                                    k.mm(V(pb), (sB, sB[:, ko, j, :]), (aw, aw[:, ko, nblk * 512:(nblk + 1) * 512]),
                                         start=(ko == 0), stop=(ko == KO - 1))
                                k.tt("dve", (modB, modB[:, mi, j, nblk * 512:(nblk + 1) * 512]), V(pb),
                                     (abB, abB[:, mi, nblk * 512:(nblk + 1) * 512]), ALU.add)
                for (gF, nw, mi) in ((g1F, nw1, 1), (g2F, nw2, 4)):
                    k.ts("dve", V(tmp), (modF, modF[:, mi, :, :]), 1.0, ALU.add)
                    k.tt("dve", V(gF), V(tmp), (nw, nw[:, l, :].rearrange("p (k o) -> p k o", o=1).to_broadcast([128, KO, 2])), ALU.mult)
                P.barrier()

        def stage_norm(l, which, HT, src_tiles, perm):
            gF = g1F if which == 1 else g2F
            mi = 0 if which == 1 else 3
            with ExitStack() as ph:
                xt = [P.sb(f"xt{i}", [128, D], F32, es=ph) for i in range(3)]
                xn = [P.sb(f"xn{i}", [128, D], F32, es=ph) for i in range(2)]
                junk = P.sb("junk", [128, D], F32, es=ph)
                st = [P.sb(f"st{i}", [128, 4], F32, es=ph) for i in range(2)]
                tmp = [P.sb(f"tmpn{i}", [128, KO, 128], F32, es=ph) for i in range(2)]
                pT = [P.ps(f"pT{i}", [128, KO, 128], F32, es=ph) for i in range(2)]
                for i in range(NT):
                    j = 1 if i < 2 else 0
                    x_ = xt[i % 3]; n_ = xn[i % 2]; s_ = st[i % 2]; t_ = tmp[i % 2]; p_ = pT[i % 2]
                    sb_, sap = src_tiles(i)
                    k.dma(V(x_), (sb_, sap))
                    k.memset("pool", (s_, s_[:, 0:1]), 0.0)
                    k.act(V(junk), V(x_), AF.Square, accum=(s_, s_[:, 0:1]))
                    k.ts("dve", (s_, s_[:, 1:2]), (s_, s_[:, 0:1]), 1.0 / D, ALU.mult, EPS, ALU.add)
                    k.act((s_, s_[:, 2:3]), (s_, s_[:, 1:2]), AF.Sqrt)
                    k.recip((s_, s_[:, 3:4]), (s_, s_[:, 2:3]))
                    k.ts("dve", V(n_), V(x_), (s_, s_[:, 3:4]), ALU.mult)
                    for ko in range(KO):
                        k.tr((p_, p_[:, ko, :]), (n_, n_[:, ko * 128:(ko + 1) * 128]), V(ident))
                    k.tt("dve", V(t_), V(p_), (gF, gF[:, :, j:j + 1].to_broadcast([128, KO, 128])), ALU.mult)
                    if perm and i >= 2:
                        r0 = 2 * (i - 2)
                        dst = HT[:, :, LC:].rearrange("p k (c r) -> p k c r", r=64)[:, :, :, r0:r0 + 2]
                        src0 = t_.a().rearrange("p k (r c) -> p k c r", r=2)
                        src1 = modF[:, mi, :, j:j + 1].rearrange("p k (a b) -> p k a b", b=1).to_broadcast([128, KO, 64, 2])
                        k.tt("pool", (HT, dst, i), (t_, src0), (modF, src1), ALU.add)
                    else:
                        k.tt("pool", (HT, HT[:, :, i * 128:(i + 1) * 128], i), V(t_),
                             (modF, modF[:, mi, :, j:j + 1].to_broadcast([128, KO, 128])), ALU.add)
                P.barrier()

        def stage_proj(HT, W, ncols, f_list, t_list):
            with ExitStack() as ph:
                wst = [P.sb(f"wst{i}", [128, KO, 512], BF16, es=ph) for i in range(2)]
                stg = [P.sb(f"stg{i}", [128, 512], F32, es=ph) for i in range(4)]
                pp = [P.ps(f"pp{i}", [128, 512], F32, es=ph) for i in range(4)]
                cnt = 0
                npan = (ncols + 511) // 512
                for pn in range(npan):
                    c0 = pn * 512
                    cw = min(512, ncols - c0)
                    w_ = wst[pn % 2]
                    k.dma((w_, w_[:, :, 0:cw]), (W[0], W[1][:, c0:c0 + cw].rearrange("(ko p) n -> p ko n", p=128)), eng="pool")
                    for (f0, f1, PF, roff) in f_list:
                        fa, fb = max(c0, f0), min(c0 + cw, f1)
                        if fa >= fb:
                            continue
                        for n0 in range(fa, fb, 128):
                            nw_ = min(128, fb - n0)
                            for tb in range(0, L, 512):
                                tw = min(512, L - tb)
                                p_ = pp[cnt % 4]; s_ = stg[cnt % 4]; cnt += 1
                                for ko in range(KO):
                                    k.mm((p_, p_[0:nw_, 0:tw]), (w_, w_[:, ko, n0 - c0:n0 - c0 + nw_]), (HT, HT[:, ko, tb:tb + tw]),
                                         start=(ko == 0), stop=(ko == KO - 1))
                                k.cp("act" if cnt % 2 else "dve", (s_, s_[0:nw_, 0:tw]), (p_, p_[0:nw_, 0:tw]))
                                r0 = n0 - f0 + roff
                                k.dma((PF, PF[r0:r0 + nw_, tb:tb + tw], ("f", n0)), (s_, s_[0:nw_, 0:tw]))
                    for (t0, t1, PT, coff) in t_list:
                        ta, tb_ = max(c0, t0), min(c0 + cw, t1)
                        if ta >= tb_:
                            continue
                        tw = tb_ - ta
                        for tt in range(NT):
                            p_ = pp[cnt % 4]; s_ = stg[cnt % 4]; cnt += 1
                            for ko in range(KO):
                                k.mm((p_, p_[:, 0:tw]), (HT, HT[:, ko, tt * 128:(tt + 1) * 128]), (w_, w_[:, ko, ta - c0:ta - c0 + tw]),
                                     start=(ko == 0), stop=(ko == KO - 1))
                            k.cp("act" if cnt % 2 else "dve", (s_, s_[:, 0:tw]), (p_, p_[:, 0:tw]))
                            cc = ta - t0 + coff
                            k.dma((PT, PT[tt * 128:(tt + 1) * 128, cc:cc + tw], ("t", tt, ta)), (s_, s_[:, 0:tw]))
                P.barrier()

        def chunk_order(z):
            if z == 0:
                return list(range(NCH))
            return [3, 2, 1, 0] + list(range(NCH - 1, 3, -1))

        def make_masks(ph):
            ms = []
            for z in range(2):
                m = P.sb(f"mask{z}", [64, 64], F32, es=ph)
                if z == 0:
                    P.op("pool", lambda e, m=m: e.affine_select(out=m.a(), in_=ones[0:64, 0:64], pattern=[[1, 64]], compare_op=ALU.is_ge,
                                                           fill=0.0, base=0, channel_multiplier=-1), reads=[ones], writes=[m])
                else:
                    P.op("pool", lambda e, m=m: e.affine_select(out=m.a(), in_=ones[0:64, 0:64], pattern=[[-1, 64]], compare_op=ALU.is_ge,
                                                           fill=0.0, base=0, channel_multiplier=1), reads=[ones], writes=[m])
                ms.append(m)
            return ms

        def mixer_hgrn(PF, PT, YM):
            BW = 1088
            NB = L // BW
            CB = BW // CH
            with ExitStack() as ph:
                masks = make_masks(ph)
                lbr = P.sb("lbr", [128, 3, 4], F32, es=ph)
                lbe = P.sb("lbe", [128, 3, 4], F32, es=ph)
                lbs = P.sb("lbs", [128, 4], F32, es=ph)
                lb = P.sb("lb", [128, 4], F32, es=ph)
                oml = P.sb("oml", [128, 4], F32, es=ph)
                noml = P.sb("noml", [128, 4], F32, es=ph)
                k.dma(V(lbr), V(I["hgrn_lower_bounds"], I["hgrn_lower_bounds"].a().rearrange("j (h p) -> p j h", p=128)),
                      allow_slow_non_contiguous=True)
                k.act(V(lbe), V(lbr), AF.Exp)
                k.tt("dve", V(lbs), (lbe, lbe[:, 0, :]), (lbe, lbe[:, 1, :]), ALU.add)
                k.tt("dve", V(lbs), V(lbs), (lbe, lbe[:, 2, :]), ALU.add)
                k.recip(V(lbs), V(lbs))
                k.tt("dve", V(lb), (lbe, lbe[:, 0, :]), V(lbs), ALU.mult)
                k.ts("dve", V(oml), V(lb), -1.0, ALU.mult, 1.0, ALU.add)
                k.ts("dve", V(noml), V(oml), -1.0, ALU.mult)
                rst = P.sb("rst", [128, BW], F32, es=ph)
                k.memset("pool", V(rst), 1.0)
                k.memset("pool", (rst, rst.a().rearrange("p (c t) -> p c t", t=CH)[:, :, 0:1]), 0.0)
                nwB = P.sb("nwB", [64, 512], F32, es=ph)
                k.dma(V(nwB), V(I["hgrn_norm_w"], I["hgrn_norm_w"][0].partition_broadcast(64)))
                oacc = P.sb("oacc", [64, NCH, 128], F32, es=ph)
                ssn = P.sb("ssn", [64, NCH], F32, es=ph)
                for h in range(4):
                    with ExitStack() as hs1:
                        t_f = P.sb("t_f", [128, BW], F32, es=hs1)
                        t_s = P.sb("t_s", [128, BW], F32, es=hs1)
                        t_lf = P.sb("t_lf", [128, BW], F32, es=hs1)
                        t_k = P.sb("t_k", [128, BW], F32, es=hs1)
                        t_b = P.sb("t_b", [128, BW], F32, es=hs1)
                        t_e = P.sb("t_e", [128, BW], F32, es=hs1)
                        t_q = P.sb("t_q", [128, BW], F32, es=hs1)
                        cd = [P.sb(f"cd{z}", [128, NCH], F32, es=hs1) for z in range(2)]
                        q_in = [P.sb(f"q_in{z}", [128, L], BF16, es=hs1) for z in range(2)]
                        k_in = [P.sb(f"k_in{z}", [128, L], BF16, es=hs1) for z in range(2)]
                        k_end1 = P.sb("k_end", [128, L], BF16, es=hs1)
                        k_end = [k_end1, k_end1]
                        kET = [P.sb(f"kET{z}", [64, NCH, 128], BF16, es=hs1) for z in range(2)]
                        vb = P.sb("vb", [64, NCH, 128], BF16, es=hs1)
                        S = [P.sb(f"S{z}", [128, 128], F32, es=hs1) for z in range(2)]
                        Sb = [P.sb(f"Sb{z}", [128, 128], BF16, es=hs1) for z in range(2)]
                        Am = [P.sb(f"Am{i}", [64, 64], BF16, es=hs1) for i in range(4)]
                        kvS = [[P.sb(f"kvS{z}_{i}", [128, 128], F32, es=hs1) for i in range(2)] for z in range(2)]
                        psA = [P.ps(f"psA{i}", [64, 64], F32, es=hs1) for i in range(2)]
                        pso = [P.ps(f"pso{i}", [64, 128], F32, es=hs1) for i in range(2)]
                        pskv = [P.ps(f"pskv{i}", [128, 128], F32, es=hs1) for i in range(2)]
                        pst = [P.ps(f"pst{i}", [64, 4, 128], BF16, es=hs1) for i in range(2)]
                        k.dma(V(vb), (PT, PT[:, h * 128:(h + 1) * 128].rearrange("(c p) d -> p c d", p=CH)), eng="pool")
                        for z in range(2):
                            end = CH - 1 if z == 0 else 0
                            for blk in range(NB):
                                tsl = slice(blk * BW, (blk + 1) * BW)
                                frow = 2432 + z * 512 + h * 128
                                k.dma(V(t_f), (PF, PF[frow:frow + 128, tsl]))
                                k.dma(V(t_q), (PF, PF[1920 + h * 128:1920 + (h + 1) * 128, tsl]))
                                k.act(V(t_s), V(t_f), AF.Sigmoid)
                                k.act(V(t_lf), V(t_s), AF.Ln, bias=(lb, lb[:, h:h + 1]), scale=(oml, oml[:, h:h + 1]))
                                k.ts("dve", V(t_k), V(t_s), (noml, noml[:, h:h + 1]), ALU.mult, (oml, oml[:, h:h + 1]), ALU.add)
                                k.scan(V(t_b), V(rst), V(t_lf), 0.0, ALU.mult, ALU.add)
                                if z == 1:
                                    k.tt("dve", V(t_f), V(t_lf), V(t_b), ALU.subtract)
                                    b3 = t_b.a().rearrange("p (c t) -> p c t", t=CH)
                                    k.tt("dve", (t_lf, t_lf.a().rearrange("p (c t) -> p c t", t=CH)),
                                         (t_f, t_f.a().rearrange("p (c t) -> p c t", t=CH)),
                                         (t_b, b3[:, :, CH - 1:CH].to_broadcast([128, CB, CH])), ALU.add)
                                    bb = t_lf
                                else:
                                    bb = t_b
                                k.act(V(t_e), V(bb), AF.Exp)
                                k.act(V(t_s), V(bb), AF.Exp, scale=-1.0)
                                e3 = t_e.a().rearrange("p (c t) -> p c t", t=CH)
                                k.cp("pool", (cd[z], cd[z][:, blk * CB:(blk + 1) * CB]), (t_e, e3[:, :, end]))
                                k.act(V(t_f), V(t_q), AF.Silu)
                                k.stt("dve", (q_in[z], q_in[z][:, tsl]), V(t_f), 128 ** -0.5, V(t_e), ALU.mult, ALU.mult)
                                k.tt("dve", V(t_k), V(t_k), V(t_s), ALU.mult)
                                k.cp("pool", (k_in[z], k_in[z][:, tsl]), V(t_k))
                                k.tt("pool", (k_end[z], k_end[z][:, tsl].rearrange("p (c t) -> p c t", t=CH)),
                                     (t_k, t_k.a().rearrange("p (c t) -> p c t", t=CH)),
                                     (t_e, e3[:, :, end:end + 1].to_broadcast([128, CB, CH])), ALU.mult)
                            for c4 in range(0, NCH, 4):
                                p_ = pst[(c4 // 4) % 2]
                                for j in range(4):
                                    c = c4 + j
                                    k.tr((p_, p_[:, j, :]), (k_end[z], k_end[z][:, c * CH:(c + 1) * CH]), V(identb))
                                k.cp("act", (kET[z], kET[z][:, c4:c4 + 4, :]), V(p_))
                        k.memset("pool", V(oacc), 0.0)
                        for z in range(2):
                            k.memset("pool", V(S[z]), 0.0)
                            k.memset("pool", V(Sb[z]), 0.0)
                        orders = [chunk_order(0), chunk_order(1)]

                        def hg_A(step, z):
                            c = orders[z][step]
                            csl = slice(c * CH, (c + 1) * CH)
                            am = Am[(step % 2) * 2 + z]
                            k.mm(V(psA[z]), (k_in[z], k_in[z][:, csl]), (q_in[z], q_in[z][:, csl]))
                            k.tt("dve", V(am), V(psA[z]), V(masks[z]), ALU.mult)
                            k.mm(V(pskv[z]), (kET[z], kET[z][:, c, :]), (vb, vb[:, c, :]))
                            k.cp("act", V(kvS[z][step % 2]), V(pskv[z]))

                        def hg_B(step, z):
                            c = orders[z][step]
                            csl = slice(c * CH, (c + 1) * CH)
                            am = Am[(step % 2) * 2 + z]
                            po = pso[z]
                            k.mm(V(po), V(am), (vb, vb[:, c, :]), start=True, stop=False)
                            k.mm(V(po), (q_in[z], q_in[z][:, csl]), V(Sb[z]), start=False, stop=True)
                            k.tt("dve", (oacc, oacc[:, c, :], c), (oacc, oacc[:, c, :], c), V(po), ALU.add)
                            k.stt("dve", V(S[z]), V(S[z]), (cd[z], cd[z][:, c:c + 1]), V(kvS[z][step % 2]), ALU.mult, ALU.add)
                            k.cp("act", V(Sb[z]), V(S[z]))

                        for z in range(2):
                            hg_A(0, z)
                        for step in range(NCH):
                            if step + 1 < NCH:
                                for z in range(2):
                                    hg_A(step + 1, z)
                            for z in range(2):
                                hg_B(step, z)
                    P.barrier()
                    with ExitStack() as hs2:
                        gt = P.sb("gt", [64, NCH, 128], F32, es=hs2)
                        k.tt("dve", V(gt), V(oacc), V(oacc), ALU.mult)
                        P.op("dve", lambda e: e.tensor_reduce(out=ssn.a(), in_=gt.a(), axis=AX.X, op=ALU.add), reads=[gt], writes=[ssn])
                        k.ts("dve", V(ssn), V(ssn), 1.0 / 128, ALU.mult, EPS, ALU.add)
                        k.act(V(ssn), V(ssn), AF.Sqrt)
                        k.recip(V(ssn), V(ssn))
                        k.tt("dve", V(oacc), V(oacc), (ssn, ssn.a().rearrange("p (c o) -> p c o", o=1).to_broadcast([64, NCH, 128])), ALU.mult)
                        k.tt("pool", V(oacc), V(oacc), (nwB, nwB[:, h * 128:(h + 1) * 128].rearrange("p (o d) -> p o d", o=1).to_broadcast([64, NCH, 128])), ALU.mult)
                        k.dma(V(gt), (PT, PT[:, 512 + h * 128:512 + (h + 1) * 128].rearrange("(c p) d -> p c d", p=CH)))
                        k.act(V(gt), V(gt), AF.Silu)
                        k.tt("dve", V(oacc), V(oacc), V(gt), ALU.mult)
                        k.dma((YM, YM[:, 512 + h * 128:512 + (h + 1) * 128].rearrange("(c p) d -> p c d", p=CH), ("h", h)), V(oacc))
                    P.barrier()
                P.barrier()

        def rwkv_shift(PF, PR):
            with ExitStack() as ph:
                mu = P.sb("mu", [128, 15], F32, es=ph)
                omu = P.sb("omu", [128, 15], F32, es=ph)
                hmu = P.sb("hmu", [128, 15], F32, es=ph)
                k.dma(V(mu), V(I["rwkv_mu"], I["rwkv_mu"][0].rearrange("(b p) -> p b", p=128)), allow_slow_non_contiguous=True)
                k.ts("dve", V(omu), V(mu), -1.0, ALU.mult, 1.0, ALU.add)
                k.ts("dve", V(hmu), V(mu), 0.5, ALU.mult)
                pt = [P.sb(f"shp{i}", [128, L + 2], F32, es=ph) for i in range(2)]
                sm = [P.sb(f"shs{i}", [128, L], F32, es=ph) for i in range(2)]
                for i in range(2):
                    k.memset("pool", (pt[i], pt[i][:, 0:1], "h0"), 0.0)
                    k.memset("pool", (pt[i], pt[i][:, L + 1:L + 2], "h1"), 0.0)
                for b in range(15):
                    p_ = pt[b % 2]; s_ = sm[b % 2]
                    k.dma((p_, p_[:, 1:L + 1], "m"), (PF, PF[b * 128:(b + 1) * 128, :]))
                    k.tt("dve", (s_, s_.a(), "a"), V(p_), (p_, p_[:, 2:L + 2]), ALU.add) if False else None
                    P.op("dve", lambda e, p_=p_, s_=s_: e.tensor_tensor(out=s_.a(), in0=p_[:, 0:L], in1=p_[:, 2:L + 2], op=ALU.add),
                         reads=[p_], writes=[s_])
                    k.cp("dve", (s_, s_[:, 255:256]), (p_, p_[:, 255:256]))
                    k.cp("dve", (s_, s_[:, 256:257]), (p_, p_[:, 258:259]))
                    k.ts("pool", V(s_), V(s_), (hmu, hmu[:, b:b + 1]), ALU.mult)
                    k.stt("dve", V(s_), (p_, p_[:, 1:L + 1]), (omu, omu[:, b:b + 1]), V(s_), ALU.mult, ALU.add)
                    k.dma((PR, PR[b * 128:(b + 1) * 128, :], b), V(s_))
                P.barrier()

        def rwkv_gate(PR, GT):
            with ExitStack() as ph:
                gd = P.sb("gd", [128, L], F32, es=ph)
                gs = P.sb("gs", [128, L], BF16, es=ph)
                g2f = P.sb("g2f", [128, 512], F32, es=ph)
                g2b = P.sb("g2b", [128, 512], BF16, es=ph)
                pg = [P.ps(f"pg{i}", [128, 512], F32, es=ph) for i in range(2)]
                sg = [P.sb(f"sg{i}", [128, 512], F32, es=ph) for i in range(2)]
                k.dma(V(gd), (PR, PR[1792:1920, :]))
                k.dma(V(g2f), V(I["rwkv_g2"], I["rwkv_g2"][0]))
                k.cp("dve", V(g2b), V(g2f))
                for q4 in range(4):
                    k.act((gs, gs[:, q4 * 1088:(q4 + 1) * 1088], q4), (gd, gd[:, q4 * 1088:(q4 + 1) * 1088]), AF.Sigmoid)
                for tt in range(NT if "gate_nomm" not in debug else 0):
                    k.mm(V(pg[tt % 2]), (gs, gs[:, tt * 128:(tt + 1) * 128]), V(g2b))
                    k.cp("dve" if tt % 2 else "act", V(sg[tt % 2]), V(pg[tt % 2]))
                    k.dma((GT, GT[tt * 128:(tt + 1) * 128, :], tt), V(sg[tt % 2]))
                P.barrier()

        def mixer_rwkv(PR, GT, YM):
            BW = 256
            NB = L // BW
            CB = BW // CH
            NL = 5
            with ExitStack() as ph:
                w2all = P.sb("w2all", [128, 512], F32, es=ph)
                a2all = P.sb("a2all", [128, 512], F32, es=ph)
                k.dma(V(w2all), V(I["rwkv_w2"], I["rwkv_w2"][0].rearrange("z r c -> (z r) c")))
                k.dma(V(a2all), V(I["rwkv_a2"], I["rwkv_a2"][0].rearrange("z r c -> (z r) c")))
                def hv(name, src):
                    t = P.sb(name, [64, 8], F32, es=ph)
                    k.dma(V(t), (src[0], src[1].rearrange("(h n) -> n h", n=64)), allow_slow_non_contiguous=True)
                    return t
                w0 = [hv(f"w0_{z}", (I["rwkv_w0"], I["rwkv_w0"][0, z])) for z in range(2)]
                a0 = [hv(f"a0_{z}", (I["rwkv_a0"], I["rwkv_a0"][0, z])) for z in range(2)]
                kkg = hv("kkg", (I["rwkv_k_k"], I["rwkv_k_k"][0]))
                kag = hv("kag", (I["rwkv_k_a"], I["rwkv_k_a"][0]))
                rkg = hv("rkg", (I["rwkv_r_k"], I["rwkv_r_k"][0]))
                oka = P.sb("oka", [64, 8], F32, es=ph)
                k.ts("dve", V(oka), V(kag), -1.0, ALU.mult, 1.0, ALU.add)
                lnw = P.sb("lnw", [64, 512], F32, es=ph)
                lnb = P.sb("lnb", [64, 512], F32, es=ph)
                k.dma(V(lnw), V(I["rwkv_ln_w"], I["rwkv_ln_w"][0].partition_broadcast(64)))
                k.dma(V(lnb), V(I["rwkv_ln_b"], I["rwkv_ln_b"][0].partition_broadcast(64)))
                rst = P.sb("rst", [64, BW], F32, es=ph)
                k.memset("pool", V(rst), 1.0)
                k.memset("pool", (rst, rst.a().rearrange("p (c t) -> p c t", t=CH)[:, :, 0:1]), 0.0)
                ones64 = P.sb("ones64", [64, 64], F32, es=ph)
                k.memset("pool", V(ones64), 1.0)
                m4 = []
                m3 = []
                for z in range(2):
                    m = P.sb(f"m4_{z}", [128, 2, 64], F32, es=ph)
                    for half in range(2):
                        for col in range(2):
                            sgn = 1 if z == 0 else -1
                            base = (-1 if col == 0 else 0)
                            P.op("pool", lambda e, m=m, half=half, col=col, sgn=sgn, base=base: e.affine_select(
                                out=m[half * 64:(half + 1) * 64, col, :], in_=ones[half * 64:(half + 1) * 64, 0:64],
                                pattern=[[sgn, 64]], compare_op=ALU.is_ge, fill=0.0, base=base, channel_multiplier=-sgn),
                                reads=[ones], writes=[m])
                    m4.append(m)
                    mm3 = P.sb(f"m3_{z}", [64, 64], F32, es=ph)
                    P.op("pool", lambda e, mm3=mm3, z=z: e.affine_select(
                        out=mm3.a(), in_=ones[0:64, 0:64], pattern=[[-1 if z == 0 else 1, 64]], compare_op=ALU.is_ge, fill=0.0,
                        base=-1, channel_multiplier=1 if z == 0 else -1), reads=[ones], writes=[mm3])
                    m3.append(mm3)
                QI0 = P.sb("QI0", [64, 64], BF16, es=ph)
                k.cp("dve", V(QI0), (ident, ident[0:64, 0:64]))

                nheads = 0 if "rw1" in debug else (1 if ("rw2" in debug or "rw3" in debug) else 8)
                for h in range(nheads):
                    with ExitStack() as hs:
                        Vt = P.sb("Vt", [64, NCH, CH], F32, es=hs)
                        oacc = P.sb("oaccr", [64, NCH, CH], F32, es=hs)
                        bon = P.sb("bon", [64, NCH], F32, es=hs)
                        hs2 = ExitStack()
                        AR = [P.sb(f"AR{z}", [64, NCH, 2, CH], BF16, es=hs2) for z in range(2)]
                        BK = [P.sb(f"BK{z}", [64, NCH, 2, CH], BF16, es=hs2) for z in range(2)]
                        BKeT = [P.sb(f"BKeT{z}", [128, NCH, CH], BF16, es=hs2) for z in range(2)]
                        UV = [P.sb(f"UV{z}", [128, NCH, CH], BF16, es=hs2) for z in range(2)]
                        ZV = P.sb("ZV", [128, NCH, CH], BF16, es=hs2)
                        gC = [P.sb(f"gC{z}", [64, NCH], F32, es=hs2) for z in range(2)]
                        k.memset("pool", (ZV, ZV[0:64, :, :], "z"), 0.0)
                        with ExitStack() as pp_:
                            def T(name, dt=F32, w=BW):
                                return P.sb(name, [64, w], dt, es=pp_)
                            t_r = T("t_r"); t_k = T("t_k"); t_v = T("t_v")
                            t_wd = P.sb("t_wd", [128, BW], F32, es=pp_)
                            t_ad = P.sb("t_ad", [128, BW], F32, es=pp_)
                            t_kk = T("t_kk"); t_q = T("t_q"); t_rn = T("t_rn")
                            t_sg = T("t_sg"); t_cs = T("t_cs"); t_x = T("t_x"); t_y = T("t_y")
                            t_eg = T("t_eg"); t_eng = T("t_eng"); t_egp = T("t_egp")
                            t_a = T("t_a"); t_km = [T("t_km0"), T("t_km1")]; t_b = T("t_b")
                            bke = P.sb("bke", [64, CB, 2, CH], BF16, es=pp_)
                            t_v2 = P.sb("t_v2", [64, CB, 2, CH], F32, es=pp_)
                            pwa = [P.ps(f"pwa{i}", [64, BW], F32, es=pp_) for i in range(2)]
                            pss = P.ps("pss", [64, BW], F32, es=pp_)
                            ptv = P.ps("ptv", [128, CB, CH], F32, es=pp_)
                            ptb = P.ps("ptb", [128, CB, CH], BF16, es=pp_)
                            pbn = P.ps("pbn", [64, NCH], F32, es=pp_)
                            for blk in range(NB):
                                tsl = slice(blk * BW, (blk + 1) * BW)
                                csl = slice(blk * CB, (blk + 1) * CB)
                                k.dma(V(t_r), (PR, PR[h * 64:(h + 1) * 64, tsl]))
                                k.dma(V(t_k), (PR, PR[512 + h * 64:512 + (h + 1) * 64, tsl]))
                                k.dma(V(t_v), (PR, PR[1024 + h * 64:1024 + (h + 1) * 64, tsl]))
                                k.dma(V(t_wd), (PR, PR[1536:1664, tsl]))
                                k.dma(V(t_ad), (PR, PR[1664:1792, tsl]))
                                k.act(V(t_wd), V(t_wd), AF.Tanh)
                                k.cp("pool", V(t_v2), (t_v, t_v.a().rearrange("p (c o t) -> p c o t", o=1, t=CH).to_broadcast([64, CB, 2, CH])))
                                for j in range(CB):
                                    k.tr((ptv, ptv[:, j, :]), (t_v2, t_v2[:, j, :, :].rearrange("p a t -> p (a t)")), (ident, ident[0:64, 0:64]))
                                k.cp("act", (Vt, Vt[:, csl, :], blk), (ptv, ptv[0:64, :, :]))
                                for z in range(2):
                                    k.cp("dve", (UV[z], UV[z][64:128, csl, :], ("v", blk)), (ptv, ptv[64:128, :, :]))
                                k.cp("dve", (ZV, ZV[64:128, csl, :], ("v", blk)), (ptv, ptv[64:128, :, :]))
                                k.ts("dve", V(t_kk), V(t_k), (kkg, kkg[:, h:h + 1]), ALU.mult)
                                k.tt("pool", V(t_q), V(t_kk), V(t_kk), ALU.mult)
                                k.mm(V(pss), V(ones64), V(t_q))
                                k.ts("dve", V(t_rn), V(pss), 1e-12, ALU.add)
                                k.act(V(t_rn), V(t_rn), AF.Sqrt)
                                k.recip(V(t_rn), V(t_rn))
                                k.tt("dve", V(t_kk), V(t_kk), V(t_rn), ALU.mult)
                                for z in range(2):
                                    end = CH - 1 if z == 0 else 0
                                    zs = slice(z * 64, (z + 1) * 64)
                                    pw = pwa[0]; pa = pwa[1]
                                    k.mm(V(pw), (w2all, w2all[zs, h * 64:(h + 1) * 64]), (t_wd, t_wd[zs, :]))
                                    k.mm(V(pa), (a2all, a2all[zs, h * 64:(h + 1) * 64]), (t_ad, t_ad[zs, :]))
                                    k.act(V(t_sg), V(pw), AF.Sigmoid, bias=(w0[z], w0[z][:, h:h + 1]))
                                    k.act(V(t_a), V(pa), AF.Sigmoid, bias=(a0[z], a0[z][:, h:h + 1]))
                                    k.scan(V(t_cs), V(rst), V(t_sg), 0.0, ALU.mult, ALU.add)
                                    if z == 1:
                                        k.tt("dve", V(t_x), V(t_sg), V(t_cs), ALU.subtract)
                                        c3 = t_cs.a().rearrange("p (c t) -> p c t", t=CH)
                                        k.tt("dve", (t_y, t_y.a().rearrange("p (c t) -> p c t", t=CH)),
                                             (t_x, t_x.a().rearrange("p (c t) -> p c t", t=CH)),
                                             (t_cs, c3[:, :, CH - 1:CH].to_broadcast([64, CB, CH])), ALU.add)
                                        cs = t_y
                                    else:
                                        cs = t_cs
                                    k.act(V(t_eg), V(cs), AF.Exp, scale=-0.6065306597126334)
                                    k.act(V(t_eng), V(cs), AF.Exp, scale=0.6065306597126334)
                                    k.tt("dve", V(t_x), V(cs), V(t_sg), ALU.subtract)
                                    k.act(V(t_egp), V(t_x), AF.Exp, scale=-0.6065306597126334)
                                    eg3 = t_eg.a().rearrange("p (c t) -> p c t", t=CH)
                                    k.cp("pool", (gC[z], gC[z][:, csl], blk), (t_eg, eg3[:, :, end]))
                                    k.ts("dve", V(t_x), V(t_a), (kag, kag[:, h:h + 1]), ALU.mult, (oka, oka[:, h:h + 1]), ALU.add)
                                    k.tt("dve", V(t_km[z]), V(t_k), V(t_x), ALU.mult)
                                    k.tt("pool", V(t_b), V(t_kk), V(t_a), ALU.mult)
                                    arz = AR[z]; bkz = BK[z]
                                    k.stt("dve", (arz, arz[:, csl, 0, :], blk), (t_kk, t_kk.a().rearrange("p (c t) -> p c t", t=CH)), -1.0,
                                          (t_egp, t_egp.a().rearrange("p (c t) -> p c t", t=CH)), ALU.mult, ALU.mult)
                                    k.tt("pool", (arz, arz[:, csl, 1, :], blk), (t_r, t_r.a().rearrange("p (c t) -> p c t", t=CH)),
                                         (t_eg, eg3), ALU.mult)
                                    k.tt("dve", V(t_b), V(t_b), V(t_eng), ALU.mult)
                                    k.tt("dve", V(t_x), V(t_km[z]), V(t_eng), ALU.mult)
                                    k.cp("pool", (bkz, bkz[:, csl, 0, :], blk), (t_b, t_b.a().rearrange("p (c t) -> p c t", t=CH)))
                                    k.cp("act", (bkz, bkz[:, csl, 1, :], blk), (t_x, t_x.a().rearrange("p (c t) -> p c t", t=CH)))
                                    gcb = eg3[:, :, end:end + 1].to_broadcast([64, CB, CH])
                                    k.tt("dve", (bke, bke[:, :, 0, :]), (t_b, t_b.a().rearrange("p (c t) -> p c t", t=CH)), (t_eg, gcb), ALU.mult)
                                    k.tt("pool", (bke, bke[:, :, 1, :]), (t_x, t_x.a().rearrange("p (c t) -> p c t", t=CH)), (t_eg, gcb), ALU.mult)
                                    for j in range(CB):
                                        k.tr((ptb, ptb[:, j, :]), (bke, bke[:, j, :, :].rearrange("p a t -> p (a t)")), (identb, identb[0:64, 0:64]))
                                    k.cp("act", (BKeT[z], BKeT[z][:, csl, :], blk), V(ptb))
                                k.tt("dve", V(t_x), V(t_km[0]), V(t_km[1]), ALU.add)
                                k.stt("dve", V(t_x), V(t_r), (rkg, rkg[:, h:h + 1]), V(t_x), ALU.mult, ALU.mult)
                                for j in range(CB):
                                    c = blk * CB + j
                                    k.mm((pbn, pbn[:, c:c + 1]), (t_x, t_x[:, j * CH:(j + 1) * CH]), (ones64, ones64[:, 0:1]))
                            k.cp("dve", V(bon), V(pbn))
                        P.barrier()
                        with ExitStack() as us:
                            Hs = [P.sb(f"Hs{z}", [64, 64], F32, es=us) for z in range(2)]
                            Hb = [P.sb(f"Hb{z}", [64, 64], BF16, es=us) for z in range(2)]
                            AM = [[P.sb(f"AM{z}{i}", [128, 128], BF16, es=us) for i in range(2)] for z in range(2)]
                            Pm = [[P.sb(f"Pm{z}{i}", [64, 64], BF16, es=us) for i in range(2)] for z in range(2)]
                            QR = [[P.sb(f"QR{z}{i}", [64, 2, 64], BF16, es=us) for i in range(2)] for z in range(2)]
                            TT = [[P.sb(f"TT{z}{i}", [64, 64], BF16, es=us) for i in range(2)] for z in range(2)]
                            Xs = [P.sb(f"Xs{z}", [64, 64], BF16, es=us) for z in range(2)]
                            bA = [P.ps(f"bA{z}", [128, 512], F32, es=us) for z in range(2)]
                            bB = [P.ps(f"bB{z}", [64, 128], F32, es=us) for z in range(2)]
                            bC = [P.ps(f"bC{z}", [64, 64], F32, es=us) for z in range(2)]
                            bD = [P.ps(f"bD{z}", [64, 128], F32, es=us) for z in range(2)]
                            vM = [bA[z][:, 0:128] for z in range(2)]
                            vX = [bA[z][0:64, 128:192] for z in range(2)]
                            vU = [bA[z][0:64, 192:256] for z in range(2)]
                            vL = [bB[z].a() for z in range(2)]
                            vP = [bC[z].a() for z in range(2)]
                            vO = [bD[z][:, 0:64] for z in range(2)]
                            vH = [bD[z][:, 64:128] for z in range(2)]
                            pM = [(bA[z], vM[z]) for z in range(2)]
                            pL = [(bB[z], vL[z]) for z in range(2)]
                            pP = [(bC[z], vP[z]) for z in range(2)]
                            pX = [(bA[z], vX[z]) for z in range(2)]
                            pU = [(bA[z], vU[z]) for z in range(2)]
                            pO = [(bD[z], vO[z]) for z in range(2)]
                            pH = [(bD[z], vH[z]) for z in range(2)]
                            k.memset("pool", V(oacc), 0.0)
                            for z in range(2):
                                k.memset("pool", V(Hs[z]), 0.0)
                                k.memset("pool", V(Hb[z]), 0.0)
                            orders = [chunk_order(0), chunk_order(1)]
                            for step in range(NCH if "rw2" not in debug else 0):
                                for z in range(2):
                                    c = orders[z][step]
                                    am = AM[z][step % 2]
                                    arc = AR[z][:, c, :, :].rearrange("p a t -> p (a t)")
                                    bkc = BK[z][:, c, :, :].rearrange("p a t -> p (a t)")
                                    k.mm(pM[z], (BK[z], bkc), (AR[z], arc))
                                    k.tt("dve", V(am), pM[z], (m4[z], m4[z].a().rearrange("p a t -> p (a t)")), ALU.mult)
                                    k.mm(pP[z], (AR[z], AR[z][:, c, 0, :]), (BK[z], BK[z][:, c, 0, :]))
                                    pm_ = Pm[z][0]
                                    k.tt("pool" if False else "dve", V(pm_), pP[z], V(m3[z]), ALU.mult)
                                    if "u1" in debug:
                                        continue
                                    qr = QR[z][0]
                                    k.cp("act", (qr, qr[:, 0, :]), (am, am[0:64, 0:64]))
                                    k.cp("pool", (qr, qr[:, 1, :]), V(QI0))
                                    for lv in range(1, NL + 1):
                                        qn = QR[z][lv % 2]
                                        pn = Pm[z][lv % 2]
                                        k.mm(pL[z], V(pm_), (qr, qr.a().rearrange("p a t -> p (a t)")))
                                        k.mm(pP[z], (qr, qr[:, 0, :]), V(pm_))
                                        k.cp("act", (qn, qn[:, 0, :]), (bB[z], vL[z][:, 0:64]))
                                        k.tt("dve", (qn, qn[:, 1, :]), (bB[z], vL[z][:, 64:128]), (qr, qr[:, 1, :]), ALU.add)
                                        k.cp("act", V(pn), pP[z])
                                        qr = qn; pm_ = pn
                                    tt_ = TT[z][step % 2]
                                    k.mm((bB[z], vL[z][:, 0:64]), V(pm_), (qr, qr[:, 1, :]))
                                    k.tt("dve", V(tt_), (bB[z], vL[z][:, 0:64]), (qr, qr[:, 1, :]), ALU.add)
                                    if "u2" in debug:
                                        continue
                                    k.mm(pX[z], (AR[z], AR[z][:, c, 0, :]), V(Hb[z]), start=True, stop=False)
                                    P.op("pe", lambda e, px=vX[z], am=am, c=c: e.matmul(px, lhsT=am[:, 0:64], rhs=ZV[:, c, :], start=False, stop=True),
                                         reads=[(am, None), (ZV, "z"), (ZV, ("v", c // CB))], writes=[(bA[z], None)], pe_acc=True)
                                    k.cp("act", V(Xs[z]), pX[z])
                                    k.mm(pU[z], V(tt_), V(Xs[z]))
                                    k.cp("dve", (UV[z], UV[z][0:64, c, :], ("u", c)), pU[z])
                                    if "u3" in debug:
                                        continue
                                    k.mm(pO[z], (AR[z], AR[z][:, c, 1, :]), V(Hb[z]), start=True, stop=False)
                                    P.op("pe", lambda e, po=vO[z], am=am, uv=UV[z], c=c: e.matmul(po, lhsT=am[:, 64:128], rhs=uv[:, c, :], start=False, stop=True),
                                         reads=[(am, None), (UV[z], ("u", c)), (UV[z], ("v", c // CB))], writes=[(bD[z], None)], pe_acc=True)
                                    k.tt("dve", (oacc, oacc[:, c, :], c), (oacc, oacc[:, c, :], c), pO[z], ALU.add)
                                    P.op("pe", lambda e, ph_=vH[z], bt=BKeT[z], uv=UV[z], c=c: e.matmul(ph_, lhsT=bt[:, c, :], rhs=uv[:, c, :], start=True, stop=True),
                                         reads=[(BKeT[z], None), (UV[z], ("u", c)), (UV[z], ("v", c // CB))], writes=[(bD[z], None)])
                                    k.stt("dve", V(Hs[z]), V(Hs[z]), (gC[z], gC[z][:, c:c + 1]), pH[z], ALU.mult, ALU.add)
                                    k.cp("act", V(Hb[z]), V(Hs[z]))
                        if "d_oacc" in debug and h == 0:
                            do = P.dram("d_oacc", [64, NCH * CH], F32, kind="ExternalOutput")
                            k.dma(V(do), (oacc, oacc.a().rearrange("p a b -> p (a b)")))
                            dm4 = P.dram("d_m4", [128, 2 * 128], F32, kind="ExternalOutput")
                            for z in range(2):
                                k.dma((dm4, dm4[:, z * 128:(z + 1) * 128]), (m4[z], m4[z].a().rearrange("p a b -> p (a b)")))
                            dm3 = P.dram("d_m3", [64, 2 * 64], F32, kind="ExternalOutput")
                            for z in range(2):
                                k.dma((dm3, dm3[:, z * 64:(z + 1) * 64]), V(m3[z]))
                            dgc = P.dram("d_gC", [64, 2 * NCH], F32, kind="ExternalOutput")
                            for z in range(2):
                                k.dma((dgc, dgc[:, z * NCH:(z + 1) * NCH]), V(gC[z]))
                            dbon = P.dram("d_bon", [64, NCH], F32, kind="ExternalOutput")
                            k.dma(V(dbon), V(bon))
                            dar = P.dram("d_AR", [64, 2 * NCH * 2 * CH], BF16, kind="ExternalOutput")
                            dbk = P.dram("d_BK", [64, 2 * NCH * 2 * CH], BF16, kind="ExternalOutput")
                            for z in range(2):
                                k.dma((dar, dar[:, z * NCH * 128:(z + 1) * NCH * 128]), (AR[z], AR[z].a().rearrange("p a b c -> p (a b c)")))
                                k.dma((dbk, dbk[:, z * NCH * 128:(z + 1) * NCH * 128]), (BK[z], BK[z].a().rearrange("p a b c -> p (a b c)")))
                        P.barrier()
                        hs2.close()
                        with ExitStack() as fs:
                            gt = P.sb("gtr", [64, NCH, CH], F32, es=fs)
                            cen = P.sb("cen", [64, NCH, CH], F32, es=fs)
                            mu_ = P.sb("mu_", [64, NCH], F32, es=fs)
                            var = P.sb("var", [64, NCH], F32, es=fs)
                            k.dma(V(gt), (GT, GT[:, h * 64:(h + 1) * 64].rearrange("(c p) d -> p c d", p=CH)))
                            P.op("dve", lambda e: e.tensor_reduce(out=mu_.a(), in_=oacc.a(), axis=AX.X, op=ALU.add), reads=[oacc], writes=[mu_])
                            k.ts("dve", V(mu_), V(mu_), 1.0 / 64, ALU.mult)
                            b3 = lambda t: t.a().rearrange("p (c o) -> p c o", o=1).to_broadcast([64, NCH, CH])
                            k.tt("dve", V(oacc), V(oacc), (mu_, b3(mu_)), ALU.subtract)
                            k.tt("pool", V(cen), V(oacc), V(oacc), ALU.mult)
                            P.op("dve", lambda e: e.tensor_reduce(out=var.a(), in_=cen.a(), axis=AX.X, op=ALU.add), reads=[cen], writes=[var])
                            k.ts("dve", V(var), V(var), 1.0 / 64, ALU.mult, 64e-5, ALU.add)
                            k.act(V(var), V(var), AF.Sqrt)
                            k.recip(V(var), V(var))
                            k.tt("dve", V(oacc), V(oacc), (var, b3(var)), ALU.mult)
                            rb = lambda t: t[:, h * 64:(h + 1) * 64].rearrange("p (o d) -> p o d", o=1).to_broadcast([64, NCH, CH])
                            k.tt("pool", V(oacc), V(oacc), (lnw, rb(lnw)), ALU.mult)
                            k.tt("dve", V(oacc), V(oacc), (lnb, rb(lnb)), ALU.add)
                            k.tt("pool", V(cen), V(Vt), (bon, b3(bon)), ALU.mult)
                            k.tt("dve", V(oacc), V(oacc), V(cen), ALU.add)
                            k.tt("dve", V(oacc), V(oacc), V(gt), ALU.mult)
                            k.dma((YM, YM[:, h * 64:(h + 1) * 64].rearrange("(c p) d -> p c d", p=CH), ("r", h)), V(oacc))
                        P.barrier()

        def lat_rows(tt, cc):
            c0 = 2 * (tt - 2) + cc
            return XL[LC:, :].rearrange("(r c) d -> c r d", c=64)[c0]

        def stage_outproj(YM, nfeat, W, xsrc, perm, tiles):
            kf = nfeat // 128
            with ExitStack() as ph:
                wb = P.sb("wob", [128, kf, D], BF16, es=ph)
                k.dma(V(wb), (W[0], W[1].rearrange("(ko p) n -> p ko n", p=128)), eng="pool")
                yt = [P.sb(f"yt{i}", [128, nfeat], F32, es=ph) for i in range(2)]
                yT = [P.sb(f"yT{i}", [128, kf, 128], BF16, es=ph) for i in range(2)]
                xo = [P.sb(f"xo{i}", [128, D], F32, es=ph) for i in range(2)]
                xn_ = [P.sb(f"xq{i}", [128, D], F32, es=ph) for i in range(2)]
                pT = [P.ps(f"poT{i}", [128, 8, 128], F32, es=ph) for i in range(2)]
                po = [P.ps(f"poo{i}", [128, 512], F32, es=ph) for i in range(2)]
                for n_, tt in enumerate(tiles):
                    j = 1 if tt < 2 else 0
                    y_ = yt[n_ % 2]; yT_ = yT[n_ % 2]; x_ = xo[n_ % 2]; q_ = xn_[n_ % 2]
                    k.dma(V(y_), (YM, YM[tt * 128:(tt + 1) * 128, :]))
                    if perm and tt >= 2:
                        for cc in range(2):
                            k.dma((x_, x_[cc * 64:(cc + 1) * 64, :], cc), (xsrc(tt)[0], lat_rows(tt, cc), tt))
                    else:
                        sb_, sap = xsrc(tt)
                        k.dma(V(x_), (sb_, sap, tt))
                    for g8 in range(0, kf, 8):
                        p_ = pT[(g8 // 8 + n_) % 2]
                        for ko in range(8):
                            k.tr((p_, p_[:, ko, :]), (y_, y_[:, (g8 + ko) * 128:(g8 + ko + 1) * 128]), V(ident))
                        k.cp("act", (yT_, yT_[:, g8:g8 + 8, :]), V(p_))
                    for nb in range(2):
                        o_ = po[nb]
                        for ko in range(kf):
                            k.mm(V(o_), (yT_, yT_[:, ko, :]), (wb, wb[:, ko, nb * 512:(nb + 1) * 512]), start=(ko == 0), stop=(ko == kf - 1))
                        k.tt("dve", (q_, q_[:, nb * 512:(nb + 1) * 512]), V(o_), (modB, modB[:, 0, j, nb * 512:(nb + 1) * 512]), ALU.mult)
                    k.tt("pool", V(q_), V(q_), V(x_), ALU.add)
                    if perm and tt >= 2:
                        for cc in range(2):
                            k.dma((XL, lat_rows(tt, cc), tt), (q_, q_[cc * 64:(cc + 1) * 64, :]))
                    else:
                        k.dma((XL, XL[tt * 128:(tt + 1) * 128, :], tt), V(q_))
                P.barrier()

        def stage_moe(l, tiles):
            nh = len(tiles) // 2
            with ExitStack() as ph:
                rw32 = P.sb("rw32", [128, KO, 32], F32, es=ph)
                k.dma(V(rw32), V(I["router_w"], I["router_w"][l].rearrange("(ko p) e -> p ko e", p=128)))
                rbB = P.sb("rbB", [128, 32], F32, es=ph)
                k.dma(V(rbB), V(I["router_b"], I["router_b"][l].partition_broadcast(128)))
                BD = P.sb("BD", [32, D], F32, es=ph)
                k.dma(V(BD), V(I["exp_b_down"], I["exp_b_down"][l]))
                bgF = P.sb("bgF", [128, 32, 8], F32, es=ph)
                buF = P.sb("buF", [128, 32, 8], F32, es=ph)
                with ExitStack() as t0:
                    btmp = P.sb("btmp", [128, 128], F32, es=t0)
                    pbt = P.ps("pbt", [128, 128], F32, es=t0)
                    for (dst, src) in ((bgF, I["exp_b_gate"]), (buF, I["exp_b_up"])):
                        for hf in range(2):
                            k.dma(V(btmp), (src, src[l, hf * 16:(hf + 1) * 16, :].rearrange("e (fb p) -> (e fb) p", p=128)))
                            k.tr(V(pbt), V(btmp), V(ident))
                            k.cp("dve", (dst, dst[:, hf * 16:(hf + 1) * 16, :].rearrange("p e f -> p (e f)")), V(pbt))
                    P.barrier()
                for half in range(2):
                    htiles = tiles[half * nh:(half + 1) * nh]
                    NTK = nh * 128
                    with ExitStack() as hs:
                        HTh = P.sb("HTh", [128, KO, NTK], BF16, es=hs)
                        LG = P.sb("LG", [128, nh, 32], F32, es=hs)
                        GW = P.sb("GW", [128, nh, 32], F32, es=hs)
                        acc = P.sb("acc", [128, nh, D], F32, es=hs)
                        with ExitStack() as ns:
                            xt = [P.sb(f"mxt{i}", [128, D], F32, es=ns) for i in range(2)]
                            xn = [P.sb(f"mxn{i}", [128, D], F32, es=ns) for i in range(2)]
                            junk = P.sb("mjunk", [128, D], F32, es=ns)
                            st = [P.sb(f"mst{i}", [128, 4], F32, es=ns) for i in range(2)]
                            tmp = [P.sb(f"mtmp{i}", [128, KO, 128], F32, es=ns) for i in range(2)]
                            h32 = [P.sb(f"mh32{i}", [128, KO, 128], F32, es=ns) for i in range(2)]
                            pT = [P.ps(f"mpT{i}", [128, KO, 128], F32, es=ns) for i in range(2)]
                            plg = [P.ps(f"plg{i}", [128, 32], F32, es=ns) for i in range(2)]
                            pgt = P.ps("pgt", [32, 128], F32, es=ns)
                            GWT = P.sb("GWT", [32, nh, 128], F32, es=ns)
                            pini = [P.ps("pini0", [128, 512], F32, es=ns)] * 2
                            m8 = P.sb("m8", [128, 8], F32, es=ns)
                            msk = P.sb("msk", [128, 32], F32, es=ns)
                            ex = P.sb("ex", [128, 32], F32, es=ns)
                            sm = P.sb("smx", [128, 4], F32, es=ns)
                            for i, tt in enumerate(htiles):
                                j = 1 if tt < 2 else 0
                                x_ = xt[i % 2]; n_ = xn[i % 2]; s_ = st[i % 2]; t_ = tmp[i % 2]; p_ = pT[i % 2]; h_ = h32[i % 2]
                                k.dma(V(x_), (XL, XL[tt * 128:(tt + 1) * 128, :]))
                                k.memset("pool", (s_, s_[:, 0:1]), 0.0)
                                k.act(V(junk), V(x_), AF.Square, accum=(s_, s_[:, 0:1]))
                                k.ts("dve", (s_, s_[:, 1:2]), (s_, s_[:, 0:1]), 1.0 / D, ALU.mult, EPS, ALU.add)
                                k.act((s_, s_[:, 2:3]), (s_, s_[:, 1:2]), AF.Sqrt)
                                k.recip((s_, s_[:, 3:4]), (s_, s_[:, 2:3]))
                                k.ts("dve", V(n_), V(x_), (s_, s_[:, 3:4]), ALU.mult)
                                for ko in range(KO):
                                    k.tr((p_, p_[:, ko, :]), (n_, n_[:, ko * 128:(ko + 1) * 128]), V(ident))
                                k.tt("dve", V(t_), V(p_), (g2F, g2F[:, :, j:j + 1].to_broadcast([128, KO, 128])), ALU.mult)
                                k.tt("pool", V(h_), V(t_), (modF, modF[:, 3, :, j:j + 1].to_broadcast([128, KO, 128])), ALU.add)
                                k.cp("act", (HTh, HTh[:, :, i * 128:(i + 1) * 128], i), V(h_))
                                pl = plg[i % 2]
                                for ko in range(KO):
                                    k.mm(V(pl), (h_, h_[:, ko, :]), (rw32, rw32[:, ko, :]), start=(ko == 0), stop=(ko == KO - 1))
                                lg = (LG, LG[:, i, :], i)
                                k.tt("dve", lg, V(pl), V(rbB), ALU.add)
                                P.op("dve", lambda e, i=i: e.max(out=m8.a(), in_=LG[:, i, :]), reads=[(LG, i)], writes=[m8])
                                k.ts("dve", V(msk), lg, (m8, m8[:, 3:4]), ALU.is_ge)
                                k.ts("dve", (sm, sm[:, 0:1]), (m8, m8[:, 0:1]), -1.0, ALU.mult)
                                k.act(V(ex), lg, AF.Exp, bias=(sm, sm[:, 0:1]))
                                k.tt("dve", V(ex), V(ex), V(msk), ALU.mult)
                                P.op("dve", lambda e: e.tensor_reduce(out=sm[:, 1:2], in_=ex.a(), axis=AX.X, op=ALU.add), reads=[ex], writes=[sm])
                                k.recip((sm, sm[:, 2:3]), (sm, sm[:, 1:2]))
                                k.ts("dve", (GW, GW[:, i, :], i), V(ex), (sm, sm[:, 2:3]), ALU.mult)
                                k.tr(V(pgt), (GW, GW[:, i, :], i), V(ident))
                                k.cp("act", (GWT, GWT[:, i, :], i), V(pgt))
                                for nb in range(2):
                                    k.mm(V(pini[nb]), (GWT, GWT[:, i, :], i), (BD, BD[:, nb * 512:(nb + 1) * 512]))
                                    k.cp("act" if nb else "dve", (acc, acc[:, i, nb * 512:(nb + 1) * 512], (i, nb)), V(pini[nb]))
                            P.barrier()
                        if f"LG{l}" in debug and half == 0:
                            dl = P.dram(f"d_GW{l}", [128, nh * 32], F32, kind="ExternalOutput")
                            k.dma(V(dl), (GW, GW.a().rearrange("p a b -> p (a b)")))
                        with ExitStack() as xs:
                            wgs = [P.sb(f"wg{i}", [128, KO, 512], BF16, es=xs) for i in range(2)]
                            wus = [P.sb(f"wu{i}", [128, KO, 512], BF16, es=xs) for i in range(2)]
                            wds = [P.sb(f"wd{i}", [128, 4, D], BF16, es=xs) for i in range(2)]
                            aTs = [P.sb(f"aT{i}", [128, 4, 512], BF16, es=xs) for i in range(2)]
                            dtmp = [P.sb(f"dtmp{i}", [128, 512], F32, es=xs) for i in range(2)]
                            g1 = [P.sb(f"g1_{i}", [128, 512], F32, es=xs) for i in range(2)]
                            sg = [P.sb(f"sg_{i}", [128, 512], F32, es=xs) for i in range(2)]
                            u1 = [P.sb(f"u1_{i}", [128, 512], F32, es=xs) for i in range(2)]
                            pg = [P.ps(f"mpg{i}", [128, 512], F32, es=xs) for i in range(2)]
                            pu = [P.ps(f"mpu{i}", [128, 512], F32, es=xs) for i in range(2)]
                            pd = [P.ps(f"mpd{i}", [128, 512], F32, es=xs) for i in range(2)]
                            if nh == 17:
                                tws = [512, 512, 384, 384, 384]
                            else:
                                tws = [512] * (NTK // 512)
                            tblocks = []
                            t_acc = 0
                            for tw in tws:
                                tblocks.append((t_acc, tw)); t_acc += tw
                            nexp = 32 if "moe_fast" not in debug else 2
                            cnt = 0
                            nblk = 0
                            cntbox = [0]

                            def emit_loads(he):
                                e, fh = he // 2, he % 2
                                wg = wgs[he % 2]; wu = wus[he % 2]; wd = wds[he % 2]
                                k.dma(V(wg), (I["exp_w_gate"], I["exp_w_gate"][l, e][:, fh * 512:(fh + 1) * 512].rearrange("(ko p) n -> p ko n", p=128)), eng="pool")
                                k.dma(V(wu), (I["exp_w_up"], I["exp_w_up"][l, e][:, fh * 512:(fh + 1) * 512].rearrange("(ko p) n -> p ko n", p=128)), eng="pool")
                                k.dma(V(wd), (I["exp_w_down"], I["exp_w_down"][l, e][fh * 512:(fh + 1) * 512, :].rearrange("(fo p) n -> p fo n", p=128)), eng="pool")

                            def emit_G(j, he, t0_, tw):
                                e, fh = he // 2, he % 2
                                wg = wgs[he % 2]; wu = wus[he % 2]
                                aT = aTs[j % 2]
                                for fbl in range(4):
                                    fb = fh * 4 + fbl
                                    cnt = cntbox[0]; cntbox[0] += 1
                                    pg_ = pg[cnt % 2]; pu_ = pu[cnt % 2]; g_ = g1[cnt % 2]; s_ = sg[cnt % 2]; u_ = u1[cnt % 2]
                                    for ko in range(KO):
                                        k.mm((pg_, pg_[:, 0:tw]), (wg, wg[:, ko, fbl * 128:(fbl + 1) * 128]), (HTh, HTh[:, ko, t0_:t0_ + tw]),
                                             start=(ko == 0), stop=(ko == KO - 1))
                                    for ko in range(KO):
                                        k.mm((pu_, pu_[:, 0:tw]), (wu, wu[:, ko, fbl * 128:(fbl + 1) * 128]), (HTh, HTh[:, ko, t0_:t0_ + tw]),
                                             start=(ko == 0), stop=(ko == KO - 1))
                                    k.ts("dve", (g_, g_[:, 0:tw]), (pg_, pg_[:, 0:tw]), (bgF, bgF[:, e, fb:fb + 1]), ALU.add, 7.0, ALU.min)
                                    k.act((s_, s_[:, 0:tw]), (g_, g_[:, 0:tw]), AF.Sigmoid, scale=1.702)
                                    k.act((u_, u_[:, 0:tw]), (pu_, pu_[:, 0:tw]), AF.Identity, bias=(buF, buF[:, e, fb:fb + 1]))
                                    k.ts("dve", (u_, u_[:, 0:tw]), (u_, u_[:, 0:tw]), 7.0, ALU.min, -7.0, ALU.max)
                                    k.tt("dve", (g_, g_[:, 0:tw]), (g_, g_[:, 0:tw]), (s_, s_[:, 0:tw]), ALU.mult)
                                    k.stt("dve", (aT, aT[:, fbl, 0:tw], fbl), (u_, u_[:, 0:tw]), 1.0, (g_, g_[:, 0:tw]), ALU.add, ALU.mult)

                            def emit_D(j, he, t0_, tw):
                                e = he // 2
                                wd = wds[he % 2]
                                aT = aTs[j % 2]
                                for ti in range(tw // 128):
                                    i = t0_ // 128 + ti
                                    for nb in range(2):
                                        pd_ = pd[nb]
                                        for fo in range(4):
                                            k.mm(V(pd_), (aT, aT[:, fo, ti * 128:(ti + 1) * 128], fo), (wd, wd[:, fo, nb * 512:(nb + 1) * 512]),
                                                 start=(fo == 0), stop=(fo == 3))
                                        asl = (acc, acc[:, i, nb * 512:(nb + 1) * 512], (i, nb))
                                        if nb == 0:
                                            k.stt("dve", asl, V(pd_), (GW, GW[:, i, e:e + 1], i), asl, ALU.mult, ALU.add)
                                        else:
                                            dt_ = dtmp[i % 2]
                                            k.act(V(dt_), V(pd_), AF.Copy, scale=(GW, GW[:, i, e:e + 1], i))
                                            k.tt("dve", asl, asl, V(dt_), ALU.add)

                            blocks = [(he, t0_, tw) for he in range(nexp * 2) for (t0_, tw) in tblocks]
                            emit_loads(0)
                            emit_G(0, *blocks[0])
                            for j in range(len(blocks)):
                                if j + 1 < len(blocks):
                                    if blocks[j + 1][0] != blocks[j][0]:
                                        emit_loads(blocks[j + 1][0])
                                    emit_G(j + 1, *blocks[j + 1])
                                emit_D(j, *blocks[j])
                            P.barrier()
                        with ExitStack() as rs:
                            xr = [P.sb(f"xr{i}", [128, D], F32, es=rs) for i in range(2)]
                            for i, tt in enumerate(htiles):
                                j = 1 if tt < 2 else 0
                                x_ = xr[i % 2]
                                k.dma(V(x_), (XL, XL[tt * 128:(tt + 1) * 128, :], ("m", tt)))
                                k.tt("dve", (acc, acc[:, i, :]), (acc, acc[:, i, :]), (modB, modB[:, 1, j, :]), ALU.mult)
                                k.tt("pool", V(x_), V(x_), (acc, acc[:, i, :]), ALU.add)
                                k.dma((XL, XL[tt * 128:(tt + 1) * 128, :], ("m", tt)), V(x_))
                            P.barrier()

        def mixer_rwkv2(PR, GT, YM):
            BW = 1088
            NB = L // BW
            CB = BW // CH
            NL = 5
            CBU = 4
            NSS = NCH // CBU
            SUB = ((0, 512), (512, 512), (1024, 64))
            ARD = P.dram("ARD", [16, 64, NCH * 128], BF16)
            BKD = P.dram("BKD", [16, 64, NCH * 128], BF16)
            BKTD = P.dram("BKTD", [16, 128, NCH * 64], BF16)
            VVD = P.dram("VVD", [8, 128, NCH * 64], BF16)
            VTD = P.dram("VTD", [8, 64, NCH * 64], F32)
            OD = P.dram("OD", [2, L, 512], F32)
            with ExitStack() as ph:
                w2all = P.sb("w2all", [128, 512], F32, es=ph)
                a2all = P.sb("a2all", [128, 512], F32, es=ph)
                k.dma(V(w2all), V(I["rwkv_w2"], I["rwkv_w2"][0].rearrange("z r c -> (z r) c")))
                k.dma(V(a2all), V(I["rwkv_a2"], I["rwkv_a2"][0].rearrange("z r c -> (z r) c")))

                def hv(name, src):
                    t = P.sb(name, [64, 8], F32, es=ph)
                    k.dma(V(t), (src[0], src[1].rearrange("(h n) -> n h", n=64)), allow_slow_non_contiguous=True)
                    return t
                w0 = [hv(f"w0_{z}", (I["rwkv_w0"], I["rwkv_w0"][0, z])) for z in range(2)]
                a0 = [hv(f"a0_{z}", (I["rwkv_a0"], I["rwkv_a0"][0, z])) for z in range(2)]
                kkg = hv("kkg", (I["rwkv_k_k"], I["rwkv_k_k"][0]))
                kag = hv("kag", (I["rwkv_k_a"], I["rwkv_k_a"][0]))
                rkg = hv("rkg", (I["rwkv_r_k"], I["rwkv_r_k"][0]))
                oka = P.sb("oka", [64, 8], F32, es=ph)
                k.ts("dve", V(oka), V(kag), -1.0, ALU.mult, 1.0, ALU.add)
                lnw = P.sb("lnw", [64, 512], F32, es=ph)
                lnb = P.sb("lnb", [64, 512], F32, es=ph)
                k.dma(V(lnw), V(I["rwkv_ln_w"], I["rwkv_ln_w"][0].partition_broadcast(64)))
                k.dma(V(lnb), V(I["rwkv_ln_b"], I["rwkv_ln_b"][0].partition_broadcast(64)))
                ones64 = P.sb("ones64", [64, 64], F32, es=ph)
                k.memset("pool", V(ones64), 1.0)
                m4 = []
                m3 = []
                for z in range(2):
                    m = P.sb(f"m4_{z}", [128, 2, 64], F32, es=ph)
                    sgn = 1 if z == 0 else -1
                    for half in range(2):
                        for col in range(2):
                            base = (-1 if col == 1 else 0)
                            P.op("pool", lambda e, m=m, half=half, col=col, sgn=sgn, base=base: e.affine_select(
                                out=m[half * 64:(half + 1) * 64, col, :], in_=ones[half * 64:(half + 1) * 64, 0:64],
                                pattern=[[sgn, 64]], compare_op=ALU.is_ge, fill=0.0, base=base, channel_multiplier=-sgn),
                                reads=[ones], writes=[m])
                    m4.append(m)
                    mm3 = P.sb(f"m3_{z}", [64, 64], F32, es=ph)
                    P.op("pool", lambda e, mm3=mm3, z=z: e.affine_select(
                        out=mm3.a(), in_=ones[0:64, 0:64], pattern=[[-1 if z == 0 else 1, 64]], compare_op=ALU.is_ge, fill=0.0,
                        base=-1, channel_multiplier=1 if z == 0 else -1), reads=[ones], writes=[mm3])
                    m3.append(mm3)
                QI0 = P.sb("QI0", [64, 64], BF16, es=ph)
                k.cp("dve", V(QI0), (ident, ident[0:64, 0:64]))
                gCall = P.sb("gCall", [64, 16, NCH], F32, es=ph)
                bonall = P.sb("bonall", [64, 8, NCH], F32, es=ph)
                with ExitStack() as pp_:
                    rst = P.sb("rst", [64, BW], F32, es=pp_)
                    k.memset("pool", V(rst), 1.0)
                    k.memset("pool", (rst, rst.a().rearrange("p (c t) -> p c t", t=CH)[:, :, 0:1]), 0.0)

                    def T(name, dt=F32):
                        return P.sb(name, [64, BW], dt, es=pp_)
                    t_r = T("t_r"); t_k = T("t_k"); t_v = T("t_v")
                    t_wd = P.sb("t_wd", [128, BW], F32, es=pp_)
                    t_ad = P.sb("t_ad", [128, BW], F32, es=pp_)
                    t_kk = T("t_kk"); t_q = T("t_q"); t_rn = T("t_rn")
                    t_sg = T("t_sg"); t_cs = T("t_cs"); t_x = T("t_x"); t_y = T("t_y")
                    t_eg = T("t_eg"); t_eng = T("t_eng"); t_egp = T("t_egp")
                    t_a = T("t_a"); t_km = [T("t_km0"), T("t_km1")]; t_b = T("t_b")
                    t_v2 = P.sb("t_v2", [64, CB, 2, CH], F32, es=pp_)
                    arb = P.sb("arb", [64, CB, 2, CH], BF16, es=pp_)
                    bkb = P.sb("bkb", [64, CB, 2, CH], BF16, es=pp_)
                    bke = P.sb("bke", [64, CB, 2, CH], BF16, es=pp_)
                    bktb = P.sb("bktb", [128, CB, CH], BF16, es=pp_)
                    zvb = P.sb("zvb", [128, CB, CH], BF16, es=pp_)
                    vtb = P.sb("vtb", [64, CB, CH], F32, es=pp_)
                    pwa = [P.ps(f"pwa{i}", [64, 512], F32, es=pp_) for i in range(2)]
                    pss = P.ps("pss", [64, 512], F32, es=pp_)
                    ptv = P.ps("ptv", [128, 4, CH], F32, es=pp_)
                    ptb = P.ps("ptb", [128, 4, CH], BF16, es=pp_)
                    pbn = P.ps("pbn", [64, NCH], F32, es=pp_)
                    k.memset("pool", (zvb, zvb[0:64, :, :], "z"), 0.0)
                    c3 = lambda t: t.a().rearrange("p (c t) -> p c t", t=CH)
                    for h in range(8):
                        for blk in range(NB):
                            tsl = slice(blk * BW, (blk + 1) * BW)
                            csl = slice(blk * CB, (blk + 1) * CB)
                            k.dma(V(t_r), (PR, PR[h * 64:(h + 1) * 64, tsl]))
                            k.dma(V(t_k), (PR, PR[512 + h * 64:512 + (h + 1) * 64, tsl]))
                            k.dma(V(t_v), (PR, PR[1024 + h * 64:1024 + (h + 1) * 64, tsl]))
                            k.dma(V(t_wd), (PR, PR[1536:1664, tsl]))
                            k.dma(V(t_ad), (PR, PR[1664:1792, tsl]))
                            k.act(V(t_wd), V(t_wd), AF.Tanh)
                            k.cp("pool", V(t_v2), (t_v, t_v.a().rearrange("p (c o t) -> p c o t", o=1, t=CH).to_broadcast([64, CB, 2, CH])))
                            for j0 in range(0, CB, 4):
                                nj = min(4, CB - j0)
                                for j in range(nj):
                                    k.tr((ptv, ptv[:, j, :]), (t_v2, t_v2[:, j0 + j, :, :].rearrange("p a t -> p (a t)")), (ident, ident[0:64, 0:64]))
                                k.cp("act", (vtb, vtb[:, j0:j0 + nj, :], j0), (ptv, ptv[0:64, 0:nj, :]))
                                k.cp("dve", (zvb, zvb[64:128, j0:j0 + nj, :], ("v", j0)), (ptv, ptv[64:128, 0:nj, :]))
                            k.dma((VTD, VTD[h][:, blk * CB * CH:(blk + 1) * CB * CH], (h, blk)), (vtb, vtb.a().rearrange("p c t -> p (c t)")))
                            k.dma((VVD, VVD[h][:, blk * CB * CH:(blk + 1) * CB * CH], (h, blk)), (zvb, zvb.a().rearrange("p c t -> p (c t)")))
                            k.ts("dve", V(t_kk), V(t_k), (kkg, kkg[:, h:h + 1]), ALU.mult)
                            k.tt("pool", V(t_q), V(t_kk), V(t_kk), ALU.mult)
                            for (s0, sw) in SUB:
                                k.mm((pss, pss[:, 0:sw]), V(ones64), (t_q, t_q[:, s0:s0 + sw]))
                                k.ts("dve", (t_rn, t_rn[:, s0:s0 + sw], s0), (pss, pss[:, 0:sw]), 1e-12, ALU.add)
                            k.act(V(t_rn), V(t_rn), AF.Sqrt)
                            k.recip(V(t_rn), V(t_rn))
                            k.tt("dve", V(t_kk), V(t_kk), V(t_rn), ALU.mult)
                            for z in range(2):
                                end = CH - 1 if z == 0 else 0
                                zs = slice(z * 64, (z + 1) * 64)
                                for (s0, sw) in SUB:
                                    pw = pwa[0]; pa = pwa[1]
                                    k.mm((pw, pw[:, 0:sw]), (w2all, w2all[zs, h * 64:(h + 1) * 64]), (t_wd, t_wd[zs, s0:s0 + sw]))
                                    k.mm((pa, pa[:, 0:sw]), (a2all, a2all[zs, h * 64:(h + 1) * 64]), (t_ad, t_ad[zs, s0:s0 + sw]))
                                    k.act((t_sg, t_sg[:, s0:s0 + sw], s0), (pw, pw[:, 0:sw]), AF.Sigmoid, bias=(w0[z], w0[z][:, h:h + 1]))
                                    k.act((t_a, t_a[:, s0:s0 + sw], s0), (pa, pa[:, 0:sw]), AF.Sigmoid, bias=(a0[z], a0[z][:, h:h + 1]))
                                k.scan(V(t_cs), V(rst), V(t_sg), 0.0, ALU.mult, ALU.add)
                                if z == 1:
                                    k.tt("dve", V(t_x), V(t_sg), V(t_cs), ALU.subtract)
                                    k.tt("dve", (t_y, c3(t_y)), (t_x, c3(t_x)), (t_cs, c3(t_cs)[:, :, CH - 1:CH].to_broadcast([64, CB, CH])), ALU.add)
                                    cs = t_y
                                else:
                                    cs = t_cs
                                k.act(V(t_eg), V(cs), AF.Exp, scale=-0.6065306597126334)
                                k.act(V(t_eng), V(cs), AF.Exp, scale=0.6065306597126334)
                                k.tt("dve", V(t_x), V(cs), V(t_sg), ALU.subtract)
                                k.act(V(t_egp), V(t_x), AF.Exp, scale=-0.6065306597126334)
                                eg3 = c3(t_eg)
                                k.cp("pool", (gCall, gCall[:, h * 2 + z, csl], (h, z, blk)), (t_eg, eg3[:, :, end]))
                                k.ts("dve", V(t_x), V(t_a), (kag, kag[:, h:h + 1]), ALU.mult, (oka, oka[:, h:h + 1]), ALU.add)
                                k.tt("dve", V(t_km[z]), V(t_k), V(t_x), ALU.mult)
                                k.tt("pool", V(t_b), V(t_kk), V(t_a), ALU.mult)
                                k.stt("dve", (arb, arb[:, :, 1, :], 1), (t_kk, c3(t_kk)), -1.0, (t_egp, c3(t_egp)), ALU.mult, ALU.mult)
                                k.tt("pool", (arb, arb[:, :, 0, :], 0), (t_r, c3(t_r)), (t_eg, eg3), ALU.mult)
                                k.tt("dve", V(t_b), V(t_b), V(t_eng), ALU.mult)
                                k.tt("dve", V(t_x), V(t_km[z]), V(t_eng), ALU.mult)
                                k.cp("pool", (bkb, bkb[:, :, 0, :], 0), (t_b, c3(t_b)))
                                k.cp("act", (bkb, bkb[:, :, 1, :], 1), (t_x, c3(t_x)))
                                gcb = eg3[:, :, end:end + 1].to_broadcast([64, CB, CH])
                                k.tt("dve", (bke, bke[:, :, 0, :], 0), (t_b, c3(t_b)), (t_eg, gcb), ALU.mult)
                                k.tt("pool", (bke, bke[:, :, 1, :], 1), (t_x, c3(t_x)), (t_eg, gcb), ALU.mult)
                                for j0 in range(0, CB, 4):
                                    nj = min(4, CB - j0)
                                    for j in range(nj):
                                        k.tr((ptb, ptb[:, j, :]), (bke, bke[:, j0 + j, :, :].rearrange("p a t -> p (a t)")), (identb, identb[0:64, 0:64]))
                                    k.cp("act", (bktb, bktb[:, j0:j0 + nj, :], j0), (ptb, ptb[:, 0:nj, :]))
                                hz = h * 2 + z
                                k.dma((ARD, ARD[hz][:, blk * CB * 128:(blk + 1) * CB * 128], (hz, blk)), (arb, arb.a().rearrange("p c a t -> p (c a t)")))
                                k.dma((BKD, BKD[hz][:, blk * CB * 128:(blk + 1) * CB * 128], (hz, blk)), (bkb, bkb.a().rearrange("p c a t -> p (c a t)")))
                                k.dma((BKTD, BKTD[hz][:, blk * CB * CH:(blk + 1) * CB * CH], (hz, blk)), (bktb, bktb.a().rearrange("p c t -> p (c t)")))
                            k.tt("dve", V(t_x), V(t_km[0]), V(t_km[1]), ALU.add)
                            k.stt("dve", V(t_x), V(t_r), (rkg, rkg[:, h:h + 1]), V(t_x), ALU.mult, ALU.mult)
                            for j in range(CB):
                                c = blk * CB + j
                                k.mm((pbn, pbn[:, c:c + 1]), (t_x, t_x[:, j * CH:(j + 1) * CH]), (ones64, ones64[:, 0:1]))
                        k.cp("dve", (bonall, bonall[:, h, :], h), V(pbn))
                    P.barrier()
                for ps_ in range(2 if "rw_preponly" not in debug else 0):
                    with ExitStack() as us:
                        chains = [(ps_ * 4 + hl, z) for hl in range(4) for z in range(2)]
                        CHN = []
                        for ci, (h, z) in enumerate(chains):
                            d_ = {}
                            d_["h"] = h; d_["z"] = z; d_["hz"] = h * 2 + z; d_["ci"] = ci
                            d_["Hs"] = P.sb(f"Hs{ci}", [64, 64], F32, es=us)
                            d_["Hb"] = P.sb(f"Hb{ci}", [64, 64], BF16, es=us)
                            d_["AM"] = [P.sb(f"AM{ci}_{i}", [128, 192], BF16, es=us) for i in range(2)]
                            for am_ in d_["AM"]:
                                k.cp("dve", (am_, am_[0:64, 128:192], "I"), (ident, ident[0:64, 0:64]))
                            d_["Pm"] = [P.sb(f"Pm{ci}_{i}", [64, 64], BF16, es=us) for i in range(1)]
                            d_["QR"] = [P.sb(f"QR{ci}_{i}", [64, 3, 64], BF16, es=us) for i in range(2)]
                            d_["TT"] = [P.sb(f"TT{ci}_{i}", [64, 64], BF16, es=us) for i in range(2)]
                            d_["Xs"] = P.sb(f"Xs{ci}", [64, 64], BF16, es=us)
                            d_["ARb"] = [P.sb(f"ARb{ci}_{i}", [64, CBU, 2, CH], BF16, es=us) for i in range(2)]
                            d_["BKb"] = [P.sb(f"BKb{ci}_{i}", [64, CBU, 2, CH], BF16, es=us) for i in range(2)]
                            d_["BKTb"] = [P.sb(f"BKTb{ci}_{i}", [128, CBU, CH], BF16, es=us) for i in range(2)]
                            d_["UVb"] = [P.sb(f"UVb{ci}_{i}", [128, CBU, CH], BF16, es=us) for i in range(2)]
                            d_["ZVb"] = [P.sb(f"ZVb{ci}_{i}", [128, CBU, CH], BF16, es=us) for i in range(2)]
                            d_["Ob"] = [P.sb(f"Ob{ci}_{i}", [64, CBU, CH], F32, es=us) for i in range(2)]
                            d_["bank"] = P.ps(f"bank{ci}", [128, 512], F32, es=us)
                            k.memset("pool", V(d_["Hs"]), 0.0)
                            k.memset("pool", V(d_["Hb"]), 0.0)
                            CHN.append(d_)

                        def clo(z, ss):
                            if z == 0:
                                return ss * CBU
                            return 0 if ss == 0 else NCH - CBU * ss

                        def loads(ss):
                            for d_ in CHN:
                                c0 = clo(d_["z"], ss); i = ss % 2; hz = d_["hz"]; h = d_["h"]
                                k.dma((d_["ARb"][i], d_["ARb"][i].a().rearrange("p c a t -> p (c a t)")), (ARD, ARD[hz][:, c0 * 128:(c0 + CBU) * 128]))
                                k.dma((d_["BKb"][i], d_["BKb"][i].a().rearrange("p c a t -> p (c a t)")), (BKD, BKD[hz][:, c0 * 128:(c0 + CBU) * 128]))
                                k.dma((d_["BKTb"][i], d_["BKTb"][i].a().rearrange("p c t -> p (c t)")), (BKTD, BKTD[hz][:, c0 * CH:(c0 + CBU) * CH]))
                                k.dma((d_["ZVb"][i], d_["ZVb"][i].a().rearrange("p c t -> p (c t)")), (VVD, VVD[h][:, c0 * CH:(c0 + CBU) * CH]))
                                k.dma((d_["UVb"][i], d_["UVb"][i][64:128, :, :].rearrange("p c t -> p (c t)"), "v"), (VVD, VVD[h][64:128, c0 * CH:(c0 + CBU) * CH]))

                        def unit_stage(d_, ss, jj, st):
                            z = d_["z"]; h = d_["h"]; hz = d_["hz"]
                            i = ss % 2
                            c0 = clo(z, ss)
                            cl = jj if z == 0 else CBU - 1 - jj
                            c = c0 + cl
                            step = ss * CBU + jj
                            ci = d_["ci"]
                            bk_ = d_["bank"]
                            vM = bk_[:, 0:128]; vL = bk_[0:64, 128:256]; vP = bk_[0:64, 256:320]; vLP = bk_[0:64, 128:320]
                            vX = bk_[0:64, 320:384]; vU = bk_[0:64, 384:448]; vH = bk_[0:64, 448:512]
                            ARb = d_["ARb"][i]; BKb = d_["BKb"][i]; BKTb = d_["BKTb"][i]; UVb = d_["UVb"][i]; ZVb = d_["ZVb"][i]; Ob = d_["Ob"][i]
                            am = d_["AM"][step % 2]
                            tt_ = d_["TT"][step % 2]
                            Hb = d_["Hb"]; Hs = d_["Hs"]; Xs = d_["Xs"]
                            ev = "act" if ci % 2 else "dve"
                            if st == 0:
                                arc = ARb[:, cl, :, :].rearrange("p a t -> p (a t)")
                                bkc = BKb[:, cl, :, :].rearrange("p a t -> p (a t)")
                                k.mm((bk_, vM), (BKb, bkc), (ARb, arc))
                                k.mm((bk_, vP), (ARb, ARb[:, cl, 1, :]), (BKb, BKb[:, cl, 0, :]))
                                k.tt("dve", (am, am[:, 0:128], "m"), (bk_, vM), (m4[z], m4[z].a().rearrange("p a t -> p (a t)")), ALU.mult)
                                k.tt("dve", V(d_["Pm"][0]), (bk_, vP), V(m3[z]), ALU.mult)
                            elif 1 <= st <= NL:
                                lv = st
                                qn = d_["QR"][lv % 2]
                                if lv == 1:
                                    rqr = (am, am[0:64, 64:192]); rr = (am, am[0:64, 128:192]); lq = (am, am[0:64, 64:128]); pm_ = V(d_["Pm"][0])
                                else:
                                    qp = d_["QR"][(lv - 1) % 2]
                                    rqr = (qp, qp[:, 0:2, :].rearrange("p a t -> p (a t)")); rr = (qp, qp[:, 1, :]); lq = (qp, qp[:, 0, :]); pm_ = (qp, qp[:, 2, :])
                                k.mm((bk_, vL), pm_, rqr, start=True, stop=False)
                                k.mm((bk_, vL[:, 64:128]), V(QI0), rr, start=False, stop=True)
                                k.mm((bk_, vP), lq, pm_)
                                k.cp(ev, (qn, qn.a().rearrange("p a t -> p (a t)")), (bk_, vLP))
                            elif st == NL + 1:
                                qp = d_["QR"][NL % 2]
                                k.mm((bk_, vL[:, 0:64]), (qp, qp[:, 2, :]), (qp, qp[:, 1, :]), start=True, stop=False)
                                k.mm((bk_, vL[:, 0:64]), V(QI0), (qp, qp[:, 1, :]), start=False, stop=True)
                                k.cp(ev, V(tt_), (bk_, vL[:, 0:64]))
                            elif st == NL + 2:
                                k.mm((bk_, vX), (ARb, ARb[:, cl, 1, :]), V(Hb), start=True, stop=False)
                                P.op("pe", lambda e, vX=vX, am=am, ZVb=ZVb, cl=cl: e.matmul(vX, lhsT=am[:, 64:128], rhs=ZVb[:, cl, :], start=False, stop=True),
                                     reads=[(am, "m"), (ZVb, None)], writes=[(bk_, None)], pe_acc=True)
                                k.cp("act", V(Xs), (bk_, vX))
                            elif st == NL + 3:
                                k.mm((bk_, vU), V(tt_), V(Xs))
                                k.cp("dve", (UVb, UVb[0:64, cl, :], ("u", cl)), (bk_, vU))
                            elif st == NL + 4:
                                k.mm((bk_, vX), (ARb, ARb[:, cl, 0, :]), V(Hb), start=True, stop=False)
                                P.op("pe", lambda e, vX=vX, am=am, UVb=UVb, cl=cl: e.matmul(vX, lhsT=am[:, 0:64], rhs=UVb[:, cl, :], start=False, stop=True),
                                     reads=[(am, "m"), (UVb, ("u", cl)), (UVb, "v")], writes=[(bk_, None)], pe_acc=True)
                                P.op("pe", lambda e, vH=vH, BKTb=BKTb, UVb=UVb, cl=cl: e.matmul(vH, lhsT=BKTb[:, cl, :], rhs=UVb[:, cl, :], start=True, stop=True),
                                     reads=[(BKTb, None), (UVb, ("u", cl)), (UVb, "v")], writes=[(bk_, None)], pe_acc=True)
                                k.cp("act", (Ob, Ob[:, cl, :], cl), (bk_, vX))
                                k.stt("dve", V(Hs), V(Hs), (gCall, gCall[:, hz, c:c + 1]), (bk_, vH), ALU.mult, ALU.add)
                                k.cp("act", V(Hb), V(Hs))

                        loads(0)
                        for ss in range(NSS):
                            if ss + 1 < NSS:
                                loads(ss + 1)
                            i = ss % 2
                            for jj in range(CBU):
                                for st in range(NL + 5):
                                    for d_ in CHN:
                                        unit_stage(d_, ss, jj, st)
                            for d_ in CHN:
                                c0 = clo(d_["z"], ss); h = d_["h"]; z = d_["z"]
                                k.dma((OD, OD[z][c0 * CH:(c0 + CBU) * CH, h * 64:(h + 1) * 64].rearrange("(c p) d -> p c d", p=CH), (z, h, ss)), V(d_["Ob"][i]))
                        P.barrier()
                with ExitStack() as fs:
                    oacc = P.sb("oaccr", [64, NCH, CH], F32, es=fs)
                    o1 = P.sb("o1r", [64, NCH, CH], F32, es=fs)
                    Vt = P.sb("Vtr", [64, NCH, CH], F32, es=fs)
                    gt = P.sb("gtr", [64, NCH, CH], F32, es=fs)
                    cen = P.sb("cen", [64, NCH, CH], F32, es=fs)
                    mu_ = P.sb("mu_", [64, NCH], F32, es=fs)
                    var = P.sb("var", [64, NCH], F32, es=fs)
                    for h in range(8):
                        k.dma(V(oacc), (OD, OD[0][:, h * 64:(h + 1) * 64].rearrange("(c p) d -> p c d", p=CH)))
                        k.dma(V(o1), (OD, OD[1][:, h * 64:(h + 1) * 64].rearrange("(c p) d -> p c d", p=CH)))
                        k.dma(V(Vt), (VTD, VTD[h].rearrange("p (c t) -> p c t", t=CH)))
                        k.dma(V(gt), (GT, GT[:, h * 64:(h + 1) * 64].rearrange("(c p) d -> p c d", p=CH)))
                        k.tt("pool", V(oacc), V(oacc), V(o1), ALU.add)
                        P.op("dve", lambda e: e.tensor_reduce(out=mu_.a(), in_=oacc.a(), axis=AX.X, op=ALU.add), reads=[oacc], writes=[mu_])
                        k.ts("dve", V(mu_), V(mu_), 1.0 / 64, ALU.mult)
                        b3 = lambda t: t.a().rearrange("p (c o) -> p c o", o=1).to_broadcast([64, NCH, CH])
                        k.tt("dve", V(oacc), V(oacc), (mu_, b3(mu_)), ALU.subtract)
                        k.tt("pool", V(cen), V(oacc), V(oacc), ALU.mult)
                        P.op("dve", lambda e: e.tensor_reduce(out=var.a(), in_=cen.a(), axis=AX.X, op=ALU.add), reads=[cen], writes=[var])
                        k.ts("dve", V(var), V(var), 1.0 / 64, ALU.mult, 64e-5, ALU.add)
                        k.act(V(var), V(var), AF.Sqrt)
                        k.recip(V(var), V(var))
                        k.tt("dve", V(oacc), V(oacc), (var, b3(var)), ALU.mult)
                        rb = lambda t: t[:, h * 64:(h + 1) * 64].rearrange("p (o d) -> p o d", o=1).to_broadcast([64, NCH, CH])
                        k.tt("pool", V(oacc), V(oacc), (lnw, rb(lnw)), ALU.mult)
                        k.tt("dve", V(oacc), V(oacc), (lnb, rb(lnb)), ALU.add)
                        k.tt("pool", V(cen), V(Vt), (bonall, bonall[:, h, :].rearrange("p (c o) -> p c o", o=1).to_broadcast([64, NCH, CH])), ALU.mult)
                        k.tt("dve", V(oacc), V(oacc), V(cen), ALU.add)
                        k.tt("dve", V(oacc), V(oacc), V(gt), ALU.mult)
                        k.dma((YM, YM[:, h * 64:(h + 1) * 64].rearrange("(c p) d -> p c d", p=CH), ("r", h)), V(oacc))
                    P.barrier()

        SEGS = ((0, LC), (LC, SEQ))

        def stage_conv(PF, PC, specs):
            with ExitStack() as ph:
                u = [P.sb(f"cu{i}", [128, L + 8], F32, es=ph) for i in range(2)]
                acc = [P.sb(f"ca{i}", [128, L], F32, es=ph) for i in range(2)]
                cw = P.sb("cw", [128, 20, 5], F32, es=ph)
                cb = P.sb("cb", [128, 20], F32, es=ph)
                for i in range(2):
                    k.memset("pool", (u[i], u[i][:, 0:2], "h0"), 0.0)
                    k.memset("pool", (u[i], u[i][:, LC + 2:LC + 6], "h1"), 0.0)
                    k.memset("pool", (u[i], u[i][:, L + 6:L + 8], "h2"), 0.0)
                bi = 0
                for (row0, nblk, wsrc, bsrc) in specs:
                    for b in range(nblk):
                        k.dma((cw, cw[:, bi, :], bi), (wsrc[0], wsrc[1][:, b * 128:(b + 1) * 128].rearrange("j p -> p j")), allow_slow_non_contiguous=True)
                        k.dma((cb, cb[:, bi:bi + 1], bi), (bsrc[0], bsrc[1][b * 128:(b + 1) * 128].rearrange("(p o) -> p o", o=1)), allow_slow_non_contiguous=True)
                        u_ = u[bi % 2]; a_ = acc[bi % 2]
                        r0 = row0 + b * 128
                        k.dma((u_, u_[:, 2:LC + 2], "c"), (PF, PF[r0:r0 + 128, 0:LC]))
                        k.dma((u_, u_[:, LC + 6:L + 6], "l"), (PF, PF[r0:r0 + 128, LC:L]))
                        for (s0, sl_) in SEGS:
                            off = 0 if s0 == 0 else 4
                            for j in range(5):
                                src = (u_, u_[:, s0 + off + j:s0 + off + j + sl_])
                                dst = (a_, a_[:, s0:s0 + sl_], s0)
                                if j == 0:
                                    k.ts("dve", dst, src, (cw, cw[:, bi, 0:1], bi), ALU.mult)
                                else:
                                    k.stt("dve", dst, src, (cw, cw[:, bi, j:j + 1], bi), dst, ALU.mult, ALU.add)
                            k.act((a_, a_[:, s0:s0 + sl_], s0), (a_, a_[:, s0:s0 + sl_], s0), AF.Silu, bias=(cb, cb[:, bi:bi + 1], bi))
                        k.dma((PC, PC[r0:r0 + 128, :], r0), V(a_))
                        bi += 1
                P.barrier()

        def mixer_mlstm(PC, PF, PT, YM, GSD):
            TB = [(t0_, min(512, L - t0_)) for t0_ in range(0, L, 512)]
            with ExitStack() as ph:
                GI = P.sb("GI", [8, L], F32, es=ph)
                GF = P.sb("GF", [8, L], F32, es=ph)
                t1 = P.sb("gt1", [8, L], F32, es=ph)
                t2 = P.sb("gt2", [8, L], F32, es=ph)
                t3 = P.sb("gt3", [8, L], F32, es=ph)
                rst = P.sb("grst", [8, L], F32, es=ph)
                ib = P.sb("ib", [8, 1], F32, es=ph)
                fb = P.sb("fb", [8, 1], F32, es=ph)
                k.dma(V(GI), (PF, PF[2560:2568, :]))
                k.dma(V(GF), (PF, PF[2568:2576, :]))
                k.dma(V(ib), V(I["mlstm_i_bias"], I["mlstm_i_bias"][0].rearrange("z (h o) -> (z h) o", o=1)), allow_slow_non_contiguous=True)
                k.dma(V(fb), V(I["mlstm_f_bias"], I["mlstm_f_bias"][0].rearrange("z (h o) -> (z h) o", o=1)), allow_slow_non_contiguous=True)
                k.memset("pool", V(rst), 1.0)
                k.memset("pool", (rst, rst.a().rearrange("p (c t) -> p c t", t=CH)[:, :, 0:1]), 0.0)
                k.ts("dve", V(GI), V(GI), (ib, ib[:, 0:1]), ALU.add)
                k.ts("dve", V(fb), V(fb), -1.0, ALU.mult)
                k.act(V(t1), V(GF), AF.Exp, bias=(fb, fb[:, 0:1]), scale=-1.0)
                k.act(V(t1), V(t1), AF.Ln, bias=1.0)
                k.ts("dve", V(t1), V(t1), -1.0, ALU.mult)
                k.scan(V(t2), V(rst), V(t1), 0.0, ALU.mult, ALU.add)
                k.tt("dve", V(t3), V(t1), V(t2), ALU.subtract)
                p3 = t2.a().rearrange("p (c t) -> p c t", t=CH)
                k.tt("dve", (t3, t3.a().rearrange("p (c t) -> p c t", t=CH)), (t3, t3.a().rearrange("p (c t) -> p c t", t=CH)),
                     (t2, p3[:, :, CH - 1:CH].to_broadcast([8, NCH, CH])), ALU.add)
                k.dma((GSD, GSD[0:4, :], 0), (t2, t2[0:4, :]))
                k.dma((GSD, GSD[4:8, :], 1), (t3, t3[4:8, :]))
                k.tt("dve", V(t2), V(GI), V(t2), ALU.subtract)
                k.tt("dve", V(t3), V(GI), V(t3), ALU.subtract)
                k.dma((GSD, GSD[8:12, :], 2), (t2, t2[0:4, :]))
                k.dma((GSD, GSD[12:16, :], 3), (t3, t3[4:8, :]))
                P.barrier()
            with ExitStack() as ph:
                masks = make_masks(ph)
                nwB = P.sb("mnwB", [64, 1024], F32, es=ph)
                k.dma(V(nwB), V(I["mlstm_norm_w"], I["mlstm_norm_w"][0].partition_broadcast(64)))
                oacc = P.sb("moacc", [64, NCH, 256], F32, es=ph)
                ssn = P.sb("mssn", [64, NCH], F32, es=ph)
                for h in range(4):
                    with ExitStack() as hs1:
                        vaug = P.sb("vaug", [64, NCH, 257], BF16, es=hs1)
                        t_q = P.sb("mt_q", [128, 512], F32, es=hs1)
                        t_k = P.sb("mt_k", [128, 512], F32, es=hs1)
                        t_e = P.sb("mt_e", [128, 512], F32, es=hs1)
                        t_s = P.sb("mt_s", [128, 512], F32, es=hs1)
                        cd = P.sb("mcd", [128, NCH], F32, es=hs1)
                        q_in = P.sb("mq_in", [128, L], BF16, es=hs1)
                        k_in = P.sb("mk_in", [128, L], BF16, es=hs1)
                        k_end = P.sb("mk_end", [128, L], BF16, es=hs1)
                        kET = P.sb("mkET", [64, NCH, 128], BF16, es=hs1)
                        S = P.sb("mS", [128, 257], F32, es=hs1)
                        Sb = P.sb("mSb", [128, 257], BF16, es=hs1)
                        Am = [P.sb(f"mAm{i}", [64, 64], BF16, es=hs1) for i in range(2)]
                        dn = P.sb("mdn", [64, 2], F32, es=hs1)
                        kvS = [P.sb(f"mkvS{i}", [128, 257], F32, es=hs1) for i in range(2)]
                        psA = P.ps("mpsA", [64, 64], F32, es=hs1)
                        pso = P.ps("mpso", [64, 257], F32, es=hs1)
                        pskv = P.ps("mpskv", [128, 257], F32, es=hs1)
                        pst = [P.ps(f"mpst{i}", [64, 4, 128], BF16, es=hs1) for i in range(2)]
                        k.dma((vaug, vaug[:, :, 0:256], "v"), (PT, PT[:, 1056 + h * 256:1056 + (h + 1) * 256].rearrange("(c p) d -> p c d", p=CH)), eng="pool")
                        k.memset("pool", (vaug, vaug[:, :, 256:257], "o"), 1.0)
                        k.memset("pool", V(oacc), 0.0)
                        for z in range(2):
                            end = CH - 1 if z == 0 else 0
                            for (t0_, tw) in TB:
                                tsl = slice(t0_, t0_ + tw)
                                ncb = tw // CH
                                c0 = t0_ // CH
                                k.dma((t_e, t_e[:, 0:tw]), (GSD, GSD[z * 4 + h, tsl].partition_broadcast(128)))
                                k.dma((t_s, t_s[:, 0:tw]), (GSD, GSD[8 + z * 4 + h, tsl].partition_broadcast(128)))
                                k.dma((t_q, t_q[:, 0:tw]), (PC, PC[1536 + h * 128:1536 + (h + 1) * 128, tsl]))
                                k.dma((t_k, t_k[:, 0:tw]), (PC, PC[2048 + h * 128:2048 + (h + 1) * 128, tsl]))
                                k.act((t_e, t_e[:, 0:tw]), (t_e, t_e[:, 0:tw]), AF.Exp)
                                k.act((t_s, t_s[:, 0:tw]), (t_s, t_s[:, 0:tw]), AF.Exp)
                                e3 = t_e[:, 0:tw].rearrange("p (c t) -> p c t", t=CH)
                                k.cp("pool", (cd, cd[:, c0:c0 + ncb]), (t_e, e3[:, :, end]))
                                k.tt("dve", (q_in, q_in[:, tsl]), (t_q, t_q[:, 0:tw]), (t_e, t_e[:, 0:tw]), ALU.mult)
                                k.stt("dve", (t_k, t_k[:, 0:tw]), (t_k, t_k[:, 0:tw]), 128 ** -0.5, (t_s, t_s[:, 0:tw]), ALU.mult, ALU.mult)
                                k.cp("pool", (k_in, k_in[:, tsl]), (t_k, t_k[:, 0:tw]))
                                k.tt("pool", (k_end, k_end[:, tsl].rearrange("p (c t) -> p c t", t=CH)),
                                     (t_k, t_k[:, 0:tw].rearrange("p (c t) -> p c t", t=CH)),
                                     (t_e, e3[:, :, end:end + 1].to_broadcast([128, ncb, CH])), ALU.mult)
                            for c4 in range(0, NCH, 4):
                                p_ = pst[(c4 // 4) % 2]
                                for j in range(4):
                                    c = c4 + j
                                    k.tr((p_, p_[:, j, :]), (k_end, k_end[:, c * CH:(c + 1) * CH]), V(identb))
                                k.cp("act", (kET, kET[:, c4:c4 + 4, :]), V(p_))
                            k.memset("pool", V(S), 0.0)
                            k.memset("pool", V(Sb), 0.0)
                            order_ = chunk_order(z)

                            def ml_A(step):
                                c = order_[step]
                                csl = slice(c * CH, (c + 1) * CH)
                                am = Am[step % 2]
                                k.mm(V(psA), (k_in, k_in[:, csl]), (q_in, q_in[:, csl]))
                                k.tt("dve", V(am), V(psA), V(masks[z]), ALU.mult)
                                k.mm(V(pskv), (kET, kET[:, c, :]), (vaug, vaug[:, c, :]))
                                k.cp("act", V(kvS[step % 2]), V(pskv))

                            def ml_B(step):
                                c = order_[step]
                                csl = slice(c * CH, (c + 1) * CH)
                                am = Am[step % 2]
                                k.mm(V(pso), V(am), (vaug, vaug[:, c, :]), start=True, stop=False)
                                k.mm(V(pso), (q_in, q_in[:, csl]), V(Sb), start=False, stop=True)
                                k.act((dn, dn[:, 0:1]), (pso, pso[:, 256:257]), AF.Abs)
                                k.ts("dve", (dn, dn[:, 0:1]), (dn, dn[:, 0:1]), 1.0, ALU.max)
                                k.recip((dn, dn[:, 1:2]), (dn, dn[:, 0:1]))
                                k.stt("dve", (oacc, oacc[:, c, :], c), (pso, pso[:, 0:256]), (dn, dn[:, 1:2]), (oacc, oacc[:, c, :], c), ALU.mult, ALU.add)
                                k.stt("dve", V(S), V(S), (cd, cd[:, c:c + 1]), V(kvS[step % 2]), ALU.mult, ALU.add)
                                k.cp("act", V(Sb), V(S))

                            ml_A(0)
                            for step in range(NCH):
                                if step + 1 < NCH:
                                    ml_A(step + 1)
                                ml_B(step)
                    P.barrier()
                    with ExitStack() as hs2:
                        gt = P.sb("mgt", [64, NCH, 256], F32, es=hs2)
                        k.tt("dve", V(gt), V(oacc), V(oacc), ALU.mult)
                        P.op("dve", lambda e: e.tensor_reduce(out=ssn.a(), in_=gt.a(), axis=AX.X, op=ALU.add), reads=[gt], writes=[ssn])
                        k.ts("dve", V(ssn), V(ssn), 1.0 / 256, ALU.mult, EPS, ALU.add)
                        k.act(V(ssn), V(ssn), AF.Sqrt)
                        k.recip(V(ssn), V(ssn))
                        k.tt("dve", V(oacc), V(oacc), (ssn, ssn.a().rearrange("p (c o) -> p c o", o=1).to_broadcast([64, NCH, 256])), ALU.mult)
                        k.tt("pool", V(oacc), V(oacc), (nwB, nwB[:, h * 256:(h + 1) * 256].rearrange("p (o d) -> p o d", o=1).to_broadcast([64, NCH, 256])), ALU.mult)
                        k.dma(V(gt), (PT, PT[:, 2080 + h * 256:2080 + (h + 1) * 256].rearrange("(c p) d -> p c d", p=CH)))
                        for q4 in range(4):
                            k.act((gt, gt[:, q4 * 17:(q4 + 1) * 17, :], q4), (gt, gt[:, q4 * 17:(q4 + 1) * 17, :], q4), AF.Sigmoid)
                        k.tt("dve", V(oacc), V(oacc), V(gt), ALU.mult)
                        k.dma((YM, YM[:, 1024 + h * 256:1024 + (h + 1) * 256].rearrange("(c p) d -> p c d", p=CH), ("m", h)), V(oacc))
                    P.barrier()

        def stage_xbt(PC, XBT):
            with ExitStack() as ph:
                ft = [P.sb(f"xft{i}", [128, 10, 128], F32, es=ph) for i in range(2)]
                ot = [P.sb(f"xot{i}", [128, 10, 128], F32, es=ph) for i in range(2)]
                pt = [P.ps(f"xpt{i}", [128, 4, 128], F32, es=ph) for i in range(3)]
                for tt in range(NT):
                    f_ = ft[tt % 2]; o_ = ot[tt % 2]
                    k.dma(V(f_), (PC, PC[0:1280, tt * 128:(tt + 1) * 128].rearrange("(b p) t -> p b t", p=128)))
                    for gi, (b0, nb_) in enumerate(((0, 4), (4, 4), (8, 2))):
                        p_ = pt[gi]
                        for j in range(nb_):
                            k.tr((p_, p_[:, j, :]), (f_, f_[:, b0 + j, :]), V(ident))
                        k.cp("act" if gi % 2 else "dve", (o_, o_[:, b0:b0 + nb_, :]), (p_, p_[:, 0:nb_, :]))
                    k.dma((XBT, XBT[tt * 128:(tt + 1) * 128, :], tt), (o_, o_.a().rearrange("p b c -> p (b c)")))
                P.barrier()

        def mixer_ssd(PC, PT, XBT, YS):
            with ExitStack() as ph:
                masks = make_masks(ph)
                ones64 = P.sb("sones64", [64, 64], F32, es=ph)
                k.memset("pool", V(ones64), 1.0)
                selend = []
                for z in range(2):
                    se = P.sb(f"selend{z}", [64, 128], F32, es=ph)
                    endp = CH - 1 if z == 0 else 0
                    P.op("pool", lambda e, se=se, endp=endp: e.affine_select(out=se.a(), in_=ones[0:64, :], pattern=[[0, 128]], compare_op=ALU.is_equal,
                                                                           fill=0.0, base=-endp, channel_multiplier=1), reads=[ones], writes=[se])
                    selend.append(se)
                dtT = P.sb("dtT", [64, NCH, 32], F32, es=ph)
                laT = P.sb("laT", [64, NCH, 32], F32, es=ph)
                dbB = P.sb("dbB", [64, 32], F32, es=ph)
                naB = P.sb("naB", [64, 32], F32, es=ph)
                dskB = P.sb("dskB", [64, 16], F32, es=ph)
                k.dma(V(dbB), V(I["ssd_dt_bias"], I["ssd_dt_bias"][0].rearrange("z h -> (z h)").partition_broadcast(64)))
                k.dma(V(naB), V(I["ssd_a_log"], I["ssd_a_log"][0].rearrange("z h -> (z h)").partition_broadcast(64)))
                k.dma(V(dskB), V(I["ssd_d"], I["ssd_d"][0].partition_broadcast(64)))
                k.act(V(naB), V(naB), AF.Exp)
                k.ts("dve", V(naB), V(naB), -1.0, ALU.mult)
                k.dma(V(dtT), (PT, PT[:, 1024:1056].rearrange("(c p) d -> p c d", p=CH)))
                k.tt("dve", V(dtT), V(dtT), (dbB, dbB.a().rearrange("p (o d) -> p o d", o=1).to_broadcast([64, NCH, 32])), ALU.add)
                k.act(V(dtT), V(dtT), AF.Exp)
                k.act(V(dtT), V(dtT), AF.Ln, bias=1.0)
                k.tt("dve", V(laT), V(dtT), (naB, naB.a().rearrange("p (o d) -> p o d", o=1).to_broadcast([64, NCH, 32])), ALU.mult)
                yacc = P.sb("yacc", [64, NCH, 256], F32, es=ph)
                for q4 in range(4):
                    g = q4 // 2
                    with ExitStack() as qs:
                        xq = P.sb("xq", [64, NCH, 256], BF16, es=qs)
                        Bf = P.sb("Bf", [128, L], BF16, es=qs)
                        Cf = P.sb("Cf", [128, L], BF16, es=qs)
                        BTt = P.sb("BTt", [64, NCH, 128], BF16, es=qs)
                        k.dma(V(xq), (XBT, XBT[:, q4 * 256:(q4 + 1) * 256].rearrange("(c p) d -> p c d", p=CH)), eng="pool")
                        k.dma(V(BTt), (XBT, XBT[:, 1024 + g * 128:1024 + (g + 1) * 128].rearrange("(c p) d -> p c d", p=CH)), eng="pool")
                        k.dma(V(Bf), (PC, PC[1024 + g * 128:1024 + (g + 1) * 128, :]), eng="pool")
                        k.dma(V(Cf), (PC, PC[1280 + g * 128:1280 + (g + 1) * 128, :]), eng="pool")
                        k.memset("pool", V(yacc), 0.0)
                        hs = [P.sb(f"hs{z}", [128, 256], F32, es=qs) for z in range(2)]
                        hsb = [P.sb(f"hsb{z}", [128, 256], BF16, es=qs) for z in range(2)]
                        G = 2
                        NJ = 2 * G
                        J = []
                        for js in range(NJ):
                            d_ = {}
                            d_["CBm"] = P.sb(f"CBm{js}", [64, 64], F32, es=qs)
                            d_["acs"] = P.sb(f"acs{js}", [64, 4], F32, es=qs)
                            d_["dg"] = P.sb(f"dg{js}", [64, 4, 64], F32, es=qs)
                            d_["seg"] = P.sb(f"seg{js}", [64, 4, 64], F32, es=qs)
                            d_["AT"] = P.sb(f"AT{js}", [64, 4, 64], BF16, es=qs)
                            d_["xdt"] = P.sb(f"xdt{js}", [64, 4, 64], BF16, es=qs)
                            d_["xde"] = P.sb(f"xde{js}", [64, 4, 64], BF16, es=qs)
                            d_["dend"] = P.sb(f"dend{js}", [64, 4], F32, es=qs)
                            d_["din"] = P.sb(f"din{js}", [64, 4], F32, es=qs)
                            d_["dchB"] = P.sb(f"dchB{js}", [128, 4], F32, es=qs)
                            d_["phsS"] = P.sb(f"phsS{js}", [128, 256], F32, es=qs)
                            J.append(d_)
                        ytmp = [P.sb(f"ytmp{z}", [64, 4, 64], F32, es=qs) for z in range(2)]
                        bankA = [P.ps(f"sbank{i}", [128, 512], F32, es=qs) for i in range(2)]
                        pyi = [P.ps(f"pyi{z}", [64, 4, 64], F32, es=qs) for z in range(2)]
                        for z in range(2):
                            k.memset("pool", V(hs[z]), 0.0)
                            k.memset("pool", V(hsb[z]), 0.0)
                        orders = [chunk_order(0), chunk_order(1)]
                        b4 = lambda t: t.a().rearrange("p (h o) -> p h o", o=1).to_broadcast([64, 4, 64])

                        def ssd_A(step, z, st):
                            js = (step % G) * 2 + z
                            d_ = J[js]
                            bk_ = bankA[js % 2]
                            c = orders[z][step]
                            csl = slice(c * CH, (c + 1) * CH)
                            end = CH - 1 if z == 0 else 0
                            hsl = slice(z * 16 + q4 * 4, z * 16 + q4 * 4 + 4)
                            acs = d_["acs"]
                            if st == 0:
                                k.mm((bk_, bk_[0:64, 0:64]), (Bf, Bf[:, csl]), (Cf, Cf[:, csl]))
                                k.mm((bk_, bk_[0:64, 64:68]), V(masks[z]), (laT, laT[:, c, hsl]))
                                k.tt("dve", V(d_["CBm"]), (bk_, bk_[0:64, 0:64]), V(masks[z]), ALU.mult)
                                k.cp("act", V(acs), (bk_, bk_[0:64, 64:68]))
                                k.tt("dve", V(d_["xdt"]), (xq, xq[:, c, :].rearrange("p (h d) -> p h d", d=64)),
                                     (dtT, dtT[:, c, hsl].rearrange("p (h o) -> p h o", o=1).to_broadcast([64, 4, 64])), ALU.mult)
                            elif st == 1:
                                k.tt("dve", V(d_["dg"]), (ident, ident[0:64, 0:64].rearrange("p (o t) -> p o t", o=1).to_broadcast([64, 4, 64])),
                                     (acs, b4(acs)), ALU.mult)
                                k.act(V(d_["din"]), V(acs), AF.Exp)
                            elif st == 2:
                                pbc = bk_[0:64, 128:384].rearrange("p (h t) -> p h t", t=64)
                                k.mm((bk_, bk_[0:64, 128:384]), V(ones64), (d_["dg"], d_["dg"].a().rearrange("p h t -> p (h t)")))
                                k.tt("dve", V(d_["seg"]), (bk_, pbc), (acs, b4(acs)), ALU.subtract)
                                k.tt("dve", V(d_["dend"]), (bk_, pbc[:, :, end]), V(acs), ALU.subtract)
                                k.mm((bk_, bk_[:, 384:388]), V(selend[z]), V(acs))
                                k.act(V(d_["dchB"]), (bk_, bk_[:, 384:388]), AF.Exp)
                            elif st == 3:
                                k.act(V(d_["seg"]), V(d_["seg"]), AF.Exp)
                                k.act(V(d_["dend"]), V(d_["dend"]), AF.Exp)
                                k.stt("dve", V(d_["AT"]), V(d_["seg"]), 1.0,
                                      (d_["CBm"], d_["CBm"].a().rearrange("p (o t) -> p o t", o=1).to_broadcast([64, 4, 64])), ALU.min, ALU.mult)
                                k.tt("dve", V(d_["xde"]), V(d_["xdt"]), (d_["dend"], b4(d_["dend"])), ALU.mult)
                            elif st == 4:
                                py = bk_[0:64, 128:384].rearrange("p (h t) -> p h t", t=64)
                                for i in range(4):
                                    k.mm((bk_, py[:, i, :]), (d_["AT"], d_["AT"][:, i, :]), (d_["xdt"], d_["xdt"][:, i, :]))
                                ya = (yacc, yacc[:, c, :].rearrange("p (h d) -> p h d", d=64), c)
                                k.tt("dve", ya, (bk_, py), ya, ALU.add)
                            elif st == 5:
                                k.mm((bk_, bk_[:, 0:256]), (BTt, BTt[:, c, :]), (d_["xde"], d_["xde"].a().rearrange("p h d -> p (h d)")))
                                k.cp("act", V(d_["phsS"]), (bk_, bk_[:, 0:256]))

                        def ssd_B(step, z, st):
                            js = (step % G) * 2 + z
                            d_ = J[js]
                            c = orders[z][step]
                            csl = slice(c * CH, (c + 1) * CH)
                            ya = (yacc, yacc[:, c, :].rearrange("p (h d) -> p h d", d=64), c)
                            if st == 0:
                                k.mm(V(pyi[z]), (Cf, Cf[:, csl]), V(hsb[z]))
                                h3 = (hs[z], hs[z].a().rearrange("p (h d) -> p h d", d=64))
                                k.tt("dve", h3, h3, (d_["dchB"], d_["dchB"].a().rearrange("p (h o) -> p h o", o=1).to_broadcast([128, 4, 64])), ALU.mult)
                                k.tt("dve", V(hs[z]), V(d_["phsS"]), V(hs[z]), ALU.add)
                                k.cp("act", V(hsb[z]), V(hs[z]))
                            else:
                                k.tt("dve", V(ytmp[z]), V(pyi[z]), (d_["din"], b4(d_["din"])), ALU.mult)
                                k.tt("dve", ya, ya, V(ytmp[z]), ALU.add)

                        for g0 in range(0, NCH, G):
                            steps = list(range(g0, min(NCH, g0 + G)))
                            for st in range(6):
                                for step in steps:
                                    for z in range(2):
                                        ssd_A(step, z, st)
                            for step in steps:
                                for st in range(2):
                                    for z in range(2):
                                        ssd_B(step, z, st)
                    P.barrier()
                    with ExitStack() as fs:
                        xf = P.sb("sxf", [64, 17, 256], F32, es=fs)
                        zf = P.sb("szf", [64, 17, 256], F32, es=fs)
                        for c17 in range(4):
                            cs_ = slice(c17 * 17, (c17 + 1) * 17)
                            rows = slice(c17 * 17 * CH, (c17 + 1) * 17 * CH)
                            k.dma(V(xf), (XBT, XBT[rows, q4 * 256:(q4 + 1) * 256].rearrange("(c p) d -> p c d", p=CH)))
                            k.dma(V(zf), (PT, PT[rows, q4 * 256:(q4 + 1) * 256].rearrange("(c p) d -> p c d", p=CH)))
                            dsk4 = dskB[:, q4 * 4:(q4 + 1) * 4].rearrange("p (a h o) -> p a h o", a=1, o=1).to_broadcast([64, 17, 4, 64])
                            k.tt("pool", (xf, xf.a().rearrange("p c (h d) -> p c h d", d=64)), (xf, xf.a().rearrange("p c (h d) -> p c h d", d=64)),
                                 (dskB, dsk4), ALU.mult)
                            k.tt("dve", V(xf), V(xf), (yacc, yacc[:, cs_, :], ("f", c17)), ALU.add)
                            k.act(V(zf), V(zf), AF.Silu)
                            k.tt("dve", V(xf), V(xf), V(zf), ALU.mult)
                            k.dma((YS, YS[rows, q4 * 256:(q4 + 1) * 256].rearrange("(c p) d -> p c d", p=CH), (q4, c17)), V(xf))
                    P.barrier()

        def stage_ssd_norm(YS, YM):
            with ExitStack() as ph:
                nwB = P.sb("snwB", [128, 1024], F32, es=ph)
                k.dma(V(nwB), V(I["ssd_norm_w"], I["ssd_norm_w"][0].partition_broadcast(128)))
                yt = [P.sb(f"syt{i}", [128, 1024], F32, es=ph) for i in range(2)]
                sq = P.sb("ssq", [128, 1024], F32, es=ph)
                st = [P.sb(f"sst{i}", [128, 2], F32, es=ph) for i in range(2)]
                for tt in range(NT):
                    y_ = yt[tt % 2]; s_ = st[tt % 2]
                    k.dma(V(y_), (YS, YS[tt * 128:(tt + 1) * 128, :]))
                    k.tt("pool", V(sq), V(y_), V(y_), ALU.mult)
                    P.op("dve", lambda e, s_=s_: e.tensor_reduce(out=s_.a(), in_=sq.a().rearrange("p (g d) -> p g d", g=2), axis=AX.X, op=ALU.add),
                         reads=[sq], writes=[s_])
                    k.ts("dve", V(s_), V(s_), 1.0 / 512, ALU.mult, EPS, ALU.add)
                    k.act(V(s_), V(s_), AF.Sqrt)
                    k.recip(V(s_), V(s_))
                    k.tt("dve", (y_, y_.a().rearrange("p (g d) -> p g d", g=2)), (y_, y_.a().rearrange("p (g d) -> p g d", g=2)),
                         (s_, s_.a().rearrange("p (g o) -> p g o", o=1).to_broadcast([128, 2, 512])), ALU.mult)
                    k.tt("pool", V(y_), V(y_), V(nwB), ALU.mult)
                    k.dma((YM, YM[tt * 128:(tt + 1) * 128, 0:1024], ("s", tt)), V(y_))
                P.barrier()

        def stage_final():
            with ExitStack() as ph:
                fnB = P.sb("fnB", [128, D], F32, es=ph)
                k.dma(V(fnB), V(I["final_norm_w"], I["final_norm_w"].a().partition_broadcast(128)))
                xt = [P.sb(f"fxt{i}", [128, D], F32, es=ph) for i in range(2)]
                junk = P.sb("fjunk", [128, D], F32, es=ph)
                st = [P.sb(f"fst{i}", [128, 4], F32, es=ph) for i in range(2)]
                for i in range(SEQ // 128):
                    x_ = xt[i % 2]; s_ = st[i % 2]
                    k.dma(V(x_), (XL, XL[LC + i * 128:LC + (i + 1) * 128, :], i))
                    k.memset("pool", (s_, s_[:, 0:1]), 0.0)
                    k.act(V(junk), V(x_), AF.Square, accum=(s_, s_[:, 0:1]))
                    k.ts("dve", (s_, s_[:, 1:2]), (s_, s_[:, 0:1]), 1.0 / D, ALU.mult, EPS, ALU.add)
                    k.act((s_, s_[:, 2:3]), (s_, s_[:, 1:2]), AF.Sqrt)
                    k.recip((s_, s_[:, 3:4]), (s_, s_[:, 2:3]))
                    k.ts("dve", V(x_), V(x_), (s_, s_[:, 3:4]), ALU.mult)
                    k.tt("pool", V(x_), V(x_), V(fnB), ALU.mult)
                    k.dma((OUT, OUT[i * 128:(i + 1) * 128, :], i), V(x_))
                P.barrier()

        def src0(i):
            if i < 2:
                return (I["ctx"], I["ctx"][i * 128:(i + 1) * 128, :])
            return (I["x"], I["x"][(i - 2) * 128:(i - 1) * 128, :])

        def src1(i):
            return (XL, XL[i * 128:(i + 1) * 128, :])

        def layer0():
            stage_mods(0)
            if "modF" in debug:
                dm = P.dram("d_modF", [128, 6 * KO * 2], F32, kind="ExternalOutput")
                k.dma(V(dm), (modF, modF.a().rearrange("p a b c -> p (a b c)")))
                dm2 = P.dram("d_modB", [128, 4 * D], F32, kind="ExternalOutput")
                k.dma(V(dm2), (modB, modB.a().rearrange("p a b c -> p (a b c)")))
            PF0 = scratch("PF0", [3456, L])
            PT0 = scratch("PT0", [L, 1024])
            YM0 = scratch("YM0", [L, 1024])
            with ExitStack() as lay:
                HT = P.sb("HT", [128, KO, L], BF16, es=lay)
                stage_norm(0, 1, HT, src0, perm=False)
                if "HT0" in debug:
                    dh = P.dram("d_HT0", [128, KO * L], BF16, kind="ExternalOutput")
                    k.dma(V(dh), (HT, HT.a().rearrange("p a b -> p (a b)")))
                if stop_after == "norm0":
                    return
                stage_proj(HT, (I["even_w_in"], I["even_w_in"][0]), 4480, [(0, 3456, PF0, 0)], [(3456, 4480, PT0, 0)])
            if stop_after == "proj0":
                return
            if "skip_hgrn" not in debug:
                mixer_hgrn(PF0, PT0, YM0)
            PR0 = scratch("PR0", [1920, L])
            GT0 = scratch("GT0", [L, 512])
            rwkv_shift(PF0, PR0)
            if "no_gate" not in debug:
                rwkv_gate(PR0, GT0)
            if stop_after == "shift0":
                return
            if "skip_rwkv" not in debug:
                if "rwkv_old" in debug:
                    mixer_rwkv(PR0, GT0, YM0)
                else:
                    mixer_rwkv2(PR0, GT0, YM0)
            if stop_after == "mix0":
                return
            stage_outproj(YM0, 1024, (I["even_w_out"], I["even_w_out"][0]), src0, False, list(range(NT)))
            if stop_after == "out0":
                return
            stage_moe(0, list(range(NT)))

        def layer1():
            stage_mods(1)
            PF1 = scratch("PF1", [2576, L])
            PT1 = scratch("PT1", [L, 3104])
            PC1 = scratch("PC1", [2560, L])
            YM1 = scratch("YM1", [L, 2048])
            GSD = scratch("GSD", [16, L])
            YS = scratch("YS", [L, 1024])
            with ExitStack() as lay:
                HT = P.sb("HT", [128, KO, L], BF16, es=lay)
                stage_norm(1, 1, HT, src1, perm=True)
                stage_proj(HT, (I["odd_w_in"], I["odd_w_in"][0]), 5680,
                           [(1024, 2560, PF1, 0), (2592, 3616, PF1, 1536), (5664, 5680, PF1, 2560)],
                           [(0, 1024, PT1, 0), (2560, 2592, PT1, 1024), (3616, 5664, PT1, 1056)])
            stage_conv(PF1, PC1, [(0, 12, (I["ssd_conv_w"], I["ssd_conv_w"][0]), (I["ssd_conv_b"], I["ssd_conv_b"][0])),
                                  (1536, 8, (I["mlstm_conv_w"], I["mlstm_conv_w"][0]), (I["mlstm_conv_b"], I["mlstm_conv_b"][0]))])
            if stop_after == "L1conv":
                return
            if "skip_mlstm" not in debug:
                mixer_mlstm(PC1, PF1, PT1, YM1, GSD)
            if stop_after == "L1mlstm":
                return
            XBT = scratch("XBT", [L, 1280])
            stage_xbt(PC1, XBT)
            mixer_ssd(PC1, PT1, XBT, YS)
            stage_ssd_norm(YS, YM1)
            if stop_after == "L1ssd":
                return
            stage_outproj(YM1, 2048, (I["odd_w_out"], I["odd_w_out"][0]), src1, True, list(range(2, NT)))
            if stop_after == "L1out":
                return
            stage_moe(1, list(range(2, NT)))
            stage_final()

        if "L1only" in debug:
            XLin = P.dram("XLin", [L, D], F32, kind="ExternalInput")
            for i4 in range(4):
                k.dma((XL, XL[i4 * 1088:(i4 + 1) * 1088, :], ("in", i4)), (XLin, XLin[i4 * 1088:(i4 + 1) * 1088, :]))
            P.barrier()
        else:
            layer0()
        if stop_after is None or stop_after.startswith("L1"):
            layer1()
        P.finish()
    return nc


_NC = None


def kernel(**inputs):
    global _NC
    if _NC is None:
        _NC = build()
    nc = _NC
    n = 8
    in_maps = []
    for b in range(n):
        m = {}
        for kk_, v in inputs.items():
            v = np.asarray(v)
            if kk_ == "x":
                m[kk_] = np.ascontiguousarray(v[b])
            elif kk_ == "c":
                m[kk_] = np.ascontiguousarray(v[b])
            elif kk_ == "ctx":
                m[kk_] = np.ascontiguousarray(v[b])
            else:
                m[kk_] = v
        in_maps.append(m)
    res = run_bass_kernel_spmd(nc, in_maps, core_ids=list(range(n)))
    return np.stack([r["out"] for r in res.results], axis=0)
```

```python
import numpy as np
import concourse.bass as bass
import concourse.mybir as mybir
from concourse.bass_utils import run_bass_kernel_spmd
from contextlib import ExitStack

F32 = mybir.dt.float32
BF16 = mybir.dt.bfloat16
AF = mybir.ActivationFunctionType
ALU = mybir.AluOpType
AX = mybir.AxisListType

ENGS = ("pe", "dve", "act", "pool", "sp")
D = 1024
KO = 8
LC = 256
SEQ = 4096
L = LC + SEQ
NT = L // 128
EPS = 1e-6
CH = 64
NCH = L // CH


class Cell:
    __slots__ = ("w", "r")

    def __init__(self):
        self.w = None
        self.r = []


class Buf:
    def __init__(self, name, h, is_dram=False, is_psum=False):
        self.name = name
        self.h = h
        self.is_dram = is_dram
        self.is_psum = is_psum
        self.base = Cell()
        self.parts = {}

    def cells(self, key):
        if key is None:
            return [self.base] + list(self.parts.values())
        c = self.parts.get(key)
        if c is None:
            c = Cell()
            c.w = self.base.w
            c.r = list(self.base.r)
            self.parts[key] = c
        return [c]

    def __getitem__(self, idx):
        return self.h[idx]

    def a(self):
        return self.h[:]


class Prog:
    def __init__(self, nc, es):
        self.nc = nc
        self.es = es
        self.q = {e: [] for e in ENGS}
        self.cnt = {e: 0 for e in ENGS}
        self.sem = {e: es.enter_context(nc.semaphore("s_" + e)) for e in ENGS}
        self.known = {e: {} for e in ENGS}
        self.dsem = {}
        self.dsem_by_id = {}
        self.phase_slots = {}
        self.ninst = 0
        self.uid = 0

    def sb(self, name, shape, dt=F32, es=None):
        self.uid += 1
        h = (es or self.es).enter_context(self.nc.sbuf_tensor(f"{name}_{self.uid}", list(shape), dt))
        return Buf(name, h)

    def ps(self, name, shape, dt=F32, es=None):
        self.uid += 1
        h = (es or self.es).enter_context(self.nc.psum_tensor(f"{name}_{self.uid}", list(shape), dt))
        return Buf(name, h, is_psum=True)

    def dram(self, name, shape, dt=F32, kind="Internal"):
        h = self.nc.dram_tensor(name, list(shape), dt, kind=kind)
        return Buf(name, h.ap(), is_dram=True)

    def dma_sem(self, name):
        if name not in self.dsem:
            self.dsem[name] = [self.es.enter_context(self.nc.semaphore("d_" + name)), 0]
            self.dsem_by_id[id(self.dsem[name][0])] = self.dsem[name]
        return self.dsem[name]

    def _norm(self, lst):
        return [(r, None) if isinstance(r, Buf) else ((r[0], None) if r[0].is_psum else r) for r in lst]

    def _deps(self, eng, reads, writes, pe_acc=False):
        need = {}

        def add(tok):
            if tok is None:
                return
            k = id(tok[0])
            if k not in need or need[k][1] < tok[1]:
                need[k] = tok

        for (b, key) in reads:
            for c in b.cells(key):
                add(c.w)
        for (b, key) in writes:
            for c in b.cells(key):
                if not (pe_acc and c.w is not None and c.w[2] == "pe"):
                    add(c.w)
                for t in c.r:
                    add(t)
        out = []
        kn = self.known[eng]
        for k, tok in need.items():
            val = tok[1]
            if k in self.dsem_by_id:
                val = self.dsem_by_id[k][1]
            if kn.get(k, 0) >= val:
                continue
            kn[k] = val
            out.append((tok[0], val))
        return out

    def _record(self, tok, reads, writes):
        for (b, key) in reads:
            for c in b.cells(key):
                c.r.append(tok)
                if len(c.r) > 16:
                    best = {}
                    for t in c.r:
                        kk = id(t[0])
                        if kk not in best or best[kk][1] < t[1]:
                            best[kk] = t
                    c.r = list(best.values())
        for (b, key) in writes:
            for c in b.cells(key):
                c.w = tok
                c.r = []

    def op(self, eng, fn, reads=(), writes=(), pe_acc=False):
        reads = self._norm(reads)
        writes = self._norm(writes)
        writes = writes + [r for r in reads if r[0].is_psum]
        waits = self._deps(eng, reads, writes, pe_acc)
        self.cnt[eng] += 1
        sem = self.sem[eng]
        tok = (sem, self.cnt[eng], eng)
        self.ninst += 1

        def run(e, waits=waits, fn=fn, sem=sem):
            for s, v in waits:
                e.wait_ge(s, v)
            fn(e).then_inc(sem, 1)

        self.q[eng].append(run)
        self._record(tok, reads, writes)
        return tok

    def dma(self, eng, out_ap, in_ap, reads=(), writes=(), semname=None, **kw):
        reads = self._norm(reads)
        writes = self._norm(writes)
        waits = self._deps(eng, reads, writes)
        if semname is None:
            sbs = [b for (b, _) in list(writes) + list(reads) if not b.is_dram]
            semname = sbs[0].name if sbs else "dram2dram"
        if semname not in self.phase_slots:
            self.phase_slots[semname] = len(self.phase_slots)
        ds = self.dma_sem(f"slot{self.phase_slots[semname]}")
        ds[1] += 16
        tok = (ds[0], ds[1], "dma")
        self.ninst += 1

        def run(e, waits=waits, sem=ds[0]):
            for s, v in waits:
                e.wait_ge(s, v)
            e.dma_start(out=out_ap, in_=in_ap, **kw).then_inc(sem, 16)

        self.q[eng].append(run)
        self._record(tok, reads, writes)
        return tok

    def barrier(self):
        self.phase_slots = {}
        toks = [(self.sem[f], self.cnt[f]) for f in ENGS if self.cnt[f] > 0]
        toks += [(v[0], v[1]) for v in self.dsem.values() if v[1] > 0]
        for e in ENGS:
            kn = self.known[e]
            ws = []
            for s, v in toks:
                if kn.get(id(s), 0) < v:
                    kn[id(s)] = v
                    ws.append((s, v))

            def run(en, ws=ws):
                for s, v in ws:
                    en.wait_ge(s, v)
            self.q[e].append(run)

    def finish(self):
        nc = self.nc
        self.barrier()
        with nc.Block() as block:
            @block.tensor
            def _(e):
                for f in self.q["pe"]:
                    f(e)

            @block.vector
            def _(e):
                for f in self.q["dve"]:
                    f(e)

            @block.scalar
            def _(e):
                for f in self.q["act"]:
                    f(e)

            @block.gpsimd
            def _(e):
                for f in self.q["pool"]:
                    f(e)

            @block.sync
            def _(e):
                for f in self.q["sp"]:
                    f(e)


def _rk(x):
    return (x[0], x[2] if len(x) > 2 else None)


class K:
    def __init__(self, P):
        self.P = P
        self.dq = 0

    def mm(self, out, lhsT, rhs, start=True, stop=True):
        return self.P.op("pe", lambda e: e.matmul(out[1], lhsT=lhsT[1], rhs=rhs[1], start=start, stop=stop),
                         reads=[_rk(lhsT), _rk(rhs)], writes=[_rk(out)], pe_acc=not start)

    def tr(self, out, in_, ident):
        return self.P.op("pe", lambda e: e.transpose(out[1], in_[1], ident[1]),
                         reads=[_rk(in_), _rk(ident)], writes=[_rk(out)])

    def act(self, out, in_, func, bias=None, scale=None, accum=None, eng="act"):
        reads = [_rk(in_)]
        kw = {}
        if bias is not None:
            if isinstance(bias, tuple):
                reads.append(_rk(bias)); kw["bias"] = bias[1]
            else:
                kw["bias"] = bias
        if scale is not None:
            if isinstance(scale, tuple):
                reads.append(_rk(scale)); kw["scale"] = scale[1]
            else:
                kw["scale"] = scale
        writes = [_rk(out)]
        if accum is not None:
            writes.append(_rk(accum)); kw["accum_out"] = accum[1]
        return self.P.op("act", lambda e: e.activation(out=out[1], in_=in_[1], func=func, **kw), reads=reads, writes=writes)

    def tt(self, eng, out, in0, in1, op):
        if eng == "pool":
            eng = "dve"
        return self.P.op(eng, lambda e: e.tensor_tensor(out=out[1], in0=in0[1], in1=in1[1], op=op),
                         reads=[_rk(in0), _rk(in1)], writes=[_rk(out)])

    def ts(self, eng, out, in0, s1, op0, s2=None, op1=None, accum=None):
        reads = [_rk(in0)]
        a1 = s1
        if isinstance(s1, tuple):
            reads.append(_rk(s1)); a1 = s1[1]
        a2 = s2
        if isinstance(s2, tuple):
            reads.append(_rk(s2)); a2 = s2[1]
        kw = {}
        if op1 is not None:
            kw["op1"] = op1
        writes = [_rk(out)]
        if accum is not None:
            writes.append(_rk(accum)); kw["accum_out"] = accum[1]
        if eng == "pool":
            eng = "dve"
        return self.P.op(eng, lambda e: e.tensor_scalar(out=out[1], in0=in0[1], scalar1=a1, scalar2=a2, op0=op0, **kw),
                         reads=reads, writes=writes)

    def stt(self, eng, out, in0, scalar, in1, op0, op1):
        reads = [_rk(in0), _rk(in1)]
        sc = scalar
        if isinstance(scalar, tuple):
            reads.append(_rk(scalar)); sc = scalar[1]
        return self.P.op(eng, lambda e: e.scalar_tensor_tensor(out=out[1], in0=in0[1], scalar=sc, in1=in1[1], op0=op0, op1=op1),
                         reads=reads, writes=[_rk(out)])

    def cp(self, eng, out, in_):
        if eng == "pool":
            eng = "act"
        if eng == "act":
            return self.P.op("act", lambda e: e.copy(out=out[1], in_=in_[1]), reads=[_rk(in_)], writes=[_rk(out)])
        return self.P.op(eng, lambda e: e.tensor_copy(out=out[1], in_=in_[1]), reads=[_rk(in_)], writes=[_rk(out)])

    def memset(self, eng, out, val):
        return self.P.op(eng, lambda e: e.memset(out[1], val), writes=[_rk(out)])

    def scan(self, out, d0, d1, init, op0, op1):
        return self.P.op("dve", lambda e: e.tensor_tensor_scan(out=out[1], data0=d0[1], data1=d1[1], initial=init, op0=op0, op1=op1),
                         reads=[_rk(d0), _rk(d1)], writes=[_rk(out)])

    def recip(self, out, in_):
        return self.P.op("dve", lambda e: e.reciprocal(out=out[1], in_=in_[1]), reads=[_rk(in_)], writes=[_rk(out)])

    def dma(self, out, in_, eng=None, **kw):
        if eng is None:
            eng = "sp" if in_[0].is_dram else "act"
        reads = [_rk(in_)]
        writes = [_rk(out)]
        return self.P.dma(eng, out[1], in_[1], reads=reads, writes=writes, **kw)


def V(buf, ap=None, key=None):
    return (buf, buf.a() if ap is None else ap, key)


def build(debug=(), stop_after=None):
    nc = bass.Bass("TRN2", target_bir_lowering=False)
    es = ExitStack()
    with es:
        P = Prog(nc, es)
        k = K(P)
        I = {}

        def inp(name, shape):
            I[name] = P.dram(name, shape, F32, kind="ExternalInput")

        inp("x", [SEQ, D]); inp("c", [D]); inp("ctx", [LC, D]); inp("c_ctx", [D])
        inp("ada_w", [2, D, 6 * D]); inp("ada_b", [2, 6 * D]); inp("norm1_w", [2, D]); inp("norm2_w", [2, D])
        inp("even_w_in", [1, D, 4480]); inp("even_w_out", [1, 1024, D])
        inp("rwkv_mu", [1, 1920]); inp("rwkv_w0", [1, 2, 512]); inp("rwkv_w2", [1, 2, 64, 512])
        inp("rwkv_a0", [1, 2, 512]); inp("rwkv_a2", [1, 2, 64, 512]); inp("rwkv_g2", [1, 128, 512])
        for n in ("rwkv_k_k", "rwkv_k_a", "rwkv_r_k", "rwkv_ln_w", "rwkv_ln_b"):
            inp(n, [1, 512])
        inp("hgrn_lower_bounds", [3, 512]); inp("hgrn_norm_w", [1, 512])
        inp("odd_w_in", [1, D, 5680]); inp("odd_w_out", [1, 2048, D])
        inp("ssd_conv_w", [1, 5, 1536]); inp("ssd_conv_b", [1, 1536]); inp("ssd_dt_bias", [1, 2, 16])
        inp("ssd_a_log", [1, 2, 16]); inp("ssd_d", [1, 16]); inp("ssd_norm_w", [1, 1024])
        inp("mlstm_conv_w", [1, 5, 1024]); inp("mlstm_conv_b", [1, 1024]); inp("mlstm_i_bias", [1, 2, 4])
        inp("mlstm_f_bias", [1, 2, 4]); inp("mlstm_norm_w", [1, 1024])
        inp("router_w", [2, D, 32]); inp("router_b", [2, 32])
        inp("exp_w_gate", [2, 32, D, 1024]); inp("exp_b_gate", [2, 32, 1024])
        inp("exp_w_up", [2, 32, D, 1024]); inp("exp_b_up", [2, 32, 1024])
        inp("exp_w_down", [2, 32, 1024, D]); inp("exp_b_down", [2, 32, D])
        inp("final_norm_w", [D])
        OUT = P.dram("out", [SEQ, D], F32, kind="ExternalOutput")

        def scratch(name, shape, dt=F32):
            return P.dram(name, shape, dt, kind="ExternalOutput" if name in debug else "Internal")

        XL = scratch("XL", [L, D])

        ident = P.sb("ident", [128, 128], F32)
        identb = P.sb("identb", [128, 128], BF16)
        ones = P.sb("ones", [128, 128], F32)
        k.memset("pool", V(ones), 1.0)
        P.op("pool", lambda e: e.affine_select(out=ident.a(), in_=ones.a(), pattern=[[-1, 128]], compare_op=ALU.is_equal,
                                               fill=0.0, base=0, channel_multiplier=1), reads=[ones], writes=[ident])
        k.cp("dve", V(identb), V(ident))

        modF = P.sb("modF", [128, 6, KO, 2], F32)
        modB = P.sb("modB", [128, 2, 2, D], F32)
        g1F = P.sb("g1F", [128, KO, 2], F32)
        g2F = P.sb("g2F", [128, KO, 2], F32)
        nw1 = P.sb("nw1", [128, 2, KO], F32)
        nw2 = P.sb("nw2", [128, 2, KO], F32)
        k.dma(V(nw1), V(I["norm1_w"], I["norm1_w"].a().rearrange("l (ko p) -> p l ko", p=128)), allow_slow_non_contiguous=True)
        k.dma(V(nw2), V(I["norm2_w"], I["norm2_w"].a().rearrange("l (ko p) -> p l ko", p=128)), allow_slow_non_contiguous=True)

        def stage_mods(l):
            with ExitStack() as ph:
                c0 = P.sb("c0", [128, KO, 2], F32, es=ph)
                s = P.sb("s", [128, KO, 2], F32, es=ph)
                sB = P.sb("sB", [128, KO, 2, 128], F32, es=ph)
                abF = P.sb("abF", [128, 48], F32, es=ph)
                abB = P.sb("abB", [128, 2, D], F32, es=ph)
                awm = [P.sb(f"awm{i}", [128, KO, D], F32, es=ph) for i in range(2)]
                psF = P.ps("psF", [128, KO, 2], F32, es=ph)
                psB = [P.ps(f"psB{i}", [128, 512], F32, es=ph) for i in range(2)]
                tmp = P.sb("tmpm", [128, KO, 2], F32, es=ph)
                k.dma((c0, c0[:, :, 0]), V(I["c"], I["c"].a().rearrange("(ko p) -> p ko", p=128)), allow_slow_non_contiguous=True)
                k.dma((c0, c0[:, :, 1]), V(I["c_ctx"], I["c_ctx"].a().rearrange("(ko p) -> p ko", p=128)), allow_slow_non_contiguous=True)
                k.act(V(s), V(c0), AF.Silu)
                k.cp("dve", V(sB), (s, s.a().rearrange("p k (j o) -> p k j o", o=1).to_broadcast([128, KO, 2, 128])))
                k.dma(V(abF), V(I["ada_b"], I["ada_b"][l].rearrange("(nb p) -> p nb", p=128)), allow_slow_non_contiguous=True)
                k.dma((abB, abB[:, 0, :]), V(I["ada_b"], I["ada_b"][l, 2 * D:3 * D].partition_broadcast(128)))
                k.dma((abB, abB[:, 1, :]), V(I["ada_b"], I["ada_b"][l, 5 * D:6 * D].partition_broadcast(128)))
                for m in range(6):
                    aw = awm[m % 2]
                    k.dma(V(aw), V(I["ada_w"], I["ada_w"][l, :, m * D:(m + 1) * D].rearrange("(ko p) n -> p ko n", p=128)))
                    if m in (0, 1, 3, 4):
                        for nb in range(KO):
                            for ko in range(KO):
                                k.mm((psF, psF[:, nb, :]), (aw, aw[:, ko, nb * 128:(nb + 1) * 128]), (s, s[:, ko, :]),
                                     start=(ko == 0), stop=(ko == KO - 1))
                        k.tt("dve", (modF, modF[:, m, :, :]), V(psF),
                             (abF, abF[:, m * 8:(m + 1) * 8].rearrange("p (k o) -> p k o", o=1).to_broadcast([128, KO, 2])), ALU.add)
                    else:
                        mi = 0 if m == 2 else 1
                        for j in range(2):
                            for nblk in range(2):
                                pb = psB[(j * 2 + nblk) % 2]
                                for ko in range(KO):
                                    k.mm(V(pb), (sB, sB[:, ko, j, :]), (aw, aw[:, ko, nblk * 512:(nblk + 1) * 512]),
                                         start=(ko == 0), stop=(ko == KO - 1))
                                k.tt("dve", (modB, modB[:, mi, j, nblk * 512:(nblk + 1) * 512]), V(pb),
                                     (abB, abB[:, mi, nblk * 512:(nblk + 1) * 512]), ALU.add)
                for (gF, nw, mi) in ((g1F, nw1, 1), (g2F, nw2, 4)):
                    k.ts("dve", V(tmp), (modF, modF[:, mi, :, :]), 1.0, ALU.add)
                    k.tt("dve", V(gF), V(tmp), (nw, nw[:, l, :].rearrange("p (k o) -> p k o", o=1).to_broadcast([128, KO, 2])), ALU.mult)
                P.barrier()

        def stage_norm(l, which, HT, src_tiles, perm):
            gF = g1F if which == 1 else g2F
            mi = 0 if which == 1 else 3
            with ExitStack() as ph:
                xt = [P.sb(f"xt{i}", [128, D], F32, es=ph) for i in range(3)]
                xn = [P.sb(f"xn{i}", [128, D], F32, es=ph) for i in range(2)]
                junk = P.sb("junk", [128, D], F32, es=ph)
                st = [P.sb(f"st{i}", [128, 4], F32, es=ph) for i in range(2)]
                tmp = [P.sb(f"tmpn{i}", [128, KO, 128], F32, es=ph) for i in range(2)]
                pT = [P.ps(f"pT{i}", [128, KO, 128], F32, es=ph) for i in range(2)]
                for i in range(NT):
                    j = 1 if i < 2 else 0
                    x_ = xt[i % 3]; n_ = xn[i % 2]; s_ = st[i % 2]; t_ = tmp[i % 2]; p_ = pT[i % 2]
                    sb_, sap = src_tiles(i)
                    k.dma(V(x_), (sb_, sap))
                    k.memset("pool", (s_, s_[:, 0:1]), 0.0)
                    k.act(V(junk), V(x_), AF.Square, accum=(s_, s_[:, 0:1]))
                    k.ts("dve", (s_, s_[:, 1:2]), (s_, s_[:, 0:1]), 1.0 / D, ALU.mult, EPS, ALU.add)
                    k.act((s_, s_[:, 2:3]), (s_, s_[:, 1:2]), AF.Sqrt)
                    k.recip((s_, s_[:, 3:4]), (s_, s_[:, 2:3]))
                    k.ts("dve", V(n_), V(x_), (s_, s_[:, 3:4]), ALU.mult)
                    for ko in range(KO):
                        k.tr((p_, p_[:, ko, :]), (n_, n_[:, ko * 128:(ko + 1) * 128]), V(ident))
                    k.tt("dve", V(t_), V(p_), (gF, gF[:, :, j:j + 1].to_broadcast([128, KO, 128])), ALU.mult)
                    if perm and i >= 2:
                        r0 = 2 * (i - 2)
                        dst = HT[:, :, LC:].rearrange("p k (c r) -> p k c r", r=64)[:, :, :, r0:r0 + 2]
                        src0 = t_.a().rearrange("p k (r c) -> p k c r", r=2)
                        src1 = modF[:, mi, :, j:j + 1].rearrange("p k (a b) -> p k a b", b=1).to_broadcast([128, KO, 64, 2])
                        k.tt("pool", (HT, dst, i), (t_, src0), (modF, src1), ALU.add)
                    else:
                        k.tt("pool", (HT, HT[:, :, i * 128:(i + 1) * 128], i), V(t_),
                             (modF, modF[:, mi, :, j:j + 1].to_broadcast([128, KO, 128])), ALU.add)
                P.barrier()

        def stage_proj(HT, W, ncols, f_list, t_list):
            with ExitStack() as ph:
                wst = [P.sb(f"wst{i}", [128, KO, 512], BF16, es=ph) for i in range(2)]
                stg = [P.sb(f"stg{i}", [128, 512], F32, es=ph) for i in range(4)]
                pp = [P.ps(f"pp{i}", [128, 512], F32, es=ph) for i in range(4)]
                cnt = 0
                npan = (ncols + 511) // 512
                for pn in range(npan):
                    c0 = pn * 512
                    cw = min(512, ncols - c0)
                    w_ = wst[pn % 2]
                    k.dma((w_, w_[:, :, 0:cw]), (W[0], W[1][:, c0:c0 + cw].rearrange("(ko p) n -> p ko n", p=128)), eng="pool")
                    for (f0, f1, PF, roff) in f_list:
                        fa, fb = max(c0, f0), min(c0 + cw, f1)
                        if fa >= fb:
                            continue
                        for n0 in range(fa, fb, 128):
                            nw_ = min(128, fb - n0)
                            for tb in range(0, L, 512):
                                tw = min(512, L - tb)
                                p_ = pp[cnt % 4]; s_ = stg[cnt % 4]; cnt += 1
                                for ko in range(KO):
                                    k.mm((p_, p_[0:nw_, 0:tw]), (w_, w_[:, ko, n0 - c0:n0 - c0 + nw_]), (HT, HT[:, ko, tb:tb + tw]),
                                         start=(ko == 0), stop=(ko == KO - 1))
                                k.cp("act" if cnt % 2 else "dve", (s_, s_[0:nw_, 0:tw]), (p_, p_[0:nw_, 0:tw]))
                                r0 = n0 - f0 + roff
                                k.dma((PF, PF[r0:r0 + nw_, tb:tb + tw], ("f", n0)), (s_, s_[0:nw_, 0:tw]))
                    for (t0, t1, PT, coff) in t_list:
                        ta, tb_ = max(c0, t0), min(c0 + cw, t1)
                        if ta >= tb_:
                            continue
                        tw = tb_ - ta
                        for tt in range(NT):
                            p_ = pp[cnt % 4]; s_ = stg[cnt % 4]; cnt += 1
                            for ko in range(KO):
                                k.mm((p_, p_[:, 0:tw]), (HT, HT[:, ko, tt * 128:(tt + 1) * 128]), (w_, w_[:, ko, ta - c0:ta - c0 + tw]),
                                     start=(ko == 0), stop=(ko == KO - 1))
                            k.cp("act" if cnt % 2 else "dve", (s_, s_[:, 0:tw]), (p_, p_[:, 0:tw]))
                            cc = ta - t0 + coff
                            k.dma((PT, PT[tt * 128:(tt + 1) * 128, cc:cc + tw], ("t", tt, ta)), (s_, s_[:, 0:tw]))
                P.barrier()

        def chunk_order(z):
            if z == 0:
                return list(range(NCH))
            return [3, 2, 1, 0] + list(range(NCH - 1, 3, -1))

        def make_masks(ph):
            ms = []
            for z in range(2):
                m = P.sb(f"mask{z}", [64, 64], F32, es=ph)
                if z == 0:
                    P.op("pool", lambda e, m=m: e.affine_select(out=m.a(), in_=ones[0:64, 0:64], pattern=[[1, 64]], compare_op=ALU.is_ge,
                                                           fill=0.0, base=0, channel_multiplier=-1), reads=[ones], writes=[m])
                else:
                    P.op("pool", lambda e, m=m: e.affine_select(out=m.a(), in_=ones[0:64, 0:64], pattern=[[-1, 64]], compare_op=ALU.is_ge,
                                                           fill=0.0, base=0, channel_multiplier=1), reads=[ones], writes=[m])
                ms.append(m)
            return ms

        def mixer_hgrn(PF, PT, YM):
            BW = 1088
            NB = L // BW
            CB = BW // CH
            with ExitStack() as ph:
                masks = make_masks(ph)
                lbr = P.sb("lbr", [128, 3, 4], F32, es=ph)
                lbe = P.sb("lbe", [128, 3, 4], F32, es=ph)
                lbs = P.sb("lbs", [128, 4], F32, es=ph)
                lb = P.sb("lb", [128, 4], F32, es=ph)
                oml = P.sb("oml", [128, 4], F32, es=ph)
                noml = P.sb("noml", [128, 4], F32, es=ph)
                k.dma(V(lbr), V(I["hgrn_lower_bounds"], I["hgrn_lower_bounds"].a().rearrange("j (h p) -> p j h", p=128)),
                      allow_slow_non_contiguous=True)
                k.act(V(lbe), V(lbr), AF.Exp)
                k.tt("dve", V(lbs), (lbe, lbe[:, 0, :]), (lbe, lbe[:, 1, :]), ALU.add)
                k.tt("dve", V(lbs), V(lbs), (lbe, lbe[:, 2, :]), ALU.add)
                k.recip(V(lbs), V(lbs))
                k.tt("dve", V(lb), (lbe, lbe[:, 0, :]), V(lbs), ALU.mult)
                k.ts("dve", V(oml), V(lb), -1.0, ALU.mult, 1.0, ALU.add)
                k.ts("dve", V(noml), V(oml), -1.0, ALU.mult)
                rst = P.sb("rst", [128, BW], F32, es=ph)
                k.memset("pool", V(rst), 1.0)
                k.memset("pool", (rst, rst.a().rearrange("p (c t) -> p c t", t=CH)[:, :, 0:1]), 0.0)
                nwB = P.sb("nwB", [64, 512], F32, es=ph)
                k.dma(V(nwB), V(I["hgrn_norm_w"], I["hgrn_norm_w"][0].partition_broadcast(64)))
                oacc = P.sb("oacc", [64, NCH, 128], F32, es=ph)
                ssn = P.sb("ssn", [64, NCH], F32, es=ph)
                for h in range(4):
                    with ExitStack() as hs1:
                        t_f = P.sb("t_f", [128, BW], F32, es=hs1)
                        t_s = P.sb("t_s", [128, BW], F32, es=hs1)
                        t_lf = P.sb("t_lf", [128, BW], F32, es=hs1)
                        t_k = P.sb("t_k", [128, BW], F32, es=hs1)
                        t_b = P.sb("t_b", [128, BW], F32, es=hs1)
                        t_e = P.sb("t_e", [128, BW], F32, es=hs1)
                        t_q = P.sb("t_q", [128, BW], F32, es=hs1)
                        cd = [P.sb(f"cd{z}", [128, NCH], F32, es=hs1) for z in range(2)]
                        q_in = [P.sb(f"q_in{z}", [128, L], BF16, es=hs1) for z in range(2)]
                        k_in = [P.sb(f"k_in{z}", [128, L], BF16, es=hs1) for z in range(2)]
                        k_end1 = P.sb("k_end", [128, L], BF16, es=hs1)
                        k_end = [k_end1, k_end1]
                        kET = [P.sb(f"kET{z}", [64, NCH, 128], BF16, es=hs1) for z in range(2)]
                        vb = P.sb("vb", [64, NCH, 128], BF16, es=hs1)
                        S = [P.sb(f"S{z}", [128, 128], F32, es=hs1) for z in range(2)]
                        Sb = [P.sb(f"Sb{z}", [128, 128], BF16, es=hs1) for z in range(2)]
                        Am = [P.sb(f"Am{i}", [64, 64], BF16, es=hs1) for i in range(4)]
                        kvS = [[P.sb(f"kvS{z}_{i}", [128, 128], F32, es=hs1) for i in range(4)] for z in range(2)]
                        AmG = [P.sb(f"AmG{i}", [64, 64], BF16, es=hs1) for i in range(8)]
                        Sb2 = [[P.sb(f"Sb2_{z}_{i}", [128, 128], BF16, es=hs1) for i in range(2)] for z in range(2)]
                        psA = [P.ps(f"psA{i}", [64, 64], F32, es=hs1) for i in range(2)]
                        pso = [P.ps(f"pso{i}", [64, 128], F32, es=hs1) for i in range(2)]
                        pskv = [P.ps(f"pskv{i}", [128, 128], F32, es=hs1) for i in range(2)]
                        pst = [P.ps(f"pst{i}", [64, 4, 128], BF16, es=hs1) for i in range(2)]
                        k.dma(V(vb), (PT, PT[:, h * 128:(h + 1) * 128].rearrange("(c p) d -> p c d", p=CH)), eng="pool")
                        for z in range(2):
                            end = CH - 1 if z == 0 else 0
                            for blk in range(NB):
                                tsl = slice(blk * BW, (blk + 1) * BW)
                                frow = 2432 + z * 512 + h * 128
                                k.dma(V(t_f), (PF, PF[frow:frow + 128, tsl]))
                                k.dma(V(t_q), (PF, PF[1920 + h * 128:1920 + (h + 1) * 128, tsl]))
                                k.act(V(t_s), V(t_f), AF.Sigmoid)
                                k.act(V(t_lf), V(t_s), AF.Ln, bias=(lb, lb[:, h:h + 1]), scale=(oml, oml[:, h:h + 1]))
                                k.ts("dve", V(t_k), V(t_s), (noml, noml[:, h:h + 1]), ALU.mult, (oml, oml[:, h:h + 1]), ALU.add)
                                k.scan(V(t_b), V(rst), V(t_lf), 0.0, ALU.mult, ALU.add)
                                if z == 1:
                                    k.tt("dve", V(t_f), V(t_lf), V(t_b), ALU.subtract)
                                    b3 = t_b.a().rearrange("p (c t) -> p c t", t=CH)
                                    k.tt("dve", (t_lf, t_lf.a().rearrange("p (c t) -> p c t", t=CH)),
                                         (t_f, t_f.a().rearrange("p (c t) -> p c t", t=CH)),
                                         (t_b, b3[:, :, CH - 1:CH].to_broadcast([128, CB, CH])), ALU.add)
                                    bb = t_lf
                                else:
                                    bb = t_b
                                k.act(V(t_e), V(bb), AF.Exp)
                                k.act(V(t_s), V(bb), AF.Exp, scale=-1.0)
                                e3 = t_e.a().rearrange("p (c t) -> p c t", t=CH)
                                k.cp("pool", (cd[z], cd[z][:, blk * CB:(blk + 1) * CB]), (t_e, e3[:, :, end]))
                                k.act(V(t_f), V(t_q), AF.Silu)
                                k.stt("dve", (q_in[z], q_in[z][:, tsl]), V(t_f), 128 ** -0.5, V(t_e), ALU.mult, ALU.mult)
                                k.tt("dve", V(t_k), V(t_k), V(t_s), ALU.mult)
                                k.cp("pool", (k_in[z], k_in[z][:, tsl]), V(t_k))
                                k.tt("pool", (k_end[z], k_end[z][:, tsl].rearrange("p (c t) -> p c t", t=CH)),
                                     (t_k, t_k.a().rearrange("p (c t) -> p c t", t=CH)),
                                     (t_e, e3[:, :, end:end + 1].to_broadcast([128, CB, CH])), ALU.mult)
                            for c4 in range(0, NCH, 4):
                                p_ = pst[(c4 // 4) % 2]
                                for j in range(4):
                                    c = c4 + j
                                    k.tr((p_, p_[:, j, :]), (k_end[z], k_end[z][:, c * CH:(c + 1) * CH]), V(identb))
                                k.cp("act", (kET[z], kET[z][:, c4:c4 + 4, :]), V(p_))
                        k.memset("pool", V(oacc), 0.0)
                        for z in range(2):
                            k.memset("pool", V(S[z]), 0.0)
                            k.memset("pool", V(Sb[z]), 0.0)
                        orders = [chunk_order(0), chunk_order(1)]

                        GH = 4

                        def hg_A(step, z, st):
                            c = orders[z][step]
                            csl = slice(c * CH, (c + 1) * CH)
                            am = AmG[(step % GH) * 2 + z]
                            if st == 0:
                                k.mm(V(psA[z]), (k_in[z], k_in[z][:, csl]), (q_in[z], q_in[z][:, csl]))
                                k.tt("dve", V(am), V(psA[z]), V(masks[z]), ALU.mult)
                            else:
                                k.mm(V(pskv[z]), (kET[z], kET[z][:, c, :]), (vb, vb[:, c, :]))
                                k.cp("act", V(kvS[z][step % GH]), V(pskv[z]))

                        def hg_B(step, z):
                            c = orders[z][step]
                            csl = slice(c * CH, (c + 1) * CH)
                            am = AmG[(step % GH) * 2 + z]
                            po = pso[z]
                            sb_prev = Sb2[z][(step + 1) % 2]
                            sb_new = Sb2[z][step % 2]
                            k.stt("dve", V(S[z]), V(S[z]), (cd[z], cd[z][:, c:c + 1]), V(kvS[z][step % GH]), ALU.mult, ALU.add)
                            k.cp("act", V(sb_new), V(S[z]))
                            k.mm(V(po), V(am), (vb, vb[:, c, :]), start=True, stop=False)
                            k.mm(V(po), (q_in[z], q_in[z][:, csl]), V(sb_prev), start=False, stop=True)
                            k.tt("dve", (oacc, oacc[:, c, :], c), (oacc, oacc[:, c, :], c), V(po), ALU.add)

                        for z in range(2):
                            for i in range(2):
                                k.memset("pool", V(Sb2[z][i]), 0.0)
                        for g0 in range(0, NCH, GH):
                            steps = list(range(g0, min(NCH, g0 + GH)))
                            for st in range(2):
                                for step in steps:
                                    for z in range(2):
                                        hg_A(step, z, st)
                            for step in steps:
                                for z in range(2):
                                    hg_B(step, z)
                    P.barrier()
                    with ExitStack() as hs2:
                        gt = P.sb("gt", [64, NCH, 128], F32, es=hs2)
                        k.tt("dve", V(gt), V(oacc), V(oacc), ALU.mult)
                        P.op("dve", lambda e: e.tensor_reduce(out=ssn.a(), in_=gt.a(), axis=AX.X, op=ALU.add), reads=[gt], writes=[ssn])
                        k.ts("dve", V(ssn), V(ssn), 1.0 / 128, ALU.mult, EPS, ALU.add)
                        k.act(V(ssn), V(ssn), AF.Sqrt)
                        k.recip(V(ssn), V(ssn))
                        k.tt("dve", V(oacc), V(oacc), (ssn, ssn.a().rearrange("p (c o) -> p c o", o=1).to_broadcast([64, NCH, 128])), ALU.mult)
                        k.tt("pool", V(oacc), V(oacc), (nwB, nwB[:, h * 128:(h + 1) * 128].rearrange("p (o d) -> p o d", o=1).to_broadcast([64, NCH, 128])), ALU.mult)
                        k.dma(V(gt), (PT, PT[:, 512 + h * 128:512 + (h + 1) * 128].rearrange("(c p) d -> p c d", p=CH)))
                        k.act(V(gt), V(gt), AF.Silu)
                        k.tt("dve", V(oacc), V(oacc), V(gt), ALU.mult)
                        k.dma((YM, YM[:, 512 + h * 128:512 + (h + 1) * 128].rearrange("(c p) d -> p c d", p=CH), ("h", h)), V(oacc))
                    P.barrier()
                P.barrier()

        def rwkv_shift(PF, PR):
            with ExitStack() as ph:
                mu = P.sb("mu", [128, 15], F32, es=ph)
                omu = P.sb("omu", [128, 15], F32, es=ph)
                hmu = P.sb("hmu", [128, 15], F32, es=ph)
                k.dma(V(mu), V(I["rwkv_mu"], I["rwkv_mu"][0].rearrange("(b p) -> p b", p=128)), allow_slow_non_contiguous=True)
                k.ts("dve", V(omu), V(mu), -1.0, ALU.mult, 1.0, ALU.add)
                k.ts("dve", V(hmu), V(mu), 0.5, ALU.mult)
                pt = [P.sb(f"shp{i}", [128, L + 2], F32, es=ph) for i in range(2)]
                sm = [P.sb(f"shs{i}", [128, L], F32, es=ph) for i in range(2)]
                for i in range(2):
                    k.memset("pool", (pt[i], pt[i][:, 0:1], "h0"), 0.0)
                    k.memset("pool", (pt[i], pt[i][:, L + 1:L + 2], "h1"), 0.0)
                for b in range(15):
                    p_ = pt[b % 2]; s_ = sm[b % 2]
                    k.dma((p_, p_[:, 1:L + 1], "m"), (PF, PF[b * 128:(b + 1) * 128, :]))
                    k.tt("dve", (s_, s_.a(), "a"), V(p_), (p_, p_[:, 2:L + 2]), ALU.add) if False else None
                    P.op("dve", lambda e, p_=p_, s_=s_: e.tensor_tensor(out=s_.a(), in0=p_[:, 0:L], in1=p_[:, 2:L + 2], op=ALU.add),
                         reads=[p_], writes=[s_])
                    k.cp("dve", (s_, s_[:, 255:256]), (p_, p_[:, 255:256]))
                    k.cp("dve", (s_, s_[:, 256:257]), (p_, p_[:, 258:259]))
                    k.ts("pool", V(s_), V(s_), (hmu, hmu[:, b:b + 1]), ALU.mult)
                    k.stt("dve", V(s_), (p_, p_[:, 1:L + 1]), (omu, omu[:, b:b + 1]), V(s_), ALU.mult, ALU.add)
                    k.dma((PR, PR[b * 128:(b + 1) * 128, :], b), V(s_))
                P.barrier()

        def rwkv_gate(PR, GT):
            with ExitStack() as ph:
                gd = P.sb("gd", [128, L], F32, es=ph)
                gs = P.sb("gs", [128, L], BF16, es=ph)
                g2f = P.sb("g2f", [128, 512], F32, es=ph)
                g2b = P.sb("g2b", [128, 512], BF16, es=ph)
                pg = [P.ps(f"pg{i}", [128, 512], F32, es=ph) for i in range(2)]
                sg = [P.sb(f"sg{i}", [128, 512], F32, es=ph) for i in range(2)]
                k.dma(V(gd), (PR, PR[1792:1920, :]))
                k.dma(V(g2f), V(I["rwkv_g2"], I["rwkv_g2"][0]))
                k.cp("dve", V(g2b), V(g2f))
                for q4 in range(4):
                    k.act((gs, gs[:, q4 * 1088:(q4 + 1) * 1088], q4), (gd, gd[:, q4 * 1088:(q4 + 1) * 1088]), AF.Sigmoid)
                for tt in range(NT if "gate_nomm" not in debug else 0):
                    k.mm(V(pg[tt % 2]), (gs, gs[:, tt * 128:(tt + 1) * 128]), V(g2b))
                    k.cp("dve" if tt % 2 else "act", V(sg[tt % 2]), V(pg[tt % 2]))
                    k.dma((GT, GT[tt * 128:(tt + 1) * 128, :], tt), V(sg[tt % 2]))
                P.barrier()

        def mixer_rwkv(PR, GT, YM):
            BW = 256
            NB = L // BW
            CB = BW // CH
            NL = 5
            with ExitStack() as ph:
                w2all = P.sb("w2all", [128, 512], F32, es=ph)
                a2all = P.sb("a2all", [128, 512], F32, es=ph)
                k.dma(V(w2all), V(I["rwkv_w2"], I["rwkv_w2"][0].rearrange("z r c -> (z r) c")))
                k.dma(V(a2all), V(I["rwkv_a2"], I["rwkv_a2"][0].rearrange("z r c -> (z r) c")))
                def hv(name, src):
                    t = P.sb(name, [64, 8], F32, es=ph)
                    k.dma(V(t), (src[0], src[1].rearrange("(h n) -> n h", n=64)), allow_slow_non_contiguous=True)
                    return t
                w0 = [hv(f"w0_{z}", (I["rwkv_w0"], I["rwkv_w0"][0, z])) for z in range(2)]
                a0 = [hv(f"a0_{z}", (I["rwkv_a0"], I["rwkv_a0"][0, z])) for z in range(2)]
                kkg = hv("kkg", (I["rwkv_k_k"], I["rwkv_k_k"][0]))
                kag = hv("kag", (I["rwkv_k_a"], I["rwkv_k_a"][0]))
                rkg = hv("rkg", (I["rwkv_r_k"], I["rwkv_r_k"][0]))
                oka = P.sb("oka", [64, 8], F32, es=ph)
                k.ts("dve", V(oka), V(kag), -1.0, ALU.mult, 1.0, ALU.add)
                lnw = P.sb("lnw", [64, 512], F32, es=ph)
                lnb = P.sb("lnb", [64, 512], F32, es=ph)
                k.dma(V(lnw), V(I["rwkv_ln_w"], I["rwkv_ln_w"][0].partition_broadcast(64)))
                k.dma(V(lnb), V(I["rwkv_ln_b"], I["rwkv_ln_b"][0].partition_broadcast(64)))
                rst = P.sb("rst", [64, BW], F32, es=ph)
                k.memset("pool", V(rst), 1.0)
                k.memset("pool", (rst, rst.a().rearrange("p (c t) -> p c t", t=CH)[:, :, 0:1]), 0.0)
                ones64 = P.sb("ones64", [64, 64], F32, es=ph)
                k.memset("pool", V(ones64), 1.0)
                m4 = []
                m3 = []
                for z in range(2):
                    m = P.sb(f"m4_{z}", [128, 2, 64], F32, es=ph)
                    for half in range(2):
                        for col in range(2):
                            sgn = 1 if z == 0 else -1
                            base = (-1 if col == 0 else 0)
                            P.op("pool", lambda e, m=m, half=half, col=col, sgn=sgn, base=base: e.affine_select(
                                out=m[half * 64:(half + 1) * 64, col, :], in_=ones[half * 64:(half + 1) * 64, 0:64],
                                pattern=[[sgn, 64]], compare_op=ALU.is_ge, fill=0.0, base=base, channel_multiplier=-sgn),
                                reads=[ones], writes=[m])
                    m4.append(m)
                    mm3 = P.sb(f"m3_{z}", [64, 64], F32, es=ph)
                    P.op("pool", lambda e, mm3=mm3, z=z: e.affine_select(
                        out=mm3.a(), in_=ones[0:64, 0:64], pattern=[[-1 if z == 0 else 1, 64]], compare_op=ALU.is_ge, fill=0.0,
                        base=-1, channel_multiplier=1 if z == 0 else -1), reads=[ones], writes=[mm3])
                    m3.append(mm3)
                QI0 = P.sb("QI0", [64, 64], BF16, es=ph)
                k.cp("dve", V(QI0), (ident, ident[0:64, 0:64]))

                nheads = 0 if "rw1" in debug else (1 if ("rw2" in debug or "rw3" in debug) else 8)
                for h in range(nheads):
                    with ExitStack() as hs:
                        Vt = P.sb("Vt", [64, NCH, CH], F32, es=hs)
                        oacc = P.sb("oaccr", [64, NCH, CH], F32, es=hs)
                        bon = P.sb("bon", [64, NCH], F32, es=hs)
                        hs2 = ExitStack()
                        AR = [P.sb(f"AR{z}", [64, NCH, 2, CH], BF16, es=hs2) for z in range(2)]
                        BK = [P.sb(f"BK{z}", [64, NCH, 2, CH], BF16, es=hs2) for z in range(2)]
                        BKeT = [P.sb(f"BKeT{z}", [128, NCH, CH], BF16, es=hs2) for z in range(2)]
                        UV = [P.sb(f"UV{z}", [128, NCH, CH], BF16, es=hs2) for z in range(2)]
                        ZV = P.sb("ZV", [128, NCH, CH], BF16, es=hs2)
                        gC = [P.sb(f"gC{z}", [64, NCH], F32, es=hs2) for z in range(2)]
                        k.memset("pool", (ZV, ZV[0:64, :, :], "z"), 0.0)
                        with ExitStack() as pp_:
                            def T(name, dt=F32, w=BW):
                                return P.sb(name, [64, w], dt, es=pp_)
                            t_r = T("t_r"); t_k = T("t_k"); t_v = T("t_v")
                            t_wd = P.sb("t_wd", [128, BW], F32, es=pp_)
                            t_ad = P.sb("t_ad", [128, BW], F32, es=pp_)
                            t_kk = T("t_kk"); t_q = T("t_q"); t_rn = T("t_rn")
                            t_sg = T("t_sg"); t_cs = T("t_cs"); t_x = T("t_x"); t_y = T("t_y")
                            t_eg = T("t_eg"); t_eng = T("t_eng"); t_egp = T("t_egp")
                            t_a = T("t_a"); t_km = [T("t_km0"), T("t_km1")]; t_b = T("t_b")
                            bke = P.sb("bke", [64, CB, 2, CH], BF16, es=pp_)
                            t_v2 = P.sb("t_v2", [64, CB, 2, CH], F32, es=pp_)
                            pwa = [P.ps(f"pwa{i}", [64, BW], F32, es=pp_) for i in range(2)]
                            pss = P.ps("pss", [64, BW], F32, es=pp_)
                            ptv = P.ps("ptv", [128, CB, CH], F32, es=pp_)
                            ptb = P.ps("ptb", [128, CB, CH], BF16, es=pp_)
                            pbn = P.ps("pbn", [64, NCH], F32, es=pp_)
                            for blk in range(NB):
                                tsl = slice(blk * BW, (blk + 1) * BW)
                                csl = slice(blk * CB, (blk + 1) * CB)
                                k.dma(V(t_r), (PR, PR[h * 64:(h + 1) * 64, tsl]))
                                k.dma(V(t_k), (PR, PR[512 + h * 64:512 + (h + 1) * 64, tsl]))
                                k.dma(V(t_v), (PR, PR[1024 + h * 64:1024 + (h + 1) * 64, tsl]))
                                k.dma(V(t_wd), (PR, PR[1536:1664, tsl]))
                                k.dma(V(t_ad), (PR, PR[1664:1792, tsl]))
                                k.act(V(t_wd), V(t_wd), AF.Tanh)
                                k.cp("pool", V(t_v2), (t_v, t_v.a().rearrange("p (c o t) -> p c o t", o=1, t=CH).to_broadcast([64, CB, 2, CH])))
                                for j in range(CB):
                                    k.tr((ptv, ptv[:, j, :]), (t_v2, t_v2[:, j, :, :].rearrange("p a t -> p (a t)")), (ident, ident[0:64, 0:64]))
                                k.cp("act", (Vt, Vt[:, csl, :], blk), (ptv, ptv[0:64, :, :]))
                                for z in range(2):
                                    k.cp("dve", (UV[z], UV[z][64:128, csl, :], ("v", blk)), (ptv, ptv[64:128, :, :]))
                                k.cp("dve", (ZV, ZV[64:128, csl, :], ("v", blk)), (ptv, ptv[64:128, :, :]))
                                k.ts("dve", V(t_kk), V(t_k), (kkg, kkg[:, h:h + 1]), ALU.mult)
                                k.tt("pool", V(t_q), V(t_kk), V(t_kk), ALU.mult)
                                k.mm(V(pss), V(ones64), V(t_q))
                                k.ts("dve", V(t_rn), V(pss), 1e-12, ALU.add)
                                k.act(V(t_rn), V(t_rn), AF.Sqrt)
                                k.recip(V(t_rn), V(t_rn))
                                k.tt("dve", V(t_kk), V(t_kk), V(t_rn), ALU.mult)
                                for z in range(2):
                                    end = CH - 1 if z == 0 else 0
                                    zs = slice(z * 64, (z + 1) * 64)
                                    pw = pwa[0]; pa = pwa[1]
                                    k.mm(V(pw), (w2all, w2all[zs, h * 64:(h + 1) * 64]), (t_wd, t_wd[zs, :]))
                                    k.mm(V(pa), (a2all, a2all[zs, h * 64:(h + 1) * 64]), (t_ad, t_ad[zs, :]))
                                    k.act(V(t_sg), V(pw), AF.Sigmoid, bias=(w0[z], w0[z][:, h:h + 1]))
                                    k.act(V(t_a), V(pa), AF.Sigmoid, bias=(a0[z], a0[z][:, h:h + 1]))
                                    k.scan(V(t_cs), V(rst), V(t_sg), 0.0, ALU.mult, ALU.add)
                                    if z == 1:
                                        k.tt("dve", V(t_x), V(t_sg), V(t_cs), ALU.subtract)
                                        c3 = t_cs.a().rearrange("p (c t) -> p c t", t=CH)
                                        k.tt("dve", (t_y, t_y.a().rearrange("p (c t) -> p c t", t=CH)),
                                             (t_x, t_x.a().rearrange("p (c t) -> p c t", t=CH)),
                                             (t_cs, c3[:, :, CH - 1:CH].to_broadcast([64, CB, CH])), ALU.add)
                                        cs = t_y
                                    else:
                                        cs = t_cs
                                    k.act(V(t_eg), V(cs), AF.Exp, scale=-0.6065306597126334)
                                    k.act(V(t_eng), V(cs), AF.Exp, scale=0.6065306597126334)
                                    k.tt("dve", V(t_x), V(cs), V(t_sg), ALU.subtract)
                                    k.act(V(t_egp), V(t_x), AF.Exp, scale=-0.6065306597126334)
                                    eg3 = t_eg.a().rearrange("p (c t) -> p c t", t=CH)
                                    k.cp("pool", (gC[z], gC[z][:, csl], blk), (t_eg, eg3[:, :, end]))
                                    k.ts("dve", V(t_x), V(t_a), (kag, kag[:, h:h + 1]), ALU.mult, (oka, oka[:, h:h + 1]), ALU.add)
                                    k.tt("dve", V(t_km[z]), V(t_k), V(t_x), ALU.mult)
                                    k.tt("pool", V(t_b), V(t_kk), V(t_a), ALU.mult)
                                    arz = AR[z]; bkz = BK[z]
                                    k.stt("dve", (arz, arz[:, csl, 0, :], blk), (t_kk, t_kk.a().rearrange("p (c t) -> p c t", t=CH)), -1.0,
                                          (t_egp, t_egp.a().rearrange("p (c t) -> p c t", t=CH)), ALU.mult, ALU.mult)
                                    k.tt("pool", (arz, arz[:, csl, 1, :], blk), (t_r, t_r.a().rearrange("p (c t) -> p c t", t=CH)),
                                         (t_eg, eg3), ALU.mult)
                                    k.tt("dve", V(t_b), V(t_b), V(t_eng), ALU.mult)
                                    k.tt("dve", V(t_x), V(t_km[z]), V(t_eng), ALU.mult)
                                    k.cp("pool", (bkz, bkz[:, csl, 0, :], blk), (t_b, t_b.a().rearrange("p (c t) -> p c t", t=CH)))
                                    k.cp("act", (bkz, bkz[:, csl, 1, :], blk), (t_x, t_x.a().rearrange("p (c t) -> p c t", t=CH)))
                                    gcb = eg3[:, :, end:end + 1].to_broadcast([64, CB, CH])
                                    k.tt("dve", (bke, bke[:, :, 0, :]), (t_b, t_b.a().rearrange("p (c t) -> p c t", t=CH)), (t_eg, gcb), ALU.mult)
                                    k.tt("pool", (bke, bke[:, :, 1, :]), (t_x, t_x.a().rearrange("p (c t) -> p c t", t=CH)), (t_eg, gcb), ALU.mult)
                                    for j in range(CB):
                                        k.tr((ptb, ptb[:, j, :]), (bke, bke[:, j, :, :].rearrange("p a t -> p (a t)")), (identb, identb[0:64, 0:64]))
                                    k.cp("act", (BKeT[z], BKeT[z][:, csl, :], blk), V(ptb))
                                k.tt("dve", V(t_x), V(t_km[0]), V(t_km[1]), ALU.add)
                                k.stt("dve", V(t_x), V(t_r), (rkg, rkg[:, h:h + 1]), V(t_x), ALU.mult, ALU.mult)
                                for j in range(CB):
                                    c = blk * CB + j
                                    k.mm((pbn, pbn[:, c:c + 1]), (t_x, t_x[:, j * CH:(j + 1) * CH]), (ones64, ones64[:, 0:1]))
                            k.cp("dve", V(bon), V(pbn))
                        P.barrier()
                        with ExitStack() as us:
                            Hs = [P.sb(f"Hs{z}", [64, 64], F32, es=us) for z in range(2)]
                            Hb = [P.sb(f"Hb{z}", [64, 64], BF16, es=us) for z in range(2)]
                            AM = [[P.sb(f"AM{z}{i}", [128, 128], BF16, es=us) for i in range(2)] for z in range(2)]
                            Pm = [[P.sb(f"Pm{z}{i}", [64, 64], BF16, es=us) for i in range(2)] for z in range(2)]
                            QR = [[P.sb(f"QR{z}{i}", [64, 2, 64], BF16, es=us) for i in range(2)] for z in range(2)]
                            TT = [[P.sb(f"TT{z}{i}", [64, 64], BF16, es=us) for i in range(2)] for z in range(2)]
                            Xs = [P.sb(f"Xs{z}", [64, 64], BF16, es=us) for z in range(2)]
                            bA = [P.ps(f"bA{z}", [128, 512], F32, es=us) for z in range(2)]
                            bB = [P.ps(f"bB{z}", [64, 128], F32, es=us) for z in range(2)]
                            bC = [P.ps(f"bC{z}", [64, 64], F32, es=us) for z in range(2)]
                            bD = [P.ps(f"bD{z}", [64, 128], F32, es=us) for z in range(2)]
                            vM = [bA[z][:, 0:128] for z in range(2)]
                            vX = [bA[z][0:64, 128:192] for z in range(2)]
                            vU = [bA[z][0:64, 192:256] for z in range(2)]
                            vL = [bB[z].a() for z in range(2)]
                            vP = [bC[z].a() for z in range(2)]
                            vO = [bD[z][:, 0:64] for z in range(2)]
                            vH = [bD[z][:, 64:128] for z in range(2)]
                            pM = [(bA[z], vM[z]) for z in range(2)]
                            pL = [(bB[z], vL[z]) for z in range(2)]
                            pP = [(bC[z], vP[z]) for z in range(2)]
                            pX = [(bA[z], vX[z]) for z in range(2)]
                            pU = [(bA[z], vU[z]) for z in range(2)]
                            pO = [(bD[z], vO[z]) for z in range(2)]
                            pH = [(bD[z], vH[z]) for z in range(2)]
                            k.memset("pool", V(oacc), 0.0)
                            for z in range(2):
                                k.memset("pool", V(Hs[z]), 0.0)
                                k.memset("pool", V(Hb[z]), 0.0)
                            orders = [chunk_order(0), chunk_order(1)]
                            for step in range(NCH if "rw2" not in debug else 0):
                                for z in range(2):
                                    c = orders[z][step]
                                    am = AM[z][step % 2]
                                    arc = AR[z][:, c, :, :].rearrange("p a t -> p (a t)")
                                    bkc = BK[z][:, c, :, :].rearrange("p a t -> p (a t)")
                                    k.mm(pM[z], (BK[z], bkc), (AR[z], arc))
                                    k.tt("dve", V(am), pM[z], (m4[z], m4[z].a().rearrange("p a t -> p (a t)")), ALU.mult)
                                    k.mm(pP[z], (AR[z], AR[z][:, c, 0, :]), (BK[z], BK[z][:, c, 0, :]))
                                    pm_ = Pm[z][0]
                                    k.tt("pool" if False else "dve", V(pm_), pP[z], V(m3[z]), ALU.mult)
                                    if "u1" in debug:
                                        continue
                                    qr = QR[z][0]
                                    k.cp("act", (qr, qr[:, 0, :]), (am, am[0:64, 0:64]))
                                    k.cp("pool", (qr, qr[:, 1, :]), V(QI0))
                                    for lv in range(1, NL + 1):
                                        qn = QR[z][lv % 2]
                                        pn = Pm[z][lv % 2]
                                        k.mm(pL[z], V(pm_), (qr, qr.a().rearrange("p a t -> p (a t)")))
                                        k.mm(pP[z], (qr, qr[:, 0, :]), V(pm_))
                                        k.cp("act", (qn, qn[:, 0, :]), (bB[z], vL[z][:, 0:64]))
                                        k.tt("dve", (qn, qn[:, 1, :]), (bB[z], vL[z][:, 64:128]), (qr, qr[:, 1, :]), ALU.add)
                                        k.cp("act", V(pn), pP[z])
                                        qr = qn; pm_ = pn
                                    tt_ = TT[z][step % 2]
                                    k.mm((bB[z], vL[z][:, 0:64]), V(pm_), (qr, qr[:, 1, :]))
                                    k.tt("dve", V(tt_), (bB[z], vL[z][:, 0:64]), (qr, qr[:, 1, :]), ALU.add)
                                    if "u2" in debug:
                                        continue
                                    k.mm(pX[z], (AR[z], AR[z][:, c, 0, :]), V(Hb[z]), start=True, stop=False)
                                    P.op("pe", lambda e, px=vX[z], am=am, c=c: e.matmul(px, lhsT=am[:, 0:64], rhs=ZV[:, c, :], start=False, stop=True),
                                         reads=[(am, None), (ZV, "z"), (ZV, ("v", c // CB))], writes=[(bA[z], None)], pe_acc=True)
                                    k.cp("act", V(Xs[z]), pX[z])
                                    k.mm(pU[z], V(tt_), V(Xs[z]))
                                    k.cp("dve", (UV[z], UV[z][0:64, c, :], ("u", c)), pU[z])
                                    if "u3" in debug:
                                        continue
                                    k.mm(pO[z], (AR[z], AR[z][:, c, 1, :]), V(Hb[z]), start=True, stop=False)
                                    P.op("pe", lambda e, po=vO[z], am=am, uv=UV[z], c=c: e.matmul(po, lhsT=am[:, 64:128], rhs=uv[:, c, :], start=False, stop=True),
                                         reads=[(am, None), (UV[z], ("u", c)), (UV[z], ("v", c // CB))], writes=[(bD[z], None)], pe_acc=True)
                                    k.tt("dve", (oacc, oacc[:, c, :], c), (oacc, oacc[:, c, :], c), pO[z], ALU.add)
                                    P.op("pe", lambda e, ph_=vH[z], bt=BKeT[z], uv=UV[z], c=c: e.matmul(ph_, lhsT=bt[:, c, :], rhs=uv[:, c, :], start=True, stop=True),
                                         reads=[(BKeT[z], None), (UV[z], ("u", c)), (UV[z], ("v", c // CB))], writes=[(bD[z], None)])
                                    k.stt("dve", V(Hs[z]), V(Hs[z]), (gC[z], gC[z][:, c:c + 1]), pH[z], ALU.mult, ALU.add)
                                    k.cp("act", V(Hb[z]), V(Hs[z]))
                        if "d_oacc" in debug and h == 0:
                            do = P.dram("d_oacc", [64, NCH * CH], F32, kind="ExternalOutput")
                            k.dma(V(do), (oacc, oacc.a().rearrange("p a b -> p (a b)")))
                            dm4 = P.dram("d_m4", [128, 2 * 128], F32, kind="ExternalOutput")
                            for z in range(2):
                                k.dma((dm4, dm4[:, z * 128:(z + 1) * 128]), (m4[z], m4[z].a().rearrange("p a b -> p (a b)")))
                            dm3 = P.dram("d_m3", [64, 2 * 64], F32, kind="ExternalOutput")
                            for z in range(2):
                                k.dma((dm3, dm3[:, z * 64:(z + 1) * 64]), V(m3[z]))
                            dgc = P.dram("d_gC", [64, 2 * NCH], F32, kind="ExternalOutput")
                            for z in range(2):
                                k.dma((dgc, dgc[:, z * NCH:(z + 1) * NCH]), V(gC[z]))
                            dbon = P.dram("d_bon", [64, NCH], F32, kind="ExternalOutput")
                            k.dma(V(dbon), V(bon))
                            dar = P.dram("d_AR", [64, 2 * NCH * 2 * CH], BF16, kind="ExternalOutput")
                            dbk = P.dram("d_BK", [64, 2 * NCH * 2 * CH], BF16, kind="ExternalOutput")
                            for z in range(2):
                                k.dma((dar, dar[:, z * NCH * 128:(z + 1) * NCH * 128]), (AR[z], AR[z].a().rearrange("p a b c -> p (a b c)")))
                                k.dma((dbk, dbk[:, z * NCH * 128:(z + 1) * NCH * 128]), (BK[z], BK[z].a().rearrange("p a b c -> p (a b c)")))
                        P.barrier()
                        hs2.close()
                        with ExitStack() as fs:
                            gt = P.sb("gtr", [64, NCH, CH], F32, es=fs)
                            cen = P.sb("cen", [64, NCH, CH], F32, es=fs)
                            mu_ = P.sb("mu_", [64, NCH], F32, es=fs)
                            var = P.sb("var", [64, NCH], F32, es=fs)
                            k.dma(V(gt), (GT, GT[:, h * 64:(h + 1) * 64].rearrange("(c p) d -> p c d", p=CH)))
                            P.op("dve", lambda e: e.tensor_reduce(out=mu_.a(), in_=oacc.a(), axis=AX.X, op=ALU.add), reads=[oacc], writes=[mu_])
                            k.ts("dve", V(mu_), V(mu_), 1.0 / 64, ALU.mult)
                            b3 = lambda t: t.a().rearrange("p (c o) -> p c o", o=1).to_broadcast([64, NCH, CH])
                            k.tt("dve", V(oacc), V(oacc), (mu_, b3(mu_)), ALU.subtract)
                            k.tt("pool", V(cen), V(oacc), V(oacc), ALU.mult)
                            P.op("dve", lambda e: e.tensor_reduce(out=var.a(), in_=cen.a(), axis=AX.X, op=ALU.add), reads=[cen], writes=[var])
                            k.ts("dve", V(var), V(var), 1.0 / 64, ALU.mult, 64e-5, ALU.add)
                            k.act(V(var), V(var), AF.Sqrt)
                            k.recip(V(var), V(var))
                            k.tt("dve", V(oacc), V(oacc), (var, b3(var)), ALU.mult)
                            rb = lambda t: t[:, h * 64:(h + 1) * 64].rearrange("p (o d) -> p o d", o=1).to_broadcast([64, NCH, CH])
                            k.tt("pool", V(oacc), V(oacc), (lnw, rb(lnw)), ALU.mult)
                            k.tt("dve", V(oacc), V(oacc), (lnb, rb(lnb)), ALU.add)
                            k.tt("pool", V(cen), V(Vt), (bon, b3(bon)), ALU.mult)
                            k.tt("dve", V(oacc), V(oacc), V(cen), ALU.add)
                            k.tt("dve", V(oacc), V(oacc), V(gt), ALU.mult)
                            k.dma((YM, YM[:, h * 64:(h + 1) * 64].rearrange("(c p) d -> p c d", p=CH), ("r", h)), V(oacc))
                        P.barrier()

        def lat_rows(tt, cc):
            c0 = 2 * (tt - 2) + cc
            return XL[LC:, :].rearrange("(r c) d -> c r d", c=64)[c0]

        def stage_outproj(YM, nfeat, W, xsrc, perm, tiles):
            kf = nfeat // 128
            with ExitStack() as ph:
                wb = P.sb("wob", [128, kf, D], BF16, es=ph)
                k.dma(V(wb), (W[0], W[1].rearrange("(ko p) n -> p ko n", p=128)), eng="pool")
                yt = [P.sb(f"yt{i}", [128, nfeat], F32, es=ph) for i in range(2)]
                yT = [P.sb(f"yT{i}", [128, kf, 128], BF16, es=ph) for i in range(2)]
                xo = [P.sb(f"xo{i}", [128, D], F32, es=ph) for i in range(2)]
                xn_ = [P.sb(f"xq{i}", [128, D], F32, es=ph) for i in range(2)]
                pT = [P.ps(f"poT{i}", [128, 8, 128], F32, es=ph) for i in range(2)]
                po = [P.ps(f"poo{i}", [128, 512], F32, es=ph) for i in range(2)]
                for n_, tt in enumerate(tiles):
                    j = 1 if tt < 2 else 0
                    y_ = yt[n_ % 2]; yT_ = yT[n_ % 2]; x_ = xo[n_ % 2]; q_ = xn_[n_ % 2]
                    k.dma(V(y_), (YM, YM[tt * 128:(tt + 1) * 128, :]))
                    if perm and tt >= 2:
                        for cc in range(2):
                            k.dma((x_, x_[cc * 64:(cc + 1) * 64, :], cc), (xsrc(tt)[0], lat_rows(tt, cc), tt))
                    else:
                        sb_, sap = xsrc(tt)
                        k.dma(V(x_), (sb_, sap, tt))
                    for g8 in range(0, kf, 8):
                        p_ = pT[(g8 // 8 + n_) % 2]
                        for ko in range(8):
                            k.tr((p_, p_[:, ko, :]), (y_, y_[:, (g8 + ko) * 128:(g8 + ko + 1) * 128]), V(ident))
                        k.cp("act", (yT_, yT_[:, g8:g8 + 8, :]), V(p_))
                    for nb in range(2):
                        o_ = po[nb]
                        for ko in range(kf):
                            k.mm(V(o_), (yT_, yT_[:, ko, :]), (wb, wb[:, ko, nb * 512:(nb + 1) * 512]), start=(ko == 0), stop=(ko == kf - 1))
                        k.tt("dve", (q_, q_[:, nb * 512:(nb + 1) * 512]), V(o_), (modB, modB[:, 0, j, nb * 512:(nb + 1) * 512]), ALU.mult)
                    k.tt("pool", V(q_), V(q_), V(x_), ALU.add)
                    if perm and tt >= 2:
                        for cc in range(2):
                            k.dma((XL, lat_rows(tt, cc), tt), (q_, q_[cc * 64:(cc + 1) * 64, :]))
                    else:
                        k.dma((XL, XL[tt * 128:(tt + 1) * 128, :], tt), V(q_))
                P.barrier()

        def stage_moe(l, tiles):
            nh = len(tiles) // 2
            with ExitStack() as ph:
                rw32 = P.sb("rw32", [128, KO, 32], F32, es=ph)
                k.dma(V(rw32), V(I["router_w"], I["router_w"][l].rearrange("(ko p) e -> p ko e", p=128)))
                rbB = P.sb("rbB", [128, 32], F32, es=ph)
                k.dma(V(rbB), V(I["router_b"], I["router_b"][l].partition_broadcast(128)))
                BD = P.sb("BD", [32, D], F32, es=ph)
                k.dma(V(BD), V(I["exp_b_down"], I["exp_b_down"][l]))
                bgF = P.sb("bgF", [128, 32, 8], F32, es=ph)
                buF = P.sb("buF", [128, 32, 8], F32, es=ph)
                with ExitStack() as t0:
                    btmp = P.sb("btmp", [128, 128], F32, es=t0)
                    pbt = P.ps("pbt", [128, 128], F32, es=t0)
                    for (dst, src) in ((bgF, I["exp_b_gate"]), (buF, I["exp_b_up"])):
                        for hf in range(2):
                            k.dma(V(btmp), (src, src[l, hf * 16:(hf + 1) * 16, :].rearrange("e (fb p) -> (e fb) p", p=128)))
                            k.tr(V(pbt), V(btmp), V(ident))
                            k.cp("dve", (dst, dst[:, hf * 16:(hf + 1) * 16, :].rearrange("p e f -> p (e f)")), V(pbt))
                    P.barrier()
                for half in range(2):
                    htiles = tiles[half * nh:(half + 1) * nh]
                    NTK = nh * 128
                    with ExitStack() as hs:
                        HTh = P.sb("HTh", [128, KO, NTK], BF16, es=hs)
                        LG = P.sb("LG", [128, nh, 32], F32, es=hs)
                        GW = P.sb("GW", [128, nh, 32], F32, es=hs)
                        acc = P.sb("acc", [128, nh, D], F32, es=hs)
                        with ExitStack() as ns:
                            xt = [P.sb(f"mxt{i}", [128, D], F32, es=ns) for i in range(2)]
                            xn = [P.sb(f"mxn{i}", [128, D], F32, es=ns) for i in range(2)]
                            junk = P.sb("mjunk", [128, D], F32, es=ns)
                            st = [P.sb(f"mst{i}", [128, 4], F32, es=ns) for i in range(2)]
                            tmp = [P.sb(f"mtmp{i}", [128, KO, 128], F32, es=ns) for i in range(2)]
                            h32 = [P.sb(f"mh32{i}", [128, KO, 128], F32, es=ns) for i in range(2)]
                            pT = [P.ps(f"mpT{i}", [128, KO, 128], F32, es=ns) for i in range(2)]
                            plg = [P.ps(f"plg{i}", [128, 32], F32, es=ns) for i in range(2)]
                            pgt = P.ps("pgt", [32, 128], F32, es=ns)
                            GWT = P.sb("GWT", [32, nh, 128], F32, es=ns)
                            pini = [P.ps("pini0", [128, 512], F32, es=ns)] * 2
                            m8 = P.sb("m8", [128, 8], F32, es=ns)
                            msk = P.sb("msk", [128, 32], F32, es=ns)
                            ex = P.sb("ex", [128, 32], F32, es=ns)
                            sm = P.sb("smx", [128, 4], F32, es=ns)
                            for i, tt in enumerate(htiles):
                                j = 1 if tt < 2 else 0
                                x_ = xt[i % 2]; n_ = xn[i % 2]; s_ = st[i % 2]; t_ = tmp[i % 2]; p_ = pT[i % 2]; h_ = h32[i % 2]
                                k.dma(V(x_), (XL, XL[tt * 128:(tt + 1) * 128, :]))
                                k.memset("pool", (s_, s_[:, 0:1]), 0.0)
                                k.act(V(junk), V(x_), AF.Square, accum=(s_, s_[:, 0:1]))
                                k.ts("dve", (s_, s_[:, 1:2]), (s_, s_[:, 0:1]), 1.0 / D, ALU.mult, EPS, ALU.add)
                                k.act((s_, s_[:, 2:3]), (s_, s_[:, 1:2]), AF.Sqrt)
                                k.recip((s_, s_[:, 3:4]), (s_, s_[:, 2:3]))
                                k.ts("dve", V(n_), V(x_), (s_, s_[:, 3:4]), ALU.mult)
                                for ko in range(KO):
                                    k.tr((p_, p_[:, ko, :]), (n_, n_[:, ko * 128:(ko + 1) * 128]), V(ident))
                                k.tt("dve", V(t_), V(p_), (g2F, g2F[:, :, j:j + 1].to_broadcast([128, KO, 128])), ALU.mult)
                                k.tt("pool", V(h_), V(t_), (modF, modF[:, 3, :, j:j + 1].to_broadcast([128, KO, 128])), ALU.add)
                                k.cp("act", (HTh, HTh[:, :, i * 128:(i + 1) * 128], i), V(h_))
                                pl = plg[i % 2]
                                for ko in range(KO):
                                    k.mm(V(pl), (h_, h_[:, ko, :]), (rw32, rw32[:, ko, :]), start=(ko == 0), stop=(ko == KO - 1))
                                lg = (LG, LG[:, i, :], i)
                                k.tt("dve", lg, V(pl), V(rbB), ALU.add)
                                P.op("dve", lambda e, i=i: e.max(out=m8.a(), in_=LG[:, i, :]), reads=[(LG, i)], writes=[m8])
                                k.ts("dve", V(msk), lg, (m8, m8[:, 3:4]), ALU.is_ge)
                                k.ts("dve", (sm, sm[:, 0:1]), (m8, m8[:, 0:1]), -1.0, ALU.mult)
                                k.act(V(ex), lg, AF.Exp, bias=(sm, sm[:, 0:1]))
                                k.tt("dve", V(ex), V(ex), V(msk), ALU.mult)
                                P.op("dve", lambda e: e.tensor_reduce(out=sm[:, 1:2], in_=ex.a(), axis=AX.X, op=ALU.add), reads=[ex], writes=[sm])
                                k.recip((sm, sm[:, 2:3]), (sm, sm[:, 1:2]))
                                k.ts("dve", (GW, GW[:, i, :], i), V(ex), (sm, sm[:, 2:3]), ALU.mult)
                                k.tr(V(pgt), (GW, GW[:, i, :], i), V(ident))
                                k.cp("act", (GWT, GWT[:, i, :], i), V(pgt))
                                for nb in range(2):
                                    k.mm(V(pini[nb]), (GWT, GWT[:, i, :], i), (BD, BD[:, nb * 512:(nb + 1) * 512]))
                                    k.cp("act" if nb else "dve", (acc, acc[:, i, nb * 512:(nb + 1) * 512], (i, nb)), V(pini[nb]))
                            P.barrier()
                        if f"LG{l}" in debug and half == 0:
                            dl = P.dram(f"d_GW{l}", [128, nh * 32], F32, kind="ExternalOutput")
                            k.dma(V(dl), (GW, GW.a().rearrange("p a b -> p (a b)")))
                        with ExitStack() as xs:
                            wgs = [P.sb(f"wg{i}", [128, KO, 512], BF16, es=xs) for i in range(2)]
                            wus = [P.sb(f"wu{i}", [128, KO, 512], BF16, es=xs) for i in range(2)]
                            wds = [P.sb(f"wd{i}", [128, 4, D], BF16, es=xs) for i in range(2)]
                            aTs = [P.sb(f"aT{i}", [128, 4, 512], BF16, es=xs) for i in range(2)]
                            dtmp = [P.sb(f"dtmp{i}", [128, 512], F32, es=xs) for i in range(2)]
                            g1 = [P.sb(f"g1_{i}", [128, 512], F32, es=xs) for i in range(2)]
                            sg = [P.sb(f"sg_{i}", [128, 512], F32, es=xs) for i in range(2)]
                            u1 = [P.sb(f"u1_{i}", [128, 512], F32, es=xs) for i in range(2)]
                            pg = [P.ps(f"mpg{i}", [128, 512], F32, es=xs) for i in range(2)]
                            pu = [P.ps(f"mpu{i}", [128, 512], F32, es=xs) for i in range(2)]
                            pd = [P.ps(f"mpd{i}", [128, 512], F32, es=xs) for i in range(2)]
                            if nh == 17:
                                tws = [512, 512, 384, 384, 384]
                            else:
                                tws = [512] * (NTK // 512)
                            tblocks = []
                            t_acc = 0
                            for tw in tws:
                                tblocks.append((t_acc, tw)); t_acc += tw
                            nexp = 32 if "moe_fast" not in debug else 2
                            cnt = 0
                            nblk = 0
                            cntbox = [0]

                            def emit_loads(he):
                                e, fh = he // 2, he % 2
                                wg = wgs[he % 2]; wu = wus[he % 2]; wd = wds[he % 2]
                                k.dma(V(wg), (I["exp_w_gate"], I["exp_w_gate"][l, e][:, fh * 512:(fh + 1) * 512].rearrange("(ko p) n -> p ko n", p=128)), eng="pool")
                                k.dma(V(wu), (I["exp_w_up"], I["exp_w_up"][l, e][:, fh * 512:(fh + 1) * 512].rearrange("(ko p) n -> p ko n", p=128)), eng="pool")
                                k.dma(V(wd), (I["exp_w_down"], I["exp_w_down"][l, e][fh * 512:(fh + 1) * 512, :].rearrange("(fo p) n -> p fo n", p=128)), eng="pool")

                            def emit_G(j, he, t0_, tw):
                                e, fh = he // 2, he % 2
                                wg = wgs[he % 2]; wu = wus[he % 2]
                                aT = aTs[j % 2]
                                for fbl in range(4):
                                    fb = fh * 4 + fbl
                                    cnt = cntbox[0]; cntbox[0] += 1
                                    pg_ = pg[cnt % 2]; pu_ = pu[cnt % 2]; g_ = g1[cnt % 2]; s_ = sg[cnt % 2]; u_ = u1[cnt % 2]
                                    for ko in range(KO):
                                        k.mm((pg_, pg_[:, 0:tw]), (wg, wg[:, ko, fbl * 128:(fbl + 1) * 128]), (HTh, HTh[:, ko, t0_:t0_ + tw]),
                                             start=(ko == 0), stop=(ko == KO - 1))
                                    for ko in range(KO):
                                        k.mm((pu_, pu_[:, 0:tw]), (wu, wu[:, ko, fbl * 128:(fbl + 1) * 128]), (HTh, HTh[:, ko, t0_:t0_ + tw]),
                                             start=(ko == 0), stop=(ko == KO - 1))
                                    k.ts("dve", (g_, g_[:, 0:tw]), (pg_, pg_[:, 0:tw]), (bgF, bgF[:, e, fb:fb + 1]), ALU.add, 7.0, ALU.min)
                                    k.act((s_, s_[:, 0:tw]), (g_, g_[:, 0:tw]), AF.Sigmoid, scale=1.702)
                                    k.act((u_, u_[:, 0:tw]), (pu_, pu_[:, 0:tw]), AF.Identity, bias=(buF, buF[:, e, fb:fb + 1]))
                                    k.ts("dve", (u_, u_[:, 0:tw]), (u_, u_[:, 0:tw]), 7.0, ALU.min, -7.0, ALU.max)
                                    k.tt("dve", (g_, g_[:, 0:tw]), (g_, g_[:, 0:tw]), (s_, s_[:, 0:tw]), ALU.mult)
                                    k.stt("dve", (aT, aT[:, fbl, 0:tw], fbl), (u_, u_[:, 0:tw]), 1.0, (g_, g_[:, 0:tw]), ALU.add, ALU.mult)

                            def emit_D(j, he, t0_, tw):
                                e = he // 2
                                wd = wds[he % 2]
                                aT = aTs[j % 2]
                                for ti in range(tw // 128):
                                    i = t0_ // 128 + ti
                                    for nb in range(2):
                                        pd_ = pd[nb]
                                        for fo in range(4):
                                            k.mm(V(pd_), (aT, aT[:, fo, ti * 128:(ti + 1) * 128], fo), (wd, wd[:, fo, nb * 512:(nb + 1) * 512]),
                                                 start=(fo == 0), stop=(fo == 3))
                                        asl = (acc, acc[:, i, nb * 512:(nb + 1) * 512], (i, nb))
                                        if nb == 0:
                                            k.stt("dve", asl, V(pd_), (GW, GW[:, i, e:e + 1], i), asl, ALU.mult, ALU.add)
                                        else:
                                            dt_ = dtmp[i % 2]
                                            k.act(V(dt_), V(pd_), AF.Copy, scale=(GW, GW[:, i, e:e + 1], i))
                                            k.tt("dve", asl, asl, V(dt_), ALU.add)

                            blocks = [(he, t0_, tw) for he in range(nexp * 2) for (t0_, tw) in tblocks]
                            emit_loads(0)
                            emit_G(0, *blocks[0])
                            for j in range(len(blocks)):
                                if j + 1 < len(blocks):
                                    if blocks[j + 1][0] != blocks[j][0]:
                                        emit_loads(blocks[j + 1][0])
                                    emit_G(j + 1, *blocks[j + 1])
                                emit_D(j, *blocks[j])
                            P.barrier()
                        with ExitStack() as rs:
                            xr = [P.sb(f"xr{i}", [128, D], F32, es=rs) for i in range(2)]
                            for i, tt in enumerate(htiles):
                                j = 1 if tt < 2 else 0
                                x_ = xr[i % 2]
                                k.dma(V(x_), (XL, XL[tt * 128:(tt + 1) * 128, :], ("m", tt)))
                                k.tt("dve", (acc, acc[:, i, :]), (acc, acc[:, i, :]), (modB, modB[:, 1, j, :]), ALU.mult)
                                k.tt("pool", V(x_), V(x_), (acc, acc[:, i, :]), ALU.add)
                                k.dma((XL, XL[tt * 128:(tt + 1) * 128, :], ("m", tt)), V(x_))
                            P.barrier()

        def mixer_rwkv2(PR, GT, YM):
            BW = 1088
            NB = L // BW
            CB = BW // CH
            NL = 5
            CBU = 4
            NSS = NCH // CBU
            SUB = ((0, 512), (512, 512), (1024, 64))
            ARD = P.dram("ARD", [16, 64, NCH * 128], BF16)
            BKD = P.dram("BKD", [16, 64, NCH * 128], BF16)
            BKTD = P.dram("BKTD", [16, 128, NCH * 64], BF16)
            VVD = P.dram("VVD", [8, 128, NCH * 64], BF16)
            VTD = P.dram("VTD", [8, 64, NCH * 64], F32)
            OD = P.dram("OD", [2, L, 512], F32)
            with ExitStack() as ph:
                w2all = P.sb("w2all", [128, 512], F32, es=ph)
                a2all = P.sb("a2all", [128, 512], F32, es=ph)
                k.dma(V(w2all), V(I["rwkv_w2"], I["rwkv_w2"][0].rearrange("z r c -> (z r) c")))
                k.dma(V(a2all), V(I["rwkv_a2"], I["rwkv_a2"][0].rearrange("z r c -> (z r) c")))

                def hv(name, src):
                    t = P.sb(name, [64, 8], F32, es=ph)
                    k.dma(V(t), (src[0], src[1].rearrange("(h n) -> n h", n=64)), allow_slow_non_contiguous=True)
                    return t
                w0 = [hv(f"w0_{z}", (I["rwkv_w0"], I["rwkv_w0"][0, z])) for z in range(2)]
                a0 = [hv(f"a0_{z}", (I["rwkv_a0"], I["rwkv_a0"][0, z])) for z in range(2)]
                kkg = hv("kkg", (I["rwkv_k_k"], I["rwkv_k_k"][0]))
                kag = hv("kag", (I["rwkv_k_a"], I["rwkv_k_a"][0]))
                rkg = hv("rkg", (I["rwkv_r_k"], I["rwkv_r_k"][0]))
                oka = P.sb("oka", [64, 8], F32, es=ph)
                k.ts("dve", V(oka), V(kag), -1.0, ALU.mult, 1.0, ALU.add)
                lnw = P.sb("lnw", [64, 512], F32, es=ph)
                lnb = P.sb("lnb", [64, 512], F32, es=ph)
                k.dma(V(lnw), V(I["rwkv_ln_w"], I["rwkv_ln_w"][0].partition_broadcast(64)))
                k.dma(V(lnb), V(I["rwkv_ln_b"], I["rwkv_ln_b"][0].partition_broadcast(64)))
                ones64 = P.sb("ones64", [64, 64], F32, es=ph)
                k.memset("pool", V(ones64), 1.0)
                m4 = []
                m3 = []
                for z in range(2):
                    m = P.sb(f"m4_{z}", [128, 2, 64], F32, es=ph)
                    sgn = 1 if z == 0 else -1
                    for half in range(2):
                        for col in range(2):
                            base = (-1 if col == 1 else 0)
                            P.op("pool", lambda e, m=m, half=half, col=col, sgn=sgn, base=base: e.affine_select(
                                out=m[half * 64:(half + 1) * 64, col, :], in_=ones[half * 64:(half + 1) * 64, 0:64],
                                pattern=[[sgn, 64]], compare_op=ALU.is_ge, fill=0.0, base=base, channel_multiplier=-sgn),
                                reads=[ones], writes=[m])
                    m4.append(m)
                    mm3 = P.sb(f"m3_{z}", [64, 64], F32, es=ph)
                    P.op("pool", lambda e, mm3=mm3, z=z: e.affine_select(
                        out=mm3.a(), in_=ones[0:64, 0:64], pattern=[[-1 if z == 0 else 1, 64]], compare_op=ALU.is_ge, fill=0.0,
                        base=-1, channel_multiplier=1 if z == 0 else -1), reads=[ones], writes=[mm3])
                    m3.append(mm3)
                QI0 = P.sb("QI0", [64, 64], BF16, es=ph)
                k.cp("dve", V(QI0), (ident, ident[0:64, 0:64]))
                gCall = P.sb("gCall", [64, 16, NCH], F32, es=ph)
                bonall = P.sb("bonall", [64, 8, NCH], F32, es=ph)
                with ExitStack() as pp_:
                    rst = P.sb("rst", [64, BW], F32, es=pp_)
                    k.memset("pool", V(rst), 1.0)
                    k.memset("pool", (rst, rst.a().rearrange("p (c t) -> p c t", t=CH)[:, :, 0:1]), 0.0)

                    def T(name, dt=F32):
                        return P.sb(name, [64, BW], dt, es=pp_)
                    t_r = T("t_r"); t_k = T("t_k"); t_v = T("t_v")
                    t_wd = P.sb("t_wd", [128, BW], F32, es=pp_)
                    t_ad = P.sb("t_ad", [128, BW], F32, es=pp_)
                    t_kk = T("t_kk"); t_q = T("t_q"); t_rn = T("t_rn")
                    t_sg = T("t_sg"); t_cs = T("t_cs"); t_x = T("t_x"); t_y = T("t_y")
                    t_eg = T("t_eg"); t_eng = T("t_eng"); t_egp = T("t_egp")
                    t_a = T("t_a"); t_km = [T("t_km0"), T("t_km1")]; t_b = T("t_b")
                    t_v2 = P.sb("t_v2", [64, CB, 2, CH], F32, es=pp_)
                    arb = P.sb("arb", [64, CB, 2, CH], BF16, es=pp_)
                    bkb = P.sb("bkb", [64, CB, 2, CH], BF16, es=pp_)
                    bke = P.sb("bke", [64, CB, 2, CH], BF16, es=pp_)
                    bktb = P.sb("bktb", [128, CB, CH], BF16, es=pp_)
                    zvb = P.sb("zvb", [128, CB, CH], BF16, es=pp_)
                    vtb = P.sb("vtb", [64, CB, CH], F32, es=pp_)
                    pwa = [P.ps(f"pwa{i}", [64, 512], F32, es=pp_) for i in range(2)]
                    pss = P.ps("pss", [64, 512], F32, es=pp_)
                    ptv = P.ps("ptv", [128, 4, CH], F32, es=pp_)
                    ptb = P.ps("ptb", [128, 4, CH], BF16, es=pp_)
                    pbn = P.ps("pbn", [64, NCH], F32, es=pp_)
                    k.memset("pool", (zvb, zvb[0:64, :, :], "z"), 0.0)
                    c3 = lambda t: t.a().rearrange("p (c t) -> p c t", t=CH)
                    for h in range(8):
                        for blk in range(NB):
                            tsl = slice(blk * BW, (blk + 1) * BW)
                            csl = slice(blk * CB, (blk + 1) * CB)
                            k.dma(V(t_r), (PR, PR[h * 64:(h + 1) * 64, tsl]))
                            k.dma(V(t_k), (PR, PR[512 + h * 64:512 + (h + 1) * 64, tsl]))
                            k.dma(V(t_v), (PR, PR[1024 + h * 64:1024 + (h + 1) * 64, tsl]))
                            k.dma(V(t_wd), (PR, PR[1536:1664, tsl]))
                            k.dma(V(t_ad), (PR, PR[1664:1792, tsl]))
                            k.act(V(t_wd), V(t_wd), AF.Tanh)
                            k.cp("pool", V(t_v2), (t_v, t_v.a().rearrange("p (c o t) -> p c o t", o=1, t=CH).to_broadcast([64, CB, 2, CH])))
                            for j0 in range(0, CB, 4):
                                nj = min(4, CB - j0)
                                for j in range(nj):
                                    k.tr((ptv, ptv[:, j, :]), (t_v2, t_v2[:, j0 + j, :, :].rearrange("p a t -> p (a t)")), (ident, ident[0:64, 0:64]))
                                k.cp("act", (vtb, vtb[:, j0:j0 + nj, :], j0), (ptv, ptv[0:64, 0:nj, :]))
                                k.cp("dve", (zvb, zvb[64:128, j0:j0 + nj, :], ("v", j0)), (ptv, ptv[64:128, 0:nj, :]))
                            k.dma((VTD, VTD[h][:, blk * CB * CH:(blk + 1) * CB * CH], (h, blk)), (vtb, vtb.a().rearrange("p c t -> p (c t)")))
                            k.dma((VVD, VVD[h][:, blk * CB * CH:(blk + 1) * CB * CH], (h, blk)), (zvb, zvb.a().rearrange("p c t -> p (c t)")))
                            k.ts("dve", V(t_kk), V(t_k), (kkg, kkg[:, h:h + 1]), ALU.mult)
                            k.tt("pool", V(t_q), V(t_kk), V(t_kk), ALU.mult)
                            for (s0, sw) in SUB:
                                k.mm((pss, pss[:, 0:sw]), V(ones64), (t_q, t_q[:, s0:s0 + sw]))
                                k.ts("dve", (t_rn, t_rn[:, s0:s0 + sw], s0), (pss, pss[:, 0:sw]), 1e-12, ALU.add)
                            k.act(V(t_rn), V(t_rn), AF.Sqrt)
                            k.recip(V(t_rn), V(t_rn))
                            k.tt("dve", V(t_kk), V(t_kk), V(t_rn), ALU.mult)
                            for z in range(2):
                                end = CH - 1 if z == 0 else 0
                                zs = slice(z * 64, (z + 1) * 64)
                                for (s0, sw) in SUB:
                                    pw = pwa[0]; pa = pwa[1]
                                    k.mm((pw, pw[:, 0:sw]), (w2all, w2all[zs, h * 64:(h + 1) * 64]), (t_wd, t_wd[zs, s0:s0 + sw]))
                                    k.mm((pa, pa[:, 0:sw]), (a2all, a2all[zs, h * 64:(h + 1) * 64]), (t_ad, t_ad[zs, s0:s0 + sw]))
                                    k.act((t_sg, t_sg[:, s0:s0 + sw], s0), (pw, pw[:, 0:sw]), AF.Sigmoid, bias=(w0[z], w0[z][:, h:h + 1]))
                                    k.act((t_a, t_a[:, s0:s0 + sw], s0), (pa, pa[:, 0:sw]), AF.Sigmoid, bias=(a0[z], a0[z][:, h:h + 1]))
                                k.scan(V(t_cs), V(rst), V(t_sg), 0.0, ALU.mult, ALU.add)
                                if z == 1:
                                    k.tt("dve", V(t_x), V(t_sg), V(t_cs), ALU.subtract)
                                    k.tt("dve", (t_y, c3(t_y)), (t_x, c3(t_x)), (t_cs, c3(t_cs)[:, :, CH - 1:CH].to_broadcast([64, CB, CH])), ALU.add)
                                    cs = t_y
                                else:
                                    cs = t_cs
                                k.act(V(t_eg), V(cs), AF.Exp, scale=-0.6065306597126334)
                                k.act(V(t_eng), V(cs), AF.Exp, scale=0.6065306597126334)
                                k.tt("dve", V(t_x), V(cs), V(t_sg), ALU.subtract)
                                k.act(V(t_egp), V(t_x), AF.Exp, scale=-0.6065306597126334)
                                eg3 = c3(t_eg)
                                k.cp("pool", (gCall, gCall[:, h * 2 + z, csl], (h, z, blk)), (t_eg, eg3[:, :, end]))
                                k.ts("dve", V(t_x), V(t_a), (kag, kag[:, h:h + 1]), ALU.mult, (oka, oka[:, h:h + 1]), ALU.add)
                                k.tt("dve", V(t_km[z]), V(t_k), V(t_x), ALU.mult)
                                k.tt("pool", V(t_b), V(t_kk), V(t_a), ALU.mult)
                                k.stt("dve", (arb, arb[:, :, 1, :], 1), (t_kk, c3(t_kk)), -1.0, (t_egp, c3(t_egp)), ALU.mult, ALU.mult)
                                k.tt("pool", (arb, arb[:, :, 0, :], 0), (t_r, c3(t_r)), (t_eg, eg3), ALU.mult)
                                k.tt("dve", V(t_b), V(t_b), V(t_eng), ALU.mult)
                                k.tt("dve", V(t_x), V(t_km[z]), V(t_eng), ALU.mult)
                                k.cp("pool", (bkb, bkb[:, :, 0, :], 0), (t_b, c3(t_b)))
                                k.cp("act", (bkb, bkb[:, :, 1, :], 1), (t_x, c3(t_x)))
                                gcb = eg3[:, :, end:end + 1].to_broadcast([64, CB, CH])
                                k.tt("dve", (bke, bke[:, :, 0, :], 0), (t_b, c3(t_b)), (t_eg, gcb), ALU.mult)
                                k.tt("pool", (bke, bke[:, :, 1, :], 1), (t_x, c3(t_x)), (t_eg, gcb), ALU.mult)
                                for j0 in range(0, CB, 4):
                                    nj = min(4, CB - j0)
                                    for j in range(nj):
                                        k.tr((ptb, ptb[:, j, :]), (bke, bke[:, j0 + j, :, :].rearrange("p a t -> p (a t)")), (identb, identb[0:64, 0:64]))
                                    k.cp("act", (bktb, bktb[:, j0:j0 + nj, :], j0), (ptb, ptb[:, 0:nj, :]))
                                hz = h * 2 + z
                                k.dma((ARD, ARD[hz][:, blk * CB * 128:(blk + 1) * CB * 128], (hz, blk)), (arb, arb.a().rearrange("p c a t -> p (c a t)")))
                                k.dma((BKD, BKD[hz][:, blk * CB * 128:(blk + 1) * CB * 128], (hz, blk)), (bkb, bkb.a().rearrange("p c a t -> p (c a t)")))
                                k.dma((BKTD, BKTD[hz][:, blk * CB * CH:(blk + 1) * CB * CH], (hz, blk)), (bktb, bktb.a().rearrange("p c t -> p (c t)")))
                            k.tt("dve", V(t_x), V(t_km[0]), V(t_km[1]), ALU.add)
                            k.stt("dve", V(t_x), V(t_r), (rkg, rkg[:, h:h + 1]), V(t_x), ALU.mult, ALU.mult)
                            for j in range(CB):
                                c = blk * CB + j
                                k.mm((pbn, pbn[:, c:c + 1]), (t_x, t_x[:, j * CH:(j + 1) * CH]), (ones64, ones64[:, 0:1]))
                        k.cp("dve", (bonall, bonall[:, h, :], h), V(pbn))
                    P.barrier()
                for ps_ in range(2 if "rw_preponly" not in debug else 0):
                    with ExitStack() as us:
                        chains = [(ps_ * 4 + hl, z) for hl in range(4) for z in range(2)]
                        CHN = []
                        for ci, (h, z) in enumerate(chains):
                            d_ = {}
                            d_["h"] = h; d_["z"] = z; d_["hz"] = h * 2 + z; d_["ci"] = ci
                            d_["Hs"] = P.sb(f"Hs{ci}", [64, 64], F32, es=us)
                            d_["Hb"] = P.sb(f"Hb{ci}", [64, 64], BF16, es=us)
                            d_["AM"] = [P.sb(f"AM{ci}_{i}", [128, 192], BF16, es=us) for i in range(2)]
                            for am_ in d_["AM"]:
                                k.cp("dve", (am_, am_[0:64, 128:192], "I"), (ident, ident[0:64, 0:64]))
                            d_["Pm"] = [P.sb(f"Pm{ci}_{i}", [64, 64], BF16, es=us) for i in range(1)]
                            d_["QR"] = [P.sb(f"QR{ci}_{i}", [64, 3, 64], BF16, es=us) for i in range(2)]
                            d_["TT"] = [P.sb(f"TT{ci}_{i}", [64, 64], BF16, es=us) for i in range(2)]
                            d_["Xs"] = P.sb(f"Xs{ci}", [64, 64], BF16, es=us)
                            d_["ARb"] = [P.sb(f"ARb{ci}_{i}", [64, CBU, 2, CH], BF16, es=us) for i in range(2)]
                            d_["BKb"] = [P.sb(f"BKb{ci}_{i}", [64, CBU, 2, CH], BF16, es=us) for i in range(2)]
                            d_["BKTb"] = [P.sb(f"BKTb{ci}_{i}", [128, CBU, CH], BF16, es=us) for i in range(2)]
                            d_["UVb"] = [P.sb(f"UVb{ci}_{i}", [128, CBU, CH], BF16, es=us) for i in range(2)]
                            d_["ZVb"] = [P.sb(f"ZVb{ci}_{i}", [128, CBU, CH], BF16, es=us) for i in range(2)]
                            d_["Ob"] = [P.sb(f"Ob{ci}_{i}", [64, CBU, CH], F32, es=us) for i in range(2)]
                            d_["bank"] = P.ps(f"bank{ci}", [128, 512], F32, es=us)
                            k.memset("pool", V(d_["Hs"]), 0.0)
                            k.memset("pool", V(d_["Hb"]), 0.0)
                            CHN.append(d_)

                        def clo(z, ss):
                            if z == 0:
                                return ss * CBU
                            return 0 if ss == 0 else NCH - CBU * ss

                        def loads(ss):
                            for d_ in CHN:
                                c0 = clo(d_["z"], ss); i = ss % 2; hz = d_["hz"]; h = d_["h"]
                                k.dma((d_["ARb"][i], d_["ARb"][i].a().rearrange("p c a t -> p (c a t)")), (ARD, ARD[hz][:, c0 * 128:(c0 + CBU) * 128]))
                                k.dma((d_["BKb"][i], d_["BKb"][i].a().rearrange("p c a t -> p (c a t)")), (BKD, BKD[hz][:, c0 * 128:(c0 + CBU) * 128]))
                                k.dma((d_["BKTb"][i], d_["BKTb"][i].a().rearrange("p c t -> p (c t)")), (BKTD, BKTD[hz][:, c0 * CH:(c0 + CBU) * CH]))
                                k.dma((d_["ZVb"][i], d_["ZVb"][i].a().rearrange("p c t -> p (c t)")), (VVD, VVD[h][:, c0 * CH:(c0 + CBU) * CH]))
                                k.dma((d_["UVb"][i], d_["UVb"][i][64:128, :, :].rearrange("p c t -> p (c t)"), "v"), (VVD, VVD[h][64:128, c0 * CH:(c0 + CBU) * CH]))

                        def unit_stage(d_, ss, jj, st):
                            z = d_["z"]; h = d_["h"]; hz = d_["hz"]
                            i = ss % 2
                            c0 = clo(z, ss)
                            cl = jj if z == 0 else CBU - 1 - jj
                            c = c0 + cl
                            step = ss * CBU + jj
                            ci = d_["ci"]
                            bk_ = d_["bank"]
                            vM = bk_[:, 0:128]; vL = bk_[0:64, 128:256]; vP = bk_[0:64, 256:320]; vLP = bk_[0:64, 128:320]
                            vX = bk_[0:64, 320:384]; vU = bk_[0:64, 384:448]; vH = bk_[0:64, 448:512]
                            ARb = d_["ARb"][i]; BKb = d_["BKb"][i]; BKTb = d_["BKTb"][i]; UVb = d_["UVb"][i]; ZVb = d_["ZVb"][i]; Ob = d_["Ob"][i]
                            am = d_["AM"][step % 2]
                            tt_ = d_["TT"][step % 2]
                            Hb = d_["Hb"]; Hs = d_["Hs"]; Xs = d_["Xs"]
                            ev = "act" if ci % 2 else "dve"
                            if st == 0:
                                arc = ARb[:, cl, :, :].rearrange("p a t -> p (a t)")
                                bkc = BKb[:, cl, :, :].rearrange("p a t -> p (a t)")
                                k.mm((bk_, vM), (BKb, bkc), (ARb, arc))
                                k.mm((bk_, vP), (ARb, ARb[:, cl, 1, :]), (BKb, BKb[:, cl, 0, :]))
                                k.tt("dve", (am, am[:, 0:128], "m"), (bk_, vM), (m4[z], m4[z].a().rearrange("p a t -> p (a t)")), ALU.mult)
                                k.tt("dve", V(d_["Pm"][0]), (bk_, vP), V(m3[z]), ALU.mult)
                            elif 1 <= st <= NL:
                                lv = st
                                qn = d_["QR"][lv % 2]
                                if lv == 1:
                                    rqr = (am, am[0:64, 64:192]); rr = (am, am[0:64, 128:192]); lq = (am, am[0:64, 64:128]); pm_ = V(d_["Pm"][0])
                                else:
                                    qp = d_["QR"][(lv - 1) % 2]
                                    rqr = (qp, qp[:, 0:2, :].rearrange("p a t -> p (a t)")); rr = (qp, qp[:, 1, :]); lq = (qp, qp[:, 0, :]); pm_ = (qp, qp[:, 2, :])
                                k.mm((bk_, vL), pm_, rqr, start=True, stop=False)
                                k.mm((bk_, vL[:, 64:128]), V(QI0), rr, start=False, stop=True)
                                k.mm((bk_, vP), lq, pm_)
                                k.cp(ev, (qn, qn.a().rearrange("p a t -> p (a t)")), (bk_, vLP))
                            elif st == NL + 1:
                                qp = d_["QR"][NL % 2]
                                k.mm((bk_, vL[:, 0:64]), (qp, qp[:, 2, :]), (qp, qp[:, 1, :]), start=True, stop=False)
                                k.mm((bk_, vL[:, 0:64]), V(QI0), (qp, qp[:, 1, :]), start=False, stop=True)
                                k.cp(ev, V(tt_), (bk_, vL[:, 0:64]))
                            elif st == NL + 2:
                                k.mm((bk_, vX), (ARb, ARb[:, cl, 1, :]), V(Hb), start=True, stop=False)
                                P.op("pe", lambda e, vX=vX, am=am, ZVb=ZVb, cl=cl: e.matmul(vX, lhsT=am[:, 64:128], rhs=ZVb[:, cl, :], start=False, stop=True),
                                     reads=[(am, "m"), (ZVb, None)], writes=[(bk_, None)], pe_acc=True)
                                k.cp("act", V(Xs), (bk_, vX))
                            elif st == NL + 3:
                                k.mm((bk_, vU), V(tt_), V(Xs))
                                k.cp("dve", (UVb, UVb[0:64, cl, :], ("u", cl)), (bk_, vU))
                            elif st == NL + 4:
                                k.mm((bk_, vX), (ARb, ARb[:, cl, 0, :]), V(Hb), start=True, stop=False)
                                P.op("pe", lambda e, vX=vX, am=am, UVb=UVb, cl=cl: e.matmul(vX, lhsT=am[:, 0:64], rhs=UVb[:, cl, :], start=False, stop=True),
                                     reads=[(am, "m"), (UVb, ("u", cl)), (UVb, "v")], writes=[(bk_, None)], pe_acc=True)
                                P.op("pe", lambda e, vH=vH, BKTb=BKTb, UVb=UVb, cl=cl: e.matmul(vH, lhsT=BKTb[:, cl, :], rhs=UVb[:, cl, :], start=True, stop=True),
                                     reads=[(BKTb, None), (UVb, ("u", cl)), (UVb, "v")], writes=[(bk_, None)], pe_acc=True)
                                k.cp("act", (Ob, Ob[:, cl, :], cl), (bk_, vX))
                                k.stt("dve", V(Hs), V(Hs), (gCall, gCall[:, hz, c:c + 1]), (bk_, vH), ALU.mult, ALU.add)
                                k.cp("act", V(Hb), V(Hs))

                        loads(0)
                        for ss in range(NSS):
                            if ss + 1 < NSS:
                                loads(ss + 1)
                            i = ss % 2
                            for jj in range(CBU):
                                for st in range(NL + 5):
                                    for d_ in CHN:
                                        unit_stage(d_, ss, jj, st)
                            for d_ in CHN:
                                c0 = clo(d_["z"], ss); h = d_["h"]; z = d_["z"]
                                k.dma((OD, OD[z][c0 * CH:(c0 + CBU) * CH, h * 64:(h + 1) * 64].rearrange("(c p) d -> p c d", p=CH), (z, h, ss)), V(d_["Ob"][i]))
                        P.barrier()
                with ExitStack() as fs:
                    oacc = P.sb("oaccr", [64, NCH, CH], F32, es=fs)
                    o1 = P.sb("o1r", [64, NCH, CH], F32, es=fs)
                    Vt = P.sb("Vtr", [64, NCH, CH], F32, es=fs)
                    gt = P.sb("gtr", [64, NCH, CH], F32, es=fs)
                    cen = P.sb("cen", [64, NCH, CH], F32, es=fs)
                    mu_ = P.sb("mu_", [64, NCH], F32, es=fs)
                    var = P.sb("var", [64, NCH], F32, es=fs)
                    for h in range(8):
                        k.dma(V(oacc), (OD, OD[0][:, h * 64:(h + 1) * 64].rearrange("(c p) d -> p c d", p=CH)))
                        k.dma(V(o1), (OD, OD[1][:, h * 64:(h + 1) * 64].rearrange("(c p) d -> p c d", p=CH)))
                        k.dma(V(Vt), (VTD, VTD[h].rearrange("p (c t) -> p c t", t=CH)))
                        k.dma(V(gt), (GT, GT[:, h * 64:(h + 1) * 64].rearrange("(c p) d -> p c d", p=CH)))
                        k.tt("pool", V(oacc), V(oacc), V(o1), ALU.add)
                        P.op("dve", lambda e: e.tensor_reduce(out=mu_.a(), in_=oacc.a(), axis=AX.X, op=ALU.add), reads=[oacc], writes=[mu_])
                        k.ts("dve", V(mu_), V(mu_), 1.0 / 64, ALU.mult)
                        b3 = lambda t: t.a().rearrange("p (c o) -> p c o", o=1).to_broadcast([64, NCH, CH])
                        k.tt("dve", V(oacc), V(oacc), (mu_, b3(mu_)), ALU.subtract)
                        k.tt("pool", V(cen), V(oacc), V(oacc), ALU.mult)
                        P.op("dve", lambda e: e.tensor_reduce(out=var.a(), in_=cen.a(), axis=AX.X, op=ALU.add), reads=[cen], writes=[var])
                        k.ts("dve", V(var), V(var), 1.0 / 64, ALU.mult, 64e-5, ALU.add)
                        k.act(V(var), V(var), AF.Sqrt)
                        k.recip(V(var), V(var))
                        k.tt("dve", V(oacc), V(oacc), (var, b3(var)), ALU.mult)
                        rb = lambda t: t[:, h * 64:(h + 1) * 64].rearrange("p (o d) -> p o d", o=1).to_broadcast([64, NCH, CH])
                        k.tt("pool", V(oacc), V(oacc), (lnw, rb(lnw)), ALU.mult)
                        k.tt("dve", V(oacc), V(oacc), (lnb, rb(lnb)), ALU.add)
                        k.tt("pool", V(cen), V(Vt), (bonall, bonall[:, h, :].rearrange("p (c o) -> p c o", o=1).to_broadcast([64, NCH, CH])), ALU.mult)
                        k.tt("dve", V(oacc), V(oacc), V(cen), ALU.add)
                        k.tt("dve", V(oacc), V(oacc), V(gt), ALU.mult)
                        k.dma((YM, YM[:, h * 64:(h + 1) * 64].rearrange("(c p) d -> p c d", p=CH), ("r", h)), V(oacc))
                    P.barrier()

        SEGS = ((0, LC), (LC, SEQ))

        def stage_conv(PF, PC, specs):
            with ExitStack() as ph:
                u = [P.sb(f"cu{i}", [128, L + 8], F32, es=ph) for i in range(2)]
                acc = [P.sb(f"ca{i}", [128, L], F32, es=ph) for i in range(2)]
                cw = P.sb("cw", [128, 20, 5], F32, es=ph)
                cb = P.sb("cb", [128, 20], F32, es=ph)
                for i in range(2):
                    k.memset("pool", (u[i], u[i][:, 0:2], "h0"), 0.0)
                    k.memset("pool", (u[i], u[i][:, LC + 2:LC + 6], "h1"), 0.0)
                    k.memset("pool", (u[i], u[i][:, L + 6:L + 8], "h2"), 0.0)
                bi = 0
                for (row0, nblk, wsrc, bsrc) in specs:
                    for b in range(nblk):
                        k.dma((cw, cw[:, bi, :], bi), (wsrc[0], wsrc[1][:, b * 128:(b + 1) * 128].rearrange("j p -> p j")), allow_slow_non_contiguous=True)
                        k.dma((cb, cb[:, bi:bi + 1], bi), (bsrc[0], bsrc[1][b * 128:(b + 1) * 128].rearrange("(p o) -> p o", o=1)), allow_slow_non_contiguous=True)
                        u_ = u[bi % 2]; a_ = acc[bi % 2]
                        r0 = row0 + b * 128
                        k.dma((u_, u_[:, 2:LC + 2], "c"), (PF, PF[r0:r0 + 128, 0:LC]))
                        k.dma((u_, u_[:, LC + 6:L + 6], "l"), (PF, PF[r0:r0 + 128, LC:L]))
                        for (s0, sl_) in SEGS:
                            off = 0 if s0 == 0 else 4
                            for j in range(5):
                                src = (u_, u_[:, s0 + off + j:s0 + off + j + sl_])
                                dst = (a_, a_[:, s0:s0 + sl_], s0)
                                if j == 0:
                                    k.ts("dve", dst, src, (cw, cw[:, bi, 0:1], bi), ALU.mult)
                                else:
                                    k.stt("dve", dst, src, (cw, cw[:, bi, j:j + 1], bi), dst, ALU.mult, ALU.add)
                            k.act((a_, a_[:, s0:s0 + sl_], s0), (a_, a_[:, s0:s0 + sl_], s0), AF.Silu, bias=(cb, cb[:, bi:bi + 1], bi))
                        k.dma((PC, PC[r0:r0 + 128, :], r0), V(a_))
                        bi += 1
                P.barrier()

        def mixer_mlstm(PC, PF, PT, YM, GSD):
            TB = [(t0_, min(512, L - t0_)) for t0_ in range(0, L, 512)]
            with ExitStack() as ph:
                GI = P.sb("GI", [8, L], F32, es=ph)
                GF = P.sb("GF", [8, L], F32, es=ph)
                t1 = P.sb("gt1", [8, L], F32, es=ph)
                t2 = P.sb("gt2", [8, L], F32, es=ph)
                t3 = P.sb("gt3", [8, L], F32, es=ph)
                rst = P.sb("grst", [8, L], F32, es=ph)
                ib = P.sb("ib", [8, 1], F32, es=ph)
                fb = P.sb("fb", [8, 1], F32, es=ph)
                k.dma(V(GI), (PF, PF[2560:2568, :]))
                k.dma(V(GF), (PF, PF[2568:2576, :]))
                k.dma(V(ib), V(I["mlstm_i_bias"], I["mlstm_i_bias"][0].rearrange("z (h o) -> (z h) o", o=1)), allow_slow_non_contiguous=True)
                k.dma(V(fb), V(I["mlstm_f_bias"], I["mlstm_f_bias"][0].rearrange("z (h o) -> (z h) o", o=1)), allow_slow_non_contiguous=True)
                k.memset("pool", V(rst), 1.0)
                k.memset("pool", (rst, rst.a().rearrange("p (c t) -> p c t", t=CH)[:, :, 0:1]), 0.0)
                k.ts("dve", V(GI), V(GI), (ib, ib[:, 0:1]), ALU.add)
                k.ts("dve", V(fb), V(fb), -1.0, ALU.mult)
                k.act(V(t1), V(GF), AF.Exp, bias=(fb, fb[:, 0:1]), scale=-1.0)
                k.act(V(t1), V(t1), AF.Ln, bias=1.0)
                k.ts("dve", V(t1), V(t1), -1.0, ALU.mult)
                k.scan(V(t2), V(rst), V(t1), 0.0, ALU.mult, ALU.add)
                k.tt("dve", V(t3), V(t1), V(t2), ALU.subtract)
                p3 = t2.a().rearrange("p (c t) -> p c t", t=CH)
                k.tt("dve", (t3, t3.a().rearrange("p (c t) -> p c t", t=CH)), (t3, t3.a().rearrange("p (c t) -> p c t", t=CH)),
                     (t2, p3[:, :, CH - 1:CH].to_broadcast([8, NCH, CH])), ALU.add)
                k.dma((GSD, GSD[0:4, :], 0), (t2, t2[0:4, :]))
                k.dma((GSD, GSD[4:8, :], 1), (t3, t3[4:8, :]))
                k.tt("dve", V(t2), V(GI), V(t2), ALU.subtract)
                k.tt("dve", V(t3), V(GI), V(t3), ALU.subtract)
                k.dma((GSD, GSD[8:12, :], 2), (t2, t2[0:4, :]))
                k.dma((GSD, GSD[12:16, :], 3), (t3, t3[4:8, :]))
                P.barrier()
            with ExitStack() as ph:
                masks = make_masks(ph)
                nwB = P.sb("mnwB", [64, 1024], F32, es=ph)
                k.dma(V(nwB), V(I["mlstm_norm_w"], I["mlstm_norm_w"][0].partition_broadcast(64)))
                oacc = P.sb("moacc", [64, NCH, 256], F32, es=ph)
                ssn = P.sb("mssn", [64, NCH], F32, es=ph)
                for h in range(4):
                    with ExitStack() as hs1:
                        vaug = P.sb("vaug", [64, NCH, 257], BF16, es=hs1)
                        t_q = P.sb("mt_q", [128, 512], F32, es=hs1)
                        t_k = P.sb("mt_k", [128, 512], F32, es=hs1)
                        t_e = P.sb("mt_e", [128, 512], F32, es=hs1)
                        t_s = P.sb("mt_s", [128, 512], F32, es=hs1)
                        cd = P.sb("mcd", [128, NCH], F32, es=hs1)
                        q_in = P.sb("mq_in", [128, L], BF16, es=hs1)
                        k_in = P.sb("mk_in", [128, L], BF16, es=hs1)
                        k_end = P.sb("mk_end", [128, L], BF16, es=hs1)
                        kET = P.sb("mkET", [64, NCH, 128], BF16, es=hs1)
                        S = P.sb("mS", [128, 257], F32, es=hs1)
                        Sb = P.sb("mSb", [128, 257], BF16, es=hs1)
                        Am = [P.sb(f"mAm{i}", [64, 64], BF16, es=hs1) for i in range(2)]
                        dn = P.sb("mdn", [64, 2], F32, es=hs1)

                        psA = P.ps("mpsA", [64, 64], F32, es=hs1)
                        pso = P.ps("mpso", [64, 257], F32, es=hs1)
                        pskv = P.ps("mpskv", [128, 257], F32, es=hs1)
                        pst = [P.ps(f"mpst{i}", [64, 4, 128], BF16, es=hs1) for i in range(2)]
                        kvS = [P.sb(f"mkvS{i}", [128, 257], F32, es=hs1) for i in range(4)]
                        AmG = [P.sb(f"mAmG{i}", [64, 64], BF16, es=hs1) for i in range(4)]
                        Sb2 = [P.sb(f"mSb2_{i}", [128, 257], BF16, es=hs1) for i in range(2)]
                        psA2 = [psA, P.ps("mpsA1", [64, 64], F32, es=hs1)]
                        pskv2 = [pskv, P.ps("mpskv1", [128, 257], F32, es=hs1)]
                        k.dma((vaug, vaug[:, :, 0:256], "v"), (PT, PT[:, 1056 + h * 256:1056 + (h + 1) * 256].rearrange("(c p) d -> p c d", p=CH)), eng="pool")
                        k.memset("pool", (vaug, vaug[:, :, 256:257], "o"), 1.0)
                        k.memset("pool", V(oacc), 0.0)
                        for z in range(2):
                            end = CH - 1 if z == 0 else 0
                            for (t0_, tw) in TB:
                                tsl = slice(t0_, t0_ + tw)
                                ncb = tw // CH
                                c0 = t0_ // CH
                                k.dma((t_e, t_e[:, 0:tw]), (GSD, GSD[z * 4 + h, tsl].partition_broadcast(128)))
                                k.dma((t_s, t_s[:, 0:tw]), (GSD, GSD[8 + z * 4 + h, tsl].partition_broadcast(128)))
                                k.dma((t_q, t_q[:, 0:tw]), (PC, PC[1536 + h * 128:1536 + (h + 1) * 128, tsl]))
                                k.dma((t_k, t_k[:, 0:tw]), (PC, PC[2048 + h * 128:2048 + (h + 1) * 128, tsl]))
                                k.act((t_e, t_e[:, 0:tw]), (t_e, t_e[:, 0:tw]), AF.Exp)
                                k.act((t_s, t_s[:, 0:tw]), (t_s, t_s[:, 0:tw]), AF.Exp)
                                e3 = t_e[:, 0:tw].rearrange("p (c t) -> p c t", t=CH)
                                k.cp("pool", (cd, cd[:, c0:c0 + ncb]), (t_e, e3[:, :, end]))
                                k.tt("dve", (q_in, q_in[:, tsl]), (t_q, t_q[:, 0:tw]), (t_e, t_e[:, 0:tw]), ALU.mult)
                                k.stt("dve", (t_k, t_k[:, 0:tw]), (t_k, t_k[:, 0:tw]), 128 ** -0.5, (t_s, t_s[:, 0:tw]), ALU.mult, ALU.mult)
                                k.cp("pool", (k_in, k_in[:, tsl]), (t_k, t_k[:, 0:tw]))
                                k.tt("pool", (k_end, k_end[:, tsl].rearrange("p (c t) -> p c t", t=CH)),
                                     (t_k, t_k[:, 0:tw].rearrange("p (c t) -> p c t", t=CH)),
                                     (t_e, e3[:, :, end:end + 1].to_broadcast([128, ncb, CH])), ALU.mult)
                            for c4 in range(0, NCH, 4):
                                p_ = pst[(c4 // 4) % 2]
                                for j in range(4):
                                    c = c4 + j
                                    k.tr((p_, p_[:, j, :]), (k_end, k_end[:, c * CH:(c + 1) * CH]), V(identb))
                                k.cp("act", (kET, kET[:, c4:c4 + 4, :]), V(p_))
                            k.memset("pool", V(S), 0.0)
                            k.memset("pool", V(Sb), 0.0)
                            order_ = chunk_order(z)

                            GM = 4

                            def ml_A(step, st):
                                c = order_[step]
                                csl = slice(c * CH, (c + 1) * CH)
                                am = AmG[step % GM]
                                if st == 0:
                                    pa = psA2[step % 2]
                                    k.mm(V(pa), (k_in, k_in[:, csl]), (q_in, q_in[:, csl]))
                                    k.tt("dve", V(am), V(pa), V(masks[z]), ALU.mult)
                                else:
                                    pk = pskv2[step % 2]
                                    k.mm(V(pk), (kET, kET[:, c, :]), (vaug, vaug[:, c, :]))
                                    k.cp("act", V(kvS[step % GM]), V(pk))

                            def ml_B(step):
                                c = order_[step]
                                csl = slice(c * CH, (c + 1) * CH)
                                am = AmG[step % GM]
                                sb_prev = Sb2[(step + 1) % 2]
                                sb_new = Sb2[step % 2]
                                k.stt("dve", V(S), V(S), (cd, cd[:, c:c + 1]), V(kvS[step % GM]), ALU.mult, ALU.add)
                                k.cp("act", V(sb_new), V(S))
                                k.mm(V(pso), V(am), (vaug, vaug[:, c, :]), start=True, stop=False)
                                k.mm(V(pso), (q_in, q_in[:, csl]), V(sb_prev), start=False, stop=True)
                                k.act((dn, dn[:, 0:1]), (pso, pso[:, 256:257]), AF.Abs)
                                k.ts("dve", (dn, dn[:, 0:1]), (dn, dn[:, 0:1]), 1.0, ALU.max)
                                k.recip((dn, dn[:, 1:2]), (dn, dn[:, 0:1]))
                                k.stt("dve", (oacc, oacc[:, c, :], c), (pso, pso[:, 0:256]), (dn, dn[:, 1:2]), (oacc, oacc[:, c, :], c), ALU.mult, ALU.add)

                            k.memset("pool", V(Sb2[0]), 0.0)
                            k.memset("pool", V(Sb2[1]), 0.0)
                            for g0 in range(0, NCH, GM):
                                steps = list(range(g0, min(NCH, g0 + GM)))
                                for st in range(2):
                                    for step in steps:
                                        ml_A(step, st)
                                for step in steps:
                                    ml_B(step)
                    P.barrier()
                    with ExitStack() as hs2:
                        gt = P.sb("mgt", [64, NCH, 256], F32, es=hs2)
                        k.tt("dve", V(gt), V(oacc), V(oacc), ALU.mult)
                        P.op("dve", lambda e: e.tensor_reduce(out=ssn.a(), in_=gt.a(), axis=AX.X, op=ALU.add), reads=[gt], writes=[ssn])
                        k.ts("dve", V(ssn), V(ssn), 1.0 / 256, ALU.mult, EPS, ALU.add)
                        k.act(V(ssn), V(ssn), AF.Sqrt)
                        k.recip(V(ssn), V(ssn))
                        k.tt("dve", V(oacc), V(oacc), (ssn, ssn.a().rearrange("p (c o) -> p c o", o=1).to_broadcast([64, NCH, 256])), ALU.mult)
                        k.tt("pool", V(oacc), V(oacc), (nwB, nwB[:, h * 256:(h + 1) * 256].rearrange("p (o d) -> p o d", o=1).to_broadcast([64, NCH, 256])), ALU.mult)
                        k.dma(V(gt), (PT, PT[:, 2080 + h * 256:2080 + (h + 1) * 256].rearrange("(c p) d -> p c d", p=CH)))
                        for q4 in range(4):
                            k.act((gt, gt[:, q4 * 17:(q4 + 1) * 17, :], q4), (gt, gt[:, q4 * 17:(q4 + 1) * 17, :], q4), AF.Sigmoid)
                        k.tt("dve", V(oacc), V(oacc), V(gt), ALU.mult)
                        k.dma((YM, YM[:, 1024 + h * 256:1024 + (h + 1) * 256].rearrange("(c p) d -> p c d", p=CH), ("m", h)), V(oacc))
                    P.barrier()

        def stage_xbt(PC, XBT):
            with ExitStack() as ph:
                ft = [P.sb(f"xft{i}", [128, 10, 128], F32, es=ph) for i in range(2)]
                ot = [P.sb(f"xot{i}", [128, 10, 128], F32, es=ph) for i in range(2)]
                pt = [P.ps(f"xpt{i}", [128, 4, 128], F32, es=ph) for i in range(3)]
                for tt in range(NT):
                    f_ = ft[tt % 2]; o_ = ot[tt % 2]
                    k.dma(V(f_), (PC, PC[0:1280, tt * 128:(tt + 1) * 128].rearrange("(b p) t -> p b t", p=128)))
                    for gi, (b0, nb_) in enumerate(((0, 4), (4, 4), (8, 2))):
                        p_ = pt[gi]
                        for j in range(nb_):
                            k.tr((p_, p_[:, j, :]), (f_, f_[:, b0 + j, :]), V(ident))
                        k.cp("act" if gi % 2 else "dve", (o_, o_[:, b0:b0 + nb_, :]), (p_, p_[:, 0:nb_, :]))
                    k.dma((XBT, XBT[tt * 128:(tt + 1) * 128, :], tt), (o_, o_.a().rearrange("p b c -> p (b c)")))
                P.barrier()

        def mixer_ssd(PC, PT, XBT, YS):
            with ExitStack() as ph:
                masks = make_masks(ph)
                ones64 = P.sb("sones64", [64, 64], F32, es=ph)
                k.memset("pool", V(ones64), 1.0)
                selend = []
                for z in range(2):
                    se = P.sb(f"selend{z}", [64, 128], F32, es=ph)
                    endp = CH - 1 if z == 0 else 0
                    P.op("pool", lambda e, se=se, endp=endp: e.affine_select(out=se.a(), in_=ones[0:64, :], pattern=[[0, 128]], compare_op=ALU.is_equal,
                                                                           fill=0.0, base=-endp, channel_multiplier=1), reads=[ones], writes=[se])
                    selend.append(se)
                dtT = P.sb("dtT", [64, NCH, 32], F32, es=ph)
                laT = P.sb("laT", [64, NCH, 32], F32, es=ph)
                dbB = P.sb("dbB", [64, 32], F32, es=ph)
                naB = P.sb("naB", [64, 32], F32, es=ph)
                dskB = P.sb("dskB", [64, 16], F32, es=ph)
                k.dma(V(dbB), V(I["ssd_dt_bias"], I["ssd_dt_bias"][0].rearrange("z h -> (z h)").partition_broadcast(64)))
                k.dma(V(naB), V(I["ssd_a_log"], I["ssd_a_log"][0].rearrange("z h -> (z h)").partition_broadcast(64)))
                k.dma(V(dskB), V(I["ssd_d"], I["ssd_d"][0].partition_broadcast(64)))
                k.act(V(naB), V(naB), AF.Exp)
                k.ts("dve", V(naB), V(naB), -1.0, ALU.mult)
                k.dma(V(dtT), (PT, PT[:, 1024:1056].rearrange("(c p) d -> p c d", p=CH)))
                k.tt("dve", V(dtT), V(dtT), (dbB, dbB.a().rearrange("p (o d) -> p o d", o=1).to_broadcast([64, NCH, 32])), ALU.add)
                k.act(V(dtT), V(dtT), AF.Exp)
                k.act(V(dtT), V(dtT), AF.Ln, bias=1.0)
                k.tt("dve", V(laT), V(dtT), (naB, naB.a().rearrange("p (o d) -> p o d", o=1).to_broadcast([64, NCH, 32])), ALU.mult)
                yacc = P.sb("yacc", [64, NCH, 256], F32, es=ph)
                for q4 in range(4):
                    g = q4 // 2
                    with ExitStack() as qs:
                        xq = P.sb("xq", [64, NCH, 256], BF16, es=qs)
                        Bf = P.sb("Bf", [128, L], BF16, es=qs)
                        Cf = P.sb("Cf", [128, L], BF16, es=qs)
                        BTt = P.sb("BTt", [64, NCH, 128], BF16, es=qs)
                        k.dma(V(xq), (XBT, XBT[:, q4 * 256:(q4 + 1) * 256].rearrange("(c p) d -> p c d", p=CH)), eng="pool")
                        k.dma(V(BTt), (XBT, XBT[:, 1024 + g * 128:1024 + (g + 1) * 128].rearrange("(c p) d -> p c d", p=CH)), eng="pool")
                        k.dma(V(Bf), (PC, PC[1024 + g * 128:1024 + (g + 1) * 128, :]), eng="pool")
                        k.dma(V(Cf), (PC, PC[1280 + g * 128:1280 + (g + 1) * 128, :]), eng="pool")
                        k.memset("pool", V(yacc), 0.0)
                        hs = [P.sb(f"hs{z}", [128, 256], F32, es=qs) for z in range(2)]
                        hsb = [P.sb(f"hsb{z}", [128, 256], BF16, es=qs) for z in range(2)]
                        G = 2
                        NJ = 2 * G
                        J = []
                        for js in range(NJ):
                            d_ = {}
                            d_["CBm"] = P.sb(f"CBm{js}", [64, 64], F32, es=qs)
                            d_["acs"] = P.sb(f"acs{js}", [64, 4], F32, es=qs)
                            d_["dg"] = P.sb(f"dg{js}", [64, 4, 64], F32, es=qs)
                            d_["seg"] = P.sb(f"seg{js}", [64, 4, 64], F32, es=qs)
                            d_["AT"] = P.sb(f"AT{js}", [64, 4, 64], BF16, es=qs)
                            d_["xdt"] = P.sb(f"xdt{js}", [64, 4, 64], BF16, es=qs)
                            d_["xde"] = P.sb(f"xde{js}", [64, 4, 64], BF16, es=qs)
                            d_["dend"] = P.sb(f"dend{js}", [64, 4], F32, es=qs)
                            d_["din"] = P.sb(f"din{js}", [64, 4], F32, es=qs)
                            d_["dchB"] = P.sb(f"dchB{js}", [128, 4], F32, es=qs)
                            d_["phsS"] = P.sb(f"phsS{js}", [128, 256], F32, es=qs)
                            J.append(d_)
                        ytmp = [P.sb(f"ytmp{z}", [64, 4, 64], F32, es=qs) for z in range(2)]
                        bankA = [P.ps(f"sbank{i}", [128, 512], F32, es=qs) for i in range(2)]
                        pyi = [P.ps(f"pyi{z}", [64, 4, 64], F32, es=qs) for z in range(2)]
                        for z in range(2):
                            k.memset("pool", V(hs[z]), 0.0)
                            k.memset("pool", V(hsb[z]), 0.0)
                        orders = [chunk_order(0), chunk_order(1)]
                        b4 = lambda t: t.a().rearrange("p (h o) -> p h o", o=1).to_broadcast([64, 4, 64])

                        def ssd_A(step, z, st):
                            js = (step % G) * 2 + z
                            d_ = J[js]
                            bk_ = bankA[js % 2]
                            c = orders[z][step]
                            csl = slice(c * CH, (c + 1) * CH)
                            end = CH - 1 if z == 0 else 0
                            hsl = slice(z * 16 + q4 * 4, z * 16 + q4 * 4 + 4)
                            acs = d_["acs"]
                            if st == 0:
                                k.mm((bk_, bk_[0:64, 0:64]), (Bf, Bf[:, csl]), (Cf, Cf[:, csl]))
                                k.mm((bk_, bk_[0:64, 64:68]), V(masks[z]), (laT, laT[:, c, hsl]))
                                k.tt("dve", V(d_["CBm"]), (bk_, bk_[0:64, 0:64]), V(masks[z]), ALU.mult)
                                k.cp("act", V(acs), (bk_, bk_[0:64, 64:68]))
                                k.tt("dve", V(d_["xdt"]), (xq, xq[:, c, :].rearrange("p (h d) -> p h d", d=64)),
                                     (dtT, dtT[:, c, hsl].rearrange("p (h o) -> p h o", o=1).to_broadcast([64, 4, 64])), ALU.mult)
                            elif st == 1:
                                k.tt("dve", V(d_["dg"]), (ident, ident[0:64, 0:64].rearrange("p (o t) -> p o t", o=1).to_broadcast([64, 4, 64])),
                                     (acs, b4(acs)), ALU.mult)
                                k.act(V(d_["din"]), V(acs), AF.Exp)
                            elif st == 2:
                                pbc = bk_[0:64, 128:384].rearrange("p (h t) -> p h t", t=64)
                                k.mm((bk_, bk_[0:64, 128:384]), V(ones64), (d_["dg"], d_["dg"].a().rearrange("p h t -> p (h t)")))
                                k.tt("dve", V(d_["seg"]), (bk_, pbc), (acs, b4(acs)), ALU.subtract)
                                k.tt("dve", V(d_["dend"]), (bk_, pbc[:, :, end]), V(acs), ALU.subtract)
                                k.mm((bk_, bk_[:, 384:388]), V(selend[z]), V(acs))
                                k.act(V(d_["dchB"]), (bk_, bk_[:, 384:388]), AF.Exp)
                            elif st == 3:
                                k.act(V(d_["seg"]), V(d_["seg"]), AF.Exp)
                                k.act(V(d_["dend"]), V(d_["dend"]), AF.Exp)
                                k.stt("dve", V(d_["AT"]), V(d_["seg"]), 1.0,
                                      (d_["CBm"], d_["CBm"].a().rearrange("p (o t) -> p o t", o=1).to_broadcast([64, 4, 64])), ALU.min, ALU.mult)
                                k.tt("dve", V(d_["xde"]), V(d_["xdt"]), (d_["dend"], b4(d_["dend"])), ALU.mult)
                            elif st == 4:
                                py = bk_[0:64, 128:384].rearrange("p (h t) -> p h t", t=64)
                                for i in range(4):
                                    k.mm((bk_, py[:, i, :]), (d_["AT"], d_["AT"][:, i, :]), (d_["xdt"], d_["xdt"][:, i, :]))
                                ya = (yacc, yacc[:, c, :].rearrange("p (h d) -> p h d", d=64), c)
                                k.tt("dve", ya, (bk_, py), ya, ALU.add)
                            elif st == 5:
                                k.mm((bk_, bk_[:, 0:256]), (BTt, BTt[:, c, :]), (d_["xde"], d_["xde"].a().rearrange("p h d -> p (h d)")))
                                k.cp("act", V(d_["phsS"]), (bk_, bk_[:, 0:256]))

                        def ssd_B(step, z, st):
                            js = (step % G) * 2 + z
                            d_ = J[js]
                            c = orders[z][step]
                            csl = slice(c * CH, (c + 1) * CH)
                            ya = (yacc, yacc[:, c, :].rearrange("p (h d) -> p h d", d=64), c)
                            if st == 0:
                                k.mm(V(pyi[z]), (Cf, Cf[:, csl]), V(hsb[z]))
                                h3 = (hs[z], hs[z].a().rearrange("p (h d) -> p h d", d=64))
                                k.tt("dve", h3, h3, (d_["dchB"], d_["dchB"].a().rearrange("p (h o) -> p h o", o=1).to_broadcast([128, 4, 64])), ALU.mult)
                                k.tt("dve", V(hs[z]), V(d_["phsS"]), V(hs[z]), ALU.add)
                                k.cp("act", V(hsb[z]), V(hs[z]))
                            else:
                                k.tt("dve", V(ytmp[z]), V(pyi[z]), (d_["din"], b4(d_["din"])), ALU.mult)
                                k.tt("dve", ya, ya, V(ytmp[z]), ALU.add)

                        for g0 in range(0, NCH, G):
                            steps = list(range(g0, min(NCH, g0 + G)))
                            for st in range(6):
                                for step in steps:
                                    for z in range(2):
                                        ssd_A(step, z, st)
                            for step in steps:
                                for st in range(2):
                                    for z in range(2):
                                        ssd_B(step, z, st)
                    P.barrier()
                    with ExitStack() as fs:
                        xf = P.sb("sxf", [64, 17, 256], F32, es=fs)
                        zf = P.sb("szf", [64, 17, 256], F32, es=fs)
                        for c17 in range(4):
                            cs_ = slice(c17 * 17, (c17 + 1) * 17)
                            rows = slice(c17 * 17 * CH, (c17 + 1) * 17 * CH)
                            k.dma(V(xf), (XBT, XBT[rows, q4 * 256:(q4 + 1) * 256].rearrange("(c p) d -> p c d", p=CH)))
                            k.dma(V(zf), (PT, PT[rows, q4 * 256:(q4 + 1) * 256].rearrange("(c p) d -> p c d", p=CH)))
                            dsk4 = dskB[:, q4 * 4:(q4 + 1) * 4].rearrange("p (a h o) -> p a h o", a=1, o=1).to_broadcast([64, 17, 4, 64])
                            k.tt("pool", (xf, xf.a().rearrange("p c (h d) -> p c h d", d=64)), (xf, xf.a().rearrange("p c (h d) -> p c h d", d=64)),
                                 (dskB, dsk4), ALU.mult)
                            k.tt("dve", V(xf), V(xf), (yacc, yacc[:, cs_, :], ("f", c17)), ALU.add)
                            k.act(V(zf), V(zf), AF.Silu)
                            k.tt("dve", V(xf), V(xf), V(zf), ALU.mult)
                            k.dma((YS, YS[rows, q4 * 256:(q4 + 1) * 256].rearrange("(c p) d -> p c d", p=CH), (q4, c17)), V(xf))
                    P.barrier()

        def stage_ssd_norm(YS, YM):
            with ExitStack() as ph:
                nwB = P.sb("snwB", [128, 1024], F32, es=ph)
                k.dma(V(nwB), V(I["ssd_norm_w"], I["ssd_norm_w"][0].partition_broadcast(128)))
                yt = [P.sb(f"syt{i}", [128, 1024], F32, es=ph) for i in range(2)]
                sq = P.sb("ssq", [128, 1024], F32, es=ph)
                st = [P.sb(f"sst{i}", [128, 2], F32, es=ph) for i in range(2)]
                for tt in range(NT):
                    y_ = yt[tt % 2]; s_ = st[tt % 2]
                    k.dma(V(y_), (YS, YS[tt * 128:(tt + 1) * 128, :]))
                    k.tt("pool", V(sq), V(y_), V(y_), ALU.mult)
                    P.op("dve", lambda e, s_=s_: e.tensor_reduce(out=s_.a(), in_=sq.a().rearrange("p (g d) -> p g d", g=2), axis=AX.X, op=ALU.add),
                         reads=[sq], writes=[s_])
                    k.ts("dve", V(s_), V(s_), 1.0 / 512, ALU.mult, EPS, ALU.add)
                    k.act(V(s_), V(s_), AF.Sqrt)
                    k.recip(V(s_), V(s_))
                    k.tt("dve", (y_, y_.a().rearrange("p (g d) -> p g d", g=2)), (y_, y_.a().rearrange("p (g d) -> p g d", g=2)),
                         (s_, s_.a().rearrange("p (g o) -> p g o", o=1).to_broadcast([128, 2, 512])), ALU.mult)
                    k.tt("pool", V(y_), V(y_), V(nwB), ALU.mult)
                    k.dma((YM, YM[tt * 128:(tt + 1) * 128, 0:1024], ("s", tt)), V(y_))
                P.barrier()

        def stage_final():
            with ExitStack() as ph:
                fnB = P.sb("fnB", [128, D], F32, es=ph)
                k.dma(V(fnB), V(I["final_norm_w"], I["final_norm_w"].a().partition_broadcast(128)))
                xt = [P.sb(f"fxt{i}", [128, D], F32, es=ph) for i in range(2)]
                junk = P.sb("fjunk", [128, D], F32, es=ph)
                st = [P.sb(f"fst{i}", [128, 4], F32, es=ph) for i in range(2)]
                for i in range(SEQ // 128):
                    x_ = xt[i % 2]; s_ = st[i % 2]
                    k.dma(V(x_), (XL, XL[LC + i * 128:LC + (i + 1) * 128, :], i))
                    k.memset("pool", (s_, s_[:, 0:1]), 0.0)
                    k.act(V(junk), V(x_), AF.Square, accum=(s_, s_[:, 0:1]))
                    k.ts("dve", (s_, s_[:, 1:2]), (s_, s_[:, 0:1]), 1.0 / D, ALU.mult, EPS, ALU.add)
                    k.act((s_, s_[:, 2:3]), (s_, s_[:, 1:2]), AF.Sqrt)
                    k.recip((s_, s_[:, 3:4]), (s_, s_[:, 2:3]))
                    k.ts("dve", V(x_), V(x_), (s_, s_[:, 3:4]), ALU.mult)
                    k.tt("pool", V(x_), V(x_), V(fnB), ALU.mult)
                    k.dma((OUT, OUT[i * 128:(i + 1) * 128, :], i), V(x_))
                P.barrier()

        def src0(i):
            if i < 2:
                return (I["ctx"], I["ctx"][i * 128:(i + 1) * 128, :])
            return (I["x"], I["x"][(i - 2) * 128:(i - 1) * 128, :])

        def src1(i):
            return (XL, XL[i * 128:(i + 1) * 128, :])

        def layer0():
            stage_mods(0)
            if "modF" in debug:
                dm = P.dram("d_modF", [128, 6 * KO * 2], F32, kind="ExternalOutput")
                k.dma(V(dm), (modF, modF.a().rearrange("p a b c -> p (a b c)")))
                dm2 = P.dram("d_modB", [128, 4 * D], F32, kind="ExternalOutput")
                k.dma(V(dm2), (modB, modB.a().rearrange("p a b c -> p (a b c)")))
            PF0 = scratch("PF0", [3456, L])
            PT0 = scratch("PT0", [L, 1024])
            YM0 = scratch("YM0", [L, 1024])
            with ExitStack() as lay:
                HT = P.sb("HT", [128, KO, L], BF16, es=lay)
                stage_norm(0, 1, HT, src0, perm=False)
                if "HT0" in debug:
                    dh = P.dram("d_HT0", [128, KO * L], BF16, kind="ExternalOutput")
                    k.dma(V(dh), (HT, HT.a().rearrange("p a b -> p (a b)")))
                if stop_after == "norm0":
                    return
                stage_proj(HT, (I["even_w_in"], I["even_w_in"][0]), 4480, [(0, 3456, PF0, 0)], [(3456, 4480, PT0, 0)])
            if stop_after == "proj0":
                return
            if "skip_hgrn" not in debug:
                mixer_hgrn(PF0, PT0, YM0)
            PR0 = scratch("PR0", [1920, L])
            GT0 = scratch("GT0", [L, 512])
            rwkv_shift(PF0, PR0)
            if "no_gate" not in debug:
                rwkv_gate(PR0, GT0)
            if stop_after == "shift0":
                return
            if "skip_rwkv" not in debug:
                if "rwkv_old" in debug:
                    mixer_rwkv(PR0, GT0, YM0)
                else:
                    mixer_rwkv2(PR0, GT0, YM0)
            if stop_after == "mix0":
                return
            stage_outproj(YM0, 1024, (I["even_w_out"], I["even_w_out"][0]), src0, False, list(range(NT)))
            if stop_after == "out0":
                return
            stage_moe(0, list(range(NT)))

        def layer1():
            stage_mods(1)
            PF1 = scratch("PF1", [2576, L])
            PT1 = scratch("PT1", [L, 3104])
            PC1 = scratch("PC1", [2560, L])
            YM1 = scratch("YM1", [L, 2048])
            GSD = scratch("GSD", [16, L])
            YS = scratch("YS", [L, 1024])
            with ExitStack() as lay:
                HT = P.sb("HT", [128, KO, L], BF16, es=lay)
                stage_norm(1, 1, HT, src1, perm=True)
                stage_proj(HT, (I["odd_w_in"], I["odd_w_in"][0]), 5680,
                           [(1024, 2560, PF1, 0), (2592, 3616, PF1, 1536), (5664, 5680, PF1, 2560)],
                           [(0, 1024, PT1, 0), (2560, 2592, PT1, 1024), (3616, 5664, PT1, 1056)])
            stage_conv(PF1, PC1, [(0, 12, (I["ssd_conv_w"], I["ssd_conv_w"][0]), (I["ssd_conv_b"], I["ssd_conv_b"][0])),
                                  (1536, 8, (I["mlstm_conv_w"], I["mlstm_conv_w"][0]), (I["mlstm_conv_b"], I["mlstm_conv_b"][0]))])
            if stop_after == "L1conv":
                return
            if "skip_mlstm" not in debug:
                mixer_mlstm(PC1, PF1, PT1, YM1, GSD)
            if stop_after == "L1mlstm":
                return
            XBT = scratch("XBT", [L, 1280])
            stage_xbt(PC1, XBT)
            mixer_ssd(PC1, PT1, XBT, YS)
            stage_ssd_norm(YS, YM1)
            if stop_after == "L1ssd":
                return
            stage_outproj(YM1, 2048, (I["odd_w_out"], I["odd_w_out"][0]), src1, True, list(range(2, NT)))
            if stop_after == "L1out":
                return
            stage_moe(1, list(range(2, NT)))
            stage_final()

        if "L1only" in debug:
            XLin = P.dram("XLin", [L, D], F32, kind="ExternalInput")
            for i4 in range(4):
                k.dma((XL, XL[i4 * 1088:(i4 + 1) * 1088, :], ("in", i4)), (XLin, XLin[i4 * 1088:(i4 + 1) * 1088, :]))
            P.barrier()
        else:
            layer0()
        if stop_after is None or stop_after.startswith("L1"):
            layer1()
        P.finish()
    return nc


_NC = None


def kernel(**inputs):
    global _NC
    if _NC is None:
        _NC = build()
    nc = _NC
    n = 8
    in_maps = []
    for b in range(n):
        m = {}
        for kk_, v in inputs.items():
            v = np.asarray(v)
            if kk_ == "x":
                m[kk_] = np.ascontiguousarray(v[b])
            elif kk_ == "c":
                m[kk_] = np.ascontiguousarray(v[b])
            elif kk_ == "ctx":
                m[kk_] = np.ascontiguousarray(v[b])
            else:
                m[kk_] = v
        in_maps.append(m)
    res = run_bass_kernel_spmd(nc, in_maps, core_ids=list(range(n)))
    return np.stack([r["out"] for r in res.results], axis=0)
```
